# Optimizing a Trainium2 kernel written in Bass

```python
import math
import jax, jax.numpy as jnp
from jax import lax
import numpy as np

D_MODEL = 1024
BATCH = 8
SEQ = 4096
DEPTH = 2

GRID_W = 64
CTX_LEN = 256
RET_HEADS = 4
RET_DK = 64
RET_DV = 64
RET_CHUNK = 128
MLA_HEADS = 8
MLA_Q_RANK = 256
MLA_KV_RANK = 128
MLA_NOPE = 64
MLA_ROPE = 32
MLA_DV = 64
MLA_QBLOCK = 128
WIN_Q_HEADS = 4
WIN_KV_HEADS = 2
WIN_DH = 64
WINDOW = 128
WIN_BLOCK = 128
N_EXPERTS = 16
EXPERT_FF = 768
CAPACITY_FACTOR = 2

ROPE_BASE = 10000.0
NORM_EPS = 1e-6
GN_EPS = 1e-5
NEG_INF = -1e30

IN_SPLITS = (RET_HEADS * RET_DK, RET_HEADS * RET_DK, RET_HEADS * RET_DV, RET_HEADS * RET_DV,
             MLA_Q_RANK, MLA_KV_RANK, MLA_ROPE,
             WIN_Q_HEADS * WIN_DH, WIN_KV_HEADS * WIN_DH, WIN_KV_HEADS * WIN_DH)
IN_COLS = sum(IN_SPLITS)

kernel_name = 'hybrid_ret_mla_swa_ec_diffusion'


def rmsnorm(x, g):
    xf = x.astype(jnp.float32)
    y = xf * lax.rsqrt(jnp.mean(xf * xf, axis=-1, keepdims=True) + NORM_EPS)
    return (y * g.astype(jnp.float32)).astype(x.dtype)


def modulate(h, shift, scale):
    return h * (1 + scale) + shift


def split_cols(p):
    offs = [int(o) for o in np.cumsum(IN_SPLITS)[:-1]]
    return jnp.split(p, offs, axis=-1)


def rope_1d(x, pos):
    dh = x.shape[-1]
    inv = ROPE_BASE ** (-jnp.arange(0, dh, 2, dtype=jnp.float32) / dh)
    ang = pos.astype(jnp.float32)[:, None] * inv[None, :]
    cos = jnp.cos(ang)[:, None, :].astype(x.dtype)
    sin = jnp.sin(ang)[:, None, :].astype(x.dtype)
    x1, x2 = jnp.split(x, 2, axis=-1)
    return jnp.concatenate([x1 * cos - x2 * sin, x1 * sin + x2 * cos], axis=-1)


def axial_rope(x, row, col):
    xr, xc = jnp.split(x, 2, axis=-1)
    return jnp.concatenate([rope_1d(xr, row), rope_1d(xc, col)], axis=-1)


def retention_scan(q, k, v, log_g, s0, inclusive):
    b, h, l, dk = q.shape
    dv = v.shape[-1]
    C = RET_CHUNK
    n = l // C
    qc = q.reshape(b, h, n, C, dk)
    kc = k.reshape(b, h, n, C, dk)
    vc = v.reshape(b, h, n, C, dv)
    idx = jnp.arange(C, dtype=jnp.float32)
    diff = idx[:, None] - idx[None, :]
    keep = (diff >= 0) if inclusive else (diff > 0)
    lg = log_g.astype(jnp.float32)
    dmat = jnp.where(keep, jnp.exp(lg[:, None, None] * jnp.where(keep, diff, 0.0)), 0.0).astype(q.dtype)
    scores = jnp.einsum('bhncd,bhnmd->bhncm', qc, kc) * dmat[:, None]
    y = jnp.einsum('bhncm,bhnme->bhnce', scores, vc)
    k_w = jnp.exp(lg[:, None] * (C - 1 - idx)[None, :]).astype(q.dtype)
    q_w = jnp.exp(lg[:, None] * (idx + 1)[None, :]).astype(q.dtype)
    chunk_kv = jnp.einsum('bhncd,bhnce->nbhde', kc * k_w[:, None, :, None], vc)
    g_chunk = jnp.exp(lg * C).astype(q.dtype)[:, None, None]

    def step(s, kv_n):
        return g_chunk * s + kv_n, s

    s_final, s_prev = lax.scan(step, s0.astype(q.dtype), chunk_kv)
    y = y + jnp.einsum('bhncd,nbhde->bhnce', qc * q_w[:, None, :, None], s_prev)
    return y.reshape(b, h, l, dv), s_final


def head_groupnorm(y):
    yf = y.astype(jnp.float32)
    mu = jnp.mean(yf, axis=-1, keepdims=True)
    var = jnp.mean(jnp.square(yf - mu), axis=-1, keepdims=True)
    return ((yf - mu) * lax.rsqrt(var + GN_EPS)).astype(y.dtype)


def retention_group(q_x, k_x, v_x, g_x, q_c, k_c, v_c, g_c, decay_f, decay_b, need_ctx):
    def heads(t):
        bb, ll, _ = t.shape
        return t.reshape(bb, ll, RET_HEADS, -1).transpose(0, 2, 1, 3)

    def flip(t):
        return t[:, :, ::-1]

    lg_f = jnp.log1p(-jnp.exp2(decay_f.astype(jnp.float32)))
    lg_b = jnp.log1p(-jnp.exp2(decay_b.astype(jnp.float32)))
    scale = RET_DK ** -0.5
    qx, kx, vx = heads(q_x), heads(k_x) * scale, heads(v_x)
    qc, kc, vc = heads(q_c), heads(k_c) * scale, heads(v_c)
    zero = jnp.zeros((qx.shape[0], RET_HEADS, RET_DK, RET_DV), qx.dtype)
    yc_f, s_f = retention_scan(qc, kc, vc, lg_f, zero, True)
    yc_b, s_b = retention_scan(flip(qc), flip(kc), flip(vc), lg_b, zero, False)
    yx_f, _ = retention_scan(qx, kx, vx, lg_f, s_f, True)
    yx_b, _ = retention_scan(flip(qx), flip(kx), flip(vx), lg_b, s_b, False)

    def finish(y, g):
        bb, hh, ll, dd = y.shape
        y = head_groupnorm(y).transpose(0, 2, 1, 3).reshape(bb, ll, hh * dd)
        return y * jax.nn.silu(g)

    out_x = finish(yx_f + flip(yx_b), g_x)
    out_c = finish(yc_f + flip(yc_b), g_c) if need_ctx else None
    return out_x, out_c


def mla_group(cq_x, ckv_x, kr_x, cq_c, ckv_c, kr_c, row, col, qnorm_g, kvnorm_g, w_uq, w_uk, w_uv, need_ctx):
    scale = (MLA_NOPE + MLA_ROPE) ** -0.5

    def queries(cq):
        q = jnp.einsum('blr,rhe->blhe', rmsnorm(cq, qnorm_g), w_uq)
        return q[..., :MLA_NOPE], q[..., MLA_NOPE:]

    def attend(ql, qr, kv, kr):
        s = (jnp.einsum('bhqr,bkr->bhqk', ql, kv) + jnp.einsum('bhqp,bkp->bhqk', qr, kr)).astype(jnp.float32) * scale
        p = jax.nn.softmax(s, axis=-1).astype(kv.dtype)
        return jnp.einsum('bhqk,bkr->bhqr', p, kv)

    def up(ol):
        o = jnp.einsum('bhlr,rhd->blhd', ol, w_uv)
        return o.reshape(o.shape[0], o.shape[1], -1)

    kv_c = rmsnorm(ckv_c, kvnorm_g)
    kv_x = rmsnorm(ckv_x, kvnorm_g)
    kr_xr = axial_rope(kr_x[:, :, None, :], row, col)[:, :, 0, :]
    kv_all = jnp.concatenate([kv_c, kv_x], axis=1)
    kr_all = jnp.concatenate([kr_c, kr_xr], axis=1)
    qn_x, qr_x = queries(cq_x)
    qr_x = axial_rope(qr_x, row, col).transpose(0, 2, 1, 3)
    ql_x = jnp.einsum('blhd,rhd->bhlr', qn_x, w_uk)
    b, h, l, r = ql_x.shape
    nb = l // MLA_QBLOCK

    def blocks(t):
        return t.reshape(b, h, nb, MLA_QBLOCK, t.shape[-1]).transpose(2, 0, 1, 3, 4)

    ol = lax.map(lambda qs: attend(qs[0], qs[1], kv_all, kr_all), (blocks(ql_x), blocks(qr_x)))
    ol = ol.transpose(1, 2, 0, 3, 4).reshape(b, h, l, r)
    out_x = up(ol)
    out_c = None
    if need_ctx:
        qn_c, qr_c = queries(cq_c)
        ql_c = jnp.einsum('blhd,rhd->bhlr', qn_c, w_uk)
        out_c = up(attend(ql_c, qr_c.transpose(0, 2, 1, 3), kv_c, kr_c))
    return out_x, out_c


def window_group(q_x, k_x, v_x, q_c, k_c, v_c, row, col, sink, need_ctx):
    b, l, _ = q_x.shape
    lc = q_c.shape[1]
    hk, g, d = WIN_KV_HEADS, WIN_Q_HEADS // WIN_KV_HEADS, WIN_DH
    W = WIN_BLOCK
    nb = l // W
    scale = d ** -0.5
    qx = axial_rope(q_x.reshape(b, l, WIN_Q_HEADS, d), row, col).reshape(b, l, hk, g, d)
    kx = axial_rope(k_x.reshape(b, l, hk, d), row, col)
    vx = v_x.reshape(b, l, hk, d)
    kc = k_c.reshape(b, lc, hk, d)
    vc = v_c.reshape(b, lc, hk, d)
    sink_l = sink.astype(jnp.float32).reshape(hk, g, 1, 1)

    def band(t):
        tp = jnp.pad(t, ((0, 0), (W, W), (0, 0), (0, 0))).reshape(b, nb + 2, W, hk, d)
        return jnp.concatenate([tp[:, :-2], tp[:, 1:-1], tp[:, 2:]], axis=2)

    kb, vb = band(kx), band(vx)
    qi = jnp.arange(nb)[:, None, None] * W + jnp.arange(W)[None, :, None]
    kj = (jnp.arange(nb)[:, None, None] - 1) * W + jnp.arange(3 * W)[None, None, :]
    mask = (jnp.abs(kj - qi) <= WINDOW) & (kj >= 0) & (kj < l)
    qb = qx.reshape(b, nb, W, hk, g, d)
    s_loc = jnp.einsum('bnqhgd,bnkhd->bnhgqk', qb, kb).astype(jnp.float32) * scale
    s_loc = jnp.where(mask[None, :, None, None], s_loc, NEG_INF)
    s_ctx = jnp.einsum('bnqhgd,bkhd->bnhgqk', qb, kc).astype(jnp.float32) * scale
    s_sink = jnp.broadcast_to(sink_l, s_loc.shape[:-1] + (1,))
    p = jax.nn.softmax(jnp.concatenate([s_loc, s_ctx, s_sink], axis=-1), axis=-1).astype(vx.dtype)
    o = (jnp.einsum('bnhgqk,bnkhd->bnqhgd', p[..., :3 * W], vb)
         + jnp.einsum('bnhgqk,bkhd->bnqhgd', p[..., 3 * W:3 * W + lc], vc))
    out_x = o.reshape(b, l, WIN_Q_HEADS * d)
    out_c = None
    if need_ctx:
        s = jnp.einsum('bqhgd,bkhd->bhgqk', q_c.reshape(b, lc, hk, g, d), kc).astype(jnp.float32) * scale
        s = jnp.concatenate([s, jnp.broadcast_to(sink_l, s.shape[:-1] + (1,))], axis=-1)
        pc = jax.nn.softmax(s, axis=-1).astype(vc.dtype)
        out_c = jnp.einsum('bhgqk,bkhd->bqhgd', pc[..., :lc], vc).reshape(b, lc, WIN_Q_HEADS * d)
    return out_x, out_c


def hybrid_mixer(px, pc, row, col, ret_decay_f, ret_decay_b, mla_qnorm_g, mla_kvnorm_g,
                 mla_w_uq, mla_w_uk, mla_w_uv, win_sink, need_ctx):
    (rq_x, rk_x, rv_x, rg_x, cq_x, ckv_x, kr_x, wq_x, wk_x, wv_x) = split_cols(px)
    (rq_c, rk_c, rv_c, rg_c, cq_c, ckv_c, kr_c, wq_c, wk_c, wv_c) = split_cols(pc)
    ret_x, ret_c = retention_group(rq_x, rk_x, rv_x, rg_x, rq_c, rk_c, rv_c, rg_c,
                                   ret_decay_f, ret_decay_b, need_ctx)
    mla_x, mla_c = mla_group(cq_x, ckv_x, kr_x, cq_c, ckv_c, kr_c, row, col, mla_qnorm_g, mla_kvnorm_g,
                             mla_w_uq, mla_w_uk, mla_w_uv, need_ctx)
    win_x, win_c = window_group(wq_x, wk_x, wv_x, wq_c, wk_c, wv_c, row, col, win_sink, need_ctx)
    y_x = jnp.concatenate([ret_x, mla_x, win_x], axis=-1)
    y_c = jnp.concatenate([ret_c, mla_c, win_c], axis=-1) if need_ctx else None
    return y_x, y_c


def expert_choice_ffn(h, router_w, w_gate, w_up, w_down):
    b, l, d = h.shape
    cap = CAPACITY_FACTOR * l // N_EXPERTS
    aff = jax.nn.softmax(jnp.einsum('bld,de->ble', h, router_w).astype(jnp.float32), axis=-1)
    gate, idx = lax.top_k(aff.transpose(0, 2, 1), cap)
    xs = jax.vmap(lambda hb, ib: hb[ib])(h, idx)
    hid = jax.nn.silu(jnp.einsum('becd,edf->becf', xs, w_gate)) * jnp.einsum('becd,edf->becf', xs, w_up)
    ys = jnp.einsum('becf,efd->becd', hid, w_down) * gate[..., None].astype(h.dtype)
    return jax.vmap(lambda yb, ib: jnp.zeros((l, d), h.dtype).at[ib.reshape(-1)].add(yb.reshape(-1, d)))(ys, idx)


def setup_inputs(seed: int = 0) -> dict:
    key = jax.random.key(seed)
    ks = iter(jax.random.split(key, 32))

    def nrm(shape, scale):
        return jax.random.normal(next(ks), shape, jnp.float32) * scale

    L, D = DEPTH, D_MODEL
    sched = -5.0 - jnp.arange(RET_HEADS, dtype=jnp.float32)
    return {
        'x': nrm((BATCH, SEQ, D), 1.0),
        'c': nrm((BATCH, D), 1.0),
        'ctx': nrm((BATCH, CTX_LEN, D), 1.0),
        'c_ctx': nrm((D,), 1.0),
        'norm1_g': 1.0 + nrm((L, D), 0.05),
        'norm2_g': 1.0 + nrm((L, D), 0.05),
        'ada_w': nrm((L, D, 6 * D), 0.5 * D ** -0.5),
        'ada_b': nrm((L, 6 * D), 0.02),
        'w_in': nrm((L, D, IN_COLS), D ** -0.5),
        'ret_decay_f': sched + nrm((L, RET_HEADS), 0.1),
        'ret_decay_b': sched + nrm((L, RET_HEADS), 0.1),
        'mla_qnorm_g': 1.0 + nrm((L, MLA_Q_RANK), 0.05),
        'mla_kvnorm_g': 1.0 + nrm((L, MLA_KV_RANK), 0.05),
        'mla_w_uq': nrm((L, MLA_Q_RANK, MLA_HEADS, MLA_NOPE + MLA_ROPE), MLA_Q_RANK ** -0.5),
        'mla_w_uk': nrm((L, MLA_KV_RANK, MLA_HEADS, MLA_NOPE), MLA_KV_RANK ** -0.5),
        'mla_w_uv': nrm((L, MLA_KV_RANK, MLA_HEADS, MLA_DV), MLA_KV_RANK ** -0.5),
        'win_sink': nrm((L, WIN_Q_HEADS), 0.5),
        'w_out': nrm((L, D, D), D ** -0.5),
        'router_w': nrm((L, D, N_EXPERTS), D ** -0.5),
        'exp_w_gate': nrm((L, N_EXPERTS, D, EXPERT_FF), D ** -0.5),
        'exp_w_up': nrm((L, N_EXPERTS, D, EXPERT_FF), D ** -0.5),
        'exp_w_down': nrm((L, N_EXPERTS, EXPERT_FF, D), EXPERT_FF ** -0.5),
        'final_g': 1.0 + nrm((D,), 0.05),
    }


def reference(x, c, ctx, c_ctx, norm1_g, norm2_g, ada_w, ada_b, w_in, ret_decay_f, ret_decay_b,
              mla_qnorm_g, mla_kvnorm_g, mla_w_uq, mla_w_uk, mla_w_uv, win_sink, w_out,
              router_w, exp_w_gate, exp_w_up, exp_w_down, final_g):
    n_tok = x.shape[1]
    rows = n_tok // GRID_W
    row = jnp.repeat(jnp.arange(rows, dtype=jnp.int32), GRID_W)
    col = jnp.tile(jnp.arange(GRID_W, dtype=jnp.int32), rows)
    for layer in range(DEPTH):
        need_ctx = layer < DEPTH - 1
        mod_x = [m[:, None, :] for m in jnp.split(jax.nn.silu(c) @ ada_w[layer] + ada_b[layer], 6, axis=-1)]
        mod_c = jnp.split(jax.nn.silu(c_ctx) @ ada_w[layer] + ada_b[layer], 6, axis=-1)
        hx = modulate(rmsnorm(x, norm1_g[layer]), mod_x[0], mod_x[1])
        hc = modulate(rmsnorm(ctx, norm1_g[layer]), mod_c[0], mod_c[1])
        yx, yc = hybrid_mixer(hx @ w_in[layer], hc @ w_in[layer], row, col,
                              ret_decay_f[layer], ret_decay_b[layer], mla_qnorm_g[layer], mla_kvnorm_g[layer],
                              mla_w_uq[layer], mla_w_uk[layer], mla_w_uv[layer], win_sink[layer], need_ctx)
        x = x + mod_x[2] * (yx @ w_out[layer])
        hx = modulate(rmsnorm(x, norm2_g[layer]), mod_x[3], mod_x[4])
        x = x + mod_x[5] * expert_choice_ffn(hx, router_w[layer], exp_w_gate[layer], exp_w_up[layer], exp_w_down[layer])
        if need_ctx:
            ctx = ctx + mod_c[2] * (yc @ w_out[layer])
            hc = modulate(rmsnorm(ctx, norm2_g[layer]), mod_c[3], mod_c[4])
            ctx = ctx + mod_c[5] * expert_choice_ffn(hc, router_w[layer], exp_w_gate[layer], exp_w_up[layer], exp_w_down[layer])
    return rmsnorm(x, final_g)
```

```python
import numpy as np
from contextlib import ExitStack
import concourse.bass as bass
import concourse.mybir as mybir
from concourse.bass_utils import run_bass_kernel_spmd

F32 = mybir.dt.float32
BF16 = mybir.dt.bfloat16
I32 = mybir.dt.int32
U32 = mybir.dt.uint32
AF = mybir.ActivationFunctionType
ALU = mybir.AluOpType
AX = mybir.AxisListType


class T:
    __slots__ = ("ap", "key")

    def __init__(self, ap, key):
        self.ap = ap
        self.key = key

    def __getitem__(self, idx):
        return T(self.ap[idx], self.key)

    def sub(self, suffix):
        return T(self.ap, (self.key, suffix))

    def sl(self, suffix, idx):
        return T(self.ap[idx], (self.key, suffix))


def _key(x):
    return x.key if isinstance(x, T) else x


class KB:
    CE = ("pe", "act", "dve", "pool")

    def __init__(self, nc, es, nds=14):
        self.nc = nc
        self.es = es
        self.E = {"pe": nc.tensor, "act": nc.scalar, "dve": nc.vector,
                  "pool": nc.gpsimd, "sp": nc.sync}
        self.csem = {e: es.enter_context(nc.semaphore("c_" + e)) for e in self.CE}
        self.cnt = {e: 0 for e in self.CE}
        self.NDS = nds
        self.dsem = [es.enter_context(nc.semaphore("d%d" % i)) for i in range(nds)]
        self.dcnt = [0] * nds
        self.dnext = 0
        self.waited = {e: {} for e in self.E}
        self.lastw = {}
        self.readers = {}
        self.nalloc = 0
        self.ninst = 0

    def sb(self, shape, dtype, name=None, es=None):
        self.nalloc += 1
        name = name or ("t%d" % self.nalloc)
        t = (es or self.es).enter_context(self.nc.sbuf_tensor(name + "_%d" % self.nalloc, list(shape), dtype))
        return T(t[:], name + "_%d" % self.nalloc)

    def ps(self, shape, dtype, name=None, es=None):
        self.nalloc += 1
        name = name or ("p%d" % self.nalloc)
        t = (es or self.es).enter_context(self.nc.psum_tensor(name + "_%d" % self.nalloc, list(shape), dtype))
        return T(t[:], name + "_%d" % self.nalloc)

    def dram(self, name, shape, dtype, kind="Internal"):
        t = self.nc.dram_tensor(name, list(shape), dtype, kind=kind)
        return T(t.ap(), name)

    def _semobj(self, semkey):
        return self.csem[semkey[1]] if semkey[0] == "c" else self.dsem[semkey[1]]

    def _wait(self, e, semkey, val):
        if self.waited[e].get(semkey, 0) >= val:
            return
        self.waited[e][semkey] = val
        self.E[e].wait_ge(self._semobj(semkey), val)

    def _deps(self, e, reads, writes, is_dma):
        for r in reads:
            lw = self.lastw.get(_key(r))
            if lw is not None:
                self._wait(e, lw[0], lw[1])
        for w in writes:
            k = _key(w)
            lw = self.lastw.get(k)
            if lw is not None:
                if not (lw[0] == ("c", e) and not is_dma):
                    self._wait(e, lw[0], lw[1])
            for sk, v in self.readers.get(k, {}).items():
                if sk == ("c", e) and not is_dma:
                    continue
                self._wait(e, sk, v)

    def _record(self, tok, reads, writes):
        for r in reads:
            d = self.readers.setdefault(_key(r), {})
            if d.get(tok[0], 0) < tok[1]:
                d[tok[0]] = tok[1]
        for w in writes:
            k = _key(w)
            self.lastw[k] = tok
            self.readers[k] = {}

    def op(self, e, fn, reads=(), writes=()):
        self._deps(e, reads, writes, False)
        ins = fn(self.E[e])
        self.cnt[e] += 1
        ins.then_inc(self.csem[e], 1)
        self._record((("c", e), self.cnt[e]), reads, writes)
        self.ninst += 1
        return ins

    def dma(self, out, in_, q="sp", fn=None, reads=None, writes=None, **kw):
        reads = [in_] if reads is None else reads
        writes = [out] if writes is None else writes
        slot = self.dnext
        self.dnext = (slot + 1) % self.NDS
        if self.dcnt[slot] > 0:
            self._wait(q, ("d", slot), 16 * self.dcnt[slot])
        self._deps(q, reads, writes, True)
        if fn is None:
            ins = self.E[q].dma_start(out=out.ap, in_=in_.ap, **kw)
        else:
            ins = fn(self.E[q])
        self.dcnt[slot] += 1
        ins.then_inc(self.dsem[slot], 16)
        self._record((("d", slot), 16 * self.dcnt[slot]), reads, writes)
        self.ninst += 1
        return ins

    def barrier(self):
        for e in self.E:
            for e2 in self.CE:
                if e2 != e and self.cnt[e2] > 0:
                    self._wait(e, ("c", e2), self.cnt[e2])
            for s in range(self.NDS):
                if self.dcnt[s] > 0:
                    self._wait(e, ("d", s), 16 * self.dcnt[s])

    def mm(self, out, lhsT, rhs, start=True, stop=True, extra_reads=()):
        return self.op("pe", lambda e: e.matmul(out.ap, lhsT.ap, rhs.ap, start=start, stop=stop),
                       reads=[lhsT, rhs, *extra_reads], writes=[out])

    def tr(self, out, in_, ident):
        return self.op("pe", lambda e: e.transpose(out.ap, in_.ap, ident.ap),
                       reads=[in_, ident], writes=[out])

    def act(self, out, in_, func, bias=None, scale=None, accum=None, e="act"):
        kw = {}
        rd = [in_]
        wr = [out]
        if bias is not None:
            if isinstance(bias, T):
                kw["bias"] = bias.ap
                rd.append(bias)
            else:
                kw["bias"] = bias
        if scale is not None:
            if isinstance(scale, T):
                kw["scale"] = scale.ap
                rd.append(scale)
            else:
                kw["scale"] = scale
        if accum is not None:
            kw["accum_out"] = accum.ap
            wr.append(accum)
        return self.op(e, lambda en: en.activation(out.ap, in_.ap, func, **kw), reads=rd, writes=wr)

    def tt(self, out, a, b, op, e="dve"):
        return self.op(e, lambda en: en.tensor_tensor(out.ap, a.ap, b.ap, op), reads=[a, b], writes=[out])

    def ts(self, out, a, s1, op0, s2=None, op1=None, e="dve", accum=None):
        rd = [a]
        wr = [out]
        v1 = s1
        v2 = s2
        if isinstance(s1, T):
            rd.append(s1)
            v1 = s1.ap
        if isinstance(s2, T):
            rd.append(s2)
            v2 = s2.ap
        kw = {}
        if op1 is not None:
            kw["op1"] = op1
        if accum is not None:
            kw["accum_out"] = accum.ap
            wr.append(accum)
        return self.op(e, lambda en: en.tensor_scalar(out.ap, a.ap, v1, v2, op0, **kw), reads=rd, writes=wr)

    def stt(self, out, a, s, b, op0, op1, e="dve"):
        rd = [a, b]
        v = s
        if isinstance(s, T):
            rd.append(s)
            v = s.ap
        return self.op(e, lambda en: en.scalar_tensor_tensor(out.ap, a.ap, v, b.ap, op0, op1), reads=rd, writes=[out])

    def copy(self, out, in_, e="dve"):
        if e == "act":
            return self.op(e, lambda en: en.activation(out.ap, in_.ap, AF.Copy), reads=[in_], writes=[out])
        return self.op(e, lambda en: en.tensor_copy(out.ap, in_.ap), reads=[in_], writes=[out])

    def memset(self, out, v, e="dve"):
        return self.op(e, lambda en: en.memset(out.ap, v), reads=[], writes=[out])

    def rsq(self, dst, src, mul, add):
        self.ts(dst, src, mul, ALU.mult, add, ALU.add)
        self.act(dst, dst, AF.Sqrt)
        self.op("dve", lambda e: e.reciprocal(dst.ap, dst.ap), reads=[dst], writes=[dst])

L_ = 2
D = 1024
NTX = 4096
NTC = 256
NT = NTX + NTC
NTILE = NT // 128
NCOL = 2464
SCALE_MLA = float((64 + 32) ** -0.5)
LN2 = float(np.log(2.0))


def _rope_tables():
    t = np.arange(NTX)
    row = (t // 64).astype(np.float32)
    col = (t % 64).astype(np.float32)

    def tab(dh_half):
        dh = dh_half
        inv = 10000.0 ** (-np.arange(0, dh, 2, dtype=np.float32) / dh)
        return inv

    def build(dtot):
        half = dtot // 2
        inv = tab(half)
        nf = half // 2
        cos = np.zeros((dtot, NTX), np.float32)
        sins = np.zeros((dtot, NTX), np.float32)
        perm = np.zeros((dtot, dtot), np.float32)
        for part, pos in ((0, row), (1, col)):
            base = part * half
            ang = pos[None, :] * inv[:, None]
            c, s = np.cos(ang), np.sin(ang)
            for j in range(nf):
                cos[base + j] = c[j]
                cos[base + nf + j] = c[j]
                sins[base + j] = -s[j]
                sins[base + nf + j] = s[j]
                perm[base + nf + j, base + j] = 1.0
                perm[base + j, base + nf + j] = 1.0
        return cos, sins, perm

    c32, s32, p32 = build(32)
    c64, s64, p64 = build(64)
    c128 = np.concatenate([c64, c64], 0)
    s128 = np.concatenate([s64, s64], 0)
    p128 = np.zeros((128, 128), np.float32)
    p128[:64, :64] = p64
    p128[64:, 64:] = p64
    rope32 = np.stack([c32, s32]).astype(np.float32)
    rope128 = np.stack([c128, s128]).astype(np.float32)
    return rope32, rope128, p32, p128


_CST = {}


def _cst_layout():
    off = 0
    for name, w in (("ident", 128), ("A", 128), ("B", 128), ("C1", 128), ("C2", 128),
                    ("colK1", 1), ("colK2", 1), ("iota", 512), ("triU", 128),
                    ("mprev", 128), ("mnext", 128), ("p128", 128), ("p32", 32),
                    ("tokid", NTILE * 16 * 2)):
        _CST[name] = (off, w)
        off += w
    return off


NCST = _cst_layout()


def _const_table():
    rope32, rope128, p32, p128 = _rope_tables()
    cst = np.zeros((128, NCST), np.float32)
    p = np.arange(128, dtype=np.float32)[:, None]
    c = np.arange(128, dtype=np.float32)[None, :]

    def put(name, arr):
        o, w = _CST[name]
        cst[:arr.shape[0], o:o + w] = arr
    put("ident", np.eye(128, dtype=np.float32))
    put("A", np.maximum(c - p, 0.0))
    put("B", np.maximum(p - c, 0.0))
    put("C1", np.broadcast_to(c + 1.0, (128, 128)))
    put("C2", np.broadcast_to(128.0 - c, (128, 128)))
    put("colK1", 127.0 - p)
    put("colK2", p)
    put("iota", np.broadcast_to(np.arange(512, dtype=np.float32)[None, :], (128, 512)))
    put("triU", (p <= c).astype(np.float32))
    put("mprev", (p >= c).astype(np.float32))
    put("mnext", (p <= c).astype(np.float32))
    put("p128", p128)
    put("p32", p32)
    rows = (np.arange(NTILE)[None, :] * 128 + np.arange(128)[:, None])
    tok = np.stack([rows // 64, rows % 64], -1).astype(np.float32)
    tok = np.broadcast_to(tok[:, :, None, :], (128, NTILE, 16, 2)).reshape(128, -1)
    put("tokid", tok)
    return cst, rope32, rope128


def _prep_shared(inp):
    f = lambda a: np.ascontiguousarray(a, dtype=np.float32)
    sh = {}
    w_in = inp["w_in"]
    s = np.cumsum([0, 256, 256, 256, 256, 256, 128, 32, 256, 128, 128])
    rq, rk, rv, rg, cq, ckv, kr, wq, wk, wv = [w_in[:, :, s[i]:s[i + 1]] for i in range(10)]
    wk2 = np.concatenate([wk[:, :, 0:64], wk[:, :, 0:64], wk[:, :, 64:128], wk[:, :, 64:128]], -1)
    wv2 = np.concatenate([wv[:, :, 0:64], wv[:, :, 0:64], wv[:, :, 64:128], wv[:, :, 64:128]], -1)
    sh["w_in_r"] = f(np.concatenate([rq, rk, cq, ckv, wq, wk2, kr, rk, rv, rg, wv2], -1))
    assert sh["w_in_r"].shape[-1] == NCOL
    uq = inp["mla_w_uq"]
    sh["w_uq_n"] = f(uq[:, :, :, :64].reshape(L_, 256, 512))
    sh["w_uq_r"] = f(uq[:, :, :, 64:].reshape(L_, 256, 256))
    uk = inp["mla_w_uk"]
    ukT = uk.transpose(0, 2, 3, 1).reshape(L_, 4, 2, 64, 128)
    sh["w_ukT"] = f(ukT.transpose(0, 2, 3, 1, 4).reshape(L_, 128, 4, 128))
    sh["w_uv"] = f(inp["mla_w_uv"].reshape(L_, 128, 512))
    sh["w_out"] = f(inp["w_out"])
    sh["router_w"] = f(inp["router_w"])
    sh["ada_w"] = f(inp["ada_w"])
    sh["ada_b"] = f(inp["ada_b"].reshape(L_, 1, 6 * D))
    sh["exp_wg"] = f(inp["exp_w_gate"])
    sh["exp_wu"] = f(inp["exp_w_up"])
    sh["exp_wd"] = f(inp["exp_w_down"])
    gb = np.stack([inp["norm1_g"][0], inp["norm2_g"][0], inp["norm1_g"][1], inp["norm2_g"][1], inp["final_g"]])
    sh["g_bc"] = f(np.broadcast_to(gb[:, None, :], (5, 128, D)))
    cols = []
    for l in range(L_):
        qg = inp["mla_qnorm_g"][l].reshape(2, 128).T
        kg = inp["mla_kvnorm_g"][l].reshape(1, 128).T
        df, db, sk = inp["ret_decay_f"][l], inp["ret_decay_b"][l], inp["win_sink"][l]
        rep = np.broadcast_to(np.concatenate([df, db, sk])[None, :], (128, 12))
        hp = (np.arange(128) >= 64).astype(np.int64)
        pp = np.stack([df[0 + hp], df[2 + hp], db[0 + hp], db[2 + hp]], -1)
        cols += [qg, kg, rep, pp]
    sh["small"] = f(np.concatenate(cols, -1))
    cst, rope32, rope128 = _const_table()
    sh["cst"] = cst
    sh["rope32"] = rope32
    sh["rope128"] = rope128
    return sh


def _prep_core(inp, b):
    f = lambda a: np.ascontiguousarray(a, dtype=np.float32)
    d = {}
    d["x0"] = f(np.concatenate([inp["ctx"][b], inp["x"][b]], 0))
    cv = np.stack([inp["c_ctx"], inp["c"][b]])
    cr = cv.reshape(2, 8, 128).transpose(0, 2, 1)
    d["crep"] = f(np.broadcast_to(cr[:, :, :, None], (2, 128, 8, 128)))
    return d

class Rot:
    def __init__(self, tiles):
        self.t = tiles
        self.i = 0

    def get(self):
        t = self.t[self.i % len(self.t)]
        self.i += 1
        return t


def TD(t, pattern, **kw):
    return T(t.ap.rearrange(pattern, **kw), t.key)


def build(upto=None, dbg=False, nlayers=L_):
    nc = bass.Bass("TRN2", target_bir_lowering=False)
    es0 = ExitStack()
    with es0:
        k = KB(nc, es0)
        kind_dbg = "ExternalOutput" if dbg else "Internal"
        din = lambda n, s, dt=F32: k.dram(n, s, dt, kind="ExternalInput")
        x0 = din("x0", [NT, D])
        crep_d = din("crep", [2, 128, 8, 128])
        w_in_d = din("w_in_r", [L_, D, NCOL])
        w_uq_n_d = din("w_uq_n", [L_, 256, 512])
        w_uq_r_d = din("w_uq_r", [L_, 256, 256])
        w_ukT_d = din("w_ukT", [L_, 128, 4, 128])
        w_uv_d = din("w_uv", [L_, 128, 512])
        w_out_d = din("w_out", [L_, D, D])
        router_d = din("router_w", [L_, D, 16])
        ada_w_d = din("ada_w", [L_, D, 6 * D])
        ada_b_d = din("ada_b", [L_, 1, 6 * D])
        wg_d = din("exp_wg", [L_, 16, D, 768])
        wu_d = din("exp_wu", [L_, 16, D, 768])
        wd_d = din("exp_wd", [L_, 16, 768, D])
        g_bc_d = din("g_bc", [5, 128, D])
        small_d = din("small", [128, L_ * 19])
        cst_d = din("cst", [128, NCST])
        rope32_d = din("rope32", [2, 32, NTX])
        rope128_d = din("rope128", [2, 128, NTX])
        out_d = k.dram("out", [NTX, D], F32, kind="ExternalOutput")
        X = k.dram("X", [NT, D], F32, kind=kind_dbg)
        MODBC = k.dram("MODBC", [2, 6, 128, D], F32, kind=kind_dbg)
        QT = k.dram("QT", [2, 128, NT], BF16, kind=kind_dbg)
        KT = k.dram("KT", [2, 128, NT], BF16, kind=kind_dbg)
        TMo = k.dram("TMo", [NT, 1024], BF16, kind=kind_dbg)
        QL = k.dram("QL", [8, 128, NT], BF16, kind=kind_dbg)
        QR = k.dram("QR", [8, 32, NT], BF16, kind=kind_dbg)
        KVT = k.dram("KVT", [128, NT], BF16, kind=kind_dbg)
        KRT = k.dram("KRT", [32, NT], BF16, kind=kind_dbg)
        VP = k.dram("VP", [NT, 512], BF16, kind=kind_dbg)
        WQT = k.dram("WQT", [2, 128, NT], BF16, kind=kind_dbg)
        WKT = k.dram("WKT", [2, 128, NT], BF16, kind=kind_dbg)
        YT = k.dram("YT", [8, 128, NT], BF16, kind=kind_dbg)
        H2 = k.dram("H2", [NT, D], BF16, kind=kind_dbg)

        cst = k.sb([128, NCST], F32, "cst")
        k.dma(cst, cst_d)
        small = k.sb([128, L_ * 19], F32, "small")
        k.dma(small, small_d)

        def C(name, rows=128):
            o, w = _CST[name]
            return cst[0:rows, o:o + w]
        ident = C("ident")
        ones_f = k.sb([128, 128], F32, "ones_f")
        k.memset(ones_f, 1.0)
        ones_b = k.sb([128, 128], BF16, "ones_b")
        k.memset(ones_b, 1.0)
        ident_b = k.sb([128, 128], BF16, "ident_b")
        k.copy(ident_b, ident)
        triU_b = k.sb([128, 128], BF16, "triU_b")
        k.copy(triU_b, C("triU"))
        mprev_b = k.sb([128, 128], BF16, "mprev_b")
        k.copy(mprev_b, C("mprev"))
        mnext_b = k.sb([128, 128], BF16, "mnext_b")
        k.copy(mnext_b, C("mnext"))
        Rtab = k.sb([128, NTILE, 16, 4], BF16, "Rtab")
        o_tok, w_tok = _CST["tokid"]
        k.copy(Rtab[:, :, :, 0:2], T(cst.ap[:, o_tok:o_tok + w_tok].rearrange("p (n e c) -> p n e c", n=NTILE, e=16), cst.key))
        affT = k.sb([128, NTILE, 16], F32, "affT")
        aff_e = k.sb([16, NT], F32, "aff_e")
        posm = k.sb([128, NTILE, 16], F32, "posm")

        EPS = 1e-6
        blocks = [(0, 256, 0)] + [(256 + 512 * i, 512, 1) for i in range(8)]

        def rows_key(t, n):
            return t.sub(("r", n))

        def stop_here(name):
            return upto is not None and upto == name

        def rstd_from_ss(ss, n_feat, es, eps=EPS):
            r = k.sb([128, 1], F32, es=es)
            k.rsq(r, ss, 1.0 / n_feat, eps)
            return r

        for l in range(nlayers):
            need_ctx = l < L_ - 1
            sm0 = l * 19
            Xsrc = x0 if l == 0 else X
            with ExitStack() as s:
                crep = k.sb([128, 2, 8, 128], F32, es=s)
                for st in range(2):
                    k.dma(crep[:, st], crep_d[st])
                sil = k.sb([128, 2, 8, 128], F32, es=s)
                k.act(sil, crep, AF.Silu)
                wb = Rot([k.sb([128, 8, 512], F32, es=s) for _ in range(2)])
                br = Rot([k.sb([1, 512], F32, es=s) for _ in range(2)])
                pp = Rot([k.ps([128, 512], F32, es=s) for _ in range(4)])
                ob = Rot([k.sb([128, 512], F32, es=s) for _ in range(4)])
                aw = TD(ada_w_d[l], "(kc p) n -> p kc n", p=128)
                for nb in range(12):
                    w = wb.get()
                    k.dma(w, aw[:, :, nb * 512:(nb + 1) * 512])
                    b_ = br.get()
                    k.dma(b_, ada_b_d[l, :, nb * 512:(nb + 1) * 512])
                    for st in range(2):
                        ps = pp.get()
                        for kc in range(8):
                            k.mm(ps, sil[:, st, kc, :], w[:, kc, :], start=(kc == 0), stop=False)
                        k.mm(ps, ones_f[0:1, :], b_, start=False, stop=True)
                        o = ob.get()
                        k.copy(o, ps, e=("act" if st == 0 else "dve"))
                        j, half = nb // 2, nb % 2
                        k.dma(MODBC.sl((st, j), np.s_[st, j, :, half * 512:(half + 1) * 512]), o)
            k.barrier()
            if stop_here("mod%d" % l):
                break

            def load_mod(st, j, es, eng_q="sp"):
                t = k.sb([128, D], F32, es=es)
                k.dma(t, MODBC.sl((st, j), np.s_[st, j]))
                return t

            def load_gs(st, j_scale, gidx, es):
                sc = load_mod(st, j_scale, es)
                gb = k.sb([128, D], F32, es=es)
                k.dma(gb, g_bc_d[gidx])
                k.stt(sc, sc, 1.0, gb, ALU.add, ALU.mult)
                return sc

            def load_cast(dst, src, stg, e="pool"):
                t = stg.get()
                sh = list(src.ap.shape)
                tv = t[0:sh[0], 0:sh[1]]
                k.dma(tv, src)
                k.copy(dst, tv, e=e)

            with ExitStack() as s:
                w_in = k.sb([128, 8, NCOL], BF16, es=s)
                stg = Rot([k.sb([128, NCOL], F32, es=s) for _ in range(2)])
                for kc in range(8):
                    load_cast(w_in[:, kc, :], w_in_d[l, kc * 128:(kc + 1) * 128, :], stg, e=("pool" if kc % 2 else "dve"))
                w_uq_n = k.sb([128, 2, 512], BF16, es=s)
                w_uq_r = k.sb([128, 2, 256], BF16, es=s)
                for kc in range(2):
                    load_cast(w_uq_n[:, kc, :], w_uq_n_d[l, kc * 128:(kc + 1) * 128, :], stg)
                    load_cast(w_uq_r[:, kc, :], w_uq_r_d[l, kc * 128:(kc + 1) * 128, :], stg)
                w_ukT = k.sb([128, 4, 128], BF16, es=s)
                load_cast(T(w_ukT.ap.rearrange("p a r -> p (a r)"), w_ukT.key), TD(w_ukT_d[l], "p a r -> p (a r)"), stg)
                w_uv = k.sb([128, 512], BF16, es=s)
                load_cast(w_uv, w_uv_d[l], stg)
                gs1 = [load_gs(st, 1, l * 2 + 0, s) for st in range(2)]
                sh1 = [load_mod(st, 0, s) for st in range(2)]
                qg = small[:, sm0 + 0:sm0 + 2]
                kg = small[:, sm0 + 2:sm0 + 3]
                xt_r = Rot([k.sb([128, D], F32, es=s) for _ in range(2)])
                xs_r = Rot([k.sb([128, D], F32, es=s) for _ in range(2)])
                junk = k.sb([128, D], F32, es=s)
                ss_pool = Rot([k.sb([128, 1], F32, es=s) for _ in range(8)])
                hT_r = Rot([k.sb([128, 8, 512], BF16, es=s) for _ in range(2)])
                ptr = Rot([k.ps([128, 4, 128], F32, es=s) for _ in range(2)])
                pfm = Rot([k.ps([128, 512], F32, es=s) for _ in range(3)])
                pex = Rot([k.ps([128, 512], F32, es=s) for _ in range(3)])
                ob16 = Rot([k.sb([128, 512], BF16, es=s) for _ in range(6)])
                f32t = Rot([k.sb([128, 512], F32, es=s) for _ in range(6)])
                rp32 = Rot([k.sb([32, 2, 512], F32, es=s) for _ in range(2)])
                rp128 = Rot([k.sb([128, 2, 512], F32, es=s) for _ in range(2)])
                cqg = k.sb([128, 2, 512], BF16, es=s)
                rstdq = k.sb([128, 512], F32, es=s)
                kvT_sb = k.sb([128, 512], BF16, es=s)
                tmo = Rot([k.sb([128, 1024], BF16, es=s) for _ in range(2)])

                def bc_rstd(dst, sq_list, nfeat, n):
                    ps = pex.get()
                    for i, sq in enumerate(sq_list):
                        k.mm(ps[:, :n], ones_f, sq, start=(i == 0), stop=(i == len(sq_list) - 1))
                    k.rsq(dst[:, :n], ps[:, :n], 1.0 / nfeat, EPS)

                def rope_apply(src_f32, M, n, tabs, perm, out_bf):
                    ps = pex.get()
                    k.mm(ps[0:M, :n], perm, src_f32[0:M, :n])
                    t1 = f32t.get()
                    k.tt(t1[0:M, :n], src_f32[0:M, :n], tabs[0:M, 0, :n], ALU.mult)
                    t2 = f32t.get()
                    k.tt(t2[0:M, :n], ps[0:M, :n], tabs[0:M, 1, :n], ALU.mult)
                    k.tt(out_bf[0:M, :n], t1[0:M, :n], t2[0:M, :n], ALU.add, e="pool")

                for (r0, n, st) in blocks:
                    nt = n // 128
                    hT = hT_r.get()
                    if st == 1:
                        t0 = r0 - NTC
                        r32 = rp32.get()
                        k.dma(r32[:, :, :n], TD(rope32_d, "a p t -> p a t")[:, :, t0:t0 + n])
                        r128 = rp128.get()
                        k.dma(r128[:, :, :n], TD(rope128_d, "a p t -> p a t")[:, :, t0:t0 + n])
                    for ti in range(nt):
                        rr = r0 + ti * 128
                        xt = xt_r.get()
                        k.dma(xt, rows_key(Xsrc, rr // 128)[rr:rr + 128, :])
                        ss = ss_pool.get()
                        k.act(junk, xt, AF.Square, accum=ss)
                        rstd = ss_pool.get()
                        k.rsq(rstd, ss, 1.0 / D, EPS)
                        xs = xs_r.get()
                        k.ts(xs, xt, rstd, ALU.mult)
                        k.tt(xs, xs, gs1[st], ALU.mult, e="pool")
                        k.tt(xs, xs, sh1[st], ALU.add, e="pool")
                        for hf in range(2):
                            pt = ptr.get()
                            for q4 in range(4):
                                kc = hf * 4 + q4
                                k.tr(pt[:, q4, :], xs[:, kc * 128:(kc + 1) * 128], ident)
                            k.copy(hT[:, hf * 4:(hf + 1) * 4, ti * 128:(ti + 1) * 128], pt, e="act")
                    def fm(c, M=128):
                        ps = pfm.get()
                        for kc in range(8):
                            k.mm(ps[0:M, :n], w_in[:, kc, c * 128:c * 128 + M], hT[:, kc, :n], start=(kc == 0), stop=(kc == 7))
                        return ps
                    for c in range(4):
                        ps = fm(c)
                        o = ob16.get()
                        k.copy(o[:, :n], ps[:, :n], e=("act" if c % 2 == 0 else "dve"))
                        dst = (QT if c < 2 else KT)
                        k.dma(dst.sl((c % 2, r0), np.s_[c % 2, :, r0:r0 + n]), o[:, :n])
                    sqs = []
                    for c2 in range(2):
                        ps = fm(4 + c2)
                        sq = f32t.get()
                        k.act(sq[:, :n], ps[:, :n], AF.Square)
                        sqs.append(sq[:, :n])
                        k.act(cqg[:, c2, :n], ps[:, :n], AF.Copy, scale=qg[:, c2:c2 + 1])
                    bc_rstd(rstdq, sqs, 256, n)
                    for pr in range(4):
                        ps = pex.get()
                        for c2 in range(2):
                            k.mm(ps[:, :n], w_uq_n[:, c2, pr * 128:(pr + 1) * 128], cqg[:, c2, :n], start=(c2 == 0), stop=(c2 == 1))
                        qn = ob16.get()
                        k.tt(qn[:, :n], ps[:, :n], rstdq[:, :n], ALU.mult)
                        for hh in range(2):
                            h = pr * 2 + hh
                            off = hh * 64
                            ps2 = pex.get()
                            k.mm(ps2[:, :n], w_ukT[off:off + 64, pr, :], qn[off:off + 64, :n])
                            o = ob16.get()
                            k.copy(o[:, :n], ps2[:, :n], e="act")
                            k.dma(QL.sl((h, r0), np.s_[h, :, r0:r0 + n]), o[:, :n])
                    for h in range(8):
                        ps = pex.get()
                        for c2 in range(2):
                            k.mm(ps[0:32, :n], w_uq_r[:, c2, h * 32:(h + 1) * 32], cqg[:, c2, :n], start=(c2 == 0), stop=(c2 == 1))
                        qr = f32t.get()
                        k.tt(qr[0:32, :n], ps[0:32, :n], rstdq[0:32, :n], ALU.mult)
                        o = ob16.get()
                        if st == 1:
                            rope_apply(qr, 32, n, r32, C("p32", 32), o)
                        else:
                            k.copy(o[0:32, :n], qr[0:32, :n], e="pool")
                        k.dma(QR.sl((h, r0), np.s_[h, :, r0:r0 + n]), o[0:32, :n])
                    ps = fm(6)
                    sq = f32t.get()
                    k.act(sq[:, :n], ps[:, :n], AF.Square)
                    kvg = f32t.get()
                    k.act(kvg[:, :n], ps[:, :n], AF.Copy, scale=kg[:, 0:1])
                    rk_ = f32t.get()
                    bc_rstd(rk_, [sq[:, :n]], 128, n)
                    k.tt(kvT_sb[:, :n], kvg[:, :n], rk_[:, :n], ALU.mult)
                    k.dma(KVT.sl(r0, np.s_[:, r0:r0 + n]), kvT_sb[:, :n])
                    for ti in range(nt):
                        ps = pex.get()
                        k.mm(ps, kvT_sb[:, ti * 128:(ti + 1) * 128], w_uv)
                        o = ob16.get()
                        k.copy(o, ps, e="act")
                        rr = r0 + ti * 128
                        k.dma(VP.sl(rr // 128, np.s_[rr:rr + 128, :]), o)
                    for c in range(4):
                        ps = fm(7 + c)
                        o = ob16.get()
                        if st == 1:
                            sf = f32t.get()
                            k.copy(sf[:, :n], ps[:, :n], e="act")
                            rope_apply(sf, 128, n, r128, C("p128"), o)
                        else:
                            k.copy(o[:, :n], ps[:, :n], e="act")
                        dst = (WQT if c < 2 else WKT)
                        k.dma(dst.sl((c % 2, r0), np.s_[c % 2, :, r0:r0 + n]), o[:, :n])
                    ps = fm(11, 32)
                    o = ob16.get()
                    if st == 1:
                        sf = f32t.get()
                        k.copy(sf[0:32, :n], ps[0:32, :n], e="act")
                        rope_apply(sf, 32, n, r32, C("p32", 32), o)
                    else:
                        k.copy(o[0:32, :n], ps[0:32, :n], e="act")
                    k.dma(KRT.sl(r0, np.s_[:, r0:r0 + n]), o[0:32, :n])
                    for ti in range(nt):
                        o = tmo.get()
                        for hf in range(2):
                            ps = pfm.get()
                            c0 = 1440 + hf * 512
                            for kc in range(8):
                                k.mm(ps, hT[:, kc, ti * 128:(ti + 1) * 128], w_in[:, kc, c0:c0 + 512], start=(kc == 0), stop=(kc == 7))
                            k.copy(o[:, hf * 512:(hf + 1) * 512], ps, e=("act" if hf == 0 else "dve"))
                        rr = r0 + ti * 128
                        k.dma(TMo.sl(rr // 128, np.s_[rr:rr + 128, :]), o)
            k.barrier()
            if stop_here("s1_%d" % l):
                break
            with ExitStack() as s:
                lg_rep = k.sb([128, 8], F32, es=s)
                lg_pp = k.sb([128, 4], F32, es=s)
                for dst, src in ((lg_rep, small[:, sm0 + 3:sm0 + 11]), (lg_pp, small[:, sm0 + 15:sm0 + 19])):
                    k.act(dst, src, AF.Exp, scale=LN2)
                    k.ts(dst, dst, -1.0, ALU.mult, 1.0, ALU.add)
                    k.act(dst, dst, AF.Ln)
                LN8 = float(np.log(0.125))
                DT = k.sb([128, 4, 128], F32, es=s)
                tmpA = k.sb([128, 128], F32, es=s)
                for h in range(4):
                    k.ts(tmpA, C("A"), lg_rep[:, h:h + 1], ALU.mult)
                    k.stt(tmpA, C("B"), lg_rep[:, 4 + h:5 + h], tmpA, ALU.mult, ALU.add)
                    k.act(DT[:, h, :], tmpA, AF.Exp)
                    k.ts(DT[:, h, :], DT[:, h, :], 0.125, ALU.mult)
                QWF = k.sb([128, 2, 128], F32, es=s)
                QWB = k.sb([128, 2, 128], F32, es=s)
                gch = k.sb([128, 4], F32, es=s)
                for pr in range(2):
                    k.act(QWF[:, pr, :], C("C1"), AF.Exp, scale=lg_pp[:, pr:pr + 1])
                    k.act(QWB[:, pr, :], C("C2"), AF.Exp, scale=lg_pp[:, 2 + pr:3 + pr])
                k.act(gch, lg_pp, AF.Exp, scale=128.0)
                KWF = k.sb([128, 256], F32, es=s)
                KWB = k.sb([128, 256], F32, es=s)
                kcol = k.sb([128, 8], F32, es=s)
                for h in range(4):
                    k.act(kcol[:, h:h + 1], C("colK1"), AF.Exp, scale=lg_rep[:, h:h + 1])
                    k.act(kcol[:, 4 + h:5 + h], C("colK2"), AF.Exp, scale=lg_rep[:, 4 + h:5 + h])
                k.ts(kcol, kcol, 0.125, ALU.mult)
                for h in range(4):
                    k.copy(KWF[:, h * 64:(h + 1) * 64], T(kcol.ap[:, h:h + 1].to_broadcast([128, 64]), kcol.key))
                    k.copy(KWB[:, h * 64:(h + 1) * 64], T(kcol.ap[:, 4 + h:5 + h].to_broadcast([128, 64]), kcol.key))
                KVd = k.sb([128, NTILE, 4, 64], F32, es=s)
                Sfb = k.sb([128, NTILE, 2, 64], BF16, es=s)
                Sbb = k.sb([128, NTILE, 2, 64], BF16, es=s)
                kv_r = Rot([k.sb([128, 512], BF16, es=s) for _ in range(3)])
                kw_r = Rot([k.sb([128, 2, 256], BF16, es=s) for _ in range(2)])
                pkv = Rot([k.ps([128, 4, 128], F32, es=s) for _ in range(2)])
                for n in range(NTILE):
                    kvt = kv_r.get()
                    k.dma(kvt, TMo.sl(n, np.s_[n * 128:(n + 1) * 128, 0:512]))
                    kw = kw_r.get()
                    k.tt(kw[:, 0, :], kvt[:, 0:256], KWF, ALU.mult)
                    k.tt(kw[:, 1, :], kvt[:, 0:256], KWB, ALU.mult, e="pool")
                    ps = pkv.get()
                    for d_ in range(2):
                        for pr in range(2):
                            k.mm(ps[:, d_ * 2 + pr, :], kw[:, d_, pr * 128:(pr + 1) * 128], kvt[:, 256 + pr * 128:256 + (pr + 1) * 128])
                    k.copy(KVd.sl(n, np.s_[0:64, n, :, :]), ps[0:64, :, 0:64], e="act")
                    k.copy(KVd.sl(n, np.s_[64:128, n, :, :]), ps[64:128, :, 64:128], e="act")
                for d_, (order, Sb_, eng) in enumerate((((list(range(NTILE))), Sfb, "dve"),
                                                       ([1, 0] + list(range(NTILE - 1, 1, -1)), Sbb, "pool"))):
                    cur = k.sb([128, 2, 64], F32, es=s)
                    k.memset(cur, 0.0, e=eng)
                    for n in order:
                        k.copy(Sb_.sl(n, np.s_[:, n, :, :]), cur, e=eng)
                        for pr in range(2):
                            if eng == "dve":
                                k.stt(cur[:, pr, :], cur[:, pr, :], gch[:, d_ * 2 + pr:d_ * 2 + pr + 1],
                                      KVd.sl(n, np.s_[:, n, d_ * 2 + pr, :]), ALU.mult, ALU.add, e=eng)
                            else:
                                k.ts(cur[:, pr, :], cur[:, pr, :], gch[:, d_ * 2 + pr:d_ * 2 + pr + 1], ALU.mult, e=eng)
                                k.tt(cur[:, pr, :], cur[:, pr, :], KVd.sl(n, np.s_[:, n, d_ * 2 + pr, :]), ALU.add, e=eng)
                qk_r = Rot([k.sb([128, 2, 2, 128], BF16, es=s) for _ in range(2)])
                vg_r = Rot([k.sb([128, 512], BF16, es=s) for _ in range(2)])
                qw_r = Rot([k.sb([128, 2, 2, 128], BF16, es=s) for _ in range(2)])
                sm_r = Rot([k.sb([128, 128], BF16, es=s) for _ in range(4)])
                pss = Rot([k.ps([128, 512], F32, es=s) for _ in range(2)])
                psy = Rot([k.ps([128, 512], F32, es=s) for _ in range(2)])
                pst = Rot([k.ps([128, 512], F32, es=s) for _ in range(2)])
                ysb_r = Rot([k.sb([128, 256], F32, es=s) for _ in range(2)])
                sq_r = Rot([k.sb([128, 256], F32, es=s) for _ in range(2)])
                st_r = Rot([k.sb([128, 16], F32, es=s) for _ in range(2)])
                sg_r = Rot([k.sb([128, 256], F32, es=s) for _ in range(2)])
                yo_r = Rot([k.sb([128, 2, 128], BF16, es=s) for _ in range(2)])
                for n in range(0 if need_ctx else 2, NTILE):
                    c0 = n * 128
                    qk = qk_r.get()
                    k.dma(qk[:, 0], TD(QT, "c p t -> p c t")[:, :, c0:c0 + 128])
                    k.dma(qk[:, 1], TD(KT, "c p t -> p c t")[:, :, c0:c0 + 128])
                    vg = vg_r.get()
                    k.dma(vg, TMo.sl(n, np.s_[c0:c0 + 128, 256:768]))
                    qw = qw_r.get()
                    k.tt(qw[:, 0], qk[:, 0], QWF, ALU.mult)
                    k.tt(qw[:, 1], qk[:, 0], QWB, ALU.mult, e="pool")
                    py = psy.get()
                    for h in range(4):
                        pr, off = h // 2, (h % 2) * 64
                        ps = pss.get()
                        k.mm(ps[:, 0:128], qk[off:off + 64, 1, pr, :], qk[off:off + 64, 0, pr, :])
                        sm = sm_r.get()
                        k.tt(sm, ps[:, 0:128], DT[:, h, :], ALU.mult)
                        yo_ = py[:, h * 64:(h + 1) * 64]
                        k.mm(yo_, sm, vg[:, h * 64:(h + 1) * 64], start=True, stop=False)
                        k.mm(yo_, qw[off:off + 64, 0, pr, :], Sfb.sl(n, np.s_[off:off + 64, n, pr, :]), start=False, stop=False)
                        k.mm(yo_, qw[off:off + 64, 1, pr, :], Sbb.sl(n, np.s_[off:off + 64, n, pr, :]), start=False, stop=True)
                    ysb = ysb_r.get()
                    k.copy(ysb, py[:, 0:256], e="act")
                    stt_ = st_r.get()
                    k.op("dve", lambda e: e.reduce_sum(stt_.ap[:, 0:4], ysb.ap.rearrange("p (h d) -> p h d", h=4), AX.X), reads=[ysb], writes=[stt_])
                    sq = sq_r.get()
                    k.tt(sq, ysb, ysb, ALU.mult, e="pool")
                    k.op("dve", lambda e: e.reduce_sum(stt_.ap[:, 4:8], sq.ap.rearrange("p (h d) -> p h d", h=4), AX.X), reads=[sq], writes=[stt_])
                    k.ts(stt_[:, 0:8], stt_[:, 0:8], 1.0 / 64, ALU.mult)
                    k.tt(stt_[:, 8:12], stt_[:, 0:4], stt_[:, 0:4], ALU.mult)
                    k.tt(stt_[:, 12:16], stt_[:, 4:8], stt_[:, 8:12], ALU.subtract)
                    k.rsq(stt_[:, 12:16], stt_[:, 12:16], 1.0, 1e-5)
                    sg = sg_r.get()
                    k.act(sg, vg[:, 256:512], AF.Silu)
                    for h in range(4):
                        k.ts(ysb[:, h * 64:(h + 1) * 64], ysb[:, h * 64:(h + 1) * 64], stt_[:, h:h + 1], ALU.subtract,
                             stt_[:, 12 + h:13 + h], ALU.mult)
                    k.tt(ysb, ysb, sg, ALU.mult, e="pool")
                    pt = pst.get()
                    for c in range(2):
                        k.tr(pt[:, c * 128:(c + 1) * 128], ysb[:, c * 128:(c + 1) * 128], ident)
                    yo = yo_r.get()
                    k.copy(T(yo.ap.rearrange("p c t -> p (c t)"), yo.key), pt[:, 0:256], e="act")
                    k.dma(TD(YT, "c p t -> p c t").sl(("ret", n), np.s_[:, 0:2, c0:c0 + 128]), yo)
            k.barrier()
            if stop_here("ret%d" % l):
                break
            with ExitStack() as s:
                kvT = k.sb([128, NT], BF16, es=s)
                k.dma(kvT, KVT)
                krT = k.sb([32, NT], BF16, es=s)
                k.dma(krT, KRT)
                vp = k.sb([128, NTILE, 512], BF16, es=s)
                for i in range(2):
                    k.dma(vp[:, i * 17:(i + 1) * 17, :], TD(VP, "(t p) c -> p t c", p=128)[:, i * 17:(i + 1) * 17, :])
                ql_r = Rot([k.sb([128, 512], BF16, es=s) for _ in range(2)])
                qr_r = Rot([k.sb([32, 512], BF16, es=s) for _ in range(2)])
                pT_r = Rot([k.sb([128, 512], BF16, es=s) for _ in range(3)])
                pss = Rot([k.ps([128, 512], F32, es=s) for _ in range(3)])
                pacc = Rot([k.ps([128, 512], F32, es=s) for _ in range(4)])
                rd_r = Rot([k.sb([128, 512], F32, es=s) for _ in range(2)])
                yp_r = Rot([k.sb([128, 512], BF16, es=s) for _ in range(2)])
                qblocks = ([(0, 256, [0, 1])] if need_ctx else []) + [(256 + 512 * i, 512, list(range(NTILE))) for i in range(8)]
                for (r0, n, kts) in qblocks:
                    for h in range(8):
                        ql = ql_r.get()
                        k.dma(ql[:, :n], QL[h, :, r0:r0 + n])
                        qr = qr_r.get()
                        k.dma(qr[:, :n], QR[h, :, r0:r0 + n])
                        pv = pacc.get()
                        dn = pacc.get()
                        for i, kt in enumerate(kts):
                            ps = pss.get()
                            k.mm(ps[:, :n], kvT[:, kt * 128:(kt + 1) * 128], ql[:, :n], start=True, stop=False)
                            k.mm(ps[:, :n], krT[:, kt * 128:(kt + 1) * 128], qr[:, :n], start=False, stop=True)
                            pT = pT_r.get()
                            k.act(pT[:, :n], ps[:, :n], AF.Exp, scale=SCALE_MLA)
                            k.mm(pv[:, :n], vp[:, kt, (h // 2) * 128:(h // 2 + 1) * 128], pT[:, :n], start=(i == 0), stop=(i == len(kts) - 1))
                            k.mm(dn[:, :n], ones_b, pT[:, :n], start=(i == 0), stop=(i == len(kts) - 1))
                        off = (h % 2) * 64
                        rd = rd_r.get()
                        k.op("dve", lambda e: e.reciprocal(rd.ap[off:off + 64, :n], dn.ap[off:off + 64, :n]), reads=[dn], writes=[rd])
                        if h % 2 == 0:
                            yp = yp_r.get()
                        k.tt(yp[off:off + 64, :n], pv[off:off + 64, :n], rd[off:off + 64, :n], ALU.mult)
                        if h % 2 == 1:
                            k.dma(YT.sl(("mla", h // 2, r0), np.s_[2 + h // 2, :, r0:r0 + n]), yp[:, :n])
            k.barrier()
            if stop_here("mla%d" % l):
                break
            with ExitStack() as s:
                wkT = k.sb([128, 2, NT], BF16, es=s)
                k.dma(wkT, TD(WKT, "c p t -> p c t"))
                wqT = k.sb([128, 2, NT], BF16, es=s)
                k.dma(wqT, TD(WQT, "c p t -> p c t"))
                wv2 = k.sb([128, NTILE, 256], BF16, es=s)
                k.dma(wv2, TD(TMo, "(t p) c -> p t c", p=128)[:, :, 768:1024])
                esink = k.sb([128, 4], F32, es=s)
                k.act(esink, small[:, sm0 + 11:sm0 + 15], AF.Exp)
                pT_r = Rot([k.sb([128, 128], BF16, es=s) for _ in range(4)])
                pss = Rot([k.ps([128, 512], F32, es=s) for _ in range(3)])
                pacc = Rot([k.ps([128, 512], F32, es=s) for _ in range(4)])
                rd_r = Rot([k.sb([128, 128], F32, es=s) for _ in range(2)])
                yp_r = Rot([k.sb([128, 128], BF16, es=s) for _ in range(2)])
                for n in range(0 if need_ctx else 2, NTILE):
                    c0 = n * 128
                    if n < 2:
                        keys = [(0, None), (1, None)]
                    else:
                        keys = []
                        if n - 1 >= 2:
                            keys.append((n - 1, mprev_b))
                        keys.append((n, None))
                        if n + 1 < NTILE:
                            keys.append((n + 1, mnext_b))
                        keys += [(0, None), (1, None)]
                    for qh in range(4):
                        hk, g = qh // 2, qh % 2
                        off = g * 64
                        pv = pacc.get()
                        dn = pacc.get()
                        for i, (kt, msk) in enumerate(keys):
                            ps = pss.get()
                            k.mm(ps[:, 0:128], wkT[off:off + 64, hk, kt * 128:(kt + 1) * 128], wqT[off:off + 64, hk, c0:c0 + 128])
                            pT = pT_r.get()
                            k.act(pT, ps[:, 0:128], AF.Exp, scale=0.125)
                            if msk is not None:
                                k.tt(pT, pT, msk, ALU.mult)
                            k.mm(pv[:, 0:128], wv2[:, kt, hk * 128:(hk + 1) * 128], pT, start=(i == 0), stop=(i == len(keys) - 1))
                            k.mm(dn[:, 0:128], ones_b, pT, start=(i == 0), stop=(i == len(keys) - 1))
                        rd = rd_r.get()
                        k.ts(rd[off:off + 64, :], dn[off:off + 64, 0:128], esink[off:off + 64, qh:qh + 1], ALU.add)
                        k.op("dve", lambda e: e.reciprocal(rd.ap[off:off + 64, :], rd.ap[off:off + 64, :]), reads=[rd], writes=[rd])
                        if g == 0:
                            yp = yp_r.get()
                        k.tt(yp[off:off + 64, :], pv[off:off + 64, 0:128], rd[off:off + 64, :], ALU.mult)
                        if g == 1:
                            k.dma(YT.sl(("win", hk, n), np.s_[6 + hk, :, c0:c0 + 128]), yp)
            k.barrier()
            if stop_here("win%d" % l):
                break
            with ExitStack() as s:
                w_out = k.sb([128, 8, D], BF16, es=s)
                stg = Rot([k.sb([128, D], F32, es=s) for _ in range(2)])
                for kc in range(8):
                    load_cast(w_out[:, kc, :], w_out_d[l, kc * 128:(kc + 1) * 128, :], stg, e=("pool" if kc % 2 else "dve"))
                rw = k.sb([128, 8, 16], F32, es=s)
                k.dma(rw, TD(router_d[l], "(kc p) e -> p kc e", p=128))
                sts = [0, 1] if need_ctx else [1]
                mod2 = {st: load_mod(st, 2, s) for st in sts}
                gs2 = {st: load_gs(st, 4, l * 2 + 1, s) for st in sts}
                sh2 = {st: load_mod(st, 3, s) for st in sts}
                yT_r = Rot([k.sb([128, 8, 128], BF16, es=s) for _ in range(2)])
                xt_r = Rot([k.sb([128, D], F32, es=s) for _ in range(2)])
                xn_r = Rot([k.sb([128, D], F32, es=s) for _ in range(2)])
                xs_r = Rot([k.sb([128, D], F32, es=s) for _ in range(2)])
                h2b_r = Rot([k.sb([128, D], BF16, es=s) for _ in range(2)])
                h2T_r = Rot([k.sb([128, 8, 128], F32, es=s) for _ in range(2)])
                junk = k.sb([128, D], F32, es=s)
                ss_pool = Rot([k.sb([128, 1], F32, es=s) for _ in range(8)])
                ex_r = Rot([k.sb([128, 16], F32, es=s) for _ in range(2)])
                pso = Rot([k.ps([128, 512], F32, es=s) for _ in range(3)])
                ptr = Rot([k.ps([128, 4, 128], F32, es=s) for _ in range(2)])
                psl = Rot([k.ps([128, 512], F32, es=s) for _ in range(2)])
                for n in range(0 if need_ctx else 2, NTILE):
                    st = 0 if n < 2 else 1
                    c0 = n * 128
                    yT = yT_r.get()
                    k.dma(yT, TD(YT, "c p t -> p c t")[:, :, c0:c0 + 128])
                    xt = xt_r.get()
                    k.dma(xt, rows_key(Xsrc, n)[c0:c0 + 128, :])
                    xn = xn_r.get()
                    for hf in range(2):
                        ps = pso.get()
                        for kc in range(8):
                            k.mm(ps, yT[:, kc, :], w_out[:, kc, hf * 512:(hf + 1) * 512], start=(kc == 0), stop=(kc == 7))
                        sl_ = np.s_[:, hf * 512:(hf + 1) * 512]
                        k.tt(xn[sl_], ps, mod2[st][sl_], ALU.mult)
                        k.tt(xn[sl_], xn[sl_], xt[sl_], ALU.add, e="pool")
                    k.dma(rows_key(X, n)[c0:c0 + 128, :], xn)
                    ss = ss_pool.get()
                    k.act(junk, xn, AF.Square, accum=ss)
                    rstd = ss_pool.get()
                    k.rsq(rstd, ss, 1.0 / D, EPS)
                    xs = xs_r.get()
                    k.ts(xs, xn, rstd, ALU.mult)
                    k.tt(xs, xs, gs2[st], ALU.mult, e="pool")
                    k.tt(xs, xs, sh2[st], ALU.add, e="pool")
                    h2b = h2b_r.get()
                    k.copy(h2b, xs, e="act")
                    k.dma(H2.sl(n, np.s_[c0:c0 + 128, :]), h2b)
                    h2T = h2T_r.get()
                    for hf in range(2):
                        pt = ptr.get()
                        for q4 in range(4):
                            kc = hf * 4 + q4
                            k.tr(pt[:, q4, :], xs[:, kc * 128:(kc + 1) * 128], ident)
                        k.copy(h2T[:, hf * 4:(hf + 1) * 4, :], pt, e="act")
                    pl = psl.get()
                    for kc in range(8):
                        k.mm(pl[:, 0:16], h2T[:, kc, :], rw[:, kc, :], start=(kc == 0), stop=(kc == 7))
                    ex = ex_r.get()
                    sm_ = ss_pool.get()
                    k.act(ex, pl[:, 0:16], AF.Exp, accum=sm_)
                    k.op("dve", lambda e: e.reciprocal(sm_.ap, sm_.ap), reads=[sm_], writes=[sm_])
                    k.ts(affT.sl(n, np.s_[:, n, :]), ex, sm_, ALU.mult)
                    pl2 = psl.get()
                    k.tr(pl2[0:16, 0:128], affT.sl(n, np.s_[:, n, :]), ident)
                    k.copy(aff_e.sl(n, np.s_[:, c0:c0 + 128]), pl2[0:16, 0:128], e="act")
            k.barrier()
            if stop_here("o%d" % l):
                break
            streams = ([(0, 2, 32)] if need_ctx else []) + [(2, 32, 512)]
            with ExitStack() as s:
                work = k.sb([16, NTX], F32, es=s)
                m8 = k.sb([16, 8], F32, es=s)
                thr = k.sb([16, 1], F32, es=s)
                mask_e = k.sb([16, NTX], F32, es=s)
                maskTb = k.sb([128, 32, 16], BF16, es=s)
                carry = k.sb([128, 16], F32, es=s)
                pos_r = Rot([k.sb([128, 16], F32, es=s) for _ in range(2)])
                ptk = Rot([k.ps([128, 512], F32, es=s) for _ in range(4)])
                ahi = k.sb([128, NTILE, 16], BF16, es=s)
                alo = k.sb([128, NTILE, 16], F32, es=s)
                k.copy(ahi, affT)
                k.tt(alo, affT, ahi, ALU.subtract)
                k.copy(Rtab[:, :, :, 2], ahi, e="pool")
                k.copy(Rtab[:, :, :, 3], alo, e="pool")
                for (t0, ntl, cap) in streams:
                    ntok = ntl * 128
                    cs = np.s_[:, t0 * 128:t0 * 128 + ntok]
                    k.copy(work[:, :ntok], aff_e[cs])
                    for r in range(cap // 8):
                        k.op("dve", lambda e: e.max(out=m8.ap, in_=work.ap[:, :ntok]), reads=[work], writes=[m8])
                        if r < cap // 8 - 1:
                            k.op("dve", lambda e: e.match_replace(out=work.ap[:, :ntok], in_to_replace=m8.ap,
                                                                  in_values=work.ap[:, :ntok], imm_value=-1.0),
                                 reads=[work, m8], writes=[work])
                    k.copy(thr, m8[:, 7:8])
                    k.ts(mask_e[:, :ntok], aff_e[cs], thr, ALU.is_ge)
                    k.memset(carry, 0.0)
                    for i in range(ntl):
                        n = t0 + i
                        pt = ptk.get()
                        k.tr(pt[:, 0:16], mask_e[:, i * 128:(i + 1) * 128], ident[0:16, 0:16])
                        k.copy(maskTb[:, i, :], pt[:, 0:16], e="act")
                        pc = ptk.get()
                        k.mm(pc[:, 0:16], triU_b, maskTb[:, i, :])
                        k.mm(pc[:, 16:32], ones_b, maskTb[:, i, :])
                        pos = pos_r.get()
                        k.tt(pos, pc[:, 0:16], carry, ALU.add)
                        k.tt(pos, pos, maskTb[:, i, :], ALU.mult)
                        k.ts(posm.sl(n, np.s_[:, n, :]), pos, -1.0, ALU.add)
                        k.tt(carry, carry, pc[:, 16:32], ALU.add)
            k.barrier()
            if stop_here("topk%d" % l):
                break
            with ExitStack() as s:
                mod5 = {st: load_mod(st, 5, s) for st in ([0, 1] if need_ctx else [1])}
                wsets = Rot([(k.sb([128, 8, 768], BF16, es=s), k.sb([128, 8, 768], BF16, es=s), k.sb([128, 6, D], BF16, es=s))
                             for _ in range(2)])
                stg = Rot([k.sb([128, D], F32, es=s) for _ in range(4)])
                Sel = k.sb([128, 32, 512], BF16, es=s)
                r4_r = Rot([k.sb([128, 4], F32, es=s) for _ in range(4)])
                idx_r = Rot([k.sb([128, 1], I32, es=s) for _ in range(6)])
                idf_r = Rot([k.sb([128, 1], F32, es=s) for _ in range(4)])
                gt_r = Rot([k.sb([128, 1], F32, es=s) for _ in range(6)])
                xs_r = Rot([k.sb([128, D], BF16, es=s) for _ in range(3)])
                xsT = k.sb([128, 8, 512], BF16, es=s)
                hid = k.sb([128, 6, 512], BF16, es=s)
                sg_r = Rot([k.sb([128, 512], F32, es=s) for _ in range(2)])
                ys_r = Rot([k.sb([128, D], F32, es=s) for _ in range(2)])
                p4 = Rot([k.ps([128, 512], F32, es=s) for _ in range(1)])
                ptb = Rot([k.ps([128, 8, 128], BF16, es=s) for _ in range(1)])
                pgu = Rot([k.ps([128, 512], F32, es=s) for _ in range(4)])
                pdn = Rot([k.ps([128, 512], F32, es=s) for _ in range(2)])
                Xall = [rows_key(X, n) for n in range(NTILE)] + [X.sub("all")]
                for e_ in range(16):
                    wg, wu, wd = wsets.get()
                    ci = 0
                    for kc in range(8):
                        load_cast(wg[:, kc, :], wg_d[l, e_, kc * 128:(kc + 1) * 128, :], stg, e="pool")
                        load_cast(wu[:, kc, :], wu_d[l, e_, kc * 128:(kc + 1) * 128, :], stg, e="pool")
                    for fc in range(6):
                        load_cast(wd[:, fc, :], wd_d[l, e_, fc * 128:(fc + 1) * 128, :], stg, e="pool")
                    for (t0, ntl, cap) in streams:
                        st = 0 if t0 == 0 else 1
                        for i in range(ntl):
                            n = t0 + i
                            k.ts(Sel[:, i, :cap], C("iota")[:, :cap], posm[:, n, e_:e_ + 1], ALU.is_equal)
                        stiles = [(s0, min(128, cap - s0)) for s0 in range(0, cap, 128)]
                        meta = []
                        for (s0, nsl) in stiles:
                            ps = p4.get()
                            for i in range(ntl):
                                k.mm(ps[0:nsl, 0:4], Sel[:, i, s0:s0 + nsl], Rtab[:, t0 + i, e_, :], start=(i == 0), stop=(i == ntl - 1))
                            r4 = r4_r.get()
                            k.copy(r4[0:nsl, :], ps[0:nsl, 0:4])
                            idf = idf_r.get()
                            k.stt(idf[0:nsl, :], r4[0:nsl, 0:1], 64.0, r4[0:nsl, 1:2], ALU.mult, ALU.add)
                            idx = idx_r.get()
                            k.copy(idx[0:nsl, :], idf[0:nsl, :])
                            gt = gt_r.get()
                            k.tt(gt[0:nsl, :], r4[0:nsl, 2:3], r4[0:nsl, 3:4], ALU.add)
                            meta.append((idx, gt))
                            xs = xs_r.get()
                            k.dma(xs[0:nsl, :], H2, q="pool", reads=[idx],
                                  fn=lambda en: en.indirect_dma_start(out=xs.ap[0:nsl, :], out_offset=None, in_=H2.ap,
                                                                      in_offset=bass.IndirectOffsetOnAxis(ap=idx.ap[0:nsl, :], axis=0)))
                            pt = ptb.get()
                            for kc in range(8):
                                k.tr(pt[:, kc, 0:nsl], xs[0:nsl, kc * 128:(kc + 1) * 128], ident_b[0:nsl, 0:nsl])
                            k.copy(xsT[:, :, s0:s0 + nsl], pt[:, :, 0:nsl], e="act")
                        for fc in range(6):
                            pg = pgu.get()
                            pu = pgu.get()
                            for kc in range(8):
                                k.mm(pg[:, :cap], wg[:, kc, fc * 128:(fc + 1) * 128], xsT[:, kc, :cap], start=(kc == 0), stop=(kc == 7))
                            for kc in range(8):
                                k.mm(pu[:, :cap], wu[:, kc, fc * 128:(fc + 1) * 128], xsT[:, kc, :cap], start=(kc == 0), stop=(kc == 7))
                            sg = sg_r.get()
                            k.act(sg[:, :cap], pg[:, :cap], AF.Silu)
                            k.tt(hid[:, fc, :cap], sg[:, :cap], pu[:, :cap], ALU.mult)
                        for si, (s0, nsl) in enumerate(stiles):
                            idx, gt = meta[si]
                            ys = ys_r.get()
                            for hf in range(2):
                                pd = pdn.get()
                                for fc in range(6):
                                    k.mm(pd[0:nsl, :], hid[:, fc, s0:s0 + nsl], wd[:, fc, hf * 512:(hf + 1) * 512], start=(fc == 0), stop=(fc == 5))
                                k.stt(ys[0:nsl, hf * 512:(hf + 1) * 512], pd[0:nsl, :], gt[0:nsl, 0:1],
                                      mod5[st][0:nsl, hf * 512:(hf + 1) * 512], ALU.mult, ALU.mult)
                            k.dma(X, ys[0:nsl, :], q="pool", reads=[ys, idx], writes=Xall,
                                  fn=lambda en: en.indirect_dma_start(out=X.ap, out_offset=bass.IndirectOffsetOnAxis(ap=idx.ap[0:nsl, :], axis=0),
                                                                      in_=ys.ap[0:nsl, :], in_offset=None, compute_op=ALU.add))
            k.barrier()
            if stop_here("exp%d" % l):
                break
        else:
            with ExitStack() as s:
                gfin = k.sb([128, D], F32, es=s)
                k.dma(gfin, g_bc_d[4])
                xt_r = Rot([k.sb([128, D], F32, es=s) for _ in range(3)])
                junk = k.sb([128, D], F32, es=s)
                ss_pool = Rot([k.sb([128, 1], F32, es=s) for _ in range(8)])
                for n in range(2, NTILE):
                    c0 = n * 128
                    xt = xt_r.get()
                    k.dma(xt, rows_key(X, n)[c0:c0 + 128, :])
                    ss = ss_pool.get()
                    k.act(junk, xt, AF.Square, accum=ss)
                    rstd = ss_pool.get()
                    k.rsq(rstd, ss, 1.0 / D, EPS)
                    k.stt(xt, xt, rstd, gfin, ALU.mult, ALU.mult)
                    k.dma(out_d.sl(n, np.s_[c0 - NTC:c0 - NTC + 128, :]), xt)
        k.barrier()
        print("ninst", k.ninst, k.cnt, flush=True)
    return nc


_NC_CACHE = {}


def kernel(**inputs):
    inp = {kk: np.asarray(v) for kk, v in inputs.items()}
    shared = _prep_shared(inp)
    if "nc" not in _NC_CACHE:
        _NC_CACHE["nc"] = build()
    nc = _NC_CACHE["nc"]
    in_maps = []
    for b in range(8):
        m = dict(shared)
        m.update(_prep_core(inp, b))
        in_maps.append(m)
    res = run_bass_kernel_spmd(nc, in_maps, core_ids=list(range(8)))
    out = np.stack([np.asarray(r["out"], dtype=np.float32) for r in res.results], 0)
    return out
```

```python
import numpy as np
from contextlib import ExitStack
import concourse.bass as bass
import concourse.mybir as mybir
from concourse.bass_utils import run_bass_kernel_spmd

F32 = mybir.dt.float32
BF16 = mybir.dt.bfloat16
I32 = mybir.dt.int32
U32 = mybir.dt.uint32
AF = mybir.ActivationFunctionType
ALU = mybir.AluOpType
AX = mybir.AxisListType


class T:
    __slots__ = ("ap", "key")

    def __init__(self, ap, key):
        self.ap = ap
        self.key = key

    def __getitem__(self, idx):
        return T(self.ap[idx], self.key)

    def sub(self, suffix):
        return T(self.ap, (self.key, suffix))

    def sl(self, suffix, idx):
        return T(self.ap[idx], (self.key, suffix))


def _key(x):
    return x.key if isinstance(x, T) else x


class KB:
    CE = ("pe", "act", "dve", "pool")

    def __init__(self, nc, es, nds=14):
        self.nc = nc
        self.es = es
        self.E = {"pe": nc.tensor, "act": nc.scalar, "dve": nc.vector,
                  "pool": nc.gpsimd, "sp": nc.sync}
        self.csem = {e: es.enter_context(nc.semaphore("c_" + e)) for e in self.CE}
        self.cnt = {e: 0 for e in self.CE}
        self.NDS = nds
        self.dsem = [es.enter_context(nc.semaphore("d%d" % i)) for i in range(nds)]
        self.dcnt = [0] * nds
        self.dnext = 0
        self.waited = {e: {} for e in self.E}
        self.lastw = {}
        self.readers = {}
        self.nalloc = 0
        self.ninst = 0

    def sb(self, shape, dtype, name=None, es=None):
        self.nalloc += 1
        name = name or ("t%d" % self.nalloc)
        t = (es or self.es).enter_context(self.nc.sbuf_tensor(name + "_%d" % self.nalloc, list(shape), dtype))
        return T(t[:], name + "_%d" % self.nalloc)

    def ps(self, shape, dtype, name=None, es=None):
        self.nalloc += 1
        name = name or ("p%d" % self.nalloc)
        t = (es or self.es).enter_context(self.nc.psum_tensor(name + "_%d" % self.nalloc, list(shape), dtype))
        return T(t[:], name + "_%d" % self.nalloc)

    def dram(self, name, shape, dtype, kind="Internal"):
        t = self.nc.dram_tensor(name, list(shape), dtype, kind=kind)
        return T(t.ap(), name)

    def _semobj(self, semkey):
        return self.csem[semkey[1]] if semkey[0] == "c" else self.dsem[semkey[1]]

    def _wait(self, e, semkey, val):
        if self.waited[e].get(semkey, 0) >= val:
            return
        self.waited[e][semkey] = val
        self.E[e].wait_ge(self._semobj(semkey), val)

    def _deps(self, e, reads, writes, is_dma):
        for r in reads:
            lw = self.lastw.get(_key(r))
            if lw is not None:
                self._wait(e, lw[0], lw[1])
        for w in writes:
            k = _key(w)
            lw = self.lastw.get(k)
            if lw is not None:
                if not (lw[0] == ("c", e) and e == "pe" and not is_dma):
                    self._wait(e, lw[0], lw[1])
            for sk, v in self.readers.get(k, {}).items():
                if sk == ("c", e) and e == "pe" and not is_dma:
                    continue
                self._wait(e, sk, v)

    def _record(self, tok, reads, writes):
        for r in reads:
            d = self.readers.setdefault(_key(r), {})
            if d.get(tok[0], 0) < tok[1]:
                d[tok[0]] = tok[1]
        for w in writes:
            k = _key(w)
            self.lastw[k] = tok
            self.readers[k] = {}

    def op(self, e, fn, reads=(), writes=()):
        self._deps(e, reads, writes, False)
        ins = fn(self.E[e])
        self.cnt[e] += 1
        ins.then_inc(self.csem[e], 1)
        self._record((("c", e), self.cnt[e]), reads, writes)
        self.ninst += 1
        return ins

    def dma(self, out, in_, q="sp", fn=None, reads=None, writes=None, **kw):
        reads = [in_] if reads is None else reads
        writes = [out] if writes is None else writes
        slot = self.dnext
        self.dnext = (slot + 1) % self.NDS
        if self.dcnt[slot] > 0:
            self._wait(q, ("d", slot), 16 * self.dcnt[slot])
        self._deps(q, reads, writes, True)
        if fn is None:
            ins = self.E[q].dma_start(out=out.ap, in_=in_.ap, **kw)
        else:
            ins = fn(self.E[q])
        self.dcnt[slot] += 1
        ins.then_inc(self.dsem[slot], 16)
        self._record((("d", slot), 16 * self.dcnt[slot]), reads, writes)
        self.ninst += 1
        return ins

    def barrier(self):
        for e in self.E:
            for e2 in self.CE:
                if e2 != e and self.cnt[e2] > 0:
                    self._wait(e, ("c", e2), self.cnt[e2])
            for s in range(self.NDS):
                if self.dcnt[s] > 0:
                    self._wait(e, ("d", s), 16 * self.dcnt[s])

    def mm(self, out, lhsT, rhs, start=True, stop=True, extra_reads=()):
        return self.op("pe", lambda e: e.matmul(out.ap, lhsT.ap, rhs.ap, start=start, stop=stop),
                       reads=[lhsT, rhs, *extra_reads], writes=[out])

    def tr(self, out, in_, ident):
        return self.op("pe", lambda e: e.transpose(out.ap, in_.ap, ident.ap),
                       reads=[in_, ident], writes=[out])

    def act(self, out, in_, func, bias=None, scale=None, accum=None, e="act"):
        kw = {}
        rd = [in_]
        wr = [out]
        if bias is not None:
            if isinstance(bias, T):
                kw["bias"] = bias.ap
                rd.append(bias)
            else:
                kw["bias"] = bias
        if scale is not None:
            if isinstance(scale, T):
                kw["scale"] = scale.ap
                rd.append(scale)
            else:
                kw["scale"] = scale
        if accum is not None:
            kw["accum_out"] = accum.ap
            wr.append(accum)
        return self.op(e, lambda en: en.activation(out.ap, in_.ap, func, **kw), reads=rd, writes=wr)

    def tt(self, out, a, b, op, e="dve"):
        return self.op(e, lambda en: en.tensor_tensor(out.ap, a.ap, b.ap, op), reads=[a, b], writes=[out])

    def ts(self, out, a, s1, op0, s2=None, op1=None, e="dve", accum=None):
        rd = [a]
        wr = [out]
        v1 = s1
        v2 = s2
        if isinstance(s1, T):
            rd.append(s1)
            v1 = s1.ap
        if isinstance(s2, T):
            rd.append(s2)
            v2 = s2.ap
        kw = {}
        if op1 is not None:
            kw["op1"] = op1
        if accum is not None:
            kw["accum_out"] = accum.ap
            wr.append(accum)
        return self.op(e, lambda en: en.tensor_scalar(out.ap, a.ap, v1, v2, op0, **kw), reads=rd, writes=wr)

    def stt(self, out, a, s, b, op0, op1, e="dve"):
        rd = [a, b]
        v = s
        if isinstance(s, T):
            rd.append(s)
            v = s.ap
        return self.op(e, lambda en: en.scalar_tensor_tensor(out.ap, a.ap, v, b.ap, op0, op1), reads=rd, writes=[out])

    def copy(self, out, in_, e="dve"):
        if e == "act":
            return self.op(e, lambda en: en.activation(out.ap, in_.ap, AF.Copy), reads=[in_], writes=[out])
        return self.op(e, lambda en: en.tensor_copy(out.ap, in_.ap), reads=[in_], writes=[out])

    def memset(self, out, v, e="dve"):
        return self.op(e, lambda en: en.memset(out.ap, v), reads=[], writes=[out])

    def rsq(self, dst, src, mul, add):
        self.ts(dst, src, mul, ALU.mult, add, ALU.add)
        self.act(dst, dst, AF.Sqrt)
        self.op("dve", lambda e: e.reciprocal(dst.ap, dst.ap), reads=[dst], writes=[dst])

L_ = 2
D = 1024
NTX = 4096
NTC = 256
NT = NTX + NTC
NTILE = NT // 128
NCOL = 2464
SCALE_MLA = float((64 + 32) ** -0.5)
LN2 = float(np.log(2.0))


def _rope_tables():
    t = np.arange(NTX)
    row = (t // 64).astype(np.float32)
    col = (t % 64).astype(np.float32)

    def tab(dh_half):
        dh = dh_half
        inv = 10000.0 ** (-np.arange(0, dh, 2, dtype=np.float32) / dh)
        return inv

    def build(dtot):
        half = dtot // 2
        inv = tab(half)
        nf = half // 2
        cos = np.zeros((dtot, NTX), np.float32)
        sins = np.zeros((dtot, NTX), np.float32)
        perm = np.zeros((dtot, dtot), np.float32)
        for part, pos in ((0, row), (1, col)):
            base = part * half
            ang = pos[None, :] * inv[:, None]
            c, s = np.cos(ang), np.sin(ang)
            for j in range(nf):
                cos[base + j] = c[j]
                cos[base + nf + j] = c[j]
                sins[base + j] = -s[j]
                sins[base + nf + j] = s[j]
                perm[base + nf + j, base + j] = 1.0
                perm[base + j, base + nf + j] = 1.0
        return cos, sins, perm

    c32, s32, p32 = build(32)
    c64, s64, p64 = build(64)
    c128 = np.concatenate([c64, c64], 0)
    s128 = np.concatenate([s64, s64], 0)
    p128 = np.zeros((128, 128), np.float32)
    p128[:64, :64] = p64
    p128[64:, 64:] = p64
    rope32 = np.stack([c32, s32]).astype(np.float32)
    rope128 = np.stack([c128, s128]).astype(np.float32)
    return rope32, rope128, p32, p128


_CST = {}


def _cst_layout():
    off = 0
    for name, w in (("ident", 128), ("A", 128), ("B", 128), ("C1", 128), ("C2", 128),
                    ("colK1", 1), ("colK2", 1), ("iota", 512), ("triU", 128),
                    ("mprev", 128), ("mnext", 128), ("p128", 128), ("p32", 32),
                    ("tokid", NTILE * 16 * 2)):
        _CST[name] = (off, w)
        off += w
    return off


NCST = _cst_layout()


def _const_table():
    rope32, rope128, p32, p128 = _rope_tables()
    cst = np.zeros((128, NCST), np.float32)
    p = np.arange(128, dtype=np.float32)[:, None]
    c = np.arange(128, dtype=np.float32)[None, :]

    def put(name, arr):
        o, w = _CST[name]
        cst[:arr.shape[0], o:o + w] = arr
    put("ident", np.eye(128, dtype=np.float32))
    put("A", np.maximum(c - p, 0.0))
    put("B", np.maximum(p - c, 0.0))
    put("C1", np.broadcast_to(c + 1.0, (128, 128)))
    put("C2", np.broadcast_to(128.0 - c, (128, 128)))
    put("colK1", 127.0 - p)
    put("colK2", p)
    put("iota", np.broadcast_to(np.arange(512, dtype=np.float32)[None, :], (128, 512)))
    put("triU", (p <= c).astype(np.float32))
    put("mprev", (p >= c).astype(np.float32))
    put("mnext", (p <= c).astype(np.float32))
    put("p128", p128)
    put("p32", p32)
    rows = (np.arange(NTILE)[None, :] * 128 + np.arange(128)[:, None])
    tok = np.stack([rows // 64, rows % 64], -1).astype(np.float32)
    tok = np.broadcast_to(tok[:, :, None, :], (128, NTILE, 16, 2)).reshape(128, -1)
    put("tokid", tok)
    return cst, rope32, rope128


def _prep_shared(inp):
    f = lambda a: np.ascontiguousarray(a, dtype=np.float32)
    sh = {}
    w_in = inp["w_in"]
    s = np.cumsum([0, 256, 256, 256, 256, 256, 128, 32, 256, 128, 128])
    rq, rk, rv, rg, cq, ckv, kr, wq, wk, wv = [w_in[:, :, s[i]:s[i + 1]] for i in range(10)]
    wk2 = np.concatenate([wk[:, :, 0:64], wk[:, :, 0:64], wk[:, :, 64:128], wk[:, :, 64:128]], -1)
    wv2 = np.concatenate([wv[:, :, 0:64], wv[:, :, 0:64], wv[:, :, 64:128], wv[:, :, 64:128]], -1)
    sh["w_in_r"] = f(np.concatenate([rq, rk, cq, ckv, wq, wk2, kr, rk, rv, rg, wv2], -1))
    assert sh["w_in_r"].shape[-1] == NCOL
    uq = inp["mla_w_uq"]
    sh["w_uq_n"] = f(uq[:, :, :, :64].reshape(L_, 256, 512))
    sh["w_uq_r"] = f(uq[:, :, :, 64:].reshape(L_, 256, 256))
    uk = inp["mla_w_uk"]
    ukT = uk.transpose(0, 2, 3, 1).reshape(L_, 4, 2, 64, 128)
    sh["w_ukT"] = f(ukT.transpose(0, 2, 3, 1, 4).reshape(L_, 128, 4, 128))
    sh["w_uv"] = f(inp["mla_w_uv"].reshape(L_, 128, 512))
    sh["w_out"] = f(inp["w_out"])
    sh["router_w"] = f(inp["router_w"])
    sh["ada_w"] = f(inp["ada_w"])
    sh["ada_b"] = f(inp["ada_b"].reshape(L_, 1, 6 * D))
    sh["exp_wg"] = f(inp["exp_w_gate"])
    sh["exp_wu"] = f(inp["exp_w_up"])
    sh["exp_wd"] = f(inp["exp_w_down"])
    gb = np.stack([inp["norm1_g"][0], inp["norm2_g"][0], inp["norm1_g"][1], inp["norm2_g"][1], inp["final_g"]])
    sh["g_bc"] = f(np.broadcast_to(gb[:, None, :], (5, 128, D)))
    cols = []
    for l in range(L_):
        qg = inp["mla_qnorm_g"][l].reshape(2, 128).T
        kg = inp["mla_kvnorm_g"][l].reshape(1, 128).T
        df, db, sk = inp["ret_decay_f"][l], inp["ret_decay_b"][l], inp["win_sink"][l]
        rep = np.broadcast_to(np.concatenate([df, db, sk])[None, :], (128, 12))
        hp = (np.arange(128) >= 64).astype(np.int64)
        pp = np.stack([df[0 + hp], df[2 + hp], db[0 + hp], db[2 + hp]], -1)
        cols += [qg, kg, rep, pp]
    sh["small"] = f(np.concatenate(cols, -1))
    cst, rope32, rope128 = _const_table()
    sh["cst"] = cst
    sh["rope32"] = rope32
    sh["rope128"] = rope128
    return sh


def _prep_core(inp, b):
    f = lambda a: np.ascontiguousarray(a, dtype=np.float32)
    d = {}
    d["x0"] = f(np.concatenate([inp["ctx"][b], inp["x"][b]], 0))
    cv = np.stack([inp["c_ctx"], inp["c"][b]])
    cr = cv.reshape(2, 8, 128).transpose(0, 2, 1)
    d["crep"] = f(np.broadcast_to(cr[:, :, :, None], (2, 128, 8, 128)))
    return d

class Rot:
    def __init__(self, tiles):
        self.t = tiles
        self.i = 0

    def get(self):
        t = self.t[self.i % len(self.t)]
        self.i += 1
        return t


def TD(t, pattern, **kw):
    return T(t.ap.rearrange(pattern, **kw), t.key)


def build(upto=None, dbg=False, nlayers=L_):
    nc = bass.Bass("TRN2", target_bir_lowering=False)
    es0 = ExitStack()
    with es0:
        k = KB(nc, es0)
        kind_dbg = "ExternalOutput" if dbg else "Internal"
        din = lambda n, s, dt=F32: k.dram(n, s, dt, kind="ExternalInput")
        x0 = din("x0", [NT, D])
        crep_d = din("crep", [2, 128, 8, 128])
        w_in_d = din("w_in_r", [L_, D, NCOL])
        w_uq_n_d = din("w_uq_n", [L_, 256, 512])
        w_uq_r_d = din("w_uq_r", [L_, 256, 256])
        w_ukT_d = din("w_ukT", [L_, 128, 4, 128])
        w_uv_d = din("w_uv", [L_, 128, 512])
        w_out_d = din("w_out", [L_, D, D])
        router_d = din("router_w", [L_, D, 16])
        ada_w_d = din("ada_w", [L_, D, 6 * D])
        ada_b_d = din("ada_b", [L_, 1, 6 * D])
        wg_d = din("exp_wg", [L_, 16, D, 768])
        wu_d = din("exp_wu", [L_, 16, D, 768])
        wd_d = din("exp_wd", [L_, 16, 768, D])
        g_bc_d = din("g_bc", [5, 128, D])
        small_d = din("small", [128, L_ * 19])
        cst_d = din("cst", [128, NCST])
        rope32_d = din("rope32", [2, 32, NTX])
        rope128_d = din("rope128", [2, 128, NTX])
        out_d = k.dram("out", [NTX, D], F32, kind="ExternalOutput")
        X = k.dram("X", [NT, D], F32, kind=kind_dbg)
        MODBC = k.dram("MODBC", [2, 6, 128, D], F32, kind=kind_dbg)
        QT = k.dram("QT", [2, 128, NT], BF16, kind=kind_dbg)
        KT = k.dram("KT", [2, 128, NT], BF16, kind=kind_dbg)
        TMo = k.dram("TMo", [NT, 1024], BF16, kind=kind_dbg)
        QL = k.dram("QL", [8, 128, NT], BF16, kind=kind_dbg)
        QR = k.dram("QR", [8, 32, NT], BF16, kind=kind_dbg)
        KVT = k.dram("KVT", [128, NT], BF16, kind=kind_dbg)
        KRT = k.dram("KRT", [32, NT], BF16, kind=kind_dbg)
        VP = k.dram("VP", [NT, 512], BF16, kind=kind_dbg)
        WQT = k.dram("WQT", [2, 128, NT], BF16, kind=kind_dbg)
        WKT = k.dram("WKT", [2, 128, NT], BF16, kind=kind_dbg)
        YT = k.dram("YT", [8, 128, NT], BF16, kind=kind_dbg)
        H2 = k.dram("H2", [NT, D], BF16, kind=kind_dbg)

        cst = k.sb([128, NCST], F32, "cst")
        k.dma(cst, cst_d)
        small = k.sb([128, L_ * 19], F32, "small")
        k.dma(small, small_d)

        def C(name, rows=128):
            o, w = _CST[name]
            return cst[0:rows, o:o + w]
        ident = C("ident")
        ones_f = k.sb([128, 128], F32, "ones_f")
        k.memset(ones_f, 1.0)
        ones_b = k.sb([128, 128], BF16, "ones_b")
        k.memset(ones_b, 1.0)
        ident_b = k.sb([128, 128], BF16, "ident_b")
        k.copy(ident_b, ident)
        triU_b = k.sb([128, 128], BF16, "triU_b")
        k.copy(triU_b, C("triU"))
        mprev_b = k.sb([128, 128], BF16, "mprev_b")
        k.copy(mprev_b, C("mprev"))
        mnext_b = k.sb([128, 128], BF16, "mnext_b")
        k.copy(mnext_b, C("mnext"))
        Rtab = k.sb([128, NTILE, 16, 4], BF16, "Rtab")
        o_tok, w_tok = _CST["tokid"]
        k.copy(Rtab[:, :, :, 0:2], T(cst.ap[:, o_tok:o_tok + w_tok].rearrange("p (n e c) -> p n e c", n=NTILE, e=16), cst.key))
        affT = k.sb([128, NTILE, 16], F32, "affT")
        posm = k.sb([128, NTILE, 16], F32, "posm")

        EPS = 1e-6
        blocks = [(0, 256, 0)] + [(256 + 512 * i, 512, 1) for i in range(8)]

        def rows_key(t, n):
            return t.sub(("r", n))

        def stop_here(name):
            return upto is not None and upto == name

        def rstd_from_ss(ss, n_feat, es, eps=EPS):
            r = k.sb([128, 1], F32, es=es)
            k.rsq(r, ss, 1.0 / n_feat, eps)
            return r

        for l in range(nlayers):
            need_ctx = l < L_ - 1
            sm0 = l * 19
            Xsrc = x0 if l == 0 else X
            with ExitStack() as s:
                crep = k.sb([128, 2, 8, 128], F32, es=s)
                for st in range(2):
                    k.dma(crep[:, st], crep_d[st])
                sil = k.sb([128, 2, 8, 128], F32, es=s)
                k.act(sil, crep, AF.Silu)
                wb = Rot([k.sb([128, 8, 512], F32, es=s) for _ in range(2)])
                br = Rot([k.sb([1, 512], F32, es=s) for _ in range(2)])
                pp = Rot([k.ps([128, 512], F32, es=s) for _ in range(4)])
                ob = Rot([k.sb([128, 512], F32, es=s) for _ in range(4)])
                aw = TD(ada_w_d[l], "(kc p) n -> p kc n", p=128)
                for nb in range(12):
                    w = wb.get()
                    k.dma(w, aw[:, :, nb * 512:(nb + 1) * 512])
                    b_ = br.get()
                    k.dma(b_, ada_b_d[l, :, nb * 512:(nb + 1) * 512])
                    for st in range(2):
                        ps = pp.get()
                        for kc in range(8):
                            k.mm(ps, sil[:, st, kc, :], w[:, kc, :], start=(kc == 0), stop=False)
                        k.mm(ps, ones_f[0:1, :], b_, start=False, stop=True)
                        o = ob.get()
                        k.copy(o, ps, e=("act" if st == 0 else "dve"))
                        j, half = nb // 2, nb % 2
                        k.dma(MODBC.sl((st, j), np.s_[st, j, :, half * 512:(half + 1) * 512]), o)
            k.barrier()
            if stop_here("mod%d" % l):
                break

            def load_mod(st, j, es, eng_q="sp"):
                t = k.sb([128, D], F32, es=es)
                k.dma(t, MODBC.sl((st, j), np.s_[st, j]))
                return t

            def load_gs(st, j_scale, gidx, es):
                sc = load_mod(st, j_scale, es)
                gb = k.sb([128, D], F32, es=es)
                k.dma(gb, g_bc_d[gidx])
                k.stt(sc, sc, 1.0, gb, ALU.add, ALU.mult)
                return sc

            def load_cast(dst, src, stg, e="pool"):
                t = stg.get()
                sh = list(src.ap.shape)
                tv = t[0:sh[0], 0:sh[1]]
                k.dma(tv, src)
                k.copy(dst, tv, e=e)

            with ExitStack() as s:
                w_in = k.sb([128, 8, NCOL], BF16, es=s)
                stg = Rot([k.sb([128, NCOL], F32, es=s) for _ in range(2)])
                for kc in range(8):
                    load_cast(w_in[:, kc, :], w_in_d[l, kc * 128:(kc + 1) * 128, :], stg, e=("pool" if kc % 2 else "dve"))
                w_uq_n = k.sb([128, 2, 512], BF16, es=s)
                w_uq_r = k.sb([128, 2, 256], BF16, es=s)
                for kc in range(2):
                    load_cast(w_uq_n[:, kc, :], w_uq_n_d[l, kc * 128:(kc + 1) * 128, :], stg)
                    load_cast(w_uq_r[:, kc, :], w_uq_r_d[l, kc * 128:(kc + 1) * 128, :], stg)
                w_ukT = k.sb([128, 4, 128], BF16, es=s)
                load_cast(T(w_ukT.ap.rearrange("p a r -> p (a r)"), w_ukT.key), TD(w_ukT_d[l], "p a r -> p (a r)"), stg)
                w_uv = k.sb([128, 512], BF16, es=s)
                load_cast(w_uv, w_uv_d[l], stg)
                gs1 = [load_gs(st, 1, l * 2 + 0, s) for st in range(2)]
                sh1 = [load_mod(st, 0, s) for st in range(2)]
                qg = small[:, sm0 + 0:sm0 + 2]
                kg = small[:, sm0 + 2:sm0 + 3]
                xt_r = Rot([k.sb([128, D], F32, es=s) for _ in range(2)])
                xs_r = Rot([k.sb([128, D], F32, es=s) for _ in range(2)])
                junk = k.sb([128, D], F32, es=s)
                ss_pool = Rot([k.sb([128, 1], F32, es=s) for _ in range(8)])
                hT_r = Rot([k.sb([128, 8, 512], BF16, es=s) for _ in range(2)])
                ptr = Rot([k.ps([128, 4, 128], F32, es=s) for _ in range(2)])
                pfm = Rot([k.ps([128, 512], F32, es=s) for _ in range(3)])
                pex = Rot([k.ps([128, 512], F32, es=s) for _ in range(3)])
                ob16 = Rot([k.sb([128, 512], BF16, es=s) for _ in range(6)])
                f32t = Rot([k.sb([128, 512], F32, es=s) for _ in range(6)])
                rp32 = Rot([k.sb([32, 2, 512], F32, es=s) for _ in range(2)])
                rp128 = Rot([k.sb([128, 2, 512], F32, es=s) for _ in range(2)])
                cqg = k.sb([128, 2, 512], BF16, es=s)
                rstdq = k.sb([128, 512], F32, es=s)
                kvT_sb = k.sb([128, 512], BF16, es=s)
                tmo = Rot([k.sb([128, 1024], BF16, es=s) for _ in range(2)])

                def bc_rstd(dst, sq_list, nfeat, n):
                    ps = pex.get()
                    for i, sq in enumerate(sq_list):
                        k.mm(ps[:, :n], ones_f, sq, start=(i == 0), stop=(i == len(sq_list) - 1))
                    k.rsq(dst[:, :n], ps[:, :n], 1.0 / nfeat, EPS)

                def rope_apply(src_f32, M, n, tabs, perm, out_bf):
                    ps = pex.get()
                    k.mm(ps[0:M, :n], perm, src_f32[0:M, :n])
                    t1 = f32t.get()
                    k.tt(t1[0:M, :n], src_f32[0:M, :n], tabs[0:M, 0, :n], ALU.mult)
                    t2 = f32t.get()
                    k.tt(t2[0:M, :n], ps[0:M, :n], tabs[0:M, 1, :n], ALU.mult)
                    k.tt(out_bf[0:M, :n], t1[0:M, :n], t2[0:M, :n], ALU.add, e="pool")

                for (r0, n, st) in blocks:
                    nt = n // 128
                    hT = hT_r.get()
                    if st == 1:
                        t0 = r0 - NTC
                        r32 = rp32.get()
                        k.dma(r32[:, :, :n], TD(rope32_d, "a p t -> p a t")[:, :, t0:t0 + n])
                        r128 = rp128.get()
                        k.dma(r128[:, :, :n], TD(rope128_d, "a p t -> p a t")[:, :, t0:t0 + n])
                    for ti in range(nt):
                        rr = r0 + ti * 128
                        xt = xt_r.get()
                        k.dma(xt, rows_key(Xsrc, rr // 128)[rr:rr + 128, :])
                        ss = ss_pool.get()
                        k.act(junk, xt, AF.Square, accum=ss)
                        rstd = ss_pool.get()
                        k.rsq(rstd, ss, 1.0 / D, EPS)
                        xs = xs_r.get()
                        k.ts(xs, xt, rstd, ALU.mult)
                        k.tt(xs, xs, gs1[st], ALU.mult, e="pool")
                        k.tt(xs, xs, sh1[st], ALU.add, e="pool")
                        for hf in range(2):
                            pt = ptr.get()
                            for q4 in range(4):
                                kc = hf * 4 + q4
                                k.tr(pt[:, q4, :], xs[:, kc * 128:(kc + 1) * 128], ident)
                            k.copy(hT[:, hf * 4:(hf + 1) * 4, ti * 128:(ti + 1) * 128], pt, e="act")
                    def fm(c, M=128):
                        ps = pfm.get()
                        for kc in range(8):
                            k.mm(ps[0:M, :n], w_in[:, kc, c * 128:c * 128 + M], hT[:, kc, :n], start=(kc == 0), stop=(kc == 7))
                        return ps
                    for c in range(4):
                        ps = fm(c)
                        o = ob16.get()
                        k.copy(o[:, :n], ps[:, :n], e=("act" if c % 2 == 0 else "dve"))
                        dst = (QT if c < 2 else KT)
                        k.dma(dst.sl((c % 2, r0), np.s_[c % 2, :, r0:r0 + n]), o[:, :n])
                    sqs = []
                    for c2 in range(2):
                        ps = fm(4 + c2)
                        sq = f32t.get()
                        k.act(sq[:, :n], ps[:, :n], AF.Square)
                        sqs.append(sq[:, :n])
                        k.act(cqg[:, c2, :n], ps[:, :n], AF.Copy, scale=qg[:, c2:c2 + 1])
                    bc_rstd(rstdq, sqs, 256, n)
                    for pr in range(4):
                        ps = pex.get()
                        for c2 in range(2):
                            k.mm(ps[:, :n], w_uq_n[:, c2, pr * 128:(pr + 1) * 128], cqg[:, c2, :n], start=(c2 == 0), stop=(c2 == 1))
                        qn = ob16.get()
                        k.tt(qn[:, :n], ps[:, :n], rstdq[:, :n], ALU.mult)
                        for hh in range(2):
                            h = pr * 2 + hh
                            off = hh * 64
                            ps2 = pex.get()
                            k.mm(ps2[:, :n], w_ukT[off:off + 64, pr, :], qn[off:off + 64, :n])
                            o = ob16.get()
                            k.copy(o[:, :n], ps2[:, :n], e="act")
                            k.dma(QL.sl((h, r0), np.s_[h, :, r0:r0 + n]), o[:, :n])
                    for h in range(8):
                        ps = pex.get()
                        for c2 in range(2):
                            k.mm(ps[0:32, :n], w_uq_r[:, c2, h * 32:(h + 1) * 32], cqg[:, c2, :n], start=(c2 == 0), stop=(c2 == 1))
                        qr = f32t.get()
                        k.tt(qr[0:32, :n], ps[0:32, :n], rstdq[0:32, :n], ALU.mult)
                        o = ob16.get()
                        if st == 1:
                            rope_apply(qr, 32, n, r32, C("p32", 32), o)
                        else:
                            k.copy(o[0:32, :n], qr[0:32, :n], e="pool")
                        k.dma(QR.sl((h, r0), np.s_[h, :, r0:r0 + n]), o[0:32, :n])
                    ps = fm(6)
                    sq = f32t.get()
                    k.act(sq[:, :n], ps[:, :n], AF.Square)
                    kvg = f32t.get()
                    k.act(kvg[:, :n], ps[:, :n], AF.Copy, scale=kg[:, 0:1])
                    rk_ = f32t.get()
                    bc_rstd(rk_, [sq[:, :n]], 128, n)
                    k.tt(kvT_sb[:, :n], kvg[:, :n], rk_[:, :n], ALU.mult)
                    k.dma(KVT.sl(r0, np.s_[:, r0:r0 + n]), kvT_sb[:, :n])
                    for ti in range(nt):
                        ps = pex.get()
                        k.mm(ps, kvT_sb[:, ti * 128:(ti + 1) * 128], w_uv)
                        o = ob16.get()
                        k.copy(o, ps, e="act")
                        rr = r0 + ti * 128
                        k.dma(VP.sl(rr // 128, np.s_[rr:rr + 128, :]), o)
                    for c in range(4):
                        ps = fm(7 + c)
                        o = ob16.get()
                        if st == 1:
                            sf = f32t.get()
                            k.copy(sf[:, :n], ps[:, :n], e="act")
                            rope_apply(sf, 128, n, r128, C("p128"), o)
                        else:
                            k.copy(o[:, :n], ps[:, :n], e="act")
                        dst = (WQT if c < 2 else WKT)
                        k.dma(dst.sl((c % 2, r0), np.s_[c % 2, :, r0:r0 + n]), o[:, :n])
                    ps = fm(11, 32)
                    o = ob16.get()
                    if st == 1:
                        sf = f32t.get()
                        k.copy(sf[0:32, :n], ps[0:32, :n], e="act")
                        rope_apply(sf, 32, n, r32, C("p32", 32), o)
                    else:
                        k.copy(o[0:32, :n], ps[0:32, :n], e="act")
                    k.dma(KRT.sl(r0, np.s_[:, r0:r0 + n]), o[0:32, :n])
                    for ti in range(nt):
                        o = tmo.get()
                        for hf in range(2):
                            ps = pfm.get()
                            c0 = 1440 + hf * 512
                            for kc in range(8):
                                k.mm(ps, hT[:, kc, ti * 128:(ti + 1) * 128], w_in[:, kc, c0:c0 + 512], start=(kc == 0), stop=(kc == 7))
                            k.copy(o[:, hf * 512:(hf + 1) * 512], ps, e=("act" if hf == 0 else "dve"))
                        rr = r0 + ti * 128
                        k.dma(TMo.sl(rr // 128, np.s_[rr:rr + 128, :]), o)
            k.barrier()
            if stop_here("s1_%d" % l):
                break
            with ExitStack() as s:
                lg_rep = k.sb([128, 8], F32, es=s)
                lg_pp = k.sb([128, 4], F32, es=s)
                for dst, src in ((lg_rep, small[:, sm0 + 3:sm0 + 11]), (lg_pp, small[:, sm0 + 15:sm0 + 19])):
                    k.act(dst, src, AF.Exp, scale=LN2)
                    k.ts(dst, dst, -1.0, ALU.mult, 1.0, ALU.add)
                    k.act(dst, dst, AF.Ln)
                LN8 = float(np.log(0.125))
                DT = k.sb([128, 4, 128], F32, es=s)
                tmpA = k.sb([128, 128], F32, es=s)
                for h in range(4):
                    k.ts(tmpA, C("A"), lg_rep[:, h:h + 1], ALU.mult)
                    k.stt(tmpA, C("B"), lg_rep[:, 4 + h:5 + h], tmpA, ALU.mult, ALU.add)
                    k.act(DT[:, h, :], tmpA, AF.Exp)
                    k.ts(DT[:, h, :], DT[:, h, :], 0.125, ALU.mult)
                QWF = k.sb([128, 2, 128], F32, es=s)
                QWB = k.sb([128, 2, 128], F32, es=s)
                gch = k.sb([128, 4], F32, es=s)
                for pr in range(2):
                    k.act(QWF[:, pr, :], C("C1"), AF.Exp, scale=lg_pp[:, pr:pr + 1])
                    k.act(QWB[:, pr, :], C("C2"), AF.Exp, scale=lg_pp[:, 2 + pr:3 + pr])
                k.act(gch, lg_pp, AF.Exp, scale=128.0)
                KWF = k.sb([128, 256], F32, es=s)
                KWB = k.sb([128, 256], F32, es=s)
                kcol = k.sb([128, 8], F32, es=s)
                for h in range(4):
                    k.act(kcol[:, h:h + 1], C("colK1"), AF.Exp, scale=lg_rep[:, h:h + 1])
                    k.act(kcol[:, 4 + h:5 + h], C("colK2"), AF.Exp, scale=lg_rep[:, 4 + h:5 + h])
                k.ts(kcol, kcol, 0.125, ALU.mult)
                for h in range(4):
                    k.copy(KWF[:, h * 64:(h + 1) * 64], T(kcol.ap[:, h:h + 1].to_broadcast([128, 64]), kcol.key))
                    k.copy(KWB[:, h * 64:(h + 1) * 64], T(kcol.ap[:, 4 + h:5 + h].to_broadcast([128, 64]), kcol.key))
                KVd = k.sb([128, NTILE, 4, 64], F32, es=s)
                Sfb = k.sb([128, NTILE, 2, 64], BF16, es=s)
                Sbb = k.sb([128, NTILE, 2, 64], BF16, es=s)
                kv_r = Rot([k.sb([128, 512], BF16, es=s) for _ in range(3)])
                kw_r = Rot([k.sb([128, 2, 256], BF16, es=s) for _ in range(2)])
                pkv = Rot([k.ps([128, 4, 128], F32, es=s) for _ in range(2)])
                for n in range(NTILE):
                    kvt = kv_r.get()
                    k.dma(kvt, TMo.sl(n, np.s_[n * 128:(n + 1) * 128, 0:512]))
                    kw = kw_r.get()
                    k.tt(kw[:, 0, :], kvt[:, 0:256], KWF, ALU.mult)
                    k.tt(kw[:, 1, :], kvt[:, 0:256], KWB, ALU.mult, e="pool")
                    ps = pkv.get()
                    for d_ in range(2):
                        for pr in range(2):
                            k.mm(ps[:, d_ * 2 + pr, :], kw[:, d_, pr * 128:(pr + 1) * 128], kvt[:, 256 + pr * 128:256 + (pr + 1) * 128])
                    k.copy(KVd.sl(n, np.s_[0:64, n, :, :]), ps[0:64, :, 0:64], e="act")
                    k.copy(KVd.sl(n, np.s_[64:128, n, :, :]), ps[64:128, :, 64:128], e="act")
                for d_, (order, Sb_, eng) in enumerate((((list(range(NTILE))), Sfb, "dve"),
                                                       ([1, 0] + list(range(NTILE - 1, 1, -1)), Sbb, "pool"))):
                    cur = k.sb([128, 2, 64], F32, es=s)
                    k.memset(cur, 0.0, e=eng)
                    for n in order:
                        k.copy(Sb_.sl(n, np.s_[:, n, :, :]), cur, e=eng)
                        for pr in range(2):
                            if eng == "dve":
                                k.stt(cur[:, pr, :], cur[:, pr, :], gch[:, d_ * 2 + pr:d_ * 2 + pr + 1],
                                      KVd.sl(n, np.s_[:, n, d_ * 2 + pr, :]), ALU.mult, ALU.add, e=eng)
                            else:
                                k.ts(cur[:, pr, :], cur[:, pr, :], gch[:, d_ * 2 + pr:d_ * 2 + pr + 1], ALU.mult, e=eng)
                                k.tt(cur[:, pr, :], cur[:, pr, :], KVd.sl(n, np.s_[:, n, d_ * 2 + pr, :]), ALU.add, e=eng)
                qk_r = Rot([k.sb([128, 2, 2, 128], BF16, es=s) for _ in range(2)])
                vg_r = Rot([k.sb([128, 512], BF16, es=s) for _ in range(2)])
                qw_r = Rot([k.sb([128, 2, 2, 128], BF16, es=s) for _ in range(2)])
                sm_r = Rot([k.sb([128, 128], BF16, es=s) for _ in range(4)])
                pss = Rot([k.ps([128, 512], F32, es=s) for _ in range(2)])
                psy = Rot([k.ps([128, 512], F32, es=s) for _ in range(2)])
                pst = Rot([k.ps([128, 512], F32, es=s) for _ in range(2)])
                ysb_r = Rot([k.sb([128, 256], F32, es=s) for _ in range(2)])
                sq_r = Rot([k.sb([128, 256], F32, es=s) for _ in range(2)])
                st_r = Rot([k.sb([128, 16], F32, es=s) for _ in range(2)])
                sg_r = Rot([k.sb([128, 256], F32, es=s) for _ in range(2)])
                yo_r = Rot([k.sb([128, 2, 128], BF16, es=s) for _ in range(2)])
                for n in range(0 if need_ctx else 2, NTILE):
                    c0 = n * 128
                    qk = qk_r.get()
                    k.dma(qk[:, 0], TD(QT, "c p t -> p c t")[:, :, c0:c0 + 128])
                    k.dma(qk[:, 1], TD(KT, "c p t -> p c t")[:, :, c0:c0 + 128])
                    vg = vg_r.get()
                    k.dma(vg, TMo.sl(n, np.s_[c0:c0 + 128, 256:768]))
                    qw = qw_r.get()
                    k.tt(qw[:, 0], qk[:, 0], QWF, ALU.mult)
                    k.tt(qw[:, 1], qk[:, 0], QWB, ALU.mult, e="pool")
                    py = psy.get()
                    for h in range(4):
                        pr, off = h // 2, (h % 2) * 64
                        ps = pss.get()
                        k.mm(ps[:, 0:128], qk[off:off + 64, 1, pr, :], qk[off:off + 64, 0, pr, :])
                        sm = sm_r.get()
                        k.tt(sm, ps[:, 0:128], DT[:, h, :], ALU.mult)
                        yo_ = py[:, h * 64:(h + 1) * 64]
                        k.mm(yo_, sm, vg[:, h * 64:(h + 1) * 64], start=True, stop=False)
                        k.mm(yo_, qw[off:off + 64, 0, pr, :], Sfb.sl(n, np.s_[off:off + 64, n, pr, :]), start=False, stop=False)
                        k.mm(yo_, qw[off:off + 64, 1, pr, :], Sbb.sl(n, np.s_[off:off + 64, n, pr, :]), start=False, stop=True)
                    ysb = ysb_r.get()
                    k.copy(ysb, py[:, 0:256], e="act")
                    stt_ = st_r.get()
                    k.op("dve", lambda e: e.reduce_sum(stt_.ap[:, 0:4], ysb.ap.rearrange("p (h d) -> p h d", h=4), AX.X), reads=[ysb], writes=[stt_])
                    sq = sq_r.get()
                    k.tt(sq, ysb, ysb, ALU.mult, e="pool")
                    k.op("dve", lambda e: e.reduce_sum(stt_.ap[:, 4:8], sq.ap.rearrange("p (h d) -> p h d", h=4), AX.X), reads=[sq], writes=[stt_])
                    k.ts(stt_[:, 0:8], stt_[:, 0:8], 1.0 / 64, ALU.mult)
                    k.tt(stt_[:, 8:12], stt_[:, 0:4], stt_[:, 0:4], ALU.mult)
                    k.tt(stt_[:, 12:16], stt_[:, 4:8], stt_[:, 8:12], ALU.subtract)
                    k.rsq(stt_[:, 12:16], stt_[:, 12:16], 1.0, 1e-5)
                    sg = sg_r.get()
                    k.act(sg, vg[:, 256:512], AF.Silu)
                    for h in range(4):
                        k.ts(ysb[:, h * 64:(h + 1) * 64], ysb[:, h * 64:(h + 1) * 64], stt_[:, h:h + 1], ALU.subtract,
                             stt_[:, 12 + h:13 + h], ALU.mult)
                    k.tt(ysb, ysb, sg, ALU.mult, e="pool")
                    pt = pst.get()
                    for c in range(2):
                        k.tr(pt[:, c * 128:(c + 1) * 128], ysb[:, c * 128:(c + 1) * 128], ident)
                    yo = yo_r.get()
                    k.copy(T(yo.ap.rearrange("p c t -> p (c t)"), yo.key), pt[:, 0:256], e="act")
                    k.dma(TD(YT, "c p t -> p c t").sl(("ret", n), np.s_[:, 0:2, c0:c0 + 128]), yo)
            k.barrier()
            if stop_here("ret%d" % l):
                break
            with ExitStack() as s:
                kvT = k.sb([128, NT], BF16, es=s)
                k.dma(kvT, KVT)
                krT = k.sb([32, NT], BF16, es=s)
                k.dma(krT, KRT)
                vp = k.sb([128, NTILE, 512], BF16, es=s)
                for i in range(2):
                    k.dma(vp[:, i * 17:(i + 1) * 17, :], TD(VP, "(t p) c -> p t c", p=128)[:, i * 17:(i + 1) * 17, :])
                ql_r = Rot([k.sb([128, 512], BF16, es=s) for _ in range(3)])
                qr_r = Rot([k.sb([32, 512], BF16, es=s) for _ in range(3)])
                pT_r = Rot([k.sb([128, 512], BF16, es=s) for _ in range(5)])
                pss = Rot([k.ps([128, 512], F32, es=s) for _ in range(4)])
                pacc = Rot([k.ps([128, 512], F32, es=s) for _ in range(4)])
                rd_r = Rot([k.sb([128, 512], F32, es=s) for _ in range(2)])
                yp_r = Rot([k.sb([128, 512], BF16, es=s) for _ in range(3)])
                qblocks = ([(0, 256, [0, 1])] if need_ctx else []) + [(256 + 512 * i, 512, list(range(NTILE))) for i in range(8)]
                LOOK = 2
                for (r0, n, kts) in qblocks:
                    pend = []

                    def emit_pv(item):
                        h, i, kt, pT, pv, dn, yp = item
                        last = (i == len(kts) - 1)
                        k.mm(pv[:, :n], vp[:, kt, (h // 2) * 128:(h // 2 + 1) * 128], pT[:, :n], start=(i == 0), stop=last)
                        k.mm(dn[:, :n], ones_b, pT[:, :n], start=(i == 0), stop=last)
                        if last:
                            off = (h % 2) * 64
                            rd = rd_r.get()
                            k.op("dve", lambda e: e.reciprocal(rd.ap[off:off + 64, :n], dn.ap[off:off + 64, :n]), reads=[dn], writes=[rd])
                            k.tt(yp[off:off + 64, :n], pv[off:off + 64, :n], rd[off:off + 64, :n], ALU.mult)
                            if h % 2 == 1:
                                k.dma(YT.sl(("mla", h // 2, r0), np.s_[2 + h // 2, :, r0:r0 + n]), yp[:, :n])
                    yp = None
                    for h in range(8):
                        ql = ql_r.get()
                        k.dma(ql[:, :n], QL[h, :, r0:r0 + n])
                        qr = qr_r.get()
                        k.dma(qr[:, :n], QR[h, :, r0:r0 + n])
                        pv = pacc.get()
                        dn = pacc.get()
                        if h % 2 == 0:
                            yp = yp_r.get()
                        for i, kt in enumerate(kts):
                            ps = pss.get()
                            k.mm(ps[:, :n], kvT[:, kt * 128:(kt + 1) * 128], ql[:, :n], start=True, stop=False)
                            k.mm(ps[:, :n], krT[:, kt * 128:(kt + 1) * 128], qr[:, :n], start=False, stop=True)
                            pT = pT_r.get()
                            k.act(pT[:, :n], ps[:, :n], AF.Exp, scale=SCALE_MLA)
                            pend.append((h, i, kt, pT, pv, dn, yp))
                            if len(pend) > LOOK:
                                emit_pv(pend.pop(0))
                    while pend:
                        emit_pv(pend.pop(0))
            k.barrier()
            if stop_here("mla%d" % l):
                break
            with ExitStack() as s:
                wkT = k.sb([128, 2, NT], BF16, es=s)
                k.dma(wkT, TD(WKT, "c p t -> p c t"))
                wqT = k.sb([128, 2, NT], BF16, es=s)
                k.dma(wqT, TD(WQT, "c p t -> p c t"))
                wv2 = k.sb([128, NTILE, 256], BF16, es=s)
                k.dma(wv2, TD(TMo, "(t p) c -> p t c", p=128)[:, :, 768:1024])
                esink = k.sb([128, 4], F32, es=s)
                k.act(esink, small[:, sm0 + 11:sm0 + 15], AF.Exp)
                pT_r = Rot([k.sb([128, 128], BF16, es=s) for _ in range(4)])
                pss = Rot([k.ps([128, 512], F32, es=s) for _ in range(3)])
                pacc = Rot([k.ps([128, 512], F32, es=s) for _ in range(4)])
                rd_r = Rot([k.sb([128, 128], F32, es=s) for _ in range(2)])
                yp_r = Rot([k.sb([128, 128], BF16, es=s) for _ in range(2)])
                for n in range(0 if need_ctx else 2, NTILE):
                    c0 = n * 128
                    if n < 2:
                        keys = [(0, None), (1, None)]
                    else:
                        keys = []
                        if n - 1 >= 2:
                            keys.append((n - 1, mprev_b))
                        keys.append((n, None))
                        if n + 1 < NTILE:
                            keys.append((n + 1, mnext_b))
                        keys += [(0, None), (1, None)]
                    for qh in range(4):
                        hk, g = qh // 2, qh % 2
                        off = g * 64
                        pv = pacc.get()
                        dn = pacc.get()
                        for i, (kt, msk) in enumerate(keys):
                            ps = pss.get()
                            k.mm(ps[:, 0:128], wkT[off:off + 64, hk, kt * 128:(kt + 1) * 128], wqT[off:off + 64, hk, c0:c0 + 128])
                            pT = pT_r.get()
                            k.act(pT, ps[:, 0:128], AF.Exp, scale=0.125)
                            if msk is not None:
                                k.tt(pT, pT, msk, ALU.mult)
                            k.mm(pv[:, 0:128], wv2[:, kt, hk * 128:(hk + 1) * 128], pT, start=(i == 0), stop=(i == len(keys) - 1))
                            k.mm(dn[:, 0:128], ones_b, pT, start=(i == 0), stop=(i == len(keys) - 1))
                        rd = rd_r.get()
                        k.ts(rd[off:off + 64, :], dn[off:off + 64, 0:128], esink[off:off + 64, qh:qh + 1], ALU.add)
                        k.op("dve", lambda e: e.reciprocal(rd.ap[off:off + 64, :], rd.ap[off:off + 64, :]), reads=[rd], writes=[rd])
                        if g == 0:
                            yp = yp_r.get()
                        k.tt(yp[off:off + 64, :], pv[off:off + 64, 0:128], rd[off:off + 64, :], ALU.mult)
                        if g == 1:
                            k.dma(YT.sl(("win", hk, n), np.s_[6 + hk, :, c0:c0 + 128]), yp)
            k.barrier()
            if stop_here("win%d" % l):
                break
            es_aff = ExitStack()
            aff_e = k.sb([16, NT], F32, "aff_e", es=es_aff)
            with ExitStack() as s:
                w_out = k.sb([128, 8, D], BF16, es=s)
                stg = Rot([k.sb([128, D], F32, es=s) for _ in range(2)])
                for kc in range(8):
                    load_cast(w_out[:, kc, :], w_out_d[l, kc * 128:(kc + 1) * 128, :], stg, e=("pool" if kc % 2 else "dve"))
                rw = k.sb([128, 8, 16], F32, es=s)
                k.dma(rw, TD(router_d[l], "(kc p) e -> p kc e", p=128))
                sts = [0, 1] if need_ctx else [1]
                mod2 = {st: load_mod(st, 2, s) for st in sts}
                gs2 = {st: load_gs(st, 4, l * 2 + 1, s) for st in sts}
                sh2 = {st: load_mod(st, 3, s) for st in sts}
                yT_r = Rot([k.sb([128, 8, 128], BF16, es=s) for _ in range(2)])
                xt_r = Rot([k.sb([128, D], F32, es=s) for _ in range(2)])
                xn_r = Rot([k.sb([128, D], F32, es=s) for _ in range(2)])
                xs_r = Rot([k.sb([128, D], F32, es=s) for _ in range(2)])
                h2b_r = Rot([k.sb([128, D], BF16, es=s) for _ in range(2)])
                h2T_r = Rot([k.sb([128, 8, 128], F32, es=s) for _ in range(2)])
                junk = k.sb([128, D], F32, es=s)
                ss_pool = Rot([k.sb([128, 1], F32, es=s) for _ in range(8)])
                ex_r = Rot([k.sb([128, 16], F32, es=s) for _ in range(2)])
                pso = Rot([k.ps([128, 512], F32, es=s) for _ in range(3)])
                ptr = Rot([k.ps([128, 4, 128], F32, es=s) for _ in range(2)])
                psl = Rot([k.ps([128, 512], F32, es=s) for _ in range(2)])
                for n in range(0 if need_ctx else 2, NTILE):
                    st = 0 if n < 2 else 1
                    c0 = n * 128
                    yT = yT_r.get()
                    k.dma(yT, TD(YT, "c p t -> p c t")[:, :, c0:c0 + 128])
                    xt = xt_r.get()
                    k.dma(xt, rows_key(Xsrc, n)[c0:c0 + 128, :])
                    xn = xn_r.get()
                    for hf in range(2):
                        ps = pso.get()
                        for kc in range(8):
                            k.mm(ps, yT[:, kc, :], w_out[:, kc, hf * 512:(hf + 1) * 512], start=(kc == 0), stop=(kc == 7))
                        sl_ = np.s_[:, hf * 512:(hf + 1) * 512]
                        k.tt(xn[sl_], ps, mod2[st][sl_], ALU.mult)
                        k.tt(xn[sl_], xn[sl_], xt[sl_], ALU.add, e="pool")
                    k.dma(rows_key(X, n)[c0:c0 + 128, :], xn)
                    ss = ss_pool.get()
                    k.act(junk, xn, AF.Square, accum=ss)
                    rstd = ss_pool.get()
                    k.rsq(rstd, ss, 1.0 / D, EPS)
                    xs = xs_r.get()
                    k.ts(xs, xn, rstd, ALU.mult)
                    k.tt(xs, xs, gs2[st], ALU.mult, e="pool")
                    k.tt(xs, xs, sh2[st], ALU.add, e="pool")
                    h2b = h2b_r.get()
                    k.copy(h2b, xs, e="act")
                    k.dma(H2.sl(n, np.s_[c0:c0 + 128, :]), h2b)
                    h2T = h2T_r.get()
                    for hf in range(2):
                        pt = ptr.get()
                        for q4 in range(4):
                            kc = hf * 4 + q4
                            k.tr(pt[:, q4, :], xs[:, kc * 128:(kc + 1) * 128], ident)
                        k.copy(h2T[:, hf * 4:(hf + 1) * 4, :], pt, e="act")
                    pl = psl.get()
                    for kc in range(8):
                        k.mm(pl[:, 0:16], h2T[:, kc, :], rw[:, kc, :], start=(kc == 0), stop=(kc == 7))
                    ex = ex_r.get()
                    sm_ = ss_pool.get()
                    k.act(ex, pl[:, 0:16], AF.Exp, accum=sm_)
                    k.op("dve", lambda e: e.reciprocal(sm_.ap, sm_.ap), reads=[sm_], writes=[sm_])
                    k.ts(affT.sl(n, np.s_[:, n, :]), ex, sm_, ALU.mult)
                    pl2 = psl.get()
                    k.tr(pl2[0:16, 0:128], affT.sl(n, np.s_[:, n, :]), ident)
                    k.copy(aff_e.sl(n, np.s_[:, c0:c0 + 128]), pl2[0:16, 0:128], e="act")
            k.barrier()
            if stop_here("o%d" % l):
                es_aff.close()
                break
            streams = ([(0, 2, 32)] if need_ctx else []) + [(2, 32, 512)]
            with ExitStack() as s:
                work = k.sb([16, NTX], F32, es=s)
                m8 = k.sb([16, 8], F32, es=s)
                thr = k.sb([16, 1], F32, es=s)
                mask_e = k.sb([16, NTX], F32, es=s)
                maskTb = k.sb([128, 32, 16], BF16, es=s)
                carry = k.sb([128, 16], F32, es=s)
                pos_r = Rot([k.sb([128, 16], F32, es=s) for _ in range(2)])
                ptk = Rot([k.ps([128, 512], F32, es=s) for _ in range(4)])
                ahi = k.sb([128, NTILE, 16], BF16, es=s)
                alo = k.sb([128, NTILE, 16], F32, es=s)
                k.copy(ahi, affT)
                k.tt(alo, affT, ahi, ALU.subtract)
                k.copy(Rtab[:, :, :, 2], ahi, e="pool")
                k.copy(Rtab[:, :, :, 3], alo, e="pool")
                for (t0, ntl, cap) in streams:
                    ntok = ntl * 128
                    cs = np.s_[:, t0 * 128:t0 * 128 + ntok]
                    k.copy(work[:, :ntok], aff_e[cs])
                    for r in range(cap // 8):
                        k.op("dve", lambda e: e.max(out=m8.ap, in_=work.ap[:, :ntok]), reads=[work], writes=[m8])
                        if r < cap // 8 - 1:
                            k.op("dve", lambda e: e.match_replace(out=work.ap[:, :ntok], in_to_replace=m8.ap,
                                                                  in_values=work.ap[:, :ntok], imm_value=-1.0),
                                 reads=[work, m8], writes=[work])
                    k.copy(thr, m8[:, 7:8])
                    k.ts(mask_e[:, :ntok], aff_e[cs], thr, ALU.is_ge)
                    k.memset(carry, 0.0)
                    for i in range(ntl):
                        n = t0 + i
                        pt = ptk.get()
                        k.tr(pt[:, 0:16], mask_e[:, i * 128:(i + 1) * 128], ident[0:16, 0:16])
                        k.copy(maskTb[:, i, :], pt[:, 0:16], e="act")
                        pc = ptk.get()
                        k.mm(pc[:, 0:16], triU_b, maskTb[:, i, :])
                        k.mm(pc[:, 16:32], ones_b, maskTb[:, i, :])
                        pos = pos_r.get()
                        k.tt(pos, pc[:, 0:16], carry, ALU.add)
                        k.tt(pos, pos, maskTb[:, i, :], ALU.mult)
                        k.ts(posm.sl(n, np.s_[:, n, :]), pos, -1.0, ALU.add)
                        k.tt(carry, carry, pc[:, 16:32], ALU.add)
            k.barrier()
            if stop_here("topk%d" % l):
                es_aff.close()
                break
            es_aff.close()
            with ExitStack() as s:
                mod5 = {st: load_mod(st, 5, s) for st in ([0, 1] if need_ctx else [1])}
                wsets = Rot([(k.sb([128, 8, 768], BF16, es=s), k.sb([128, 8, 768], BF16, es=s), k.sb([128, 6, D], BF16, es=s))
                             for _ in range(2)])
                stg = Rot([k.sb([128, D], F32, es=s) for _ in range(4)])
                Sel = k.sb([128, 32, 512], BF16, es=s)
                iota16 = k.sb([128, 512], mybir.dt.int16, es=s)
                k.copy(iota16, C("iota"))
                r4T_r = Rot([k.sb([4, 512], F32, es=s) for _ in range(2)])
                r4_r = Rot([k.sb([128, 16], F32, es=s) for _ in range(3)])
                idx_r = Rot([k.sb([128, 1], I32, es=s) for _ in range(12)])
                idf_r = Rot([k.sb([128, 1], F32, es=s) for _ in range(4)])
                gt_r = Rot([k.sb([128, 1], F32, es=s) for _ in range(12)])
                xs_r = Rot([k.sb([128, D], BF16, es=s) for _ in range(8)])
                xsT_r = Rot([k.sb([128, 8, 512], BF16, es=s) for _ in range(2)])
                hid = k.sb([128, 6, 512], BF16, es=s)
                sg_r = Rot([k.sb([128, 512], F32, es=s) for _ in range(2)])
                ys_r = Rot([k.sb([128, D], F32, es=s) for _ in range(2)])
                p4 = Rot([k.ps([128, 512], F32, es=s) for _ in range(1)])
                ptb = Rot([k.ps([128, 8, 128], BF16, es=s) for _ in range(1)])
                pgu = Rot([k.ps([128, 512], F32, es=s) for _ in range(4)])
                pdn = Rot([k.ps([128, 512], F32, es=s) for _ in range(2)])
                Xall = [rows_key(X, n) for n in range(NTILE)] + [X.sub("all")]
                cast_eng = Rot(["act", "dve"])
                wcache = {}

                def weights(e_):
                    wg, wu, wd = wsets.get()
                    for kc in range(8):
                        load_cast(wg[:, kc, :], wg_d[l, e_, kc * 128:(kc + 1) * 128, :], stg, e=cast_eng.get())
                        load_cast(wu[:, kc, :], wu_d[l, e_, kc * 128:(kc + 1) * 128, :], stg, e=cast_eng.get())
                    for fc in range(6):
                        load_cast(wd[:, fc, :], wd_d[l, e_, fc * 128:(fc + 1) * 128, :], stg, e=cast_eng.get())
                    wcache[e_] = (wg, wu, wd)

                def selbuild(u):
                    e_, (t0, ntl, cap) = u
                    for i in range(ntl):
                        k.ts(Sel[:, i, :cap], iota16[:, :cap], posm[:, t0 + i, e_:e_ + 1], ALU.is_equal)

                def idxpart(u):
                    e_, (t0, ntl, cap) = u
                    ps = p4.get()
                    for i in range(ntl):
                        k.mm(ps[0:4, :cap], Rtab[:, t0 + i, e_, :], Sel[:, i, :cap], start=(i == 0), stop=(i == ntl - 1))
                    r4T = r4T_r.get()
                    k.copy(r4T[:, :cap], ps[0:4, :cap])
                    stiles = [(s0, min(128, cap - s0)) for s0 in range(0, cap, 128)]
                    for si, (s0, nsl) in enumerate(stiles):
                        k.tr(ps[0:nsl, si * 4:(si + 1) * 4], r4T[0:4, s0:s0 + nsl], ident[0:4, 0:4])
                    nsl0 = stiles[0][1]
                    r4 = r4_r.get()
                    k.copy(r4[0:nsl0, 0:4 * len(stiles)], ps[0:nsl0, 0:4 * len(stiles)])
                    meta = []
                    xss = []
                    for si, (s0, nsl) in enumerate(stiles):
                        c4 = si * 4
                        idf = idf_r.get()
                        k.stt(idf[0:nsl, :], r4[0:nsl, c4:c4 + 1], 64.0, r4[0:nsl, c4 + 1:c4 + 2], ALU.mult, ALU.add)
                        idx = idx_r.get()
                        k.copy(idx[0:nsl, :], idf[0:nsl, :])
                        gt = gt_r.get()
                        k.tt(gt[0:nsl, :], r4[0:nsl, c4 + 2:c4 + 3], r4[0:nsl, c4 + 3:c4 + 4], ALU.add)
                        meta.append((idx, gt))
                        xs = xs_r.get()
                        k.dma(xs[0:nsl, :], H2, q="pool", reads=[idx],
                              fn=lambda en: en.indirect_dma_start(out=xs.ap[0:nsl, :], out_offset=None, in_=H2.ap,
                                                                  in_offset=bass.IndirectOffsetOnAxis(ap=idx.ap[0:nsl, :], axis=0)))
                        xss.append(xs)
                    return [u, stiles, meta, xss, None]

                def xtrans(item):
                    u, stiles, meta, xss, _ = item
                    xsT = xsT_r.get()
                    for si, (s0, nsl) in enumerate(stiles):
                        xs = xss[si]
                        pt = ptb.get()
                        for kc in range(8):
                            k.tr(pt[:, kc, 0:nsl], xs[0:nsl, kc * 128:(kc + 1) * 128], ident_b[0:nsl, 0:nsl])
                        k.copy(xsT[:, :, s0:s0 + nsl], pt[:, :, 0:nsl], e="act")
                    item[4] = xsT

                def ffn(item):
                    (e_, (t0, ntl, cap)), stiles, meta, xss, xsT = item
                    wg, wu, wd = wcache[e_]
                    for fc in range(6):
                        pg = pgu.get()
                        pu = pgu.get()
                        for kc in range(8):
                            k.mm(pg[:, :cap], wg[:, kc, fc * 128:(fc + 1) * 128], xsT[:, kc, :cap], start=(kc == 0), stop=(kc == 7))
                        for kc in range(8):
                            k.mm(pu[:, :cap], wu[:, kc, fc * 128:(fc + 1) * 128], xsT[:, kc, :cap], start=(kc == 0), stop=(kc == 7))
                        sg = sg_r.get()
                        k.act(sg[:, :cap], pg[:, :cap], AF.Silu)
                        k.tt(hid[:, fc, :cap], sg[:, :cap], pu[:, :cap], ALU.mult)

                def down(item):
                    (e_, (t0, ntl, cap)), stiles, meta, xss, xsT = item
                    st = 0 if t0 == 0 else 1
                    wg, wu, wd = wcache[e_]
                    for si, (s0, nsl) in enumerate(stiles):
                        idx, gt = meta[si]
                        ys = ys_r.get()
                        for hf in range(2):
                            pd = pdn.get()
                            for fc in range(6):
                                k.mm(pd[0:nsl, :], hid[:, fc, s0:s0 + nsl], wd[:, fc, hf * 512:(hf + 1) * 512], start=(fc == 0), stop=(fc == 5))
                            k.stt(ys[0:nsl, hf * 512:(hf + 1) * 512], pd[0:nsl, :], gt[0:nsl, 0:1],
                                  mod5[st][0:nsl, hf * 512:(hf + 1) * 512], ALU.mult, ALU.mult)
                        k.dma(X, ys[0:nsl, :], q="pool", reads=[ys, idx], writes=Xall,
                              fn=lambda en: en.indirect_dma_start(out=X.ap, out_offset=bass.IndirectOffsetOnAxis(ap=idx.ap[0:nsl, :], axis=0),
                                                                  in_=ys.ap[0:nsl, :], in_offset=None, compute_op=ALU.add))

                units = [(e_, stm) for e_ in range(16) for stm in reversed(streams)]
                NU = len(units)
                weights(0)
                weights(1)
                nxt_w = 2
                selbuild(units[0])
                items = {0: idxpart(units[0])}
                xtrans(items[0])
                if NU > 1:
                    selbuild(units[1])
                for ui in range(NU):
                    ffn(items[ui])
                    if ui + 1 < NU:
                        items[ui + 1] = idxpart(units[ui + 1])
                    down(items[ui])
                    if ui + 1 < NU:
                        xtrans(items[ui + 1])
                    if ui + 2 < NU:
                        selbuild(units[ui + 2])
                    e_done = units[ui][0]
                    if (ui + 1 == NU or units[ui + 1][0] != e_done) and nxt_w < 16:
                        weights(nxt_w)
                        nxt_w += 1
                    del items[ui]
            k.barrier()
            if stop_here("exp%d" % l):
                break
        else:
            with ExitStack() as s:
                gfin = k.sb([128, D], F32, es=s)
                k.dma(gfin, g_bc_d[4])
                xt_r = Rot([k.sb([128, D], F32, es=s) for _ in range(3)])
                junk = k.sb([128, D], F32, es=s)
                ss_pool = Rot([k.sb([128, 1], F32, es=s) for _ in range(8)])
                for n in range(2, NTILE):
                    c0 = n * 128
                    xt = xt_r.get()
                    k.dma(xt, rows_key(X, n)[c0:c0 + 128, :])
                    ss = ss_pool.get()
                    k.act(junk, xt, AF.Square, accum=ss)
                    rstd = ss_pool.get()
                    k.rsq(rstd, ss, 1.0 / D, EPS)
                    k.stt(xt, xt, rstd, gfin, ALU.mult, ALU.mult)
                    k.dma(out_d.sl(n, np.s_[c0 - NTC:c0 - NTC + 128, :]), xt)
        k.barrier()
        print("ninst", k.ninst, k.cnt, flush=True)
    return nc


_NC_CACHE = {}


def kernel(**inputs):
    inp = {kk: np.asarray(v) for kk, v in inputs.items()}
    shared = _prep_shared(inp)
    if "nc" not in _NC_CACHE:
        _NC_CACHE["nc"] = build()
    nc = _NC_CACHE["nc"]
    in_maps = []
    for b in range(8):
        m = dict(shared)
        m.update(_prep_core(inp, b))
        in_maps.append(m)
    res = run_bass_kernel_spmd(nc, in_maps, core_ids=list(range(8)))
    out = np.stack([np.asarray(r["out"], dtype=np.float32) for r in res.results], 0)
    return out
```

```python
import numpy as np
from contextlib import ExitStack
import concourse.bass as bass
import concourse.mybir as mybir
from concourse.bass_utils import run_bass_kernel_spmd

F32 = mybir.dt.float32
BF16 = mybir.dt.bfloat16
I32 = mybir.dt.int32
U32 = mybir.dt.uint32
AF = mybir.ActivationFunctionType
ALU = mybir.AluOpType
AX = mybir.AxisListType


class T:
    __slots__ = ("ap", "key")

    def __init__(self, ap, key):
        self.ap = ap
        self.key = key

    def __getitem__(self, idx):
        return T(self.ap[idx], self.key)

    def sub(self, suffix):
        return T(self.ap, (self.key, suffix))

    def sl(self, suffix, idx):
        return T(self.ap[idx], (self.key, suffix))


def _key(x):
    return x.key if isinstance(x, T) else x


class KB:
    CE = ("pe", "act", "dve", "pool")

    def __init__(self, nc, es, nds=14):
        self.nc = nc
        self.es = es
        self.E = {"pe": nc.tensor, "act": nc.scalar, "dve": nc.vector,
                  "pool": nc.gpsimd, "sp": nc.sync}
        self.csem = {e: es.enter_context(nc.semaphore("c_" + e)) for e in self.CE}
        self.cnt = {e: 0 for e in self.CE}
        self.NDS = nds
        self.dsem = [es.enter_context(nc.semaphore("d%d" % i)) for i in range(nds)]
        self.dcnt = [0] * nds
        self.dnext = 0
        self.waited = {e: {} for e in self.E}
        self.lastw = {}
        self.readers = {}
        self.nalloc = 0
        self.ninst = 0

    def sb(self, shape, dtype, name=None, es=None):
        self.nalloc += 1
        name = name or ("t%d" % self.nalloc)
        t = (es or self.es).enter_context(self.nc.sbuf_tensor(name + "_%d" % self.nalloc, list(shape), dtype))
        return T(t[:], name + "_%d" % self.nalloc)

    def ps(self, shape, dtype, name=None, es=None):
        self.nalloc += 1
        name = name or ("p%d" % self.nalloc)
        t = (es or self.es).enter_context(self.nc.psum_tensor(name + "_%d" % self.nalloc, list(shape), dtype))
        return T(t[:], name + "_%d" % self.nalloc)

    def dram(self, name, shape, dtype, kind="Internal"):
        t = self.nc.dram_tensor(name, list(shape), dtype, kind=kind)
        return T(t.ap(), name)

    def _semobj(self, semkey):
        return self.csem[semkey[1]] if semkey[0] == "c" else self.dsem[semkey[1]]

    def _wait(self, e, semkey, val):
        if self.waited[e].get(semkey, 0) >= val:
            return
        self.waited[e][semkey] = val
        self.E[e].wait_ge(self._semobj(semkey), val)

    def _deps(self, e, reads, writes, is_dma):
        for r in reads:
            lw = self.lastw.get(_key(r))
            if lw is not None:
                self._wait(e, lw[0], lw[1])
        for w in writes:
            k = _key(w)
            lw = self.lastw.get(k)
            if lw is not None:
                if not (lw[0] == ("c", e) and e == "pe" and not is_dma):
                    self._wait(e, lw[0], lw[1])
            for sk, v in self.readers.get(k, {}).items():
                if sk == ("c", e) and e == "pe" and not is_dma:
                    continue
                self._wait(e, sk, v)

    def _record(self, tok, reads, writes):
        for r in reads:
            d = self.readers.setdefault(_key(r), {})
            if d.get(tok[0], 0) < tok[1]:
                d[tok[0]] = tok[1]
        for w in writes:
            k = _key(w)
            self.lastw[k] = tok
            self.readers[k] = {}

    def op(self, e, fn, reads=(), writes=()):
        self._deps(e, reads, writes, False)
        ins = fn(self.E[e])
        self.cnt[e] += 1
        ins.then_inc(self.csem[e], 1)
        self._record((("c", e), self.cnt[e]), reads, writes)
        self.ninst += 1
        return ins

    def dma(self, out, in_, q="sp", fn=None, reads=None, writes=None, **kw):
        reads = [in_] if reads is None else reads
        writes = [out] if writes is None else writes
        slot = self.dnext
        self.dnext = (slot + 1) % self.NDS
        if self.dcnt[slot] > 0:
            self._wait(q, ("d", slot), 16 * self.dcnt[slot])
        self._deps(q, reads, writes, True)
        if fn is None:
            ins = self.E[q].dma_start(out=out.ap, in_=in_.ap, **kw)
        else:
            ins = fn(self.E[q])
        self.dcnt[slot] += 1
        ins.then_inc(self.dsem[slot], 16)
        self._record((("d", slot), 16 * self.dcnt[slot]), reads, writes)
        self.ninst += 1
        return ins

    def barrier(self):
        for e in self.E:
            for e2 in self.CE:
                if e2 != e and self.cnt[e2] > 0:
                    self._wait(e, ("c", e2), self.cnt[e2])
            for s in range(self.NDS):
                if self.dcnt[s] > 0:
                    self._wait(e, ("d", s), 16 * self.dcnt[s])

    def mm(self, out, lhsT, rhs, start=True, stop=True, extra_reads=()):
        return self.op("pe", lambda e: e.matmul(out.ap, lhsT.ap, rhs.ap, start=start, stop=stop),
                       reads=[lhsT, rhs, *extra_reads], writes=[out])

    def tr(self, out, in_, ident):
        return self.op("pe", lambda e: e.transpose(out.ap, in_.ap, ident.ap),
                       reads=[in_, ident], writes=[out])

    def act(self, out, in_, func, bias=None, scale=None, accum=None, e="act"):
        kw = {}
        rd = [in_]
        wr = [out]
        if bias is not None:
            if isinstance(bias, T):
                kw["bias"] = bias.ap
                rd.append(bias)
            else:
                kw["bias"] = bias
        if scale is not None:
            if isinstance(scale, T):
                kw["scale"] = scale.ap
                rd.append(scale)
            else:
                kw["scale"] = scale
        if accum is not None:
            kw["accum_out"] = accum.ap
            wr.append(accum)
        return self.op(e, lambda en: en.activation(out.ap, in_.ap, func, **kw), reads=rd, writes=wr)

    def tt(self, out, a, b, op, e="dve"):
        return self.op(e, lambda en: en.tensor_tensor(out.ap, a.ap, b.ap, op), reads=[a, b], writes=[out])

    def ts(self, out, a, s1, op0, s2=None, op1=None, e="dve", accum=None):
        rd = [a]
        wr = [out]
        v1 = s1
        v2 = s2
        if isinstance(s1, T):
            rd.append(s1)
            v1 = s1.ap
        if isinstance(s2, T):
            rd.append(s2)
            v2 = s2.ap
        kw = {}
        if op1 is not None:
            kw["op1"] = op1
        if accum is not None:
            kw["accum_out"] = accum.ap
            wr.append(accum)
        return self.op(e, lambda en: en.tensor_scalar(out.ap, a.ap, v1, v2, op0, **kw), reads=rd, writes=wr)

    def stt(self, out, a, s, b, op0, op1, e="dve"):
        rd = [a, b]
        v = s
        if isinstance(s, T):
            rd.append(s)
            v = s.ap
        return self.op(e, lambda en: en.scalar_tensor_tensor(out.ap, a.ap, v, b.ap, op0, op1), reads=rd, writes=[out])

    def copy(self, out, in_, e="dve"):
        if e == "act":
            return self.op(e, lambda en: en.activation(out.ap, in_.ap, AF.Copy), reads=[in_], writes=[out])
        return self.op(e, lambda en: en.tensor_copy(out.ap, in_.ap), reads=[in_], writes=[out])

    def memset(self, out, v, e="dve"):
        return self.op(e, lambda en: en.memset(out.ap, v), reads=[], writes=[out])

    def rsq(self, dst, src, mul, add):
        self.ts(dst, src, mul, ALU.mult, add, ALU.add)
        self.act(dst, dst, AF.Sqrt)
        self.op("dve", lambda e: e.reciprocal(dst.ap, dst.ap), reads=[dst], writes=[dst])

L_ = 2
D = 1024
NTX = 4096
NTC = 256
NT = NTX + NTC
NTILE = NT // 128
NCOL = 2464
SCALE_MLA = float((64 + 32) ** -0.5)
LN2 = float(np.log(2.0))


def _rope_tables():
    t = np.arange(NTX)
    row = (t // 64).astype(np.float32)
    col = (t % 64).astype(np.float32)

    def tab(dh_half):
        dh = dh_half
        inv = 10000.0 ** (-np.arange(0, dh, 2, dtype=np.float32) / dh)
        return inv

    def build(dtot):
        half = dtot // 2
        inv = tab(half)
        nf = half // 2
        cos = np.zeros((dtot, NTX), np.float32)
        sins = np.zeros((dtot, NTX), np.float32)
        perm = np.zeros((dtot, dtot), np.float32)
        for part, pos in ((0, row), (1, col)):
            base = part * half
            ang = pos[None, :] * inv[:, None]
            c, s = np.cos(ang), np.sin(ang)
            for j in range(nf):
                cos[base + j] = c[j]
                cos[base + nf + j] = c[j]
                sins[base + j] = -s[j]
                sins[base + nf + j] = s[j]
                perm[base + nf + j, base + j] = 1.0
                perm[base + j, base + nf + j] = 1.0
        return cos, sins, perm

    c32, s32, p32 = build(32)
    c64, s64, p64 = build(64)
    c128 = np.concatenate([c64, c64], 0)
    s128 = np.concatenate([s64, s64], 0)
    p128 = np.zeros((128, 128), np.float32)
    p128[:64, :64] = p64
    p128[64:, 64:] = p64
    rope32 = np.stack([c32, s32]).astype(np.float32)
    rope128 = np.stack([c128, s128]).astype(np.float32)
    return rope32, rope128, p32, p128


_CST = {}


def _cst_layout():
    off = 0
    for name, w in (("ident", 128), ("A", 128), ("B", 128), ("C1", 128), ("C2", 128),
                    ("colK1", 1), ("colK2", 1), ("iota", 512), ("triU", 128),
                    ("mprev", 128), ("mnext", 128), ("p128", 128), ("p32", 32), ("pswap", 128), ("bmat", 128),
                    ("tokid", NTILE * 16 * 2)):
        _CST[name] = (off, w)
        off += w
    return off


NCST = _cst_layout()


def _const_table():
    rope32, rope128, p32, p128 = _rope_tables()
    cst = np.zeros((128, NCST), np.float32)
    p = np.arange(128, dtype=np.float32)[:, None]
    c = np.arange(128, dtype=np.float32)[None, :]

    def put(name, arr):
        o, w = _CST[name]
        cst[:arr.shape[0], o:o + w] = arr
    put("ident", np.eye(128, dtype=np.float32))
    put("A", np.maximum(c - p, 0.0))
    put("B", np.maximum(p - c, 0.0))
    put("C1", np.broadcast_to(c + 1.0, (128, 128)))
    put("C2", np.broadcast_to(128.0 - c, (128, 128)))
    put("colK1", 127.0 - p)
    put("colK2", p)
    put("iota", np.broadcast_to(np.arange(512, dtype=np.float32)[None, :], (128, 512)))
    put("triU", (p <= c).astype(np.float32))
    put("mprev", (p >= c).astype(np.float32))
    put("mnext", (p <= c).astype(np.float32))
    put("p128", p128)
    put("p32", p32)
    put("pswap", (np.arange(128)[:, None] == (np.arange(128)[None, :] + 64) % 128).astype(np.float32))
    put("bmat", (np.arange(128)[:, None] // 8 == np.arange(128)[None, :] // 8).astype(np.float32))
    rows = (np.arange(NTILE)[None, :] * 128 + np.arange(128)[:, None])
    tok = np.stack([rows // 64, rows % 64], -1).astype(np.float32)
    tok = np.broadcast_to(tok[:, :, None, :], (128, NTILE, 16, 2)).reshape(128, -1)
    put("tokid", tok)
    return cst, rope32, rope128


def _prep_shared(inp):
    f = lambda a: np.ascontiguousarray(a, dtype=np.float32)
    sh = {}
    w_in = inp["w_in"]
    s = np.cumsum([0, 256, 256, 256, 256, 256, 128, 32, 256, 128, 128])
    rq, rk, rv, rg, cq, ckv, kr, wq, wk, wv = [w_in[:, :, s[i]:s[i + 1]] for i in range(10)]
    wk2 = np.concatenate([wk[:, :, 0:64], wk[:, :, 0:64], wk[:, :, 64:128], wk[:, :, 64:128]], -1)
    wv2 = np.concatenate([wv[:, :, 0:64], wv[:, :, 0:64], wv[:, :, 64:128], wv[:, :, 64:128]], -1)
    sh["w_in_r"] = f(np.concatenate([rq, rk, cq, ckv, wq, wk2, kr, rk, rv, rg, wv2], -1))
    assert sh["w_in_r"].shape[-1] == NCOL
    uq = inp["mla_w_uq"]
    sh["w_uq_n"] = f(uq[:, :, :, :64].reshape(L_, 256, 512))
    sh["w_uq_r"] = f(uq[:, :, :, 64:].reshape(L_, 256, 256))
    uk = inp["mla_w_uk"]
    ukT = uk.transpose(0, 2, 3, 1).reshape(L_, 4, 2, 64, 128)
    sh["w_ukT"] = f(ukT.transpose(0, 2, 3, 1, 4).reshape(L_, 128, 4, 128))
    sh["w_uv"] = f(inp["mla_w_uv"].reshape(L_, 128, 512))
    sh["w_out"] = f(inp["w_out"])
    sh["router_w"] = f(inp["router_w"])
    sh["ada_w"] = f(inp["ada_w"])
    sh["ada_b"] = f(inp["ada_b"].reshape(L_, 1, 6 * D))
    sh["exp_wg"] = f(inp["exp_w_gate"])
    sh["exp_wu"] = f(inp["exp_w_up"])
    sh["exp_wd"] = f(inp["exp_w_down"])
    gb = np.stack([inp["norm1_g"][0], inp["norm2_g"][0], inp["norm1_g"][1], inp["norm2_g"][1], inp["final_g"]])
    sh["g_bc"] = f(np.broadcast_to(gb[:, None, :], (5, 128, D)))
    cols = []
    for l in range(L_):
        qg = inp["mla_qnorm_g"][l].reshape(2, 128).T
        kg = inp["mla_kvnorm_g"][l].reshape(1, 128).T
        df, db, sk = inp["ret_decay_f"][l], inp["ret_decay_b"][l], inp["win_sink"][l]
        rep = np.broadcast_to(np.concatenate([df, db, sk])[None, :], (128, 12))
        hp = (np.arange(128) >= 64).astype(np.int64)
        pp = np.stack([df[0 + hp], df[2 + hp], db[0 + hp], db[2 + hp]], -1)
        cols += [qg, kg, rep, pp]
    sh["small"] = f(np.concatenate(cols, -1))
    cst, rope32, rope128 = _const_table()
    sh["cst"] = cst
    sh["rope32"] = rope32
    sh["rope128"] = rope128
    return sh


def _prep_core(inp, b):
    f = lambda a: np.ascontiguousarray(a, dtype=np.float32)
    d = {}
    d["x0"] = f(np.concatenate([inp["ctx"][b], inp["x"][b]], 0))
    cv = np.stack([inp["c_ctx"], inp["c"][b]])
    cr = cv.reshape(2, 8, 128).transpose(0, 2, 1)
    d["crep"] = f(np.broadcast_to(cr[:, :, :, None], (2, 128, 8, 128)))
    return d

class Rot:
    def __init__(self, tiles):
        self.t = tiles
        self.i = 0

    def get(self):
        t = self.t[self.i % len(self.t)]
        self.i += 1
        return t


def TD(t, pattern, **kw):
    return T(t.ap.rearrange(pattern, **kw), t.key)


def build(upto=None, dbg=False, nlayers=L_):
    nc = bass.Bass("TRN2", target_bir_lowering=False)
    es0 = ExitStack()
    with es0:
        k = KB(nc, es0)
        kind_dbg = "ExternalOutput" if dbg else "Internal"
        din = lambda n, s, dt=F32: k.dram(n, s, dt, kind="ExternalInput")
        x0 = din("x0", [NT, D])
        crep_d = din("crep", [2, 128, 8, 128])
        w_in_d = din("w_in_r", [L_, D, NCOL])
        w_uq_n_d = din("w_uq_n", [L_, 256, 512])
        w_uq_r_d = din("w_uq_r", [L_, 256, 256])
        w_ukT_d = din("w_ukT", [L_, 128, 4, 128])
        w_uv_d = din("w_uv", [L_, 128, 512])
        w_out_d = din("w_out", [L_, D, D])
        router_d = din("router_w", [L_, D, 16])
        ada_w_d = din("ada_w", [L_, D, 6 * D])
        ada_b_d = din("ada_b", [L_, 1, 6 * D])
        wg_d = din("exp_wg", [L_, 16, D, 768])
        wu_d = din("exp_wu", [L_, 16, D, 768])
        wd_d = din("exp_wd", [L_, 16, 768, D])
        g_bc_d = din("g_bc", [5, 128, D])
        small_d = din("small", [128, L_ * 19])
        cst_d = din("cst", [128, NCST])
        rope32_d = din("rope32", [2, 32, NTX])
        rope128_d = din("rope128", [2, 128, NTX])
        out_d = k.dram("out", [NTX, D], F32, kind="ExternalOutput")
        X = k.dram("X", [NT, D], F32, kind=kind_dbg)
        MODBC = k.dram("MODBC", [2, 6, 128, D], F32, kind=kind_dbg)
        QT = k.dram("QT", [2, 128, NT], BF16, kind=kind_dbg)
        KT = k.dram("KT", [2, 128, NT], BF16, kind=kind_dbg)
        TMo = k.dram("TMo", [NT, 1024], BF16, kind=kind_dbg)
        QL = k.dram("QL", [8, 128, NT], BF16, kind=kind_dbg)
        QR = k.dram("QR", [8, 32, NT], BF16, kind=kind_dbg)
        KVT = k.dram("KVT", [128, NT], BF16, kind=kind_dbg)
        KRT = k.dram("KRT", [32, NT], BF16, kind=kind_dbg)
        VP = k.dram("VP", [NT, 512], BF16, kind=kind_dbg)
        WQT = k.dram("WQT", [2, 128, NT], BF16, kind=kind_dbg)
        WKT = k.dram("WKT", [2, 128, NT], BF16, kind=kind_dbg)
        YT = k.dram("YT", [8, 128, NT], BF16, kind=kind_dbg)
        H2 = k.dram("H2", [NT, D], BF16, kind=kind_dbg)
        AFFD = k.dram("AFFD", [16, NTX], F32)
        THRD = k.dram("THRD", [128, 1], F32)

        cst = k.sb([128, NCST], F32, "cst")
        k.dma(cst, cst_d)
        small = k.sb([128, L_ * 19], F32, "small")
        k.dma(small, small_d)

        def C(name, rows=128):
            o, w = _CST[name]
            return cst[0:rows, o:o + w]
        ident = C("ident")
        ones_f = k.sb([128, 128], F32, "ones_f")
        k.memset(ones_f, 1.0)
        ones_b = k.sb([128, 128], BF16, "ones_b")
        k.memset(ones_b, 1.0)
        ident_b = k.sb([128, 128], BF16, "ident_b")
        k.copy(ident_b, ident)
        triU_b = k.sb([128, 128], BF16, "triU_b")
        k.copy(triU_b, C("triU"))
        mprev_b = k.sb([128, 128], BF16, "mprev_b")
        k.copy(mprev_b, C("mprev"))
        mnext_b = k.sb([128, 128], BF16, "mnext_b")
        k.copy(mnext_b, C("mnext"))
        Rtab = k.sb([128, NTILE, 16, 4], BF16, "Rtab")
        o_tok, w_tok = _CST["tokid"]
        k.copy(Rtab[:, :, :, 0:2], T(cst.ap[:, o_tok:o_tok + w_tok].rearrange("p (n e c) -> p n e c", n=NTILE, e=16), cst.key))
        affT = k.sb([128, NTILE, 16], F32, "affT")
        posm = k.sb([128, NTILE, 16], F32, "posm")

        EPS = 1e-6
        blocks = [(0, 256, 0)] + [(256 + 512 * i, 512, 1) for i in range(8)]

        def rows_key(t, n):
            return t.sub(("r", n))

        def stop_here(name):
            return upto is not None and upto == name

        def rstd_from_ss(ss, n_feat, es, eps=EPS):
            r = k.sb([128, 1], F32, es=es)
            k.rsq(r, ss, 1.0 / n_feat, eps)
            return r

        for l in range(nlayers):
            need_ctx = l < L_ - 1
            sm0 = l * 19
            Xsrc = x0 if l == 0 else X
            with ExitStack() as s:
                crep = k.sb([128, 2, 8, 128], F32, es=s)
                for st in range(2):
                    k.dma(crep[:, st], crep_d[st])
                sil = k.sb([128, 2, 8, 128], F32, es=s)
                k.act(sil, crep, AF.Silu)
                wb = Rot([k.sb([128, 8, 512], F32, es=s) for _ in range(2)])
                br = Rot([k.sb([1, 512], F32, es=s) for _ in range(2)])
                pp = Rot([k.ps([128, 512], F32, es=s) for _ in range(4)])
                ob = Rot([k.sb([128, 512], F32, es=s) for _ in range(4)])
                aw = TD(ada_w_d[l], "(kc p) n -> p kc n", p=128)
                for nb in range(12):
                    w = wb.get()
                    k.dma(w, aw[:, :, nb * 512:(nb + 1) * 512])
                    b_ = br.get()
                    k.dma(b_, ada_b_d[l, :, nb * 512:(nb + 1) * 512])
                    for st in range(2):
                        ps = pp.get()
                        for kc in range(8):
                            k.mm(ps, sil[:, st, kc, :], w[:, kc, :], start=(kc == 0), stop=False)
                        k.mm(ps, ones_f[0:1, :], b_, start=False, stop=True)
                        o = ob.get()
                        k.copy(o, ps, e=("act" if st == 0 else "dve"))
                        j, half = nb // 2, nb % 2
                        k.dma(MODBC.sl((st, j), np.s_[st, j, :, half * 512:(half + 1) * 512]), o)
            k.barrier()
            if stop_here("mod%d" % l):
                break

            def load_mod(st, j, es, eng_q="sp"):
                t = k.sb([128, D], F32, es=es)
                k.dma(t, MODBC.sl((st, j), np.s_[st, j]))
                return t

            def load_gs(st, j_scale, gidx, es):
                sc = load_mod(st, j_scale, es)
                gb = k.sb([128, D], F32, es=es)
                k.dma(gb, g_bc_d[gidx])
                k.stt(sc, sc, 1.0, gb, ALU.add, ALU.mult)
                return sc

            def load_cast(dst, src, stg, e="pool"):
                t = stg.get()
                sh = list(src.ap.shape)
                tv = t[0:sh[0], 0:sh[1]]
                k.dma(tv, src)
                k.copy(dst, tv, e=e)

            with ExitStack() as s:
                w_in = k.sb([128, 8, NCOL], BF16, es=s)
                stg = Rot([k.sb([128, NCOL], F32, es=s) for _ in range(2)])
                for kc in range(8):
                    load_cast(w_in[:, kc, :], w_in_d[l, kc * 128:(kc + 1) * 128, :], stg, e=("pool" if kc % 2 else "dve"))
                w_uq_n = k.sb([128, 2, 512], BF16, es=s)
                w_uq_r = k.sb([128, 2, 256], BF16, es=s)
                for kc in range(2):
                    load_cast(w_uq_n[:, kc, :], w_uq_n_d[l, kc * 128:(kc + 1) * 128, :], stg)
                    load_cast(w_uq_r[:, kc, :], w_uq_r_d[l, kc * 128:(kc + 1) * 128, :], stg)
                w_ukT = k.sb([128, 4, 128], BF16, es=s)
                load_cast(T(w_ukT.ap.rearrange("p a r -> p (a r)"), w_ukT.key), TD(w_ukT_d[l], "p a r -> p (a r)"), stg)
                w_uv = k.sb([128, 512], BF16, es=s)
                load_cast(w_uv, w_uv_d[l], stg)
                gs1 = [load_gs(st, 1, l * 2 + 0, s) for st in range(2)]
                sh1 = [load_mod(st, 0, s) for st in range(2)]
                qg = small[:, sm0 + 0:sm0 + 2]
                kg = small[:, sm0 + 2:sm0 + 3]
                xt_r = Rot([k.sb([128, D], F32, es=s) for _ in range(2)])
                xs_r = Rot([k.sb([128, D], F32, es=s) for _ in range(2)])
                junk = k.sb([128, D], F32, es=s)
                ss_pool = Rot([k.sb([128, 1], F32, es=s) for _ in range(8)])
                hT_r = Rot([k.sb([128, 8, 512], BF16, es=s) for _ in range(2)])
                ptr = Rot([k.ps([128, 4, 128], F32, es=s) for _ in range(2)])
                pfm = Rot([k.ps([128, 512], F32, es=s) for _ in range(3)])
                pex = Rot([k.ps([128, 512], F32, es=s) for _ in range(3)])
                ob16 = Rot([k.sb([128, 512], BF16, es=s) for _ in range(6)])
                f32t = Rot([k.sb([128, 512], F32, es=s) for _ in range(6)])
                rp32 = Rot([k.sb([32, 2, 512], F32, es=s) for _ in range(2)])
                rp128 = Rot([k.sb([128, 2, 512], F32, es=s) for _ in range(2)])
                cqg = k.sb([128, 2, 512], BF16, es=s)
                rstdq = k.sb([128, 512], F32, es=s)
                kvT_sb = k.sb([128, 512], BF16, es=s)
                tmo = Rot([k.sb([128, 1024], BF16, es=s) for _ in range(2)])

                def bc_rstd(dst, sq_list, nfeat, n):
                    ps = pex.get()
                    for i, sq in enumerate(sq_list):
                        k.mm(ps[:, :n], ones_f, sq, start=(i == 0), stop=(i == len(sq_list) - 1))
                    k.rsq(dst[:, :n], ps[:, :n], 1.0 / nfeat, EPS)

                def rope_apply(src_f32, M, n, tabs, perm, out_bf):
                    ps = pex.get()
                    k.mm(ps[0:M, :n], perm, src_f32[0:M, :n])
                    t1 = f32t.get()
                    k.tt(t1[0:M, :n], src_f32[0:M, :n], tabs[0:M, 0, :n], ALU.mult)
                    t2 = f32t.get()
                    k.tt(t2[0:M, :n], ps[0:M, :n], tabs[0:M, 1, :n], ALU.mult)
                    k.tt(out_bf[0:M, :n], t1[0:M, :n], t2[0:M, :n], ALU.add, e="pool")

                for (r0, n, st) in blocks:
                    nt = n // 128
                    hT = hT_r.get()
                    if st == 1:
                        t0 = r0 - NTC
                        r32 = rp32.get()
                        k.dma(r32[:, :, :n], TD(rope32_d, "a p t -> p a t")[:, :, t0:t0 + n])
                        r128 = rp128.get()
                        k.dma(r128[:, :, :n], TD(rope128_d, "a p t -> p a t")[:, :, t0:t0 + n])
                    for ti in range(nt):
                        rr = r0 + ti * 128
                        xt = xt_r.get()
                        k.dma(xt, rows_key(Xsrc, rr // 128)[rr:rr + 128, :])
                        ss = ss_pool.get()
                        k.act(junk, xt, AF.Square, accum=ss)
                        rstd = ss_pool.get()
                        k.rsq(rstd, ss, 1.0 / D, EPS)
                        xs = xs_r.get()
                        k.ts(xs, xt, rstd, ALU.mult)
                        k.tt(xs, xs, gs1[st], ALU.mult, e="pool")
                        k.tt(xs, xs, sh1[st], ALU.add, e="pool")
                        for hf in range(2):
                            pt = ptr.get()
                            for q4 in range(4):
                                kc = hf * 4 + q4
                                k.tr(pt[:, q4, :], xs[:, kc * 128:(kc + 1) * 128], ident)
                            k.copy(hT[:, hf * 4:(hf + 1) * 4, ti * 128:(ti + 1) * 128], pt, e="act")
                    def fm(c, M=128):
                        ps = pfm.get()
                        for kc in range(8):
                            k.mm(ps[0:M, :n], w_in[:, kc, c * 128:c * 128 + M], hT[:, kc, :n], start=(kc == 0), stop=(kc == 7))
                        return ps
                    for c in range(4):
                        ps = fm(c)
                        o = ob16.get()
                        k.copy(o[:, :n], ps[:, :n], e=("act" if c % 2 == 0 else "dve"))
                        dst = (QT if c < 2 else KT)
                        k.dma(dst.sl((c % 2, r0), np.s_[c % 2, :, r0:r0 + n]), o[:, :n])
                    sqs = []
                    for c2 in range(2):
                        ps = fm(4 + c2)
                        sq = f32t.get()
                        k.act(sq[:, :n], ps[:, :n], AF.Square)
                        sqs.append(sq[:, :n])
                        k.act(cqg[:, c2, :n], ps[:, :n], AF.Copy, scale=qg[:, c2:c2 + 1])
                    bc_rstd(rstdq, sqs, 256, n)
                    for pr in range(4):
                        ps = pex.get()
                        for c2 in range(2):
                            k.mm(ps[:, :n], w_uq_n[:, c2, pr * 128:(pr + 1) * 128], cqg[:, c2, :n], start=(c2 == 0), stop=(c2 == 1))
                        qn = ob16.get()
                        k.tt(qn[:, :n], ps[:, :n], rstdq[:, :n], ALU.mult)
                        for hh in range(2):
                            h = pr * 2 + hh
                            off = hh * 64
                            ps2 = pex.get()
                            k.mm(ps2[:, :n], w_ukT[off:off + 64, pr, :], qn[off:off + 64, :n])
                            o = ob16.get()
                            k.copy(o[:, :n], ps2[:, :n], e="act")
                            k.dma(QL.sl((h, r0), np.s_[h, :, r0:r0 + n]), o[:, :n])
                    for h in range(8):
                        ps = pex.get()
                        for c2 in range(2):
                            k.mm(ps[0:32, :n], w_uq_r[:, c2, h * 32:(h + 1) * 32], cqg[:, c2, :n], start=(c2 == 0), stop=(c2 == 1))
                        qr = f32t.get()
                        k.tt(qr[0:32, :n], ps[0:32, :n], rstdq[0:32, :n], ALU.mult)
                        o = ob16.get()
                        if st == 1:
                            rope_apply(qr, 32, n, r32, C("p32", 32), o)
                        else:
                            k.copy(o[0:32, :n], qr[0:32, :n], e="pool")
                        k.dma(QR.sl((h, r0), np.s_[h, :, r0:r0 + n]), o[0:32, :n])
                    ps = fm(6)
                    sq = f32t.get()
                    k.act(sq[:, :n], ps[:, :n], AF.Square)
                    kvg = f32t.get()
                    k.act(kvg[:, :n], ps[:, :n], AF.Copy, scale=kg[:, 0:1])
                    rk_ = f32t.get()
                    bc_rstd(rk_, [sq[:, :n]], 128, n)
                    k.tt(kvT_sb[:, :n], kvg[:, :n], rk_[:, :n], ALU.mult)
                    k.dma(KVT.sl(r0, np.s_[:, r0:r0 + n]), kvT_sb[:, :n])
                    for ti in range(nt):
                        ps = pex.get()
                        k.mm(ps, kvT_sb[:, ti * 128:(ti + 1) * 128], w_uv)
                        o = ob16.get()
                        k.copy(o, ps, e="act")
                        rr = r0 + ti * 128
                        k.dma(VP.sl(rr // 128, np.s_[rr:rr + 128, :]), o)
                    for c in range(4):
                        ps = fm(7 + c)
                        o = ob16.get()
                        if st == 1:
                            sf = f32t.get()
                            k.copy(sf[:, :n], ps[:, :n], e="act")
                            rope_apply(sf, 128, n, r128, C("p128"), o)
                        else:
                            k.copy(o[:, :n], ps[:, :n], e="act")
                        dst = (WQT if c < 2 else WKT)
                        k.dma(dst.sl((c % 2, r0), np.s_[c % 2, :, r0:r0 + n]), o[:, :n])
                    ps = fm(11, 32)
                    o = ob16.get()
                    if st == 1:
                        sf = f32t.get()
                        k.copy(sf[0:32, :n], ps[0:32, :n], e="act")
                        rope_apply(sf, 32, n, r32, C("p32", 32), o)
                    else:
                        k.copy(o[0:32, :n], ps[0:32, :n], e="act")
                    k.dma(KRT.sl(r0, np.s_[:, r0:r0 + n]), o[0:32, :n])
                    for ti in range(nt):
                        o = tmo.get()
                        for hf in range(2):
                            ps = pfm.get()
                            c0 = 1440 + hf * 512
                            for kc in range(8):
                                k.mm(ps, hT[:, kc, ti * 128:(ti + 1) * 128], w_in[:, kc, c0:c0 + 512], start=(kc == 0), stop=(kc == 7))
                            k.copy(o[:, hf * 512:(hf + 1) * 512], ps, e=("act" if hf == 0 else "dve"))
                        rr = r0 + ti * 128
                        k.dma(TMo.sl(rr // 128, np.s_[rr:rr + 128, :]), o)
            k.barrier()
            if stop_here("s1_%d" % l):
                break
            with ExitStack() as s:
                lg_rep = k.sb([128, 8], F32, es=s)
                lg_pp = k.sb([128, 4], F32, es=s)
                for dst, src in ((lg_rep, small[:, sm0 + 3:sm0 + 11]), (lg_pp, small[:, sm0 + 15:sm0 + 19])):
                    k.act(dst, src, AF.Exp, scale=LN2)
                    k.ts(dst, dst, -1.0, ALU.mult, 1.0, ALU.add)
                    k.act(dst, dst, AF.Ln)
                LN8 = float(np.log(0.125))
                DT = k.sb([128, 4, 128], F32, es=s)
                tmpA = k.sb([128, 128], F32, es=s)
                for h in range(4):
                    k.ts(tmpA, C("A"), lg_rep[:, h:h + 1], ALU.mult)
                    k.stt(tmpA, C("B"), lg_rep[:, 4 + h:5 + h], tmpA, ALU.mult, ALU.add)
                    k.act(DT[:, h, :], tmpA, AF.Exp)
                    k.ts(DT[:, h, :], DT[:, h, :], 0.125, ALU.mult)
                QWF = k.sb([128, 2, 128], F32, es=s)
                QWB = k.sb([128, 2, 128], F32, es=s)
                gch = k.sb([128, 4], F32, es=s)
                for pr in range(2):
                    k.act(QWF[:, pr, :], C("C1"), AF.Exp, scale=lg_pp[:, pr:pr + 1])
                    k.act(QWB[:, pr, :], C("C2"), AF.Exp, scale=lg_pp[:, 2 + pr:3 + pr])
                k.act(gch, lg_pp, AF.Exp, scale=128.0)
                KWF = k.sb([128, 256], F32, es=s)
                KWB = k.sb([128, 256], F32, es=s)
                kcol = k.sb([128, 8], F32, es=s)
                for h in range(4):
                    k.act(kcol[:, h:h + 1], C("colK1"), AF.Exp, scale=lg_rep[:, h:h + 1])
                    k.act(kcol[:, 4 + h:5 + h], C("colK2"), AF.Exp, scale=lg_rep[:, 4 + h:5 + h])
                k.ts(kcol, kcol, 0.125, ALU.mult)
                for h in range(4):
                    k.copy(KWF[:, h * 64:(h + 1) * 64], T(kcol.ap[:, h:h + 1].to_broadcast([128, 64]), kcol.key))
                    k.copy(KWB[:, h * 64:(h + 1) * 64], T(kcol.ap[:, 4 + h:5 + h].to_broadcast([128, 64]), kcol.key))
                KVd = k.sb([128, NTILE, 4, 64], F32, es=s)
                Sfb = k.sb([128, NTILE, 2, 64], BF16, es=s)
                Sbb = k.sb([128, NTILE, 2, 64], BF16, es=s)
                kv_r = Rot([k.sb([128, 512], BF16, es=s) for _ in range(3)])
                kw_r = Rot([k.sb([128, 2, 256], BF16, es=s) for _ in range(2)])
                pkv = Rot([k.ps([128, 4, 128], F32, es=s) for _ in range(2)])
                for n in range(NTILE):
                    kvt = kv_r.get()
                    k.dma(kvt, TMo.sl(n, np.s_[n * 128:(n + 1) * 128, 0:512]))
                    kw = kw_r.get()
                    k.tt(kw[:, 0, :], kvt[:, 0:256], KWF, ALU.mult)
                    k.tt(kw[:, 1, :], kvt[:, 0:256], KWB, ALU.mult, e="pool")
                    ps = pkv.get()
                    for d_ in range(2):
                        for pr in range(2):
                            k.mm(ps[:, d_ * 2 + pr, :], kw[:, d_, pr * 128:(pr + 1) * 128], kvt[:, 256 + pr * 128:256 + (pr + 1) * 128])
                    k.copy(KVd.sl(n, np.s_[0:64, n, :, :]), ps[0:64, :, 0:64], e="act")
                    k.copy(KVd.sl(n, np.s_[64:128, n, :, :]), ps[64:128, :, 64:128], e="act")
                for d_, (order, Sb_, eng) in enumerate((((list(range(NTILE))), Sfb, "dve"),
                                                       ([1, 0] + list(range(NTILE - 1, 1, -1)), Sbb, "pool"))):
                    cur = k.sb([128, 2, 64], F32, es=s)
                    k.memset(cur, 0.0, e=eng)
                    for n in order:
                        k.copy(Sb_.sl(n, np.s_[:, n, :, :]), cur, e=eng)
                        for pr in range(2):
                            if eng == "dve":
                                k.stt(cur[:, pr, :], cur[:, pr, :], gch[:, d_ * 2 + pr:d_ * 2 + pr + 1],
                                      KVd.sl(n, np.s_[:, n, d_ * 2 + pr, :]), ALU.mult, ALU.add, e=eng)
                            else:
                                k.ts(cur[:, pr, :], cur[:, pr, :], gch[:, d_ * 2 + pr:d_ * 2 + pr + 1], ALU.mult, e=eng)
                                k.tt(cur[:, pr, :], cur[:, pr, :], KVd.sl(n, np.s_[:, n, d_ * 2 + pr, :]), ALU.add, e=eng)
                qk_r = Rot([k.sb([128, 2, 2, 128], BF16, es=s) for _ in range(2)])
                vg_r = Rot([k.sb([128, 512], BF16, es=s) for _ in range(2)])
                qw_r = Rot([k.sb([128, 2, 2, 128], BF16, es=s) for _ in range(2)])
                sm_r = Rot([k.sb([128, 128], BF16, es=s) for _ in range(4)])
                pss = Rot([k.ps([128, 512], F32, es=s) for _ in range(2)])
                psy = Rot([k.ps([128, 512], F32, es=s) for _ in range(2)])
                pst = Rot([k.ps([128, 512], F32, es=s) for _ in range(2)])
                ysb_r = Rot([k.sb([128, 256], F32, es=s) for _ in range(2)])
                sq_r = Rot([k.sb([128, 256], F32, es=s) for _ in range(2)])
                st_r = Rot([k.sb([128, 16], F32, es=s) for _ in range(2)])
                sg_r = Rot([k.sb([128, 256], F32, es=s) for _ in range(2)])
                yo_r = Rot([k.sb([128, 2, 128], BF16, es=s) for _ in range(2)])
                for n in range(0 if need_ctx else 2, NTILE):
                    c0 = n * 128
                    qk = qk_r.get()
                    k.dma(qk[:, 0], TD(QT, "c p t -> p c t")[:, :, c0:c0 + 128])
                    k.dma(qk[:, 1], TD(KT, "c p t -> p c t")[:, :, c0:c0 + 128])
                    vg = vg_r.get()
                    k.dma(vg, TMo.sl(n, np.s_[c0:c0 + 128, 256:768]))
                    qw = qw_r.get()
                    k.tt(qw[:, 0], qk[:, 0], QWF, ALU.mult)
                    k.tt(qw[:, 1], qk[:, 0], QWB, ALU.mult, e="pool")
                    py = psy.get()
                    for h in range(4):
                        pr, off = h // 2, (h % 2) * 64
                        ps = pss.get()
                        k.mm(ps[:, 0:128], qk[off:off + 64, 1, pr, :], qk[off:off + 64, 0, pr, :])
                        sm = sm_r.get()
                        k.tt(sm, ps[:, 0:128], DT[:, h, :], ALU.mult)
                        yo_ = py[:, h * 64:(h + 1) * 64]
                        k.mm(yo_, sm, vg[:, h * 64:(h + 1) * 64], start=True, stop=False)
                        k.mm(yo_, qw[off:off + 64, 0, pr, :], Sfb.sl(n, np.s_[off:off + 64, n, pr, :]), start=False, stop=False)
                        k.mm(yo_, qw[off:off + 64, 1, pr, :], Sbb.sl(n, np.s_[off:off + 64, n, pr, :]), start=False, stop=True)
                    ysb = ysb_r.get()
                    k.copy(ysb, py[:, 0:256], e="act")
                    stt_ = st_r.get()
                    k.op("dve", lambda e: e.reduce_sum(stt_.ap[:, 0:4], ysb.ap.rearrange("p (h d) -> p h d", h=4), AX.X), reads=[ysb], writes=[stt_])
                    sq = sq_r.get()
                    k.tt(sq, ysb, ysb, ALU.mult, e="pool")
                    k.op("dve", lambda e: e.reduce_sum(stt_.ap[:, 4:8], sq.ap.rearrange("p (h d) -> p h d", h=4), AX.X), reads=[sq], writes=[stt_])
                    k.ts(stt_[:, 0:8], stt_[:, 0:8], 1.0 / 64, ALU.mult)
                    k.tt(stt_[:, 8:12], stt_[:, 0:4], stt_[:, 0:4], ALU.mult)
                    k.tt(stt_[:, 12:16], stt_[:, 4:8], stt_[:, 8:12], ALU.subtract)
                    k.rsq(stt_[:, 12:16], stt_[:, 12:16], 1.0, 1e-5)
                    sg = sg_r.get()
                    k.act(sg, vg[:, 256:512], AF.Silu)
                    for h in range(4):
                        k.ts(ysb[:, h * 64:(h + 1) * 64], ysb[:, h * 64:(h + 1) * 64], stt_[:, h:h + 1], ALU.subtract,
                             stt_[:, 12 + h:13 + h], ALU.mult)
                    k.tt(ysb, ysb, sg, ALU.mult, e="pool")
                    pt = pst.get()
                    for c in range(2):
                        k.tr(pt[:, c * 128:(c + 1) * 128], ysb[:, c * 128:(c + 1) * 128], ident)
                    yo = yo_r.get()
                    k.copy(T(yo.ap.rearrange("p c t -> p (c t)"), yo.key), pt[:, 0:256], e="act")
                    k.dma(TD(YT, "c p t -> p c t").sl(("ret", n), np.s_[:, 0:2, c0:c0 + 128]), yo)
            k.barrier()
            if stop_here("ret%d" % l):
                break
            with ExitStack() as s:
                kvT = k.sb([128, NT], BF16, es=s)
                k.dma(kvT, KVT)
                krT = k.sb([128, NT], BF16, es=s)
                k.memset(krT, 0.0, e="pool")
                k.dma(krT[0:32, :], KRT)
                vp2 = k.sb([128, NTILE, 8, 128], BF16, es=s)
                VPv = TD(VP, "(t p) (h d) -> p t h d", p=128, d=64)
                for par in range(2):
                    k.memset(vp2[:, :, par::2, (1 - par) * 64:(2 - par) * 64], 1.0, e=("dve" if par == 0 else "pool"))
                    for h in range(par, 8, 2):
                        k.dma(vp2[:, :, h, par * 64:(par + 1) * 64], VPv[:, :, h, :])
                rd_sets = []
                for par in range(2):
                    tl = [k.sb([128, 512], F32, es=s) for _ in range(2)]
                    for t_ in tl:
                        k.memset(t_, 0.0)
                    rd_sets.append(Rot(tl))
                ql_r = Rot([k.sb([128, 512], BF16, es=s) for _ in range(3)])
                qr_l = [k.sb([128, 512], BF16, es=s) for _ in range(3)]
                for t_ in qr_l:
                    k.memset(t_, 0.0)
                qr_r = Rot(qr_l)
                pT_r = Rot([k.sb([128, 512], BF16, es=s) for _ in range(5)])
                pss = Rot([k.ps([128, 512], F32, es=s) for _ in range(4)])
                pacc = Rot([k.ps([128, 512], F32, es=s) for _ in range(4)])
                sw_r = Rot([k.sb([128, 512], F32, es=s) for _ in range(2)])
                yp_r = Rot([k.sb([128, 512], BF16, es=s) for _ in range(3)])
                qblocks = ([(0, 256, [0, 1])] if need_ctx else []) + [(256 + 512 * i, 512, list(range(NTILE))) for i in range(8)]
                LOOK = 2
                for (r0, n, kts) in qblocks:
                    pend = []

                    def emit_pv(item):
                        h, i, kt, pT, pv, yp = item
                        last = (i == len(kts) - 1)
                        k.mm(pv[:, :n], vp2[:, kt, h, :], pT[:, :n], start=(i == 0), stop=last)
                        if last:
                            off = (h % 2) * 64
                            dof = 64 - off
                            rd = rd_sets[h % 2].get()
                            k.op("dve", lambda e: e.reciprocal(rd.ap[dof:dof + 64, :n], pv.ap[dof:dof + 64, :n]), reads=[pv], writes=[rd])
                            sws = sw_r.get()
                            k.dma(sws[off:off + 64, :n], rd[dof:dof + 64, :n])
                            k.tt(yp[off:off + 64, :n], pv[off:off + 64, :n], sws[off:off + 64, :n], ALU.mult)
                            if h % 2 == 1:
                                k.dma(YT.sl(("mla", h // 2, r0), np.s_[2 + h // 2, :, r0:r0 + n]), yp[:, :n])
                    yp = None
                    for h in range(8):
                        ql = ql_r.get()
                        k.dma(ql[:, :n], QL[h, :, r0:r0 + n])
                        qr = qr_r.get()
                        k.dma(qr[0:32, :n], QR[h, :, r0:r0 + n])
                        pv = pacc.get()
                        if h % 2 == 0:
                            yp = yp_r.get()
                        for i, kt in enumerate(kts):
                            ps = pss.get()
                            k.mm(ps[:, :n], kvT[:, kt * 128:(kt + 1) * 128], ql[:, :n], start=True, stop=False)
                            k.mm(ps[:, :n], krT[:, kt * 128:(kt + 1) * 128], qr[:, :n], start=False, stop=True)
                            pT = pT_r.get()
                            k.act(pT[:, :n], ps[:, :n], AF.Exp, scale=SCALE_MLA)
                            pend.append((h, i, kt, pT, pv, yp))
                            if len(pend) > LOOK:
                                emit_pv(pend.pop(0))
                    while pend:
                        emit_pv(pend.pop(0))
            k.barrier()
            if stop_here("mla%d" % l):
                break
            with ExitStack() as s:
                wkT = k.sb([128, 2, 2, NT], BF16, es=s)
                k.memset(wkT, 0.0, e="pool")
                for hk_ in range(2):
                    for g_ in range(2):
                        k.dma(wkT[g_ * 64:(g_ + 1) * 64, hk_, g_, :], WKT[hk_, g_ * 64:(g_ + 1) * 64, :])
                wqT = k.sb([128, 2, NT], BF16, es=s)
                k.dma(wqT, TD(WQT, "c p t -> p c t"))
                wv3 = k.sb([128, NTILE, 4, 128], BF16, es=s)
                WVv = T(TMo.ap[:, 768:1024].rearrange("(t p) (q d) -> p t q d", p=128, d=64), TMo.key)
                for par in range(2):
                    k.memset(wv3[:, :, par::2, (1 - par) * 64:(2 - par) * 64], 1.0, e=("dve" if par == 0 else "pool"))
                    for q_ in range(par, 4, 2):
                        k.dma(wv3[:, :, q_, par * 64:(par + 1) * 64], WVv[:, :, q_, :])
                esink = k.sb([128, 4], F32, es=s)
                k.act(esink, small[:, sm0 + 11:sm0 + 15], AF.Exp)
                rd_sets = []
                for par in range(2):
                    tl = [k.sb([128, 128], F32, es=s) for _ in range(2)]
                    for t_ in tl:
                        k.memset(t_, 0.0)
                    rd_sets.append(Rot(tl))
                pT_r = Rot([k.sb([128, 128], BF16, es=s) for _ in range(6)])
                pss = Rot([k.ps([128, 512], F32, es=s) for _ in range(4)])
                pacc = Rot([k.ps([128, 512], F32, es=s) for _ in range(3)])
                psw = Rot([k.ps([128, 512], F32, es=s) for _ in range(1)])
                sw_r = Rot([k.sb([128, 128], F32, es=s) for _ in range(2)])
                yp_r = Rot([k.sb([128, 128], BF16, es=s) for _ in range(3)])
                pend = []

                def emit_pvw(item):
                    n, qh, i, nk, kt, pT, pv, yp = item
                    hk, g = qh // 2, qh % 2
                    off = g * 64
                    dof = 64 - off
                    c0 = n * 128
                    last = (i == nk - 1)
                    k.mm(pv[:, 0:128], wv3[:, kt, qh, :], pT, start=(i == 0), stop=last)
                    if last:
                        rd = rd_sets[g].get()
                        k.ts(rd[dof:dof + 64, :], pv[dof:dof + 64, 0:128], esink[dof:dof + 64, qh:qh + 1], ALU.add)
                        k.op("dve", lambda e: e.reciprocal(rd.ap[dof:dof + 64, :], rd.ap[dof:dof + 64, :]), reads=[rd], writes=[rd])
                        sw = psw.get()
                        k.mm(sw[:, 0:128], C("pswap"), rd)
                        sws = sw_r.get()
                        k.copy(sws[off:off + 64, :], sw[off:off + 64, 0:128], e="act")
                        k.tt(yp[off:off + 64, :], pv[off:off + 64, 0:128], sws[off:off + 64, :], ALU.mult)
                        if g == 1:
                            k.dma(YT.sl(("win", hk, n), np.s_[6 + hk, :, c0:c0 + 128]), yp)
                yp = None
                for n in range(0 if need_ctx else 2, NTILE):
                    c0 = n * 128
                    if n < 2:
                        keys = [(0, None), (1, None)]
                    else:
                        keys = []
                        if n - 1 >= 2:
                            keys.append((n - 1, mprev_b))
                        keys.append((n, None))
                        if n + 1 < NTILE:
                            keys.append((n + 1, mnext_b))
                        keys += [(0, None), (1, None)]
                    for qh in range(4):
                        hk, g = qh // 2, qh % 2
                        off = g * 64
                        pv = pacc.get()
                        if g == 0:
                            yp = yp_r.get()
                        for i, (kt, msk) in enumerate(keys):
                            ps = pss.get()
                            k.mm(ps[:, 0:128], wkT[:, hk, g, kt * 128:(kt + 1) * 128], wqT[:, hk, c0:c0 + 128])
                            pT = pT_r.get()
                            k.act(pT, ps[:, 0:128], AF.Exp, scale=0.125)
                            if msk is not None:
                                k.tt(pT, pT, msk, ALU.mult)
                            pend.append((n, qh, i, len(keys), kt, pT, pv, yp))
                            if len(pend) > 3:
                                emit_pvw(pend.pop(0))
                while pend:
                    emit_pvw(pend.pop(0))
            k.barrier()
            if stop_here("win%d" % l):
                break
            es_aff = ExitStack()
            aff_e = k.sb([16, NT], F32, "aff_e", es=es_aff)
            with ExitStack() as s:
                w_out = k.sb([128, 8, D], BF16, es=s)
                stg = Rot([k.sb([128, D], F32, es=s) for _ in range(2)])
                for kc in range(8):
                    load_cast(w_out[:, kc, :], w_out_d[l, kc * 128:(kc + 1) * 128, :], stg, e=("pool" if kc % 2 else "dve"))
                rw = k.sb([128, 8, 16], F32, es=s)
                k.dma(rw, TD(router_d[l], "(kc p) e -> p kc e", p=128))
                sts = [0, 1] if need_ctx else [1]
                mod2 = {st: load_mod(st, 2, s) for st in sts}
                gs2 = {st: load_gs(st, 4, l * 2 + 1, s) for st in sts}
                sh2 = {st: load_mod(st, 3, s) for st in sts}
                yT_r = Rot([k.sb([128, 8, 128], BF16, es=s) for _ in range(2)])
                xt_r = Rot([k.sb([128, D], F32, es=s) for _ in range(2)])
                xn_r = Rot([k.sb([128, D], F32, es=s) for _ in range(2)])
                xs_r = Rot([k.sb([128, D], F32, es=s) for _ in range(2)])
                h2b_r = Rot([k.sb([128, D], BF16, es=s) for _ in range(2)])
                h2T_r = Rot([k.sb([128, 8, 128], F32, es=s) for _ in range(2)])
                junk = k.sb([128, D], F32, es=s)
                ss_pool = Rot([k.sb([128, 1], F32, es=s) for _ in range(8)])
                ex_r = Rot([k.sb([128, 16], F32, es=s) for _ in range(2)])
                pso = Rot([k.ps([128, 512], F32, es=s) for _ in range(3)])
                ptr = Rot([k.ps([128, 4, 128], F32, es=s) for _ in range(2)])
                psl = Rot([k.ps([128, 512], F32, es=s) for _ in range(2)])
                for n in range(0 if need_ctx else 2, NTILE):
                    st = 0 if n < 2 else 1
                    c0 = n * 128
                    yT = yT_r.get()
                    k.dma(yT, TD(YT, "c p t -> p c t")[:, :, c0:c0 + 128])
                    xt = xt_r.get()
                    k.dma(xt, rows_key(Xsrc, n)[c0:c0 + 128, :])
                    xn = xn_r.get()
                    for hf in range(2):
                        ps = pso.get()
                        for kc in range(8):
                            k.mm(ps, yT[:, kc, :], w_out[:, kc, hf * 512:(hf + 1) * 512], start=(kc == 0), stop=(kc == 7))
                        sl_ = np.s_[:, hf * 512:(hf + 1) * 512]
                        k.tt(xn[sl_], ps, mod2[st][sl_], ALU.mult)
                        k.tt(xn[sl_], xn[sl_], xt[sl_], ALU.add, e="pool")
                    k.dma(rows_key(X, n)[c0:c0 + 128, :], xn)
                    ss = ss_pool.get()
                    k.act(junk, xn, AF.Square, accum=ss)
                    rstd = ss_pool.get()
                    k.rsq(rstd, ss, 1.0 / D, EPS)
                    xs = xs_r.get()
                    k.ts(xs, xn, rstd, ALU.mult)
                    k.tt(xs, xs, gs2[st], ALU.mult, e="pool")
                    k.tt(xs, xs, sh2[st], ALU.add, e="pool")
                    h2b = h2b_r.get()
                    k.copy(h2b, xs, e="act")
                    k.dma(H2.sl(n, np.s_[c0:c0 + 128, :]), h2b)
                    h2T = h2T_r.get()
                    for hf in range(2):
                        pt = ptr.get()
                        for q4 in range(4):
                            kc = hf * 4 + q4
                            k.tr(pt[:, q4, :], xs[:, kc * 128:(kc + 1) * 128], ident)
                        k.copy(h2T[:, hf * 4:(hf + 1) * 4, :], pt, e="act")
                    pl = psl.get()
                    for kc in range(8):
                        k.mm(pl[:, 0:16], h2T[:, kc, :], rw[:, kc, :], start=(kc == 0), stop=(kc == 7))
                    ex = ex_r.get()
                    sm_ = ss_pool.get()
                    k.act(ex, pl[:, 0:16], AF.Exp, accum=sm_)
                    k.op("dve", lambda e: e.reciprocal(sm_.ap, sm_.ap), reads=[sm_], writes=[sm_])
                    k.ts(affT.sl(n, np.s_[:, n, :]), ex, sm_, ALU.mult)
                    pl2 = psl.get()
                    k.tr(pl2[0:16, 0:128], affT.sl(n, np.s_[:, n, :]), ident)
                    k.copy(aff_e.sl(n, np.s_[:, c0:c0 + 128]), pl2[0:16, 0:128], e="act")
            k.barrier()
            if stop_here("o%d" % l):
                es_aff.close()
                break
            streams = ([(0, 2, 32)] if need_ctx else []) + [(2, 32, 512)]
            with ExitStack() as s:
                work = k.sb([16, NTX], F32, es=s)
                m8 = k.sb([16, 8], F32, es=s)
                thr = k.sb([16, 1], F32, es=s)
                aff128 = k.sb([128, 512], F32, es=s)
                junk16 = k.sb([128, 512], BF16, es=s)
                lo = k.sb([128, 1], F32, es=s)
                hi = k.sb([128, 1], F32, es=s)
                mid = k.sb([128, 1], F32, es=s)
                cnt = k.sb([128, 1], F32, es=s)
                half = k.sb([128, 1], F32, es=s)
                k.memset(half, 0.5)
                mge = k.sb([128, 1], U32, es=s)
                mlt = k.sb([128, 1], U32, es=s)
                thr16 = k.sb([16, 8], F32, es=s)
                mask_e = k.sb([16, NTX], F32, es=s)
                maskTb = k.sb([128, 32, 16], BF16, es=s)
                carry = k.sb([128, 16], F32, es=s)
                pos_r = Rot([k.sb([128, 16], F32, es=s) for _ in range(2)])
                ptk = Rot([k.ps([128, 512], F32, es=s) for _ in range(4)])
                ahi = k.sb([128, NTILE, 16], BF16, es=s)
                alo = k.sb([128, NTILE, 16], F32, es=s)
                k.copy(ahi, affT)
                k.tt(alo, affT, ahi, ALU.subtract)
                k.copy(Rtab[:, :, :, 2], ahi, e="pool")
                k.copy(Rtab[:, :, :, 3], alo, e="pool")
                for (t0, ntl, cap) in streams:
                    ntok = ntl * 128
                    cs = np.s_[:, t0 * 128:t0 * 128 + ntok]
                    if cap <= 64:
                        k.copy(work[:, :ntok], aff_e[cs])
                        for r in range(cap // 8):
                            k.op("dve", lambda e: e.max(out=m8.ap, in_=work.ap[:, :ntok]), reads=[work], writes=[m8])
                            if r < cap // 8 - 1:
                                k.op("dve", lambda e: e.match_replace(out=work.ap[:, :ntok], in_to_replace=m8.ap,
                                                                      in_values=work.ap[:, :ntok], imm_value=-1.0),
                                     reads=[work, m8], writes=[work])
                        k.copy(thr, m8[:, 7:8])
                    else:
                        k.dma(AFFD, aff_e[cs])
                        k.dma(aff128, TD(AFFD, "e (s t) -> (e s) t", s=8))
                        k.memset(lo, 0.0)
                        k.memset(hi, 2.0)
                        for it in range(40):
                            k.stt(mid, lo, hi, half, ALU.add, ALU.mult)
                            k.ts(junk16, aff128, mid, ALU.is_ge, 0.0, ALU.add, accum=cnt)
                            pc = ptk.get()
                            k.mm(pc[:, 0:1], C("bmat"), cnt)
                            k.ts(mge, pc[:, 0:1], cap - 0.5, ALU.is_ge)
                            k.ts(mlt, pc[:, 0:1], cap - 0.5, ALU.is_lt)
                            k.op("dve", lambda e: e.copy_predicated(lo.ap, mge.ap, mid.ap), reads=[mge, mid], writes=[lo])
                            k.op("dve", lambda e: e.copy_predicated(hi.ap, mlt.ap, mid.ap), reads=[mlt, mid], writes=[hi])
                        k.dma(THRD, lo)
                        k.dma(thr16, TD(THRD, "(e s) o -> e (s o)", s=8))
                        k.copy(thr, thr16[:, 0:1])
                    k.ts(mask_e[:, :ntok], aff_e[cs], thr, ALU.is_ge)
                    k.memset(carry, 0.0)
                    for i in range(ntl):
                        n = t0 + i
                        pt = ptk.get()
                        k.tr(pt[:, 0:16], mask_e[:, i * 128:(i + 1) * 128], ident[0:16, 0:16])
                        k.copy(maskTb[:, i, :], pt[:, 0:16], e="act")
                        pc = ptk.get()
                        k.mm(pc[:, 0:16], triU_b, maskTb[:, i, :])
                        k.mm(pc[:, 16:32], ones_b, maskTb[:, i, :])
                        pos = pos_r.get()
                        k.tt(pos, pc[:, 0:16], carry, ALU.add)
                        k.tt(pos, pos, maskTb[:, i, :], ALU.mult)
                        k.ts(posm.sl(n, np.s_[:, n, :]), pos, -1.0, ALU.add)
                        k.tt(carry, carry, pc[:, 16:32], ALU.add)
            k.barrier()
            if stop_here("topk%d" % l):
                es_aff.close()
                break
            es_aff.close()
            with ExitStack() as s:
                mod5 = {st: load_mod(st, 5, s) for st in ([0, 1] if need_ctx else [1])}
                wsets = Rot([(k.sb([128, 8, 768], BF16, es=s), k.sb([128, 8, 768], BF16, es=s), k.sb([128, 6, D], BF16, es=s))
                             for _ in range(2)])
                stg = Rot([k.sb([128, D], F32, es=s) for _ in range(4)])
                Sel = k.sb([128, 32, 512], BF16, es=s)
                iota16 = k.sb([128, 512], mybir.dt.int16, es=s)
                k.copy(iota16, C("iota"))
                r4T_r = Rot([k.sb([4, 512], F32, es=s) for _ in range(2)])
                r4_r = Rot([k.sb([128, 16], F32, es=s) for _ in range(3)])
                idx_r = Rot([k.sb([128, 1], I32, es=s) for _ in range(12)])
                idf_r = Rot([k.sb([128, 1], F32, es=s) for _ in range(4)])
                gt_r = Rot([k.sb([128, 1], F32, es=s) for _ in range(12)])
                xs_r = Rot([k.sb([128, D], BF16, es=s) for _ in range(8)])
                xsT_r = Rot([k.sb([128, 8, 512], BF16, es=s) for _ in range(2)])
                hid = k.sb([128, 6, 512], BF16, es=s)
                sg_r = Rot([k.sb([128, 512], F32, es=s) for _ in range(2)])
                ys_r = Rot([k.sb([128, D], F32, es=s) for _ in range(2)])
                p4 = Rot([k.ps([128, 512], F32, es=s) for _ in range(1)])
                ptb = Rot([k.ps([128, 8, 128], BF16, es=s) for _ in range(1)])
                pgu = Rot([k.ps([128, 512], F32, es=s) for _ in range(4)])
                pdn = Rot([k.ps([128, 512], F32, es=s) for _ in range(2)])
                Xall = [rows_key(X, n) for n in range(NTILE)] + [X.sub("all")]
                cast_eng = Rot(["act", "dve"])
                wcache = {}

                def weights(e_):
                    wg, wu, wd = wsets.get()
                    for kc in range(8):
                        load_cast(wg[:, kc, :], wg_d[l, e_, kc * 128:(kc + 1) * 128, :], stg, e=cast_eng.get())
                        load_cast(wu[:, kc, :], wu_d[l, e_, kc * 128:(kc + 1) * 128, :], stg, e=cast_eng.get())
                    for fc in range(6):
                        load_cast(wd[:, fc, :], wd_d[l, e_, fc * 128:(fc + 1) * 128, :], stg, e=cast_eng.get())
                    wcache[e_] = (wg, wu, wd)

                def selbuild(u):
                    e_, (t0, ntl, cap) = u
                    for i in range(ntl):
                        k.ts(Sel[:, i, :cap], iota16[:, :cap], posm[:, t0 + i, e_:e_ + 1], ALU.is_equal)

                def idxpart(u):
                    e_, (t0, ntl, cap) = u
                    ps = p4.get()
                    for i in range(ntl):
                        k.mm(ps[0:4, :cap], Rtab[:, t0 + i, e_, :], Sel[:, i, :cap], start=(i == 0), stop=(i == ntl - 1))
                    r4T = r4T_r.get()
                    k.copy(r4T[:, :cap], ps[0:4, :cap])
                    stiles = [(s0, min(128, cap - s0)) for s0 in range(0, cap, 128)]
                    for si, (s0, nsl) in enumerate(stiles):
                        k.tr(ps[0:nsl, si * 4:(si + 1) * 4], r4T[0:4, s0:s0 + nsl], ident[0:4, 0:4])
                    nsl0 = stiles[0][1]
                    r4 = r4_r.get()
                    k.copy(r4[0:nsl0, 0:4 * len(stiles)], ps[0:nsl0, 0:4 * len(stiles)])
                    meta = []
                    xss = []
                    for si, (s0, nsl) in enumerate(stiles):
                        c4 = si * 4
                        idf = idf_r.get()
                        k.stt(idf[0:nsl, :], r4[0:nsl, c4:c4 + 1], 64.0, r4[0:nsl, c4 + 1:c4 + 2], ALU.mult, ALU.add)
                        idx = idx_r.get()
                        k.copy(idx[0:nsl, :], idf[0:nsl, :])
                        gt = gt_r.get()
                        k.tt(gt[0:nsl, :], r4[0:nsl, c4 + 2:c4 + 3], r4[0:nsl, c4 + 3:c4 + 4], ALU.add)
                        meta.append((idx, gt))
                        xs = xs_r.get()
                        k.dma(xs[0:nsl, :], H2, q="pool", reads=[idx],
                              fn=lambda en: en.indirect_dma_start(out=xs.ap[0:nsl, :], out_offset=None, in_=H2.ap,
                                                                  in_offset=bass.IndirectOffsetOnAxis(ap=idx.ap[0:nsl, :], axis=0)))
                        xss.append(xs)
                    return [u, stiles, meta, xss, None]

                def xtrans(item):
                    u, stiles, meta, xss, _ = item
                    xsT = xsT_r.get()
                    for si, (s0, nsl) in enumerate(stiles):
                        xs = xss[si]
                        pt = ptb.get()
                        for kc in range(8):
                            k.tr(pt[:, kc, 0:nsl], xs[0:nsl, kc * 128:(kc + 1) * 128], ident_b[0:nsl, 0:nsl])
                        k.copy(xsT[:, :, s0:s0 + nsl], pt[:, :, 0:nsl], e="act")
                    item[4] = xsT

                def ffn(item):
                    (e_, (t0, ntl, cap)), stiles, meta, xss, xsT = item
                    wg, wu, wd = wcache[e_]
                    for fc in range(6):
                        pg = pgu.get()
                        pu = pgu.get()
                        for kc in range(8):
                            k.mm(pg[:, :cap], wg[:, kc, fc * 128:(fc + 1) * 128], xsT[:, kc, :cap], start=(kc == 0), stop=(kc == 7))
                        for kc in range(8):
                            k.mm(pu[:, :cap], wu[:, kc, fc * 128:(fc + 1) * 128], xsT[:, kc, :cap], start=(kc == 0), stop=(kc == 7))
                        sg = sg_r.get()
                        k.act(sg[:, :cap], pg[:, :cap], AF.Silu)
                        k.tt(hid[:, fc, :cap], sg[:, :cap], pu[:, :cap], ALU.mult)

                def down(item):
                    (e_, (t0, ntl, cap)), stiles, meta, xss, xsT = item
                    st = 0 if t0 == 0 else 1
                    wg, wu, wd = wcache[e_]
                    for si, (s0, nsl) in enumerate(stiles):
                        idx, gt = meta[si]
                        ys = ys_r.get()
                        for hf in range(2):
                            pd = pdn.get()
                            for fc in range(6):
                                k.mm(pd[0:nsl, :], hid[:, fc, s0:s0 + nsl], wd[:, fc, hf * 512:(hf + 1) * 512], start=(fc == 0), stop=(fc == 5))
                            k.stt(ys[0:nsl, hf * 512:(hf + 1) * 512], pd[0:nsl, :], gt[0:nsl, 0:1],
                                  mod5[st][0:nsl, hf * 512:(hf + 1) * 512], ALU.mult, ALU.mult)
                        k.dma(X, ys[0:nsl, :], q="pool", reads=[ys, idx], writes=Xall,
                              fn=lambda en: en.indirect_dma_start(out=X.ap, out_offset=bass.IndirectOffsetOnAxis(ap=idx.ap[0:nsl, :], axis=0),
                                                                  in_=ys.ap[0:nsl, :], in_offset=None, compute_op=ALU.add))

                units = [(e_, stm) for e_ in range(16) for stm in reversed(streams)]
                NU = len(units)
                weights(0)
                weights(1)
                nxt_w = 2
                selbuild(units[0])
                items = {0: idxpart(units[0])}
                xtrans(items[0])
                if NU > 1:
                    selbuild(units[1])
                for ui in range(NU):
                    ffn(items[ui])
                    if ui + 1 < NU:
                        items[ui + 1] = idxpart(units[ui + 1])
                    down(items[ui])
                    if ui + 1 < NU:
                        xtrans(items[ui + 1])
                    if ui + 2 < NU:
                        selbuild(units[ui + 2])
                    e_done = units[ui][0]
                    if (ui + 1 == NU or units[ui + 1][0] != e_done) and nxt_w < 16:
                        weights(nxt_w)
                        nxt_w += 1
                    del items[ui]
            k.barrier()
            if stop_here("exp%d" % l):
                break
        else:
            with ExitStack() as s:
                gfin = k.sb([128, D], F32, es=s)
                k.dma(gfin, g_bc_d[4])
                xt_r = Rot([k.sb([128, D], F32, es=s) for _ in range(3)])
                junk = k.sb([128, D], F32, es=s)
                ss_pool = Rot([k.sb([128, 1], F32, es=s) for _ in range(8)])
                for n in range(2, NTILE):
                    c0 = n * 128
                    xt = xt_r.get()
                    k.dma(xt, rows_key(X, n)[c0:c0 + 128, :])
                    ss = ss_pool.get()
                    k.act(junk, xt, AF.Square, accum=ss)
                    rstd = ss_pool.get()
                    k.rsq(rstd, ss, 1.0 / D, EPS)
                    k.stt(xt, xt, rstd, gfin, ALU.mult, ALU.mult)
                    k.dma(out_d.sl(n, np.s_[c0 - NTC:c0 - NTC + 128, :]), xt)
        k.barrier()
        print("ninst", k.ninst, k.cnt, flush=True)
    return nc


_NC_CACHE = {}


def kernel(**inputs):
    inp = {kk: np.asarray(v) for kk, v in inputs.items()}
    shared = _prep_shared(inp)
    if "nc" not in _NC_CACHE:
        _NC_CACHE["nc"] = build()
    nc = _NC_CACHE["nc"]
    in_maps = []
    for b in range(8):
        m = dict(shared)
        m.update(_prep_core(inp, b))
        in_maps.append(m)
    res = run_bass_kernel_spmd(nc, in_maps, core_ids=list(range(8)))
    out = np.stack([np.asarray(r["out"], dtype=np.float32) for r in res.results], 0)
    return out
```

```python
import numpy as np
from contextlib import ExitStack
import concourse.bass as bass
import concourse.mybir as mybir
from concourse.bass_utils import run_bass_kernel_spmd

F32 = mybir.dt.float32
BF16 = mybir.dt.bfloat16
I32 = mybir.dt.int32
U32 = mybir.dt.uint32
AF = mybir.ActivationFunctionType
ALU = mybir.AluOpType
AX = mybir.AxisListType


class T:
    __slots__ = ("ap", "key")

    def __init__(self, ap, key):
        self.ap = ap
        self.key = key

    def __getitem__(self, idx):
        return T(self.ap[idx], self.key)

    def sub(self, suffix):
        return T(self.ap, (self.key, suffix))

    def sl(self, suffix, idx):
        return T(self.ap[idx], (self.key, suffix))


def _key(x):
    return x.key if isinstance(x, T) else x


class KB:
    CE = ("pe", "act", "dve", "pool")

    def __init__(self, nc, es, nds=14):
        self.nc = nc
        self.es = es
        self.E = {"pe": nc.tensor, "act": nc.scalar, "dve": nc.vector,
                  "pool": nc.gpsimd, "sp": nc.sync}
        self.csem = {e: es.enter_context(nc.semaphore("c_" + e)) for e in self.CE}
        self.cnt = {e: 0 for e in self.CE}
        self.NDS = nds
        self.dsem = [es.enter_context(nc.semaphore("d%d" % i)) for i in range(nds)]
        self.dcnt = [0] * nds
        self.dnext = 0
        self.waited = {e: {} for e in self.E}
        self.lastw = {}
        self.readers = {}
        self.nalloc = 0
        self.ninst = 0

    def sb(self, shape, dtype, name=None, es=None):
        self.nalloc += 1
        name = name or ("t%d" % self.nalloc)
        t = (es or self.es).enter_context(self.nc.sbuf_tensor(name + "_%d" % self.nalloc, list(shape), dtype))
        return T(t[:], name + "_%d" % self.nalloc)

    def ps(self, shape, dtype, name=None, es=None):
        self.nalloc += 1
        name = name or ("p%d" % self.nalloc)
        t = (es or self.es).enter_context(self.nc.psum_tensor(name + "_%d" % self.nalloc, list(shape), dtype))
        return T(t[:], name + "_%d" % self.nalloc)

    def dram(self, name, shape, dtype, kind="Internal"):
        t = self.nc.dram_tensor(name, list(shape), dtype, kind=kind)
        return T(t.ap(), name)

    def _semobj(self, semkey):
        return self.csem[semkey[1]] if semkey[0] == "c" else self.dsem[semkey[1]]

    def _wait(self, e, semkey, val):
        if self.waited[e].get(semkey, 0) >= val:
            return
        self.waited[e][semkey] = val
        self.E[e].wait_ge(self._semobj(semkey), val)

    def _deps(self, e, reads, writes, is_dma):
        for r in reads:
            lw = self.lastw.get(_key(r))
            if lw is not None:
                self._wait(e, lw[0], lw[1])
        for w in writes:
            k = _key(w)
            lw = self.lastw.get(k)
            if lw is not None:
                if not (lw[0] == ("c", e) and e == "pe" and not is_dma):
                    self._wait(e, lw[0], lw[1])
            for sk, v in self.readers.get(k, {}).items():
                if sk == ("c", e) and e == "pe" and not is_dma:
                    continue
                self._wait(e, sk, v)

    def _record(self, tok, reads, writes):
        for r in reads:
            d = self.readers.setdefault(_key(r), {})
            if d.get(tok[0], 0) < tok[1]:
                d[tok[0]] = tok[1]
        for w in writes:
            k = _key(w)
            self.lastw[k] = tok
            self.readers[k] = {}

    def op(self, e, fn, reads=(), writes=()):
        self._deps(e, reads, writes, False)
        ins = fn(self.E[e])
        self.cnt[e] += 1
        ins.then_inc(self.csem[e], 1)
        self._record((("c", e), self.cnt[e]), reads, writes)
        self.ninst += 1
        return ins

    def dma(self, out, in_, q="sp", fn=None, reads=None, writes=None, **kw):
        reads = [in_] if reads is None else reads
        writes = [out] if writes is None else writes
        slot = self.dnext
        self.dnext = (slot + 1) % self.NDS
        if self.dcnt[slot] > 0:
            self._wait(q, ("d", slot), 16 * self.dcnt[slot])
        self._deps(q, reads, writes, True)
        if fn is None:
            ins = self.E[q].dma_start(out=out.ap, in_=in_.ap, **kw)
        else:
            ins = fn(self.E[q])
        self.dcnt[slot] += 1
        ins.then_inc(self.dsem[slot], 16)
        self._record((("d", slot), 16 * self.dcnt[slot]), reads, writes)
        self.ninst += 1
        return ins

    def barrier(self):
        for e in self.E:
            for e2 in self.CE:
                if e2 != e and self.cnt[e2] > 0:
                    self._wait(e, ("c", e2), self.cnt[e2])
            for s in range(self.NDS):
                if self.dcnt[s] > 0:
                    self._wait(e, ("d", s), 16 * self.dcnt[s])

    def mm(self, out, lhsT, rhs, start=True, stop=True, extra_reads=()):
        return self.op("pe", lambda e: e.matmul(out.ap, lhsT.ap, rhs.ap, start=start, stop=stop),
                       reads=[lhsT, rhs, *extra_reads], writes=[out])

    def tr(self, out, in_, ident):
        return self.op("pe", lambda e: e.transpose(out.ap, in_.ap, ident.ap),
                       reads=[in_, ident], writes=[out])

    def act(self, out, in_, func, bias=None, scale=None, accum=None, e="act"):
        kw = {}
        rd = [in_]
        wr = [out]
        if bias is not None:
            if isinstance(bias, T):
                kw["bias"] = bias.ap
                rd.append(bias)
            else:
                kw["bias"] = bias
        if scale is not None:
            if isinstance(scale, T):
                kw["scale"] = scale.ap
                rd.append(scale)
            else:
                kw["scale"] = scale
        if accum is not None:
            kw["accum_out"] = accum.ap
            wr.append(accum)
        return self.op(e, lambda en: en.activation(out.ap, in_.ap, func, **kw), reads=rd, writes=wr)

    def tt(self, out, a, b, op, e="dve"):
        return self.op(e, lambda en: en.tensor_tensor(out.ap, a.ap, b.ap, op), reads=[a, b], writes=[out])

    def ts(self, out, a, s1, op0, s2=None, op1=None, e="dve", accum=None):
        rd = [a]
        wr = [out]
        v1 = s1
        v2 = s2
        if isinstance(s1, T):
            rd.append(s1)
            v1 = s1.ap
        if isinstance(s2, T):
            rd.append(s2)
            v2 = s2.ap
        kw = {}
        if op1 is not None:
            kw["op1"] = op1
        if accum is not None:
            kw["accum_out"] = accum.ap
            wr.append(accum)
        return self.op(e, lambda en: en.tensor_scalar(out.ap, a.ap, v1, v2, op0, **kw), reads=rd, writes=wr)

    def stt(self, out, a, s, b, op0, op1, e="dve"):
        rd = [a, b]
        v = s
        if isinstance(s, T):
            rd.append(s)
            v = s.ap
        return self.op(e, lambda en: en.scalar_tensor_tensor(out.ap, a.ap, v, b.ap, op0, op1), reads=rd, writes=[out])

    def copy(self, out, in_, e="dve"):
        if e == "act":
            return self.op(e, lambda en: en.activation(out.ap, in_.ap, AF.Copy), reads=[in_], writes=[out])
        return self.op(e, lambda en: en.tensor_copy(out.ap, in_.ap), reads=[in_], writes=[out])

    def memset(self, out, v, e="dve"):
        return self.op(e, lambda en: en.memset(out.ap, v), reads=[], writes=[out])

    def rsq(self, dst, src, mul, add):
        self.ts(dst, src, mul, ALU.mult, add, ALU.add)
        self.act(dst, dst, AF.Sqrt)
        self.op("dve", lambda e: e.reciprocal(dst.ap, dst.ap), reads=[dst], writes=[dst])

L_ = 2
D = 1024
NTX = 4096
NTC = 256
NT = NTX + NTC
NTILE = NT // 128
NCOL = 2464
SCALE_MLA = float((64 + 32) ** -0.5)
LN2 = float(np.log(2.0))


def _rope_tables():
    t = np.arange(NTX)
    row = (t // 64).astype(np.float32)
    col = (t % 64).astype(np.float32)

    def tab(dh_half):
        dh = dh_half
        inv = 10000.0 ** (-np.arange(0, dh, 2, dtype=np.float32) / dh)
        return inv

    def build(dtot):
        half = dtot // 2
        inv = tab(half)
        nf = half // 2
        cos = np.zeros((dtot, NTX), np.float32)
        sins = np.zeros((dtot, NTX), np.float32)
        perm = np.zeros((dtot, dtot), np.float32)
        for part, pos in ((0, row), (1, col)):
            base = part * half
            ang = pos[None, :] * inv[:, None]
            c, s = np.cos(ang), np.sin(ang)
            for j in range(nf):
                cos[base + j] = c[j]
                cos[base + nf + j] = c[j]
                sins[base + j] = -s[j]
                sins[base + nf + j] = s[j]
                perm[base + nf + j, base + j] = 1.0
                perm[base + j, base + nf + j] = 1.0
        return cos, sins, perm

    c32, s32, p32 = build(32)
    c64, s64, p64 = build(64)
    c128 = np.concatenate([c64, c64], 0)
    s128 = np.concatenate([s64, s64], 0)
    p128 = np.zeros((128, 128), np.float32)
    p128[:64, :64] = p64
    p128[64:, 64:] = p64
    rope32 = np.stack([np.tile(c32, (4, 1)), np.tile(s32, (4, 1))]).astype(np.float32)
    rope128 = np.stack([c128, s128]).astype(np.float32)
    return rope32, rope128, p32, p128


_CST = {}


def _cst_layout():
    off = 0
    for name, w in (("ident", 128), ("A", 128), ("B", 128), ("C1", 128), ("C2", 128),
                    ("colK1", 1), ("colK2", 1), ("iota", 512), ("triU", 128),
                    ("mprev", 128), ("mnext", 128), ("p128", 128), ("p32", 32), ("pswap", 128), ("bmat", 128), ("p32x4", 128),
                    ("tokid", NTILE * 16 * 2)):
        _CST[name] = (off, w)
        off += w
    return off


NCST = _cst_layout()


def _const_table():
    rope32, rope128, p32, p128 = _rope_tables()
    cst = np.zeros((128, NCST), np.float32)
    p = np.arange(128, dtype=np.float32)[:, None]
    c = np.arange(128, dtype=np.float32)[None, :]

    def put(name, arr):
        o, w = _CST[name]
        cst[:arr.shape[0], o:o + w] = arr
    put("ident", np.eye(128, dtype=np.float32))
    put("A", np.maximum(c - p, 0.0))
    put("B", np.maximum(p - c, 0.0))
    put("C1", np.broadcast_to(c + 1.0, (128, 128)))
    put("C2", np.broadcast_to(128.0 - c, (128, 128)))
    put("colK1", 127.0 - p)
    put("colK2", p)
    put("iota", np.broadcast_to(np.arange(512, dtype=np.float32)[None, :], (128, 512)))
    put("triU", (p <= c).astype(np.float32))
    put("mprev", (p >= c).astype(np.float32))
    put("mnext", (p <= c).astype(np.float32))
    put("p128", p128)
    put("p32", p32)
    p32x4 = np.zeros((128, 128), np.float32)
    for i_ in range(4):
        p32x4[i_ * 32:(i_ + 1) * 32, i_ * 32:(i_ + 1) * 32] = p32
    put("p32x4", p32x4)
    put("pswap", (np.arange(128)[:, None] == (np.arange(128)[None, :] + 64) % 128).astype(np.float32))
    put("bmat", (np.arange(128)[:, None] // 8 == np.arange(128)[None, :] // 8).astype(np.float32))
    rows = (np.arange(NTILE)[None, :] * 128 + np.arange(128)[:, None])
    tok = np.stack([rows // 64, rows % 64], -1).astype(np.float32)
    tok = np.broadcast_to(tok[:, :, None, :], (128, NTILE, 16, 2)).reshape(128, -1)
    put("tokid", tok)
    return cst, rope32, rope128


def _prep_shared(inp):
    f = lambda a: np.ascontiguousarray(a, dtype=np.float32)
    sh = {}
    w_in = inp["w_in"]
    s = np.cumsum([0, 256, 256, 256, 256, 256, 128, 32, 256, 128, 128])
    rq, rk, rv, rg, cq, ckv, kr, wq, wk, wv = [w_in[:, :, s[i]:s[i + 1]] for i in range(10)]
    wk2 = np.concatenate([wk[:, :, 0:64], wk[:, :, 0:64], wk[:, :, 64:128], wk[:, :, 64:128]], -1)
    wv2 = np.concatenate([wv[:, :, 0:64], wv[:, :, 0:64], wv[:, :, 64:128], wv[:, :, 64:128]], -1)
    sh["w_in_r"] = f(np.concatenate([rq, rk, cq, ckv, wq, wk2, kr, rk, rv, rg, wv2], -1))
    assert sh["w_in_r"].shape[-1] == NCOL
    uq = inp["mla_w_uq"]
    sh["w_uq_n"] = f(uq[:, :, :, :64].reshape(L_, 256, 512))
    sh["w_uq_r"] = f(uq[:, :, :, 64:].reshape(L_, 256, 256))
    sh["w_uk"] = f(inp["mla_w_uk"].reshape(L_, 128, 512))
    sh["w_uv"] = f(inp["mla_w_uv"].reshape(L_, 128, 512))
    sh["w_out"] = f(inp["w_out"])
    sh["router_w"] = f(inp["router_w"])
    sh["ada_w"] = f(inp["ada_w"])
    sh["ada_b"] = f(inp["ada_b"].reshape(L_, 1, 6 * D))
    sh["exp_wg"] = f(inp["exp_w_gate"])
    sh["exp_wu"] = f(inp["exp_w_up"])
    sh["exp_wd"] = f(inp["exp_w_down"])
    gb = np.stack([inp["norm1_g"][0], inp["norm2_g"][0], inp["norm1_g"][1], inp["norm2_g"][1], inp["final_g"]])
    sh["g_bc"] = f(np.broadcast_to(gb[:, None, :], (5, 128, D)))
    cols = []
    for l in range(L_):
        qg = inp["mla_qnorm_g"][l].reshape(2, 128).T
        kg = inp["mla_kvnorm_g"][l].reshape(1, 128).T
        df, db, sk = inp["ret_decay_f"][l], inp["ret_decay_b"][l], inp["win_sink"][l]
        rep = np.broadcast_to(np.concatenate([df, db, sk])[None, :], (128, 12))
        hp = (np.arange(128) >= 64).astype(np.int64)
        pp = np.stack([df[0 + hp], df[2 + hp], db[0 + hp], db[2 + hp]], -1)
        cols += [qg, kg, rep, pp]
    sh["small"] = f(np.concatenate(cols, -1))
    cst, rope32, rope128 = _const_table()
    sh["cst"] = cst
    sh["rope32"] = rope32
    sh["rope128"] = rope128
    return sh


def _prep_core(inp, b):
    f = lambda a: np.ascontiguousarray(a, dtype=np.float32)
    d = {}
    d["x0"] = f(np.concatenate([inp["ctx"][b], inp["x"][b]], 0))
    cv = np.stack([inp["c_ctx"], inp["c"][b]])
    cr = cv.reshape(2, 8, 128).transpose(0, 2, 1)
    d["crep"] = f(np.broadcast_to(cr[:, :, :, None], (2, 128, 8, 128)))
    return d

class Rot:
    def __init__(self, tiles):
        self.t = tiles
        self.i = 0

    def get(self):
        t = self.t[self.i % len(self.t)]
        self.i += 1
        return t


def TD(t, pattern, **kw):
    return T(t.ap.rearrange(pattern, **kw), t.key)


def build(upto=None, dbg=False, nlayers=L_):
    nc = bass.Bass("TRN2", target_bir_lowering=False)
    es0 = ExitStack()
    with es0:
        k = KB(nc, es0)
        kind_dbg = "ExternalOutput" if dbg else "Internal"
        din = lambda n, s, dt=F32: k.dram(n, s, dt, kind="ExternalInput")
        x0 = din("x0", [NT, D])
        crep_d = din("crep", [2, 128, 8, 128])
        w_in_d = din("w_in_r", [L_, D, NCOL])
        w_uq_n_d = din("w_uq_n", [L_, 256, 512])
        w_uq_r_d = din("w_uq_r", [L_, 256, 256])
        w_uk_d = din("w_uk", [L_, 128, 512])
        w_uv_d = din("w_uv", [L_, 128, 512])
        w_out_d = din("w_out", [L_, D, D])
        router_d = din("router_w", [L_, D, 16])
        ada_w_d = din("ada_w", [L_, D, 6 * D])
        ada_b_d = din("ada_b", [L_, 1, 6 * D])
        wg_d = din("exp_wg", [L_, 16, D, 768])
        wu_d = din("exp_wu", [L_, 16, D, 768])
        wd_d = din("exp_wd", [L_, 16, 768, D])
        g_bc_d = din("g_bc", [5, 128, D])
        small_d = din("small", [128, L_ * 19])
        cst_d = din("cst", [128, NCST])
        rope32_d = din("rope32", [2, 128, NTX])
        rope128_d = din("rope128", [2, 128, NTX])
        out_d = k.dram("out", [NTX, D], F32, kind="ExternalOutput")
        X = k.dram("X", [NT, D], F32, kind=kind_dbg)
        MODBC = k.dram("MODBC", [2, 6, 128, D], F32, kind=kind_dbg)
        QT = k.dram("QT", [2, 128, NT], BF16, kind=kind_dbg)
        KT = k.dram("KT", [2, 128, NT], BF16, kind=kind_dbg)
        TMo = k.dram("TMo", [NT, 1024], BF16, kind=kind_dbg)
        QN = k.dram("QN", [4, 128, NT], BF16, kind=kind_dbg)
        KN = k.dram("KN", [4, 128, NT], BF16, kind=kind_dbg)
        QR = k.dram("QR", [8, 32, NT], BF16, kind=kind_dbg)
        KVT = k.dram("KVT", [128, NT], BF16, kind=kind_dbg)
        KRT = k.dram("KRT", [32, NT], BF16, kind=kind_dbg)
        VP = k.dram("VP", [NT, 512], BF16, kind=kind_dbg)
        WQT = k.dram("WQT", [2, 128, NT], BF16, kind=kind_dbg)
        WKT = k.dram("WKT", [2, 128, NT], BF16, kind=kind_dbg)
        YT = k.dram("YT", [8, 128, NT], BF16, kind=kind_dbg)
        H2 = k.dram("H2", [NT, D], BF16, kind=kind_dbg)
        AFFD = k.dram("AFFD", [16, NTX], F32)
        THRD = k.dram("THRD", [128, 1], F32)

        cst = k.sb([128, NCST], F32, "cst")
        k.dma(cst, cst_d)
        small = k.sb([128, L_ * 19], F32, "small")
        k.dma(small, small_d)

        def C(name, rows=128):
            o, w = _CST[name]
            return cst[0:rows, o:o + w]
        ident = C("ident")
        ones_f = k.sb([128, 128], F32, "ones_f")
        k.memset(ones_f, 1.0)
        ones_b = k.sb([128, 128], BF16, "ones_b")
        k.memset(ones_b, 1.0)
        ident_b = k.sb([128, 128], BF16, "ident_b")
        k.copy(ident_b, ident)
        triU_b = k.sb([128, 128], BF16, "triU_b")
        k.copy(triU_b, C("triU"))
        mprev_b = k.sb([128, 128], BF16, "mprev_b")
        k.copy(mprev_b, C("mprev"))
        mnext_b = k.sb([128, 128], BF16, "mnext_b")
        k.copy(mnext_b, C("mnext"))
        Rtab = k.sb([128, NTILE, 16, 4], BF16, "Rtab")
        o_tok, w_tok = _CST["tokid"]
        k.copy(Rtab[:, :, :, 0:2], T(cst.ap[:, o_tok:o_tok + w_tok].rearrange("p (n e c) -> p n e c", n=NTILE, e=16), cst.key))
        affT = k.sb([128, NTILE, 16], F32, "affT")
        posm = k.sb([128, NTILE, 16], F32, "posm")

        EPS = 1e-6
        blocks = [(0, 256, 0)] + [(256 + 512 * i, 512, 1) for i in range(8)]

        def rows_key(t, n):
            return t.sub(("r", n))

        def stop_here(name):
            return upto is not None and upto == name

        def rstd_from_ss(ss, n_feat, es, eps=EPS):
            r = k.sb([128, 1], F32, es=es)
            k.rsq(r, ss, 1.0 / n_feat, eps)
            return r

        for l in range(nlayers):
            need_ctx = l < L_ - 1
            sm0 = l * 19
            Xsrc = x0 if l == 0 else X
            with ExitStack() as s:
                crep = k.sb([128, 2, 8, 128], F32, es=s)
                for st in range(2):
                    k.dma(crep[:, st], crep_d[st])
                sil = k.sb([128, 2, 8, 128], F32, es=s)
                k.act(sil, crep, AF.Silu)
                wb = Rot([k.sb([128, 8, 512], F32, es=s) for _ in range(2)])
                br = Rot([k.sb([1, 512], F32, es=s) for _ in range(2)])
                pp = Rot([k.ps([128, 512], F32, es=s) for _ in range(4)])
                ob = Rot([k.sb([128, 512], F32, es=s) for _ in range(4)])
                aw = TD(ada_w_d[l], "(kc p) n -> p kc n", p=128)
                for nb in range(12):
                    w = wb.get()
                    k.dma(w, aw[:, :, nb * 512:(nb + 1) * 512])
                    b_ = br.get()
                    k.dma(b_, ada_b_d[l, :, nb * 512:(nb + 1) * 512])
                    for st in range(2):
                        ps = pp.get()
                        for kc in range(8):
                            k.mm(ps, sil[:, st, kc, :], w[:, kc, :], start=(kc == 0), stop=False)
                        k.mm(ps, ones_f[0:1, :], b_, start=False, stop=True)
                        o = ob.get()
                        k.copy(o, ps, e=("act" if st == 0 else "dve"))
                        j, half = nb // 2, nb % 2
                        k.dma(MODBC.sl((st, j), np.s_[st, j, :, half * 512:(half + 1) * 512]), o)
            k.barrier()
            if stop_here("mod%d" % l):
                break

            def load_mod(st, j, es, eng_q="sp"):
                t = k.sb([128, D], F32, es=es)
                k.dma(t, MODBC.sl((st, j), np.s_[st, j]))
                return t

            def load_gs(st, j_scale, gidx, es):
                sc = load_mod(st, j_scale, es)
                gb = k.sb([128, D], F32, es=es)
                k.dma(gb, g_bc_d[gidx])
                k.stt(sc, sc, 1.0, gb, ALU.add, ALU.mult)
                return sc

            def load_cast(dst, src, stg, e="pool"):
                t = stg.get()
                sh = list(src.ap.shape)
                tv = t[0:sh[0], 0:sh[1]]
                k.dma(tv, src)
                k.copy(dst, tv, e=e)

            with ExitStack() as s:
                w_in = k.sb([128, 8, NCOL], BF16, es=s)
                stg = Rot([k.sb([128, NCOL], F32, es=s) for _ in range(2)])
                for kc in range(8):
                    load_cast(w_in[:, kc, :], w_in_d[l, kc * 128:(kc + 1) * 128, :], stg, e=("pool" if kc % 2 else "dve"))
                w_uq_n = k.sb([128, 2, 512], BF16, es=s)
                w_uq_r = k.sb([128, 2, 256], BF16, es=s)
                for kc in range(2):
                    load_cast(w_uq_n[:, kc, :], w_uq_n_d[l, kc * 128:(kc + 1) * 128, :], stg)
                    load_cast(w_uq_r[:, kc, :], w_uq_r_d[l, kc * 128:(kc + 1) * 128, :], stg)
                w_uk = k.sb([128, 512], BF16, es=s)
                load_cast(w_uk, w_uk_d[l], stg)
                w_uv = k.sb([128, 512], BF16, es=s)
                load_cast(w_uv, w_uv_d[l], stg)
                gs1 = [load_gs(st, 1, l * 2 + 0, s) for st in range(2)]
                sh1 = [load_mod(st, 0, s) for st in range(2)]
                qg = small[:, sm0 + 0:sm0 + 2]
                kg = small[:, sm0 + 2:sm0 + 3]
                xt_r = Rot([k.sb([128, D], F32, es=s) for _ in range(3)])
                xs_r = Rot([k.sb([128, D], F32, es=s) for _ in range(3)])
                junk = k.sb([128, D], F32, es=s)
                ss_pool = Rot([k.sb([128, 1], F32, es=s) for _ in range(8)])
                hT_r = Rot([k.sb([128, 8, 512], BF16, es=s) for _ in range(2)])
                ptr = Rot([k.ps([128, 8, 128], BF16, es=s) for _ in range(2)])
                xb_r = Rot([k.sb([128, D], BF16, es=s) for _ in range(2)])
                pfm = Rot([k.ps([128, 512], F32, es=s) for _ in range(3)])
                pex = Rot([k.ps([128, 512], F32, es=s) for _ in range(3)])
                ob16 = Rot([k.sb([128, 512], BF16, es=s) for _ in range(6)])
                f32t = Rot([k.sb([128, 512], F32, es=s) for _ in range(6)])
                rp32 = Rot([k.sb([128, 2, 512], F32, es=s) for _ in range(2)])
                rp128 = Rot([k.sb([128, 2, 512], F32, es=s) for _ in range(2)])
                cqg = k.sb([128, 2, 512], BF16, es=s)
                rstdq = k.sb([128, 512], F32, es=s)
                kvT_sb = k.sb([128, 512], BF16, es=s)
                tmo = Rot([k.sb([128, 1024], BF16, es=s) for _ in range(2)])

                def bc_rstd(dst, sq_list, nfeat, n):
                    ps = pex.get()
                    for i, sq in enumerate(sq_list):
                        k.mm(ps[:, :n], ones_f, sq, start=(i == 0), stop=(i == len(sq_list) - 1))
                    k.rsq(dst[:, :n], ps[:, :n], 1.0 / nfeat, EPS)

                def rope_apply(src_f32, M, n, tabs, perm, out_bf):
                    ps = pex.get()
                    k.mm(ps[0:M, :n], perm, src_f32[0:M, :n])
                    t1 = f32t.get()
                    k.tt(t1[0:M, :n], src_f32[0:M, :n], tabs[0:M, 0, :n], ALU.mult)
                    t2 = f32t.get()
                    k.tt(t2[0:M, :n], ps[0:M, :n], tabs[0:M, 1, :n], ALU.mult)
                    k.tt(out_bf[0:M, :n], t1[0:M, :n], t2[0:M, :n], ALU.add, e="pool")

                def norm_phase(blk):
                    (r0, n, st) = blk
                    nt = n // 128
                    hT = hT_r.get()
                    r32 = r128 = None
                    if st == 1:
                        t0 = r0 - NTC
                        r32 = rp32.get()
                        k.dma(r32[:, :, :n], TD(rope32_d, "a p t -> p a t")[:, :, t0:t0 + n])
                        r128 = rp128.get()
                        k.dma(r128[:, :, :n], TD(rope128_d, "a p t -> p a t")[:, :, t0:t0 + n])
                    for ti in range(nt):
                        rr = r0 + ti * 128
                        xt = xt_r.get()
                        k.dma(xt, rows_key(Xsrc, rr // 128)[rr:rr + 128, :])
                        ss = ss_pool.get()
                        k.act(junk, xt, AF.Square, accum=ss)
                        rstd = ss_pool.get()
                        k.rsq(rstd, ss, 1.0 / D, EPS)
                        xs = xs_r.get()
                        k.stt(xs, xt, rstd, gs1[st], ALU.mult, ALU.mult)
                        xb = xb_r.get()
                        k.tt(xb, xs, sh1[st], ALU.add)
                        pt = ptr.get()
                        for kc in range(8):
                            k.tr(pt[:, kc, :], xb[:, kc * 128:(kc + 1) * 128], ident_b)
                        k.copy(hT[:, :, ti * 128:(ti + 1) * 128], pt, e="act")
                    return (hT, r32, r128)

                def proj_phase(blk, ctx_):
                    (r0, n, st) = blk
                    nt = n // 128
                    hT, r32, r128 = ctx_
                    def fm(c, M=128):
                        ps = pfm.get()
                        for kc in range(8):
                            k.mm(ps[0:M, :n], w_in[:, kc, c * 128:c * 128 + M], hT[:, kc, :n], start=(kc == 0), stop=(kc == 7))
                        return ps
                    for c in range(4):
                        ps = fm(c)
                        o = ob16.get()
                        k.copy(o[:, :n], ps[:, :n], e=("act" if c % 2 == 0 else "dve"))
                        dst = (QT if c < 2 else KT)
                        k.dma(dst.sl((c % 2, r0), np.s_[c % 2, :, r0:r0 + n]), o[:, :n])
                    sqs = []
                    for c2 in range(2):
                        ps = fm(4 + c2)
                        sq = f32t.get()
                        k.act(sq[:, :n], ps[:, :n], AF.Square)
                        sqs.append(sq[:, :n])
                        k.act(cqg[:, c2, :n], ps[:, :n], AF.Copy, scale=qg[:, c2:c2 + 1])
                    bc_rstd(rstdq, sqs, 256, n)
                    for pr in range(4):
                        ps = pex.get()
                        for c2 in range(2):
                            k.mm(ps[:, :n], w_uq_n[:, c2, pr * 128:(pr + 1) * 128], cqg[:, c2, :n], start=(c2 == 0), stop=(c2 == 1))
                        qn = ob16.get()
                        k.tt(qn[:, :n], ps[:, :n], rstdq[:, :n], ALU.mult)
                        k.dma(QN.sl((pr, r0), np.s_[pr, :, r0:r0 + n]), qn[:, :n])
                    for g4 in range(2):
                        ps = pex.get()
                        for c2 in range(2):
                            k.mm(ps[:, :n], w_uq_r[:, c2, g4 * 128:(g4 + 1) * 128], cqg[:, c2, :n], start=(c2 == 0), stop=(c2 == 1))
                        qr = f32t.get()
                        k.tt(qr[:, :n], ps[:, :n], rstdq[:, :n], ALU.mult)
                        o = ob16.get()
                        if st == 1:
                            rope_apply(qr, 128, n, r32, C("p32x4"), o)
                        else:
                            k.copy(o[:, :n], qr[:, :n], e="pool")
                        for hh in range(4):
                            h = g4 * 4 + hh
                            k.dma(QR.sl((h, r0), np.s_[h, :, r0:r0 + n]), o[hh * 32:(hh + 1) * 32, :n])
                    ps = fm(6)
                    sq = f32t.get()
                    k.act(sq[:, :n], ps[:, :n], AF.Square)
                    kvg = f32t.get()
                    k.act(kvg[:, :n], ps[:, :n], AF.Copy, scale=kg[:, 0:1])
                    rk_ = f32t.get()
                    bc_rstd(rk_, [sq[:, :n]], 128, n)
                    k.tt(kvT_sb[:, :n], kvg[:, :n], rk_[:, :n], ALU.mult)
                    k.dma(KVT.sl(r0, np.s_[:, r0:r0 + n]), kvT_sb[:, :n])
                    for pr in range(4):
                        ps = pex.get()
                        k.mm(ps[:, :n], w_uk[:, pr * 128:(pr + 1) * 128], kvT_sb[:, :n])
                        o = ob16.get()
                        k.copy(o[:, :n], ps[:, :n], e=("act" if pr % 2 == 0 else "dve"))
                        k.dma(KN.sl((pr, r0), np.s_[pr, :, r0:r0 + n]), o[:, :n])
                    for ti in range(nt):
                        ps = pex.get()
                        k.mm(ps, kvT_sb[:, ti * 128:(ti + 1) * 128], w_uv)
                        o = ob16.get()
                        k.copy(o, ps, e="act")
                        rr = r0 + ti * 128
                        k.dma(VP.sl(rr // 128, np.s_[rr:rr + 128, :]), o)
                    for c in range(4):
                        ps = fm(7 + c)
                        o = ob16.get()
                        if st == 1:
                            sf = f32t.get()
                            k.copy(sf[:, :n], ps[:, :n], e="act")
                            rope_apply(sf, 128, n, r128, C("p128"), o)
                        else:
                            k.copy(o[:, :n], ps[:, :n], e="act")
                        dst = (WQT if c < 2 else WKT)
                        k.dma(dst.sl((c % 2, r0), np.s_[c % 2, :, r0:r0 + n]), o[:, :n])
                    ps = fm(11, 32)
                    o = ob16.get()
                    if st == 1:
                        sf = f32t.get()
                        k.copy(sf[0:32, :n], ps[0:32, :n], e="act")
                        rope_apply(sf, 32, n, r32, C("p32", 32), o)
                    else:
                        k.copy(o[0:32, :n], ps[0:32, :n], e="act")
                    k.dma(KRT.sl(r0, np.s_[:, r0:r0 + n]), o[0:32, :n])
                    for ti in range(nt):
                        o = tmo.get()
                        for hf in range(2):
                            ps = pfm.get()
                            c0 = 1440 + hf * 512
                            for kc in range(8):
                                k.mm(ps, hT[:, kc, ti * 128:(ti + 1) * 128], w_in[:, kc, c0:c0 + 512], start=(kc == 0), stop=(kc == 7))
                            k.copy(o[:, hf * 512:(hf + 1) * 512], ps, e=("act" if hf == 0 else "dve"))
                        rr = r0 + ti * 128
                        k.dma(TMo.sl(rr // 128, np.s_[rr:rr + 128, :]), o)
                pctx = norm_phase(blocks[0])
                for bi, blk in enumerate(blocks):
                    nctx = norm_phase(blocks[bi + 1]) if bi + 1 < len(blocks) else None
                    proj_phase(blk, pctx)
                    pctx = nctx
            k.barrier()
            if stop_here("s1_%d" % l):
                break
            with ExitStack() as s:
                lg_rep = k.sb([128, 8], F32, es=s)
                lg_pp = k.sb([128, 4], F32, es=s)
                for dst, src in ((lg_rep, small[:, sm0 + 3:sm0 + 11]), (lg_pp, small[:, sm0 + 15:sm0 + 19])):
                    k.act(dst, src, AF.Exp, scale=LN2)
                    k.ts(dst, dst, -1.0, ALU.mult, 1.0, ALU.add)
                    k.act(dst, dst, AF.Ln)
                LN8 = float(np.log(0.125))
                DT = k.sb([128, 4, 128], F32, es=s)
                tmpA = k.sb([128, 128], F32, es=s)
                for h in range(4):
                    k.ts(tmpA, C("A"), lg_rep[:, h:h + 1], ALU.mult)
                    k.stt(tmpA, C("B"), lg_rep[:, 4 + h:5 + h], tmpA, ALU.mult, ALU.add)
                    k.act(DT[:, h, :], tmpA, AF.Exp)
                    k.ts(DT[:, h, :], DT[:, h, :], 0.125, ALU.mult)
                QWF = k.sb([128, 2, 128], F32, es=s)
                QWB = k.sb([128, 2, 128], F32, es=s)
                gch = k.sb([128, 4], F32, es=s)
                for pr in range(2):
                    k.act(QWF[:, pr, :], C("C1"), AF.Exp, scale=lg_pp[:, pr:pr + 1])
                    k.act(QWB[:, pr, :], C("C2"), AF.Exp, scale=lg_pp[:, 2 + pr:3 + pr])
                k.act(gch, lg_pp, AF.Exp, scale=128.0)
                KWF = k.sb([128, 256], F32, es=s)
                KWB = k.sb([128, 256], F32, es=s)
                kcol = k.sb([128, 8], F32, es=s)
                for h in range(4):
                    k.act(kcol[:, h:h + 1], C("colK1"), AF.Exp, scale=lg_rep[:, h:h + 1])
                    k.act(kcol[:, 4 + h:5 + h], C("colK2"), AF.Exp, scale=lg_rep[:, 4 + h:5 + h])
                k.ts(kcol, kcol, 0.125, ALU.mult)
                for h in range(4):
                    k.copy(KWF[:, h * 64:(h + 1) * 64], T(kcol.ap[:, h:h + 1].to_broadcast([128, 64]), kcol.key))
                    k.copy(KWB[:, h * 64:(h + 1) * 64], T(kcol.ap[:, 4 + h:5 + h].to_broadcast([128, 64]), kcol.key))
                KVd = k.sb([128, NTILE, 4, 64], F32, es=s)
                Sfb = k.sb([128, NTILE, 2, 64], BF16, es=s)
                Sbb = k.sb([128, NTILE, 2, 64], BF16, es=s)
                kv_r = Rot([k.sb([128, 512], BF16, es=s) for _ in range(3)])
                kw_r = Rot([k.sb([128, 2, 256], BF16, es=s) for _ in range(2)])
                pkv = Rot([k.ps([128, 4, 128], F32, es=s) for _ in range(2)])
                fwd_order = list(range(NTILE))
                bwd_order = [1, 0] + list(range(NTILE - 1, 1, -1))
                r1_order = []
                for a_, b_ in zip(fwd_order, bwd_order):
                    for t_ in (a_, b_):
                        if t_ not in r1_order:
                            r1_order.append(t_)
                curs = []
                for d_ in range(2):
                    c_ = k.sb([128, 2, 64], F32, es=s)
                    k.memset(c_, 0.0)
                    curs.append(c_)
                ptrs = [0, 0]
                done = set()

                def scan_step(d_, n):
                    Sb_ = Sfb if d_ == 0 else Sbb
                    cur = curs[d_]
                    k.copy(Sb_.sl(n, np.s_[:, n, :, :]), cur)
                    for pr in range(2):
                        k.stt(cur[:, pr, :], cur[:, pr, :], gch[:, d_ * 2 + pr:d_ * 2 + pr + 1],
                              KVd.sl(n, np.s_[:, n, d_ * 2 + pr, :]), ALU.mult, ALU.add)
                for n in r1_order:
                    kvt = kv_r.get()
                    k.dma(kvt, TMo.sl(n, np.s_[n * 128:(n + 1) * 128, 0:512]))
                    kw = kw_r.get()
                    k.tt(kw[:, 0, :], kvt[:, 0:256], KWF, ALU.mult)
                    k.tt(kw[:, 1, :], kvt[:, 0:256], KWB, ALU.mult)
                    ps = pkv.get()
                    for d_ in range(2):
                        for pr in range(2):
                            k.mm(ps[:, d_ * 2 + pr, :], kw[:, d_, pr * 128:(pr + 1) * 128], kvt[:, 256 + pr * 128:256 + (pr + 1) * 128])
                    k.copy(KVd.sl(n, np.s_[0:64, n, :, :]), ps[0:64, :, 0:64], e="act")
                    k.copy(KVd.sl(n, np.s_[64:128, n, :, :]), ps[64:128, :, 64:128], e="act")
                    done.add(n)
                    for d_, order in ((0, fwd_order), (1, bwd_order)):
                        while ptrs[d_] < NTILE and order[ptrs[d_]] in done:
                            scan_step(d_, order[ptrs[d_]])
                            ptrs[d_] += 1
                qk_r = Rot([k.sb([128, 2, 2, 128], BF16, es=s) for _ in range(3)])
                vg_r = Rot([k.sb([128, 512], BF16, es=s) for _ in range(3)])
                qw_r = Rot([k.sb([128, 2, 2, 128], BF16, es=s) for _ in range(2)])
                sm_r = Rot([k.sb([128, 128], BF16, es=s) for _ in range(4)])
                pss = Rot([k.ps([128, 512], F32, es=s) for _ in range(2)])
                psy = Rot([k.ps([128, 512], F32, es=s) for _ in range(2)])
                ysb_r = Rot([k.sb([128, 256], F32, es=s) for _ in range(3)])
                sq_r = Rot([k.sb([128, 256], F32, es=s) for _ in range(2)])
                st_r = Rot([k.sb([128, 16], F32, es=s) for _ in range(2)])
                sg_r = Rot([k.sb([128, 256], F32, es=s) for _ in range(3)])
                yo_r = Rot([k.sb([128, 2, 128], BF16, es=s) for _ in range(2)])
                yb_r = Rot([k.sb([128, 256], BF16, es=s) for _ in range(2)])
                ptb2 = Rot([k.ps([128, 2, 128], BF16, es=s) for _ in range(2)])

                def r2A(n):
                    c0 = n * 128
                    qk = qk_r.get()
                    k.dma(qk[:, 0], TD(QT, "c p t -> p c t")[:, :, c0:c0 + 128])
                    k.dma(qk[:, 1], TD(KT, "c p t -> p c t")[:, :, c0:c0 + 128])
                    vg = vg_r.get()
                    k.dma(vg, TMo.sl(n, np.s_[c0:c0 + 128, 256:768]))
                    qw = qw_r.get()
                    k.tt(qw[:, 0], qk[:, 0], QWF, ALU.mult)
                    k.tt(qw[:, 1], qk[:, 0], QWB, ALU.mult)
                    py = psy.get()
                    for h in range(4):
                        pr, off = h // 2, (h % 2) * 64
                        ps = pss.get()
                        k.mm(ps[:, 0:128], qk[off:off + 64, 1, pr, :], qk[off:off + 64, 0, pr, :])
                        sm = sm_r.get()
                        k.tt(sm, ps[:, 0:128], DT[:, h, :], ALU.mult)
                        yo_ = py[:, h * 64:(h + 1) * 64]
                        k.mm(yo_, sm, vg[:, h * 64:(h + 1) * 64], start=True, stop=False)
                        k.mm(yo_, qw[off:off + 64, 0, pr, :], Sfb.sl(n, np.s_[off:off + 64, n, pr, :]), start=False, stop=False)
                        k.mm(yo_, qw[off:off + 64, 1, pr, :], Sbb.sl(n, np.s_[off:off + 64, n, pr, :]), start=False, stop=True)
                    ysb = ysb_r.get()
                    k.copy(ysb, py[:, 0:256], e="act")
                    sg = sg_r.get()
                    k.act(sg, vg[:, 256:512], AF.Silu)
                    return (ysb, sg)

                def r2B(n, ctx_):
                    ysb, sg = ctx_
                    c0 = n * 128
                    stt_ = st_r.get()
                    k.op("dve", lambda e: e.reduce_sum(stt_.ap[:, 0:4], ysb.ap.rearrange("p (h d) -> p h d", h=4), AX.X), reads=[ysb], writes=[stt_])
                    sq = sq_r.get()
                    k.tt(sq, ysb, ysb, ALU.mult)
                    k.op("dve", lambda e: e.reduce_sum(stt_.ap[:, 4:8], sq.ap.rearrange("p (h d) -> p h d", h=4), AX.X), reads=[sq], writes=[stt_])
                    k.ts(stt_[:, 0:8], stt_[:, 0:8], 1.0 / 64, ALU.mult)
                    k.tt(stt_[:, 8:12], stt_[:, 0:4], stt_[:, 0:4], ALU.mult)
                    k.tt(stt_[:, 12:16], stt_[:, 4:8], stt_[:, 8:12], ALU.subtract)
                    k.rsq(stt_[:, 12:16], stt_[:, 12:16], 1.0, 1e-5)
                    for h in range(4):
                        k.ts(ysb[:, h * 64:(h + 1) * 64], ysb[:, h * 64:(h + 1) * 64], stt_[:, h:h + 1], ALU.subtract,
                             stt_[:, 12 + h:13 + h], ALU.mult)
                    yb = yb_r.get()
                    k.tt(yb, ysb, sg, ALU.mult)
                    pt = ptb2.get()
                    for c in range(2):
                        k.tr(pt[:, c, :], yb[:, c * 128:(c + 1) * 128], ident_b)
                    yo = yo_r.get()
                    k.copy(yo, pt, e="act")
                    k.dma(TD(YT, "c p t -> p c t").sl(("ret", n), np.s_[:, 0:2, c0:c0 + 128]), yo)
                tiles_r = list(range(0 if need_ctx else 2, NTILE))
                pc_ = r2A(tiles_r[0])
                for ti_, n in enumerate(tiles_r):
                    nc_ = r2A(tiles_r[ti_ + 1]) if ti_ + 1 < len(tiles_r) else None
                    r2B(n, pc_)
                    pc_ = nc_
            k.barrier()
            if stop_here("ret%d" % l):
                break
            with ExitStack() as s:
                vp2 = k.sb([128, NTILE, 8, 128], BF16, es=s)
                VPv = TD(VP, "(t p) (h d) -> p t h d", p=128, d=64)
                for par in range(2):
                    k.memset(vp2[:, :, par::2, (1 - par) * 64:(2 - par) * 64], 1.0, e=("dve" if par == 0 else "pool"))
                    for h in range(par, 8, 2):
                        k.dma(vp2[:, :, h, par * 64:(par + 1) * 64], VPv[:, :, h, :])
                kh_l = [k.sb([128, NT], BF16, es=s) for _ in range(2)]
                for t_ in kh_l:
                    k.memset(t_, 0.0, e="pool")
                kh_r = Rot(kh_l)
                q_l = [k.sb([128, 512], BF16, es=s) for _ in range(3)]
                for t_ in q_l:
                    k.memset(t_, 0.0)
                q_r = Rot(q_l)
                rd_sets = []
                for par in range(2):
                    tl = [k.sb([128, 512], F32, es=s) for _ in range(2)]
                    for t_ in tl:
                        k.memset(t_, 0.0)
                    rd_sets.append(Rot(tl))
                pT_r = Rot([k.sb([128, 2, 512], BF16, es=s) for _ in range(4)])
                pss = Rot([k.ps([128, 2, 512], F32, es=s) for _ in range(2)])
                pacc = Rot([k.ps([128, 512], F32, es=s) for _ in range(4)])
                sw_r = Rot([k.sb([128, 512], F32, es=s) for _ in range(2)])
                yp_r = Rot([k.sb([128, 512], BF16, es=s) for _ in range(3)])
                qblocks = ([(0, 256, [0, 1])] if need_ctx else []) + [(256 + 512 * i, 512, list(range(NTILE))) for i in range(8)]
                LOOK = 1
                pend = []

                def emit_pv2(item):
                    h, r0, n, j, npair, kt2, pT, pv = item
                    for u_ in range(2):
                        first = (j == 0 and u_ == 0)
                        last = (j == npair - 1 and u_ == 1)
                        k.mm(pv[:, :n], vp2[:, kt2[u_], h, :], pT[:, u_, :n], start=first, stop=last)
                    if j == npair - 1:
                        off = (h % 2) * 64
                        dof = 64 - off
                        rd = rd_sets[h % 2].get()
                        k.op("dve", lambda e: e.reciprocal(rd.ap[dof:dof + 64, :n], pv.ap[dof:dof + 64, :n]), reads=[pv], writes=[rd])
                        sws = sw_r.get()
                        k.dma(sws[off:off + 64, :n], rd[dof:dof + 64, :n])
                        yp = yp_r.get()
                        k.tt(yp[off:off + 64, :n], pv[off:off + 64, :n], sws[off:off + 64, :n], ALU.mult)
                        k.dma(YT.sl(("mla", h, r0), np.s_[2 + h // 2, off:off + 64, r0:r0 + n]), yp[off:off + 64, :n])
                for h in range(8):
                    off = (h % 2) * 64
                    kh = kh_r.get()
                    k.dma(kh[0:64, :], KN[h // 2, off:off + 64, :])
                    k.dma(kh[64:96, :], KRT)
                    for (r0, n, kts) in qblocks:
                        q = q_r.get()
                        k.dma(q[0:64, :n], QN[h // 2, off:off + 64, r0:r0 + n])
                        k.dma(q[64:96, :n], QR[h, :, r0:r0 + n])
                        pv = pacc.get()
                        npair = len(kts) // 2
                        for j in range(npair):
                            kt2 = (kts[2 * j], kts[2 * j + 1])
                            ps = pss.get()
                            for u_ in range(2):
                                k.mm(ps[:, u_, :n], kh[:, kt2[u_] * 128:(kt2[u_] + 1) * 128], q[:, :n])
                            pT = pT_r.get()
                            k.act(pT[:, :, :n], ps[:, :, :n], AF.Exp, scale=SCALE_MLA)
                            pend.append((h, r0, n, j, npair, kt2, pT, pv))
                            if len(pend) > LOOK:
                                emit_pv2(pend.pop(0))
                while pend:
                    emit_pv2(pend.pop(0))
            k.barrier()
            if stop_here("mla%d" % l):
                break
            with ExitStack() as s:
                wkT = k.sb([128, 2, 2, NT], BF16, es=s)
                k.memset(wkT, 0.0, e="pool")
                for hk_ in range(2):
                    for g_ in range(2):
                        k.dma(wkT[g_ * 64:(g_ + 1) * 64, hk_, g_, :], WKT[hk_, g_ * 64:(g_ + 1) * 64, :])
                wqT = k.sb([128, 2, NT], BF16, es=s)
                k.dma(wqT, TD(WQT, "c p t -> p c t"))
                wv3 = k.sb([128, NTILE, 4, 128], BF16, es=s)
                WVv = T(TMo.ap[:, 768:1024].rearrange("(t p) (q d) -> p t q d", p=128, d=64), TMo.key)
                for par in range(2):
                    k.memset(wv3[:, :, par::2, (1 - par) * 64:(2 - par) * 64], 1.0, e=("dve" if par == 0 else "pool"))
                    for q_ in range(par, 4, 2):
                        k.dma(wv3[:, :, q_, par * 64:(par + 1) * 64], WVv[:, :, q_, :])
                esink = k.sb([128, 4], F32, es=s)
                k.act(esink, small[:, sm0 + 11:sm0 + 15], AF.Exp)
                rd_sets = []
                for par in range(2):
                    tl = [k.sb([128, 128], F32, es=s) for _ in range(2)]
                    for t_ in tl:
                        k.memset(t_, 0.0)
                    rd_sets.append(Rot(tl))
                pT_r = Rot([k.sb([128, 128], BF16, es=s) for _ in range(6)])
                pss = Rot([k.ps([128, 512], F32, es=s) for _ in range(4)])
                pacc = Rot([k.ps([128, 512], F32, es=s) for _ in range(3)])
                psw = Rot([k.ps([128, 512], F32, es=s) for _ in range(1)])
                sw_r = Rot([k.sb([128, 128], F32, es=s) for _ in range(2)])
                yp_r = Rot([k.sb([128, 128], BF16, es=s) for _ in range(3)])
                pend = []

                def emit_pvw(item):
                    n, qh, i, nk, kt, pT, pv, yp = item
                    hk, g = qh // 2, qh % 2
                    off = g * 64
                    dof = 64 - off
                    c0 = n * 128
                    last = (i == nk - 1)
                    k.mm(pv[:, 0:128], wv3[:, kt, qh, :], pT, start=(i == 0), stop=last)
                    if last:
                        rd = rd_sets[g].get()
                        k.ts(rd[dof:dof + 64, :], pv[dof:dof + 64, 0:128], esink[dof:dof + 64, qh:qh + 1], ALU.add)
                        k.op("dve", lambda e: e.reciprocal(rd.ap[dof:dof + 64, :], rd.ap[dof:dof + 64, :]), reads=[rd], writes=[rd])
                        sw = psw.get()
                        k.mm(sw[:, 0:128], C("pswap"), rd)
                        sws = sw_r.get()
                        k.copy(sws[off:off + 64, :], sw[off:off + 64, 0:128], e="act")
                        k.tt(yp[off:off + 64, :], pv[off:off + 64, 0:128], sws[off:off + 64, :], ALU.mult)
                        if g == 1:
                            k.dma(YT.sl(("win", hk, n), np.s_[6 + hk, :, c0:c0 + 128]), yp)
                yp = None
                for n in range(0 if need_ctx else 2, NTILE):
                    c0 = n * 128
                    if n < 2:
                        keys = [(0, None), (1, None)]
                    else:
                        keys = []
                        if n - 1 >= 2:
                            keys.append((n - 1, mprev_b))
                        keys.append((n, None))
                        if n + 1 < NTILE:
                            keys.append((n + 1, mnext_b))
                        keys += [(0, None), (1, None)]
                    for qh in range(4):
                        hk, g = qh // 2, qh % 2
                        off = g * 64
                        pv = pacc.get()
                        if g == 0:
                            yp = yp_r.get()
                        for i, (kt, msk) in enumerate(keys):
                            ps = pss.get()
                            k.mm(ps[:, 0:128], wkT[:, hk, g, kt * 128:(kt + 1) * 128], wqT[:, hk, c0:c0 + 128])
                            pT = pT_r.get()
                            k.act(pT, ps[:, 0:128], AF.Exp, scale=0.125)
                            if msk is not None:
                                k.tt(pT, pT, msk, ALU.mult)
                            pend.append((n, qh, i, len(keys), kt, pT, pv, yp))
                            if len(pend) > 3:
                                emit_pvw(pend.pop(0))
                while pend:
                    emit_pvw(pend.pop(0))
            k.barrier()
            if stop_here("win%d" % l):
                break
            es_aff = ExitStack()
            aff_e = k.sb([16, NT], F32, "aff_e", es=es_aff)
            with ExitStack() as s:
                w_out = k.sb([128, 8, D], BF16, es=s)
                stg = Rot([k.sb([128, D], F32, es=s) for _ in range(2)])
                for kc in range(8):
                    load_cast(w_out[:, kc, :], w_out_d[l, kc * 128:(kc + 1) * 128, :], stg, e=("pool" if kc % 2 else "dve"))
                rw = k.sb([128, 8, 16], F32, es=s)
                k.dma(rw, TD(router_d[l], "(kc p) e -> p kc e", p=128))
                sts = [0, 1] if need_ctx else [1]
                mod2 = {st: load_mod(st, 2, s) for st in sts}
                gs2 = {st: load_gs(st, 4, l * 2 + 1, s) for st in sts}
                sh2 = {st: load_mod(st, 3, s) for st in sts}
                yT_r = Rot([k.sb([128, 8, 128], BF16, es=s) for _ in range(2)])
                xt_r = Rot([k.sb([128, D], F32, es=s) for _ in range(2)])
                xn_r = Rot([k.sb([128, D], F32, es=s) for _ in range(2)])
                xs_r = Rot([k.sb([128, D], F32, es=s) for _ in range(3)])
                h2b_r = Rot([k.sb([128, D], BF16, es=s) for _ in range(2)])
                h2T_r = Rot([k.sb([128, 8, 128], F32, es=s) for _ in range(2)])
                junk = k.sb([128, D], F32, es=s)
                ss_pool = Rot([k.sb([128, 1], F32, es=s) for _ in range(8)])
                ex_r = Rot([k.sb([128, 16], F32, es=s) for _ in range(2)])
                pso = Rot([k.ps([128, 512], F32, es=s) for _ in range(3)])
                ptr = Rot([k.ps([128, 4, 128], F32, es=s) for _ in range(2)])
                psl = Rot([k.ps([128, 512], F32, es=s) for _ in range(2)])
                def phaseA(n):
                    st = 0 if n < 2 else 1
                    c0 = n * 128
                    yT = yT_r.get()
                    k.dma(yT, TD(YT, "c p t -> p c t")[:, :, c0:c0 + 128])
                    xt = xt_r.get()
                    k.dma(xt, rows_key(Xsrc, n)[c0:c0 + 128, :])
                    xn = xn_r.get()
                    for hf in range(2):
                        ps = pso.get()
                        for kc in range(8):
                            k.mm(ps, yT[:, kc, :], w_out[:, kc, hf * 512:(hf + 1) * 512], start=(kc == 0), stop=(kc == 7))
                        sl_ = np.s_[:, hf * 512:(hf + 1) * 512]
                        k.tt(xn[sl_], ps, mod2[st][sl_], ALU.mult)
                        k.tt(xn[sl_], xn[sl_], xt[sl_], ALU.add)
                    k.dma(rows_key(X, n)[c0:c0 + 128, :], xn)
                    ss = ss_pool.get()
                    k.act(junk, xn, AF.Square, accum=ss)
                    rstd = ss_pool.get()
                    k.rsq(rstd, ss, 1.0 / D, EPS)
                    xs = xs_r.get()
                    k.stt(xs, xn, rstd, gs2[st], ALU.mult, ALU.mult)
                    k.tt(xs, xs, sh2[st], ALU.add)
                    h2b = h2b_r.get()
                    k.copy(h2b, xs, e="act")
                    k.dma(H2.sl(n, np.s_[c0:c0 + 128, :]), h2b)
                    return xs

                def phaseB(n, xs):
                    c0 = n * 128
                    h2T = h2T_r.get()
                    for hf in range(2):
                        pt = ptr.get()
                        for q4 in range(4):
                            kc = hf * 4 + q4
                            k.tr(pt[:, q4, :], xs[:, kc * 128:(kc + 1) * 128], ident)
                        k.copy(h2T[:, hf * 4:(hf + 1) * 4, :], pt, e="act")
                    pl = psl.get()
                    for kc in range(8):
                        k.mm(pl[:, 0:16], h2T[:, kc, :], rw[:, kc, :], start=(kc == 0), stop=(kc == 7))
                    ex = ex_r.get()
                    sm_ = ss_pool.get()
                    k.act(ex, pl[:, 0:16], AF.Exp, accum=sm_)
                    k.op("dve", lambda e: e.reciprocal(sm_.ap, sm_.ap), reads=[sm_], writes=[sm_])
                    k.ts(affT.sl(n, np.s_[:, n, :]), ex, sm_, ALU.mult)
                    pl2 = psl.get()
                    k.tr(pl2[0:16, 0:128], affT.sl(n, np.s_[:, n, :]), ident)
                    k.copy(aff_e.sl(n, np.s_[:, c0:c0 + 128]), pl2[0:16, 0:128], e="act")
                tiles_o = list(range(0 if need_ctx else 2, NTILE))
                pxs = phaseA(tiles_o[0])
                for ti_, n in enumerate(tiles_o):
                    nxs = phaseA(tiles_o[ti_ + 1]) if ti_ + 1 < len(tiles_o) else None
                    phaseB(n, pxs)
                    pxs = nxs
            k.barrier()
            if stop_here("o%d" % l):
                es_aff.close()
                break
            streams = ([(0, 2, 32)] if need_ctx else []) + [(2, 32, 512)]
            with ExitStack() as s:
                work = k.sb([16, NTX], F32, es=s)
                m8 = k.sb([16, 8], F32, es=s)
                thr = k.sb([16, 1], F32, es=s)
                aff128 = k.sb([128, 512], F32, es=s)
                junk16 = k.sb([128, 512], BF16, es=s)
                lo = k.sb([128, 1], F32, es=s)
                hi = k.sb([128, 1], F32, es=s)
                mid = k.sb([128, 1], F32, es=s)
                cnt = k.sb([128, 1], F32, es=s)
                half = k.sb([128, 1], F32, es=s)
                k.memset(half, 0.5)
                mge = k.sb([128, 1], U32, es=s)
                mlt = k.sb([128, 1], U32, es=s)
                thr16 = k.sb([16, 8], F32, es=s)
                mask_e = k.sb([16, NTX], F32, es=s)
                maskTb = k.sb([128, 32, 16], BF16, es=s)
                carry = k.sb([128, 16], F32, es=s)
                pos_r = Rot([k.sb([128, 16], F32, es=s) for _ in range(2)])
                ptk = Rot([k.ps([128, 512], F32, es=s) for _ in range(4)])
                ahi = k.sb([128, NTILE, 16], BF16, es=s)
                alo = k.sb([128, NTILE, 16], F32, es=s)
                k.copy(ahi, affT)
                k.tt(alo, affT, ahi, ALU.subtract)
                k.copy(Rtab[:, :, :, 2], ahi, e="pool")
                k.copy(Rtab[:, :, :, 3], alo, e="pool")
                for (t0, ntl, cap) in streams:
                    ntok = ntl * 128
                    cs = np.s_[:, t0 * 128:t0 * 128 + ntok]
                    if cap <= 64:
                        k.copy(work[:, :ntok], aff_e[cs])
                        for r in range(cap // 8):
                            k.op("dve", lambda e: e.max(out=m8.ap, in_=work.ap[:, :ntok]), reads=[work], writes=[m8])
                            if r < cap // 8 - 1:
                                k.op("dve", lambda e: e.match_replace(out=work.ap[:, :ntok], in_to_replace=m8.ap,
                                                                      in_values=work.ap[:, :ntok], imm_value=-1.0),
                                     reads=[work, m8], writes=[work])
                        k.copy(thr, m8[:, 7:8])
                    else:
                        k.dma(AFFD, aff_e[cs])
                        k.dma(aff128, TD(AFFD, "e (s t) -> (e s) t", s=8))
                        k.memset(lo, 0.0)
                        k.memset(hi, 2.0)
                        for it in range(40):
                            k.stt(mid, lo, hi, half, ALU.add, ALU.mult)
                            k.ts(junk16, aff128, mid, ALU.is_ge, 0.0, ALU.add, accum=cnt)
                            pc = ptk.get()
                            k.mm(pc[:, 0:1], C("bmat"), cnt)
                            k.ts(mge, pc[:, 0:1], cap - 0.5, ALU.is_ge)
                            k.ts(mlt, pc[:, 0:1], cap - 0.5, ALU.is_lt)
                            k.op("dve", lambda e: e.copy_predicated(lo.ap, mge.ap, mid.ap), reads=[mge, mid], writes=[lo])
                            k.op("dve", lambda e: e.copy_predicated(hi.ap, mlt.ap, mid.ap), reads=[mlt, mid], writes=[hi])
                        k.dma(THRD, lo)
                        k.dma(thr16, TD(THRD, "(e s) o -> e (s o)", s=8))
                        k.copy(thr, thr16[:, 0:1])
                    k.ts(mask_e[:, :ntok], aff_e[cs], thr, ALU.is_ge)
                    k.memset(carry, 0.0)
                    for i in range(ntl):
                        n = t0 + i
                        pt = ptk.get()
                        k.tr(pt[:, 0:16], mask_e[:, i * 128:(i + 1) * 128], ident[0:16, 0:16])
                        k.copy(maskTb[:, i, :], pt[:, 0:16], e="act")
                        pc = ptk.get()
                        k.mm(pc[:, 0:16], triU_b, maskTb[:, i, :])
                        k.mm(pc[:, 16:32], ones_b, maskTb[:, i, :])
                        pos = pos_r.get()
                        k.tt(pos, pc[:, 0:16], carry, ALU.add)
                        k.tt(pos, pos, maskTb[:, i, :], ALU.mult)
                        k.ts(posm.sl(n, np.s_[:, n, :]), pos, -1.0, ALU.add)
                        k.tt(carry, carry, pc[:, 16:32], ALU.add)
            k.barrier()
            if stop_here("topk%d" % l):
                es_aff.close()
                break
            es_aff.close()
            with ExitStack() as s:
                mod5 = {st: load_mod(st, 5, s) for st in ([0, 1] if need_ctx else [1])}
                wsets = Rot([(k.sb([128, 8, 768], BF16, es=s), k.sb([128, 8, 768], BF16, es=s), k.sb([128, 6, D], BF16, es=s))
                             for _ in range(2)])
                stg = Rot([k.sb([128, D], F32, es=s) for _ in range(4)])
                Sel = k.sb([128, 32, 512], BF16, es=s)
                iota16 = k.sb([128, 512], mybir.dt.int16, es=s)
                k.copy(iota16, C("iota"))
                r4T_r = Rot([k.sb([4, 512], F32, es=s) for _ in range(2)])
                r4_r = Rot([k.sb([128, 16], F32, es=s) for _ in range(3)])
                idx_r = Rot([k.sb([128, 1], I32, es=s) for _ in range(12)])
                idf_r = Rot([k.sb([128, 1], F32, es=s) for _ in range(4)])
                gt_r = Rot([k.sb([128, 1], F32, es=s) for _ in range(12)])
                xs_r = Rot([k.sb([128, D], BF16, es=s) for _ in range(8)])
                xsT_r = Rot([k.sb([128, 8, 512], BF16, es=s) for _ in range(2)])
                hid = k.sb([128, 6, 512], BF16, es=s)
                sg_r = Rot([k.sb([128, 512], F32, es=s) for _ in range(2)])
                ys_r = Rot([k.sb([128, D], F32, es=s) for _ in range(2)])
                p4 = Rot([k.ps([128, 512], F32, es=s) for _ in range(1)])
                ptb = Rot([k.ps([128, 8, 128], BF16, es=s) for _ in range(1)])
                pgu = Rot([k.ps([128, 512], F32, es=s) for _ in range(4)])
                pdn = Rot([k.ps([128, 512], F32, es=s) for _ in range(2)])
                Xall = [rows_key(X, n) for n in range(NTILE)] + [X.sub("all")]
                cast_eng = Rot(["act", "dve"])
                wcache = {}

                def weights(e_):
                    wg, wu, wd = wsets.get()
                    for kc in range(8):
                        load_cast(wg[:, kc, :], wg_d[l, e_, kc * 128:(kc + 1) * 128, :], stg, e=cast_eng.get())
                        load_cast(wu[:, kc, :], wu_d[l, e_, kc * 128:(kc + 1) * 128, :], stg, e=cast_eng.get())
                    for fc in range(6):
                        load_cast(wd[:, fc, :], wd_d[l, e_, fc * 128:(fc + 1) * 128, :], stg, e=cast_eng.get())
                    wcache[e_] = (wg, wu, wd)

                def selbuild(u):
                    e_, (t0, ntl, cap) = u
                    for i in range(ntl):
                        k.ts(Sel[:, i, :cap], iota16[:, :cap], posm[:, t0 + i, e_:e_ + 1], ALU.is_equal)

                def idxpart(u):
                    e_, (t0, ntl, cap) = u
                    ps = p4.get()
                    for i in range(ntl):
                        k.mm(ps[0:4, :cap], Rtab[:, t0 + i, e_, :], Sel[:, i, :cap], start=(i == 0), stop=(i == ntl - 1))
                    r4T = r4T_r.get()
                    k.copy(r4T[:, :cap], ps[0:4, :cap])
                    stiles = [(s0, min(128, cap - s0)) for s0 in range(0, cap, 128)]
                    for si, (s0, nsl) in enumerate(stiles):
                        k.tr(ps[0:nsl, si * 4:(si + 1) * 4], r4T[0:4, s0:s0 + nsl], ident[0:4, 0:4])
                    nsl0 = stiles[0][1]
                    r4 = r4_r.get()
                    k.copy(r4[0:nsl0, 0:4 * len(stiles)], ps[0:nsl0, 0:4 * len(stiles)])
                    meta = []
                    xss = []
                    for si, (s0, nsl) in enumerate(stiles):
                        c4 = si * 4
                        idf = idf_r.get()
                        k.stt(idf[0:nsl, :], r4[0:nsl, c4:c4 + 1], 64.0, r4[0:nsl, c4 + 1:c4 + 2], ALU.mult, ALU.add)
                        idx = idx_r.get()
                        k.copy(idx[0:nsl, :], idf[0:nsl, :])
                        gt = gt_r.get()
                        k.tt(gt[0:nsl, :], r4[0:nsl, c4 + 2:c4 + 3], r4[0:nsl, c4 + 3:c4 + 4], ALU.add)
                        meta.append((idx, gt))
                        xs = xs_r.get()
                        k.dma(xs[0:nsl, :], H2, q="pool", reads=[idx],
                              fn=lambda en: en.indirect_dma_start(out=xs.ap[0:nsl, :], out_offset=None, in_=H2.ap,
                                                                  in_offset=bass.IndirectOffsetOnAxis(ap=idx.ap[0:nsl, :], axis=0)))
                        xss.append(xs)
                    return [u, stiles, meta, xss, None]

                def xtrans(item):
                    u, stiles, meta, xss, _ = item
                    xsT = xsT_r.get()
                    for si, (s0, nsl) in enumerate(stiles):
                        xs = xss[si]
                        pt = ptb.get()
                        for kc in range(8):
                            k.tr(pt[:, kc, 0:nsl], xs[0:nsl, kc * 128:(kc + 1) * 128], ident_b[0:nsl, 0:nsl])
                        k.copy(xsT[:, :, s0:s0 + nsl], pt[:, :, 0:nsl], e="act")
                    item[4] = xsT

                def ffn(item):
                    (e_, (t0, ntl, cap)), stiles, meta, xss, xsT = item
                    wg, wu, wd = wcache[e_]
                    for fc in range(6):
                        pg = pgu.get()
                        pu = pgu.get()
                        for kc in range(8):
                            k.mm(pg[:, :cap], wg[:, kc, fc * 128:(fc + 1) * 128], xsT[:, kc, :cap], start=(kc == 0), stop=(kc == 7))
                        for kc in range(8):
                            k.mm(pu[:, :cap], wu[:, kc, fc * 128:(fc + 1) * 128], xsT[:, kc, :cap], start=(kc == 0), stop=(kc == 7))
                        sg = sg_r.get()
                        k.act(sg[:, :cap], pg[:, :cap], AF.Silu)
                        k.tt(hid[:, fc, :cap], sg[:, :cap], pu[:, :cap], ALU.mult)

                def down(item):
                    (e_, (t0, ntl, cap)), stiles, meta, xss, xsT = item
                    st = 0 if t0 == 0 else 1
                    wg, wu, wd = wcache[e_]
                    for si, (s0, nsl) in enumerate(stiles):
                        idx, gt = meta[si]
                        ys = ys_r.get()
                        for hf in range(2):
                            pd = pdn.get()
                            for fc in range(6):
                                k.mm(pd[0:nsl, :], hid[:, fc, s0:s0 + nsl], wd[:, fc, hf * 512:(hf + 1) * 512], start=(fc == 0), stop=(fc == 5))
                            k.stt(ys[0:nsl, hf * 512:(hf + 1) * 512], pd[0:nsl, :], gt[0:nsl, 0:1],
                                  mod5[st][0:nsl, hf * 512:(hf + 1) * 512], ALU.mult, ALU.mult)
                        k.dma(X, ys[0:nsl, :], q="pool", reads=[ys, idx], writes=Xall,
                              fn=lambda en: en.indirect_dma_start(out=X.ap, out_offset=bass.IndirectOffsetOnAxis(ap=idx.ap[0:nsl, :], axis=0),
                                                                  in_=ys.ap[0:nsl, :], in_offset=None, compute_op=ALU.add))

                units = [(e_, stm) for e_ in range(16) for stm in reversed(streams)]
                NU = len(units)
                weights(0)
                weights(1)
                nxt_w = 2
                selbuild(units[0])
                items = {0: idxpart(units[0])}
                xtrans(items[0])
                if NU > 1:
                    selbuild(units[1])
                for ui in range(NU):
                    ffn(items[ui])
                    if ui + 1 < NU:
                        items[ui + 1] = idxpart(units[ui + 1])
                    down(items[ui])
                    if ui + 1 < NU:
                        xtrans(items[ui + 1])
                    if ui + 2 < NU:
                        selbuild(units[ui + 2])
                    e_done = units[ui][0]
                    if (ui + 1 == NU or units[ui + 1][0] != e_done) and nxt_w < 16:
                        weights(nxt_w)
                        nxt_w += 1
                    del items[ui]
            k.barrier()
            if stop_here("exp%d" % l):
                break
        else:
            with ExitStack() as s:
                gfin = k.sb([128, D], F32, es=s)
                k.dma(gfin, g_bc_d[4])
                xt_r = Rot([k.sb([128, D], F32, es=s) for _ in range(3)])
                junk = k.sb([128, D], F32, es=s)
                ss_pool = Rot([k.sb([128, 1], F32, es=s) for _ in range(8)])
                for n in range(2, NTILE):
                    c0 = n * 128
                    xt = xt_r.get()
                    k.dma(xt, rows_key(X, n)[c0:c0 + 128, :])
                    ss = ss_pool.get()
                    k.act(junk, xt, AF.Square, accum=ss)
                    rstd = ss_pool.get()
                    k.rsq(rstd, ss, 1.0 / D, EPS)
                    k.stt(xt, xt, rstd, gfin, ALU.mult, ALU.mult)
                    k.dma(out_d.sl(n, np.s_[c0 - NTC:c0 - NTC + 128, :]), xt)
        k.barrier()
        print("ninst", k.ninst, k.cnt, flush=True)
    return nc


_NC_CACHE = {}


def kernel(**inputs):
    inp = {kk: np.asarray(v) for kk, v in inputs.items()}
    shared = _prep_shared(inp)
    if "nc" not in _NC_CACHE:
        _NC_CACHE["nc"] = build()
    nc = _NC_CACHE["nc"]
    in_maps = []
    for b in range(8):
        m = dict(shared)
        m.update(_prep_core(inp, b))
        in_maps.append(m)
    res = run_bass_kernel_spmd(nc, in_maps, core_ids=list(range(8)))
    out = np.stack([np.asarray(r["out"], dtype=np.float32) for r in res.results], 0)
    return out
```

```python
import numpy as np
from contextlib import ExitStack
import concourse.bass as bass
import concourse.mybir as mybir
from concourse.bass_utils import run_bass_kernel_spmd

F32 = mybir.dt.float32
BF16 = mybir.dt.bfloat16
I32 = mybir.dt.int32
U32 = mybir.dt.uint32
AF = mybir.ActivationFunctionType
ALU = mybir.AluOpType
AX = mybir.AxisListType


class T:
    __slots__ = ("ap", "key")

    def __init__(self, ap, key):
        self.ap = ap
        self.key = key

    def __getitem__(self, idx):
        return T(self.ap[idx], self.key)

    def sub(self, suffix):
        return T(self.ap, (self.key, suffix))

    def sl(self, suffix, idx):
        return T(self.ap[idx], (self.key, suffix))


def _key(x):
    return x.key if isinstance(x, T) else x


class KB:
    CE = ("pe", "act", "dve", "pool")

    def __init__(self, nc, es, nds=14):
        self.nc = nc
        self.es = es
        self.E = {"pe": nc.tensor, "act": nc.scalar, "dve": nc.vector,
                  "pool": nc.gpsimd, "sp": nc.sync}
        self.csem = {e: es.enter_context(nc.semaphore("c_" + e)) for e in self.CE}
        self.cnt = {e: 0 for e in self.CE}
        self.NDS = nds
        self.dsem = [es.enter_context(nc.semaphore("d%d" % i)) for i in range(nds)]
        self.dcnt = [0] * nds
        self.dnext = 0
        self.waited = {e: {} for e in self.E}
        self.lastw = {}
        self.readers = {}
        self.nalloc = 0
        self.ninst = 0

    def sb(self, shape, dtype, name=None, es=None):
        self.nalloc += 1
        name = name or ("t%d" % self.nalloc)
        t = (es or self.es).enter_context(self.nc.sbuf_tensor(name + "_%d" % self.nalloc, list(shape), dtype))
        return T(t[:], name + "_%d" % self.nalloc)

    def ps(self, shape, dtype, name=None, es=None):
        self.nalloc += 1
        name = name or ("p%d" % self.nalloc)
        t = (es or self.es).enter_context(self.nc.psum_tensor(name + "_%d" % self.nalloc, list(shape), dtype))
        return T(t[:], name + "_%d" % self.nalloc)

    def dram(self, name, shape, dtype, kind="Internal"):
        t = self.nc.dram_tensor(name, list(shape), dtype, kind=kind)
        return T(t.ap(), name)

    def _semobj(self, semkey):
        return self.csem[semkey[1]] if semkey[0] == "c" else self.dsem[semkey[1]]

    def _wait(self, e, semkey, val):
        if self.waited[e].get(semkey, 0) >= val:
            return
        self.waited[e][semkey] = val
        self.E[e].wait_ge(self._semobj(semkey), val)

    def _deps(self, e, reads, writes, is_dma):
        for r in reads:
            lw = self.lastw.get(_key(r))
            if lw is not None:
                self._wait(e, lw[0], lw[1])
        for w in writes:
            k = _key(w)
            lw = self.lastw.get(k)
            if lw is not None:
                if not (lw[0] == ("c", e) and e == "pe" and not is_dma):
                    self._wait(e, lw[0], lw[1])
            for sk, v in self.readers.get(k, {}).items():
                if sk == ("c", e) and e == "pe" and not is_dma:
                    continue
                self._wait(e, sk, v)

    def _record(self, tok, reads, writes):
        for r in reads:
            d = self.readers.setdefault(_key(r), {})
            if d.get(tok[0], 0) < tok[1]:
                d[tok[0]] = tok[1]
        for w in writes:
            k = _key(w)
            self.lastw[k] = tok
            self.readers[k] = {}

    def op(self, e, fn, reads=(), writes=()):
        self._deps(e, reads, writes, False)
        ins = fn(self.E[e])
        self.cnt[e] += 1
        ins.then_inc(self.csem[e], 1)
        self._record((("c", e), self.cnt[e]), reads, writes)
        self.ninst += 1
        return ins

    def dma(self, out, in_, q="sp", fn=None, reads=None, writes=None, **kw):
        reads = [in_] if reads is None else reads
        writes = [out] if writes is None else writes
        slot = self.dnext
        self.dnext = (slot + 1) % self.NDS
        if self.dcnt[slot] > 0:
            self._wait(q, ("d", slot), 16 * self.dcnt[slot])
        self._deps(q, reads, writes, True)
        if fn is None:
            ins = self.E[q].dma_start(out=out.ap, in_=in_.ap, **kw)
        else:
            ins = fn(self.E[q])
        self.dcnt[slot] += 1
        ins.then_inc(self.dsem[slot], 16)
        self._record((("d", slot), 16 * self.dcnt[slot]), reads, writes)
        self.ninst += 1
        return ins

    def barrier(self):
        for e in self.E:
            for e2 in self.CE:
                if e2 != e and self.cnt[e2] > 0:
                    self._wait(e, ("c", e2), self.cnt[e2])
            for s in range(self.NDS):
                if self.dcnt[s] > 0:
                    self._wait(e, ("d", s), 16 * self.dcnt[s])

    def mm(self, out, lhsT, rhs, start=True, stop=True, extra_reads=()):
        return self.op("pe", lambda e: e.matmul(out.ap, lhsT.ap, rhs.ap, start=start, stop=stop),
                       reads=[lhsT, rhs, *extra_reads], writes=[out])

    def tr(self, out, in_, ident):
        return self.op("pe", lambda e: e.transpose(out.ap, in_.ap, ident.ap),
                       reads=[in_, ident], writes=[out])

    def act(self, out, in_, func, bias=None, scale=None, accum=None, e="act"):
        kw = {}
        rd = [in_]
        wr = [out]
        if bias is not None:
            if isinstance(bias, T):
                kw["bias"] = bias.ap
                rd.append(bias)
            else:
                kw["bias"] = bias
        if scale is not None:
            if isinstance(scale, T):
                kw["scale"] = scale.ap
                rd.append(scale)
            else:
                kw["scale"] = scale
        if accum is not None:
            kw["accum_out"] = accum.ap
            wr.append(accum)
        return self.op(e, lambda en: en.activation(out.ap, in_.ap, func, **kw), reads=rd, writes=wr)

    def tt(self, out, a, b, op, e="dve"):
        return self.op(e, lambda en: en.tensor_tensor(out.ap, a.ap, b.ap, op), reads=[a, b], writes=[out])

    def ts(self, out, a, s1, op0, s2=None, op1=None, e="dve", accum=None):
        rd = [a]
        wr = [out]
        v1 = s1
        v2 = s2
        if isinstance(s1, T):
            rd.append(s1)
            v1 = s1.ap
        if isinstance(s2, T):
            rd.append(s2)
            v2 = s2.ap
        kw = {}
        if op1 is not None:
            kw["op1"] = op1
        if accum is not None:
            kw["accum_out"] = accum.ap
            wr.append(accum)
        return self.op(e, lambda en: en.tensor_scalar(out.ap, a.ap, v1, v2, op0, **kw), reads=rd, writes=wr)

    def stt(self, out, a, s, b, op0, op1, e="dve"):
        rd = [a, b]
        v = s
        if isinstance(s, T):
            rd.append(s)
            v = s.ap
        return self.op(e, lambda en: en.scalar_tensor_tensor(out.ap, a.ap, v, b.ap, op0, op1), reads=rd, writes=[out])

    def copy(self, out, in_, e="dve"):
        if e == "act":
            return self.op(e, lambda en: en.activation(out.ap, in_.ap, AF.Copy), reads=[in_], writes=[out])
        return self.op(e, lambda en: en.tensor_copy(out.ap, in_.ap), reads=[in_], writes=[out])

    def memset(self, out, v, e="dve"):
        return self.op(e, lambda en: en.memset(out.ap, v), reads=[], writes=[out])

    def rsq(self, dst, src, mul, add):
        self.ts(dst, src, mul, ALU.mult, add, ALU.add)
        self.act(dst, dst, AF.Sqrt)
        self.op("dve", lambda e: e.reciprocal(dst.ap, dst.ap), reads=[dst], writes=[dst])

L_ = 2
D = 1024
NTX = 4096
NTC = 256
NT = NTX + NTC
NTILE = NT // 128
NCOL = 2464
SCALE_MLA = float((64 + 32) ** -0.5)
LN2 = float(np.log(2.0))


def _rope_tables():
    t = np.arange(NTX)
    row = (t // 64).astype(np.float32)
    col = (t % 64).astype(np.float32)

    def tab(dh_half):
        dh = dh_half
        inv = 10000.0 ** (-np.arange(0, dh, 2, dtype=np.float32) / dh)
        return inv

    def build(dtot):
        half = dtot // 2
        inv = tab(half)
        nf = half // 2
        cos = np.zeros((dtot, NTX), np.float32)
        sins = np.zeros((dtot, NTX), np.float32)
        perm = np.zeros((dtot, dtot), np.float32)
        for part, pos in ((0, row), (1, col)):
            base = part * half
            ang = pos[None, :] * inv[:, None]
            c, s = np.cos(ang), np.sin(ang)
            for j in range(nf):
                cos[base + j] = c[j]
                cos[base + nf + j] = c[j]
                sins[base + j] = -s[j]
                sins[base + nf + j] = s[j]
                perm[base + nf + j, base + j] = 1.0
                perm[base + j, base + nf + j] = 1.0
        return cos, sins, perm

    c32, s32, p32 = build(32)
    c64, s64, p64 = build(64)
    c128 = np.concatenate([c64, c64], 0)
    s128 = np.concatenate([s64, s64], 0)
    p128 = np.zeros((128, 128), np.float32)
    p128[:64, :64] = p64
    p128[64:, 64:] = p64
    rope32 = np.stack([np.tile(c32, (4, 1)), np.tile(s32, (4, 1))]).astype(np.float32)
    rope128 = np.stack([c128, s128]).astype(np.float32)
    return rope32, rope128, p32, p128


_CST = {}


def _cst_layout():
    off = 0
    for name, w in (("ident", 128), ("A", 128), ("B", 128), ("C1", 128), ("C2", 128),
                    ("colK1", 1), ("colK2", 1), ("iota", 512), ("triU", 128),
                    ("mprev", 128), ("mnext", 128), ("p128", 128), ("p32", 32), ("pswap", 128), ("bmat", 128), ("p32x4", 128),
                    ("tokid", NTILE * 16 * 2)):
        _CST[name] = (off, w)
        off += w
    return off


NCST = _cst_layout()


def _const_table():
    rope32, rope128, p32, p128 = _rope_tables()
    cst = np.zeros((128, NCST), np.float32)
    p = np.arange(128, dtype=np.float32)[:, None]
    c = np.arange(128, dtype=np.float32)[None, :]

    def put(name, arr):
        o, w = _CST[name]
        cst[:arr.shape[0], o:o + w] = arr
    put("ident", np.eye(128, dtype=np.float32))
    put("A", np.maximum(c - p, 0.0))
    put("B", np.maximum(p - c, 0.0))
    put("C1", np.broadcast_to(c + 1.0, (128, 128)))
    put("C2", np.broadcast_to(128.0 - c, (128, 128)))
    put("colK1", 127.0 - p)
    put("colK2", p)
    put("iota", np.broadcast_to(np.arange(512, dtype=np.float32)[None, :], (128, 512)))
    put("triU", (p <= c).astype(np.float32))
    put("mprev", (p >= c).astype(np.float32))
    put("mnext", (p <= c).astype(np.float32))
    put("p128", p128)
    put("p32", p32)
    p32x4 = np.zeros((128, 128), np.float32)
    for i_ in range(4):
        p32x4[i_ * 32:(i_ + 1) * 32, i_ * 32:(i_ + 1) * 32] = p32
    put("p32x4", p32x4)
    put("pswap", (np.arange(128)[:, None] == (np.arange(128)[None, :] + 64) % 128).astype(np.float32))
    put("bmat", (np.arange(128)[:, None] // 8 == np.arange(128)[None, :] // 8).astype(np.float32))
    rows = (np.arange(NTILE)[None, :] * 128 + np.arange(128)[:, None])
    tok = np.stack([rows // 64, rows % 64], -1).astype(np.float32)
    tok = np.broadcast_to(tok[:, :, None, :], (128, NTILE, 16, 2)).reshape(128, -1)
    put("tokid", tok)
    return cst, rope32, rope128


def _prep_shared(inp):
    f = lambda a: np.ascontiguousarray(a, dtype=np.float32)
    sh = {}
    w_in = inp["w_in"]
    s = np.cumsum([0, 256, 256, 256, 256, 256, 128, 32, 256, 128, 128])
    rq, rk, rv, rg, cq, ckv, kr, wq, wk, wv = [w_in[:, :, s[i]:s[i + 1]] for i in range(10)]
    wk2 = np.concatenate([wk[:, :, 0:64], wk[:, :, 0:64], wk[:, :, 64:128], wk[:, :, 64:128]], -1)
    wv2 = np.concatenate([wv[:, :, 0:64], wv[:, :, 0:64], wv[:, :, 64:128], wv[:, :, 64:128]], -1)
    sh["w_in_r"] = f(np.concatenate([rq, rk, cq, ckv, wq, wk2, kr, rk, rv, rg, wv2], -1))
    assert sh["w_in_r"].shape[-1] == NCOL
    uq = inp["mla_w_uq"]
    sh["w_uq_n"] = f(uq[:, :, :, :64].reshape(L_, 256, 512))
    sh["w_uq_r"] = f(uq[:, :, :, 64:].reshape(L_, 256, 256))
    sh["w_uk"] = f(inp["mla_w_uk"].reshape(L_, 128, 512))
    sh["w_uv"] = f(inp["mla_w_uv"].reshape(L_, 128, 512))
    sh["w_out"] = f(inp["w_out"])
    sh["router_w"] = f(inp["router_w"])
    sh["ada_w"] = f(inp["ada_w"])
    sh["ada_b"] = f(inp["ada_b"].reshape(L_, 1, 6 * D))
    sh["exp_wg"] = f(inp["exp_w_gate"])
    sh["exp_wu"] = f(inp["exp_w_up"])
    sh["exp_wd"] = f(inp["exp_w_down"])
    gb = np.stack([inp["norm1_g"][0], inp["norm2_g"][0], inp["norm1_g"][1], inp["norm2_g"][1], inp["final_g"]])
    sh["g_bc"] = f(np.broadcast_to(gb[:, None, :], (5, 128, D)))
    cols = []
    for l in range(L_):
        qg = inp["mla_qnorm_g"][l].reshape(2, 128).T
        kg = inp["mla_kvnorm_g"][l].reshape(1, 128).T
        df, db, sk = inp["ret_decay_f"][l], inp["ret_decay_b"][l], inp["win_sink"][l]
        rep = np.broadcast_to(np.concatenate([df, db, sk])[None, :], (128, 12))
        hp = (np.arange(128) >= 64).astype(np.int64)
        pp = np.stack([df[0 + hp], df[2 + hp], db[0 + hp], db[2 + hp]], -1)
        cols += [qg, kg, rep, pp]
    sh["small"] = f(np.concatenate(cols, -1))
    cst, rope32, rope128 = _const_table()
    sh["cst"] = cst
    sh["rope32"] = rope32
    sh["rope128"] = rope128
    return sh


def _prep_core(inp, b):
    f = lambda a: np.ascontiguousarray(a, dtype=np.float32)
    d = {}
    d["x0"] = f(np.concatenate([inp["ctx"][b], inp["x"][b]], 0))
    cv = np.stack([inp["c_ctx"], inp["c"][b]])
    cr = cv.reshape(2, 8, 128).transpose(0, 2, 1)
    d["crep"] = f(np.broadcast_to(cr[:, :, :, None], (2, 128, 8, 128)))
    return d

class Rot:
    def __init__(self, tiles):
        self.t = tiles
        self.i = 0

    def get(self):
        t = self.t[self.i % len(self.t)]
        self.i += 1
        return t


def TD(t, pattern, **kw):
    return T(t.ap.rearrange(pattern, **kw), t.key)


def build(upto=None, dbg=False, nlayers=L_):
    nc = bass.Bass("TRN2", target_bir_lowering=False)
    es0 = ExitStack()
    with es0:
        k = KB(nc, es0)
        kind_dbg = "ExternalOutput" if dbg else "Internal"
        din = lambda n, s, dt=F32: k.dram(n, s, dt, kind="ExternalInput")
        x0 = din("x0", [NT, D])
        crep_d = din("crep", [2, 128, 8, 128])
        w_in_d = din("w_in_r", [L_, D, NCOL])
        w_uq_n_d = din("w_uq_n", [L_, 256, 512])
        w_uq_r_d = din("w_uq_r", [L_, 256, 256])
        w_uk_d = din("w_uk", [L_, 128, 512])
        w_uv_d = din("w_uv", [L_, 128, 512])
        w_out_d = din("w_out", [L_, D, D])
        router_d = din("router_w", [L_, D, 16])
        ada_w_d = din("ada_w", [L_, D, 6 * D])
        ada_b_d = din("ada_b", [L_, 1, 6 * D])
        wg_d = din("exp_wg", [L_, 16, D, 768])
        wu_d = din("exp_wu", [L_, 16, D, 768])
        wd_d = din("exp_wd", [L_, 16, 768, D])
        g_bc_d = din("g_bc", [5, 128, D])
        small_d = din("small", [128, L_ * 19])
        cst_d = din("cst", [128, NCST])
        rope32_d = din("rope32", [2, 128, NTX])
        rope128_d = din("rope128", [2, 128, NTX])
        out_d = k.dram("out", [NTX, D], F32, kind="ExternalOutput")
        X = k.dram("X", [NT, D], F32, kind=kind_dbg)
        MODBC = k.dram("MODBC", [2, 6, 128, D], F32, kind=kind_dbg)
        QT = k.dram("QT", [2, 128, NT], BF16, kind=kind_dbg)
        KT = k.dram("KT", [2, 128, NT], BF16, kind=kind_dbg)
        TMo = k.dram("TMo", [NT, 1024], BF16, kind=kind_dbg)
        QN = k.dram("QN", [4, 128, NT], BF16, kind=kind_dbg)
        KN = k.dram("KN", [4, 128, NT], BF16, kind=kind_dbg)
        QR = k.dram("QR", [8, 32, NT], BF16, kind=kind_dbg)
        KVT = k.dram("KVT", [128, NT], BF16, kind=kind_dbg)
        KRT = k.dram("KRT", [32, NT], BF16, kind=kind_dbg)
        VP = k.dram("VP", [NT, 512], BF16, kind=kind_dbg)
        WQT = k.dram("WQT", [2, 128, NT], BF16, kind=kind_dbg)
        WKT = k.dram("WKT", [2, 128, NT], BF16, kind=kind_dbg)
        YT = k.dram("YT", [8, 128, NT], BF16, kind=kind_dbg)
        H2 = k.dram("H2", [NT, D], BF16, kind=kind_dbg)
        AFFD = k.dram("AFFD", [16, NTX], F32)
        THRD = k.dram("THRD", [128, 1], F32)

        cst = k.sb([128, NCST], F32, "cst")
        k.dma(cst, cst_d)
        small = k.sb([128, L_ * 19], F32, "small")
        k.dma(small, small_d)

        def C(name, rows=128):
            o, w = _CST[name]
            return cst[0:rows, o:o + w]
        ident = C("ident")
        ones_f = k.sb([128, 128], F32, "ones_f")
        k.memset(ones_f, 1.0)
        ones_b = k.sb([128, 128], BF16, "ones_b")
        k.memset(ones_b, 1.0)
        ident_b = k.sb([128, 128], BF16, "ident_b")
        k.copy(ident_b, ident)
        triU_b = k.sb([128, 128], BF16, "triU_b")
        k.copy(triU_b, C("triU"))
        mprev_b = k.sb([128, 128], BF16, "mprev_b")
        k.copy(mprev_b, C("mprev"))
        mnext_b = k.sb([128, 128], BF16, "mnext_b")
        k.copy(mnext_b, C("mnext"))
        Rtab = k.sb([128, NTILE, 16, 4], BF16, "Rtab")
        o_tok, w_tok = _CST["tokid"]
        k.copy(Rtab[:, :, :, 0:2], T(cst.ap[:, o_tok:o_tok + w_tok].rearrange("p (n e c) -> p n e c", n=NTILE, e=16), cst.key))
        affT = k.sb([128, NTILE, 16], F32, "affT")
        posm = k.sb([128, NTILE, 16], F32, "posm")

        EPS = 1e-6
        blocks = [(0, 256, 0)] + [(256 + 512 * i, 512, 1) for i in range(8)]

        def rows_key(t, n):
            return t.sub(("r", n))

        def stop_here(name):
            return upto is not None and upto == name

        def rstd_from_ss(ss, n_feat, es, eps=EPS):
            r = k.sb([128, 1], F32, es=es)
            k.rsq(r, ss, 1.0 / n_feat, eps)
            return r

        for l in range(nlayers):
            need_ctx = l < L_ - 1
            sm0 = l * 19
            Xsrc = x0 if l == 0 else X
            with ExitStack() as s:
                crep = k.sb([128, 2, 8, 128], F32, es=s)
                for st in range(2):
                    k.dma(crep[:, st], crep_d[st])
                sil = k.sb([128, 2, 8, 128], F32, es=s)
                k.act(sil, crep, AF.Silu)
                wb = Rot([k.sb([128, 8, 512], F32, es=s) for _ in range(2)])
                br = Rot([k.sb([1, 512], F32, es=s) for _ in range(2)])
                pp = Rot([k.ps([128, 512], F32, es=s) for _ in range(4)])
                ob = Rot([k.sb([128, 512], F32, es=s) for _ in range(4)])
                aw = TD(ada_w_d[l], "(kc p) n -> p kc n", p=128)
                for nb in range(12):
                    w = wb.get()
                    k.dma(w, aw[:, :, nb * 512:(nb + 1) * 512])
                    b_ = br.get()
                    k.dma(b_, ada_b_d[l, :, nb * 512:(nb + 1) * 512])
                    for st in range(2):
                        ps = pp.get()
                        for kc in range(8):
                            k.mm(ps, sil[:, st, kc, :], w[:, kc, :], start=(kc == 0), stop=False)
                        k.mm(ps, ones_f[0:1, :], b_, start=False, stop=True)
                        o = ob.get()
                        k.copy(o, ps, e=("act" if st == 0 else "dve"))
                        j, half = nb // 2, nb % 2
                        k.dma(MODBC.sl((st, j), np.s_[st, j, :, half * 512:(half + 1) * 512]), o)
            k.barrier()
            if stop_here("mod%d" % l):
                break

            def load_mod(st, j, es, eng_q="sp"):
                t = k.sb([128, D], F32, es=es)
                k.dma(t, MODBC.sl((st, j), np.s_[st, j]))
                return t

            def load_gs(st, j_scale, gidx, es):
                sc = load_mod(st, j_scale, es)
                gb = k.sb([128, D], F32, es=es)
                k.dma(gb, g_bc_d[gidx])
                k.stt(sc, sc, 1.0, gb, ALU.add, ALU.mult)
                return sc

            def load_cast(dst, src, stg, e="pool"):
                t = stg.get()
                sh = list(src.ap.shape)
                tv = t[0:sh[0], 0:sh[1]]
                k.dma(tv, src)
                k.copy(dst, tv, e=e)

            with ExitStack() as s:
                w_in = k.sb([128, 8, NCOL], BF16, es=s)
                stg = Rot([k.sb([128, NCOL], F32, es=s) for _ in range(2)])
                for kc in range(8):
                    load_cast(w_in[:, kc, :], w_in_d[l, kc * 128:(kc + 1) * 128, :], stg, e=("pool" if kc % 2 else "dve"))
                w_uq_n = k.sb([128, 2, 512], BF16, es=s)
                w_uq_r = k.sb([128, 2, 256], BF16, es=s)
                for kc in range(2):
                    load_cast(w_uq_n[:, kc, :], w_uq_n_d[l, kc * 128:(kc + 1) * 128, :], stg)
                    load_cast(w_uq_r[:, kc, :], w_uq_r_d[l, kc * 128:(kc + 1) * 128, :], stg)
                w_uk = k.sb([128, 512], BF16, es=s)
                load_cast(w_uk, w_uk_d[l], stg)
                w_uv = k.sb([128, 512], BF16, es=s)
                load_cast(w_uv, w_uv_d[l], stg)
                gs1 = [load_gs(st, 1, l * 2 + 0, s) for st in range(2)]
                sh1 = [load_mod(st, 0, s) for st in range(2)]
                qg = small[:, sm0 + 0:sm0 + 2]
                kg = small[:, sm0 + 2:sm0 + 3]
                xt_r = Rot([k.sb([128, D], F32, es=s) for _ in range(3)])
                xs_r = Rot([k.sb([128, D], F32, es=s) for _ in range(3)])
                junk = k.sb([128, D], F32, es=s)
                ss_pool = Rot([k.sb([128, 1], F32, es=s) for _ in range(8)])
                hT_r = Rot([k.sb([128, 8, 512], BF16, es=s) for _ in range(2)])
                ptr = Rot([k.ps([128, 8, 128], BF16, es=s) for _ in range(2)])
                xb_r = Rot([k.sb([128, D], BF16, es=s) for _ in range(2)])
                pfm = Rot([k.ps([128, 512], F32, es=s) for _ in range(3)])
                pex = Rot([k.ps([128, 512], F32, es=s) for _ in range(3)])
                ob16 = Rot([k.sb([128, 512], BF16, es=s) for _ in range(6)])
                f32t = Rot([k.sb([128, 512], F32, es=s) for _ in range(6)])
                rp32 = Rot([k.sb([128, 2, 512], F32, es=s) for _ in range(2)])
                rp128 = Rot([k.sb([128, 2, 512], F32, es=s) for _ in range(2)])
                cqg = k.sb([128, 2, 512], BF16, es=s)
                rstdq = k.sb([128, 512], F32, es=s)
                kvT_sb = k.sb([128, 512], BF16, es=s)
                tmo = Rot([k.sb([128, 1024], BF16, es=s) for _ in range(2)])

                def bc_rstd(dst, sq_list, nfeat, n):
                    ps = pex.get()
                    for i, sq in enumerate(sq_list):
                        k.mm(ps[:, :n], ones_f, sq, start=(i == 0), stop=(i == len(sq_list) - 1))
                    k.rsq(dst[:, :n], ps[:, :n], 1.0 / nfeat, EPS)

                def rope_apply(src_f32, M, n, tabs, perm, out_bf):
                    ps = pex.get()
                    k.mm(ps[0:M, :n], perm, src_f32[0:M, :n])
                    t1 = f32t.get()
                    k.tt(t1[0:M, :n], src_f32[0:M, :n], tabs[0:M, 0, :n], ALU.mult)
                    t2 = f32t.get()
                    k.tt(t2[0:M, :n], ps[0:M, :n], tabs[0:M, 1, :n], ALU.mult)
                    k.tt(out_bf[0:M, :n], t1[0:M, :n], t2[0:M, :n], ALU.add, e="pool")

                def norm_phase(blk):
                    (r0, n, st) = blk
                    nt = n // 128
                    hT = hT_r.get()
                    r32 = r128 = None
                    if st == 1:
                        t0 = r0 - NTC
                        r32 = rp32.get()
                        k.dma(r32[:, :, :n], TD(rope32_d, "a p t -> p a t")[:, :, t0:t0 + n])
                        r128 = rp128.get()
                        k.dma(r128[:, :, :n], TD(rope128_d, "a p t -> p a t")[:, :, t0:t0 + n])
                    for ti in range(nt):
                        rr = r0 + ti * 128
                        xt = xt_r.get()
                        k.dma(xt, rows_key(Xsrc, rr // 128)[rr:rr + 128, :])
                        ss = ss_pool.get()
                        k.act(junk, xt, AF.Square, accum=ss)
                        rstd = ss_pool.get()
                        k.rsq(rstd, ss, 1.0 / D, EPS)
                        xs = xs_r.get()
                        k.stt(xs, xt, rstd, gs1[st], ALU.mult, ALU.mult)
                        xb = xb_r.get()
                        k.tt(xb, xs, sh1[st], ALU.add)
                        pt = ptr.get()
                        for kc in range(8):
                            k.tr(pt[:, kc, :], xb[:, kc * 128:(kc + 1) * 128], ident_b)
                        k.copy(hT[:, :, ti * 128:(ti + 1) * 128], pt, e="act")
                    return (hT, r32, r128)

                def proj_phase(blk, ctx_):
                    (r0, n, st) = blk
                    nt = n // 128
                    hT, r32, r128 = ctx_
                    def fm(c, M=128):
                        ps = pfm.get()
                        for kc in range(8):
                            k.mm(ps[0:M, :n], w_in[:, kc, c * 128:c * 128 + M], hT[:, kc, :n], start=(kc == 0), stop=(kc == 7))
                        return ps
                    for c in range(4):
                        ps = fm(c)
                        o = ob16.get()
                        k.copy(o[:, :n], ps[:, :n], e=("act" if c % 2 == 0 else "dve"))
                        dst = (QT if c < 2 else KT)
                        k.dma(dst.sl((c % 2, r0), np.s_[c % 2, :, r0:r0 + n]), o[:, :n])
                    sqs = []
                    for c2 in range(2):
                        ps = fm(4 + c2)
                        sq = f32t.get()
                        k.act(sq[:, :n], ps[:, :n], AF.Square)
                        sqs.append(sq[:, :n])
                        k.act(cqg[:, c2, :n], ps[:, :n], AF.Copy, scale=qg[:, c2:c2 + 1])
                    bc_rstd(rstdq, sqs, 256, n)
                    for pr in range(4):
                        ps = pex.get()
                        for c2 in range(2):
                            k.mm(ps[:, :n], w_uq_n[:, c2, pr * 128:(pr + 1) * 128], cqg[:, c2, :n], start=(c2 == 0), stop=(c2 == 1))
                        qn = ob16.get()
                        k.tt(qn[:, :n], ps[:, :n], rstdq[:, :n], ALU.mult)
                        k.dma(QN.sl((pr, r0), np.s_[pr, :, r0:r0 + n]), qn[:, :n])
                    for g4 in range(2):
                        ps = pex.get()
                        for c2 in range(2):
                            k.mm(ps[:, :n], w_uq_r[:, c2, g4 * 128:(g4 + 1) * 128], cqg[:, c2, :n], start=(c2 == 0), stop=(c2 == 1))
                        qr = f32t.get()
                        k.tt(qr[:, :n], ps[:, :n], rstdq[:, :n], ALU.mult)
                        o = ob16.get()
                        if st == 1:
                            rope_apply(qr, 128, n, r32, C("p32x4"), o)
                        else:
                            k.copy(o[:, :n], qr[:, :n], e="pool")
                        for hh in range(4):
                            h = g4 * 4 + hh
                            k.dma(QR.sl((h, r0), np.s_[h, :, r0:r0 + n]), o[hh * 32:(hh + 1) * 32, :n])
                    ps = fm(6)
                    sq = f32t.get()
                    k.act(sq[:, :n], ps[:, :n], AF.Square)
                    kvg = f32t.get()
                    k.act(kvg[:, :n], ps[:, :n], AF.Copy, scale=kg[:, 0:1])
                    rk_ = f32t.get()
                    bc_rstd(rk_, [sq[:, :n]], 128, n)
                    k.tt(kvT_sb[:, :n], kvg[:, :n], rk_[:, :n], ALU.mult)
                    k.dma(KVT.sl(r0, np.s_[:, r0:r0 + n]), kvT_sb[:, :n])
                    for pr in range(4):
                        ps = pex.get()
                        k.mm(ps[:, :n], w_uk[:, pr * 128:(pr + 1) * 128], kvT_sb[:, :n])
                        o = ob16.get()
                        k.copy(o[:, :n], ps[:, :n], e=("act" if pr % 2 == 0 else "dve"))
                        k.dma(KN.sl((pr, r0), np.s_[pr, :, r0:r0 + n]), o[:, :n])
                    for ti in range(nt):
                        ps = pex.get()
                        k.mm(ps, kvT_sb[:, ti * 128:(ti + 1) * 128], w_uv)
                        o = ob16.get()
                        k.copy(o, ps, e="act")
                        rr = r0 + ti * 128
                        k.dma(VP.sl(rr // 128, np.s_[rr:rr + 128, :]), o)
                    for c in range(4):
                        ps = fm(7 + c)
                        o = ob16.get()
                        if st == 1:
                            sf = f32t.get()
                            k.copy(sf[:, :n], ps[:, :n], e="act")
                            rope_apply(sf, 128, n, r128, C("p128"), o)
                        else:
                            k.copy(o[:, :n], ps[:, :n], e="act")
                        dst = (WQT if c < 2 else WKT)
                        k.dma(dst.sl((c % 2, r0), np.s_[c % 2, :, r0:r0 + n]), o[:, :n])
                    ps = fm(11, 32)
                    o = ob16.get()
                    if st == 1:
                        sf = f32t.get()
                        k.copy(sf[0:32, :n], ps[0:32, :n], e="act")
                        rope_apply(sf, 32, n, r32, C("p32", 32), o)
                    else:
                        k.copy(o[0:32, :n], ps[0:32, :n], e="act")
                    k.dma(KRT.sl(r0, np.s_[:, r0:r0 + n]), o[0:32, :n])
                    for ti in range(nt):
                        o = tmo.get()
                        for hf in range(2):
                            ps = pfm.get()
                            c0 = 1440 + hf * 512
                            for kc in range(8):
                                k.mm(ps, hT[:, kc, ti * 128:(ti + 1) * 128], w_in[:, kc, c0:c0 + 512], start=(kc == 0), stop=(kc == 7))
                            k.copy(o[:, hf * 512:(hf + 1) * 512], ps, e=("act" if hf == 0 else "dve"))
                        rr = r0 + ti * 128
                        k.dma(TMo.sl(rr // 128, np.s_[rr:rr + 128, :]), o)
                pctx = norm_phase(blocks[0])
                for bi, blk in enumerate(blocks):
                    nctx = norm_phase(blocks[bi + 1]) if bi + 1 < len(blocks) else None
                    proj_phase(blk, pctx)
                    pctx = nctx
            k.barrier()
            if stop_here("s1_%d" % l):
                break
            with ExitStack() as s:
                lg_rep = k.sb([128, 8], F32, es=s)
                lg_pp = k.sb([128, 4], F32, es=s)
                for dst, src in ((lg_rep, small[:, sm0 + 3:sm0 + 11]), (lg_pp, small[:, sm0 + 15:sm0 + 19])):
                    k.act(dst, src, AF.Exp, scale=LN2)
                    k.ts(dst, dst, -1.0, ALU.mult, 1.0, ALU.add)
                    k.act(dst, dst, AF.Ln)
                LN8 = float(np.log(0.125))
                DT = k.sb([128, 4, 128], F32, es=s)
                tmpA = k.sb([128, 128], F32, es=s)
                for h in range(4):
                    k.ts(tmpA, C("A"), lg_rep[:, h:h + 1], ALU.mult)
                    k.stt(tmpA, C("B"), lg_rep[:, 4 + h:5 + h], tmpA, ALU.mult, ALU.add)
                    k.act(DT[:, h, :], tmpA, AF.Exp)
                    k.ts(DT[:, h, :], DT[:, h, :], 0.125, ALU.mult)
                QWF = k.sb([128, 2, 128], F32, es=s)
                QWB = k.sb([128, 2, 128], F32, es=s)
                gch = k.sb([128, 4], F32, es=s)
                for pr in range(2):
                    k.act(QWF[:, pr, :], C("C1"), AF.Exp, scale=lg_pp[:, pr:pr + 1])
                    k.act(QWB[:, pr, :], C("C2"), AF.Exp, scale=lg_pp[:, 2 + pr:3 + pr])
                k.act(gch, lg_pp, AF.Exp, scale=128.0)
                KWF = k.sb([128, 256], F32, es=s)
                KWB = k.sb([128, 256], F32, es=s)
                kcol = k.sb([128, 8], F32, es=s)
                for h in range(4):
                    k.act(kcol[:, h:h + 1], C("colK1"), AF.Exp, scale=lg_rep[:, h:h + 1])
                    k.act(kcol[:, 4 + h:5 + h], C("colK2"), AF.Exp, scale=lg_rep[:, 4 + h:5 + h])
                k.ts(kcol, kcol, 0.125, ALU.mult)
                for h in range(4):
                    k.copy(KWF[:, h * 64:(h + 1) * 64], T(kcol.ap[:, h:h + 1].to_broadcast([128, 64]), kcol.key))
                    k.copy(KWB[:, h * 64:(h + 1) * 64], T(kcol.ap[:, 4 + h:5 + h].to_broadcast([128, 64]), kcol.key))
                KVd = k.sb([128, NTILE, 4, 64], F32, es=s)
                Sfb = k.sb([128, NTILE, 2, 64], BF16, es=s)
                Sbb = k.sb([128, NTILE, 2, 64], BF16, es=s)
                kv_r = Rot([k.sb([128, 512], BF16, es=s) for _ in range(3)])
                kw_r = Rot([k.sb([128, 2, 256], BF16, es=s) for _ in range(2)])
                pkv = Rot([k.ps([128, 4, 128], F32, es=s) for _ in range(2)])
                fwd_order = list(range(NTILE))
                bwd_order = [1, 0] + list(range(NTILE - 1, 1, -1))
                r1_order = []
                for a_, b_ in zip(fwd_order, bwd_order):
                    for t_ in (a_, b_):
                        if t_ not in r1_order:
                            r1_order.append(t_)
                curs = []
                for d_ in range(2):
                    c_ = k.sb([128, 2, 64], F32, es=s)
                    k.memset(c_, 0.0)
                    curs.append(c_)
                ptrs = [0, 0]
                done = set()

                def scan_step(d_, n):
                    Sb_ = Sfb if d_ == 0 else Sbb
                    cur = curs[d_]
                    k.copy(Sb_.sl(n, np.s_[:, n, :, :]), cur)
                    for pr in range(2):
                        k.stt(cur[:, pr, :], cur[:, pr, :], gch[:, d_ * 2 + pr:d_ * 2 + pr + 1],
                              KVd.sl(n, np.s_[:, n, d_ * 2 + pr, :]), ALU.mult, ALU.add)
                for n in r1_order:
                    kvt = kv_r.get()
                    k.dma(kvt, TMo.sl(n, np.s_[n * 128:(n + 1) * 128, 0:512]))
                    kw = kw_r.get()
                    k.tt(kw[:, 0, :], kvt[:, 0:256], KWF, ALU.mult)
                    k.tt(kw[:, 1, :], kvt[:, 0:256], KWB, ALU.mult)
                    ps = pkv.get()
                    for d_ in range(2):
                        for pr in range(2):
                            k.mm(ps[:, d_ * 2 + pr, :], kw[:, d_, pr * 128:(pr + 1) * 128], kvt[:, 256 + pr * 128:256 + (pr + 1) * 128])
                    k.copy(KVd.sl(n, np.s_[0:64, n, :, :]), ps[0:64, :, 0:64], e="act")
                    k.copy(KVd.sl(n, np.s_[64:128, n, :, :]), ps[64:128, :, 64:128], e="act")
                    done.add(n)
                    for d_, order in ((0, fwd_order), (1, bwd_order)):
                        while ptrs[d_] < NTILE and order[ptrs[d_]] in done:
                            scan_step(d_, order[ptrs[d_]])
                            ptrs[d_] += 1
                qk_r = Rot([k.sb([128, 2, 2, 128], BF16, es=s) for _ in range(3)])
                vg_r = Rot([k.sb([128, 512], BF16, es=s) for _ in range(3)])
                qw_r = Rot([k.sb([128, 2, 2, 128], BF16, es=s) for _ in range(2)])
                sm_r = Rot([k.sb([128, 128], BF16, es=s) for _ in range(4)])
                pss = Rot([k.ps([128, 512], F32, es=s) for _ in range(2)])
                psy = Rot([k.ps([128, 512], F32, es=s) for _ in range(2)])
                ysb_r = Rot([k.sb([128, 256], F32, es=s) for _ in range(3)])
                sq_r = Rot([k.sb([128, 256], F32, es=s) for _ in range(2)])
                st_r = Rot([k.sb([128, 16], F32, es=s) for _ in range(2)])
                sg_r = Rot([k.sb([128, 256], F32, es=s) for _ in range(3)])
                yo_r = Rot([k.sb([128, 2, 128], BF16, es=s) for _ in range(2)])
                yb_r = Rot([k.sb([128, 256], BF16, es=s) for _ in range(2)])
                ptb2 = Rot([k.ps([128, 2, 128], BF16, es=s) for _ in range(2)])

                def r2A(n):
                    c0 = n * 128
                    qk = qk_r.get()
                    k.dma(qk[:, 0], TD(QT, "c p t -> p c t")[:, :, c0:c0 + 128])
                    k.dma(qk[:, 1], TD(KT, "c p t -> p c t")[:, :, c0:c0 + 128])
                    vg = vg_r.get()
                    k.dma(vg, TMo.sl(n, np.s_[c0:c0 + 128, 256:768]))
                    qw = qw_r.get()
                    k.tt(qw[:, 0], qk[:, 0], QWF, ALU.mult)
                    k.tt(qw[:, 1], qk[:, 0], QWB, ALU.mult)
                    py = psy.get()
                    for h in range(4):
                        pr, off = h // 2, (h % 2) * 64
                        ps = pss.get()
                        k.mm(ps[:, 0:128], qk[off:off + 64, 1, pr, :], qk[off:off + 64, 0, pr, :])
                        sm = sm_r.get()
                        k.tt(sm, ps[:, 0:128], DT[:, h, :], ALU.mult)
                        yo_ = py[:, h * 64:(h + 1) * 64]
                        k.mm(yo_, sm, vg[:, h * 64:(h + 1) * 64], start=True, stop=False)
                        k.mm(yo_, qw[off:off + 64, 0, pr, :], Sfb.sl(n, np.s_[off:off + 64, n, pr, :]), start=False, stop=False)
                        k.mm(yo_, qw[off:off + 64, 1, pr, :], Sbb.sl(n, np.s_[off:off + 64, n, pr, :]), start=False, stop=True)
                    ysb = ysb_r.get()
                    k.copy(ysb, py[:, 0:256], e="act")
                    sg = sg_r.get()
                    k.act(sg, vg[:, 256:512], AF.Silu)
                    return (ysb, sg)

                def r2B(n, ctx_):
                    ysb, sg = ctx_
                    c0 = n * 128
                    stt_ = st_r.get()
                    k.op("dve", lambda e: e.reduce_sum(stt_.ap[:, 0:4], ysb.ap.rearrange("p (h d) -> p h d", h=4), AX.X), reads=[ysb], writes=[stt_])
                    sq = sq_r.get()
                    k.tt(sq, ysb, ysb, ALU.mult)
                    k.op("dve", lambda e: e.reduce_sum(stt_.ap[:, 4:8], sq.ap.rearrange("p (h d) -> p h d", h=4), AX.X), reads=[sq], writes=[stt_])
                    k.ts(stt_[:, 0:8], stt_[:, 0:8], 1.0 / 64, ALU.mult)
                    k.tt(stt_[:, 8:12], stt_[:, 0:4], stt_[:, 0:4], ALU.mult)
                    k.tt(stt_[:, 12:16], stt_[:, 4:8], stt_[:, 8:12], ALU.subtract)
                    k.rsq(stt_[:, 12:16], stt_[:, 12:16], 1.0, 1e-5)
                    for h in range(4):
                        k.ts(ysb[:, h * 64:(h + 1) * 64], ysb[:, h * 64:(h + 1) * 64], stt_[:, h:h + 1], ALU.subtract,
                             stt_[:, 12 + h:13 + h], ALU.mult)
                    yb = yb_r.get()
                    k.tt(yb, ysb, sg, ALU.mult)
                    pt = ptb2.get()
                    for c in range(2):
                        k.tr(pt[:, c, :], yb[:, c * 128:(c + 1) * 128], ident_b)
                    yo = yo_r.get()
                    k.copy(yo, pt, e="act")
                    k.dma(TD(YT, "c p t -> p c t").sl(("ret", n), np.s_[:, 0:2, c0:c0 + 128]), yo)
                tiles_r = list(range(0 if need_ctx else 2, NTILE))
                pc_ = r2A(tiles_r[0])
                for ti_, n in enumerate(tiles_r):
                    nc_ = r2A(tiles_r[ti_ + 1]) if ti_ + 1 < len(tiles_r) else None
                    r2B(n, pc_)
                    pc_ = nc_
            k.barrier()
            if stop_here("ret%d" % l):
                break
            with ExitStack() as s:
                vp2 = k.sb([128, NTILE, 8, 128], BF16, es=s)
                VPv = TD(VP, "(t p) (h d) -> p t h d", p=128, d=64)
                for par in range(2):
                    k.memset(vp2[:, :, par::2, (1 - par) * 64:(2 - par) * 64], 1.0, e=("dve" if par == 0 else "pool"))
                    for h in range(par, 8, 2):
                        k.dma(vp2[:, :, h, par * 64:(par + 1) * 64], VPv[:, :, h, :])
                kh_l = [k.sb([128, NT], BF16, es=s) for _ in range(2)]
                for t_ in kh_l:
                    k.memset(t_, 0.0, e="pool")
                kh_r = Rot(kh_l)
                q_l = [k.sb([128, 512], BF16, es=s) for _ in range(3)]
                for t_ in q_l:
                    k.memset(t_, 0.0)
                q_r = Rot(q_l)
                rd_sets = []
                for par in range(2):
                    tl = [k.sb([128, 512], F32, es=s) for _ in range(2)]
                    for t_ in tl:
                        k.memset(t_, 0.0)
                    rd_sets.append(Rot(tl))
                pT_r = Rot([k.sb([128, 2, 512], BF16, es=s) for _ in range(4)])
                pss = Rot([k.ps([128, 2, 512], F32, es=s) for _ in range(2)])
                pacc = Rot([k.ps([128, 512], F32, es=s) for _ in range(4)])
                sw_r = Rot([k.sb([128, 512], F32, es=s) for _ in range(2)])
                yp_r = Rot([k.sb([128, 512], BF16, es=s) for _ in range(3)])
                qblocks = ([(0, 256, [0, 1])] if need_ctx else []) + [(256 + 512 * i, 512, list(range(NTILE))) for i in range(8)]
                LOOK = 1
                pend = []

                def emit_pv2(item):
                    h, r0, n, j, npair, kt2, pT, pv = item
                    for u_ in range(2):
                        first = (j == 0 and u_ == 0)
                        last = (j == npair - 1 and u_ == 1)
                        k.mm(pv[:, :n], vp2[:, kt2[u_], h, :], pT[:, u_, :n], start=first, stop=last)
                    if j == npair - 1:
                        off = (h % 2) * 64
                        dof = 64 - off
                        rd = rd_sets[h % 2].get()
                        k.op("dve", lambda e: e.reciprocal(rd.ap[dof:dof + 64, :n], pv.ap[dof:dof + 64, :n]), reads=[pv], writes=[rd])
                        sws = sw_r.get()
                        k.dma(sws[off:off + 64, :n], rd[dof:dof + 64, :n])
                        yp = yp_r.get()
                        k.tt(yp[off:off + 64, :n], pv[off:off + 64, :n], sws[off:off + 64, :n], ALU.mult)
                        k.dma(YT.sl(("mla", h, r0), np.s_[2 + h // 2, off:off + 64, r0:r0 + n]), yp[off:off + 64, :n])
                for h in range(8):
                    off = (h % 2) * 64
                    kh = kh_r.get()
                    k.dma(kh[0:64, :], KN[h // 2, off:off + 64, :])
                    k.dma(kh[64:96, :], KRT)
                    for (r0, n, kts) in qblocks:
                        q = q_r.get()
                        k.dma(q[0:64, :n], QN[h // 2, off:off + 64, r0:r0 + n])
                        k.dma(q[64:96, :n], QR[h, :, r0:r0 + n])
                        pv = pacc.get()
                        npair = len(kts) // 2
                        for j in range(npair):
                            kt2 = (kts[2 * j], kts[2 * j + 1])
                            ps = pss.get()
                            for u_ in range(2):
                                k.mm(ps[:, u_, :n], kh[:, kt2[u_] * 128:(kt2[u_] + 1) * 128], q[:, :n])
                            pT = pT_r.get()
                            k.act(pT[:, :, :n], ps[:, :, :n], AF.Exp, scale=SCALE_MLA)
                            pend.append((h, r0, n, j, npair, kt2, pT, pv))
                            if len(pend) > LOOK:
                                emit_pv2(pend.pop(0))
                while pend:
                    emit_pv2(pend.pop(0))
            k.barrier()
            if stop_here("mla%d" % l):
                break
            with ExitStack() as s:
                wkT = k.sb([128, 2, 2, NT], BF16, es=s)
                k.memset(wkT, 0.0, e="pool")
                for hk_ in range(2):
                    for g_ in range(2):
                        k.dma(wkT[g_ * 64:(g_ + 1) * 64, hk_, g_, :], WKT[hk_, g_ * 64:(g_ + 1) * 64, :])
                wqT = k.sb([128, 2, NT], BF16, es=s)
                k.dma(wqT, TD(WQT, "c p t -> p c t"))
                wv3 = k.sb([128, NTILE, 4, 128], BF16, es=s)
                WVv = T(TMo.ap[:, 768:1024].rearrange("(t p) (q d) -> p t q d", p=128, d=64), TMo.key)
                for par in range(2):
                    k.memset(wv3[:, :, par::2, (1 - par) * 64:(2 - par) * 64], 1.0, e=("dve" if par == 0 else "pool"))
                    for q_ in range(par, 4, 2):
                        k.dma(wv3[:, :, q_, par * 64:(par + 1) * 64], WVv[:, :, q_, :])
                esink = k.sb([128, 4], F32, es=s)
                k.act(esink, small[:, sm0 + 11:sm0 + 15], AF.Exp)
                rd_sets = []
                for par in range(2):
                    tl = [k.sb([128, 128], F32, es=s) for _ in range(2)]
                    for t_ in tl:
                        k.memset(t_, 0.0)
                    rd_sets.append(Rot(tl))
                pT_r = Rot([k.sb([128, 128], BF16, es=s) for _ in range(6)])
                pss = Rot([k.ps([128, 512], F32, es=s) for _ in range(4)])
                pacc = Rot([k.ps([128, 512], F32, es=s) for _ in range(3)])
                psw = Rot([k.ps([128, 512], F32, es=s) for _ in range(1)])
                sw_r = Rot([k.sb([128, 128], F32, es=s) for _ in range(2)])
                yp_r = Rot([k.sb([128, 128], BF16, es=s) for _ in range(3)])
                pend = []

                def emit_pvw(item):
                    n, qh, i, nk, kt, pT, pv, yp = item
                    hk, g = qh // 2, qh % 2
                    off = g * 64
                    dof = 64 - off
                    c0 = n * 128
                    last = (i == nk - 1)
                    k.mm(pv[:, 0:128], wv3[:, kt, qh, :], pT, start=(i == 0), stop=last)
                    if last:
                        rd = rd_sets[g].get()
                        k.ts(rd[dof:dof + 64, :], pv[dof:dof + 64, 0:128], esink[dof:dof + 64, qh:qh + 1], ALU.add)
                        k.op("dve", lambda e: e.reciprocal(rd.ap[dof:dof + 64, :], rd.ap[dof:dof + 64, :]), reads=[rd], writes=[rd])
                        sw = psw.get()
                        k.mm(sw[:, 0:128], C("pswap"), rd)
                        sws = sw_r.get()
                        k.copy(sws[off:off + 64, :], sw[off:off + 64, 0:128], e="act")
                        k.tt(yp[off:off + 64, :], pv[off:off + 64, 0:128], sws[off:off + 64, :], ALU.mult)
                        if g == 1:
                            k.dma(YT.sl(("win", hk, n), np.s_[6 + hk, :, c0:c0 + 128]), yp)
                yp = None
                for n in range(0 if need_ctx else 2, NTILE):
                    c0 = n * 128
                    if n < 2:
                        keys = [(0, None), (1, None)]
                    else:
                        keys = []
                        if n - 1 >= 2:
                            keys.append((n - 1, mprev_b))
                        keys.append((n, None))
                        if n + 1 < NTILE:
                            keys.append((n + 1, mnext_b))
                        keys += [(0, None), (1, None)]
                    for qh in range(4):
                        hk, g = qh // 2, qh % 2
                        off = g * 64
                        pv = pacc.get()
                        if g == 0:
                            yp = yp_r.get()
                        for i, (kt, msk) in enumerate(keys):
                            ps = pss.get()
                            k.mm(ps[:, 0:128], wkT[:, hk, g, kt * 128:(kt + 1) * 128], wqT[:, hk, c0:c0 + 128])
                            pT = pT_r.get()
                            k.act(pT, ps[:, 0:128], AF.Exp, scale=0.125)
                            if msk is not None:
                                k.tt(pT, pT, msk, ALU.mult)
                            pend.append((n, qh, i, len(keys), kt, pT, pv, yp))
                            if len(pend) > 3:
                                emit_pvw(pend.pop(0))
                while pend:
                    emit_pvw(pend.pop(0))
            k.barrier()
            if stop_here("win%d" % l):
                break
            es_aff = ExitStack()
            aff_e = k.sb([16, NT], F32, "aff_e", es=es_aff)
            with ExitStack() as s:
                w_out = k.sb([128, 8, D], BF16, es=s)
                stg = Rot([k.sb([128, D], F32, es=s) for _ in range(2)])
                for kc in range(8):
                    load_cast(w_out[:, kc, :], w_out_d[l, kc * 128:(kc + 1) * 128, :], stg, e=("pool" if kc % 2 else "dve"))
                rw = k.sb([128, 8, 16], F32, es=s)
                k.dma(rw, TD(router_d[l], "(kc p) e -> p kc e", p=128))
                sts = [0, 1] if need_ctx else [1]
                mod2 = {st: load_mod(st, 2, s) for st in sts}
                gs2 = {st: load_gs(st, 4, l * 2 + 1, s) for st in sts}
                sh2 = {st: load_mod(st, 3, s) for st in sts}
                yT_r = Rot([k.sb([128, 8, 128], BF16, es=s) for _ in range(2)])
                xt_r = Rot([k.sb([128, D], F32, es=s) for _ in range(2)])
                xn_r = Rot([k.sb([128, D], F32, es=s) for _ in range(2)])
                xs_r = Rot([k.sb([128, D], F32, es=s) for _ in range(3)])
                h2b_r = Rot([k.sb([128, D], BF16, es=s) for _ in range(2)])
                h2T_r = Rot([k.sb([128, 8, 128], F32, es=s) for _ in range(2)])
                junk = k.sb([128, D], F32, es=s)
                ss_pool = Rot([k.sb([128, 1], F32, es=s) for _ in range(8)])
                ex_r = Rot([k.sb([128, 16], F32, es=s) for _ in range(2)])
                pso = Rot([k.ps([128, 512], F32, es=s) for _ in range(3)])
                ptr = Rot([k.ps([128, 4, 128], F32, es=s) for _ in range(2)])
                psl = Rot([k.ps([128, 512], F32, es=s) for _ in range(2)])
                def phaseA(n):
                    st = 0 if n < 2 else 1
                    c0 = n * 128
                    yT = yT_r.get()
                    k.dma(yT, TD(YT, "c p t -> p c t")[:, :, c0:c0 + 128])
                    xt = xt_r.get()
                    k.dma(xt, rows_key(Xsrc, n)[c0:c0 + 128, :])
                    xn = xn_r.get()
                    for hf in range(2):
                        ps = pso.get()
                        for kc in range(8):
                            k.mm(ps, yT[:, kc, :], w_out[:, kc, hf * 512:(hf + 1) * 512], start=(kc == 0), stop=(kc == 7))
                        sl_ = np.s_[:, hf * 512:(hf + 1) * 512]
                        k.tt(xn[sl_], ps, mod2[st][sl_], ALU.mult)
                        k.tt(xn[sl_], xn[sl_], xt[sl_], ALU.add)
                    k.dma(rows_key(X, n)[c0:c0 + 128, :], xn)
                    ss = ss_pool.get()
                    k.act(junk, xn, AF.Square, accum=ss)
                    rstd = ss_pool.get()
                    k.rsq(rstd, ss, 1.0 / D, EPS)
                    xs = xs_r.get()
                    k.stt(xs, xn, rstd, gs2[st], ALU.mult, ALU.mult)
                    k.tt(xs, xs, sh2[st], ALU.add)
                    h2b = h2b_r.get()
                    k.copy(h2b, xs, e="act")
                    k.dma(H2.sl(n, np.s_[c0:c0 + 128, :]), h2b)
                    return xs

                def phaseB(n, xs):
                    c0 = n * 128
                    h2T = h2T_r.get()
                    for hf in range(2):
                        pt = ptr.get()
                        for q4 in range(4):
                            kc = hf * 4 + q4
                            k.tr(pt[:, q4, :], xs[:, kc * 128:(kc + 1) * 128], ident)
                        k.copy(h2T[:, hf * 4:(hf + 1) * 4, :], pt, e="act")
                    pl = psl.get()
                    for kc in range(8):
                        k.mm(pl[:, 0:16], h2T[:, kc, :], rw[:, kc, :], start=(kc == 0), stop=(kc == 7))
                    ex = ex_r.get()
                    sm_ = ss_pool.get()
                    k.act(ex, pl[:, 0:16], AF.Exp, accum=sm_)
                    k.op("dve", lambda e: e.reciprocal(sm_.ap, sm_.ap), reads=[sm_], writes=[sm_])
                    k.ts(affT.sl(n, np.s_[:, n, :]), ex, sm_, ALU.mult)
                    pl2 = psl.get()
                    k.tr(pl2[0:16, 0:128], affT.sl(n, np.s_[:, n, :]), ident)
                    k.copy(aff_e.sl(n, np.s_[:, c0:c0 + 128]), pl2[0:16, 0:128], e="act")
                tiles_o = list(range(0 if need_ctx else 2, NTILE))
                pxs = phaseA(tiles_o[0])
                for ti_, n in enumerate(tiles_o):
                    nxs = phaseA(tiles_o[ti_ + 1]) if ti_ + 1 < len(tiles_o) else None
                    phaseB(n, pxs)
                    pxs = nxs
            k.barrier()
            if stop_here("o%d" % l):
                es_aff.close()
                break
            streams = ([(0, 2, 32)] if need_ctx else []) + [(2, 32, 512)]
            with ExitStack() as s:
                work = k.sb([16, NTX], F32, es=s)
                m8 = k.sb([16, 8], F32, es=s)
                thr = k.sb([16, 1], F32, es=s)
                aff128 = k.sb([128, 512], F32, es=s)
                junk16 = k.sb([128, 512], BF16, es=s)
                lo = k.sb([128, 1], F32, es=s)
                hi = k.sb([128, 1], F32, es=s)
                mid = k.sb([128, 1], F32, es=s)
                cnt = k.sb([128, 1], F32, es=s)
                half = k.sb([128, 1], F32, es=s)
                k.memset(half, 0.5)
                mge = k.sb([128, 1], U32, es=s)
                mlt = k.sb([128, 1], U32, es=s)
                thr16 = k.sb([16, 8], F32, es=s)
                mask_e = k.sb([16, NTX], F32, es=s)
                maskTb = k.sb([128, 32, 16], BF16, es=s)
                carry = k.sb([128, 16], F32, es=s)
                pos_r = Rot([k.sb([128, 16], F32, es=s) for _ in range(2)])
                ptk = Rot([k.ps([128, 512], F32, es=s) for _ in range(4)])
                ahi = k.sb([128, NTILE, 16], BF16, es=s)
                alo = k.sb([128, NTILE, 16], F32, es=s)
                k.copy(ahi, affT)
                k.tt(alo, affT, ahi, ALU.subtract)
                k.copy(Rtab[:, :, :, 2], ahi, e="pool")
                k.copy(Rtab[:, :, :, 3], alo, e="pool")
                for (t0, ntl, cap) in streams:
                    ntok = ntl * 128
                    cs = np.s_[:, t0 * 128:t0 * 128 + ntok]
                    if cap <= 64:
                        k.copy(work[:, :ntok], aff_e[cs])
                        for r in range(cap // 8):
                            k.op("dve", lambda e: e.max(out=m8.ap, in_=work.ap[:, :ntok]), reads=[work], writes=[m8])
                            if r < cap // 8 - 1:
                                k.op("dve", lambda e: e.match_replace(out=work.ap[:, :ntok], in_to_replace=m8.ap,
                                                                      in_values=work.ap[:, :ntok], imm_value=-1.0),
                                     reads=[work, m8], writes=[work])
                        k.copy(thr, m8[:, 7:8])
                    else:
                        k.dma(AFFD, aff_e[cs])
                        k.dma(aff128, TD(AFFD, "e (s t) -> (e s) t", s=8))
                        k.memset(lo, 0.0)
                        k.memset(hi, 2.0)
                        for it in range(40):
                            k.stt(mid, lo, hi, half, ALU.add, ALU.mult)
                            k.ts(junk16, aff128, mid, ALU.is_ge, 0.0, ALU.add, accum=cnt)
                            pc = ptk.get()
                            k.mm(pc[:, 0:1], C("bmat"), cnt)
                            k.ts(mge, pc[:, 0:1], cap - 0.5, ALU.is_ge)
                            k.ts(mlt, pc[:, 0:1], cap - 0.5, ALU.is_lt)
                            k.op("dve", lambda e: e.copy_predicated(lo.ap, mge.ap, mid.ap), reads=[mge, mid], writes=[lo])
                            k.op("dve", lambda e: e.copy_predicated(hi.ap, mlt.ap, mid.ap), reads=[mlt, mid], writes=[hi])
                        k.dma(THRD, lo)
                        k.dma(thr16, TD(THRD, "(e s) o -> e (s o)", s=8))
                        k.copy(thr, thr16[:, 0:1])
                    k.ts(mask_e[:, :ntok], aff_e[cs], thr, ALU.is_ge)
                    k.memset(carry, 0.0)
                    for i in range(ntl):
                        n = t0 + i
                        pt = ptk.get()
                        k.tr(pt[:, 0:16], mask_e[:, i * 128:(i + 1) * 128], ident[0:16, 0:16])
                        k.copy(maskTb[:, i, :], pt[:, 0:16], e="act")
                        pc = ptk.get()
                        k.mm(pc[:, 0:16], triU_b, maskTb[:, i, :])
                        k.mm(pc[:, 16:32], ones_b, maskTb[:, i, :])
                        pos = pos_r.get()
                        k.tt(pos, pc[:, 0:16], carry, ALU.add)
                        k.tt(pos, pos, maskTb[:, i, :], ALU.mult)
                        k.ts(posm.sl(n, np.s_[:, n, :]), pos, -1.0, ALU.add)
                        k.tt(carry, carry, pc[:, 16:32], ALU.add)
            k.barrier()
            if stop_here("topk%d" % l):
                es_aff.close()
                break
            es_aff.close()
            with ExitStack() as s:
                mod5 = {st: load_mod(st, 5, s) for st in ([0, 1] if need_ctx else [1])}
                wsets = Rot([(k.sb([128, 8, 768], BF16, es=s), k.sb([128, 8, 768], BF16, es=s), k.sb([128, 6, D], BF16, es=s))
                             for _ in range(2)])
                stg = Rot([k.sb([128, 768], F32, es=s) for _ in range(4)])
                Sel = k.sb([128, 32, 512], BF16, es=s)
                iota16 = k.sb([128, 512], mybir.dt.int16, es=s)
                k.copy(iota16, C("iota"))
                r4T_r = Rot([k.sb([4, 512], F32, es=s) for _ in range(2)])
                r4_r = Rot([k.sb([128, 16], F32, es=s) for _ in range(3)])
                idx_r = Rot([k.sb([128, 1], I32, es=s) for _ in range(16)])
                idf_r = Rot([k.sb([128, 1], F32, es=s) for _ in range(4)])
                gt_r = Rot([k.sb([128, 1], F32, es=s) for _ in range(16)])
                xs_r = Rot([k.sb([128, D], BF16, es=s) for _ in range(10)])
                xsT_r = Rot([k.sb([128, 8, 512], BF16, es=s) for _ in range(2)])
                hid = k.sb([128, 6, 512], BF16, es=s)
                sg_r = Rot([k.sb([128, 512], F32, es=s) for _ in range(2)])
                ys_r = Rot([k.sb([128, D], F32, es=s) for _ in range(2)])
                p4 = Rot([k.ps([128, 512], F32, es=s) for _ in range(1)])
                ptb = Rot([k.ps([128, 8, 128], BF16, es=s) for _ in range(1)])
                pgu = Rot([k.ps([128, 512], F32, es=s) for _ in range(4)])
                pdn = Rot([k.ps([128, 512], F32, es=s) for _ in range(2)])
                Xall = [rows_key(X, n) for n in range(NTILE)] + [X.sub("all")]
                cast_eng = Rot(["act", "dve"])
                wcache = {}

                wgen = {"g": None}

                def weights_gen(e_, wg, wu, wd):
                    for kc in range(8):
                        load_cast(wg[:, kc, :], wg_d[l, e_, kc * 128:(kc + 1) * 128, :], stg, e=cast_eng.get())
                        yield
                        load_cast(wu[:, kc, :], wu_d[l, e_, kc * 128:(kc + 1) * 128, :], stg, e=cast_eng.get())
                        yield
                    for fc in range(6):
                        for hf_ in range(2):
                            load_cast(wd[:, fc, hf_ * 512:(hf_ + 1) * 512], wd_d[l, e_, fc * 128:(fc + 1) * 128, hf_ * 512:(hf_ + 1) * 512], stg, e=cast_eng.get())
                            yield

                def feed(n_):
                    for _ in range(n_):
                        if wgen["g"] is None:
                            return
                        try:
                            next(wgen["g"])
                        except StopIteration:
                            wgen["g"] = None

                def weights(e_, now=False):
                    feed(10000)
                    wg, wu, wd = wsets.get()
                    wcache[e_] = (wg, wu, wd)
                    wgen["g"] = weights_gen(e_, wg, wu, wd)
                    if now:
                        feed(10000)

                def selbuild(u):
                    e_, (t0, ntl, cap) = u
                    for i in range(ntl):
                        k.ts(Sel[:, i, :cap], iota16[:, :cap], posm[:, t0 + i, e_:e_ + 1], ALU.is_equal)

                def idxpart(u):
                    e_, (t0, ntl, cap) = u
                    ps = p4.get()
                    for i in range(ntl):
                        k.mm(ps[0:4, :cap], Rtab[:, t0 + i, e_, :], Sel[:, i, :cap], start=(i == 0), stop=(i == ntl - 1))
                    r4T = r4T_r.get()
                    k.copy(r4T[:, :cap], ps[0:4, :cap])
                    stiles = [(s0, min(128, cap - s0)) for s0 in range(0, cap, 128)]
                    for si, (s0, nsl) in enumerate(stiles):
                        k.tr(ps[0:nsl, si * 4:(si + 1) * 4], r4T[0:4, s0:s0 + nsl], ident[0:4, 0:4])
                    nsl0 = stiles[0][1]
                    r4 = r4_r.get()
                    k.copy(r4[0:nsl0, 0:4 * len(stiles)], ps[0:nsl0, 0:4 * len(stiles)])
                    meta = []
                    xss = []
                    for si, (s0, nsl) in enumerate(stiles):
                        c4 = si * 4
                        idf = idf_r.get()
                        k.stt(idf[0:nsl, :], r4[0:nsl, c4:c4 + 1], 64.0, r4[0:nsl, c4 + 1:c4 + 2], ALU.mult, ALU.add)
                        idx = idx_r.get()
                        k.copy(idx[0:nsl, :], idf[0:nsl, :])
                        gt = gt_r.get()
                        k.tt(gt[0:nsl, :], r4[0:nsl, c4 + 2:c4 + 3], r4[0:nsl, c4 + 3:c4 + 4], ALU.add)
                        meta.append((idx, gt))
                        xs = xs_r.get()
                        k.dma(xs[0:nsl, :], H2, q="pool", reads=[idx],
                              fn=lambda en: en.indirect_dma_start(out=xs.ap[0:nsl, :], out_offset=None, in_=H2.ap,
                                                                  in_offset=bass.IndirectOffsetOnAxis(ap=idx.ap[0:nsl, :], axis=0)))
                        xss.append(xs)
                    return [u, stiles, meta, xss, None]

                def xtrans(item):
                    u, stiles, meta, xss, _ = item
                    xsT = xsT_r.get()
                    for si, (s0, nsl) in enumerate(stiles):
                        xs = xss[si]
                        pt = ptb.get()
                        for kc in range(8):
                            k.tr(pt[:, kc, 0:nsl], xs[0:nsl, kc * 128:(kc + 1) * 128], ident_b[0:nsl, 0:nsl])
                        k.copy(xsT[:, :, s0:s0 + nsl], pt[:, :, 0:nsl], e="act")
                    item[4] = xsT

                def ffn(item):
                    (e_, (t0, ntl, cap)), stiles, meta, xss, xsT = item
                    wg, wu, wd = wcache[e_]
                    for fc in range(6):
                        pg = pgu.get()
                        pu = pgu.get()
                        for kc in range(8):
                            k.mm(pg[:, :cap], wg[:, kc, fc * 128:(fc + 1) * 128], xsT[:, kc, :cap], start=(kc == 0), stop=(kc == 7))
                        for kc in range(8):
                            k.mm(pu[:, :cap], wu[:, kc, fc * 128:(fc + 1) * 128], xsT[:, kc, :cap], start=(kc == 0), stop=(kc == 7))
                        sg = sg_r.get()
                        k.act(sg[:, :cap], pg[:, :cap], AF.Silu)
                        k.tt(hid[:, fc, :cap], sg[:, :cap], pu[:, :cap], ALU.mult)
                        feed(2)

                def down(item):
                    (e_, (t0, ntl, cap)), stiles, meta, xss, xsT = item
                    st = 0 if t0 == 0 else 1
                    wg, wu, wd = wcache[e_]
                    for si, (s0, nsl) in enumerate(stiles):
                        idx, gt = meta[si]
                        ys = ys_r.get()
                        for hf in range(2):
                            pd = pdn.get()
                            for fc in range(6):
                                k.mm(pd[0:nsl, :], hid[:, fc, s0:s0 + nsl], wd[:, fc, hf * 512:(hf + 1) * 512], start=(fc == 0), stop=(fc == 5))
                            k.stt(ys[0:nsl, hf * 512:(hf + 1) * 512], pd[0:nsl, :], gt[0:nsl, 0:1],
                                  mod5[st][0:nsl, hf * 512:(hf + 1) * 512], ALU.mult, ALU.mult)
                            feed(2)
                        k.dma(X, ys[0:nsl, :], q="pool", reads=[ys, idx], writes=Xall,
                              fn=lambda en: en.indirect_dma_start(out=X.ap, out_offset=bass.IndirectOffsetOnAxis(ap=idx.ap[0:nsl, :], axis=0),
                                                                  in_=ys.ap[0:nsl, :], in_offset=None, compute_op=ALU.add))

                units = [(e_, stm) for e_ in range(16) for stm in reversed(streams)]
                NU = len(units)
                weights(0, now=True)
                weights(1, now=True)
                nxt_w = 2
                selbuild(units[0])
                items = {0: idxpart(units[0])}
                if NU > 1:
                    selbuild(units[1])
                    items[1] = idxpart(units[1])
                xtrans(items[0])
                if NU > 2:
                    selbuild(units[2])
                for ui in range(NU):
                    ffn(items[ui])
                    if ui + 1 < NU:
                        xtrans(items[ui + 1])
                    if ui + 2 < NU:
                        items[ui + 2] = idxpart(units[ui + 2])
                    down(items[ui])
                    if ui + 3 < NU:
                        selbuild(units[ui + 3])
                    e_done = units[ui][0]
                    if (ui + 1 == NU or units[ui + 1][0] != e_done) and nxt_w < 16:
                        weights(nxt_w)
                        nxt_w += 1
                    del items[ui]
            k.barrier()
            if stop_here("exp%d" % l):
                break
        else:
            with ExitStack() as s:
                gfin = k.sb([128, D], F32, es=s)
                k.dma(gfin, g_bc_d[4])
                xt_r = Rot([k.sb([128, D], F32, es=s) for _ in range(3)])
                junk = k.sb([128, D], F32, es=s)
                ss_pool = Rot([k.sb([128, 1], F32, es=s) for _ in range(8)])
                for n in range(2, NTILE):
                    c0 = n * 128
                    xt = xt_r.get()
                    k.dma(xt, rows_key(X, n)[c0:c0 + 128, :])
                    ss = ss_pool.get()
                    k.act(junk, xt, AF.Square, accum=ss)
                    rstd = ss_pool.get()
                    k.rsq(rstd, ss, 1.0 / D, EPS)
                    k.stt(xt, xt, rstd, gfin, ALU.mult, ALU.mult)
                    k.dma(out_d.sl(n, np.s_[c0 - NTC:c0 - NTC + 128, :]), xt)
        k.barrier()
        print("ninst", k.ninst, k.cnt, flush=True)
    return nc


_NC_CACHE = {}


def kernel(**inputs):
    inp = {kk: np.asarray(v) for kk, v in inputs.items()}
    shared = _prep_shared(inp)
    if "nc" not in _NC_CACHE:
        _NC_CACHE["nc"] = build()
    nc = _NC_CACHE["nc"]
    in_maps = []
    for b in range(8):
        m = dict(shared)
        m.update(_prep_core(inp, b))
        in_maps.append(m)
    res = run_bass_kernel_spmd(nc, in_maps, core_ids=list(range(8)))
    out = np.stack([np.asarray(r["out"], dtype=np.float32) for r in res.results], 0)
    return out
```

```python
import numpy as np
from contextlib import ExitStack
import concourse.bass as bass
import concourse.mybir as mybir
from concourse.bass_utils import run_bass_kernel_spmd

F32 = mybir.dt.float32
BF16 = mybir.dt.bfloat16
I32 = mybir.dt.int32
U32 = mybir.dt.uint32
AF = mybir.ActivationFunctionType
ALU = mybir.AluOpType
AX = mybir.AxisListType


class T:
    __slots__ = ("ap", "key")

    def __init__(self, ap, key):
        self.ap = ap
        self.key = key

    def __getitem__(self, idx):
        return T(self.ap[idx], self.key)

    def sub(self, suffix):
        return T(self.ap, (self.key, suffix))

    def sl(self, suffix, idx):
        return T(self.ap[idx], (self.key, suffix))


def _key(x):
    return x.key if isinstance(x, T) else x


class KB:
    CE = ("pe", "act", "dve", "pool")

    def __init__(self, nc, es, nds=14):
        self.nc = nc
        self.es = es
        self.E = {"pe": nc.tensor, "act": nc.scalar, "dve": nc.vector,
                  "pool": nc.gpsimd, "sp": nc.sync}
        self.csem = {e: es.enter_context(nc.semaphore("c_" + e)) for e in self.CE}
        self.cnt = {e: 0 for e in self.CE}
        self.NDS = nds
        self.dsem = [es.enter_context(nc.semaphore("d%d" % i)) for i in range(nds)]
        self.dcnt = [0] * nds
        self.dnext = 0
        self.waited = {e: {} for e in self.E}
        self.lastw = {}
        self.readers = {}
        self.nalloc = 0
        self.ninst = 0

    def sb(self, shape, dtype, name=None, es=None):
        self.nalloc += 1
        name = name or ("t%d" % self.nalloc)
        t = (es or self.es).enter_context(self.nc.sbuf_tensor(name + "_%d" % self.nalloc, list(shape), dtype))
        return T(t[:], name + "_%d" % self.nalloc)

    def ps(self, shape, dtype, name=None, es=None):
        self.nalloc += 1
        name = name or ("p%d" % self.nalloc)
        t = (es or self.es).enter_context(self.nc.psum_tensor(name + "_%d" % self.nalloc, list(shape), dtype))
        return T(t[:], name + "_%d" % self.nalloc)

    def dram(self, name, shape, dtype, kind="Internal"):
        t = self.nc.dram_tensor(name, list(shape), dtype, kind=kind)
        return T(t.ap(), name)

    def _semobj(self, semkey):
        return self.csem[semkey[1]] if semkey[0] == "c" else self.dsem[semkey[1]]

    def _wait(self, e, semkey, val):
        if self.waited[e].get(semkey, 0) >= val:
            return
        self.waited[e][semkey] = val
        self.E[e].wait_ge(self._semobj(semkey), val)

    def _deps(self, e, reads, writes, is_dma):
        for r in reads:
            lw = self.lastw.get(_key(r))
            if lw is not None:
                self._wait(e, lw[0], lw[1])
        for w in writes:
            k = _key(w)
            lw = self.lastw.get(k)
            if lw is not None:
                if not (lw[0] == ("c", e) and e == "pe" and not is_dma):
                    self._wait(e, lw[0], lw[1])
            for sk, v in self.readers.get(k, {}).items():
                if sk == ("c", e) and e == "pe" and not is_dma:
                    continue
                self._wait(e, sk, v)

    def _record(self, tok, reads, writes):
        for r in reads:
            d = self.readers.setdefault(_key(r), {})
            if d.get(tok[0], 0) < tok[1]:
                d[tok[0]] = tok[1]
        for w in writes:
            k = _key(w)
            self.lastw[k] = tok
            self.readers[k] = {}

    def op(self, e, fn, reads=(), writes=()):
        self._deps(e, reads, writes, False)
        ins = fn(self.E[e])
        self.cnt[e] += 1
        ins.then_inc(self.csem[e], 1)
        self._record((("c", e), self.cnt[e]), reads, writes)
        self.ninst += 1
        return ins

    def dma(self, out, in_, q="sp", fn=None, reads=None, writes=None, **kw):
        reads = [in_] if reads is None else reads
        writes = [out] if writes is None else writes
        slot = self.dnext
        self.dnext = (slot + 1) % self.NDS
        if self.dcnt[slot] > 0:
            self._wait(q, ("d", slot), 16 * self.dcnt[slot])
        self._deps(q, reads, writes, True)
        if fn is None:
            ins = self.E[q].dma_start(out=out.ap, in_=in_.ap, **kw)
        else:
            ins = fn(self.E[q])
        self.dcnt[slot] += 1
        ins.then_inc(self.dsem[slot], 16)
        self._record((("d", slot), 16 * self.dcnt[slot]), reads, writes)
        self.ninst += 1
        return ins

    def barrier(self):
        for e in self.E:
            for e2 in self.CE:
                if e2 != e and self.cnt[e2] > 0:
                    self._wait(e, ("c", e2), self.cnt[e2])
            for s in range(self.NDS):
                if self.dcnt[s] > 0:
                    self._wait(e, ("d", s), 16 * self.dcnt[s])

    def mm(self, out, lhsT, rhs, start=True, stop=True, extra_reads=()):
        return self.op("pe", lambda e: e.matmul(out.ap, lhsT.ap, rhs.ap, start=start, stop=stop),
                       reads=[lhsT, rhs, *extra_reads], writes=[out])

    def tr(self, out, in_, ident):
        return self.op("pe", lambda e: e.transpose(out.ap, in_.ap, ident.ap),
                       reads=[in_, ident], writes=[out])

    def act(self, out, in_, func, bias=None, scale=None, accum=None, e="act"):
        kw = {}
        rd = [in_]
        wr = [out]
        if bias is not None:
            if isinstance(bias, T):
                kw["bias"] = bias.ap
                rd.append(bias)
            else:
                kw["bias"] = bias
        if scale is not None:
            if isinstance(scale, T):
                kw["scale"] = scale.ap
                rd.append(scale)
            else:
                kw["scale"] = scale
        if accum is not None:
            kw["accum_out"] = accum.ap
            wr.append(accum)
        return self.op(e, lambda en: en.activation(out.ap, in_.ap, func, **kw), reads=rd, writes=wr)

    def tt(self, out, a, b, op, e="dve"):
        return self.op(e, lambda en: en.tensor_tensor(out.ap, a.ap, b.ap, op), reads=[a, b], writes=[out])

    def ts(self, out, a, s1, op0, s2=None, op1=None, e="dve", accum=None):
        rd = [a]
        wr = [out]
        v1 = s1
        v2 = s2
        if isinstance(s1, T):
            rd.append(s1)
            v1 = s1.ap
        if isinstance(s2, T):
            rd.append(s2)
            v2 = s2.ap
        kw = {}
        if op1 is not None:
            kw["op1"] = op1
        if accum is not None:
            kw["accum_out"] = accum.ap
            wr.append(accum)
        return self.op(e, lambda en: en.tensor_scalar(out.ap, a.ap, v1, v2, op0, **kw), reads=rd, writes=wr)

    def stt(self, out, a, s, b, op0, op1, e="dve"):
        rd = [a, b]
        v = s
        if isinstance(s, T):
            rd.append(s)
            v = s.ap
        return self.op(e, lambda en: en.scalar_tensor_tensor(out.ap, a.ap, v, b.ap, op0, op1), reads=rd, writes=[out])

    def copy(self, out, in_, e="dve"):
        if e == "act":
            return self.op(e, lambda en: en.activation(out.ap, in_.ap, AF.Copy), reads=[in_], writes=[out])
        return self.op(e, lambda en: en.tensor_copy(out.ap, in_.ap), reads=[in_], writes=[out])

    def memset(self, out, v, e="dve"):
        return self.op(e, lambda en: en.memset(out.ap, v), reads=[], writes=[out])

    def rsq(self, dst, src, mul, add):
        self.ts(dst, src, mul, ALU.mult, add, ALU.add)
        self.act(dst, dst, AF.Sqrt)
        self.op("dve", lambda e: e.reciprocal(dst.ap, dst.ap), reads=[dst], writes=[dst])

L_ = 2
D = 1024
NTX = 4096
NTC = 256
NT = NTX + NTC
NTILE = NT // 128
NCOL = 2464
SCALE_MLA = float((64 + 32) ** -0.5)
LN2 = float(np.log(2.0))


def _rope_tables():
    t = np.arange(NTX)
    row = (t // 64).astype(np.float32)
    col = (t % 64).astype(np.float32)

    def tab(dh_half):
        dh = dh_half
        inv = 10000.0 ** (-np.arange(0, dh, 2, dtype=np.float32) / dh)
        return inv

    def build(dtot):
        half = dtot // 2
        inv = tab(half)
        nf = half // 2
        cos = np.zeros((dtot, NTX), np.float32)
        sins = np.zeros((dtot, NTX), np.float32)
        perm = np.zeros((dtot, dtot), np.float32)
        for part, pos in ((0, row), (1, col)):
            base = part * half
            ang = pos[None, :] * inv[:, None]
            c, s = np.cos(ang), np.sin(ang)
            for j in range(nf):
                cos[base + j] = c[j]
                cos[base + nf + j] = c[j]
                sins[base + j] = -s[j]
                sins[base + nf + j] = s[j]
                perm[base + nf + j, base + j] = 1.0
                perm[base + j, base + nf + j] = 1.0
        return cos, sins, perm

    c32, s32, p32 = build(32)
    c64, s64, p64 = build(64)
    c128 = np.concatenate([c64, c64], 0)
    s128 = np.concatenate([s64, s64], 0)
    p128 = np.zeros((128, 128), np.float32)
    p128[:64, :64] = p64
    p128[64:, 64:] = p64
    rope32 = np.stack([np.tile(c32, (4, 1)), np.tile(s32, (4, 1))]).astype(np.float32)
    rope128 = np.stack([c128, s128]).astype(np.float32)
    return rope32, rope128, p32, p128


_CST = {}


def _cst_layout():
    off = 0
    for name, w in (("ident", 128), ("A", 128), ("B", 128), ("C1", 128), ("C2", 128),
                    ("colK1", 1), ("colK2", 1), ("iota", 512), ("triU", 128),
                    ("mprev", 128), ("mnext", 128), ("p128", 128), ("p32", 32), ("pswap", 128), ("bmat", 128), ("p32x4", 128),
                    ("tokid", NTILE * 16 * 2)):
        _CST[name] = (off, w)
        off += w
    return off


NCST = _cst_layout()


def _const_table():
    rope32, rope128, p32, p128 = _rope_tables()
    cst = np.zeros((128, NCST), np.float32)
    p = np.arange(128, dtype=np.float32)[:, None]
    c = np.arange(128, dtype=np.float32)[None, :]

    def put(name, arr):
        o, w = _CST[name]
        cst[:arr.shape[0], o:o + w] = arr
    put("ident", np.eye(128, dtype=np.float32))
    put("A", np.maximum(c - p, 0.0))
    put("B", np.maximum(p - c, 0.0))
    put("C1", np.broadcast_to(c + 1.0, (128, 128)))
    put("C2", np.broadcast_to(128.0 - c, (128, 128)))
    put("colK1", 127.0 - p)
    put("colK2", p)
    put("iota", np.broadcast_to(np.arange(512, dtype=np.float32)[None, :], (128, 512)))
    put("triU", (p <= c).astype(np.float32))
    put("mprev", (p >= c).astype(np.float32))
    put("mnext", (p <= c).astype(np.float32))
    put("p128", p128)
    put("p32", p32)
    p32x4 = np.zeros((128, 128), np.float32)
    for i_ in range(4):
        p32x4[i_ * 32:(i_ + 1) * 32, i_ * 32:(i_ + 1) * 32] = p32
    put("p32x4", p32x4)
    put("pswap", (np.arange(128)[:, None] == (np.arange(128)[None, :] + 64) % 128).astype(np.float32))
    put("bmat", (np.arange(128)[:, None] // 8 == np.arange(128)[None, :] // 8).astype(np.float32))
    rows = (np.arange(NTILE)[None, :] * 128 + np.arange(128)[:, None])
    tok = np.stack([rows // 64, rows % 64], -1).astype(np.float32)
    tok = np.broadcast_to(tok[:, :, None, :], (128, NTILE, 16, 2)).reshape(128, -1)
    put("tokid", tok)
    return cst, rope32, rope128


def _prep_shared(inp):
    f = lambda a: np.ascontiguousarray(a, dtype=np.float32)
    sh = {}
    w_in = inp["w_in"]
    s = np.cumsum([0, 256, 256, 256, 256, 256, 128, 32, 256, 128, 128])
    rq, rk, rv, rg, cq, ckv, kr, wq, wk, wv = [w_in[:, :, s[i]:s[i + 1]] for i in range(10)]
    wk2 = np.concatenate([wk[:, :, 0:64], wk[:, :, 0:64], wk[:, :, 64:128], wk[:, :, 64:128]], -1)
    wv2 = np.concatenate([wv[:, :, 0:64], wv[:, :, 0:64], wv[:, :, 64:128], wv[:, :, 64:128]], -1)
    sh["w_in_r"] = f(np.concatenate([rq, rk, cq, ckv, wq, wk2, kr, rk, rv, rg, wv2], -1))
    assert sh["w_in_r"].shape[-1] == NCOL
    uq = inp["mla_w_uq"]
    sh["w_uq_n"] = f(uq[:, :, :, :64].reshape(L_, 256, 512))
    sh["w_uq_r"] = f(uq[:, :, :, 64:].reshape(L_, 256, 256))
    sh["w_uk"] = f(inp["mla_w_uk"].reshape(L_, 128, 512))
    sh["w_uv"] = f(inp["mla_w_uv"].reshape(L_, 128, 512))
    sh["w_out"] = f(inp["w_out"])
    sh["router_w"] = f(inp["router_w"])
    sh["ada_w"] = f(inp["ada_w"])
    sh["ada_b"] = f(inp["ada_b"].reshape(L_, 1, 6 * D))
    sh["exp_wg"] = f(inp["exp_w_gate"])
    sh["exp_wu"] = f(inp["exp_w_up"])
    sh["exp_wd"] = f(inp["exp_w_down"])
    gb = np.stack([inp["norm1_g"][0], inp["norm2_g"][0], inp["norm1_g"][1], inp["norm2_g"][1], inp["final_g"]])
    sh["g_bc"] = f(np.broadcast_to(gb[:, None, :], (5, 128, D)))
    cols = []
    for l in range(L_):
        qg = inp["mla_qnorm_g"][l].reshape(2, 128).T
        kg = inp["mla_kvnorm_g"][l].reshape(1, 128).T
        df, db, sk = inp["ret_decay_f"][l], inp["ret_decay_b"][l], inp["win_sink"][l]
        rep = np.broadcast_to(np.concatenate([df, db, sk])[None, :], (128, 12))
        hp = (np.arange(128) >= 64).astype(np.int64)
        pp = np.stack([df[0 + hp], df[2 + hp], db[0 + hp], db[2 + hp]], -1)
        cols += [qg, kg, rep, pp]
    sh["small"] = f(np.concatenate(cols, -1))
    cst, rope32, rope128 = _const_table()
    sh["cst"] = cst
    sh["rope32"] = rope32
    sh["rope128"] = rope128
    return sh


def _prep_core(inp, b):
    f = lambda a: np.ascontiguousarray(a, dtype=np.float32)
    d = {}
    d["x0"] = f(np.concatenate([inp["ctx"][b], inp["x"][b]], 0))
    cv = np.stack([inp["c_ctx"], inp["c"][b]])
    cr = cv.reshape(2, 8, 128).transpose(0, 2, 1)
    d["crep"] = f(np.broadcast_to(cr[:, :, :, None], (2, 128, 8, 128)))
    return d

class Rot:
    def __init__(self, tiles):
        self.t = tiles
        self.i = 0

    def get(self):
        t = self.t[self.i % len(self.t)]
        self.i += 1
        return t


def TD(t, pattern, **kw):
    return T(t.ap.rearrange(pattern, **kw), t.key)


def build(upto=None, dbg=False, nlayers=L_):
    nc = bass.Bass("TRN2", target_bir_lowering=False)
    es0 = ExitStack()
    with es0:
        k = KB(nc, es0)
        kind_dbg = "ExternalOutput" if dbg else "Internal"
        din = lambda n, s, dt=F32: k.dram(n, s, dt, kind="ExternalInput")
        x0 = din("x0", [NT, D])
        crep_d = din("crep", [2, 128, 8, 128])
        w_in_d = din("w_in_r", [L_, D, NCOL])
        w_uq_n_d = din("w_uq_n", [L_, 256, 512])
        w_uq_r_d = din("w_uq_r", [L_, 256, 256])
        w_uk_d = din("w_uk", [L_, 128, 512])
        w_uv_d = din("w_uv", [L_, 128, 512])
        w_out_d = din("w_out", [L_, D, D])
        router_d = din("router_w", [L_, D, 16])
        ada_w_d = din("ada_w", [L_, D, 6 * D])
        ada_b_d = din("ada_b", [L_, 1, 6 * D])
        wg_d = din("exp_wg", [L_, 16, D, 768])
        wu_d = din("exp_wu", [L_, 16, D, 768])
        wd_d = din("exp_wd", [L_, 16, 768, D])
        g_bc_d = din("g_bc", [5, 128, D])
        small_d = din("small", [128, L_ * 19])
        cst_d = din("cst", [128, NCST])
        rope32_d = din("rope32", [2, 128, NTX])
        rope128_d = din("rope128", [2, 128, NTX])
        out_d = k.dram("out", [NTX, D], F32, kind="ExternalOutput")
        X = k.dram("X", [NT, D], F32, kind=kind_dbg)
        MODBC = k.dram("MODBC", [2, 6, 128, D], F32, kind=kind_dbg)
        QT = k.dram("QT", [2, 128, NT], BF16, kind=kind_dbg)
        KT = k.dram("KT", [2, 128, NT], BF16, kind=kind_dbg)
        TMo = k.dram("TMo", [NT, 1024], BF16, kind=kind_dbg)
        QN = k.dram("QN", [4, 128, NT], BF16, kind=kind_dbg)
        KN = k.dram("KN", [4, 128, NT], BF16, kind=kind_dbg)
        QR = k.dram("QR", [8, 32, NT], BF16, kind=kind_dbg)
        KVT = k.dram("KVT", [128, NT], BF16, kind=kind_dbg)
        KRT = k.dram("KRT", [32, NT], BF16, kind=kind_dbg)
        VP = k.dram("VP", [NT, 512], BF16, kind=kind_dbg)
        WQT = k.dram("WQT", [2, 128, NT], BF16, kind=kind_dbg)
        WKT = k.dram("WKT", [2, 128, NT], BF16, kind=kind_dbg)
        YT = k.dram("YT", [8, 128, NT], BF16, kind=kind_dbg)
        H2 = k.dram("H2", [NT, D], BF16, kind=kind_dbg)
        AFFD = k.dram("AFFD", [16, NTX], F32)
        THRD = k.dram("THRD", [128, 1], F32)

        cst = k.sb([128, NCST], F32, "cst")
        k.dma(cst, cst_d)
        small = k.sb([128, L_ * 19], F32, "small")
        k.dma(small, small_d)

        def C(name, rows=128):
            o, w = _CST[name]
            return cst[0:rows, o:o + w]
        ident = C("ident")
        ones_f = k.sb([128, 128], F32, "ones_f")
        k.memset(ones_f, 1.0)
        ones_b = k.sb([128, 128], BF16, "ones_b")
        k.memset(ones_b, 1.0)
        ident_b = k.sb([128, 128], BF16, "ident_b")
        k.copy(ident_b, ident)
        triU_b = k.sb([128, 128], BF16, "triU_b")
        k.copy(triU_b, C("triU"))
        mprev_b = k.sb([128, 128], BF16, "mprev_b")
        k.copy(mprev_b, C("mprev"))
        mnext_b = k.sb([128, 128], BF16, "mnext_b")
        k.copy(mnext_b, C("mnext"))
        Rtab = k.sb([128, NTILE, 16, 4], BF16, "Rtab")
        o_tok, w_tok = _CST["tokid"]
        k.copy(Rtab[:, :, :, 0:2], T(cst.ap[:, o_tok:o_tok + w_tok].rearrange("p (n e c) -> p n e c", n=NTILE, e=16), cst.key))
        affT = k.sb([128, NTILE, 16], F32, "affT")
        posm = k.sb([128, NTILE, 16], F32, "posm")

        EPS = 1e-6
        blocks = [(0, 256, 0)] + [(256 + 512 * i, 512, 1) for i in range(8)]

        def rows_key(t, n):
            return t.sub(("r", n))

        def stop_here(name):
            return upto is not None and upto == name

        def rstd_from_ss(ss, n_feat, es, eps=EPS):
            r = k.sb([128, 1], F32, es=es)
            k.rsq(r, ss, 1.0 / n_feat, eps)
            return r

        for l in range(nlayers):
            need_ctx = l < L_ - 1
            sm0 = l * 19
            Xsrc = x0 if l == 0 else X
            with ExitStack() as s:
                crep = k.sb([128, 2, 8, 128], F32, es=s)
                for st in range(2):
                    k.dma(crep[:, st], crep_d[st])
                sil = k.sb([128, 2, 8, 128], F32, es=s)
                k.act(sil, crep, AF.Silu)
                wb = Rot([k.sb([128, 8, 512], F32, es=s) for _ in range(2)])
                br = Rot([k.sb([1, 512], F32, es=s) for _ in range(2)])
                pp = Rot([k.ps([128, 512], F32, es=s) for _ in range(4)])
                ob = Rot([k.sb([128, 512], F32, es=s) for _ in range(4)])
                aw = TD(ada_w_d[l], "(kc p) n -> p kc n", p=128)
                for nb in range(12):
                    w = wb.get()
                    k.dma(w, aw[:, :, nb * 512:(nb + 1) * 512])
                    b_ = br.get()
                    k.dma(b_, ada_b_d[l, :, nb * 512:(nb + 1) * 512])
                    for st in range(2):
                        ps = pp.get()
                        for kc in range(8):
                            k.mm(ps, sil[:, st, kc, :], w[:, kc, :], start=(kc == 0), stop=False)
                        k.mm(ps, ones_f[0:1, :], b_, start=False, stop=True)
                        o = ob.get()
                        k.copy(o, ps, e=("act" if st == 0 else "dve"))
                        j, half = nb // 2, nb % 2
                        k.dma(MODBC.sl((st, j), np.s_[st, j, :, half * 512:(half + 1) * 512]), o)
            k.barrier()
            if stop_here("mod%d" % l):
                break

            def load_mod(st, j, es, eng_q="sp"):
                t = k.sb([128, D], F32, es=es)
                k.dma(t, MODBC.sl((st, j), np.s_[st, j]))
                return t

            def load_gs(st, j_scale, gidx, es):
                sc = load_mod(st, j_scale, es)
                gb = k.sb([128, D], F32, es=es)
                k.dma(gb, g_bc_d[gidx])
                k.stt(sc, sc, 1.0, gb, ALU.add, ALU.mult)
                return sc

            def load_cast(dst, src, stg, e="pool"):
                t = stg.get()
                sh = list(src.ap.shape)
                tv = t[0:sh[0], 0:sh[1]]
                k.dma(tv, src)
                k.copy(dst, tv, e=e)

            with ExitStack() as s:
                w_in = k.sb([128, 8, NCOL], BF16, es=s)
                stg = Rot([k.sb([128, NCOL], F32, es=s) for _ in range(2)])
                for kc in range(8):
                    load_cast(w_in[:, kc, :], w_in_d[l, kc * 128:(kc + 1) * 128, :], stg, e=("pool" if kc % 2 else "dve"))
                w_uq_n = k.sb([128, 2, 512], BF16, es=s)
                w_uq_r = k.sb([128, 2, 256], BF16, es=s)
                for kc in range(2):
                    load_cast(w_uq_n[:, kc, :], w_uq_n_d[l, kc * 128:(kc + 1) * 128, :], stg)
                    load_cast(w_uq_r[:, kc, :], w_uq_r_d[l, kc * 128:(kc + 1) * 128, :], stg)
                w_uk = k.sb([128, 512], BF16, es=s)
                load_cast(w_uk, w_uk_d[l], stg)
                w_uv = k.sb([128, 512], BF16, es=s)
                load_cast(w_uv, w_uv_d[l], stg)
                gs1 = [load_gs(st, 1, l * 2 + 0, s) for st in range(2)]
                sh1 = [load_mod(st, 0, s) for st in range(2)]
                qg = small[:, sm0 + 0:sm0 + 2]
                kg = small[:, sm0 + 2:sm0 + 3]
                xt_r = Rot([k.sb([128, D], F32, es=s) for _ in range(3)])
                xs_r = Rot([k.sb([128, D], F32, es=s) for _ in range(3)])
                junk = k.sb([128, D], F32, es=s)
                ss_pool = Rot([k.sb([128, 1], F32, es=s) for _ in range(8)])
                hT_r = Rot([k.sb([128, 8, 512], BF16, es=s) for _ in range(2)])
                ptr = Rot([k.ps([128, 8, 128], BF16, es=s) for _ in range(2)])
                xb_r = Rot([k.sb([128, D], BF16, es=s) for _ in range(2)])
                pfm = Rot([k.ps([128, 512], F32, es=s) for _ in range(3)])
                pex = Rot([k.ps([128, 512], F32, es=s) for _ in range(3)])
                ob16 = Rot([k.sb([128, 512], BF16, es=s) for _ in range(6)])
                f32t = Rot([k.sb([128, 512], F32, es=s) for _ in range(6)])
                rp32 = Rot([k.sb([128, 2, 512], F32, es=s) for _ in range(2)])
                rp128 = Rot([k.sb([128, 2, 512], F32, es=s) for _ in range(2)])
                cqg = k.sb([128, 2, 512], BF16, es=s)
                rstdq = k.sb([128, 512], F32, es=s)
                kvT_sb = k.sb([128, 512], BF16, es=s)
                tmo = Rot([k.sb([128, 1024], BF16, es=s) for _ in range(2)])

                def bc_rstd(dst, sq_list, nfeat, n):
                    ps = pex.get()
                    for i, sq in enumerate(sq_list):
                        k.mm(ps[:, :n], ones_f, sq, start=(i == 0), stop=(i == len(sq_list) - 1))
                    k.rsq(dst[:, :n], ps[:, :n], 1.0 / nfeat, EPS)

                def rope_apply(src_f32, M, n, tabs, perm, out_bf):
                    ps = pex.get()
                    k.mm(ps[0:M, :n], perm, src_f32[0:M, :n])
                    t1 = f32t.get()
                    k.tt(t1[0:M, :n], src_f32[0:M, :n], tabs[0:M, 0, :n], ALU.mult)
                    t2 = f32t.get()
                    k.tt(t2[0:M, :n], ps[0:M, :n], tabs[0:M, 1, :n], ALU.mult)
                    k.tt(out_bf[0:M, :n], t1[0:M, :n], t2[0:M, :n], ALU.add, e="pool")

                def norm_phase(blk):
                    (r0, n, st) = blk
                    nt = n // 128
                    hT = hT_r.get()
                    r32 = r128 = None
                    if st == 1:
                        t0 = r0 - NTC
                        r32 = rp32.get()
                        k.dma(r32[:, :, :n], TD(rope32_d, "a p t -> p a t")[:, :, t0:t0 + n])
                        r128 = rp128.get()
                        k.dma(r128[:, :, :n], TD(rope128_d, "a p t -> p a t")[:, :, t0:t0 + n])
                    for ti in range(nt):
                        rr = r0 + ti * 128
                        xt = xt_r.get()
                        k.dma(xt, rows_key(Xsrc, rr // 128)[rr:rr + 128, :])
                        ss = ss_pool.get()
                        k.act(junk, xt, AF.Square, accum=ss)
                        rstd = ss_pool.get()
                        k.rsq(rstd, ss, 1.0 / D, EPS)
                        xs = xs_r.get()
                        k.stt(xs, xt, rstd, gs1[st], ALU.mult, ALU.mult)
                        xb = xb_r.get()
                        k.tt(xb, xs, sh1[st], ALU.add)
                        pt = ptr.get()
                        for kc in range(8):
                            k.tr(pt[:, kc, :], xb[:, kc * 128:(kc + 1) * 128], ident_b)
                        k.copy(hT[:, :, ti * 128:(ti + 1) * 128], pt, e="act")
                    return (hT, r32, r128)

                def proj_phase(blk, ctx_):
                    (r0, n, st) = blk
                    nt = n // 128
                    hT, r32, r128 = ctx_
                    def fm(c, M=128):
                        ps = pfm.get()
                        for kc in range(8):
                            k.mm(ps[0:M, :n], w_in[:, kc, c * 128:c * 128 + M], hT[:, kc, :n], start=(kc == 0), stop=(kc == 7))
                        return ps
                    for c in range(4):
                        ps = fm(c)
                        o = ob16.get()
                        k.copy(o[:, :n], ps[:, :n], e=("act" if c % 2 == 0 else "dve"))
                        dst = (QT if c < 2 else KT)
                        k.dma(dst.sl((c % 2, r0), np.s_[c % 2, :, r0:r0 + n]), o[:, :n])
                    sqs = []
                    for c2 in range(2):
                        ps = fm(4 + c2)
                        sq = f32t.get()
                        k.act(sq[:, :n], ps[:, :n], AF.Square)
                        sqs.append(sq[:, :n])
                        k.act(cqg[:, c2, :n], ps[:, :n], AF.Copy, scale=qg[:, c2:c2 + 1])
                    bc_rstd(rstdq, sqs, 256, n)
                    for pr in range(4):
                        ps = pex.get()
                        for c2 in range(2):
                            k.mm(ps[:, :n], w_uq_n[:, c2, pr * 128:(pr + 1) * 128], cqg[:, c2, :n], start=(c2 == 0), stop=(c2 == 1))
                        qn = ob16.get()
                        k.tt(qn[:, :n], ps[:, :n], rstdq[:, :n], ALU.mult)
                        k.dma(QN.sl((pr, r0), np.s_[pr, :, r0:r0 + n]), qn[:, :n])
                    for g4 in range(2):
                        ps = pex.get()
                        for c2 in range(2):
                            k.mm(ps[:, :n], w_uq_r[:, c2, g4 * 128:(g4 + 1) * 128], cqg[:, c2, :n], start=(c2 == 0), stop=(c2 == 1))
                        qr = f32t.get()
                        k.tt(qr[:, :n], ps[:, :n], rstdq[:, :n], ALU.mult)
                        o = ob16.get()
                        if st == 1:
                            rope_apply(qr, 128, n, r32, C("p32x4"), o)
                        else:
                            k.copy(o[:, :n], qr[:, :n], e="pool")
                        for hh in range(4):
                            h = g4 * 4 + hh
                            k.dma(QR.sl((h, r0), np.s_[h, :, r0:r0 + n]), o[hh * 32:(hh + 1) * 32, :n])
                    ps = fm(6)
                    sq = f32t.get()
                    k.act(sq[:, :n], ps[:, :n], AF.Square)
                    kvg = f32t.get()
                    k.act(kvg[:, :n], ps[:, :n], AF.Copy, scale=kg[:, 0:1])
                    rk_ = f32t.get()
                    bc_rstd(rk_, [sq[:, :n]], 128, n)
                    k.tt(kvT_sb[:, :n], kvg[:, :n], rk_[:, :n], ALU.mult)
                    k.dma(KVT.sl(r0, np.s_[:, r0:r0 + n]), kvT_sb[:, :n])
                    for pr in range(4):
                        ps = pex.get()
                        k.mm(ps[:, :n], w_uk[:, pr * 128:(pr + 1) * 128], kvT_sb[:, :n])
                        o = ob16.get()
                        k.copy(o[:, :n], ps[:, :n], e=("act" if pr % 2 == 0 else "dve"))
                        k.dma(KN.sl((pr, r0), np.s_[pr, :, r0:r0 + n]), o[:, :n])
                    for ti in range(nt):
                        ps = pex.get()
                        k.mm(ps, kvT_sb[:, ti * 128:(ti + 1) * 128], w_uv)
                        o = ob16.get()
                        k.copy(o, ps, e="act")
                        rr = r0 + ti * 128
                        k.dma(VP.sl(rr // 128, np.s_[rr:rr + 128, :]), o)
                    for c in range(4):
                        ps = fm(7 + c)
                        o = ob16.get()
                        if st == 1:
                            sf = f32t.get()
                            k.copy(sf[:, :n], ps[:, :n], e="act")
                            rope_apply(sf, 128, n, r128, C("p128"), o)
                        else:
                            k.copy(o[:, :n], ps[:, :n], e="act")
                        dst = (WQT if c < 2 else WKT)
                        k.dma(dst.sl((c % 2, r0), np.s_[c % 2, :, r0:r0 + n]), o[:, :n])
                    ps = fm(11, 32)
                    o = ob16.get()
                    if st == 1:
                        sf = f32t.get()
                        k.copy(sf[0:32, :n], ps[0:32, :n], e="act")
                        rope_apply(sf, 32, n, r32, C("p32", 32), o)
                    else:
                        k.copy(o[0:32, :n], ps[0:32, :n], e="act")
                    k.dma(KRT.sl(r0, np.s_[:, r0:r0 + n]), o[0:32, :n])
                    for ti in range(nt):
                        o = tmo.get()
                        for hf in range(2):
                            ps = pfm.get()
                            c0 = 1440 + hf * 512
                            for kc in range(8):
                                k.mm(ps, hT[:, kc, ti * 128:(ti + 1) * 128], w_in[:, kc, c0:c0 + 512], start=(kc == 0), stop=(kc == 7))
                            k.copy(o[:, hf * 512:(hf + 1) * 512], ps, e=("act" if hf == 0 else "dve"))
                        rr = r0 + ti * 128
                        k.dma(TMo.sl(rr // 128, np.s_[rr:rr + 128, :]), o)
                pctx = norm_phase(blocks[0])
                for bi, blk in enumerate(blocks):
                    nctx = norm_phase(blocks[bi + 1]) if bi + 1 < len(blocks) else None
                    proj_phase(blk, pctx)
                    pctx = nctx
            k.barrier()
            if stop_here("s1_%d" % l):
                break
            with ExitStack() as s:
                lg_rep = k.sb([128, 8], F32, es=s)
                lg_pp = k.sb([128, 4], F32, es=s)
                for dst, src in ((lg_rep, small[:, sm0 + 3:sm0 + 11]), (lg_pp, small[:, sm0 + 15:sm0 + 19])):
                    k.act(dst, src, AF.Exp, scale=LN2)
                    k.ts(dst, dst, -1.0, ALU.mult, 1.0, ALU.add)
                    k.act(dst, dst, AF.Ln)
                LN8 = float(np.log(0.125))
                DT = k.sb([128, 4, 128], F32, es=s)
                tmpA = k.sb([128, 128], F32, es=s)
                for h in range(4):
                    k.ts(tmpA, C("A"), lg_rep[:, h:h + 1], ALU.mult)
                    k.stt(tmpA, C("B"), lg_rep[:, 4 + h:5 + h], tmpA, ALU.mult, ALU.add)
                    k.act(DT[:, h, :], tmpA, AF.Exp)
                    k.ts(DT[:, h, :], DT[:, h, :], 0.125, ALU.mult)
                QWF = k.sb([128, 2, 128], F32, es=s)
                QWB = k.sb([128, 2, 128], F32, es=s)
                gch = k.sb([128, 4], F32, es=s)
                for pr in range(2):
                    k.act(QWF[:, pr, :], C("C1"), AF.Exp, scale=lg_pp[:, pr:pr + 1])
                    k.act(QWB[:, pr, :], C("C2"), AF.Exp, scale=lg_pp[:, 2 + pr:3 + pr])
                k.act(gch, lg_pp, AF.Exp, scale=128.0)
                KWF = k.sb([128, 256], F32, es=s)
                KWB = k.sb([128, 256], F32, es=s)
                kcol = k.sb([128, 8], F32, es=s)
                for h in range(4):
                    k.act(kcol[:, h:h + 1], C("colK1"), AF.Exp, scale=lg_rep[:, h:h + 1])
                    k.act(kcol[:, 4 + h:5 + h], C("colK2"), AF.Exp, scale=lg_rep[:, 4 + h:5 + h])
                k.ts(kcol, kcol, 0.125, ALU.mult)
                for h in range(4):
                    k.copy(KWF[:, h * 64:(h + 1) * 64], T(kcol.ap[:, h:h + 1].to_broadcast([128, 64]), kcol.key))
                    k.copy(KWB[:, h * 64:(h + 1) * 64], T(kcol.ap[:, 4 + h:5 + h].to_broadcast([128, 64]), kcol.key))
                KVd = k.sb([128, NTILE, 4, 64], F32, es=s)
                Sfb = k.sb([128, NTILE, 2, 64], BF16, es=s)
                Sbb = k.sb([128, NTILE, 2, 64], BF16, es=s)
                kv_r = Rot([k.sb([128, 512], BF16, es=s) for _ in range(3)])
                kw_r = Rot([k.sb([128, 2, 256], BF16, es=s) for _ in range(2)])
                pkv = Rot([k.ps([128, 4, 128], F32, es=s) for _ in range(2)])
                fwd_order = list(range(NTILE))
                bwd_order = [1, 0] + list(range(NTILE - 1, 1, -1))
                r1_order = []
                for a_, b_ in zip(fwd_order, bwd_order):
                    for t_ in (a_, b_):
                        if t_ not in r1_order:
                            r1_order.append(t_)
                curs = []
                for d_ in range(2):
                    c_ = k.sb([128, 2, 64], F32, es=s)
                    k.memset(c_, 0.0)
                    curs.append(c_)
                ptrs = [0, 0]
                done = set()

                def scan_step(d_, n):
                    Sb_ = Sfb if d_ == 0 else Sbb
                    cur = curs[d_]
                    k.copy(Sb_.sl(n, np.s_[:, n, :, :]), cur)
                    for pr in range(2):
                        k.stt(cur[:, pr, :], cur[:, pr, :], gch[:, d_ * 2 + pr:d_ * 2 + pr + 1],
                              KVd.sl(n, np.s_[:, n, d_ * 2 + pr, :]), ALU.mult, ALU.add)
                for n in r1_order:
                    kvt = kv_r.get()
                    k.dma(kvt, TMo.sl(n, np.s_[n * 128:(n + 1) * 128, 0:512]))
                    kw = kw_r.get()
                    k.tt(kw[:, 0, :], kvt[:, 0:256], KWF, ALU.mult)
                    k.tt(kw[:, 1, :], kvt[:, 0:256], KWB, ALU.mult)
                    ps = pkv.get()
                    for d_ in range(2):
                        for pr in range(2):
                            k.mm(ps[:, d_ * 2 + pr, :], kw[:, d_, pr * 128:(pr + 1) * 128], kvt[:, 256 + pr * 128:256 + (pr + 1) * 128])
                    k.copy(KVd.sl(n, np.s_[0:64, n, :, :]), ps[0:64, :, 0:64], e="act")
                    k.copy(KVd.sl(n, np.s_[64:128, n, :, :]), ps[64:128, :, 64:128], e="act")
                    done.add(n)
                    for d_, order in ((0, fwd_order), (1, bwd_order)):
                        while ptrs[d_] < NTILE and order[ptrs[d_]] in done:
                            scan_step(d_, order[ptrs[d_]])
                            ptrs[d_] += 1
                qk_r = Rot([k.sb([128, 2, 2, 128], BF16, es=s) for _ in range(3)])
                vg_r = Rot([k.sb([128, 512], BF16, es=s) for _ in range(3)])
                qw_r = Rot([k.sb([128, 2, 2, 128], BF16, es=s) for _ in range(2)])
                sm_r = Rot([k.sb([128, 512], BF16, es=s) for _ in range(4)])
                pss = Rot([k.ps([128, 512], F32, es=s) for _ in range(2)] + [T(p_.ap.rearrange("p a b -> p (a b)"), p_.key) for p_ in pkv.t])
                psy = Rot([k.ps([128, 512], F32, es=s) for _ in range(2)])
                ysb_r = Rot([k.sb([128, 256], F32, es=s) for _ in range(3)])
                sq_r = Rot([k.sb([128, 256], F32, es=s) for _ in range(2)])
                st_r = Rot([k.sb([128, 16], F32, es=s) for _ in range(2)])
                sg_r = Rot([k.sb([128, 256], F32, es=s) for _ in range(3)])
                yo_r = Rot([k.sb([128, 2, 128], BF16, es=s) for _ in range(2)])
                yb_r = Rot([k.sb([128, 256], BF16, es=s) for _ in range(2)])
                ptb2 = Rot([k.ps([128, 2, 128], BF16, es=s) for _ in range(2)])

                sgall = k.sb([128, NTILE, 256], BF16, es=s)
                for n in range(0 if need_ctx else 2, NTILE):
                    gt_ = vg_r.get()
                    k.dma(gt_[:, 0:256], TMo.sl(n, np.s_[n * 128:(n + 1) * 128, 512:768]))
                    k.act(sgall[:, n, :], gt_[:, 0:256], AF.Silu)

                def r2A(n):
                    c0 = n * 128
                    qk = qk_r.get()
                    k.dma(qk[:, 0], TD(QT, "c p t -> p c t")[:, :, c0:c0 + 128])
                    k.dma(qk[:, 1], TD(KT, "c p t -> p c t")[:, :, c0:c0 + 128])
                    vg = vg_r.get()
                    k.dma(vg, TMo.sl(n, np.s_[c0:c0 + 128, 256:768]))
                    qw = qw_r.get()
                    k.tt(qw[:, 0], qk[:, 0], QWF, ALU.mult)
                    k.tt(qw[:, 1], qk[:, 0], QWB, ALU.mult)
                    py = psy.get()
                    psA = pss.get()
                    psB = pss.get()
                    for h in range(4):
                        pr, off = h // 2, (h % 2) * 64
                        pb = psA if h % 2 == 0 else psB
                        k.mm(pb[:, pr * 128:(pr + 1) * 128], qk[off:off + 64, 1, pr, :], qk[off:off + 64, 0, pr, :])
                    sm = sm_r.get()
                    smv = T(sm.ap.rearrange("p (a c) -> p a c", a=4), sm.key)
                    k.tt(smv[:, 0::2, :], T(psA.ap[:, 0:256].rearrange("p (a c) -> p a c", a=2), psA.key), DT[:, 0::2, :], ALU.mult)
                    k.tt(smv[:, 1::2, :], T(psB.ap[:, 0:256].rearrange("p (a c) -> p a c", a=2), psB.key), DT[:, 1::2, :], ALU.mult)
                    for h in range(4):
                        pr, off = h // 2, (h % 2) * 64
                        yo_ = py[:, h * 64:(h + 1) * 64]
                        k.mm(yo_, sm[:, h * 128:(h + 1) * 128], vg[:, h * 64:(h + 1) * 64], start=True, stop=False)
                        k.mm(yo_, qw[off:off + 64, 0, pr, :], Sfb.sl(n, np.s_[off:off + 64, n, pr, :]), start=False, stop=False)
                        k.mm(yo_, qw[off:off + 64, 1, pr, :], Sbb.sl(n, np.s_[off:off + 64, n, pr, :]), start=False, stop=True)
                    ysb = ysb_r.get()
                    k.copy(ysb, py[:, 0:256], e="act")
                    return (ysb, sgall[:, n, :])

                def r2B(n, ctx_):
                    ysb, sg = ctx_
                    c0 = n * 128
                    stt_ = st_r.get()
                    k.op("dve", lambda e: e.reduce_sum(stt_.ap[:, 0:4], ysb.ap.rearrange("p (h d) -> p h d", h=4), AX.X), reads=[ysb], writes=[stt_])
                    sq = sq_r.get()
                    k.tt(sq, ysb, ysb, ALU.mult)
                    k.op("dve", lambda e: e.reduce_sum(stt_.ap[:, 4:8], sq.ap.rearrange("p (h d) -> p h d", h=4), AX.X), reads=[sq], writes=[stt_])
                    k.ts(stt_[:, 0:8], stt_[:, 0:8], 1.0 / 64, ALU.mult)
                    k.tt(stt_[:, 8:12], stt_[:, 0:4], stt_[:, 0:4], ALU.mult)
                    k.tt(stt_[:, 12:16], stt_[:, 4:8], stt_[:, 8:12], ALU.subtract)
                    k.rsq(stt_[:, 12:16], stt_[:, 12:16], 1.0, 1e-5)
                    for h in range(4):
                        k.ts(ysb[:, h * 64:(h + 1) * 64], ysb[:, h * 64:(h + 1) * 64], stt_[:, h:h + 1], ALU.subtract,
                             stt_[:, 12 + h:13 + h], ALU.mult)
                    yb = yb_r.get()
                    k.tt(yb, ysb, sg, ALU.mult)
                    pt = ptb2.get()
                    for c in range(2):
                        k.tr(pt[:, c, :], yb[:, c * 128:(c + 1) * 128], ident_b)
                    yo = yo_r.get()
                    k.copy(yo, pt, e="act")
                    k.dma(TD(YT, "c p t -> p c t").sl(("ret", n), np.s_[:, 0:2, c0:c0 + 128]), yo)
                tiles_r = list(range(0 if need_ctx else 2, NTILE))
                pc_ = r2A(tiles_r[0])
                for ti_, n in enumerate(tiles_r):
                    nc_ = r2A(tiles_r[ti_ + 1]) if ti_ + 1 < len(tiles_r) else None
                    r2B(n, pc_)
                    pc_ = nc_
            k.barrier()
            if stop_here("ret%d" % l):
                break
            with ExitStack() as s:
                vp2 = k.sb([128, NTILE, 8, 128], BF16, es=s)
                VPv = TD(VP, "(t p) (h d) -> p t h d", p=128, d=64)
                for par in range(2):
                    k.memset(vp2[:, :, par::2, (1 - par) * 64:(2 - par) * 64], 1.0, e=("dve" if par == 0 else "pool"))
                    for h in range(par, 8, 2):
                        k.dma(vp2[:, :, h, par * 64:(par + 1) * 64], VPv[:, :, h, :])
                kh_l = [k.sb([128, NT], BF16, es=s) for _ in range(2)]
                for t_ in kh_l:
                    k.memset(t_, 0.0, e="pool")
                kh_r = Rot(kh_l)
                q_l = [k.sb([128, 512], BF16, es=s) for _ in range(3)]
                for t_ in q_l:
                    k.memset(t_, 0.0)
                q_r = Rot(q_l)
                rd_sets = []
                for par in range(2):
                    tl = [k.sb([128, 512], F32, es=s) for _ in range(2)]
                    for t_ in tl:
                        k.memset(t_, 0.0)
                    rd_sets.append(Rot(tl))
                pT_r = Rot([k.sb([128, 2, 512], BF16, es=s) for _ in range(4)])
                pss = Rot([k.ps([128, 2, 512], F32, es=s) for _ in range(2)])
                pacc = Rot([k.ps([128, 512], F32, es=s) for _ in range(4)])
                sw_r = Rot([k.sb([128, 512], F32, es=s) for _ in range(2)])
                yp_r = Rot([k.sb([128, 512], BF16, es=s) for _ in range(3)])
                qblocks = ([(0, 256, [0, 1])] if need_ctx else []) + [(256 + 512 * i, 512, list(range(NTILE))) for i in range(8)]
                LOOK = 1
                pend = []

                def emit_pv2(item):
                    h, r0, n, j, npair, kt2, pT, pv = item
                    for u_ in range(2):
                        first = (j == 0 and u_ == 0)
                        last = (j == npair - 1 and u_ == 1)
                        k.mm(pv[:, :n], vp2[:, kt2[u_], h, :], pT[:, u_, :n], start=first, stop=last)
                    if j == npair - 1:
                        off = (h % 2) * 64
                        dof = 64 - off
                        rd = rd_sets[h % 2].get()
                        k.op("dve", lambda e: e.reciprocal(rd.ap[dof:dof + 64, :n], pv.ap[dof:dof + 64, :n]), reads=[pv], writes=[rd])
                        sws = sw_r.get()
                        k.dma(sws[off:off + 64, :n], rd[dof:dof + 64, :n])
                        yp = yp_r.get()
                        k.tt(yp[off:off + 64, :n], pv[off:off + 64, :n], sws[off:off + 64, :n], ALU.mult)
                        k.dma(YT.sl(("mla", h, r0), np.s_[2 + h // 2, off:off + 64, r0:r0 + n]), yp[off:off + 64, :n])
                for h in range(8):
                    off = (h % 2) * 64
                    kh = kh_r.get()
                    k.dma(kh[0:64, :], KN[h // 2, off:off + 64, :])
                    k.dma(kh[64:96, :], KRT)
                    for (r0, n, kts) in qblocks:
                        q = q_r.get()
                        k.dma(q[0:64, :n], QN[h // 2, off:off + 64, r0:r0 + n])
                        k.dma(q[64:96, :n], QR[h, :, r0:r0 + n])
                        pv = pacc.get()
                        npair = len(kts) // 2
                        for j in range(npair):
                            kt2 = (kts[2 * j], kts[2 * j + 1])
                            ps = pss.get()
                            for u_ in range(2):
                                k.mm(ps[:, u_, :n], kh[:, kt2[u_] * 128:(kt2[u_] + 1) * 128], q[:, :n])
                            pT = pT_r.get()
                            k.act(pT[:, :, :n], ps[:, :, :n], AF.Exp, scale=SCALE_MLA)
                            pend.append((h, r0, n, j, npair, kt2, pT, pv))
                            if len(pend) > LOOK:
                                emit_pv2(pend.pop(0))
                while pend:
                    emit_pv2(pend.pop(0))
            k.barrier()
            if stop_here("mla%d" % l):
                break
            with ExitStack() as s:
                wkT = k.sb([128, 2, 2, NT], BF16, es=s)
                k.memset(wkT, 0.0, e="pool")
                for hk_ in range(2):
                    for g_ in range(2):
                        k.dma(wkT[g_ * 64:(g_ + 1) * 64, hk_, g_, :], WKT[hk_, g_ * 64:(g_ + 1) * 64, :])
                wqT = k.sb([128, 2, NT], BF16, es=s)
                k.dma(wqT, TD(WQT, "c p t -> p c t"))
                wv3 = k.sb([128, NTILE, 4, 128], BF16, es=s)
                WVv = T(TMo.ap[:, 768:1024].rearrange("(t p) (q d) -> p t q d", p=128, d=64), TMo.key)
                for par in range(2):
                    k.memset(wv3[:, :, par::2, (1 - par) * 64:(2 - par) * 64], 1.0, e=("dve" if par == 0 else "pool"))
                    for q_ in range(par, 4, 2):
                        k.dma(wv3[:, :, q_, par * 64:(par + 1) * 64], WVv[:, :, q_, :])
                esink = k.sb([128, 4], F32, es=s)
                k.act(esink, small[:, sm0 + 11:sm0 + 15], AF.Exp)
                rd_sets = []
                for par in range(2):
                    tl = [k.sb([128, 128], F32, es=s) for _ in range(2)]
                    for t_ in tl:
                        k.memset(t_, 0.0)
                    rd_sets.append(Rot(tl))
                pT_r = Rot([k.sb([128, 128], BF16, es=s) for _ in range(6)])
                pss = Rot([k.ps([128, 512], F32, es=s) for _ in range(4)])
                pacc = Rot([k.ps([128, 512], F32, es=s) for _ in range(3)])
                psw = Rot([k.ps([128, 512], F32, es=s) for _ in range(1)])
                sw_r = Rot([k.sb([128, 128], F32, es=s) for _ in range(2)])
                yp_r = Rot([k.sb([128, 128], BF16, es=s) for _ in range(3)])
                pend = []

                def emit_pvw(item):
                    n, qh, i, nk, kt, pT, pv, yp = item
                    hk, g = qh // 2, qh % 2
                    off = g * 64
                    dof = 64 - off
                    c0 = n * 128
                    last = (i == nk - 1)
                    k.mm(pv[:, 0:128], wv3[:, kt, qh, :], pT, start=(i == 0), stop=last)
                    if last:
                        rd = rd_sets[g].get()
                        k.ts(rd[dof:dof + 64, :], pv[dof:dof + 64, 0:128], esink[dof:dof + 64, qh:qh + 1], ALU.add)
                        k.op("dve", lambda e: e.reciprocal(rd.ap[dof:dof + 64, :], rd.ap[dof:dof + 64, :]), reads=[rd], writes=[rd])
                        sw = psw.get()
                        k.mm(sw[:, 0:128], C("pswap"), rd)
                        sws = sw_r.get()
                        k.copy(sws[off:off + 64, :], sw[off:off + 64, 0:128], e="act")
                        k.tt(yp[off:off + 64, :], pv[off:off + 64, 0:128], sws[off:off + 64, :], ALU.mult)
                        if g == 1:
                            k.dma(YT.sl(("win", hk, n), np.s_[6 + hk, :, c0:c0 + 128]), yp)
                yp = None
                for n in range(0 if need_ctx else 2, NTILE):
                    c0 = n * 128
                    if n < 2:
                        keys = [(0, None), (1, None)]
                    else:
                        keys = []
                        if n - 1 >= 2:
                            keys.append((n - 1, mprev_b))
                        keys.append((n, None))
                        if n + 1 < NTILE:
                            keys.append((n + 1, mnext_b))
                        keys += [(0, None), (1, None)]
                    for qh in range(4):
                        hk, g = qh // 2, qh % 2
                        off = g * 64
                        pv = pacc.get()
                        if g == 0:
                            yp = yp_r.get()
                        for i, (kt, msk) in enumerate(keys):
                            ps = pss.get()
                            k.mm(ps[:, 0:128], wkT[:, hk, g, kt * 128:(kt + 1) * 128], wqT[:, hk, c0:c0 + 128])
                            pT = pT_r.get()
                            k.act(pT, ps[:, 0:128], AF.Exp, scale=0.125)
                            if msk is not None:
                                k.tt(pT, pT, msk, ALU.mult)
                            pend.append((n, qh, i, len(keys), kt, pT, pv, yp))
                            if len(pend) > 3:
                                emit_pvw(pend.pop(0))
                while pend:
                    emit_pvw(pend.pop(0))
            k.barrier()
            if stop_here("win%d" % l):
                break
            es_aff = ExitStack()
            aff_e = k.sb([16, NT], F32, "aff_e", es=es_aff)
            with ExitStack() as s:
                w_out = k.sb([128, 8, D], BF16, es=s)
                stg = Rot([k.sb([128, D], F32, es=s) for _ in range(2)])
                for kc in range(8):
                    load_cast(w_out[:, kc, :], w_out_d[l, kc * 128:(kc + 1) * 128, :], stg, e=("pool" if kc % 2 else "dve"))
                rw = k.sb([128, 8, 16], F32, es=s)
                k.dma(rw, TD(router_d[l], "(kc p) e -> p kc e", p=128))
                sts = [0, 1] if need_ctx else [1]
                mod2 = {st: load_mod(st, 2, s) for st in sts}
                gs2 = {st: load_gs(st, 4, l * 2 + 1, s) for st in sts}
                sh2 = {st: load_mod(st, 3, s) for st in sts}
                yT_r = Rot([k.sb([128, 8, 128], BF16, es=s) for _ in range(2)])
                xt_r = Rot([k.sb([128, D], F32, es=s) for _ in range(2)])
                xn_r = Rot([k.sb([128, D], F32, es=s) for _ in range(2)])
                xs_r = Rot([k.sb([128, D], F32, es=s) for _ in range(3)])
                h2b_r = Rot([k.sb([128, D], BF16, es=s) for _ in range(2)])
                h2T_r = Rot([k.sb([128, 8, 128], F32, es=s) for _ in range(2)])
                junk = k.sb([128, D], F32, es=s)
                ss_pool = Rot([k.sb([128, 1], F32, es=s) for _ in range(8)])
                ex_r = Rot([k.sb([128, 16], F32, es=s) for _ in range(2)])
                pso = Rot([k.ps([128, 512], F32, es=s) for _ in range(3)])
                ptr = Rot([k.ps([128, 4, 128], F32, es=s) for _ in range(2)])
                psl = Rot([k.ps([128, 512], F32, es=s) for _ in range(2)])
                def phaseA(n):
                    st = 0 if n < 2 else 1
                    c0 = n * 128
                    yT = yT_r.get()
                    k.dma(yT, TD(YT, "c p t -> p c t")[:, :, c0:c0 + 128])
                    xt = xt_r.get()
                    k.dma(xt, rows_key(Xsrc, n)[c0:c0 + 128, :])
                    xn = xn_r.get()
                    for hf in range(2):
                        ps = pso.get()
                        for kc in range(8):
                            k.mm(ps, yT[:, kc, :], w_out[:, kc, hf * 512:(hf + 1) * 512], start=(kc == 0), stop=(kc == 7))
                        sl_ = np.s_[:, hf * 512:(hf + 1) * 512]
                        k.tt(xn[sl_], ps, mod2[st][sl_], ALU.mult)
                        k.tt(xn[sl_], xn[sl_], xt[sl_], ALU.add)
                    k.dma(rows_key(X, n)[c0:c0 + 128, :], xn)
                    ss = ss_pool.get()
                    k.act(junk, xn, AF.Square, accum=ss)
                    rstd = ss_pool.get()
                    k.rsq(rstd, ss, 1.0 / D, EPS)
                    xs = xs_r.get()
                    k.stt(xs, xn, rstd, gs2[st], ALU.mult, ALU.mult)
                    k.tt(xs, xs, sh2[st], ALU.add)
                    h2b = h2b_r.get()
                    k.copy(h2b, xs, e="act")
                    k.dma(H2.sl(n, np.s_[c0:c0 + 128, :]), h2b)
                    return xs

                def phaseB(n, xs):
                    c0 = n * 128
                    h2T = h2T_r.get()
                    for hf in range(2):
                        pt = ptr.get()
                        for q4 in range(4):
                            kc = hf * 4 + q4
                            k.tr(pt[:, q4, :], xs[:, kc * 128:(kc + 1) * 128], ident)
                        k.copy(h2T[:, hf * 4:(hf + 1) * 4, :], pt, e="act")
                    pl = psl.get()
                    for kc in range(8):
                        k.mm(pl[:, 0:16], h2T[:, kc, :], rw[:, kc, :], start=(kc == 0), stop=(kc == 7))
                    ex = ex_r.get()
                    sm_ = ss_pool.get()
                    k.act(ex, pl[:, 0:16], AF.Exp, accum=sm_)
                    k.op("dve", lambda e: e.reciprocal(sm_.ap, sm_.ap), reads=[sm_], writes=[sm_])
                    k.ts(affT.sl(n, np.s_[:, n, :]), ex, sm_, ALU.mult)
                    pl2 = psl.get()
                    k.tr(pl2[0:16, 0:128], affT.sl(n, np.s_[:, n, :]), ident)
                    k.copy(aff_e.sl(n, np.s_[:, c0:c0 + 128]), pl2[0:16, 0:128], e="act")
                tiles_o = list(range(0 if need_ctx else 2, NTILE))
                pxs = phaseA(tiles_o[0])
                for ti_, n in enumerate(tiles_o):
                    nxs = phaseA(tiles_o[ti_ + 1]) if ti_ + 1 < len(tiles_o) else None
                    phaseB(n, pxs)
                    pxs = nxs
            k.barrier()
            if stop_here("o%d" % l):
                es_aff.close()
                break
            streams = ([(0, 2, 32)] if need_ctx else []) + [(2, 32, 512)]
            with ExitStack() as s:
                work = k.sb([16, NTX], F32, es=s)
                m8 = k.sb([16, 8], F32, es=s)
                thr = k.sb([16, 1], F32, es=s)
                aff128 = k.sb([128, 512], F32, es=s)
                junk16 = k.sb([128, 512], BF16, es=s)
                lo = k.sb([128, 1], F32, es=s)
                hi = k.sb([128, 1], F32, es=s)
                mid = k.sb([128, 1], F32, es=s)
                cnt = k.sb([128, 1], F32, es=s)
                half = k.sb([128, 1], F32, es=s)
                k.memset(half, 0.5)
                mge = k.sb([128, 1], U32, es=s)
                mlt = k.sb([128, 1], U32, es=s)
                thr16 = k.sb([16, 8], F32, es=s)
                mask_e = k.sb([16, NTX], F32, es=s)
                maskTb = k.sb([128, 32, 16], BF16, es=s)
                carry = k.sb([128, 16], F32, es=s)
                pos_r = Rot([k.sb([128, 16], F32, es=s) for _ in range(2)])
                ptk = Rot([k.ps([128, 512], F32, es=s) for _ in range(4)])
                ahi = k.sb([128, NTILE, 16], BF16, es=s)
                alo = k.sb([128, NTILE, 16], F32, es=s)
                k.copy(ahi, affT)
                k.tt(alo, affT, ahi, ALU.subtract)
                k.copy(Rtab[:, :, :, 2], ahi, e="pool")
                k.copy(Rtab[:, :, :, 3], alo, e="pool")
                for (t0, ntl, cap) in streams:
                    ntok = ntl * 128
                    cs = np.s_[:, t0 * 128:t0 * 128 + ntok]
                    if cap <= 64:
                        k.copy(work[:, :ntok], aff_e[cs])
                        for r in range(cap // 8):
                            k.op("dve", lambda e: e.max(out=m8.ap, in_=work.ap[:, :ntok]), reads=[work], writes=[m8])
                            if r < cap // 8 - 1:
                                k.op("dve", lambda e: e.match_replace(out=work.ap[:, :ntok], in_to_replace=m8.ap,
                                                                      in_values=work.ap[:, :ntok], imm_value=-1.0),
                                     reads=[work, m8], writes=[work])
                        k.copy(thr, m8[:, 7:8])
                    else:
                        k.dma(AFFD, aff_e[cs])
                        k.dma(aff128, TD(AFFD, "e (s t) -> (e s) t", s=8))
                        k.memset(lo, 0.0)
                        k.memset(hi, 2.0)
                        for it in range(40):
                            k.stt(mid, lo, hi, half, ALU.add, ALU.mult)
                            k.ts(junk16, aff128, mid, ALU.is_ge, 0.0, ALU.add, accum=cnt)
                            pc = ptk.get()
                            k.mm(pc[:, 0:1], C("bmat"), cnt)
                            k.ts(mge, pc[:, 0:1], cap - 0.5, ALU.is_ge)
                            k.ts(mlt, pc[:, 0:1], cap - 0.5, ALU.is_lt)
                            k.op("dve", lambda e: e.copy_predicated(lo.ap, mge.ap, mid.ap), reads=[mge, mid], writes=[lo])
                            k.op("dve", lambda e: e.copy_predicated(hi.ap, mlt.ap, mid.ap), reads=[mlt, mid], writes=[hi])
                        k.dma(THRD, lo)
                        k.dma(thr16, TD(THRD, "(e s) o -> e (s o)", s=8))
                        k.copy(thr, thr16[:, 0:1])
                    k.ts(mask_e[:, :ntok], aff_e[cs], thr, ALU.is_ge)
                    k.memset(carry, 0.0)
                    for i in range(ntl):
                        n = t0 + i
                        pt = ptk.get()
                        k.tr(pt[:, 0:16], mask_e[:, i * 128:(i + 1) * 128], ident[0:16, 0:16])
                        k.copy(maskTb[:, i, :], pt[:, 0:16], e="act")
                        pc = ptk.get()
                        k.mm(pc[:, 0:16], triU_b, maskTb[:, i, :])
                        k.mm(pc[:, 16:32], ones_b, maskTb[:, i, :])
                        pos = pos_r.get()
                        k.tt(pos, pc[:, 0:16], carry, ALU.add)
                        k.tt(pos, pos, maskTb[:, i, :], ALU.mult)
                        k.ts(posm.sl(n, np.s_[:, n, :]), pos, -1.0, ALU.add)
                        k.tt(carry, carry, pc[:, 16:32], ALU.add)
            k.barrier()
            if stop_here("topk%d" % l):
                es_aff.close()
                break
            es_aff.close()
            with ExitStack() as s:
                mod5 = {st: load_mod(st, 5, s) for st in ([0, 1] if need_ctx else [1])}
                wsets = Rot([(k.sb([128, 8, 768], BF16, es=s), k.sb([128, 8, 768], BF16, es=s), k.sb([128, 6, D], BF16, es=s))
                             for _ in range(2)])
                stg = Rot([k.sb([128, 768], F32, es=s) for _ in range(4)])
                Sel = k.sb([128, 32, 512], BF16, es=s)
                iota16 = k.sb([128, 512], mybir.dt.int16, es=s)
                k.copy(iota16, C("iota"))
                r4T_r = Rot([k.sb([4, 512], F32, es=s) for _ in range(2)])
                r4_r = Rot([k.sb([128, 16], F32, es=s) for _ in range(3)])
                idx_r = Rot([k.sb([128, 1], I32, es=s) for _ in range(16)])
                idf_r = Rot([k.sb([128, 1], F32, es=s) for _ in range(4)])
                gt_r = Rot([k.sb([128, 1], F32, es=s) for _ in range(16)])
                xs_r = Rot([k.sb([128, D], BF16, es=s) for _ in range(10)])
                xsT_r = Rot([k.sb([128, 8, 512], BF16, es=s) for _ in range(2)])
                hid = k.sb([128, 6, 512], BF16, es=s)
                sg_r = Rot([k.sb([128, 512], F32, es=s) for _ in range(2)])
                ys_r = Rot([k.sb([128, D], F32, es=s) for _ in range(2)])
                p4 = Rot([k.ps([128, 512], F32, es=s) for _ in range(1)])
                ptb = Rot([k.ps([128, 8, 128], BF16, es=s) for _ in range(1)])
                pgu = Rot([k.ps([128, 512], F32, es=s) for _ in range(4)])
                pdn = Rot([k.ps([128, 512], F32, es=s) for _ in range(2)])
                Xall = [rows_key(X, n) for n in range(NTILE)] + [X.sub("all")]
                cast_eng = Rot(["act", "dve"])
                wcache = {}

                wgen = {"g": None}

                def weights_gen(e_, wg, wu, wd):
                    for kc in range(8):
                        load_cast(wg[:, kc, :], wg_d[l, e_, kc * 128:(kc + 1) * 128, :], stg, e=cast_eng.get())
                        yield
                        load_cast(wu[:, kc, :], wu_d[l, e_, kc * 128:(kc + 1) * 128, :], stg, e=cast_eng.get())
                        yield
                    for fc in range(6):
                        for hf_ in range(2):
                            load_cast(wd[:, fc, hf_ * 512:(hf_ + 1) * 512], wd_d[l, e_, fc * 128:(fc + 1) * 128, hf_ * 512:(hf_ + 1) * 512], stg, e=cast_eng.get())
                            yield

                def feed(n_):
                    for _ in range(n_):
                        if wgen["g"] is None:
                            return
                        try:
                            next(wgen["g"])
                        except StopIteration:
                            wgen["g"] = None

                def weights(e_, now=False):
                    feed(10000)
                    wg, wu, wd = wsets.get()
                    wcache[e_] = (wg, wu, wd)
                    wgen["g"] = weights_gen(e_, wg, wu, wd)
                    if now:
                        feed(10000)

                def selbuild(u):
                    e_, (t0, ntl, cap) = u
                    for i in range(ntl):
                        k.ts(Sel[:, i, :cap], iota16[:, :cap], posm[:, t0 + i, e_:e_ + 1], ALU.is_equal)

                def idxpart(u):
                    e_, (t0, ntl, cap) = u
                    ps = p4.get()
                    for i in range(ntl):
                        k.mm(ps[0:4, :cap], Rtab[:, t0 + i, e_, :], Sel[:, i, :cap], start=(i == 0), stop=(i == ntl - 1))
                    r4T = r4T_r.get()
                    k.copy(r4T[:, :cap], ps[0:4, :cap])
                    stiles = [(s0, min(128, cap - s0)) for s0 in range(0, cap, 128)]
                    for si, (s0, nsl) in enumerate(stiles):
                        k.tr(ps[0:nsl, si * 4:(si + 1) * 4], r4T[0:4, s0:s0 + nsl], ident[0:4, 0:4])
                    nsl0 = stiles[0][1]
                    r4 = r4_r.get()
                    k.copy(r4[0:nsl0, 0:4 * len(stiles)], ps[0:nsl0, 0:4 * len(stiles)])
                    meta = []
                    xss = []
                    for si, (s0, nsl) in enumerate(stiles):
                        c4 = si * 4
                        idf = idf_r.get()
                        k.stt(idf[0:nsl, :], r4[0:nsl, c4:c4 + 1], 64.0, r4[0:nsl, c4 + 1:c4 + 2], ALU.mult, ALU.add)
                        idx = idx_r.get()
                        k.copy(idx[0:nsl, :], idf[0:nsl, :])
                        gt = gt_r.get()
                        k.tt(gt[0:nsl, :], r4[0:nsl, c4 + 2:c4 + 3], r4[0:nsl, c4 + 3:c4 + 4], ALU.add)
                        meta.append((idx, gt))
                        xs = xs_r.get()
                        k.dma(xs[0:nsl, :], H2, q="pool", reads=[idx],
                              fn=lambda en: en.indirect_dma_start(out=xs.ap[0:nsl, :], out_offset=None, in_=H2.ap,
                                                                  in_offset=bass.IndirectOffsetOnAxis(ap=idx.ap[0:nsl, :], axis=0)))
                        xss.append(xs)
                    return [u, stiles, meta, xss, None]

                def xtrans(item):
                    u, stiles, meta, xss, _ = item
                    xsT = xsT_r.get()
                    for si, (s0, nsl) in enumerate(stiles):
                        xs = xss[si]
                        pt = ptb.get()
                        for kc in range(8):
                            k.tr(pt[:, kc, 0:nsl], xs[0:nsl, kc * 128:(kc + 1) * 128], ident_b[0:nsl, 0:nsl])
                        k.copy(xsT[:, :, s0:s0 + nsl], pt[:, :, 0:nsl], e="act")
                    item[4] = xsT

                def ffn(item):
                    (e_, (t0, ntl, cap)), stiles, meta, xss, xsT = item
                    wg, wu, wd = wcache[e_]
                    for fc in range(6):
                        pg = pgu.get()
                        pu = pgu.get()
                        for kc in range(8):
                            k.mm(pg[:, :cap], wg[:, kc, fc * 128:(fc + 1) * 128], xsT[:, kc, :cap], start=(kc == 0), stop=(kc == 7))
                        for kc in range(8):
                            k.mm(pu[:, :cap], wu[:, kc, fc * 128:(fc + 1) * 128], xsT[:, kc, :cap], start=(kc == 0), stop=(kc == 7))
                        sg = sg_r.get()
                        k.act(sg[:, :cap], pg[:, :cap], AF.Silu)
                        k.tt(hid[:, fc, :cap], sg[:, :cap], pu[:, :cap], ALU.mult)
                        feed(2)

                def down(item):
                    (e_, (t0, ntl, cap)), stiles, meta, xss, xsT = item
                    st = 0 if t0 == 0 else 1
                    wg, wu, wd = wcache[e_]
                    for si, (s0, nsl) in enumerate(stiles):
                        idx, gt = meta[si]
                        ys = ys_r.get()
                        for hf in range(2):
                            pd = pdn.get()
                            for fc in range(6):
                                k.mm(pd[0:nsl, :], hid[:, fc, s0:s0 + nsl], wd[:, fc, hf * 512:(hf + 1) * 512], start=(fc == 0), stop=(fc == 5))
                            k.stt(ys[0:nsl, hf * 512:(hf + 1) * 512], pd[0:nsl, :], gt[0:nsl, 0:1],
                                  mod5[st][0:nsl, hf * 512:(hf + 1) * 512], ALU.mult, ALU.mult)
                            feed(2)
                        k.dma(X, ys[0:nsl, :], q="pool", reads=[ys, idx], writes=Xall,
                              fn=lambda en: en.indirect_dma_start(out=X.ap, out_offset=bass.IndirectOffsetOnAxis(ap=idx.ap[0:nsl, :], axis=0),
                                                                  in_=ys.ap[0:nsl, :], in_offset=None, compute_op=ALU.add))

                units = [(e_, stm) for e_ in range(16) for stm in reversed(streams)]
                NU = len(units)
                weights(0, now=True)
                weights(1, now=True)
                nxt_w = 2
                selbuild(units[0])
                items = {0: idxpart(units[0])}
                if NU > 1:
                    selbuild(units[1])
                    items[1] = idxpart(units[1])
                xtrans(items[0])
                if NU > 2:
                    selbuild(units[2])
                for ui in range(NU):
                    ffn(items[ui])
                    if ui + 1 < NU:
                        xtrans(items[ui + 1])
                    if ui + 2 < NU:
                        items[ui + 2] = idxpart(units[ui + 2])
                    down(items[ui])
                    if ui + 3 < NU:
                        selbuild(units[ui + 3])
                    e_done = units[ui][0]
                    if (ui + 1 == NU or units[ui + 1][0] != e_done) and nxt_w < 16:
                        weights(nxt_w)
                        nxt_w += 1
                    del items[ui]
            k.barrier()
            if stop_here("exp%d" % l):
                break
        else:
            with ExitStack() as s:
                gfin = k.sb([128, D], F32, es=s)
                k.dma(gfin, g_bc_d[4])
                xt_r = Rot([k.sb([128, D], F32, es=s) for _ in range(5)])
                junk = k.sb([128, D], F32, es=s)
                ss_pool = Rot([k.sb([128, 1], F32, es=s) for _ in range(12)])
                def finA(n):
                    c0 = n * 128
                    xt = xt_r.get()
                    k.dma(xt, rows_key(X, n)[c0:c0 + 128, :])
                    ss = ss_pool.get()
                    k.act(junk, xt, AF.Square, accum=ss)
                    return (xt, ss)

                def finB(n, c_):
                    xt, ss = c_
                    c0 = n * 128
                    rstd = ss_pool.get()
                    k.rsq(rstd, ss, 1.0 / D, EPS)
                    k.stt(xt, xt, rstd, gfin, ALU.mult, ALU.mult)
                    k.dma(out_d.sl(n, np.s_[c0 - NTC:c0 - NTC + 128, :]), xt)
                tl_ = list(range(2, NTILE))
                q_ = [finA(tl_[0]), finA(tl_[1])]
                for i_, n in enumerate(tl_):
                    if i_ + 2 < len(tl_):
                        q_.append(finA(tl_[i_ + 2]))
                    finB(n, q_.pop(0))
        k.barrier()
        print("ninst", k.ninst, k.cnt, flush=True)
    return nc


_NC_CACHE = {}


def kernel(**inputs):
    inp = {kk: np.asarray(v) for kk, v in inputs.items()}
    shared = _prep_shared(inp)
    if "nc" not in _NC_CACHE:
        _NC_CACHE["nc"] = build()
    nc = _NC_CACHE["nc"]
    in_maps = []
    for b in range(8):
        m = dict(shared)
        m.update(_prep_core(inp, b))
        in_maps.append(m)
    res = run_bass_kernel_spmd(nc, in_maps, core_ids=list(range(8)))
    out = np.stack([np.asarray(r["out"], dtype=np.float32) for r in res.results], 0)
    return out
```

```python
import numpy as np
from contextlib import ExitStack
import concourse.bass as bass
import concourse.mybir as mybir
from concourse.bass_utils import run_bass_kernel_spmd

F32 = mybir.dt.float32
BF16 = mybir.dt.bfloat16
I32 = mybir.dt.int32
U32 = mybir.dt.uint32
AF = mybir.ActivationFunctionType
ALU = mybir.AluOpType
AX = mybir.AxisListType


class T:
    __slots__ = ("ap", "key")

    def __init__(self, ap, key):
        self.ap = ap
        self.key = key

    def __getitem__(self, idx):
        return T(self.ap[idx], self.key)

    def sub(self, suffix):
        return T(self.ap, (self.key, suffix))

    def sl(self, suffix, idx):
        return T(self.ap[idx], (self.key, suffix))


def _key(x):
    return x.key if isinstance(x, T) else x


class KB:
    CE = ("pe", "act", "dve", "pool")

    def __init__(self, nc, es, nds=14):
        self.nc = nc
        self.es = es
        self.E = {"pe": nc.tensor, "act": nc.scalar, "dve": nc.vector,
                  "pool": nc.gpsimd, "sp": nc.sync}
        self.csem = {e: es.enter_context(nc.semaphore("c_" + e)) for e in self.CE}
        self.cnt = {e: 0 for e in self.CE}
        self.NDS = nds
        self.dsem = [es.enter_context(nc.semaphore("d%d" % i)) for i in range(nds)]
        self.dcnt = [0] * nds
        self.dnext = 0
        self.waited = {e: {} for e in self.E}
        self.lastw = {}
        self.readers = {}
        self.nalloc = 0
        self.ninst = 0

    def sb(self, shape, dtype, name=None, es=None):
        self.nalloc += 1
        name = name or ("t%d" % self.nalloc)
        t = (es or self.es).enter_context(self.nc.sbuf_tensor(name + "_%d" % self.nalloc, list(shape), dtype))
        return T(t[:], name + "_%d" % self.nalloc)

    def ps(self, shape, dtype, name=None, es=None):
        self.nalloc += 1
        name = name or ("p%d" % self.nalloc)
        t = (es or self.es).enter_context(self.nc.psum_tensor(name + "_%d" % self.nalloc, list(shape), dtype))
        return T(t[:], name + "_%d" % self.nalloc)

    def dram(self, name, shape, dtype, kind="Internal"):
        t = self.nc.dram_tensor(name, list(shape), dtype, kind=kind)
        return T(t.ap(), name)

    def _semobj(self, semkey):
        return self.csem[semkey[1]] if semkey[0] == "c" else self.dsem[semkey[1]]

    def _wait(self, e, semkey, val):
        if self.waited[e].get(semkey, 0) >= val:
            return
        self.waited[e][semkey] = val
        self.E[e].wait_ge(self._semobj(semkey), val)

    def _deps(self, e, reads, writes, is_dma):
        for r in reads:
            lw = self.lastw.get(_key(r))
            if lw is not None:
                self._wait(e, lw[0], lw[1])
        for w in writes:
            k = _key(w)
            lw = self.lastw.get(k)
            if lw is not None:
                if not (lw[0] == ("c", e) and e == "pe" and not is_dma):
                    self._wait(e, lw[0], lw[1])
            for sk, v in self.readers.get(k, {}).items():
                if sk == ("c", e) and e == "pe" and not is_dma:
                    continue
                self._wait(e, sk, v)

    def _record(self, tok, reads, writes):
        for r in reads:
            d = self.readers.setdefault(_key(r), {})
            if d.get(tok[0], 0) < tok[1]:
                d[tok[0]] = tok[1]
        for w in writes:
            k = _key(w)
            self.lastw[k] = tok
            self.readers[k] = {}

    def op(self, e, fn, reads=(), writes=()):
        self._deps(e, reads, writes, False)
        ins = fn(self.E[e])
        self.cnt[e] += 1
        ins.then_inc(self.csem[e], 1)
        self._record((("c", e), self.cnt[e]), reads, writes)
        self.ninst += 1
        return ins

    def dma(self, out, in_, q="sp", fn=None, reads=None, writes=None, **kw):
        reads = [in_] if reads is None else reads
        writes = [out] if writes is None else writes
        slot = self.dnext
        self.dnext = (slot + 1) % self.NDS
        if self.dcnt[slot] > 0:
            self._wait(q, ("d", slot), 16 * self.dcnt[slot])
        self._deps(q, reads, writes, True)
        if fn is None:
            ins = self.E[q].dma_start(out=out.ap, in_=in_.ap, **kw)
        else:
            ins = fn(self.E[q])
        self.dcnt[slot] += 1
        ins.then_inc(self.dsem[slot], 16)
        self._record((("d", slot), 16 * self.dcnt[slot]), reads, writes)
        self.ninst += 1
        return ins

    def barrier(self):
        for e in self.E:
            for e2 in self.CE:
                if e2 != e and self.cnt[e2] > 0:
                    self._wait(e, ("c", e2), self.cnt[e2])
            for s in range(self.NDS):
                if self.dcnt[s] > 0:
                    self._wait(e, ("d", s), 16 * self.dcnt[s])

    def mm(self, out, lhsT, rhs, start=True, stop=True, extra_reads=()):
        return self.op("pe", lambda e: e.matmul(out.ap, lhsT.ap, rhs.ap, start=start, stop=stop),
                       reads=[lhsT, rhs, *extra_reads], writes=[out])

    def tr(self, out, in_, ident):
        return self.op("pe", lambda e: e.transpose(out.ap, in_.ap, ident.ap),
                       reads=[in_, ident], writes=[out])

    def act(self, out, in_, func, bias=None, scale=None, accum=None, e="act"):
        kw = {}
        rd = [in_]
        wr = [out]
        if bias is not None:
            if isinstance(bias, T):
                kw["bias"] = bias.ap
                rd.append(bias)
            else:
                kw["bias"] = bias
        if scale is not None:
            if isinstance(scale, T):
                kw["scale"] = scale.ap
                rd.append(scale)
            else:
                kw["scale"] = scale
        if accum is not None:
            kw["accum_out"] = accum.ap
            wr.append(accum)
        return self.op(e, lambda en: en.activation(out.ap, in_.ap, func, **kw), reads=rd, writes=wr)

    def tt(self, out, a, b, op, e="dve"):
        return self.op(e, lambda en: en.tensor_tensor(out.ap, a.ap, b.ap, op), reads=[a, b], writes=[out])

    def ts(self, out, a, s1, op0, s2=None, op1=None, e="dve", accum=None):
        rd = [a]
        wr = [out]
        v1 = s1
        v2 = s2
        if isinstance(s1, T):
            rd.append(s1)
            v1 = s1.ap
        if isinstance(s2, T):
            rd.append(s2)
            v2 = s2.ap
        kw = {}
        if op1 is not None:
            kw["op1"] = op1
        if accum is not None:
            kw["accum_out"] = accum.ap
            wr.append(accum)
        return self.op(e, lambda en: en.tensor_scalar(out.ap, a.ap, v1, v2, op0, **kw), reads=rd, writes=wr)

    def stt(self, out, a, s, b, op0, op1, e="dve"):
        rd = [a, b]
        v = s
        if isinstance(s, T):
            rd.append(s)
            v = s.ap
        return self.op(e, lambda en: en.scalar_tensor_tensor(out.ap, a.ap, v, b.ap, op0, op1), reads=rd, writes=[out])

    def copy(self, out, in_, e="dve"):
        if e == "act":
            return self.op(e, lambda en: en.activation(out.ap, in_.ap, AF.Copy), reads=[in_], writes=[out])
        return self.op(e, lambda en: en.tensor_copy(out.ap, in_.ap), reads=[in_], writes=[out])

    def memset(self, out, v, e="dve"):
        return self.op(e, lambda en: en.memset(out.ap, v), reads=[], writes=[out])

    def rsq(self, dst, src, mul, add):
        self.ts(dst, src, mul, ALU.mult, add, ALU.add)
        self.act(dst, dst, AF.Sqrt)
        self.op("dve", lambda e: e.reciprocal(dst.ap, dst.ap), reads=[dst], writes=[dst])

L_ = 2
D = 1024
NTX = 4096
NTC = 256
NT = NTX + NTC
NTILE = NT // 128
NCOL = 2464
SCALE_MLA = float((64 + 32) ** -0.5)
LN2 = float(np.log(2.0))


def _rope_tables():
    t = np.arange(NTX)
    row = (t // 64).astype(np.float32)
    col = (t % 64).astype(np.float32)

    def tab(dh_half):
        dh = dh_half
        inv = 10000.0 ** (-np.arange(0, dh, 2, dtype=np.float32) / dh)
        return inv

    def build(dtot):
        half = dtot // 2
        inv = tab(half)
        nf = half // 2
        cos = np.zeros((dtot, NTX), np.float32)
        sins = np.zeros((dtot, NTX), np.float32)
        perm = np.zeros((dtot, dtot), np.float32)
        for part, pos in ((0, row), (1, col)):
            base = part * half
            ang = pos[None, :] * inv[:, None]
            c, s = np.cos(ang), np.sin(ang)
            for j in range(nf):
                cos[base + j] = c[j]
                cos[base + nf + j] = c[j]
                sins[base + j] = -s[j]
                sins[base + nf + j] = s[j]
                perm[base + nf + j, base + j] = 1.0
                perm[base + j, base + nf + j] = 1.0
        return cos, sins, perm

    c32, s32, p32 = build(32)
    c64, s64, p64 = build(64)
    c128 = np.concatenate([c64, c64], 0)
    s128 = np.concatenate([s64, s64], 0)
    p128 = np.zeros((128, 128), np.float32)
    p128[:64, :64] = p64
    p128[64:, 64:] = p64
    rope32 = np.stack([np.tile(c32, (4, 1)), np.tile(s32, (4, 1))]).astype(np.float32)
    rope128 = np.stack([c128, s128]).astype(np.float32)
    return rope32, rope128, p32, p128


_CST = {}


def _cst_layout():
    off = 0
    for name, w in (("ident", 128), ("A", 128), ("B", 128), ("C1", 128), ("C2", 128),
                    ("colK1", 1), ("colK2", 1), ("iota", 512), ("triU", 128),
                    ("mprev", 128), ("mnext", 128), ("p128", 128), ("p32", 32), ("pswap", 128), ("bmat", 128), ("p32x4", 128),
                    ("tokid", NTILE * 16 * 2)):
        _CST[name] = (off, w)
        off += w
    return off


NCST = _cst_layout()


def _const_table():
    rope32, rope128, p32, p128 = _rope_tables()
    cst = np.zeros((128, NCST), np.float32)
    p = np.arange(128, dtype=np.float32)[:, None]
    c = np.arange(128, dtype=np.float32)[None, :]

    def put(name, arr):
        o, w = _CST[name]
        cst[:arr.shape[0], o:o + w] = arr
    put("ident", np.eye(128, dtype=np.float32))
    put("A", np.maximum(c - p, 0.0))
    put("B", np.maximum(p - c, 0.0))
    put("C1", np.broadcast_to(c + 1.0, (128, 128)))
    put("C2", np.broadcast_to(128.0 - c, (128, 128)))
    put("colK1", 127.0 - p)
    put("colK2", p)
    put("iota", np.broadcast_to(np.arange(512, dtype=np.float32)[None, :], (128, 512)))
    put("triU", (p <= c).astype(np.float32))
    put("mprev", (p >= c).astype(np.float32))
    put("mnext", (p <= c).astype(np.float32))
    put("p128", p128)
    put("p32", p32)
    p32x4 = np.zeros((128, 128), np.float32)
    for i_ in range(4):
        p32x4[i_ * 32:(i_ + 1) * 32, i_ * 32:(i_ + 1) * 32] = p32
    put("p32x4", p32x4)
    put("pswap", (np.arange(128)[:, None] == (np.arange(128)[None, :] + 64) % 128).astype(np.float32))
    put("bmat", (np.arange(128)[:, None] // 8 == np.arange(128)[None, :] // 8).astype(np.float32))
    rows = (np.arange(NTILE)[None, :] * 128 + np.arange(128)[:, None])
    tok = np.stack([rows // 64, rows % 64], -1).astype(np.float32)
    tok = np.broadcast_to(tok[:, :, None, :], (128, NTILE, 16, 2)).reshape(128, -1)
    put("tokid", tok)
    return cst, rope32, rope128


def _prep_shared(inp):
    f = lambda a: np.ascontiguousarray(a, dtype=np.float32)
    sh = {}
    w_in = inp["w_in"]
    s = np.cumsum([0, 256, 256, 256, 256, 256, 128, 32, 256, 128, 128])
    rq, rk, rv, rg, cq, ckv, kr, wq, wk, wv = [w_in[:, :, s[i]:s[i + 1]] for i in range(10)]
    wk2 = np.concatenate([wk[:, :, 0:64], wk[:, :, 0:64], wk[:, :, 64:128], wk[:, :, 64:128]], -1)
    wv2 = np.concatenate([wv[:, :, 0:64], wv[:, :, 0:64], wv[:, :, 64:128], wv[:, :, 64:128]], -1)
    sh["w_in_r"] = f(np.concatenate([rq, rk, cq, ckv, wq, wk2, kr, rk, rv, rg, wv2], -1))
    assert sh["w_in_r"].shape[-1] == NCOL
    uq = inp["mla_w_uq"]
    sh["w_uq_n"] = f(uq[:, :, :, :64].reshape(L_, 256, 512))
    sh["w_uq_r"] = f(uq[:, :, :, 64:].reshape(L_, 256, 256))
    sh["w_uk"] = f(inp["mla_w_uk"].reshape(L_, 128, 512))
    sh["w_uv"] = f(inp["mla_w_uv"].reshape(L_, 128, 512))
    sh["w_out"] = f(inp["w_out"])
    sh["router_w"] = f(inp["router_w"])
    sh["ada_w"] = f(inp["ada_w"])
    sh["ada_b"] = f(inp["ada_b"].reshape(L_, 1, 6 * D))
    sh["exp_wg"] = f(inp["exp_w_gate"])
    sh["exp_wu"] = f(inp["exp_w_up"])
    sh["exp_wd"] = f(inp["exp_w_down"])
    gb = np.stack([inp["norm1_g"][0], inp["norm2_g"][0], inp["norm1_g"][1], inp["norm2_g"][1], inp["final_g"]])
    sh["g_bc"] = f(np.broadcast_to(gb[:, None, :], (5, 128, D)))
    cols = []
    for l in range(L_):
        qg = inp["mla_qnorm_g"][l].reshape(2, 128).T
        kg = inp["mla_kvnorm_g"][l].reshape(1, 128).T
        df, db, sk = inp["ret_decay_f"][l], inp["ret_decay_b"][l], inp["win_sink"][l]
        rep = np.broadcast_to(np.concatenate([df, db, sk])[None, :], (128, 12))
        hp = (np.arange(128) >= 64).astype(np.int64)
        pp = np.stack([df[0 + hp], df[2 + hp], db[0 + hp], db[2 + hp]], -1)
        cols += [qg, kg, rep, pp]
    sh["small"] = f(np.concatenate(cols, -1))
    cst, rope32, rope128 = _const_table()
    sh["cst"] = cst
    sh["rope32"] = rope32
    sh["rope128"] = rope128
    return sh


def _prep_core(inp, b):
    f = lambda a: np.ascontiguousarray(a, dtype=np.float32)
    d = {}
    d["x0"] = f(np.concatenate([inp["ctx"][b], inp["x"][b]], 0))
    cv = np.stack([inp["c_ctx"], inp["c"][b]])
    cr = cv.reshape(2, 8, 128).transpose(0, 2, 1)
    d["crep"] = f(np.broadcast_to(cr[:, :, :, None], (2, 128, 8, 128)))
    return d

class Rot:
    def __init__(self, tiles):
        self.t = tiles
        self.i = 0

    def get(self):
        t = self.t[self.i % len(self.t)]
        self.i += 1
        return t


def TD(t, pattern, **kw):
    return T(t.ap.rearrange(pattern, **kw), t.key)


def build(upto=None, dbg=False, nlayers=L_):
    nc = bass.Bass("TRN2", target_bir_lowering=False)
    es0 = ExitStack()
    with es0:
        k = KB(nc, es0)
        kind_dbg = "ExternalOutput" if dbg else "Internal"
        din = lambda n, s, dt=F32: k.dram(n, s, dt, kind="ExternalInput")
        x0 = din("x0", [NT, D])
        crep_d = din("crep", [2, 128, 8, 128])
        w_in_d = din("w_in_r", [L_, D, NCOL])
        w_uq_n_d = din("w_uq_n", [L_, 256, 512])
        w_uq_r_d = din("w_uq_r", [L_, 256, 256])
        w_uk_d = din("w_uk", [L_, 128, 512])
        w_uv_d = din("w_uv", [L_, 128, 512])
        w_out_d = din("w_out", [L_, D, D])
        router_d = din("router_w", [L_, D, 16])
        ada_w_d = din("ada_w", [L_, D, 6 * D])
        ada_b_d = din("ada_b", [L_, 1, 6 * D])
        wg_d = din("exp_wg", [L_, 16, D, 768])
        wu_d = din("exp_wu", [L_, 16, D, 768])
        wd_d = din("exp_wd", [L_, 16, 768, D])
        g_bc_d = din("g_bc", [5, 128, D])
        small_d = din("small", [128, L_ * 19])
        cst_d = din("cst", [128, NCST])
        rope32_d = din("rope32", [2, 128, NTX])
        rope128_d = din("rope128", [2, 128, NTX])
        out_d = k.dram("out", [NTX, D], F32, kind="ExternalOutput")
        X = k.dram("X", [NT, D], F32, kind=kind_dbg)
        MODBC = k.dram("MODBC", [2, 6, 128, D], F32, kind=kind_dbg)
        QT = k.dram("QT", [2, 128, NT], BF16, kind=kind_dbg)
        KT = k.dram("KT", [2, 128, NT], BF16, kind=kind_dbg)
        TMo = k.dram("TMo", [NT, 1024], BF16, kind=kind_dbg)
        QN = k.dram("QN", [4, 128, NT], BF16, kind=kind_dbg)
        KN = k.dram("KN", [4, 128, NT], BF16, kind=kind_dbg)
        QR = k.dram("QR", [8, 32, NT], BF16, kind=kind_dbg)
        KVT = k.dram("KVT", [128, NT], BF16, kind=kind_dbg)
        KRT = k.dram("KRT", [32, NT], BF16, kind=kind_dbg)
        VP = k.dram("VP", [NT, 512], BF16, kind=kind_dbg)
        WQT = k.dram("WQT", [2, 128, NT], BF16, kind=kind_dbg)
        WKT = k.dram("WKT", [2, 128, NT], BF16, kind=kind_dbg)
        YT = k.dram("YT", [8, 128, NT], BF16, kind=kind_dbg)
        H2 = k.dram("H2", [NT, D], BF16, kind=kind_dbg)
        AFFD = k.dram("AFFD", [16, NTX], F32)
        THRD = k.dram("THRD", [128, 1], F32)

        cst = k.sb([128, NCST], F32, "cst")
        k.dma(cst, cst_d)
        small = k.sb([128, L_ * 19], F32, "small")
        k.dma(small, small_d)

        def C(name, rows=128):
            o, w = _CST[name]
            return cst[0:rows, o:o + w]
        ident = C("ident")
        ones_f = k.sb([128, 128], F32, "ones_f")
        k.memset(ones_f, 1.0)
        ones_b = k.sb([128, 128], BF16, "ones_b")
        k.memset(ones_b, 1.0)
        ident_b = k.sb([128, 128], BF16, "ident_b")
        k.copy(ident_b, ident)
        triU_b = k.sb([128, 128], BF16, "triU_b")
        k.copy(triU_b, C("triU"))
        mprev_b = k.sb([128, 128], BF16, "mprev_b")
        k.copy(mprev_b, C("mprev"))
        mnext_b = k.sb([128, 128], BF16, "mnext_b")
        k.copy(mnext_b, C("mnext"))
        Rtab = k.sb([128, NTILE, 16, 4], BF16, "Rtab")
        o_tok, w_tok = _CST["tokid"]
        k.copy(Rtab[:, :, :, 0:2], T(cst.ap[:, o_tok:o_tok + w_tok].rearrange("p (n e c) -> p n e c", n=NTILE, e=16), cst.key))
        affT = k.sb([128, NTILE, 16], F32, "affT")
        posm = k.sb([128, NTILE, 16], F32, "posm")

        EPS = 1e-6
        blocks = [(0, 256, 0)] + [(256 + 512 * i, 512, 1) for i in range(8)]

        def rows_key(t, n):
            return t.sub(("r", n))

        def stop_here(name):
            return upto is not None and upto == name

        def rstd_from_ss(ss, n_feat, es, eps=EPS):
            r = k.sb([128, 1], F32, es=es)
            k.rsq(r, ss, 1.0 / n_feat, eps)
            return r

        for l in range(nlayers):
            need_ctx = l < L_ - 1
            sm0 = l * 19
            Xsrc = x0 if l == 0 else X
            with ExitStack() as s:
                crep = k.sb([128, 2, 8, 128], F32, es=s)
                for st in range(2):
                    k.dma(crep[:, st], crep_d[st])
                sil = k.sb([128, 2, 8, 128], F32, es=s)
                k.act(sil, crep, AF.Silu)
                wb = Rot([k.sb([128, 8, 512], F32, es=s) for _ in range(3)])
                br = Rot([k.sb([1, 512], F32, es=s) for _ in range(2)])
                pp = Rot([k.ps([128, 512], F32, es=s) for _ in range(4)])
                ob = Rot([k.sb([128, 512], F32, es=s) for _ in range(4)])
                aw = TD(ada_w_d[l], "(kc p) n -> p kc n", p=128)
                for nb in range(12):
                    w = wb.get()
                    for q4 in range(4):
                        k.dma(w.sl(q4, np.s_[:, q4 * 2:(q4 + 1) * 2, :]), aw[:, q4 * 2:(q4 + 1) * 2, nb * 512:(nb + 1) * 512])
                    b_ = br.get()
                    k.dma(b_, ada_b_d[l, :, nb * 512:(nb + 1) * 512])
                    for st in range(2):
                        ps = pp.get()
                        for kc in range(8):
                            k.mm(ps, sil[:, st, kc, :], w.sl(kc // 2, np.s_[:, kc, :]), start=(kc == 0), stop=False)
                        k.mm(ps, ones_f[0:1, :], b_, start=False, stop=True)
                        o = ob.get()
                        k.copy(o, ps, e=("act" if st == 0 else "dve"))
                        j, half = nb // 2, nb % 2
                        k.dma(MODBC.sl((st, j), np.s_[st, j, :, half * 512:(half + 1) * 512]), o)
            k.barrier()
            if stop_here("mod%d" % l):
                break

            def load_mod(st, j, es, eng_q="sp"):
                t = k.sb([128, D], F32, es=es)
                k.dma(t, MODBC.sl((st, j), np.s_[st, j]))
                return t

            def load_gs(st, j_scale, gidx, es):
                sc = load_mod(st, j_scale, es)
                gb = k.sb([128, D], F32, es=es)
                k.dma(gb, g_bc_d[gidx])
                k.stt(sc, sc, 1.0, gb, ALU.add, ALU.mult)
                return sc

            def load_cast(dst, src, stg, e="pool"):
                t = stg.get()
                sh = list(src.ap.shape)
                tv = t[0:sh[0], 0:sh[1]]
                k.dma(tv, src)
                k.copy(dst, tv, e=e)

            with ExitStack() as s:
                w_in = k.sb([128, 8, NCOL], BF16, es=s)
                stg = Rot([k.sb([128, NCOL], F32, es=s) for _ in range(2)])
                for kc in range(8):
                    load_cast(w_in[:, kc, :], w_in_d[l, kc * 128:(kc + 1) * 128, :], stg, e=("pool" if kc % 2 else "dve"))
                w_uq_n = k.sb([128, 2, 512], BF16, es=s)
                w_uq_r = k.sb([128, 2, 256], BF16, es=s)
                for kc in range(2):
                    load_cast(w_uq_n[:, kc, :], w_uq_n_d[l, kc * 128:(kc + 1) * 128, :], stg)
                    load_cast(w_uq_r[:, kc, :], w_uq_r_d[l, kc * 128:(kc + 1) * 128, :], stg)
                w_uk = k.sb([128, 512], BF16, es=s)
                load_cast(w_uk, w_uk_d[l], stg)
                w_uv = k.sb([128, 512], BF16, es=s)
                load_cast(w_uv, w_uv_d[l], stg)
                gs1 = [load_gs(st, 1, l * 2 + 0, s) for st in range(2)]
                sh1 = [load_mod(st, 0, s) for st in range(2)]
                qg = small[:, sm0 + 0:sm0 + 2]
                kg = small[:, sm0 + 2:sm0 + 3]
                xt_r = Rot([k.sb([128, D], F32, es=s) for _ in range(3)])
                xs_r = Rot([k.sb([128, D], F32, es=s) for _ in range(3)])
                junk = k.sb([128, D], F32, es=s)
                ss_pool = Rot([k.sb([128, 1], F32, es=s) for _ in range(8)])
                hT_r = Rot([k.sb([128, 8, 512], BF16, es=s) for _ in range(2)])
                ptr = Rot([k.ps([128, 8, 128], BF16, es=s) for _ in range(2)])
                xb_r = Rot([k.sb([128, D], BF16, es=s) for _ in range(5)])
                pfm = Rot([k.ps([128, 512], F32, es=s) for _ in range(3)])
                pex = Rot([k.ps([128, 512], F32, es=s) for _ in range(3)])
                ob16 = Rot([k.sb([128, 512], BF16, es=s) for _ in range(6)])
                f32t = Rot([k.sb([128, 512], F32, es=s) for _ in range(6)])
                rp32 = Rot([k.sb([128, 2, 512], F32, es=s) for _ in range(2)])
                rp128 = Rot([k.sb([128, 2, 512], F32, es=s) for _ in range(2)])
                cqg = k.sb([128, 2, 512], BF16, es=s)
                rstdq = k.sb([128, 512], F32, es=s)
                kvT_sb = k.sb([128, 512], BF16, es=s)
                tmo = Rot([k.sb([128, 1024], BF16, es=s) for _ in range(2)])

                def bc_rstd(dst, sq_list, nfeat, n):
                    ps = pex.get()
                    for i, sq in enumerate(sq_list):
                        k.mm(ps[:, :n], ones_f, sq, start=(i == 0), stop=(i == len(sq_list) - 1))
                    k.rsq(dst[:, :n], ps[:, :n], 1.0 / nfeat, EPS)

                def rope_apply(src_f32, M, n, tabs, perm, out_bf):
                    ps = pex.get()
                    k.mm(ps[0:M, :n], perm, src_f32[0:M, :n])
                    t1 = f32t.get()
                    k.tt(t1[0:M, :n], src_f32[0:M, :n], tabs[0:M, 0, :n], ALU.mult)
                    t2 = f32t.get()
                    k.tt(t2[0:M, :n], ps[0:M, :n], tabs[0:M, 1, :n], ALU.mult)
                    k.tt(out_bf[0:M, :n], t1[0:M, :n], t2[0:M, :n], ALU.add, e="pool")

                class NormState:
                    pass

                def norm_begin(blk):
                    (r0, n, st) = blk
                    ns = NormState()
                    ns.blk = blk
                    ns.hT = hT_r.get()
                    ns.r32 = ns.r128 = None
                    ns.xbs = []
                    if st == 1:
                        t0 = r0 - NTC
                        ns.r32 = rp32.get()
                        k.dma(ns.r32[:, :, :n], TD(rope32_d, "a p t -> p a t")[:, :, t0:t0 + n])
                        ns.r128 = rp128.get()
                        k.dma(ns.r128[:, :, :n], TD(rope128_d, "a p t -> p a t")[:, :, t0:t0 + n])
                    return ns

                def norm_ew(ns, ti):
                    (r0, n, st) = ns.blk
                    if ti >= n // 128:
                        return
                    rr = r0 + ti * 128
                    xt = xt_r.get()
                    k.dma(xt, rows_key(Xsrc, rr // 128)[rr:rr + 128, :])
                    ss = ss_pool.get()
                    k.act(junk, xt, AF.Square, accum=ss)
                    rstd = ss_pool.get()
                    k.rsq(rstd, ss, 1.0 / D, EPS)
                    xs = xs_r.get()
                    k.stt(xs, xt, rstd, gs1[st], ALU.mult, ALU.mult)
                    xb = xb_r.get()
                    k.tt(xb, xs, sh1[st], ALU.add)
                    ns.xbs.append(xb)

                def norm_tr(ns):
                    (r0, n, st) = ns.blk
                    for ti, xb in enumerate(ns.xbs):
                        pt = ptr.get()
                        for kc in range(8):
                            k.tr(pt[:, kc, :], xb[:, kc * 128:(kc + 1) * 128], ident_b)
                        k.copy(ns.hT[:, :, ti * 128:(ti + 1) * 128], pt, e="act")
                    return (ns.hT, ns.r32, ns.r128)

                def proj_phase(blk, ctx_, hook):
                    (r0, n, st) = blk
                    nt = n // 128
                    hT, r32, r128 = ctx_
                    def fm(c, M=128):
                        ps = pfm.get()
                        for kc in range(8):
                            k.mm(ps[0:M, :n], w_in[:, kc, c * 128:c * 128 + M], hT[:, kc, :n], start=(kc == 0), stop=(kc == 7))
                        return ps
                    for c in range(4):
                        ps = fm(c)
                        o = ob16.get()
                        k.copy(o[:, :n], ps[:, :n], e=("act" if c % 2 == 0 else "dve"))
                        dst = (QT if c < 2 else KT)
                        k.dma(dst.sl((c % 2, r0), np.s_[c % 2, :, r0:r0 + n]), o[:, :n])
                    hook(0)
                    sqs = []
                    for c2 in range(2):
                        ps = fm(4 + c2)
                        sq = f32t.get()
                        k.act(sq[:, :n], ps[:, :n], AF.Square)
                        sqs.append(sq[:, :n])
                        k.act(cqg[:, c2, :n], ps[:, :n], AF.Copy, scale=qg[:, c2:c2 + 1])
                    bc_rstd(rstdq, sqs, 256, n)
                    for pr in range(4):
                        ps = pex.get()
                        for c2 in range(2):
                            k.mm(ps[:, :n], w_uq_n[:, c2, pr * 128:(pr + 1) * 128], cqg[:, c2, :n], start=(c2 == 0), stop=(c2 == 1))
                        qn = ob16.get()
                        k.tt(qn[:, :n], ps[:, :n], rstdq[:, :n], ALU.mult)
                        k.dma(QN.sl((pr, r0), np.s_[pr, :, r0:r0 + n]), qn[:, :n])
                    for g4 in range(2):
                        ps = pex.get()
                        for c2 in range(2):
                            k.mm(ps[:, :n], w_uq_r[:, c2, g4 * 128:(g4 + 1) * 128], cqg[:, c2, :n], start=(c2 == 0), stop=(c2 == 1))
                        qr = f32t.get()
                        k.tt(qr[:, :n], ps[:, :n], rstdq[:, :n], ALU.mult)
                        o = ob16.get()
                        if st == 1:
                            rope_apply(qr, 128, n, r32, C("p32x4"), o)
                        else:
                            k.copy(o[:, :n], qr[:, :n], e="pool")
                        for hh in range(4):
                            h = g4 * 4 + hh
                            k.dma(QR.sl((h, r0), np.s_[h, :, r0:r0 + n]), o[hh * 32:(hh + 1) * 32, :n])
                    hook(1)
                    ps = fm(6)
                    sq = f32t.get()
                    k.act(sq[:, :n], ps[:, :n], AF.Square)
                    kvg = f32t.get()
                    k.act(kvg[:, :n], ps[:, :n], AF.Copy, scale=kg[:, 0:1])
                    rk_ = f32t.get()
                    bc_rstd(rk_, [sq[:, :n]], 128, n)
                    k.tt(kvT_sb[:, :n], kvg[:, :n], rk_[:, :n], ALU.mult)
                    k.dma(KVT.sl(r0, np.s_[:, r0:r0 + n]), kvT_sb[:, :n])
                    for pr in range(4):
                        ps = pex.get()
                        k.mm(ps[:, :n], w_uk[:, pr * 128:(pr + 1) * 128], kvT_sb[:, :n])
                        o = ob16.get()
                        k.copy(o[:, :n], ps[:, :n], e=("act" if pr % 2 == 0 else "dve"))
                        k.dma(KN.sl((pr, r0), np.s_[pr, :, r0:r0 + n]), o[:, :n])
                    for ti in range(nt):
                        ps = pex.get()
                        k.mm(ps, kvT_sb[:, ti * 128:(ti + 1) * 128], w_uv)
                        o = ob16.get()
                        k.copy(o, ps, e="act")
                        rr = r0 + ti * 128
                        k.dma(VP.sl(rr // 128, np.s_[rr:rr + 128, :]), o)
                    hook(2)
                    for c in range(4):
                        ps = fm(7 + c)
                        o = ob16.get()
                        if st == 1:
                            sf = f32t.get()
                            k.copy(sf[:, :n], ps[:, :n], e="act")
                            rope_apply(sf, 128, n, r128, C("p128"), o)
                        else:
                            k.copy(o[:, :n], ps[:, :n], e="act")
                        dst = (WQT if c < 2 else WKT)
                        k.dma(dst.sl((c % 2, r0), np.s_[c % 2, :, r0:r0 + n]), o[:, :n])
                    ps = fm(11, 32)
                    o = ob16.get()
                    if st == 1:
                        sf = f32t.get()
                        k.copy(sf[0:32, :n], ps[0:32, :n], e="act")
                        rope_apply(sf, 32, n, r32, C("p32", 32), o)
                    else:
                        k.copy(o[0:32, :n], ps[0:32, :n], e="act")
                    k.dma(KRT.sl(r0, np.s_[:, r0:r0 + n]), o[0:32, :n])
                    hook(3)
                    for ti in range(nt):
                        o = tmo.get()
                        for hf in range(2):
                            ps = pfm.get()
                            c0 = 1440 + hf * 512
                            for kc in range(8):
                                k.mm(ps, hT[:, kc, ti * 128:(ti + 1) * 128], w_in[:, kc, c0:c0 + 512], start=(kc == 0), stop=(kc == 7))
                            k.copy(o[:, hf * 512:(hf + 1) * 512], ps, e=("act" if hf == 0 else "dve"))
                        rr = r0 + ti * 128
                        k.dma(TMo.sl(rr // 128, np.s_[rr:rr + 128, :]), o)
                ns0 = norm_begin(blocks[0])
                for ti_ in range(4):
                    norm_ew(ns0, ti_)
                pctx = norm_tr(ns0)
                for bi, blk in enumerate(blocks):
                    nsn = norm_begin(blocks[bi + 1]) if bi + 1 < len(blocks) else None
                    proj_phase(blk, pctx, (lambda i_: norm_ew(nsn, i_)) if nsn is not None else (lambda i_: None))
                    pctx = norm_tr(nsn) if nsn is not None else None
            k.barrier()
            if stop_here("s1_%d" % l):
                break
            es_mla_pre = ExitStack()
            vp2 = k.sb([128, NTILE, 8, 128], BF16, es=es_mla_pre)
            VPv = TD(VP, "(t p) (h d) -> p t h d", p=128, d=64)
            for par in range(2):
                k.memset(vp2[:, :, par::2, (1 - par) * 64:(2 - par) * 64], 1.0, e="pool")
                for h in range(par, 8, 2):
                    k.dma(vp2[:, :, h, par * 64:(par + 1) * 64], VPv[:, :, h, :])
            with ExitStack() as s:
                lg_rep = k.sb([128, 8], F32, es=s)
                lg_pp = k.sb([128, 4], F32, es=s)
                for dst, src in ((lg_rep, small[:, sm0 + 3:sm0 + 11]), (lg_pp, small[:, sm0 + 15:sm0 + 19])):
                    k.act(dst, src, AF.Exp, scale=LN2)
                    k.ts(dst, dst, -1.0, ALU.mult, 1.0, ALU.add)
                    k.act(dst, dst, AF.Ln)
                LN8 = float(np.log(0.125))
                DT = k.sb([128, 4, 128], F32, es=s)
                tmpA = k.sb([128, 128], F32, es=s)
                for h in range(4):
                    k.ts(tmpA, C("A"), lg_rep[:, h:h + 1], ALU.mult)
                    k.stt(tmpA, C("B"), lg_rep[:, 4 + h:5 + h], tmpA, ALU.mult, ALU.add)
                    k.act(DT[:, h, :], tmpA, AF.Exp)
                    k.ts(DT[:, h, :], DT[:, h, :], 0.125, ALU.mult)
                QWF = k.sb([128, 2, 128], F32, es=s)
                QWB = k.sb([128, 2, 128], F32, es=s)
                gch = k.sb([128, 4], F32, es=s)
                for pr in range(2):
                    k.act(QWF[:, pr, :], C("C1"), AF.Exp, scale=lg_pp[:, pr:pr + 1])
                    k.act(QWB[:, pr, :], C("C2"), AF.Exp, scale=lg_pp[:, 2 + pr:3 + pr])
                k.act(gch, lg_pp, AF.Exp, scale=128.0)
                KWF = k.sb([128, 256], F32, es=s)
                KWB = k.sb([128, 256], F32, es=s)
                kcol = k.sb([128, 8], F32, es=s)
                for h in range(4):
                    k.act(kcol[:, h:h + 1], C("colK1"), AF.Exp, scale=lg_rep[:, h:h + 1])
                    k.act(kcol[:, 4 + h:5 + h], C("colK2"), AF.Exp, scale=lg_rep[:, 4 + h:5 + h])
                k.ts(kcol, kcol, 0.125, ALU.mult)
                for h in range(4):
                    k.copy(KWF[:, h * 64:(h + 1) * 64], T(kcol.ap[:, h:h + 1].to_broadcast([128, 64]), kcol.key))
                    k.copy(KWB[:, h * 64:(h + 1) * 64], T(kcol.ap[:, 4 + h:5 + h].to_broadcast([128, 64]), kcol.key))
                KVd = k.sb([128, NTILE, 4, 64], F32, es=s)
                Sfb = k.sb([128, NTILE, 2, 64], BF16, es=s)
                Sbb = k.sb([128, NTILE, 2, 64], BF16, es=s)
                kv_r = Rot([k.sb([128, 512], BF16, es=s) for _ in range(3)])
                kw_r = Rot([k.sb([128, 2, 256], BF16, es=s) for _ in range(2)])
                pkv = Rot([k.ps([128, 4, 128], F32, es=s) for _ in range(2)])
                fwd_order = list(range(NTILE))
                bwd_order = [1, 0] + list(range(NTILE - 1, 1, -1))
                r1_order = []
                for a_, b_ in zip(fwd_order, bwd_order):
                    for t_ in (a_, b_):
                        if t_ not in r1_order:
                            r1_order.append(t_)
                curs = []
                for d_ in range(2):
                    c_ = k.sb([128, 2, 64], F32, es=s)
                    k.memset(c_, 0.0)
                    curs.append(c_)
                ptrs = [0, 0]
                done = set()

                def scan_step(d_, n):
                    Sb_ = Sfb if d_ == 0 else Sbb
                    cur = curs[d_]
                    k.copy(Sb_.sl(n, np.s_[:, n, :, :]), cur)
                    for pr in range(2):
                        k.stt(cur[:, pr, :], cur[:, pr, :], gch[:, d_ * 2 + pr:d_ * 2 + pr + 1],
                              KVd.sl(n, np.s_[:, n, d_ * 2 + pr, :]), ALU.mult, ALU.add)
                for n in r1_order:
                    kvt = kv_r.get()
                    k.dma(kvt, TMo.sl(n, np.s_[n * 128:(n + 1) * 128, 0:512]))
                    kw = kw_r.get()
                    k.tt(kw[:, 0, :], kvt[:, 0:256], KWF, ALU.mult)
                    k.tt(kw[:, 1, :], kvt[:, 0:256], KWB, ALU.mult)
                    ps = pkv.get()
                    for d_ in range(2):
                        for pr in range(2):
                            k.mm(ps[:, d_ * 2 + pr, :], kw[:, d_, pr * 128:(pr + 1) * 128], kvt[:, 256 + pr * 128:256 + (pr + 1) * 128])
                    k.copy(KVd.sl(n, np.s_[0:64, n, :, :]), ps[0:64, :, 0:64], e="act")
                    k.copy(KVd.sl(n, np.s_[64:128, n, :, :]), ps[64:128, :, 64:128], e="act")
                    done.add(n)
                    for d_, order in ((0, fwd_order), (1, bwd_order)):
                        while ptrs[d_] < NTILE and order[ptrs[d_]] in done:
                            scan_step(d_, order[ptrs[d_]])
                            ptrs[d_] += 1
                qk_r = Rot([k.sb([128, 2, 2, 128], BF16, es=s) for _ in range(3)])
                vg_r = Rot([k.sb([128, 512], BF16, es=s) for _ in range(3)])
                qw_r = Rot([k.sb([128, 2, 2, 128], BF16, es=s) for _ in range(2)])
                sm_r = Rot([k.sb([128, 512], BF16, es=s) for _ in range(4)])
                pss = Rot([k.ps([128, 512], F32, es=s) for _ in range(2)] + [T(p_.ap.rearrange("p a b -> p (a b)"), p_.key) for p_ in pkv.t])
                psy = Rot([k.ps([128, 512], F32, es=s) for _ in range(2)])
                ysb_r = Rot([k.sb([128, 256], F32, es=s) for _ in range(3)])
                sq_r = Rot([k.sb([128, 256], F32, es=s) for _ in range(2)])
                st_r = Rot([k.sb([128, 16], F32, es=s) for _ in range(2)])
                yo_r = Rot([k.sb([128, 2, 128], BF16, es=s) for _ in range(2)])
                yb_r = Rot([k.sb([128, 256], BF16, es=s) for _ in range(2)])
                ptb2 = Rot([k.ps([128, 2, 128], BF16, es=s) for _ in range(2)])

                sgall = k.sb([128, NTILE, 256], BF16, es=s)
                for n in range(0 if need_ctx else 2, NTILE):
                    gt_ = vg_r.get()
                    k.dma(gt_[:, 0:256], TMo.sl(n, np.s_[n * 128:(n + 1) * 128, 512:768]))
                    k.act(sgall[:, n, :], gt_[:, 0:256], AF.Silu)

                def r2A(n):
                    c0 = n * 128
                    qk = qk_r.get()
                    k.dma(qk[:, 0], TD(QT, "c p t -> p c t")[:, :, c0:c0 + 128])
                    k.dma(qk[:, 1], TD(KT, "c p t -> p c t")[:, :, c0:c0 + 128])
                    vg = vg_r.get()
                    k.dma(vg, TMo.sl(n, np.s_[c0:c0 + 128, 256:768]))
                    qw = qw_r.get()
                    k.tt(qw[:, 0], qk[:, 0], QWF, ALU.mult)
                    k.tt(qw[:, 1], qk[:, 0], QWB, ALU.mult)
                    py = psy.get()
                    psA = pss.get()
                    psB = pss.get()
                    for h in range(4):
                        pr, off = h // 2, (h % 2) * 64
                        pb = psA if h % 2 == 0 else psB
                        k.mm(pb[:, pr * 128:(pr + 1) * 128], qk[off:off + 64, 1, pr, :], qk[off:off + 64, 0, pr, :])
                    sm = sm_r.get()
                    smv = T(sm.ap.rearrange("p (a c) -> p a c", a=4), sm.key)
                    k.tt(smv[:, 0::2, :], T(psA.ap[:, 0:256].rearrange("p (a c) -> p a c", a=2), psA.key), DT[:, 0::2, :], ALU.mult)
                    k.tt(smv[:, 1::2, :], T(psB.ap[:, 0:256].rearrange("p (a c) -> p a c", a=2), psB.key), DT[:, 1::2, :], ALU.mult)
                    for h in range(4):
                        pr, off = h // 2, (h % 2) * 64
                        yo_ = py[:, h * 64:(h + 1) * 64]
                        k.mm(yo_, sm[:, h * 128:(h + 1) * 128], vg[:, h * 64:(h + 1) * 64], start=True, stop=False)
                        k.mm(yo_, qw[off:off + 64, 0, pr, :], Sfb.sl(n, np.s_[off:off + 64, n, pr, :]), start=False, stop=False)
                        k.mm(yo_, qw[off:off + 64, 1, pr, :], Sbb.sl(n, np.s_[off:off + 64, n, pr, :]), start=False, stop=True)
                    ysb = ysb_r.get()
                    k.copy(ysb, py[:, 0:256], e="act")
                    return (ysb, sgall[:, n, :])

                def r2B(n, ctx_):
                    ysb, sg = ctx_
                    c0 = n * 128
                    stt_ = st_r.get()
                    k.op("dve", lambda e: e.reduce_sum(stt_.ap[:, 0:4], ysb.ap.rearrange("p (h d) -> p h d", h=4), AX.X), reads=[ysb], writes=[stt_])
                    sq = sq_r.get()
                    k.tt(sq, ysb, ysb, ALU.mult)
                    k.op("dve", lambda e: e.reduce_sum(stt_.ap[:, 4:8], sq.ap.rearrange("p (h d) -> p h d", h=4), AX.X), reads=[sq], writes=[stt_])
                    k.ts(stt_[:, 0:8], stt_[:, 0:8], 1.0 / 64, ALU.mult)
                    k.tt(stt_[:, 8:12], stt_[:, 0:4], stt_[:, 0:4], ALU.mult)
                    k.tt(stt_[:, 12:16], stt_[:, 4:8], stt_[:, 8:12], ALU.subtract)
                    k.rsq(stt_[:, 12:16], stt_[:, 12:16], 1.0, 1e-5)
                    for h in range(4):
                        k.ts(ysb[:, h * 64:(h + 1) * 64], ysb[:, h * 64:(h + 1) * 64], stt_[:, h:h + 1], ALU.subtract,
                             stt_[:, 12 + h:13 + h], ALU.mult)
                    yb = yb_r.get()
                    k.tt(yb, ysb, sg, ALU.mult)
                    pt = ptb2.get()
                    for c in range(2):
                        k.tr(pt[:, c, :], yb[:, c * 128:(c + 1) * 128], ident_b)
                    yo = yo_r.get()
                    k.copy(yo, pt, e="act")
                    k.dma(TD(YT, "c p t -> p c t").sl(("ret", n), np.s_[:, 0:2, c0:c0 + 128]), yo)
                tiles_r = list(range(0 if need_ctx else 2, NTILE))
                pc_ = r2A(tiles_r[0])
                for ti_, n in enumerate(tiles_r):
                    nc_ = r2A(tiles_r[ti_ + 1]) if ti_ + 1 < len(tiles_r) else None
                    r2B(n, pc_)
                    pc_ = nc_
            k.barrier()
            if stop_here("ret%d" % l):
                es_mla_pre.close()
                break
            with ExitStack() as s:
                kh_l = [k.sb([128, NT], BF16, es=s) for _ in range(2)]
                for t_ in kh_l:
                    k.memset(t_, 0.0, e="pool")
                kh_r = Rot(kh_l)
                q_l = [k.sb([128, 512], BF16, es=s) for _ in range(3)]
                for t_ in q_l:
                    k.memset(t_, 0.0)
                q_r = Rot(q_l)
                rd_sets = []
                for par in range(2):
                    tl = [k.sb([128, 512], F32, es=s) for _ in range(2)]
                    for t_ in tl:
                        k.memset(t_, 0.0)
                    rd_sets.append(Rot(tl))
                pT_r = Rot([k.sb([128, 2, 512], BF16, es=s) for _ in range(4)])
                pss = Rot([k.ps([128, 2, 512], F32, es=s) for _ in range(2)])
                pacc = Rot([k.ps([128, 512], F32, es=s) for _ in range(4)])
                sw_r = Rot([k.sb([128, 512], F32, es=s) for _ in range(2)])
                yp_r = Rot([k.sb([128, 512], BF16, es=s) for _ in range(3)])
                qblocks = ([(0, 256, [0, 1])] if need_ctx else []) + [(256 + 512 * i, 512, list(range(NTILE))) for i in range(8)]
                LOOK = 1
                pend = []

                def emit_pv2(item):
                    h, r0, n, j, npair, kt2, pT, pv = item
                    for u_ in range(2):
                        first = (j == 0 and u_ == 0)
                        last = (j == npair - 1 and u_ == 1)
                        k.mm(pv[:, :n], vp2[:, kt2[u_], h, :], pT[:, u_, :n], start=first, stop=last)
                    if j == npair - 1:
                        off = (h % 2) * 64
                        dof = 64 - off
                        rd = rd_sets[h % 2].get()
                        k.op("dve", lambda e: e.reciprocal(rd.ap[dof:dof + 64, :n], pv.ap[dof:dof + 64, :n]), reads=[pv], writes=[rd])
                        sws = sw_r.get()
                        k.dma(sws[off:off + 64, :n], rd[dof:dof + 64, :n])
                        yp = yp_r.get()
                        k.tt(yp[off:off + 64, :n], pv[off:off + 64, :n], sws[off:off + 64, :n], ALU.mult)
                        k.dma(YT.sl(("mla", h, r0), np.s_[2 + h // 2, off:off + 64, r0:r0 + n]), yp[off:off + 64, :n])
                for h in range(8):
                    off = (h % 2) * 64
                    kh = kh_r.get()
                    k.dma(kh[0:64, :], KN[h // 2, off:off + 64, :])
                    k.dma(kh[64:96, :], KRT)
                    for (r0, n, kts) in qblocks:
                        q = q_r.get()
                        k.dma(q[0:64, :n], QN[h // 2, off:off + 64, r0:r0 + n])
                        k.dma(q[64:96, :n], QR[h, :, r0:r0 + n])
                        pv = pacc.get()
                        npair = len(kts) // 2
                        for j in range(npair):
                            kt2 = (kts[2 * j], kts[2 * j + 1])
                            ps = pss.get()
                            for u_ in range(2):
                                k.mm(ps[:, u_, :n], kh[:, kt2[u_] * 128:(kt2[u_] + 1) * 128], q[:, :n])
                            pT = pT_r.get()
                            k.act(pT[:, :, :n], ps[:, :, :n], AF.Exp, scale=SCALE_MLA)
                            pend.append((h, r0, n, j, npair, kt2, pT, pv))
                            if len(pend) > LOOK:
                                emit_pv2(pend.pop(0))
                while pend:
                    emit_pv2(pend.pop(0))
            k.barrier()
            es_mla_pre.close()
            if stop_here("mla%d" % l):
                break
            with ExitStack() as s:
                wkT = k.sb([128, 2, 2, NT], BF16, es=s)
                k.memset(wkT, 0.0, e="pool")
                for hk_ in range(2):
                    for g_ in range(2):
                        k.dma(wkT[g_ * 64:(g_ + 1) * 64, hk_, g_, :], WKT[hk_, g_ * 64:(g_ + 1) * 64, :])
                wqT = k.sb([128, 2, NT], BF16, es=s)
                k.dma(wqT, TD(WQT, "c p t -> p c t"))
                wv3 = k.sb([128, NTILE, 4, 128], BF16, es=s)
                WVv = T(TMo.ap[:, 768:1024].rearrange("(t p) (q d) -> p t q d", p=128, d=64), TMo.key)
                for par in range(2):
                    k.memset(wv3[:, :, par::2, (1 - par) * 64:(2 - par) * 64], 1.0, e=("dve" if par == 0 else "pool"))
                    for q_ in range(par, 4, 2):
                        k.dma(wv3[:, :, q_, par * 64:(par + 1) * 64], WVv[:, :, q_, :])
                esink = k.sb([128, 4], F32, es=s)
                k.act(esink, small[:, sm0 + 11:sm0 + 15], AF.Exp)
                rd_sets = []
                for par in range(2):
                    tl = [k.sb([128, 128], F32, es=s) for _ in range(2)]
                    for t_ in tl:
                        k.memset(t_, 0.0)
                    rd_sets.append(Rot(tl))
                pT_r = Rot([k.sb([128, 128], BF16, es=s) for _ in range(6)])
                pss = Rot([k.ps([128, 512], F32, es=s) for _ in range(4)])
                pacc = Rot([k.ps([128, 512], F32, es=s) for _ in range(3)])
                psw = Rot([k.ps([128, 512], F32, es=s) for _ in range(1)])
                sw_r = Rot([k.sb([128, 128], F32, es=s) for _ in range(2)])
                yp_r = Rot([k.sb([128, 128], BF16, es=s) for _ in range(3)])
                pend = []

                def emit_pvw(item):
                    n, qh, i, nk, kt, pT, pv, yp = item
                    hk, g = qh // 2, qh % 2
                    off = g * 64
                    dof = 64 - off
                    c0 = n * 128
                    last = (i == nk - 1)
                    k.mm(pv[:, 0:128], wv3[:, kt, qh, :], pT, start=(i == 0), stop=last)
                    if last:
                        rd = rd_sets[g].get()
                        k.ts(rd[dof:dof + 64, :], pv[dof:dof + 64, 0:128], esink[dof:dof + 64, qh:qh + 1], ALU.add)
                        k.op("dve", lambda e: e.reciprocal(rd.ap[dof:dof + 64, :], rd.ap[dof:dof + 64, :]), reads=[rd], writes=[rd])
                        sw = psw.get()
                        k.mm(sw[:, 0:128], C("pswap"), rd)
                        sws = sw_r.get()
                        k.copy(sws[off:off + 64, :], sw[off:off + 64, 0:128], e="act")
                        k.tt(yp[off:off + 64, :], pv[off:off + 64, 0:128], sws[off:off + 64, :], ALU.mult)
                        if g == 1:
                            k.dma(YT.sl(("win", hk, n), np.s_[6 + hk, :, c0:c0 + 128]), yp)
                yp = None
                for n in range(0 if need_ctx else 2, NTILE):
                    c0 = n * 128
                    if n < 2:
                        keys = [(0, None), (1, None)]
                    else:
                        keys = []
                        if n - 1 >= 2:
                            keys.append((n - 1, mprev_b))
                        keys.append((n, None))
                        if n + 1 < NTILE:
                            keys.append((n + 1, mnext_b))
                        keys += [(0, None), (1, None)]
                    for qh in range(4):
                        hk, g = qh // 2, qh % 2
                        off = g * 64
                        pv = pacc.get()
                        if g == 0:
                            yp = yp_r.get()
                        for i, (kt, msk) in enumerate(keys):
                            ps = pss.get()
                            k.mm(ps[:, 0:128], wkT[:, hk, g, kt * 128:(kt + 1) * 128], wqT[:, hk, c0:c0 + 128])
                            pT = pT_r.get()
                            k.act(pT, ps[:, 0:128], AF.Exp, scale=0.125)
                            if msk is not None:
                                k.tt(pT, pT, msk, ALU.mult)
                            pend.append((n, qh, i, len(keys), kt, pT, pv, yp))
                            if len(pend) > 3:
                                emit_pvw(pend.pop(0))
                while pend:
                    emit_pvw(pend.pop(0))
            k.barrier()
            if stop_here("win%d" % l):
                break
            es_aff = ExitStack()
            aff_e = k.sb([16, NT], F32, "aff_e", es=es_aff)
            with ExitStack() as s:
                w_out = k.sb([128, 8, D], BF16, es=s)
                stg = Rot([k.sb([128, D], F32, es=s) for _ in range(2)])
                for kc in range(8):
                    load_cast(w_out[:, kc, :], w_out_d[l, kc * 128:(kc + 1) * 128, :], stg, e=("pool" if kc % 2 else "dve"))
                rw = k.sb([128, 8, 16], F32, es=s)
                k.dma(rw, TD(router_d[l], "(kc p) e -> p kc e", p=128))
                sts = [0, 1] if need_ctx else [1]
                mod2 = {st: load_mod(st, 2, s) for st in sts}
                gs2 = {st: load_gs(st, 4, l * 2 + 1, s) for st in sts}
                sh2 = {st: load_mod(st, 3, s) for st in sts}
                yT_r = Rot([k.sb([128, 8, 128], BF16, es=s) for _ in range(2)])
                xt_r = Rot([k.sb([128, D], F32, es=s) for _ in range(2)])
                xn_r = Rot([k.sb([128, D], F32, es=s) for _ in range(2)])
                xs_r = Rot([k.sb([128, D], F32, es=s) for _ in range(3)])
                h2b_r = Rot([k.sb([128, D], BF16, es=s) for _ in range(2)])
                h2T_r = Rot([k.sb([128, 8, 128], F32, es=s) for _ in range(2)])
                junk = k.sb([128, D], F32, es=s)
                ss_pool = Rot([k.sb([128, 1], F32, es=s) for _ in range(8)])
                ex_r = Rot([k.sb([128, 16], F32, es=s) for _ in range(2)])
                pso = Rot([k.ps([128, 512], F32, es=s) for _ in range(3)])
                ptr = Rot([k.ps([128, 4, 128], F32, es=s) for _ in range(2)])
                psl = Rot([k.ps([128, 512], F32, es=s) for _ in range(2)])
                def phaseA(n):
                    st = 0 if n < 2 else 1
                    c0 = n * 128
                    yT = yT_r.get()
                    k.dma(yT, TD(YT, "c p t -> p c t")[:, :, c0:c0 + 128])
                    xt = xt_r.get()
                    k.dma(xt, rows_key(Xsrc, n)[c0:c0 + 128, :])
                    xn = xn_r.get()
                    for hf in range(2):
                        ps = pso.get()
                        for kc in range(8):
                            k.mm(ps, yT[:, kc, :], w_out[:, kc, hf * 512:(hf + 1) * 512], start=(kc == 0), stop=(kc == 7))
                        sl_ = np.s_[:, hf * 512:(hf + 1) * 512]
                        k.tt(xn[sl_], ps, mod2[st][sl_], ALU.mult)
                        k.tt(xn[sl_], xn[sl_], xt[sl_], ALU.add)
                    k.dma(rows_key(X, n)[c0:c0 + 128, :], xn)
                    ss = ss_pool.get()
                    k.act(junk, xn, AF.Square, accum=ss)
                    rstd = ss_pool.get()
                    k.rsq(rstd, ss, 1.0 / D, EPS)
                    xs = xs_r.get()
                    k.stt(xs, xn, rstd, gs2[st], ALU.mult, ALU.mult)
                    k.tt(xs, xs, sh2[st], ALU.add)
                    h2b = h2b_r.get()
                    k.copy(h2b, xs, e="act")
                    k.dma(H2.sl(n, np.s_[c0:c0 + 128, :]), h2b)
                    return xs

                def phaseB(n, xs):
                    c0 = n * 128
                    h2T = h2T_r.get()
                    for hf in range(2):
                        pt = ptr.get()
                        for q4 in range(4):
                            kc = hf * 4 + q4
                            k.tr(pt[:, q4, :], xs[:, kc * 128:(kc + 1) * 128], ident)
                        k.copy(h2T[:, hf * 4:(hf + 1) * 4, :], pt, e="act")
                    pl = psl.get()
                    for kc in range(8):
                        k.mm(pl[:, 0:16], h2T[:, kc, :], rw[:, kc, :], start=(kc == 0), stop=(kc == 7))
                    ex = ex_r.get()
                    sm_ = ss_pool.get()
                    k.act(ex, pl[:, 0:16], AF.Exp, accum=sm_)
                    k.op("dve", lambda e: e.reciprocal(sm_.ap, sm_.ap), reads=[sm_], writes=[sm_])
                    k.ts(affT.sl(n, np.s_[:, n, :]), ex, sm_, ALU.mult)
                    pl2 = psl.get()
                    k.tr(pl2[0:16, 0:128], affT.sl(n, np.s_[:, n, :]), ident)
                    k.copy(aff_e.sl(n, np.s_[:, c0:c0 + 128]), pl2[0:16, 0:128], e="act")
                tiles_o = list(range(0 if need_ctx else 2, NTILE))
                pxs = phaseA(tiles_o[0])
                for ti_, n in enumerate(tiles_o):
                    nxs = phaseA(tiles_o[ti_ + 1]) if ti_ + 1 < len(tiles_o) else None
                    phaseB(n, pxs)
                    pxs = nxs
            k.barrier()
            if stop_here("o%d" % l):
                es_aff.close()
                break
            streams = ([(0, 2, 32)] if need_ctx else []) + [(2, 32, 512)]
            with ExitStack() as s:
                work = k.sb([16, NTX], F32, es=s)
                m8 = k.sb([16, 8], F32, es=s)
                thr = k.sb([16, 1], F32, es=s)
                aff128 = k.sb([128, 512], F32, es=s)
                junk16 = k.sb([128, 512], BF16, es=s)
                lo = k.sb([128, 1], F32, es=s)
                hi = k.sb([128, 1], F32, es=s)
                mid = k.sb([128, 1], F32, es=s)
                cnt = k.sb([128, 1], F32, es=s)
                half = k.sb([128, 1], F32, es=s)
                k.memset(half, 0.5)
                mge = k.sb([128, 1], U32, es=s)
                mlt = k.sb([128, 1], U32, es=s)
                thr16 = k.sb([16, 8], F32, es=s)
                mask_e = k.sb([16, NTX], F32, es=s)
                maskTb = k.sb([128, 32, 16], BF16, es=s)
                carry = k.sb([128, 16], F32, es=s)
                pos_r = Rot([k.sb([128, 16], F32, es=s) for _ in range(2)])
                ptk = Rot([k.ps([128, 512], F32, es=s) for _ in range(4)])
                ahi = k.sb([128, NTILE, 16], BF16, es=s)
                alo = k.sb([128, NTILE, 16], F32, es=s)
                k.copy(ahi, affT)
                k.tt(alo, affT, ahi, ALU.subtract)
                k.copy(Rtab[:, :, :, 2], ahi, e="pool")
                k.copy(Rtab[:, :, :, 3], alo, e="pool")
                for (t0, ntl, cap) in streams:
                    ntok = ntl * 128
                    cs = np.s_[:, t0 * 128:t0 * 128 + ntok]
                    if cap <= 64:
                        k.copy(work[:, :ntok], aff_e[cs])
                        for r in range(cap // 8):
                            k.op("dve", lambda e: e.max(out=m8.ap, in_=work.ap[:, :ntok]), reads=[work], writes=[m8])
                            if r < cap // 8 - 1:
                                k.op("dve", lambda e: e.match_replace(out=work.ap[:, :ntok], in_to_replace=m8.ap,
                                                                      in_values=work.ap[:, :ntok], imm_value=-1.0),
                                     reads=[work, m8], writes=[work])
                        k.copy(thr, m8[:, 7:8])
                    else:
                        k.dma(AFFD, aff_e[cs])
                        k.dma(aff128, TD(AFFD, "e (s t) -> (e s) t", s=8))
                        k.memset(lo, 0.0)
                        k.memset(hi, 2.0)
                        for it in range(40):
                            k.stt(mid, lo, hi, half, ALU.add, ALU.mult)
                            k.ts(junk16, aff128, mid, ALU.is_ge, 0.0, ALU.add, accum=cnt)
                            pc = ptk.get()
                            k.mm(pc[:, 0:1], C("bmat"), cnt)
                            k.ts(mge, pc[:, 0:1], cap - 0.5, ALU.is_ge)
                            k.ts(mlt, pc[:, 0:1], cap - 0.5, ALU.is_lt)
                            k.op("dve", lambda e: e.copy_predicated(lo.ap, mge.ap, mid.ap), reads=[mge, mid], writes=[lo])
                            k.op("dve", lambda e: e.copy_predicated(hi.ap, mlt.ap, mid.ap), reads=[mlt, mid], writes=[hi])
                        k.dma(THRD, lo)
                        k.dma(thr16, TD(THRD, "(e s) o -> e (s o)", s=8))
                        k.copy(thr, thr16[:, 0:1])
                    k.ts(mask_e[:, :ntok], aff_e[cs], thr, ALU.is_ge)
                    k.memset(carry, 0.0)
                    for i in range(ntl):
                        n = t0 + i
                        pt = ptk.get()
                        k.tr(pt[:, 0:16], mask_e[:, i * 128:(i + 1) * 128], ident[0:16, 0:16])
                        k.copy(maskTb[:, i, :], pt[:, 0:16], e="act")
                        pc = ptk.get()
                        k.mm(pc[:, 0:16], triU_b, maskTb[:, i, :])
                        k.mm(pc[:, 16:32], ones_b, maskTb[:, i, :])
                        pos = pos_r.get()
                        k.tt(pos, pc[:, 0:16], carry, ALU.add)
                        k.tt(pos, pos, maskTb[:, i, :], ALU.mult)
                        k.ts(posm.sl(n, np.s_[:, n, :]), pos, -1.0, ALU.add)
                        k.tt(carry, carry, pc[:, 16:32], ALU.add)
            k.barrier()
            if stop_here("topk%d" % l):
                es_aff.close()
                break
            es_aff.close()
            with ExitStack() as s:
                mod5 = {st: load_mod(st, 5, s) for st in ([0, 1] if need_ctx else [1])}
                wsets = Rot([(k.sb([128, 8, 768], BF16, es=s), k.sb([128, 8, 768], BF16, es=s), k.sb([128, 6, D], BF16, es=s))
                             for _ in range(2)])
                stg = Rot([k.sb([128, 768], F32, es=s) for _ in range(4)])
                Sel = k.sb([128, 32, 512], BF16, es=s)
                iota16 = k.sb([128, 512], mybir.dt.int16, es=s)
                k.copy(iota16, C("iota"))
                r4T_r = Rot([k.sb([4, 512], F32, es=s) for _ in range(2)])
                r4_r = Rot([k.sb([128, 16], F32, es=s) for _ in range(3)])
                idx_r = Rot([k.sb([128, 1], I32, es=s) for _ in range(16)])
                idf_r = Rot([k.sb([128, 1], F32, es=s) for _ in range(4)])
                gt_r = Rot([k.sb([128, 1], F32, es=s) for _ in range(16)])
                xs_r = Rot([k.sb([128, D], BF16, es=s) for _ in range(10)])
                xsT_r = Rot([k.sb([128, 8, 512], BF16, es=s) for _ in range(2)])
                hid = k.sb([128, 6, 512], BF16, es=s)
                sg_r = Rot([k.sb([128, 512], F32, es=s) for _ in range(2)])
                ys_r = Rot([k.sb([128, D], F32, es=s) for _ in range(2)])
                p4 = Rot([k.ps([128, 512], F32, es=s) for _ in range(1)])
                ptb = Rot([k.ps([128, 8, 128], BF16, es=s) for _ in range(1)])
                pgu = Rot([k.ps([128, 512], F32, es=s) for _ in range(4)])
                pdn = Rot([k.ps([128, 512], F32, es=s) for _ in range(2)])
                Xall = [rows_key(X, n) for n in range(NTILE)] + [X.sub("all")]
                cast_eng = Rot(["act", "dve"])
                wcache = {}

                wgen = {"g": None}

                def weights_gen(e_, wg, wu, wd):
                    for kc in range(8):
                        load_cast(wg[:, kc, :], wg_d[l, e_, kc * 128:(kc + 1) * 128, :], stg, e=cast_eng.get())
                        yield
                        load_cast(wu[:, kc, :], wu_d[l, e_, kc * 128:(kc + 1) * 128, :], stg, e=cast_eng.get())
                        yield
                    for fc in range(6):
                        for hf_ in range(2):
                            load_cast(wd[:, fc, hf_ * 512:(hf_ + 1) * 512], wd_d[l, e_, fc * 128:(fc + 1) * 128, hf_ * 512:(hf_ + 1) * 512], stg, e=cast_eng.get())
                            yield

                def feed(n_):
                    for _ in range(n_):
                        if wgen["g"] is None:
                            return
                        try:
                            next(wgen["g"])
                        except StopIteration:
                            wgen["g"] = None

                def weights(e_, now=False):
                    feed(10000)
                    wg, wu, wd = wsets.get()
                    wcache[e_] = (wg, wu, wd)
                    wgen["g"] = weights_gen(e_, wg, wu, wd)
                    if now:
                        feed(10000)

                def selbuild(u):
                    e_, (t0, ntl, cap) = u
                    for i in range(ntl):
                        k.ts(Sel[:, i, :cap], iota16[:, :cap], posm[:, t0 + i, e_:e_ + 1], ALU.is_equal)

                def idxpart(u):
                    e_, (t0, ntl, cap) = u
                    ps = p4.get()
                    for i in range(ntl):
                        k.mm(ps[0:4, :cap], Rtab[:, t0 + i, e_, :], Sel[:, i, :cap], start=(i == 0), stop=(i == ntl - 1))
                    r4T = r4T_r.get()
                    k.copy(r4T[:, :cap], ps[0:4, :cap])
                    stiles = [(s0, min(128, cap - s0)) for s0 in range(0, cap, 128)]
                    for si, (s0, nsl) in enumerate(stiles):
                        k.tr(ps[0:nsl, si * 4:(si + 1) * 4], r4T[0:4, s0:s0 + nsl], ident[0:4, 0:4])
                    nsl0 = stiles[0][1]
                    r4 = r4_r.get()
                    k.copy(r4[0:nsl0, 0:4 * len(stiles)], ps[0:nsl0, 0:4 * len(stiles)])
                    meta = []
                    xss = []
                    for si, (s0, nsl) in enumerate(stiles):
                        c4 = si * 4
                        idf = idf_r.get()
                        k.stt(idf[0:nsl, :], r4[0:nsl, c4:c4 + 1], 64.0, r4[0:nsl, c4 + 1:c4 + 2], ALU.mult, ALU.add)
                        idx = idx_r.get()
                        k.copy(idx[0:nsl, :], idf[0:nsl, :])
                        gt = gt_r.get()
                        k.tt(gt[0:nsl, :], r4[0:nsl, c4 + 2:c4 + 3], r4[0:nsl, c4 + 3:c4 + 4], ALU.add)
                        meta.append((idx, gt))
                        xs = xs_r.get()
                        k.dma(xs[0:nsl, :], H2, q="pool", reads=[idx],
                              fn=lambda en: en.indirect_dma_start(out=xs.ap[0:nsl, :], out_offset=None, in_=H2.ap,
                                                                  in_offset=bass.IndirectOffsetOnAxis(ap=idx.ap[0:nsl, :], axis=0)))
                        xss.append(xs)
                    return [u, stiles, meta, xss, None]

                def xtrans(item):
                    u, stiles, meta, xss, _ = item
                    xsT = xsT_r.get()
                    for si, (s0, nsl) in enumerate(stiles):
                        xs = xss[si]
                        pt = ptb.get()
                        for kc in range(8):
                            k.tr(pt[:, kc, 0:nsl], xs[0:nsl, kc * 128:(kc + 1) * 128], ident_b[0:nsl, 0:nsl])
                        k.copy(xsT[:, :, s0:s0 + nsl], pt[:, :, 0:nsl], e="act")
                    item[4] = xsT

                def ffn(item):
                    (e_, (t0, ntl, cap)), stiles, meta, xss, xsT = item
                    wg, wu, wd = wcache[e_]
                    for fc in range(6):
                        pg = pgu.get()
                        pu = pgu.get()
                        for kc in range(8):
                            k.mm(pg[:, :cap], wg[:, kc, fc * 128:(fc + 1) * 128], xsT[:, kc, :cap], start=(kc == 0), stop=(kc == 7))
                        for kc in range(8):
                            k.mm(pu[:, :cap], wu[:, kc, fc * 128:(fc + 1) * 128], xsT[:, kc, :cap], start=(kc == 0), stop=(kc == 7))
                        sg = sg_r.get()
                        k.act(sg[:, :cap], pg[:, :cap], AF.Silu)
                        k.tt(hid[:, fc, :cap], sg[:, :cap], pu[:, :cap], ALU.mult)
                        feed(2)

                def down(item):
                    (e_, (t0, ntl, cap)), stiles, meta, xss, xsT = item
                    st = 0 if t0 == 0 else 1
                    wg, wu, wd = wcache[e_]
                    for si, (s0, nsl) in enumerate(stiles):
                        idx, gt = meta[si]
                        ys = ys_r.get()
                        for hf in range(2):
                            pd = pdn.get()
                            for fc in range(6):
                                k.mm(pd[0:nsl, :], hid[:, fc, s0:s0 + nsl], wd[:, fc, hf * 512:(hf + 1) * 512], start=(fc == 0), stop=(fc == 5))
                            k.stt(ys[0:nsl, hf * 512:(hf + 1) * 512], pd[0:nsl, :], gt[0:nsl, 0:1],
                                  mod5[st][0:nsl, hf * 512:(hf + 1) * 512], ALU.mult, ALU.mult)
                            feed(2)
                        k.dma(X, ys[0:nsl, :], q="pool", reads=[ys, idx], writes=Xall,
                              fn=lambda en: en.indirect_dma_start(out=X.ap, out_offset=bass.IndirectOffsetOnAxis(ap=idx.ap[0:nsl, :], axis=0),
                                                                  in_=ys.ap[0:nsl, :], in_offset=None, compute_op=ALU.add))

                units = [(e_, stm) for e_ in range(16) for stm in reversed(streams)]
                NU = len(units)
                weights(0, now=True)
                weights(1, now=True)
                nxt_w = 2
                selbuild(units[0])
                items = {0: idxpart(units[0])}
                if NU > 1:
                    selbuild(units[1])
                    items[1] = idxpart(units[1])
                xtrans(items[0])
                if NU > 2:
                    selbuild(units[2])
                for ui in range(NU):
                    ffn(items[ui])
                    if ui + 1 < NU:
                        xtrans(items[ui + 1])
                    if ui + 2 < NU:
                        items[ui + 2] = idxpart(units[ui + 2])
                    down(items[ui])
                    if ui + 3 < NU:
                        selbuild(units[ui + 3])
                    e_done = units[ui][0]
                    if (ui + 1 == NU or units[ui + 1][0] != e_done) and nxt_w < 16:
                        weights(nxt_w)
                        nxt_w += 1
                    del items[ui]
            k.barrier()
            if stop_here("exp%d" % l):
                break
        else:
            with ExitStack() as s:
                gfin = k.sb([128, D], F32, es=s)
                k.dma(gfin, g_bc_d[4])
                xt_r = Rot([k.sb([128, D], F32, es=s) for _ in range(5)])
                junk = k.sb([128, D], F32, es=s)
                ss_pool = Rot([k.sb([128, 1], F32, es=s) for _ in range(12)])
                def finA(n):
                    c0 = n * 128
                    xt = xt_r.get()
                    k.dma(xt, rows_key(X, n)[c0:c0 + 128, :])
                    ss = ss_pool.get()
                    k.act(junk, xt, AF.Square, accum=ss)
                    return (xt, ss)

                def finB(n, c_):
                    xt, ss = c_
                    c0 = n * 128
                    rstd = ss_pool.get()
                    k.rsq(rstd, ss, 1.0 / D, EPS)
                    k.stt(xt, xt, rstd, gfin, ALU.mult, ALU.mult)
                    k.dma(out_d.sl(n, np.s_[c0 - NTC:c0 - NTC + 128, :]), xt)
                tl_ = list(range(2, NTILE))
                q_ = [finA(tl_[0]), finA(tl_[1])]
                for i_, n in enumerate(tl_):
                    if i_ + 2 < len(tl_):
                        q_.append(finA(tl_[i_ + 2]))
                    finB(n, q_.pop(0))
        k.barrier()
        print("ninst", k.ninst, k.cnt, flush=True)
    return nc


_NC_CACHE = {}


def kernel(**inputs):
    inp = {kk: np.asarray(v) for kk, v in inputs.items()}
    shared = _prep_shared(inp)
    if "nc" not in _NC_CACHE:
        _NC_CACHE["nc"] = build()
    nc = _NC_CACHE["nc"]
    in_maps = []
    for b in range(8):
        m = dict(shared)
        m.update(_prep_core(inp, b))
        in_maps.append(m)
    res = run_bass_kernel_spmd(nc, in_maps, core_ids=list(range(8)))
    out = np.stack([np.asarray(r["out"], dtype=np.float32) for r in res.results], 0)
    return out
```

```python
import numpy as np
from contextlib import ExitStack
import concourse.bass as bass
import concourse.mybir as mybir
from concourse.bass_utils import run_bass_kernel_spmd

F32 = mybir.dt.float32
BF16 = mybir.dt.bfloat16
I32 = mybir.dt.int32
U32 = mybir.dt.uint32
AF = mybir.ActivationFunctionType
ALU = mybir.AluOpType
AX = mybir.AxisListType


class T:
    __slots__ = ("ap", "key")

    def __init__(self, ap, key):
        self.ap = ap
        self.key = key

    def __getitem__(self, idx):
        return T(self.ap[idx], self.key)

    def sub(self, suffix):
        return T(self.ap, (self.key, suffix))

    def sl(self, suffix, idx):
        return T(self.ap[idx], (self.key, suffix))


def _key(x):
    return x.key if isinstance(x, T) else x


class KB:
    CE = ("pe", "act", "dve", "pool")

    def __init__(self, nc, es, nds=14):
        self.nc = nc
        self.es = es
        self.E = {"pe": nc.tensor, "act": nc.scalar, "dve": nc.vector,
                  "pool": nc.gpsimd, "sp": nc.sync}
        self.csem = {e: es.enter_context(nc.semaphore("c_" + e)) for e in self.CE}
        self.cnt = {e: 0 for e in self.CE}
        self.NDS = nds
        self.dsem = [es.enter_context(nc.semaphore("d%d" % i)) for i in range(nds)]
        self.dcnt = [0] * nds
        self.dnext = 0
        self.waited = {e: {} for e in self.E}
        self.lastw = {}
        self.readers = {}
        self.nalloc = 0
        self.ninst = 0

    def sb(self, shape, dtype, name=None, es=None):
        self.nalloc += 1
        name = name or ("t%d" % self.nalloc)
        t = (es or self.es).enter_context(self.nc.sbuf_tensor(name + "_%d" % self.nalloc, list(shape), dtype))
        return T(t[:], name + "_%d" % self.nalloc)

    def ps(self, shape, dtype, name=None, es=None):
        self.nalloc += 1
        name = name or ("p%d" % self.nalloc)
        t = (es or self.es).enter_context(self.nc.psum_tensor(name + "_%d" % self.nalloc, list(shape), dtype))
        return T(t[:], name + "_%d" % self.nalloc)

    def dram(self, name, shape, dtype, kind="Internal"):
        t = self.nc.dram_tensor(name, list(shape), dtype, kind=kind)
        return T(t.ap(), name)

    def _semobj(self, semkey):
        return self.csem[semkey[1]] if semkey[0] == "c" else self.dsem[semkey[1]]

    def _wait(self, e, semkey, val):
        if self.waited[e].get(semkey, 0) >= val:
            return
        self.waited[e][semkey] = val
        self.E[e].wait_ge(self._semobj(semkey), val)

    def _deps(self, e, reads, writes, is_dma):
        for r in reads:
            lw = self.lastw.get(_key(r))
            if lw is not None:
                self._wait(e, lw[0], lw[1])
        for w in writes:
            k = _key(w)
            lw = self.lastw.get(k)
            if lw is not None:
                if not (lw[0] == ("c", e) and e == "pe" and not is_dma):
                    self._wait(e, lw[0], lw[1])
            for sk, v in self.readers.get(k, {}).items():
                if sk == ("c", e) and e == "pe" and not is_dma:
                    continue
                self._wait(e, sk, v)

    def _record(self, tok, reads, writes):
        for r in reads:
            d = self.readers.setdefault(_key(r), {})
            if d.get(tok[0], 0) < tok[1]:
                d[tok[0]] = tok[1]
        for w in writes:
            k = _key(w)
            self.lastw[k] = tok
            self.readers[k] = {}

    def op(self, e, fn, reads=(), writes=()):
        self._deps(e, reads, writes, False)
        ins = fn(self.E[e])
        self.cnt[e] += 1
        ins.then_inc(self.csem[e], 1)
        self._record((("c", e), self.cnt[e]), reads, writes)
        self.ninst += 1
        return ins

    def dma(self, out, in_, q="sp", fn=None, reads=None, writes=None, **kw):
        reads = [in_] if reads is None else reads
        writes = [out] if writes is None else writes
        slot = self.dnext
        self.dnext = (slot + 1) % self.NDS
        if self.dcnt[slot] > 0:
            self._wait(q, ("d", slot), 16 * self.dcnt[slot])
        self._deps(q, reads, writes, True)
        if fn is None:
            ins = self.E[q].dma_start(out=out.ap, in_=in_.ap, **kw)
        else:
            ins = fn(self.E[q])
        self.dcnt[slot] += 1
        ins.then_inc(self.dsem[slot], 16)
        self._record((("d", slot), 16 * self.dcnt[slot]), reads, writes)
        self.ninst += 1
        return ins

    def barrier(self):
        for e in self.E:
            for e2 in self.CE:
                if e2 != e and self.cnt[e2] > 0:
                    self._wait(e, ("c", e2), self.cnt[e2])
            for s in range(self.NDS):
                if self.dcnt[s] > 0:
                    self._wait(e, ("d", s), 16 * self.dcnt[s])

    def mm(self, out, lhsT, rhs, start=True, stop=True, extra_reads=()):
        return self.op("pe", lambda e: e.matmul(out.ap, lhsT.ap, rhs.ap, start=start, stop=stop),
                       reads=[lhsT, rhs, *extra_reads], writes=[out])

    def tr(self, out, in_, ident):
        return self.op("pe", lambda e: e.transpose(out.ap, in_.ap, ident.ap),
                       reads=[in_, ident], writes=[out])

    def act(self, out, in_, func, bias=None, scale=None, accum=None, e="act"):
        kw = {}
        rd = [in_]
        wr = [out]
        if bias is not None:
            if isinstance(bias, T):
                kw["bias"] = bias.ap
                rd.append(bias)
            else:
                kw["bias"] = bias
        if scale is not None:
            if isinstance(scale, T):
                kw["scale"] = scale.ap
                rd.append(scale)
            else:
                kw["scale"] = scale
        if accum is not None:
            kw["accum_out"] = accum.ap
            wr.append(accum)
        return self.op(e, lambda en: en.activation(out.ap, in_.ap, func, **kw), reads=rd, writes=wr)

    def tt(self, out, a, b, op, e="dve"):
        return self.op(e, lambda en: en.tensor_tensor(out.ap, a.ap, b.ap, op), reads=[a, b], writes=[out])

    def ts(self, out, a, s1, op0, s2=None, op1=None, e="dve", accum=None):
        rd = [a]
        wr = [out]
        v1 = s1
        v2 = s2
        if isinstance(s1, T):
            rd.append(s1)
            v1 = s1.ap
        if isinstance(s2, T):
            rd.append(s2)
            v2 = s2.ap
        kw = {}
        if op1 is not None:
            kw["op1"] = op1
        if accum is not None:
            kw["accum_out"] = accum.ap
            wr.append(accum)
        return self.op(e, lambda en: en.tensor_scalar(out.ap, a.ap, v1, v2, op0, **kw), reads=rd, writes=wr)

    def stt(self, out, a, s, b, op0, op1, e="dve"):
        rd = [a, b]
        v = s
        if isinstance(s, T):
            rd.append(s)
            v = s.ap
        return self.op(e, lambda en: en.scalar_tensor_tensor(out.ap, a.ap, v, b.ap, op0, op1), reads=rd, writes=[out])

    def copy(self, out, in_, e="dve"):
        if e == "act":
            return self.op(e, lambda en: en.activation(out.ap, in_.ap, AF.Copy), reads=[in_], writes=[out])
        return self.op(e, lambda en: en.tensor_copy(out.ap, in_.ap), reads=[in_], writes=[out])

    def memset(self, out, v, e="dve"):
        return self.op(e, lambda en: en.memset(out.ap, v), reads=[], writes=[out])

    def rsq(self, dst, src, mul, add):
        self.ts(dst, src, mul, ALU.mult, add, ALU.add)
        self.act(dst, dst, AF.Sqrt)
        self.op("dve", lambda e: e.reciprocal(dst.ap, dst.ap), reads=[dst], writes=[dst])

L_ = 2
D = 1024
NTX = 4096
NTC = 256
NT = NTX + NTC
NTILE = NT // 128
NCOL = 2464
SCALE_MLA = float((64 + 32) ** -0.5)
LN2 = float(np.log(2.0))


def _rope_tables():
    t = np.arange(NTX)
    row = (t // 64).astype(np.float32)
    col = (t % 64).astype(np.float32)

    def tab(dh_half):
        dh = dh_half
        inv = 10000.0 ** (-np.arange(0, dh, 2, dtype=np.float32) / dh)
        return inv

    def build(dtot):
        half = dtot // 2
        inv = tab(half)
        nf = half // 2
        cos = np.zeros((dtot, NTX), np.float32)
        sins = np.zeros((dtot, NTX), np.float32)
        perm = np.zeros((dtot, dtot), np.float32)
        for part, pos in ((0, row), (1, col)):
            base = part * half
            ang = pos[None, :] * inv[:, None]
            c, s = np.cos(ang), np.sin(ang)
            for j in range(nf):
                cos[base + j] = c[j]
                cos[base + nf + j] = c[j]
                sins[base + j] = -s[j]
                sins[base + nf + j] = s[j]
                perm[base + nf + j, base + j] = 1.0
                perm[base + j, base + nf + j] = 1.0
        return cos, sins, perm

    c32, s32, p32 = build(32)
    c64, s64, p64 = build(64)
    c128 = np.concatenate([c64, c64], 0)
    s128 = np.concatenate([s64, s64], 0)
    p128 = np.zeros((128, 128), np.float32)
    p128[:64, :64] = p64
    p128[64:, 64:] = p64
    rope32 = np.stack([np.tile(c32, (4, 1)), np.tile(s32, (4, 1))]).astype(np.float32)
    rope128 = np.stack([c128, s128]).astype(np.float32)
    return rope32, rope128, p32, p128


_CST = {}


def _cst_layout():
    off = 0
    for name, w in (("ident", 128), ("A", 128), ("B", 128), ("C1", 128), ("C2", 128),
                    ("colK1", 1), ("colK2", 1), ("iota", 512), ("triU", 128),
                    ("mprev", 128), ("mnext", 128), ("p128", 128), ("p32", 32), ("pswap", 128), ("bmat", 128), ("p32x4", 128),
                    ("tokid", NTILE * 16 * 2)):
        _CST[name] = (off, w)
        off += w
    return off


NCST = _cst_layout()


def _const_table():
    rope32, rope128, p32, p128 = _rope_tables()
    cst = np.zeros((128, NCST), np.float32)
    p = np.arange(128, dtype=np.float32)[:, None]
    c = np.arange(128, dtype=np.float32)[None, :]

    def put(name, arr):
        o, w = _CST[name]
        cst[:arr.shape[0], o:o + w] = arr
    put("ident", np.eye(128, dtype=np.float32))
    put("A", np.maximum(c - p, 0.0))
    put("B", np.maximum(p - c, 0.0))
    put("C1", np.broadcast_to(c + 1.0, (128, 128)))
    put("C2", np.broadcast_to(128.0 - c, (128, 128)))
    put("colK1", 127.0 - p)
    put("colK2", p)
    put("iota", np.broadcast_to(np.arange(512, dtype=np.float32)[None, :], (128, 512)))
    put("triU", (p <= c).astype(np.float32))
    put("mprev", (p >= c).astype(np.float32))
    put("mnext", (p <= c).astype(np.float32))
    put("p128", p128)
    put("p32", p32)
    p32x4 = np.zeros((128, 128), np.float32)
    for i_ in range(4):
        p32x4[i_ * 32:(i_ + 1) * 32, i_ * 32:(i_ + 1) * 32] = p32
    put("p32x4", p32x4)
    put("pswap", (np.arange(128)[:, None] == (np.arange(128)[None, :] + 64) % 128).astype(np.float32))
    put("bmat", (np.arange(128)[:, None] // 8 == np.arange(128)[None, :] // 8).astype(np.float32))
    rows = (np.arange(NTILE)[None, :] * 128 + np.arange(128)[:, None])
    tok = np.stack([rows // 64, rows % 64], -1).astype(np.float32)
    tok = np.broadcast_to(tok[:, :, None, :], (128, NTILE, 16, 2)).reshape(128, -1)
    put("tokid", tok)
    return cst, rope32, rope128


def _prep_shared(inp):
    f = lambda a: np.ascontiguousarray(a, dtype=np.float32)
    sh = {}
    w_in = inp["w_in"]
    s = np.cumsum([0, 256, 256, 256, 256, 256, 128, 32, 256, 128, 128])
    rq, rk, rv, rg, cq, ckv, kr, wq, wk, wv = [w_in[:, :, s[i]:s[i + 1]] for i in range(10)]
    wk2 = np.concatenate([wk[:, :, 0:64], wk[:, :, 0:64], wk[:, :, 64:128], wk[:, :, 64:128]], -1)
    wv2 = np.concatenate([wv[:, :, 0:64], wv[:, :, 0:64], wv[:, :, 64:128], wv[:, :, 64:128]], -1)
    sh["w_in_r"] = f(np.concatenate([rq, rk, cq, ckv, wq, wk2, kr, rk, rv, rg, wv2], -1))
    assert sh["w_in_r"].shape[-1] == NCOL
    uq = inp["mla_w_uq"]
    sh["w_uq_n"] = f(uq[:, :, :, :64].reshape(L_, 256, 512))
    sh["w_uq_r"] = f(uq[:, :, :, 64:].reshape(L_, 256, 256))
    sh["w_uk"] = f(inp["mla_w_uk"].reshape(L_, 128, 512))
    sh["w_uv"] = f(inp["mla_w_uv"].reshape(L_, 128, 512))
    sh["w_out"] = f(inp["w_out"])
    sh["router_w"] = f(inp["router_w"])
    sh["ada_w"] = f(inp["ada_w"])
    sh["ada_b"] = f(inp["ada_b"].reshape(L_, 1, 6 * D))
    sh["exp_wg"] = f(inp["exp_w_gate"])
    sh["exp_wu"] = f(inp["exp_w_up"])
    sh["exp_wd"] = f(inp["exp_w_down"])
    gb = np.stack([inp["norm1_g"][0], inp["norm2_g"][0], inp["norm1_g"][1], inp["norm2_g"][1], inp["final_g"]])
    sh["g_bc"] = f(np.broadcast_to(gb[:, None, :], (5, 128, D)))
    cols = []
    for l in range(L_):
        qg = inp["mla_qnorm_g"][l].reshape(2, 128).T
        kg = inp["mla_kvnorm_g"][l].reshape(1, 128).T
        df, db, sk = inp["ret_decay_f"][l], inp["ret_decay_b"][l], inp["win_sink"][l]
        rep = np.broadcast_to(np.concatenate([df, db, sk])[None, :], (128, 12))
        hp = (np.arange(128) >= 64).astype(np.int64)
        pp = np.stack([df[0 + hp], df[2 + hp], db[0 + hp], db[2 + hp]], -1)
        cols += [qg, kg, rep, pp]
    sh["small"] = f(np.concatenate(cols, -1))
    cst, rope32, rope128 = _const_table()
    sh["cst"] = cst
    sh["rope32"] = rope32
    sh["rope128"] = rope128
    return sh


def _prep_core(inp, b):
    f = lambda a: np.ascontiguousarray(a, dtype=np.float32)
    d = {}
    d["x0"] = f(np.concatenate([inp["ctx"][b], inp["x"][b]], 0))
    cv = np.stack([inp["c_ctx"], inp["c"][b]])
    cr = cv.reshape(2, 8, 128).transpose(0, 2, 1)
    d["crep"] = f(np.broadcast_to(cr[:, :, :, None], (2, 128, 8, 128)))
    return d

class Rot:
    def __init__(self, tiles):
        self.t = tiles
        self.i = 0

    def get(self):
        t = self.t[self.i % len(self.t)]
        self.i += 1
        return t


def TD(t, pattern, **kw):
    return T(t.ap.rearrange(pattern, **kw), t.key)


def build(upto=None, dbg=False, nlayers=L_):
    nc = bass.Bass("TRN2", target_bir_lowering=False)
    es0 = ExitStack()
    with es0:
        k = KB(nc, es0)
        kind_dbg = "ExternalOutput" if dbg else "Internal"
        din = lambda n, s, dt=F32: k.dram(n, s, dt, kind="ExternalInput")
        x0 = din("x0", [NT, D])
        crep_d = din("crep", [2, 128, 8, 128])
        w_in_d = din("w_in_r", [L_, D, NCOL])
        w_uq_n_d = din("w_uq_n", [L_, 256, 512])
        w_uq_r_d = din("w_uq_r", [L_, 256, 256])
        w_uk_d = din("w_uk", [L_, 128, 512])
        w_uv_d = din("w_uv", [L_, 128, 512])
        w_out_d = din("w_out", [L_, D, D])
        router_d = din("router_w", [L_, D, 16])
        ada_w_d = din("ada_w", [L_, D, 6 * D])
        ada_b_d = din("ada_b", [L_, 1, 6 * D])
        wg_d = din("exp_wg", [L_, 16, D, 768])
        wu_d = din("exp_wu", [L_, 16, D, 768])
        wd_d = din("exp_wd", [L_, 16, 768, D])
        g_bc_d = din("g_bc", [5, 128, D])
        small_d = din("small", [128, L_ * 19])
        cst_d = din("cst", [128, NCST])
        rope32_d = din("rope32", [2, 128, NTX])
        rope128_d = din("rope128", [2, 128, NTX])
        out_d = k.dram("out", [NTX, D], F32, kind="ExternalOutput")
        X = k.dram("X", [NT, D], F32, kind=kind_dbg)
        MODBC = k.dram("MODBC", [2, 6, 128, D], F32, kind=kind_dbg)
        QT = k.dram("QT", [2, 128, NT], BF16, kind=kind_dbg)
        KT = k.dram("KT", [2, 128, NT], BF16, kind=kind_dbg)
        TMo = k.dram("TMo", [NT, 1024], BF16, kind=kind_dbg)
        QN = k.dram("QN", [4, 128, NT], BF16, kind=kind_dbg)
        KN = k.dram("KN", [4, 128, NT], BF16, kind=kind_dbg)
        QR = k.dram("QR", [8, 32, NT], BF16, kind=kind_dbg)
        KVT = k.dram("KVT", [128, NT], BF16, kind=kind_dbg)
        KRT = k.dram("KRT", [32, NT], BF16, kind=kind_dbg)
        VP = k.dram("VP", [NT, 512], BF16, kind=kind_dbg)
        WQT = k.dram("WQT", [2, 128, NT], BF16, kind=kind_dbg)
        WKT = k.dram("WKT", [2, 128, NT], BF16, kind=kind_dbg)
        YT = k.dram("YT", [8, 128, NT], BF16, kind=kind_dbg)
        H2 = k.dram("H2", [NT, D], BF16, kind=kind_dbg)
        AFFD = k.dram("AFFD", [16, NTX], F32)
        THRD = k.dram("THRD", [128, 1], F32)

        cst = k.sb([128, NCST], F32, "cst")
        k.dma(cst, cst_d)
        small = k.sb([128, L_ * 19], F32, "small")
        k.dma(small, small_d)

        def C(name, rows=128):
            o, w = _CST[name]
            return cst[0:rows, o:o + w]
        ident = C("ident")
        ones_f = k.sb([128, 128], F32, "ones_f")
        k.memset(ones_f, 1.0)
        ones_b = k.sb([128, 128], BF16, "ones_b")
        k.memset(ones_b, 1.0)
        ident_b = k.sb([128, 128], BF16, "ident_b")
        k.copy(ident_b, ident)
        triU_b = k.sb([128, 128], BF16, "triU_b")
        k.copy(triU_b, C("triU"))
        mprev_b = k.sb([128, 128], BF16, "mprev_b")
        k.copy(mprev_b, C("mprev"))
        mnext_b = k.sb([128, 128], BF16, "mnext_b")
        k.copy(mnext_b, C("mnext"))
        Rtab = k.sb([128, NTILE, 16, 4], BF16, "Rtab")
        o_tok, w_tok = _CST["tokid"]
        k.copy(Rtab[:, :, :, 0:2], T(cst.ap[:, o_tok:o_tok + w_tok].rearrange("p (n e c) -> p n e c", n=NTILE, e=16), cst.key))
        affT = k.sb([128, NTILE, 16], F32, "affT")
        posm = k.sb([128, NTILE, 16], F32, "posm")

        EPS = 1e-6
        blocks = [(0, 256, 0)] + [(256 + 512 * i, 512, 1) for i in range(8)]

        def rows_key(t, n):
            return t.sub(("r", n))

        def stop_here(name):
            return upto is not None and upto == name

        def rstd_from_ss(ss, n_feat, es, eps=EPS):
            r = k.sb([128, 1], F32, es=es)
            k.rsq(r, ss, 1.0 / n_feat, eps)
            return r

        for l in range(nlayers):
            need_ctx = l < L_ - 1
            sm0 = l * 19
            Xsrc = x0 if l == 0 else X
            with ExitStack() as s:
                crep = k.sb([128, 2, 8, 128], F32, es=s)
                for st in range(2):
                    k.dma(crep[:, st], crep_d[st])
                sil = k.sb([128, 2, 8, 128], F32, es=s)
                k.act(sil, crep, AF.Silu)
                wb = Rot([k.sb([128, 8, 512], F32, es=s) for _ in range(3)])
                br = Rot([k.sb([1, 512], F32, es=s) for _ in range(2)])
                pp = Rot([k.ps([128, 512], F32, es=s) for _ in range(4)])
                ob = Rot([k.sb([128, 512], F32, es=s) for _ in range(4)])
                aw = TD(ada_w_d[l], "(kc p) n -> p kc n", p=128)
                for nb in range(12):
                    w = wb.get()
                    for q4 in range(4):
                        k.dma(w.sl(q4, np.s_[:, q4 * 2:(q4 + 1) * 2, :]), aw[:, q4 * 2:(q4 + 1) * 2, nb * 512:(nb + 1) * 512])
                    b_ = br.get()
                    k.dma(b_, ada_b_d[l, :, nb * 512:(nb + 1) * 512])
                    for st in range(2):
                        ps = pp.get()
                        for kc in range(8):
                            k.mm(ps, sil[:, st, kc, :], w.sl(kc // 2, np.s_[:, kc, :]), start=(kc == 0), stop=False)
                        k.mm(ps, ones_f[0:1, :], b_, start=False, stop=True)
                        o = ob.get()
                        k.copy(o, ps, e=("act" if st == 0 else "dve"))
                        j, half = nb // 2, nb % 2
                        k.dma(MODBC.sl((st, j), np.s_[st, j, :, half * 512:(half + 1) * 512]), o)
            k.barrier()
            if stop_here("mod%d" % l):
                break

            def load_mod(st, j, es, eng_q="sp"):
                t = k.sb([128, D], F32, es=es)
                k.dma(t, MODBC.sl((st, j), np.s_[st, j]))
                return t

            def load_gs(st, j_scale, gidx, es):
                sc = load_mod(st, j_scale, es)
                gb = k.sb([128, D], F32, es=es)
                k.dma(gb, g_bc_d[gidx])
                k.stt(sc, sc, 1.0, gb, ALU.add, ALU.mult)
                return sc

            def load_cast(dst, src, stg, e="pool"):
                t = stg.get()
                sh = list(src.ap.shape)
                tv = t[0:sh[0], 0:sh[1]]
                k.dma(tv, src)
                k.copy(dst, tv, e=e)

            with ExitStack() as s:
                w_in = k.sb([128, 8, NCOL], BF16, es=s)
                stg = Rot([k.sb([128, NCOL], F32, es=s) for _ in range(2)])
                for kc in range(8):
                    load_cast(w_in[:, kc, :], w_in_d[l, kc * 128:(kc + 1) * 128, :], stg, e=("pool" if kc % 2 else "dve"))
                w_uq_n = k.sb([128, 2, 512], BF16, es=s)
                w_uq_r = k.sb([128, 2, 256], BF16, es=s)
                for kc in range(2):
                    load_cast(w_uq_n[:, kc, :], w_uq_n_d[l, kc * 128:(kc + 1) * 128, :], stg)
                    load_cast(w_uq_r[:, kc, :], w_uq_r_d[l, kc * 128:(kc + 1) * 128, :], stg)
                w_uk = k.sb([128, 512], BF16, es=s)
                load_cast(w_uk, w_uk_d[l], stg)
                w_uv = k.sb([128, 512], BF16, es=s)
                load_cast(w_uv, w_uv_d[l], stg)
                gs1 = [load_gs(st, 1, l * 2 + 0, s) for st in range(2)]
                sh1 = [load_mod(st, 0, s) for st in range(2)]
                qg = small[:, sm0 + 0:sm0 + 2]
                kg = small[:, sm0 + 2:sm0 + 3]
                xt_r = Rot([k.sb([128, D], F32, es=s) for _ in range(3)])
                xs_r = Rot([k.sb([128, D], F32, es=s) for _ in range(3)])
                junk = k.sb([128, D], F32, es=s)
                ss_pool = Rot([k.sb([128, 1], F32, es=s) for _ in range(8)])
                hT_r = Rot([k.sb([128, 8, 512], BF16, es=s) for _ in range(2)])
                ptr = Rot([k.ps([128, 8, 128], BF16, es=s) for _ in range(2)])
                xb_r = Rot([k.sb([128, D], BF16, es=s) for _ in range(5)])
                pfm = Rot([k.ps([128, 512], F32, es=s) for _ in range(3)])
                pex = Rot([k.ps([128, 512], F32, es=s) for _ in range(3)])
                ob16 = Rot([k.sb([128, 512], BF16, es=s) for _ in range(6)])
                f32t = Rot([k.sb([128, 512], F32, es=s) for _ in range(6)])
                rp32 = Rot([k.sb([128, 2, 512], F32, es=s) for _ in range(2)])
                rp128 = Rot([k.sb([128, 2, 512], F32, es=s) for _ in range(2)])
                cqg = k.sb([128, 2, 512], BF16, es=s)
                rstdq = k.sb([128, 512], F32, es=s)
                kvT_sb = k.sb([128, 512], BF16, es=s)
                tmo = Rot([k.sb([128, 1024], BF16, es=s) for _ in range(2)])

                def bc_rstd(dst, sq_list, nfeat, n):
                    ps = pex.get()
                    for i, sq in enumerate(sq_list):
                        k.mm(ps[:, :n], ones_f, sq, start=(i == 0), stop=(i == len(sq_list) - 1))
                    k.rsq(dst[:, :n], ps[:, :n], 1.0 / nfeat, EPS)

                def rope_apply(src_f32, M, n, tabs, perm, out_bf):
                    ps = pex.get()
                    k.mm(ps[0:M, :n], perm, src_f32[0:M, :n])
                    t1 = f32t.get()
                    k.tt(t1[0:M, :n], src_f32[0:M, :n], tabs[0:M, 0, :n], ALU.mult)
                    t2 = f32t.get()
                    k.tt(t2[0:M, :n], ps[0:M, :n], tabs[0:M, 1, :n], ALU.mult)
                    k.tt(out_bf[0:M, :n], t1[0:M, :n], t2[0:M, :n], ALU.add, e="pool")

                class NormState:
                    pass

                def norm_begin(blk):
                    (r0, n, st) = blk
                    ns = NormState()
                    ns.blk = blk
                    ns.hT = hT_r.get()
                    ns.r32 = ns.r128 = None
                    ns.xbs = []
                    if st == 1:
                        t0 = r0 - NTC
                        ns.r32 = rp32.get()
                        k.dma(ns.r32[:, :, :n], TD(rope32_d, "a p t -> p a t")[:, :, t0:t0 + n])
                        ns.r128 = rp128.get()
                        k.dma(ns.r128[:, :, :n], TD(rope128_d, "a p t -> p a t")[:, :, t0:t0 + n])
                    return ns

                def norm_ew(ns, ti):
                    (r0, n, st) = ns.blk
                    if ti >= n // 128:
                        return
                    rr = r0 + ti * 128
                    xt = xt_r.get()
                    k.dma(xt, rows_key(Xsrc, rr // 128)[rr:rr + 128, :])
                    ss = ss_pool.get()
                    k.act(junk, xt, AF.Square, accum=ss)
                    rstd = ss_pool.get()
                    k.rsq(rstd, ss, 1.0 / D, EPS)
                    xs = xs_r.get()
                    k.stt(xs, xt, rstd, gs1[st], ALU.mult, ALU.mult)
                    xb = xb_r.get()
                    k.tt(xb, xs, sh1[st], ALU.add)
                    ns.xbs.append(xb)

                def norm_tr(ns):
                    (r0, n, st) = ns.blk
                    for ti, xb in enumerate(ns.xbs):
                        pt = ptr.get()
                        for kc in range(8):
                            k.tr(pt[:, kc, :], xb[:, kc * 128:(kc + 1) * 128], ident_b)
                        k.copy(ns.hT[:, :, ti * 128:(ti + 1) * 128], pt, e="act")
                    return (ns.hT, ns.r32, ns.r128)

                def proj_phase(blk, ctx_, hook):
                    (r0, n, st) = blk
                    nt = n // 128
                    hT, r32, r128 = ctx_
                    def fm(c, M=128):
                        ps = pfm.get()
                        for kc in range(8):
                            k.mm(ps[0:M, :n], w_in[:, kc, c * 128:c * 128 + M], hT[:, kc, :n], start=(kc == 0), stop=(kc == 7))
                        return ps
                    for c in range(4):
                        ps = fm(c)
                        o = ob16.get()
                        k.copy(o[:, :n], ps[:, :n], e=("act" if c % 2 == 0 else "dve"))
                        dst = (QT if c < 2 else KT)
                        k.dma(dst.sl((c % 2, r0), np.s_[c % 2, :, r0:r0 + n]), o[:, :n])
                    hook(0)
                    sqs = []
                    for c2 in range(2):
                        ps = fm(4 + c2)
                        sq = f32t.get()
                        k.act(sq[:, :n], ps[:, :n], AF.Square)
                        sqs.append(sq[:, :n])
                        k.act(cqg[:, c2, :n], ps[:, :n], AF.Copy, scale=qg[:, c2:c2 + 1])
                    bc_rstd(rstdq, sqs, 256, n)
                    for pr in range(4):
                        ps = pex.get()
                        for c2 in range(2):
                            k.mm(ps[:, :n], w_uq_n[:, c2, pr * 128:(pr + 1) * 128], cqg[:, c2, :n], start=(c2 == 0), stop=(c2 == 1))
                        qn = ob16.get()
                        k.tt(qn[:, :n], ps[:, :n], rstdq[:, :n], ALU.mult)
                        k.dma(QN.sl((pr, r0), np.s_[pr, :, r0:r0 + n]), qn[:, :n])
                    for g4 in range(2):
                        ps = pex.get()
                        for c2 in range(2):
                            k.mm(ps[:, :n], w_uq_r[:, c2, g4 * 128:(g4 + 1) * 128], cqg[:, c2, :n], start=(c2 == 0), stop=(c2 == 1))
                        qr = f32t.get()
                        k.tt(qr[:, :n], ps[:, :n], rstdq[:, :n], ALU.mult)
                        o = ob16.get()
                        if st == 1:
                            rope_apply(qr, 128, n, r32, C("p32x4"), o)
                        else:
                            k.copy(o[:, :n], qr[:, :n], e="pool")
                        for hh in range(4):
                            h = g4 * 4 + hh
                            k.dma(QR.sl((h, r0), np.s_[h, :, r0:r0 + n]), o[hh * 32:(hh + 1) * 32, :n])
                    hook(1)
                    ps = fm(6)
                    sq = f32t.get()
                    k.act(sq[:, :n], ps[:, :n], AF.Square)
                    kvg = f32t.get()
                    k.act(kvg[:, :n], ps[:, :n], AF.Copy, scale=kg[:, 0:1])
                    rk_ = f32t.get()
                    bc_rstd(rk_, [sq[:, :n]], 128, n)
                    k.tt(kvT_sb[:, :n], kvg[:, :n], rk_[:, :n], ALU.mult)
                    k.dma(KVT.sl(r0, np.s_[:, r0:r0 + n]), kvT_sb[:, :n])
                    for pr in range(4):
                        ps = pex.get()
                        k.mm(ps[:, :n], w_uk[:, pr * 128:(pr + 1) * 128], kvT_sb[:, :n])
                        o = ob16.get()
                        k.copy(o[:, :n], ps[:, :n], e=("act" if pr % 2 == 0 else "dve"))
                        k.dma(KN.sl((pr, r0), np.s_[pr, :, r0:r0 + n]), o[:, :n])
                    for ti in range(nt):
                        ps = pex.get()
                        k.mm(ps, kvT_sb[:, ti * 128:(ti + 1) * 128], w_uv)
                        o = ob16.get()
                        k.copy(o, ps, e="act")
                        rr = r0 + ti * 128
                        k.dma(VP.sl(rr // 128, np.s_[rr:rr + 128, :]), o)
                    hook(2)
                    for c in range(4):
                        ps = fm(7 + c)
                        o = ob16.get()
                        if st == 1:
                            sf = f32t.get()
                            k.copy(sf[:, :n], ps[:, :n], e="act")
                            rope_apply(sf, 128, n, r128, C("p128"), o)
                        else:
                            k.copy(o[:, :n], ps[:, :n], e="act")
                        dst = (WQT if c < 2 else WKT)
                        k.dma(dst.sl((c % 2, r0), np.s_[c % 2, :, r0:r0 + n]), o[:, :n])
                    ps = fm(11, 32)
                    o = ob16.get()
                    if st == 1:
                        sf = f32t.get()
                        k.copy(sf[0:32, :n], ps[0:32, :n], e="act")
                        rope_apply(sf, 32, n, r32, C("p32", 32), o)
                    else:
                        k.copy(o[0:32, :n], ps[0:32, :n], e="act")
                    k.dma(KRT.sl(r0, np.s_[:, r0:r0 + n]), o[0:32, :n])
                    hook(3)
                    for ti in range(nt):
                        o = tmo.get()
                        for hf in range(2):
                            ps = pfm.get()
                            c0 = 1440 + hf * 512
                            for kc in range(8):
                                k.mm(ps, hT[:, kc, ti * 128:(ti + 1) * 128], w_in[:, kc, c0:c0 + 512], start=(kc == 0), stop=(kc == 7))
                            k.copy(o[:, hf * 512:(hf + 1) * 512], ps, e=("act" if hf == 0 else "dve"))
                        rr = r0 + ti * 128
                        k.dma(TMo.sl(rr // 128, np.s_[rr:rr + 128, :]), o)
                ns0 = norm_begin(blocks[0])
                for ti_ in range(4):
                    norm_ew(ns0, ti_)
                pctx = norm_tr(ns0)
                for bi, blk in enumerate(blocks):
                    nsn = norm_begin(blocks[bi + 1]) if bi + 1 < len(blocks) else None
                    proj_phase(blk, pctx, (lambda i_: norm_ew(nsn, i_)) if nsn is not None else (lambda i_: None))
                    pctx = norm_tr(nsn) if nsn is not None else None
            k.barrier()
            if stop_here("s1_%d" % l):
                break
            es_mla_pre = ExitStack()
            vp2 = k.sb([128, NTILE, 8, 128], BF16, es=es_mla_pre)
            VPv = TD(VP, "(t p) (h d) -> p t h d", p=128, d=64)
            for par in range(2):
                k.memset(vp2[:, :, par::2, (1 - par) * 64:(2 - par) * 64], 1.0, e="pool")
                for h in range(par, 8, 2):
                    k.dma(vp2[:, :, h, par * 64:(par + 1) * 64], VPv[:, :, h, :])
            with ExitStack() as s:
                lg_rep = k.sb([128, 8], F32, es=s)
                lg_pp = k.sb([128, 4], F32, es=s)
                for dst, src in ((lg_rep, small[:, sm0 + 3:sm0 + 11]), (lg_pp, small[:, sm0 + 15:sm0 + 19])):
                    k.act(dst, src, AF.Exp, scale=LN2)
                    k.ts(dst, dst, -1.0, ALU.mult, 1.0, ALU.add)
                    k.act(dst, dst, AF.Ln)
                LN8 = float(np.log(0.125))
                DT = k.sb([128, 4, 128], F32, es=s)
                tmpA = k.sb([128, 128], F32, es=s)
                for h in range(4):
                    k.ts(tmpA, C("A"), lg_rep[:, h:h + 1], ALU.mult)
                    k.stt(tmpA, C("B"), lg_rep[:, 4 + h:5 + h], tmpA, ALU.mult, ALU.add)
                    k.act(DT[:, h, :], tmpA, AF.Exp)
                    k.ts(DT[:, h, :], DT[:, h, :], 0.125, ALU.mult)
                QWF = k.sb([128, 2, 128], F32, es=s)
                QWB = k.sb([128, 2, 128], F32, es=s)
                gch = k.sb([128, 4], F32, es=s)
                for pr in range(2):
                    k.act(QWF[:, pr, :], C("C1"), AF.Exp, scale=lg_pp[:, pr:pr + 1])
                    k.act(QWB[:, pr, :], C("C2"), AF.Exp, scale=lg_pp[:, 2 + pr:3 + pr])
                k.act(gch, lg_pp, AF.Exp, scale=128.0)
                KWF = k.sb([128, 256], F32, es=s)
                KWB = k.sb([128, 256], F32, es=s)
                kcol = k.sb([128, 8], F32, es=s)
                for h in range(4):
                    k.act(kcol[:, h:h + 1], C("colK1"), AF.Exp, scale=lg_rep[:, h:h + 1])
                    k.act(kcol[:, 4 + h:5 + h], C("colK2"), AF.Exp, scale=lg_rep[:, 4 + h:5 + h])
                k.ts(kcol, kcol, 0.125, ALU.mult)
                for h in range(4):
                    k.copy(KWF[:, h * 64:(h + 1) * 64], T(kcol.ap[:, h:h + 1].to_broadcast([128, 64]), kcol.key))
                    k.copy(KWB[:, h * 64:(h + 1) * 64], T(kcol.ap[:, 4 + h:5 + h].to_broadcast([128, 64]), kcol.key))
                KVd = k.sb([128, NTILE, 4, 64], F32, es=s)
                Sfb = k.sb([128, NTILE, 2, 64], BF16, es=s)
                Sbb = k.sb([128, NTILE, 2, 64], BF16, es=s)
                kv_r = Rot([k.sb([128, 512], BF16, es=s) for _ in range(3)])
                kw_r = Rot([k.sb([128, 2, 256], BF16, es=s) for _ in range(2)])
                pkv = Rot([k.ps([128, 4, 128], F32, es=s) for _ in range(2)])
                fwd_order = list(range(NTILE))
                bwd_order = [1, 0] + list(range(NTILE - 1, 1, -1))
                r1_order = []
                for a_, b_ in zip(fwd_order, bwd_order):
                    for t_ in (a_, b_):
                        if t_ not in r1_order:
                            r1_order.append(t_)
                curs = []
                for d_ in range(2):
                    c_ = k.sb([128, 2, 64], F32, es=s)
                    k.memset(c_, 0.0)
                    curs.append(c_)
                ptrs = [0, 0]
                done = set()

                def scan_step(d_, n):
                    Sb_ = Sfb if d_ == 0 else Sbb
                    cur = curs[d_]
                    k.copy(Sb_.sl(n, np.s_[:, n, :, :]), cur)
                    for pr in range(2):
                        k.stt(cur[:, pr, :], cur[:, pr, :], gch[:, d_ * 2 + pr:d_ * 2 + pr + 1],
                              KVd.sl(n, np.s_[:, n, d_ * 2 + pr, :]), ALU.mult, ALU.add)
                for n in r1_order:
                    kvt = kv_r.get()
                    k.dma(kvt, TMo.sl(n, np.s_[n * 128:(n + 1) * 128, 0:512]))
                    kw = kw_r.get()
                    k.tt(kw[:, 0, :], kvt[:, 0:256], KWF, ALU.mult)
                    k.tt(kw[:, 1, :], kvt[:, 0:256], KWB, ALU.mult)
                    ps = pkv.get()
                    for d_ in range(2):
                        for pr in range(2):
                            k.mm(ps[:, d_ * 2 + pr, :], kw[:, d_, pr * 128:(pr + 1) * 128], kvt[:, 256 + pr * 128:256 + (pr + 1) * 128])
                    k.copy(KVd.sl(n, np.s_[0:64, n, :, :]), ps[0:64, :, 0:64], e="act")
                    k.copy(KVd.sl(n, np.s_[64:128, n, :, :]), ps[64:128, :, 64:128], e="act")
                    done.add(n)
                    for d_, order in ((0, fwd_order), (1, bwd_order)):
                        while ptrs[d_] < NTILE and order[ptrs[d_]] in done:
                            scan_step(d_, order[ptrs[d_]])
                            ptrs[d_] += 1
                qk_r = Rot([k.sb([128, 2, 2, 128], BF16, es=s) for _ in range(3)])
                vg_r = Rot([k.sb([128, 512], BF16, es=s) for _ in range(3)])
                qw_r = Rot([k.sb([128, 2, 2, 128], BF16, es=s) for _ in range(2)])
                sm_r = Rot([k.sb([128, 512], BF16, es=s) for _ in range(4)])
                pss = Rot([k.ps([128, 512], F32, es=s) for _ in range(2)] + [T(p_.ap.rearrange("p a b -> p (a b)"), p_.key) for p_ in pkv.t])
                psy = Rot([k.ps([128, 512], F32, es=s) for _ in range(2)])
                ysb_r = Rot([k.sb([128, 256], F32, es=s) for _ in range(3)])
                sq_r = Rot([k.sb([128, 256], F32, es=s) for _ in range(2)])
                st_r = Rot([k.sb([128, 16], F32, es=s) for _ in range(2)])
                yo_r = Rot([k.sb([128, 2, 128], BF16, es=s) for _ in range(2)])
                yb_r = Rot([k.sb([128, 256], BF16, es=s) for _ in range(2)])
                ptb2 = Rot([k.ps([128, 2, 128], BF16, es=s) for _ in range(2)])

                sgall = k.sb([128, NTILE, 256], BF16, es=s)
                for n in range(0 if need_ctx else 2, NTILE):
                    gt_ = vg_r.get()
                    k.dma(gt_[:, 0:256], TMo.sl(n, np.s_[n * 128:(n + 1) * 128, 512:768]))
                    k.act(sgall[:, n, :], gt_[:, 0:256], AF.Silu)

                def r2A(n):
                    c0 = n * 128
                    qk = qk_r.get()
                    k.dma(qk[:, 0], TD(QT, "c p t -> p c t")[:, :, c0:c0 + 128])
                    k.dma(qk[:, 1], TD(KT, "c p t -> p c t")[:, :, c0:c0 + 128])
                    vg = vg_r.get()
                    k.dma(vg, TMo.sl(n, np.s_[c0:c0 + 128, 256:768]))
                    qw = qw_r.get()
                    k.tt(qw[:, 0], qk[:, 0], QWF, ALU.mult)
                    k.tt(qw[:, 1], qk[:, 0], QWB, ALU.mult)
                    py = psy.get()
                    psA = pss.get()
                    psB = pss.get()
                    for h in range(4):
                        pr, off = h // 2, (h % 2) * 64
                        pb = psA if h % 2 == 0 else psB
                        k.mm(pb[:, pr * 128:(pr + 1) * 128], qk[off:off + 64, 1, pr, :], qk[off:off + 64, 0, pr, :])
                    sm = sm_r.get()
                    smv = T(sm.ap.rearrange("p (a c) -> p a c", a=4), sm.key)
                    k.tt(smv[:, 0::2, :], T(psA.ap[:, 0:256].rearrange("p (a c) -> p a c", a=2), psA.key), DT[:, 0::2, :], ALU.mult)
                    k.tt(smv[:, 1::2, :], T(psB.ap[:, 0:256].rearrange("p (a c) -> p a c", a=2), psB.key), DT[:, 1::2, :], ALU.mult)
                    for h in range(4):
                        pr, off = h // 2, (h % 2) * 64
                        yo_ = py[:, h * 64:(h + 1) * 64]
                        k.mm(yo_, sm[:, h * 128:(h + 1) * 128], vg[:, h * 64:(h + 1) * 64], start=True, stop=False)
                        k.mm(yo_, qw[off:off + 64, 0, pr, :], Sfb.sl(n, np.s_[off:off + 64, n, pr, :]), start=False, stop=False)
                        k.mm(yo_, qw[off:off + 64, 1, pr, :], Sbb.sl(n, np.s_[off:off + 64, n, pr, :]), start=False, stop=True)
                    ysb = ysb_r.get()
                    k.copy(ysb, py[:, 0:256], e="act")
                    return (ysb, sgall[:, n, :])

                def r2B(n, ctx_):
                    ysb, sg = ctx_
                    c0 = n * 128
                    stt_ = st_r.get()
                    k.op("dve", lambda e: e.reduce_sum(stt_.ap[:, 0:4], ysb.ap.rearrange("p (h d) -> p h d", h=4), AX.X), reads=[ysb], writes=[stt_])
                    sq = sq_r.get()
                    k.tt(sq, ysb, ysb, ALU.mult)
                    k.op("dve", lambda e: e.reduce_sum(stt_.ap[:, 4:8], sq.ap.rearrange("p (h d) -> p h d", h=4), AX.X), reads=[sq], writes=[stt_])
                    k.ts(stt_[:, 0:8], stt_[:, 0:8], 1.0 / 64, ALU.mult)
                    k.tt(stt_[:, 8:12], stt_[:, 0:4], stt_[:, 0:4], ALU.mult)
                    k.tt(stt_[:, 12:16], stt_[:, 4:8], stt_[:, 8:12], ALU.subtract)
                    k.rsq(stt_[:, 12:16], stt_[:, 12:16], 1.0, 1e-5)
                    for h in range(4):
                        k.ts(ysb[:, h * 64:(h + 1) * 64], ysb[:, h * 64:(h + 1) * 64], stt_[:, h:h + 1], ALU.subtract,
                             stt_[:, 12 + h:13 + h], ALU.mult)
                    yb = yb_r.get()
                    k.tt(yb, ysb, sg, ALU.mult)
                    pt = ptb2.get()
                    for c in range(2):
                        k.tr(pt[:, c, :], yb[:, c * 128:(c + 1) * 128], ident_b)
                    yo = yo_r.get()
                    k.copy(yo, pt, e="act")
                    k.dma(TD(YT, "c p t -> p c t").sl(("ret", n), np.s_[:, 0:2, c0:c0 + 128]), yo, q="pool")
                tiles_r = list(range(0 if need_ctx else 2, NTILE))
                pc_ = r2A(tiles_r[0])
                for ti_, n in enumerate(tiles_r):
                    nc_ = r2A(tiles_r[ti_ + 1]) if ti_ + 1 < len(tiles_r) else None
                    r2B(n, pc_)
                    pc_ = nc_
            k.barrier()
            if stop_here("ret%d" % l):
                es_mla_pre.close()
                break
            with ExitStack() as s:
                kh_l = [k.sb([128, NT], BF16, es=s) for _ in range(2)]
                for t_ in kh_l:
                    k.memset(t_, 0.0, e="pool")
                kh_r = Rot(kh_l)
                q_l = [k.sb([128, 512], BF16, es=s) for _ in range(3)]
                for t_ in q_l:
                    k.memset(t_, 0.0)
                q_r = Rot(q_l)
                rd_sets = []
                for par in range(2):
                    tl = [k.sb([128, 512], F32, es=s) for _ in range(2)]
                    for t_ in tl:
                        k.memset(t_, 0.0)
                    rd_sets.append(Rot(tl))
                pT_r = Rot([k.sb([128, 2, 512], BF16, es=s) for _ in range(4)])
                pss = Rot([k.ps([128, 2, 512], F32, es=s) for _ in range(2)])
                pacc = Rot([k.ps([128, 512], F32, es=s) for _ in range(4)])
                sw_r = Rot([k.sb([128, 512], F32, es=s) for _ in range(2)])
                yp_r = Rot([k.sb([128, 512], BF16, es=s) for _ in range(3)])
                qblocks = ([(0, 256, [0, 1])] if need_ctx else []) + [(256 + 512 * i, 512, list(range(NTILE))) for i in range(8)]
                LOOK = 1
                pend = []

                def emit_pv2(item):
                    h, r0, n, j, npair, kt2, pT, pv = item
                    for u_ in range(2):
                        first = (j == 0 and u_ == 0)
                        last = (j == npair - 1 and u_ == 1)
                        k.mm(pv[:, :n], vp2[:, kt2[u_], h, :], pT[:, u_, :n], start=first, stop=last)
                    if j == npair - 1:
                        off = (h % 2) * 64
                        dof = 64 - off
                        rd = rd_sets[h % 2].get()
                        k.op("dve", lambda e: e.reciprocal(rd.ap[dof:dof + 64, :n], pv.ap[dof:dof + 64, :n]), reads=[pv], writes=[rd])
                        sws = sw_r.get()
                        k.dma(sws[off:off + 64, :n], rd[dof:dof + 64, :n])
                        yp = yp_r.get()
                        k.tt(yp[off:off + 64, :n], pv[off:off + 64, :n], sws[off:off + 64, :n], ALU.mult)
                        k.dma(YT.sl(("mla", h, r0), np.s_[2 + h // 2, off:off + 64, r0:r0 + n]), yp[off:off + 64, :n])
                for h in range(8):
                    off = (h % 2) * 64
                    kh = kh_r.get()
                    k.dma(kh[0:64, :], KN[h // 2, off:off + 64, :])
                    k.dma(kh[64:96, :], KRT)
                    for (r0, n, kts) in qblocks:
                        q = q_r.get()
                        k.dma(q[0:64, :n], QN[h // 2, off:off + 64, r0:r0 + n])
                        k.dma(q[64:96, :n], QR[h, :, r0:r0 + n])
                        pv = pacc.get()
                        npair = len(kts) // 2
                        for j in range(npair):
                            kt2 = (kts[2 * j], kts[2 * j + 1])
                            ps = pss.get()
                            for u_ in range(2):
                                k.mm(ps[:, u_, :n], kh[:, kt2[u_] * 128:(kt2[u_] + 1) * 128], q[:, :n])
                            pT = pT_r.get()
                            k.act(pT[:, :, :n], ps[:, :, :n], AF.Exp, scale=SCALE_MLA)
                            pend.append((h, r0, n, j, npair, kt2, pT, pv))
                            if len(pend) > LOOK:
                                emit_pv2(pend.pop(0))
                while pend:
                    emit_pv2(pend.pop(0))
            k.barrier()
            es_mla_pre.close()
            if stop_here("mla%d" % l):
                break
            with ExitStack() as s:
                wkT = k.sb([128, 2, 2, NT], BF16, es=s)
                k.memset(wkT, 0.0, e="pool")
                for hk_ in range(2):
                    for g_ in range(2):
                        k.dma(wkT[g_ * 64:(g_ + 1) * 64, hk_, g_, :], WKT[hk_, g_ * 64:(g_ + 1) * 64, :])
                wqT = k.sb([128, 2, NT], BF16, es=s)
                k.dma(wqT, TD(WQT, "c p t -> p c t"))
                wv3 = k.sb([128, NTILE, 4, 128], BF16, es=s)
                WVv = T(TMo.ap[:, 768:1024].rearrange("(t p) (q d) -> p t q d", p=128, d=64), TMo.key)
                for par in range(2):
                    k.memset(wv3[:, :, par::2, (1 - par) * 64:(2 - par) * 64], 1.0, e=("dve" if par == 0 else "pool"))
                    for q_ in range(par, 4, 2):
                        k.dma(wv3[:, :, q_, par * 64:(par + 1) * 64], WVv[:, :, q_, :])
                esink = k.sb([128, 4], F32, es=s)
                k.act(esink, small[:, sm0 + 11:sm0 + 15], AF.Exp)
                rd_sets = []
                for par in range(2):
                    tl = [k.sb([128, 128], F32, es=s) for _ in range(2)]
                    for t_ in tl:
                        k.memset(t_, 0.0)
                    rd_sets.append(Rot(tl))
                pT_r = Rot([k.sb([128, 5, 128], BF16, es=s) for _ in range(3)])
                pss = Rot([k.ps([128, 512], F32, es=s) for _ in range(4)])
                pacc = Rot([k.ps([128, 512], F32, es=s) for _ in range(4)])
                sw_r = Rot([k.sb([128, 128], F32, es=s) for _ in range(4)])
                yp_r = Rot([k.sb([128, 128], BF16, es=s) for _ in range(4)])
                pend = []

                def emit_pvw(item):
                    n, qh, keys, pT, pv, yp = item
                    hk, g = qh // 2, qh % 2
                    off = g * 64
                    dof = 64 - off
                    c0 = n * 128
                    nk = len(keys)
                    for i, (kt, msk) in enumerate(keys):
                        k.mm(pv[:, 0:128], wv3[:, kt, qh, :], pT[:, i, :], start=(i == 0), stop=(i == nk - 1))
                    rd = rd_sets[g].get()
                    k.ts(rd[dof:dof + 64, :], pv[dof:dof + 64, 0:128], esink[dof:dof + 64, qh:qh + 1], ALU.add)
                    k.op("dve", lambda e: e.reciprocal(rd.ap[dof:dof + 64, :], rd.ap[dof:dof + 64, :]), reads=[rd], writes=[rd])
                    sws = sw_r.get()
                    k.dma(sws[off:off + 64, :], rd[dof:dof + 64, :])
                    pendF.append((n, qh, pv, yp, sws))
                    if len(pendF) > 1:
                        emit_fin(pendF.pop(0))

                def emit_fin(item):
                    n, qh, pv, yp, sws = item
                    hk, g = qh // 2, qh % 2
                    off = g * 64
                    c0 = n * 128
                    k.tt(yp[off:off + 64, :], pv[off:off + 64, 0:128], sws[off:off + 64, :], ALU.mult)
                    if g == 1:
                        k.dma(YT.sl(("win", hk, n), np.s_[6 + hk, :, c0:c0 + 128]), yp)
                pendF = []
                yp = None
                for n in range(0 if need_ctx else 2, NTILE):
                    c0 = n * 128
                    if n < 2:
                        keys = [(0, None), (1, None)]
                    else:
                        keys = []
                        if n - 1 >= 2:
                            keys.append((n - 1, mprev_b))
                        keys.append((n, None))
                        if n + 1 < NTILE:
                            keys.append((n + 1, mnext_b))
                        keys += [(0, None), (1, None)]
                    for qh in range(4):
                        hk, g = qh // 2, qh % 2
                        pv = pacc.get()
                        if g == 0:
                            yp = yp_r.get()
                        psA = pss.get()
                        psB = pss.get() if len(keys) > 4 else None
                        for i, (kt, msk) in enumerate(keys):
                            dst = psA[:, i * 128:(i + 1) * 128] if i < 4 else psB[:, 0:128]
                            k.mm(dst, wkT[:, hk, g, kt * 128:(kt + 1) * 128], wqT[:, hk, c0:c0 + 128])
                        pT = pT_r.get()
                        na = min(4, len(keys))
                        k.act(T(pT.ap[:, 0:na, :].rearrange("p a c -> p (a c)"), pT.key), psA[:, 0:na * 128], AF.Exp, scale=0.125)
                        if psB is not None:
                            k.act(pT[:, 4, :], psB[:, 0:128], AF.Exp, scale=0.125)
                        for i, (kt, msk) in enumerate(keys):
                            if msk is not None:
                                k.tt(pT[:, i, :], pT[:, i, :], msk, ALU.mult)
                        pend.append((n, qh, keys, pT, pv, yp))
                        if len(pend) > 1:
                            emit_pvw(pend.pop(0))
                while pend:
                    emit_pvw(pend.pop(0))
                while pendF:
                    emit_fin(pendF.pop(0))
            k.barrier()
            if stop_here("win%d" % l):
                break
            es_aff = ExitStack()
            aff_e = k.sb([16, NT], F32, "aff_e", es=es_aff)
            with ExitStack() as s:
                w_out = k.sb([128, 8, D], BF16, es=s)
                stg = Rot([k.sb([128, D], F32, es=s) for _ in range(2)])
                for kc in range(8):
                    load_cast(w_out[:, kc, :], w_out_d[l, kc * 128:(kc + 1) * 128, :], stg, e=("pool" if kc % 2 else "dve"))
                rw = k.sb([128, 8, 16], F32, es=s)
                k.dma(rw, TD(router_d[l], "(kc p) e -> p kc e", p=128))
                sts = [0, 1] if need_ctx else [1]
                mod2 = {st: load_mod(st, 2, s) for st in sts}
                gs2 = {st: load_gs(st, 4, l * 2 + 1, s) for st in sts}
                sh2 = {st: load_mod(st, 3, s) for st in sts}
                yT_r = Rot([k.sb([128, 8, 128], BF16, es=s) for _ in range(2)])
                xt_r = Rot([k.sb([128, D], F32, es=s) for _ in range(2)])
                xn_r = Rot([k.sb([128, D], F32, es=s) for _ in range(2)])
                xs_r = Rot([k.sb([128, D], F32, es=s) for _ in range(3)])
                h2b_r = Rot([k.sb([128, D], BF16, es=s) for _ in range(2)])
                h2T_r = Rot([k.sb([128, 8, 128], F32, es=s) for _ in range(2)])
                junk = k.sb([128, D], F32, es=s)
                ss_pool = Rot([k.sb([128, 1], F32, es=s) for _ in range(8)])
                ex_r = Rot([k.sb([128, 16], F32, es=s) for _ in range(2)])
                pso = Rot([k.ps([128, 512], F32, es=s) for _ in range(3)])
                ptr = Rot([k.ps([128, 4, 128], F32, es=s) for _ in range(2)])
                psl = Rot([k.ps([128, 512], F32, es=s) for _ in range(2)])
                def phaseA(n):
                    st = 0 if n < 2 else 1
                    c0 = n * 128
                    yT = yT_r.get()
                    k.dma(yT, TD(YT, "c p t -> p c t")[:, :, c0:c0 + 128])
                    xt = xt_r.get()
                    k.dma(xt, rows_key(Xsrc, n)[c0:c0 + 128, :])
                    xn = xn_r.get()
                    for hf in range(2):
                        ps = pso.get()
                        for kc in range(8):
                            k.mm(ps, yT[:, kc, :], w_out[:, kc, hf * 512:(hf + 1) * 512], start=(kc == 0), stop=(kc == 7))
                        sl_ = np.s_[:, hf * 512:(hf + 1) * 512]
                        k.tt(xn[sl_], ps, mod2[st][sl_], ALU.mult)
                        k.tt(xn[sl_], xn[sl_], xt[sl_], ALU.add)
                    k.dma(rows_key(X, n)[c0:c0 + 128, :], xn, q="pool")
                    ss = ss_pool.get()
                    k.act(junk, xn, AF.Square, accum=ss)
                    rstd = ss_pool.get()
                    k.rsq(rstd, ss, 1.0 / D, EPS)
                    xs = xs_r.get()
                    k.stt(xs, xn, rstd, gs2[st], ALU.mult, ALU.mult)
                    k.tt(xs, xs, sh2[st], ALU.add)
                    h2b = h2b_r.get()
                    k.copy(h2b, xs, e="act")
                    k.dma(H2.sl(n, np.s_[c0:c0 + 128, :]), h2b, q="pool")
                    return xs

                def phaseB(n, xs):
                    c0 = n * 128
                    h2T = h2T_r.get()
                    for hf in range(2):
                        pt = ptr.get()
                        for q4 in range(4):
                            kc = hf * 4 + q4
                            k.tr(pt[:, q4, :], xs[:, kc * 128:(kc + 1) * 128], ident)
                        k.copy(h2T[:, hf * 4:(hf + 1) * 4, :], pt, e="act")
                    pl = psl.get()
                    for kc in range(8):
                        k.mm(pl[:, 0:16], h2T[:, kc, :], rw[:, kc, :], start=(kc == 0), stop=(kc == 7))
                    ex = ex_r.get()
                    sm_ = ss_pool.get()
                    k.act(ex, pl[:, 0:16], AF.Exp, accum=sm_)
                    k.op("dve", lambda e: e.reciprocal(sm_.ap, sm_.ap), reads=[sm_], writes=[sm_])
                    k.ts(affT.sl(n, np.s_[:, n, :]), ex, sm_, ALU.mult)
                    pl2 = psl.get()
                    k.tr(pl2[0:16, 0:128], affT.sl(n, np.s_[:, n, :]), ident)
                    k.copy(aff_e.sl(n, np.s_[:, c0:c0 + 128]), pl2[0:16, 0:128], e="act")
                tiles_o = list(range(0 if need_ctx else 2, NTILE))
                pxs = phaseA(tiles_o[0])
                for ti_, n in enumerate(tiles_o):
                    nxs = phaseA(tiles_o[ti_ + 1]) if ti_ + 1 < len(tiles_o) else None
                    phaseB(n, pxs)
                    pxs = nxs
            k.barrier()
            if stop_here("o%d" % l):
                es_aff.close()
                break
            streams = ([(0, 2, 32)] if need_ctx else []) + [(2, 32, 512)]
            with ExitStack() as s:
                work = k.sb([16, NTX], F32, es=s)
                m8 = k.sb([16, 8], F32, es=s)
                thr = k.sb([16, 1], F32, es=s)
                aff128 = k.sb([128, 512], F32, es=s)
                junk16 = k.sb([128, 512], BF16, es=s)
                lo = k.sb([128, 1], F32, es=s)
                hi = k.sb([128, 1], F32, es=s)
                mid = k.sb([128, 1], F32, es=s)
                cnt = k.sb([128, 1], F32, es=s)
                half = k.sb([128, 1], F32, es=s)
                k.memset(half, 0.5)
                mge = k.sb([128, 1], U32, es=s)
                mlt = k.sb([128, 1], U32, es=s)
                thr16 = k.sb([16, 8], F32, es=s)
                mask_e = k.sb([16, NTX], F32, es=s)
                maskTb = k.sb([128, 32, 16], BF16, es=s)
                carry = k.sb([128, 16], F32, es=s)
                pos_r = Rot([k.sb([128, 16], F32, es=s) for _ in range(2)])
                ptk = Rot([k.ps([128, 512], F32, es=s) for _ in range(4)])
                ahi = k.sb([128, NTILE, 16], BF16, es=s)
                alo = k.sb([128, NTILE, 16], F32, es=s)
                k.copy(ahi, affT)
                k.tt(alo, affT, ahi, ALU.subtract)
                k.copy(Rtab[:, :, :, 2], ahi, e="pool")
                k.copy(Rtab[:, :, :, 3], alo, e="pool")
                for (t0, ntl, cap) in streams:
                    ntok = ntl * 128
                    cs = np.s_[:, t0 * 128:t0 * 128 + ntok]
                    if cap <= 64:
                        k.copy(work[:, :ntok], aff_e[cs])
                        for r in range(cap // 8):
                            k.op("dve", lambda e: e.max(out=m8.ap, in_=work.ap[:, :ntok]), reads=[work], writes=[m8])
                            if r < cap // 8 - 1:
                                k.op("dve", lambda e: e.match_replace(out=work.ap[:, :ntok], in_to_replace=m8.ap,
                                                                      in_values=work.ap[:, :ntok], imm_value=-1.0),
                                     reads=[work, m8], writes=[work])
                        k.copy(thr, m8[:, 7:8])
                    else:
                        k.dma(AFFD, aff_e[cs])
                        k.dma(aff128, TD(AFFD, "e (s t) -> (e s) t", s=8))
                        k.memset(lo, 0.0)
                        k.memset(hi, 2.0)
                        for it in range(40):
                            k.stt(mid, lo, hi, half, ALU.add, ALU.mult)
                            k.ts(junk16, aff128, mid, ALU.is_ge, 0.0, ALU.add, accum=cnt)
                            pc = ptk.get()
                            k.mm(pc[:, 0:1], C("bmat"), cnt)
                            k.ts(mge, pc[:, 0:1], cap - 0.5, ALU.is_ge)
                            k.ts(mlt, pc[:, 0:1], cap - 0.5, ALU.is_lt)
                            k.op("dve", lambda e: e.copy_predicated(lo.ap, mge.ap, mid.ap), reads=[mge, mid], writes=[lo])
                            k.op("dve", lambda e: e.copy_predicated(hi.ap, mlt.ap, mid.ap), reads=[mlt, mid], writes=[hi])
                        k.dma(THRD, lo)
                        k.dma(thr16, TD(THRD, "(e s) o -> e (s o)", s=8))
                        k.copy(thr, thr16[:, 0:1])
                    k.ts(mask_e[:, :ntok], aff_e[cs], thr, ALU.is_ge)
                    k.memset(carry, 0.0)
                    for i in range(ntl):
                        n = t0 + i
                        pt = ptk.get()
                        k.tr(pt[:, 0:16], mask_e[:, i * 128:(i + 1) * 128], ident[0:16, 0:16])
                        k.copy(maskTb[:, i, :], pt[:, 0:16], e="act")
                        pc = ptk.get()
                        k.mm(pc[:, 0:16], triU_b, maskTb[:, i, :])
                        k.mm(pc[:, 16:32], ones_b, maskTb[:, i, :])
                        pos = pos_r.get()
                        k.tt(pos, pc[:, 0:16], carry, ALU.add)
                        k.tt(pos, pos, maskTb[:, i, :], ALU.mult)
                        k.ts(posm.sl(n, np.s_[:, n, :]), pos, -1.0, ALU.add)
                        k.tt(carry, carry, pc[:, 16:32], ALU.add)
            k.barrier()
            if stop_here("topk%d" % l):
                es_aff.close()
                break
            es_aff.close()
            with ExitStack() as s:
                mod5 = {st: load_mod(st, 5, s) for st in ([0, 1] if need_ctx else [1])}
                wsets = Rot([(k.sb([128, 8, 768], BF16, es=s), k.sb([128, 8, 768], BF16, es=s), k.sb([128, 6, D], BF16, es=s))
                             for _ in range(2)])
                stg = Rot([k.sb([128, 768], F32, es=s) for _ in range(4)])
                Sel = k.sb([128, 32, 512], BF16, es=s)
                iota16 = k.sb([128, 512], mybir.dt.int16, es=s)
                k.copy(iota16, C("iota"))
                r4T_r = Rot([k.sb([4, 512], F32, es=s) for _ in range(2)])
                r4_r = Rot([k.sb([128, 16], F32, es=s) for _ in range(3)])
                idx_r = Rot([k.sb([128, 1], I32, es=s) for _ in range(16)])
                idf_r = Rot([k.sb([128, 1], F32, es=s) for _ in range(4)])
                gt_r = Rot([k.sb([128, 1], F32, es=s) for _ in range(16)])
                xs_r = Rot([k.sb([128, D], BF16, es=s) for _ in range(10)])
                xsT_r = Rot([k.sb([128, 8, 512], BF16, es=s) for _ in range(2)])
                hid = k.sb([128, 6, 512], BF16, es=s)
                sg_r = Rot([k.sb([128, 512], F32, es=s) for _ in range(2)])
                ys_r = Rot([k.sb([128, D], F32, es=s) for _ in range(2)])
                p4 = Rot([k.ps([128, 512], F32, es=s) for _ in range(1)])
                ptb = Rot([k.ps([128, 8, 128], BF16, es=s) for _ in range(1)])
                pgu = Rot([k.ps([128, 512], F32, es=s) for _ in range(4)])
                pdn = Rot([k.ps([128, 512], F32, es=s) for _ in range(2)])
                Xall = [rows_key(X, n) for n in range(NTILE)] + [X.sub("all")]
                cast_eng = Rot(["act", "dve"])
                wcache = {}

                wgen = {"g": None}

                def weights_gen(e_, wg, wu, wd):
                    for kc in range(8):
                        load_cast(wg[:, kc, :], wg_d[l, e_, kc * 128:(kc + 1) * 128, :], stg, e=cast_eng.get())
                        yield
                        load_cast(wu[:, kc, :], wu_d[l, e_, kc * 128:(kc + 1) * 128, :], stg, e=cast_eng.get())
                        yield
                    for fc in range(6):
                        for hf_ in range(2):
                            load_cast(wd[:, fc, hf_ * 512:(hf_ + 1) * 512], wd_d[l, e_, fc * 128:(fc + 1) * 128, hf_ * 512:(hf_ + 1) * 512], stg, e=cast_eng.get())
                            yield

                def feed(n_):
                    for _ in range(n_):
                        if wgen["g"] is None:
                            return
                        try:
                            next(wgen["g"])
                        except StopIteration:
                            wgen["g"] = None

                def weights(e_, now=False):
                    feed(10000)
                    wg, wu, wd = wsets.get()
                    wcache[e_] = (wg, wu, wd)
                    wgen["g"] = weights_gen(e_, wg, wu, wd)
                    if now:
                        feed(10000)

                def selbuild(u):
                    e_, (t0, ntl, cap) = u
                    for i in range(ntl):
                        k.ts(Sel[:, i, :cap], iota16[:, :cap], posm[:, t0 + i, e_:e_ + 1], ALU.is_equal)

                def idxpart(u):
                    e_, (t0, ntl, cap) = u
                    ps = p4.get()
                    for i in range(ntl):
                        k.mm(ps[0:4, :cap], Rtab[:, t0 + i, e_, :], Sel[:, i, :cap], start=(i == 0), stop=(i == ntl - 1))
                    r4T = r4T_r.get()
                    k.copy(r4T[:, :cap], ps[0:4, :cap])
                    stiles = [(s0, min(128, cap - s0)) for s0 in range(0, cap, 128)]
                    for si, (s0, nsl) in enumerate(stiles):
                        k.tr(ps[0:nsl, si * 4:(si + 1) * 4], r4T[0:4, s0:s0 + nsl], ident[0:4, 0:4])
                    nsl0 = stiles[0][1]
                    r4 = r4_r.get()
                    k.copy(r4[0:nsl0, 0:4 * len(stiles)], ps[0:nsl0, 0:4 * len(stiles)])
                    meta = []
                    xss = []
                    for si, (s0, nsl) in enumerate(stiles):
                        c4 = si * 4
                        idf = idf_r.get()
                        k.stt(idf[0:nsl, :], r4[0:nsl, c4:c4 + 1], 64.0, r4[0:nsl, c4 + 1:c4 + 2], ALU.mult, ALU.add)
                        idx = idx_r.get()
                        k.copy(idx[0:nsl, :], idf[0:nsl, :])
                        gt = gt_r.get()
                        k.tt(gt[0:nsl, :], r4[0:nsl, c4 + 2:c4 + 3], r4[0:nsl, c4 + 3:c4 + 4], ALU.add)
                        meta.append((idx, gt))
                        xs = xs_r.get()
                        k.dma(xs[0:nsl, :], H2, q="pool", reads=[idx],
                              fn=lambda en: en.indirect_dma_start(out=xs.ap[0:nsl, :], out_offset=None, in_=H2.ap,
                                                                  in_offset=bass.IndirectOffsetOnAxis(ap=idx.ap[0:nsl, :], axis=0)))
                        xss.append(xs)
                    return [u, stiles, meta, xss, None]

                def xtrans(item):
                    u, stiles, meta, xss, _ = item
                    xsT = xsT_r.get()
                    for si, (s0, nsl) in enumerate(stiles):
                        xs = xss[si]
                        pt = ptb.get()
                        for kc in range(8):
                            k.tr(pt[:, kc, 0:nsl], xs[0:nsl, kc * 128:(kc + 1) * 128], ident_b[0:nsl, 0:nsl])
                        k.copy(xsT[:, :, s0:s0 + nsl], pt[:, :, 0:nsl], e="act")
                    item[4] = xsT

                def ffn(item):
                    (e_, (t0, ntl, cap)), stiles, meta, xss, xsT = item
                    wg, wu, wd = wcache[e_]
                    for fc in range(6):
                        pg = pgu.get()
                        pu = pgu.get()
                        for kc in range(8):
                            k.mm(pg[:, :cap], wg[:, kc, fc * 128:(fc + 1) * 128], xsT[:, kc, :cap], start=(kc == 0), stop=(kc == 7))
                        for kc in range(8):
                            k.mm(pu[:, :cap], wu[:, kc, fc * 128:(fc + 1) * 128], xsT[:, kc, :cap], start=(kc == 0), stop=(kc == 7))
                        sg = sg_r.get()
                        k.act(sg[:, :cap], pg[:, :cap], AF.Silu)
                        k.tt(hid[:, fc, :cap], sg[:, :cap], pu[:, :cap], ALU.mult)
                        feed(2)

                def down(item):
                    (e_, (t0, ntl, cap)), stiles, meta, xss, xsT = item
                    st = 0 if t0 == 0 else 1
                    wg, wu, wd = wcache[e_]
                    for si, (s0, nsl) in enumerate(stiles):
                        idx, gt = meta[si]
                        ys = ys_r.get()
                        for hf in range(2):
                            pd = pdn.get()
                            for fc in range(6):
                                k.mm(pd[0:nsl, :], hid[:, fc, s0:s0 + nsl], wd[:, fc, hf * 512:(hf + 1) * 512], start=(fc == 0), stop=(fc == 5))
                            k.stt(ys[0:nsl, hf * 512:(hf + 1) * 512], pd[0:nsl, :], gt[0:nsl, 0:1],
                                  mod5[st][0:nsl, hf * 512:(hf + 1) * 512], ALU.mult, ALU.mult)
                            feed(2)
                        k.dma(X, ys[0:nsl, :], q="pool", reads=[ys, idx], writes=Xall,
                              fn=lambda en: en.indirect_dma_start(out=X.ap, out_offset=bass.IndirectOffsetOnAxis(ap=idx.ap[0:nsl, :], axis=0),
                                                                  in_=ys.ap[0:nsl, :], in_offset=None, compute_op=ALU.add))

                units = [(e_, stm) for e_ in range(16) for stm in reversed(streams)]
                NU = len(units)
                weights(0, now=True)
                weights(1, now=True)
                nxt_w = 2
                selbuild(units[0])
                items = {0: idxpart(units[0])}
                if NU > 1:
                    selbuild(units[1])
                    items[1] = idxpart(units[1])
                xtrans(items[0])
                if NU > 2:
                    selbuild(units[2])
                for ui in range(NU):
                    ffn(items[ui])
                    if ui + 1 < NU:
                        xtrans(items[ui + 1])
                    if ui + 2 < NU:
                        items[ui + 2] = idxpart(units[ui + 2])
                    down(items[ui])
                    if ui + 3 < NU:
                        selbuild(units[ui + 3])
                    e_done = units[ui][0]
                    if (ui + 1 == NU or units[ui + 1][0] != e_done) and nxt_w < 16:
                        weights(nxt_w)
                        nxt_w += 1
                    del items[ui]
            k.barrier()
            if stop_here("exp%d" % l):
                break
        else:
            with ExitStack() as s:
                gfin = k.sb([128, D], F32, es=s)
                k.dma(gfin, g_bc_d[4])
                xt_r = Rot([k.sb([128, D], F32, es=s) for _ in range(5)])
                junk = k.sb([128, D], F32, es=s)
                ss_pool = Rot([k.sb([128, 1], F32, es=s) for _ in range(12)])
                def finA(n):
                    c0 = n * 128
                    xt = xt_r.get()
                    k.dma(xt, rows_key(X, n)[c0:c0 + 128, :])
                    ss = ss_pool.get()
                    k.act(junk, xt, AF.Square, accum=ss)
                    return (xt, ss)

                def finB(n, c_):
                    xt, ss = c_
                    c0 = n * 128
                    rstd = ss_pool.get()
                    k.rsq(rstd, ss, 1.0 / D, EPS)
                    k.stt(xt, xt, rstd, gfin, ALU.mult, ALU.mult)
                    k.dma(out_d.sl(n, np.s_[c0 - NTC:c0 - NTC + 128, :]), xt, q="pool")
                tl_ = list(range(2, NTILE))
                q_ = [finA(tl_[0]), finA(tl_[1])]
                for i_, n in enumerate(tl_):
                    if i_ + 2 < len(tl_):
                        q_.append(finA(tl_[i_ + 2]))
                    finB(n, q_.pop(0))
        k.barrier()
        print("ninst", k.ninst, k.cnt, flush=True)
    return nc


_NC_CACHE = {}


def kernel(**inputs):
    inp = {kk: np.asarray(v) for kk, v in inputs.items()}
    shared = _prep_shared(inp)
    if "nc" not in _NC_CACHE:
        _NC_CACHE["nc"] = build()
    nc = _NC_CACHE["nc"]
    in_maps = []
    for b in range(8):
        m = dict(shared)
        m.update(_prep_core(inp, b))
        in_maps.append(m)
    res = run_bass_kernel_spmd(nc, in_maps, core_ids=list(range(8)))
    out = np.stack([np.asarray(r["out"], dtype=np.float32) for r in res.results], 0)
    return out
```

```python
import numpy as np
from contextlib import ExitStack
import concourse.bass as bass
import concourse.mybir as mybir
from concourse.bass_utils import run_bass_kernel_spmd

F32 = mybir.dt.float32
BF16 = mybir.dt.bfloat16
I32 = mybir.dt.int32
U32 = mybir.dt.uint32
AF = mybir.ActivationFunctionType
ALU = mybir.AluOpType
AX = mybir.AxisListType


class T:
    __slots__ = ("ap", "key")

    def __init__(self, ap, key):
        self.ap = ap
        self.key = key

    def __getitem__(self, idx):
        return T(self.ap[idx], self.key)

    def sub(self, suffix):
        return T(self.ap, (self.key, suffix))

    def sl(self, suffix, idx):
        return T(self.ap[idx], (self.key, suffix))


def _key(x):
    return x.key if isinstance(x, T) else x


class KB:
    CE = ("pe", "act", "dve", "pool")

    def __init__(self, nc, es, nds=14):
        self.nc = nc
        self.es = es
        self.E = {"pe": nc.tensor, "act": nc.scalar, "dve": nc.vector,
                  "pool": nc.gpsimd, "sp": nc.sync}
        self.csem = {e: es.enter_context(nc.semaphore("c_" + e)) for e in self.CE}
        self.cnt = {e: 0 for e in self.CE}
        self.NDS = nds
        self.dsem = [es.enter_context(nc.semaphore("d%d" % i)) for i in range(nds)]
        self.dcnt = [0] * nds
        self.dnext = 0
        self.waited = {e: {} for e in self.E}
        self.lastw = {}
        self.readers = {}
        self.nalloc = 0
        self.ninst = 0

    def sb(self, shape, dtype, name=None, es=None):
        self.nalloc += 1
        name = name or ("t%d" % self.nalloc)
        t = (es or self.es).enter_context(self.nc.sbuf_tensor(name + "_%d" % self.nalloc, list(shape), dtype))
        return T(t[:], name + "_%d" % self.nalloc)

    def ps(self, shape, dtype, name=None, es=None):
        self.nalloc += 1
        name = name or ("p%d" % self.nalloc)
        t = (es or self.es).enter_context(self.nc.psum_tensor(name + "_%d" % self.nalloc, list(shape), dtype))
        return T(t[:], name + "_%d" % self.nalloc)

    def dram(self, name, shape, dtype, kind="Internal"):
        t = self.nc.dram_tensor(name, list(shape), dtype, kind=kind)
        return T(t.ap(), name)

    def _semobj(self, semkey):
        return self.csem[semkey[1]] if semkey[0] == "c" else self.dsem[semkey[1]]

    def _wait(self, e, semkey, val):
        if self.waited[e].get(semkey, 0) >= val:
            return
        self.waited[e][semkey] = val
        self.E[e].wait_ge(self._semobj(semkey), val)

    def _deps(self, e, reads, writes, is_dma):
        for r in reads:
            lw = self.lastw.get(_key(r))
            if lw is not None:
                self._wait(e, lw[0], lw[1])
        for w in writes:
            k = _key(w)
            lw = self.lastw.get(k)
            if lw is not None:
                if not (lw[0] == ("c", e) and e == "pe" and not is_dma):
                    self._wait(e, lw[0], lw[1])
            for sk, v in self.readers.get(k, {}).items():
                if sk == ("c", e) and e == "pe" and not is_dma:
                    continue
                self._wait(e, sk, v)

    def _record(self, tok, reads, writes):
        for r in reads:
            d = self.readers.setdefault(_key(r), {})
            if d.get(tok[0], 0) < tok[1]:
                d[tok[0]] = tok[1]
        for w in writes:
            k = _key(w)
            self.lastw[k] = tok
            self.readers[k] = {}

    def op(self, e, fn, reads=(), writes=()):
        self._deps(e, reads, writes, False)
        ins = fn(self.E[e])
        self.cnt[e] += 1
        ins.then_inc(self.csem[e], 1)
        self._record((("c", e), self.cnt[e]), reads, writes)
        self.ninst += 1
        return ins

    def dma(self, out, in_, q="sp", fn=None, reads=None, writes=None, **kw):
        reads = [in_] if reads is None else reads
        writes = [out] if writes is None else writes
        slot = self.dnext
        self.dnext = (slot + 1) % self.NDS
        if self.dcnt[slot] > 0:
            self._wait(q, ("d", slot), 16 * self.dcnt[slot])
        self._deps(q, reads, writes, True)
        if fn is None:
            ins = self.E[q].dma_start(out=out.ap, in_=in_.ap, **kw)
        else:
            ins = fn(self.E[q])
        self.dcnt[slot] += 1
        ins.then_inc(self.dsem[slot], 16)
        self._record((("d", slot), 16 * self.dcnt[slot]), reads, writes)
        self.ninst += 1
        return ins

    def barrier(self):
        for e in self.E:
            for e2 in self.CE:
                if e2 != e and self.cnt[e2] > 0:
                    self._wait(e, ("c", e2), self.cnt[e2])
            for s in range(self.NDS):
                if self.dcnt[s] > 0:
                    self._wait(e, ("d", s), 16 * self.dcnt[s])

    def mm(self, out, lhsT, rhs, start=True, stop=True, extra_reads=()):
        return self.op("pe", lambda e: e.matmul(out.ap, lhsT.ap, rhs.ap, start=start, stop=stop),
                       reads=[lhsT, rhs, *extra_reads], writes=[out])

    def tr(self, out, in_, ident):
        return self.op("pe", lambda e: e.transpose(out.ap, in_.ap, ident.ap),
                       reads=[in_, ident], writes=[out])

    def act(self, out, in_, func, bias=None, scale=None, accum=None, e="act"):
        kw = {}
        rd = [in_]
        wr = [out]
        if bias is not None:
            if isinstance(bias, T):
                kw["bias"] = bias.ap
                rd.append(bias)
            else:
                kw["bias"] = bias
        if scale is not None:
            if isinstance(scale, T):
                kw["scale"] = scale.ap
                rd.append(scale)
            else:
                kw["scale"] = scale
        if accum is not None:
            kw["accum_out"] = accum.ap
            wr.append(accum)
        return self.op(e, lambda en: en.activation(out.ap, in_.ap, func, **kw), reads=rd, writes=wr)

    def tt(self, out, a, b, op, e="dve"):
        return self.op(e, lambda en: en.tensor_tensor(out.ap, a.ap, b.ap, op), reads=[a, b], writes=[out])

    def ts(self, out, a, s1, op0, s2=None, op1=None, e="dve", accum=None):
        rd = [a]
        wr = [out]
        v1 = s1
        v2 = s2
        if isinstance(s1, T):
            rd.append(s1)
            v1 = s1.ap
        if isinstance(s2, T):
            rd.append(s2)
            v2 = s2.ap
        kw = {}
        if op1 is not None:
            kw["op1"] = op1
        if accum is not None:
            kw["accum_out"] = accum.ap
            wr.append(accum)
        return self.op(e, lambda en: en.tensor_scalar(out.ap, a.ap, v1, v2, op0, **kw), reads=rd, writes=wr)

    def stt(self, out, a, s, b, op0, op1, e="dve"):
        rd = [a, b]
        v = s
        if isinstance(s, T):
            rd.append(s)
            v = s.ap
        return self.op(e, lambda en: en.scalar_tensor_tensor(out.ap, a.ap, v, b.ap, op0, op1), reads=rd, writes=[out])

    def copy(self, out, in_, e="dve"):
        if e == "act":
            return self.op(e, lambda en: en.activation(out.ap, in_.ap, AF.Copy), reads=[in_], writes=[out])
        return self.op(e, lambda en: en.tensor_copy(out.ap, in_.ap), reads=[in_], writes=[out])

    def memset(self, out, v, e="dve"):
        return self.op(e, lambda en: en.memset(out.ap, v), reads=[], writes=[out])

    def rsq(self, dst, src, mul, add):
        self.ts(dst, src, mul, ALU.mult, add, ALU.add)
        self.act(dst, dst, AF.Sqrt)
        self.op("dve", lambda e: e.reciprocal(dst.ap, dst.ap), reads=[dst], writes=[dst])

L_ = 2
D = 1024
NTX = 4096
NTC = 256
NT = NTX + NTC
NTILE = NT // 128
NCOL = 2464
SCALE_MLA = float((64 + 32) ** -0.5)
LN2 = float(np.log(2.0))


def _rope_tables():
    t = np.arange(NTX)
    row = (t // 64).astype(np.float32)
    col = (t % 64).astype(np.float32)

    def tab(dh_half):
        dh = dh_half
        inv = 10000.0 ** (-np.arange(0, dh, 2, dtype=np.float32) / dh)
        return inv

    def build(dtot):
        half = dtot // 2
        inv = tab(half)
        nf = half // 2
        cos = np.zeros((dtot, NTX), np.float32)
        sins = np.zeros((dtot, NTX), np.float32)
        perm = np.zeros((dtot, dtot), np.float32)
        for part, pos in ((0, row), (1, col)):
            base = part * half
            ang = pos[None, :] * inv[:, None]
            c, s = np.cos(ang), np.sin(ang)
            for j in range(nf):
                cos[base + j] = c[j]
                cos[base + nf + j] = c[j]
                sins[base + j] = -s[j]
                sins[base + nf + j] = s[j]
                perm[base + nf + j, base + j] = 1.0
                perm[base + j, base + nf + j] = 1.0
        return cos, sins, perm

    c32, s32, p32 = build(32)
    c64, s64, p64 = build(64)
    c128 = np.concatenate([c64, c64], 0)
    s128 = np.concatenate([s64, s64], 0)
    p128 = np.zeros((128, 128), np.float32)
    p128[:64, :64] = p64
    p128[64:, 64:] = p64
    rope32 = np.stack([np.tile(c32, (4, 1)), np.tile(s32, (4, 1))]).astype(np.float32)
    rope128 = np.stack([c128, s128]).astype(np.float32)
    return rope32, rope128, p32, p128


_CST = {}


def _cst_layout():
    off = 0
    for name, w in (("ident", 128), ("A", 128), ("B", 128), ("C1", 128), ("C2", 128),
                    ("colK1", 1), ("colK2", 1), ("iota", 512), ("triU", 128),
                    ("mprev", 128), ("mnext", 128), ("p128", 128), ("p32", 32), ("pswap", 128), ("bmat", 128), ("p32x4", 128),
                    ("tokid", NTILE * 16 * 2)):
        _CST[name] = (off, w)
        off += w
    return off


NCST = _cst_layout()


def _const_table():
    rope32, rope128, p32, p128 = _rope_tables()
    cst = np.zeros((128, NCST), np.float32)
    p = np.arange(128, dtype=np.float32)[:, None]
    c = np.arange(128, dtype=np.float32)[None, :]

    def put(name, arr):
        o, w = _CST[name]
        cst[:arr.shape[0], o:o + w] = arr
    put("ident", np.eye(128, dtype=np.float32))
    put("A", np.maximum(c - p, 0.0))
    put("B", np.maximum(p - c, 0.0))
    put("C1", np.broadcast_to(c + 1.0, (128, 128)))
    put("C2", np.broadcast_to(128.0 - c, (128, 128)))
    put("colK1", 127.0 - p)
    put("colK2", p)
    put("iota", np.broadcast_to(np.arange(512, dtype=np.float32)[None, :], (128, 512)))
    put("triU", (p <= c).astype(np.float32))
    put("mprev", (p >= c).astype(np.float32))
    put("mnext", (p <= c).astype(np.float32))
    put("p128", p128)
    put("p32", p32)
    p32x4 = np.zeros((128, 128), np.float32)
    for i_ in range(4):
        p32x4[i_ * 32:(i_ + 1) * 32, i_ * 32:(i_ + 1) * 32] = p32
    put("p32x4", p32x4)
    put("pswap", (np.arange(128)[:, None] == (np.arange(128)[None, :] + 64) % 128).astype(np.float32))
    put("bmat", (np.arange(128)[:, None] // 8 == np.arange(128)[None, :] // 8).astype(np.float32))
    rows = (np.arange(NTILE)[None, :] * 128 + np.arange(128)[:, None])
    tok = np.stack([rows // 64, rows % 64], -1).astype(np.float32)
    tok = np.broadcast_to(tok[:, :, None, :], (128, NTILE, 16, 2)).reshape(128, -1)
    put("tokid", tok)
    return cst, rope32, rope128


def _prep_shared(inp):
    f = lambda a: np.ascontiguousarray(a, dtype=np.float32)
    sh = {}
    w_in = inp["w_in"]
    s = np.cumsum([0, 256, 256, 256, 256, 256, 128, 32, 256, 128, 128])
    rq, rk, rv, rg, cq, ckv, kr, wq, wk, wv = [w_in[:, :, s[i]:s[i + 1]] for i in range(10)]
    wk2 = np.concatenate([wk[:, :, 0:64], wk[:, :, 0:64], wk[:, :, 64:128], wk[:, :, 64:128]], -1)
    wv2 = np.concatenate([wv[:, :, 0:64], wv[:, :, 0:64], wv[:, :, 64:128], wv[:, :, 64:128]], -1)
    sh["w_in_r"] = f(np.concatenate([rq, rk, cq, ckv, wq, wk2, kr, rk, rv, rg, wv2], -1))
    assert sh["w_in_r"].shape[-1] == NCOL
    uq = inp["mla_w_uq"]
    sh["w_uq_n"] = f(uq[:, :, :, :64].reshape(L_, 256, 512))
    sh["w_uq_r"] = f(uq[:, :, :, 64:].reshape(L_, 256, 256))
    sh["w_uk"] = f(inp["mla_w_uk"].reshape(L_, 128, 512))
    sh["w_uv"] = f(inp["mla_w_uv"].reshape(L_, 128, 512))
    sh["w_out"] = f(inp["w_out"])
    sh["router_w"] = f(inp["router_w"])
    sh["ada_w"] = f(inp["ada_w"])
    sh["ada_b"] = f(inp["ada_b"].reshape(L_, 1, 6 * D))
    sh["exp_wg"] = f(inp["exp_w_gate"])
    sh["exp_wu"] = f(inp["exp_w_up"])
    sh["exp_wd"] = f(inp["exp_w_down"])
    gb = np.stack([inp["norm1_g"][0], inp["norm2_g"][0], inp["norm1_g"][1], inp["norm2_g"][1], inp["final_g"]])
    sh["g_bc"] = f(np.broadcast_to(gb[:, None, :], (5, 128, D)))
    cols = []
    for l in range(L_):
        qg = inp["mla_qnorm_g"][l].reshape(2, 128).T
        kg = inp["mla_kvnorm_g"][l].reshape(1, 128).T
        df, db, sk = inp["ret_decay_f"][l], inp["ret_decay_b"][l], inp["win_sink"][l]
        rep = np.broadcast_to(np.concatenate([df, db, sk])[None, :], (128, 12))
        hp = (np.arange(128) >= 64).astype(np.int64)
        pp = np.stack([df[0 + hp], df[2 + hp], db[0 + hp], db[2 + hp]], -1)
        cols += [qg, kg, rep, pp]
    sh["small"] = f(np.concatenate(cols, -1))
    cst, rope32, rope128 = _const_table()
    sh["cst"] = cst
    sh["rope32"] = rope32
    sh["rope128"] = rope128
    return sh


def _prep_core(inp, b):
    f = lambda a: np.ascontiguousarray(a, dtype=np.float32)
    d = {}
    d["x0"] = f(np.concatenate([inp["ctx"][b], inp["x"][b]], 0))
    cv = np.stack([inp["c_ctx"], inp["c"][b]])
    cr = cv.reshape(2, 8, 128).transpose(0, 2, 1)
    d["crep"] = f(np.broadcast_to(cr[:, :, :, None], (2, 128, 8, 128)))
    return d

class Rot:
    def __init__(self, tiles):
        self.t = tiles
        self.i = 0

    def get(self):
        t = self.t[self.i % len(self.t)]
        self.i += 1
        return t


def TD(t, pattern, **kw):
    return T(t.ap.rearrange(pattern, **kw), t.key)


def build(upto=None, dbg=False, nlayers=L_):
    nc = bass.Bass("TRN2", target_bir_lowering=False)
    es0 = ExitStack()
    with es0:
        k = KB(nc, es0)
        kind_dbg = "ExternalOutput" if dbg else "Internal"
        din = lambda n, s, dt=F32: k.dram(n, s, dt, kind="ExternalInput")
        x0 = din("x0", [NT, D])
        crep_d = din("crep", [2, 128, 8, 128])
        w_in_d = din("w_in_r", [L_, D, NCOL])
        w_uq_n_d = din("w_uq_n", [L_, 256, 512])
        w_uq_r_d = din("w_uq_r", [L_, 256, 256])
        w_uk_d = din("w_uk", [L_, 128, 512])
        w_uv_d = din("w_uv", [L_, 128, 512])
        w_out_d = din("w_out", [L_, D, D])
        router_d = din("router_w", [L_, D, 16])
        ada_w_d = din("ada_w", [L_, D, 6 * D])
        ada_b_d = din("ada_b", [L_, 1, 6 * D])
        wg_d = din("exp_wg", [L_, 16, D, 768])
        wu_d = din("exp_wu", [L_, 16, D, 768])
        wd_d = din("exp_wd", [L_, 16, 768, D])
        g_bc_d = din("g_bc", [5, 128, D])
        small_d = din("small", [128, L_ * 19])
        cst_d = din("cst", [128, NCST])
        rope32_d = din("rope32", [2, 128, NTX])
        rope128_d = din("rope128", [2, 128, NTX])
        out_d = k.dram("out", [NTX, D], F32, kind="ExternalOutput")
        X = k.dram("X", [NT, D], F32, kind=kind_dbg)
        MODBC = k.dram("MODBC", [2, 6, 128, D], F32, kind=kind_dbg)
        QT = k.dram("QT", [2, 128, NT], BF16, kind=kind_dbg)
        KT = k.dram("KT", [2, 128, NT], BF16, kind=kind_dbg)
        TMo = k.dram("TMo", [NT, 1024], BF16, kind=kind_dbg)
        QN = k.dram("QN", [4, 128, NT], BF16, kind=kind_dbg)
        KN = k.dram("KN", [4, 128, NT], BF16, kind=kind_dbg)
        QR = k.dram("QR", [8, 32, NT], BF16, kind=kind_dbg)
        KVT = k.dram("KVT", [128, NT], BF16, kind=kind_dbg)
        KRT = k.dram("KRT", [32, NT], BF16, kind=kind_dbg)
        VP = k.dram("VP", [NT, 512], BF16, kind=kind_dbg)
        WQT = k.dram("WQT", [2, 128, NT], BF16, kind=kind_dbg)
        WKT = k.dram("WKT", [2, 128, NT], BF16, kind=kind_dbg)
        YT = k.dram("YT", [8, 128, NT], BF16, kind=kind_dbg)
        H2 = k.dram("H2", [NT, D], BF16, kind=kind_dbg)
        AFFD = k.dram("AFFD", [16, NTX], F32)
        THRD = k.dram("THRD", [128, 1], F32)

        cst = k.sb([128, NCST], F32, "cst")
        k.dma(cst, cst_d)
        small = k.sb([128, L_ * 19], F32, "small")
        k.dma(small, small_d)

        def C(name, rows=128):
            o, w = _CST[name]
            return cst[0:rows, o:o + w]
        ident = C("ident")
        ones_f = k.sb([128, 128], F32, "ones_f")
        k.memset(ones_f, 1.0)
        ones_b = k.sb([128, 128], BF16, "ones_b")
        k.memset(ones_b, 1.0)
        ident_b = k.sb([128, 128], BF16, "ident_b")
        k.copy(ident_b, ident)
        triU_b = k.sb([128, 128], BF16, "triU_b")
        k.copy(triU_b, C("triU"))
        mprev_b = k.sb([128, 128], BF16, "mprev_b")
        k.copy(mprev_b, C("mprev"))
        mnext_b = k.sb([128, 128], BF16, "mnext_b")
        k.copy(mnext_b, C("mnext"))
        Rtab = k.sb([128, NTILE, 16, 4], BF16, "Rtab")
        o_tok, w_tok = _CST["tokid"]
        k.copy(Rtab[:, :, :, 0:2], T(cst.ap[:, o_tok:o_tok + w_tok].rearrange("p (n e c) -> p n e c", n=NTILE, e=16), cst.key))
        affT = k.sb([128, NTILE, 16], F32, "affT")
        posm = k.sb([128, NTILE, 16], F32, "posm")

        EPS = 1e-6
        blocks = [(0, 256, 0)] + [(256 + 512 * i, 512, 1) for i in range(8)]

        def rows_key(t, n):
            return t.sub(("r", n))

        def stop_here(name):
            return upto is not None and upto == name

        def rstd_from_ss(ss, n_feat, es, eps=EPS):
            r = k.sb([128, 1], F32, es=es)
            k.rsq(r, ss, 1.0 / n_feat, eps)
            return r

        for l in range(nlayers):
            need_ctx = l < L_ - 1
            sm0 = l * 19
            Xsrc = x0 if l == 0 else X
            with ExitStack() as s:
                crep = k.sb([128, 2, 8, 128], F32, es=s)
                for st in range(2):
                    k.dma(crep[:, st], crep_d[st])
                sil = k.sb([128, 2, 8, 128], F32, es=s)
                k.act(sil, crep, AF.Silu)
                wb = Rot([k.sb([128, 8, 512], F32, es=s) for _ in range(3)])
                br = Rot([k.sb([1, 512], F32, es=s) for _ in range(2)])
                pp = Rot([k.ps([128, 512], F32, es=s) for _ in range(4)])
                ob = Rot([k.sb([128, 512], F32, es=s) for _ in range(4)])
                aw = TD(ada_w_d[l], "(kc p) n -> p kc n", p=128)
                for nb in range(12):
                    w = wb.get()
                    for q4 in range(4):
                        k.dma(w.sl(q4, np.s_[:, q4 * 2:(q4 + 1) * 2, :]), aw[:, q4 * 2:(q4 + 1) * 2, nb * 512:(nb + 1) * 512])
                    b_ = br.get()
                    k.dma(b_, ada_b_d[l, :, nb * 512:(nb + 1) * 512])
                    for st in range(2):
                        ps = pp.get()
                        for kc in range(8):
                            k.mm(ps, sil[:, st, kc, :], w.sl(kc // 2, np.s_[:, kc, :]), start=(kc == 0), stop=False)
                        k.mm(ps, ones_f[0:1, :], b_, start=False, stop=True)
                        o = ob.get()
                        k.copy(o, ps, e=("act" if st == 0 else "dve"))
                        j, half = nb // 2, nb % 2
                        k.dma(MODBC.sl((st, j), np.s_[st, j, :, half * 512:(half + 1) * 512]), o)
            k.barrier()
            if stop_here("mod%d" % l):
                break

            def load_mod(st, j, es, eng_q="sp"):
                t = k.sb([128, D], F32, es=es)
                k.dma(t, MODBC.sl((st, j), np.s_[st, j]))
                return t

            def load_gs(st, j_scale, gidx, es):
                sc = load_mod(st, j_scale, es)
                gb = k.sb([128, D], F32, es=es)
                k.dma(gb, g_bc_d[gidx])
                k.stt(sc, sc, 1.0, gb, ALU.add, ALU.mult)
                return sc

            def load_cast(dst, src, stg, e="pool"):
                t = stg.get()
                sh = list(src.ap.shape)
                tv = t[0:sh[0], 0:sh[1]]
                k.dma(tv, src)
                k.copy(dst, tv, e=e)

            with ExitStack() as s:
                w_in = k.sb([128, 8, NCOL], BF16, es=s)
                stg = Rot([k.sb([128, NCOL], F32, es=s) for _ in range(2)])
                for kc in range(8):
                    load_cast(w_in[:, kc, :], w_in_d[l, kc * 128:(kc + 1) * 128, :], stg, e=("pool" if kc % 2 else "dve"))
                w_uq_n = k.sb([128, 2, 512], BF16, es=s)
                w_uq_r = k.sb([128, 2, 256], BF16, es=s)
                for kc in range(2):
                    load_cast(w_uq_n[:, kc, :], w_uq_n_d[l, kc * 128:(kc + 1) * 128, :], stg)
                    load_cast(w_uq_r[:, kc, :], w_uq_r_d[l, kc * 128:(kc + 1) * 128, :], stg)
                w_uk = k.sb([128, 512], BF16, es=s)
                load_cast(w_uk, w_uk_d[l], stg)
                w_uv = k.sb([128, 512], BF16, es=s)
                load_cast(w_uv, w_uv_d[l], stg)
                gs1 = [load_gs(st, 1, l * 2 + 0, s) for st in range(2)]
                sh1 = [load_mod(st, 0, s) for st in range(2)]
                qg = small[:, sm0 + 0:sm0 + 2]
                kg = small[:, sm0 + 2:sm0 + 3]
                xt_r = Rot([k.sb([128, D], F32, es=s) for _ in range(3)])
                xs_r = Rot([k.sb([128, D], F32, es=s) for _ in range(3)])
                junk = k.sb([128, D], F32, es=s)
                ss_pool = Rot([k.sb([128, 1], F32, es=s) for _ in range(8)])
                hT_r = Rot([k.sb([128, 8, 512], BF16, es=s) for _ in range(2)])
                ptr = Rot([k.ps([128, 8, 128], BF16, es=s) for _ in range(2)])
                xb_r = Rot([k.sb([128, D], BF16, es=s) for _ in range(5)])
                pfm = Rot([k.ps([128, 512], F32, es=s) for _ in range(3)])
                pex = Rot([k.ps([128, 512], F32, es=s) for _ in range(3)])
                ob16 = Rot([k.sb([128, 512], BF16, es=s) for _ in range(6)])
                f32t = Rot([k.sb([128, 512], F32, es=s) for _ in range(6)])
                rp32 = Rot([k.sb([128, 2, 512], F32, es=s) for _ in range(2)])
                rp128 = Rot([k.sb([128, 2, 512], F32, es=s) for _ in range(2)])
                cqg = k.sb([128, 2, 512], BF16, es=s)
                rstdq = k.sb([128, 512], F32, es=s)
                kvT_sb = k.sb([128, 512], BF16, es=s)
                tmo = Rot([k.sb([128, 1024], BF16, es=s) for _ in range(2)])

                def bc_rstd(dst, sq_list, nfeat, n):
                    ps = pex.get()
                    for i, sq in enumerate(sq_list):
                        k.mm(ps[:, :n], ones_f, sq, start=(i == 0), stop=(i == len(sq_list) - 1))
                    k.rsq(dst[:, :n], ps[:, :n], 1.0 / nfeat, EPS)

                def rope_apply(src_f32, M, n, tabs, perm, out_bf):
                    ps = pex.get()
                    k.mm(ps[0:M, :n], perm, src_f32[0:M, :n])
                    t1 = f32t.get()
                    k.tt(t1[0:M, :n], src_f32[0:M, :n], tabs[0:M, 0, :n], ALU.mult)
                    t2 = f32t.get()
                    k.tt(t2[0:M, :n], ps[0:M, :n], tabs[0:M, 1, :n], ALU.mult)
                    k.tt(out_bf[0:M, :n], t1[0:M, :n], t2[0:M, :n], ALU.add, e="pool")

                class NormState:
                    pass

                def norm_begin(blk):
                    (r0, n, st) = blk
                    ns = NormState()
                    ns.blk = blk
                    ns.hT = hT_r.get()
                    ns.r32 = ns.r128 = None
                    ns.xbs = []
                    if st == 1:
                        t0 = r0 - NTC
                        ns.r32 = rp32.get()
                        k.dma(ns.r32[:, :, :n], TD(rope32_d, "a p t -> p a t")[:, :, t0:t0 + n])
                        ns.r128 = rp128.get()
                        k.dma(ns.r128[:, :, :n], TD(rope128_d, "a p t -> p a t")[:, :, t0:t0 + n])
                    return ns

                def norm_ew(ns, ti):
                    (r0, n, st) = ns.blk
                    if ti >= n // 128:
                        return
                    rr = r0 + ti * 128
                    xt = xt_r.get()
                    k.dma(xt, rows_key(Xsrc, rr // 128)[rr:rr + 128, :])
                    ss = ss_pool.get()
                    k.act(junk, xt, AF.Square, accum=ss)
                    rstd = ss_pool.get()
                    k.rsq(rstd, ss, 1.0 / D, EPS)
                    xs = xs_r.get()
                    k.stt(xs, xt, rstd, gs1[st], ALU.mult, ALU.mult)
                    xb = xb_r.get()
                    k.tt(xb, xs, sh1[st], ALU.add)
                    ns.xbs.append(xb)

                def norm_tr(ns):
                    (r0, n, st) = ns.blk
                    for ti, xb in enumerate(ns.xbs):
                        pt = ptr.get()
                        for kc in range(8):
                            k.tr(pt[:, kc, :], xb[:, kc * 128:(kc + 1) * 128], ident_b)
                        k.copy(ns.hT[:, :, ti * 128:(ti + 1) * 128], pt, e="act")
                    return (ns.hT, ns.r32, ns.r128)

                def proj_phase(blk, ctx_, hook):
                    (r0, n, st) = blk
                    nt = n // 128
                    hT, r32, r128 = ctx_
                    def fm(c, M=128):
                        ps = pfm.get()
                        for kc in range(8):
                            k.mm(ps[0:M, :n], w_in[:, kc, c * 128:c * 128 + M], hT[:, kc, :n], start=(kc == 0), stop=(kc == 7))
                        return ps
                    for c in range(4):
                        ps = fm(c)
                        o = ob16.get()
                        k.copy(o[:, :n], ps[:, :n], e=("act" if c % 2 == 0 else "dve"))
                        dst = (QT if c < 2 else KT)
                        k.dma(dst.sl((c % 2, r0), np.s_[c % 2, :, r0:r0 + n]), o[:, :n])
                    hook(0)
                    sqs = []
                    for c2 in range(2):
                        ps = fm(4 + c2)
                        sq = f32t.get()
                        k.act(sq[:, :n], ps[:, :n], AF.Square)
                        sqs.append(sq[:, :n])
                        k.act(cqg[:, c2, :n], ps[:, :n], AF.Copy, scale=qg[:, c2:c2 + 1])
                    bc_rstd(rstdq, sqs, 256, n)
                    for pr in range(4):
                        ps = pex.get()
                        for c2 in range(2):
                            k.mm(ps[:, :n], w_uq_n[:, c2, pr * 128:(pr + 1) * 128], cqg[:, c2, :n], start=(c2 == 0), stop=(c2 == 1))
                        qn = ob16.get()
                        k.tt(qn[:, :n], ps[:, :n], rstdq[:, :n], ALU.mult)
                        k.dma(QN.sl((pr, r0), np.s_[pr, :, r0:r0 + n]), qn[:, :n])
                    for g4 in range(2):
                        ps = pex.get()
                        for c2 in range(2):
                            k.mm(ps[:, :n], w_uq_r[:, c2, g4 * 128:(g4 + 1) * 128], cqg[:, c2, :n], start=(c2 == 0), stop=(c2 == 1))
                        qr = f32t.get()
                        k.tt(qr[:, :n], ps[:, :n], rstdq[:, :n], ALU.mult)
                        o = ob16.get()
                        if st == 1:
                            rope_apply(qr, 128, n, r32, C("p32x4"), o)
                        else:
                            k.copy(o[:, :n], qr[:, :n], e="pool")
                        for hh in range(4):
                            h = g4 * 4 + hh
                            k.dma(QR.sl((h, r0), np.s_[h, :, r0:r0 + n]), o[hh * 32:(hh + 1) * 32, :n])
                    hook(1)
                    ps = fm(6)
                    sq = f32t.get()
                    k.act(sq[:, :n], ps[:, :n], AF.Square)
                    kvg = f32t.get()
                    k.act(kvg[:, :n], ps[:, :n], AF.Copy, scale=kg[:, 0:1])
                    rk_ = f32t.get()
                    bc_rstd(rk_, [sq[:, :n]], 128, n)
                    k.tt(kvT_sb[:, :n], kvg[:, :n], rk_[:, :n], ALU.mult)
                    k.dma(KVT.sl(r0, np.s_[:, r0:r0 + n]), kvT_sb[:, :n])
                    for pr in range(4):
                        ps = pex.get()
                        k.mm(ps[:, :n], w_uk[:, pr * 128:(pr + 1) * 128], kvT_sb[:, :n])
                        o = ob16.get()
                        k.copy(o[:, :n], ps[:, :n], e=("act" if pr % 2 == 0 else "dve"))
                        k.dma(KN.sl((pr, r0), np.s_[pr, :, r0:r0 + n]), o[:, :n])
                    for ti in range(nt):
                        ps = pex.get()
                        k.mm(ps, kvT_sb[:, ti * 128:(ti + 1) * 128], w_uv)
                        o = ob16.get()
                        k.copy(o, ps, e="act")
                        rr = r0 + ti * 128
                        k.dma(VP.sl(rr // 128, np.s_[rr:rr + 128, :]), o)
                    hook(2)
                    for c in range(4):
                        ps = fm(7 + c)
                        o = ob16.get()
                        if st == 1:
                            sf = f32t.get()
                            k.copy(sf[:, :n], ps[:, :n], e="act")
                            rope_apply(sf, 128, n, r128, C("p128"), o)
                        else:
                            k.copy(o[:, :n], ps[:, :n], e="act")
                        dst = (WQT if c < 2 else WKT)
                        k.dma(dst.sl((c % 2, r0), np.s_[c % 2, :, r0:r0 + n]), o[:, :n])
                    ps = fm(11, 32)
                    o = ob16.get()
                    if st == 1:
                        sf = f32t.get()
                        k.copy(sf[0:32, :n], ps[0:32, :n], e="act")
                        rope_apply(sf, 32, n, r32, C("p32", 32), o)
                    else:
                        k.copy(o[0:32, :n], ps[0:32, :n], e="act")
                    k.dma(KRT.sl(r0, np.s_[:, r0:r0 + n]), o[0:32, :n])
                    hook(3)
                    for ti in range(nt):
                        o = tmo.get()
                        for hf in range(2):
                            ps = pfm.get()
                            c0 = 1440 + hf * 512
                            for kc in range(8):
                                k.mm(ps, hT[:, kc, ti * 128:(ti + 1) * 128], w_in[:, kc, c0:c0 + 512], start=(kc == 0), stop=(kc == 7))
                            k.copy(o[:, hf * 512:(hf + 1) * 512], ps, e=("act" if hf == 0 else "dve"))
                        rr = r0 + ti * 128
                        k.dma(TMo.sl(rr // 128, np.s_[rr:rr + 128, :]), o)
                ns0 = norm_begin(blocks[0])
                for ti_ in range(4):
                    norm_ew(ns0, ti_)
                pctx = norm_tr(ns0)
                for bi, blk in enumerate(blocks):
                    nsn = norm_begin(blocks[bi + 1]) if bi + 1 < len(blocks) else None
                    proj_phase(blk, pctx, (lambda i_: norm_ew(nsn, i_)) if nsn is not None else (lambda i_: None))
                    pctx = norm_tr(nsn) if nsn is not None else None
            k.barrier()
            if stop_here("s1_%d" % l):
                break
            es_mla_pre = ExitStack()
            vp2 = k.sb([128, NTILE, 8, 128], BF16, es=es_mla_pre)
            VPv = TD(VP, "(t p) (h d) -> p t h d", p=128, d=64)
            for par in range(2):
                k.memset(vp2[:, :, par::2, (1 - par) * 64:(2 - par) * 64], 1.0, e="pool")
                for h in range(par, 8, 2):
                    k.dma(vp2[:, :, h, par * 64:(par + 1) * 64], VPv[:, :, h, :])
            with ExitStack() as s:
                lg_rep = k.sb([128, 8], F32, es=s)
                lg_pp = k.sb([128, 4], F32, es=s)
                for dst, src in ((lg_rep, small[:, sm0 + 3:sm0 + 11]), (lg_pp, small[:, sm0 + 15:sm0 + 19])):
                    k.act(dst, src, AF.Exp, scale=LN2)
                    k.ts(dst, dst, -1.0, ALU.mult, 1.0, ALU.add)
                    k.act(dst, dst, AF.Ln)
                LN8 = float(np.log(0.125))
                DT = k.sb([128, 4, 128], F32, es=s)
                tmpA = k.sb([128, 128], F32, es=s)
                for h in range(4):
                    k.ts(tmpA, C("A"), lg_rep[:, h:h + 1], ALU.mult)
                    k.stt(tmpA, C("B"), lg_rep[:, 4 + h:5 + h], tmpA, ALU.mult, ALU.add)
                    k.act(DT[:, h, :], tmpA, AF.Exp)
                    k.ts(DT[:, h, :], DT[:, h, :], 0.125, ALU.mult)
                QWF = k.sb([128, 2, 128], F32, es=s)
                QWB = k.sb([128, 2, 128], F32, es=s)
                gch = k.sb([128, 4], F32, es=s)
                for pr in range(2):
                    k.act(QWF[:, pr, :], C("C1"), AF.Exp, scale=lg_pp[:, pr:pr + 1])
                    k.act(QWB[:, pr, :], C("C2"), AF.Exp, scale=lg_pp[:, 2 + pr:3 + pr])
                k.act(gch, lg_pp, AF.Exp, scale=128.0)
                KWF = k.sb([128, 256], F32, es=s)
                KWB = k.sb([128, 256], F32, es=s)
                kcol = k.sb([128, 8], F32, es=s)
                for h in range(4):
                    k.act(kcol[:, h:h + 1], C("colK1"), AF.Exp, scale=lg_rep[:, h:h + 1])
                    k.act(kcol[:, 4 + h:5 + h], C("colK2"), AF.Exp, scale=lg_rep[:, 4 + h:5 + h])
                k.ts(kcol, kcol, 0.125, ALU.mult)
                for h in range(4):
                    k.copy(KWF[:, h * 64:(h + 1) * 64], T(kcol.ap[:, h:h + 1].to_broadcast([128, 64]), kcol.key))
                    k.copy(KWB[:, h * 64:(h + 1) * 64], T(kcol.ap[:, 4 + h:5 + h].to_broadcast([128, 64]), kcol.key))
                KVd = k.sb([128, NTILE, 4, 64], F32, es=s)
                Sfb = k.sb([128, NTILE, 2, 64], BF16, es=s)
                Sbb = k.sb([128, NTILE, 2, 64], BF16, es=s)
                kv_r = Rot([k.sb([128, 512], BF16, es=s) for _ in range(3)])
                kw_r = Rot([k.sb([128, 2, 256], BF16, es=s) for _ in range(2)])
                pkv = Rot([k.ps([128, 4, 128], F32, es=s) for _ in range(2)])
                fwd_order = list(range(NTILE))
                bwd_order = [1, 0] + list(range(NTILE - 1, 1, -1))
                r1_order = []
                for a_, b_ in zip(fwd_order, bwd_order):
                    for t_ in (a_, b_):
                        if t_ not in r1_order:
                            r1_order.append(t_)
                curs = []
                for d_ in range(2):
                    c_ = k.sb([128, 2, 64], F32, es=s)
                    k.memset(c_, 0.0)
                    curs.append(c_)
                ptrs = [0, 0]
                done = set()

                def scan_step(d_, n):
                    Sb_ = Sfb if d_ == 0 else Sbb
                    cur = curs[d_]
                    k.copy(Sb_.sl(n, np.s_[:, n, :, :]), cur)
                    for pr in range(2):
                        k.stt(cur[:, pr, :], cur[:, pr, :], gch[:, d_ * 2 + pr:d_ * 2 + pr + 1],
                              KVd.sl(n, np.s_[:, n, d_ * 2 + pr, :]), ALU.mult, ALU.add)
                for n in r1_order:
                    kvt = kv_r.get()
                    k.dma(kvt, TMo.sl(n, np.s_[n * 128:(n + 1) * 128, 0:512]))
                    kw = kw_r.get()
                    k.tt(kw[:, 0, :], kvt[:, 0:256], KWF, ALU.mult)
                    k.tt(kw[:, 1, :], kvt[:, 0:256], KWB, ALU.mult)
                    ps = pkv.get()
                    for d_ in range(2):
                        for pr in range(2):
                            k.mm(ps[:, d_ * 2 + pr, :], kw[:, d_, pr * 128:(pr + 1) * 128], kvt[:, 256 + pr * 128:256 + (pr + 1) * 128])
                    k.copy(KVd.sl(n, np.s_[0:64, n, :, :]), ps[0:64, :, 0:64], e="act")
                    k.copy(KVd.sl(n, np.s_[64:128, n, :, :]), ps[64:128, :, 64:128], e="act")
                    done.add(n)
                    for d_, order in ((0, fwd_order), (1, bwd_order)):
                        while ptrs[d_] < NTILE and order[ptrs[d_]] in done:
                            scan_step(d_, order[ptrs[d_]])
                            ptrs[d_] += 1
                qk_r = Rot([k.sb([128, 2, 2, 128], BF16, es=s) for _ in range(3)])
                vg_r = Rot([k.sb([128, 512], BF16, es=s) for _ in range(3)])
                qw_r = Rot([k.sb([128, 2, 2, 128], BF16, es=s) for _ in range(2)])
                sm_r = Rot([k.sb([128, 512], BF16, es=s) for _ in range(4)])
                pss = Rot([k.ps([128, 512], F32, es=s) for _ in range(2)] + [T(p_.ap.rearrange("p a b -> p (a b)"), p_.key) for p_ in pkv.t])
                psy = Rot([k.ps([128, 512], F32, es=s) for _ in range(2)])
                ysb_r = Rot([k.sb([128, 256], F32, es=s) for _ in range(3)])
                sq_r = Rot([k.sb([128, 256], F32, es=s) for _ in range(2)])
                st_r = Rot([k.sb([128, 16], F32, es=s) for _ in range(2)])
                yo_r = Rot([k.sb([128, 2, 128], BF16, es=s) for _ in range(2)])
                yb_r = Rot([k.sb([128, 256], BF16, es=s) for _ in range(2)])
                ptb2 = Rot([k.ps([128, 2, 128], BF16, es=s) for _ in range(2)])

                sgall = k.sb([128, NTILE, 256], BF16, es=s)
                for n in range(0 if need_ctx else 2, NTILE):
                    gt_ = vg_r.get()
                    k.dma(gt_[:, 0:256], TMo.sl(n, np.s_[n * 128:(n + 1) * 128, 512:768]))
                    k.act(sgall[:, n, :], gt_[:, 0:256], AF.Silu)

                def r2A(n):
                    c0 = n * 128
                    qk = qk_r.get()
                    k.dma(qk[:, 0], TD(QT, "c p t -> p c t")[:, :, c0:c0 + 128])
                    k.dma(qk[:, 1], TD(KT, "c p t -> p c t")[:, :, c0:c0 + 128])
                    vg = vg_r.get()
                    k.dma(vg, TMo.sl(n, np.s_[c0:c0 + 128, 256:768]))
                    qw = qw_r.get()
                    k.tt(qw[:, 0], qk[:, 0], QWF, ALU.mult)
                    k.tt(qw[:, 1], qk[:, 0], QWB, ALU.mult)
                    py = psy.get()
                    psA = pss.get()
                    psB = pss.get()
                    for h in range(4):
                        pr, off = h // 2, (h % 2) * 64
                        pb = psA if h % 2 == 0 else psB
                        k.mm(pb[:, pr * 128:(pr + 1) * 128], qk[off:off + 64, 1, pr, :], qk[off:off + 64, 0, pr, :])
                    sm = sm_r.get()
                    smv = T(sm.ap.rearrange("p (a c) -> p a c", a=4), sm.key)
                    k.tt(smv[:, 0::2, :], T(psA.ap[:, 0:256].rearrange("p (a c) -> p a c", a=2), psA.key), DT[:, 0::2, :], ALU.mult)
                    k.tt(smv[:, 1::2, :], T(psB.ap[:, 0:256].rearrange("p (a c) -> p a c", a=2), psB.key), DT[:, 1::2, :], ALU.mult)
                    for h in range(4):
                        pr, off = h // 2, (h % 2) * 64
                        yo_ = py[:, h * 64:(h + 1) * 64]
                        k.mm(yo_, sm[:, h * 128:(h + 1) * 128], vg[:, h * 64:(h + 1) * 64], start=True, stop=False)
                        k.mm(yo_, qw[off:off + 64, 0, pr, :], Sfb.sl(n, np.s_[off:off + 64, n, pr, :]), start=False, stop=False)
                        k.mm(yo_, qw[off:off + 64, 1, pr, :], Sbb.sl(n, np.s_[off:off + 64, n, pr, :]), start=False, stop=True)
                    ysb = ysb_r.get()
                    k.copy(ysb, py[:, 0:256], e="act")
                    return (ysb, sgall[:, n, :])

                def r2B(n, ctx_):
                    ysb, sg = ctx_
                    c0 = n * 128
                    stt_ = st_r.get()
                    k.op("dve", lambda e: e.reduce_sum(stt_.ap[:, 0:4], ysb.ap.rearrange("p (h d) -> p h d", h=4), AX.X), reads=[ysb], writes=[stt_])
                    sq = sq_r.get()
                    k.tt(sq, ysb, ysb, ALU.mult)
                    k.op("dve", lambda e: e.reduce_sum(stt_.ap[:, 4:8], sq.ap.rearrange("p (h d) -> p h d", h=4), AX.X), reads=[sq], writes=[stt_])
                    k.ts(stt_[:, 0:8], stt_[:, 0:8], 1.0 / 64, ALU.mult)
                    k.tt(stt_[:, 8:12], stt_[:, 0:4], stt_[:, 0:4], ALU.mult)
                    k.tt(stt_[:, 12:16], stt_[:, 4:8], stt_[:, 8:12], ALU.subtract)
                    k.rsq(stt_[:, 12:16], stt_[:, 12:16], 1.0, 1e-5)
                    for h in range(4):
                        k.ts(ysb[:, h * 64:(h + 1) * 64], ysb[:, h * 64:(h + 1) * 64], stt_[:, h:h + 1], ALU.subtract,
                             stt_[:, 12 + h:13 + h], ALU.mult)
                    yb = yb_r.get()
                    k.tt(yb, ysb, sg, ALU.mult)
                    pt = ptb2.get()
                    for c in range(2):
                        k.tr(pt[:, c, :], yb[:, c * 128:(c + 1) * 128], ident_b)
                    yo = yo_r.get()
                    k.copy(yo, pt, e="act")
                    k.dma(TD(YT, "c p t -> p c t").sl(("ret", n), np.s_[:, 0:2, c0:c0 + 128]), yo, q="pool")
                tiles_r = list(range(0 if need_ctx else 2, NTILE))
                pc_ = r2A(tiles_r[0])
                for ti_, n in enumerate(tiles_r):
                    nc_ = r2A(tiles_r[ti_ + 1]) if ti_ + 1 < len(tiles_r) else None
                    r2B(n, pc_)
                    pc_ = nc_
            k.barrier()
            if stop_here("ret%d" % l):
                es_mla_pre.close()
                break
            with ExitStack() as s:
                kh_l = [k.sb([128, NT], BF16, es=s) for _ in range(2)]
                for t_ in kh_l:
                    k.memset(t_, 0.0, e="pool")
                kh_r = Rot(kh_l)
                q_l = [k.sb([128, 512], BF16, es=s) for _ in range(3)]
                for t_ in q_l:
                    k.memset(t_, 0.0)
                q_r = Rot(q_l)
                rd_sets = []
                for par in range(2):
                    tl = [k.sb([128, 512], F32, es=s) for _ in range(2)]
                    for t_ in tl:
                        k.memset(t_, 0.0)
                    rd_sets.append(Rot(tl))
                pT_r = Rot([k.sb([128, 2, 512], BF16, es=s) for _ in range(4)])
                pss = Rot([k.ps([128, 2, 512], F32, es=s) for _ in range(2)])
                pacc = Rot([k.ps([128, 512], F32, es=s) for _ in range(4)])
                sw_r = Rot([k.sb([128, 512], F32, es=s) for _ in range(2)])
                yp_r = Rot([k.sb([128, 512], BF16, es=s) for _ in range(3)])
                qblocks = ([(0, 256, [0, 1])] if need_ctx else []) + [(256 + 512 * i, 512, list(range(NTILE))) for i in range(8)]
                LOOK = 1
                pend = []

                def emit_pv2(item):
                    h, r0, n, j, npair, kt2, pT, pv = item
                    for u_ in range(2):
                        first = (j == 0 and u_ == 0)
                        last = (j == npair - 1 and u_ == 1)
                        k.mm(pv[:, :n], vp2[:, kt2[u_], h, :], pT[:, u_, :n], start=first, stop=last)
                    if j == npair - 1:
                        off = (h % 2) * 64
                        dof = 64 - off
                        rd = rd_sets[h % 2].get()
                        k.op("dve", lambda e: e.reciprocal(rd.ap[dof:dof + 64, :n], pv.ap[dof:dof + 64, :n]), reads=[pv], writes=[rd])
                        sws = sw_r.get()
                        k.dma(sws[off:off + 64, :n], rd[dof:dof + 64, :n])
                        yp = yp_r.get()
                        k.tt(yp[off:off + 64, :n], pv[off:off + 64, :n], sws[off:off + 64, :n], ALU.mult)
                        k.dma(YT.sl(("mla", h, r0), np.s_[2 + h // 2, off:off + 64, r0:r0 + n]), yp[off:off + 64, :n], q="pool")
                for h in range(8):
                    off = (h % 2) * 64
                    kh = kh_r.get()
                    k.dma(kh[0:64, :], KN[h // 2, off:off + 64, :])
                    k.dma(kh[64:96, :], KRT)
                    for (r0, n, kts) in qblocks:
                        q = q_r.get()
                        k.dma(q[0:64, :n], QN[h // 2, off:off + 64, r0:r0 + n])
                        k.dma(q[64:96, :n], QR[h, :, r0:r0 + n])
                        pv = pacc.get()
                        npair = len(kts) // 2
                        for j in range(npair):
                            kt2 = (kts[2 * j], kts[2 * j + 1])
                            ps = pss.get()
                            for u_ in range(2):
                                k.mm(ps[:, u_, :n], kh[:, kt2[u_] * 128:(kt2[u_] + 1) * 128], q[:, :n])
                            pT = pT_r.get()
                            k.act(pT[:, :, :n], ps[:, :, :n], AF.Exp, scale=SCALE_MLA)
                            pend.append((h, r0, n, j, npair, kt2, pT, pv))
                            if len(pend) > LOOK:
                                emit_pv2(pend.pop(0))
                while pend:
                    emit_pv2(pend.pop(0))
            k.barrier()
            es_mla_pre.close()
            if stop_here("mla%d" % l):
                break
            with ExitStack() as s:
                wkT = k.sb([128, 2, 2, NT], BF16, es=s)
                k.memset(wkT, 0.0, e="pool")
                for hk_ in range(2):
                    for g_ in range(2):
                        k.dma(wkT[g_ * 64:(g_ + 1) * 64, hk_, g_, :], WKT[hk_, g_ * 64:(g_ + 1) * 64, :])
                wqT = k.sb([128, 2, NT], BF16, es=s)
                k.dma(wqT, TD(WQT, "c p t -> p c t"))
                wv3 = k.sb([128, NTILE, 4, 128], BF16, es=s)
                WVv = T(TMo.ap[:, 768:1024].rearrange("(t p) (q d) -> p t q d", p=128, d=64), TMo.key)
                for par in range(2):
                    k.memset(wv3[:, :, par::2, (1 - par) * 64:(2 - par) * 64], 1.0, e=("dve" if par == 0 else "pool"))
                    for q_ in range(par, 4, 2):
                        k.dma(wv3[:, :, q_, par * 64:(par + 1) * 64], WVv[:, :, q_, :])
                esink = k.sb([128, 4], F32, es=s)
                k.act(esink, small[:, sm0 + 11:sm0 + 15], AF.Exp)
                rd_sets = []
                for par in range(2):
                    tl = [k.sb([128, 128], F32, es=s) for _ in range(2)]
                    for t_ in tl:
                        k.memset(t_, 0.0)
                    rd_sets.append(Rot(tl))
                pT_r = Rot([k.sb([128, 5, 128], BF16, es=s) for _ in range(3)])
                pss = Rot([k.ps([128, 512], F32, es=s) for _ in range(4)])
                pacc = Rot([k.ps([128, 512], F32, es=s) for _ in range(4)])
                sw_r = Rot([k.sb([128, 128], F32, es=s) for _ in range(4)])
                yp_r = Rot([k.sb([128, 128], BF16, es=s) for _ in range(4)])
                pend = []

                def emit_pvw(item):
                    n, qh, keys, pT, pv, yp = item
                    hk, g = qh // 2, qh % 2
                    off = g * 64
                    dof = 64 - off
                    c0 = n * 128
                    nk = len(keys)
                    for i, (kt, msk) in enumerate(keys):
                        k.mm(pv[:, 0:128], wv3[:, kt, qh, :], pT[:, i, :], start=(i == 0), stop=(i == nk - 1))
                    rd = rd_sets[g].get()
                    k.ts(rd[dof:dof + 64, :], pv[dof:dof + 64, 0:128], esink[dof:dof + 64, qh:qh + 1], ALU.add)
                    k.op("dve", lambda e: e.reciprocal(rd.ap[dof:dof + 64, :], rd.ap[dof:dof + 64, :]), reads=[rd], writes=[rd])
                    sws = sw_r.get()
                    k.dma(sws[off:off + 64, :], rd[dof:dof + 64, :])
                    pendF.append((n, qh, pv, yp, sws))
                    if len(pendF) > 1:
                        emit_fin(pendF.pop(0))

                def emit_fin(item):
                    n, qh, pv, yp, sws = item
                    hk, g = qh // 2, qh % 2
                    off = g * 64
                    c0 = n * 128
                    k.tt(yp[off:off + 64, :], pv[off:off + 64, 0:128], sws[off:off + 64, :], ALU.mult)
                    if g == 1:
                        k.dma(YT.sl(("win", hk, n), np.s_[6 + hk, :, c0:c0 + 128]), yp, q="pool")
                pendF = []
                yp = None
                for n in range(0 if need_ctx else 2, NTILE):
                    c0 = n * 128
                    if n < 2:
                        keys = [(0, None), (1, None)]
                    else:
                        keys = []
                        if n - 1 >= 2:
                            keys.append((n - 1, mprev_b))
                        keys.append((n, None))
                        if n + 1 < NTILE:
                            keys.append((n + 1, mnext_b))
                        keys += [(0, None), (1, None)]
                    for qh in range(4):
                        hk, g = qh // 2, qh % 2
                        pv = pacc.get()
                        if g == 0:
                            yp = yp_r.get()
                        psA = pss.get()
                        psB = pss.get() if len(keys) > 4 else None
                        for i, (kt, msk) in enumerate(keys):
                            dst = psA[:, i * 128:(i + 1) * 128] if i < 4 else psB[:, 0:128]
                            k.mm(dst, wkT[:, hk, g, kt * 128:(kt + 1) * 128], wqT[:, hk, c0:c0 + 128])
                        pT = pT_r.get()
                        na = min(4, len(keys))
                        k.act(T(pT.ap[:, 0:na, :].rearrange("p a c -> p (a c)"), pT.key), psA[:, 0:na * 128], AF.Exp, scale=0.125)
                        if psB is not None:
                            k.act(pT[:, 4, :], psB[:, 0:128], AF.Exp, scale=0.125)
                        for i, (kt, msk) in enumerate(keys):
                            if msk is not None:
                                k.tt(pT[:, i, :], pT[:, i, :], msk, ALU.mult)
                        pend.append((n, qh, keys, pT, pv, yp))
                        if len(pend) > 1:
                            emit_pvw(pend.pop(0))
                while pend:
                    emit_pvw(pend.pop(0))
                while pendF:
                    emit_fin(pendF.pop(0))
            k.barrier()
            if stop_here("win%d" % l):
                break
            es_aff = ExitStack()
            aff_e = k.sb([16, NT], F32, "aff_e", es=es_aff)
            with ExitStack() as s:
                w_out = k.sb([128, 8, D], BF16, es=s)
                stg = Rot([k.sb([128, D], F32, es=s) for _ in range(2)])
                for kc in range(8):
                    load_cast(w_out[:, kc, :], w_out_d[l, kc * 128:(kc + 1) * 128, :], stg, e=("pool" if kc % 2 else "dve"))
                rw = k.sb([128, 8, 16], F32, es=s)
                k.dma(rw, TD(router_d[l], "(kc p) e -> p kc e", p=128))
                sts = [0, 1] if need_ctx else [1]
                mod2 = {st: load_mod(st, 2, s) for st in sts}
                gs2 = {st: load_gs(st, 4, l * 2 + 1, s) for st in sts}
                sh2 = {st: load_mod(st, 3, s) for st in sts}
                yT_r = Rot([k.sb([128, 8, 128], BF16, es=s) for _ in range(2)])
                xt_r = Rot([k.sb([128, D], F32, es=s) for _ in range(2)])
                xn_r = Rot([k.sb([128, D], F32, es=s) for _ in range(2)])
                xs_r = Rot([k.sb([128, D], F32, es=s) for _ in range(3)])
                h2b_r = Rot([k.sb([128, D], BF16, es=s) for _ in range(2)])
                h2T_r = Rot([k.sb([128, 8, 128], F32, es=s) for _ in range(2)])
                junk = k.sb([128, D], F32, es=s)
                ss_pool = Rot([k.sb([128, 1], F32, es=s) for _ in range(8)])
                ex_r = Rot([k.sb([128, 16], F32, es=s) for _ in range(2)])
                pso = Rot([k.ps([128, 512], F32, es=s) for _ in range(3)])
                ptr = Rot([k.ps([128, 4, 128], F32, es=s) for _ in range(2)])
                psl = Rot([k.ps([128, 512], F32, es=s) for _ in range(2)])
                def phaseA(n):
                    st = 0 if n < 2 else 1
                    c0 = n * 128
                    yT = yT_r.get()
                    k.dma(yT, TD(YT, "c p t -> p c t")[:, :, c0:c0 + 128])
                    xt = xt_r.get()
                    k.dma(xt, rows_key(Xsrc, n)[c0:c0 + 128, :])
                    xn = xn_r.get()
                    for hf in range(2):
                        ps = pso.get()
                        for kc in range(8):
                            k.mm(ps, yT[:, kc, :], w_out[:, kc, hf * 512:(hf + 1) * 512], start=(kc == 0), stop=(kc == 7))
                        sl_ = np.s_[:, hf * 512:(hf + 1) * 512]
                        k.tt(xn[sl_], ps, mod2[st][sl_], ALU.mult)
                        k.tt(xn[sl_], xn[sl_], xt[sl_], ALU.add)
                    k.dma(rows_key(X, n)[c0:c0 + 128, :], xn, q="pool")
                    ss = ss_pool.get()
                    k.act(junk, xn, AF.Square, accum=ss)
                    rstd = ss_pool.get()
                    k.rsq(rstd, ss, 1.0 / D, EPS)
                    xs = xs_r.get()
                    k.stt(xs, xn, rstd, gs2[st], ALU.mult, ALU.mult)
                    k.tt(xs, xs, sh2[st], ALU.add)
                    h2b = h2b_r.get()
                    k.copy(h2b, xs, e="act")
                    k.dma(H2.sl(n, np.s_[c0:c0 + 128, :]), h2b, q="pool")
                    return xs

                def phaseB(n, xs):
                    c0 = n * 128
                    h2T = h2T_r.get()
                    for hf in range(2):
                        pt = ptr.get()
                        for q4 in range(4):
                            kc = hf * 4 + q4
                            k.tr(pt[:, q4, :], xs[:, kc * 128:(kc + 1) * 128], ident)
                        k.copy(h2T[:, hf * 4:(hf + 1) * 4, :], pt, e="act")
                    pl = psl.get()
                    for kc in range(8):
                        k.mm(pl[:, 0:16], h2T[:, kc, :], rw[:, kc, :], start=(kc == 0), stop=(kc == 7))
                    ex = ex_r.get()
                    sm_ = ss_pool.get()
                    k.act(ex, pl[:, 0:16], AF.Exp, accum=sm_)
                    k.op("dve", lambda e: e.reciprocal(sm_.ap, sm_.ap), reads=[sm_], writes=[sm_])
                    k.ts(affT.sl(n, np.s_[:, n, :]), ex, sm_, ALU.mult)
                    pl2 = psl.get()
                    k.tr(pl2[0:16, 0:128], affT.sl(n, np.s_[:, n, :]), ident)
                    k.copy(aff_e.sl(n, np.s_[:, c0:c0 + 128]), pl2[0:16, 0:128], e="act")
                tiles_o = list(range(0 if need_ctx else 2, NTILE))
                pxs = phaseA(tiles_o[0])
                for ti_, n in enumerate(tiles_o):
                    nxs = phaseA(tiles_o[ti_ + 1]) if ti_ + 1 < len(tiles_o) else None
                    phaseB(n, pxs)
                    pxs = nxs
            k.barrier()
            if stop_here("o%d" % l):
                es_aff.close()
                break
            streams = ([(0, 2, 32)] if need_ctx else []) + [(2, 32, 512)]
            with ExitStack() as s:
                work = k.sb([16, NTX], F32, es=s)
                m8 = k.sb([16, 8], F32, es=s)
                thr = k.sb([16, 1], F32, es=s)
                aff128 = k.sb([128, 512], F32, es=s)
                junk16 = k.sb([128, 512], BF16, es=s)
                lo = k.sb([128, 1], F32, es=s)
                hi = k.sb([128, 1], F32, es=s)
                mid = k.sb([128, 1], F32, es=s)
                cnt = k.sb([128, 1], F32, es=s)
                half = k.sb([128, 1], F32, es=s)
                k.memset(half, 0.5)
                mge = k.sb([128, 1], U32, es=s)
                mlt = k.sb([128, 1], U32, es=s)
                thr16 = k.sb([16, 8], F32, es=s)
                mask_e = k.sb([16, NTX], F32, es=s)
                maskTb = k.sb([128, 32, 16], BF16, es=s)
                carry = k.sb([128, 16], F32, es=s)
                pos_r = Rot([k.sb([128, 16], F32, es=s) for _ in range(2)])
                ptk = Rot([k.ps([128, 512], F32, es=s) for _ in range(4)])
                ahi = k.sb([128, NTILE, 16], BF16, es=s)
                alo = k.sb([128, NTILE, 16], F32, es=s)
                k.copy(ahi, affT)
                k.tt(alo, affT, ahi, ALU.subtract)
                k.copy(Rtab[:, :, :, 2], ahi, e="pool")
                k.copy(Rtab[:, :, :, 3], alo, e="pool")
                for (t0, ntl, cap) in streams:
                    ntok = ntl * 128
                    cs = np.s_[:, t0 * 128:t0 * 128 + ntok]
                    if cap <= 64:
                        k.copy(work[:, :ntok], aff_e[cs])
                        for r in range(cap // 8):
                            k.op("dve", lambda e: e.max(out=m8.ap, in_=work.ap[:, :ntok]), reads=[work], writes=[m8])
                            if r < cap // 8 - 1:
                                k.op("dve", lambda e: e.match_replace(out=work.ap[:, :ntok], in_to_replace=m8.ap,
                                                                      in_values=work.ap[:, :ntok], imm_value=-1.0),
                                     reads=[work, m8], writes=[work])
                        k.copy(thr, m8[:, 7:8])
                    else:
                        k.dma(AFFD, aff_e[cs])
                        k.dma(aff128, TD(AFFD, "e (s t) -> (e s) t", s=8))
                        k.memset(lo, 0.0)
                        k.memset(hi, 2.0)
                        for it in range(40):
                            k.stt(mid, lo, hi, half, ALU.add, ALU.mult)
                            k.ts(junk16, aff128, mid, ALU.is_ge, 0.0, ALU.add, accum=cnt)
                            pc = ptk.get()
                            k.mm(pc[:, 0:1], C("bmat"), cnt)
                            k.ts(mge, pc[:, 0:1], cap - 0.5, ALU.is_ge)
                            k.ts(mlt, pc[:, 0:1], cap - 0.5, ALU.is_lt)
                            k.op("dve", lambda e: e.copy_predicated(lo.ap, mge.ap, mid.ap), reads=[mge, mid], writes=[lo])
                            k.op("dve", lambda e: e.copy_predicated(hi.ap, mlt.ap, mid.ap), reads=[mlt, mid], writes=[hi])
                        k.dma(THRD, lo)
                        k.dma(thr16, TD(THRD, "(e s) o -> e (s o)", s=8))
                        k.copy(thr, thr16[:, 0:1])
                    k.ts(mask_e[:, :ntok], aff_e[cs], thr, ALU.is_ge)
                    k.memset(carry, 0.0)
                    for i in range(ntl):
                        n = t0 + i
                        pt = ptk.get()
                        k.tr(pt[:, 0:16], mask_e[:, i * 128:(i + 1) * 128], ident[0:16, 0:16])
                        k.copy(maskTb[:, i, :], pt[:, 0:16], e="act")
                        pc = ptk.get()
                        k.mm(pc[:, 0:16], triU_b, maskTb[:, i, :])
                        k.mm(pc[:, 16:32], ones_b, maskTb[:, i, :])
                        pos = pos_r.get()
                        k.tt(pos, pc[:, 0:16], carry, ALU.add)
                        k.tt(pos, pos, maskTb[:, i, :], ALU.mult)
                        k.ts(posm.sl(n, np.s_[:, n, :]), pos, -1.0, ALU.add)
                        k.tt(carry, carry, pc[:, 16:32], ALU.add)
            k.barrier()
            if stop_here("topk%d" % l):
                es_aff.close()
                break
            es_aff.close()
            with ExitStack() as s:
                mod5 = {st: load_mod(st, 5, s) for st in ([0, 1] if need_ctx else [1])}
                wsets = Rot([(k.sb([128, 8, 768], BF16, es=s), k.sb([128, 8, 768], BF16, es=s), k.sb([128, 6, D], BF16, es=s))
                             for _ in range(2)])
                stg = Rot([k.sb([128, 768], F32, es=s) for _ in range(4)])
                Sel = k.sb([128, 32, 512], BF16, es=s)
                iota16 = k.sb([128, 512], mybir.dt.int16, es=s)
                k.copy(iota16, C("iota"))
                r4T_r = Rot([k.sb([4, 512], F32, es=s) for _ in range(2)])
                r4_r = Rot([k.sb([128, 16], F32, es=s) for _ in range(3)])
                idx_r = Rot([k.sb([128, 1], I32, es=s) for _ in range(16)])
                idf_r = Rot([k.sb([128, 1], F32, es=s) for _ in range(4)])
                gt_r = Rot([k.sb([128, 1], F32, es=s) for _ in range(16)])
                xs_r = Rot([k.sb([128, D], BF16, es=s) for _ in range(10)])
                xsT_r = Rot([k.sb([128, 8, 512], BF16, es=s) for _ in range(2)])
                hid = k.sb([128, 6, 512], BF16, es=s)
                sg_r = Rot([k.sb([128, 512], F32, es=s) for _ in range(2)])
                ys_r = Rot([k.sb([128, D], F32, es=s) for _ in range(2)])
                p4 = Rot([k.ps([128, 512], F32, es=s) for _ in range(1)])
                ptb = Rot([k.ps([128, 8, 128], BF16, es=s) for _ in range(1)])
                pgu = Rot([k.ps([128, 512], F32, es=s) for _ in range(4)])
                pdn = Rot([k.ps([128, 512], F32, es=s) for _ in range(2)])
                Xall = [rows_key(X, n) for n in range(NTILE)] + [X.sub("all")]
                cast_eng = Rot(["act", "dve"])
                wcache = {}

                wgen = {"g": None}

                def weights_gen(e_, wg, wu, wd):
                    for kc in range(8):
                        load_cast(wg[:, kc, :], wg_d[l, e_, kc * 128:(kc + 1) * 128, :], stg, e=cast_eng.get())
                        yield
                        load_cast(wu[:, kc, :], wu_d[l, e_, kc * 128:(kc + 1) * 128, :], stg, e=cast_eng.get())
                        yield
                    for fc in range(6):
                        for hf_ in range(2):
                            load_cast(wd[:, fc, hf_ * 512:(hf_ + 1) * 512], wd_d[l, e_, fc * 128:(fc + 1) * 128, hf_ * 512:(hf_ + 1) * 512], stg, e=cast_eng.get())
                            yield

                def feed(n_):
                    for _ in range(n_):
                        if wgen["g"] is None:
                            return
                        try:
                            next(wgen["g"])
                        except StopIteration:
                            wgen["g"] = None

                def weights(e_, now=False):
                    feed(10000)
                    wg, wu, wd = wsets.get()
                    wcache[e_] = (wg, wu, wd)
                    wgen["g"] = weights_gen(e_, wg, wu, wd)
                    if now:
                        feed(10000)

                sgen = {"g": None}

                def sel_gen(u):
                    e_, (t0, ntl, cap) = u
                    for i in range(ntl):
                        k.ts(Sel[:, i, :cap], iota16[:, :cap], posm[:, t0 + i, e_:e_ + 1], ALU.is_equal)
                        yield

                def sfeed(n_):
                    for _ in range(n_):
                        if sgen["g"] is None:
                            return
                        try:
                            next(sgen["g"])
                        except StopIteration:
                            sgen["g"] = None

                def selbuild(u, now=True):
                    sfeed(10000)
                    sgen["g"] = sel_gen(u)
                    if now:
                        sfeed(10000)

                def idxpart(u):
                    e_, (t0, ntl, cap) = u
                    ps = p4.get()
                    for i in range(ntl):
                        k.mm(ps[0:4, :cap], Rtab[:, t0 + i, e_, :], Sel[:, i, :cap], start=(i == 0), stop=(i == ntl - 1))
                    r4T = r4T_r.get()
                    k.copy(r4T[:, :cap], ps[0:4, :cap])
                    stiles = [(s0, min(128, cap - s0)) for s0 in range(0, cap, 128)]
                    for si, (s0, nsl) in enumerate(stiles):
                        k.tr(ps[0:nsl, si * 4:(si + 1) * 4], r4T[0:4, s0:s0 + nsl], ident[0:4, 0:4])
                    nsl0 = stiles[0][1]
                    r4 = r4_r.get()
                    k.copy(r4[0:nsl0, 0:4 * len(stiles)], ps[0:nsl0, 0:4 * len(stiles)])
                    meta = []
                    xss = []
                    for si, (s0, nsl) in enumerate(stiles):
                        c4 = si * 4
                        idf = idf_r.get()
                        k.stt(idf[0:nsl, :], r4[0:nsl, c4:c4 + 1], 64.0, r4[0:nsl, c4 + 1:c4 + 2], ALU.mult, ALU.add)
                        idx = idx_r.get()
                        k.copy(idx[0:nsl, :], idf[0:nsl, :])
                        gt = gt_r.get()
                        k.tt(gt[0:nsl, :], r4[0:nsl, c4 + 2:c4 + 3], r4[0:nsl, c4 + 3:c4 + 4], ALU.add)
                        meta.append((idx, gt))
                        xs = xs_r.get()
                        k.dma(xs[0:nsl, :], H2, q="pool", reads=[idx],
                              fn=lambda en: en.indirect_dma_start(out=xs.ap[0:nsl, :], out_offset=None, in_=H2.ap,
                                                                  in_offset=bass.IndirectOffsetOnAxis(ap=idx.ap[0:nsl, :], axis=0)))
                        xss.append(xs)
                    return [u, stiles, meta, xss, None]

                def xtrans(item):
                    u, stiles, meta, xss, _ = item
                    xsT = xsT_r.get()
                    for si, (s0, nsl) in enumerate(stiles):
                        xs = xss[si]
                        pt = ptb.get()
                        for kc in range(8):
                            k.tr(pt[:, kc, 0:nsl], xs[0:nsl, kc * 128:(kc + 1) * 128], ident_b[0:nsl, 0:nsl])
                        k.copy(xsT[:, :, s0:s0 + nsl], pt[:, :, 0:nsl], e="act")
                    item[4] = xsT

                def ffn(item):
                    (e_, (t0, ntl, cap)), stiles, meta, xss, xsT = item
                    wg, wu, wd = wcache[e_]
                    for fc in range(6):
                        pg = pgu.get()
                        pu = pgu.get()
                        for kc in range(8):
                            k.mm(pg[:, :cap], wg[:, kc, fc * 128:(fc + 1) * 128], xsT[:, kc, :cap], start=(kc == 0), stop=(kc == 7))
                        for kc in range(8):
                            k.mm(pu[:, :cap], wu[:, kc, fc * 128:(fc + 1) * 128], xsT[:, kc, :cap], start=(kc == 0), stop=(kc == 7))
                        sg = sg_r.get()
                        k.act(sg[:, :cap], pg[:, :cap], AF.Silu)
                        k.tt(hid[:, fc, :cap], sg[:, :cap], pu[:, :cap], ALU.mult)
                        feed(2)
                        sfeed(6)

                def down(item):
                    (e_, (t0, ntl, cap)), stiles, meta, xss, xsT = item
                    st = 0 if t0 == 0 else 1
                    wg, wu, wd = wcache[e_]
                    for si, (s0, nsl) in enumerate(stiles):
                        idx, gt = meta[si]
                        ys = ys_r.get()
                        for hf in range(2):
                            pd = pdn.get()
                            for fc in range(6):
                                k.mm(pd[0:nsl, :], hid[:, fc, s0:s0 + nsl], wd[:, fc, hf * 512:(hf + 1) * 512], start=(fc == 0), stop=(fc == 5))
                            k.stt(ys[0:nsl, hf * 512:(hf + 1) * 512], pd[0:nsl, :], gt[0:nsl, 0:1],
                                  mod5[st][0:nsl, hf * 512:(hf + 1) * 512], ALU.mult, ALU.mult)
                            feed(2)
                        k.dma(X, ys[0:nsl, :], q="pool", reads=[ys, idx], writes=Xall,
                              fn=lambda en: en.indirect_dma_start(out=X.ap, out_offset=bass.IndirectOffsetOnAxis(ap=idx.ap[0:nsl, :], axis=0),
                                                                  in_=ys.ap[0:nsl, :], in_offset=None, compute_op=ALU.add))

                units = [(e_, stm) for e_ in range(16) for stm in reversed(streams)]
                NU = len(units)
                weights(0, now=True)
                weights(1, now=True)
                nxt_w = 2
                selbuild(units[0])
                items = {0: idxpart(units[0])}
                if NU > 1:
                    selbuild(units[1])
                    items[1] = idxpart(units[1])
                xtrans(items[0])
                if NU > 2:
                    selbuild(units[2])
                for ui in range(NU):
                    ffn(items[ui])
                    if ui + 1 < NU:
                        xtrans(items[ui + 1])
                    if ui + 2 < NU:
                        sfeed(10000)
                        items[ui + 2] = idxpart(units[ui + 2])
                    down(items[ui])
                    if ui + 3 < NU:
                        selbuild(units[ui + 3], now=False)
                    e_done = units[ui][0]
                    if (ui + 1 == NU or units[ui + 1][0] != e_done) and nxt_w < 16:
                        weights(nxt_w)
                        nxt_w += 1
                    del items[ui]
            k.barrier()
            if stop_here("exp%d" % l):
                break
        else:
            with ExitStack() as s:
                gfin = k.sb([128, D], F32, es=s)
                k.dma(gfin, g_bc_d[4])
                xt_r = Rot([k.sb([128, D], F32, es=s) for _ in range(5)])
                junk = k.sb([128, D], F32, es=s)
                ss_pool = Rot([k.sb([128, 1], F32, es=s) for _ in range(12)])
                def finA(n):
                    c0 = n * 128
                    xt = xt_r.get()
                    k.dma(xt, rows_key(X, n)[c0:c0 + 128, :])
                    ss = ss_pool.get()
                    k.act(junk, xt, AF.Square, accum=ss)
                    return (xt, ss)

                def finB(n, c_):
                    xt, ss = c_
                    c0 = n * 128
                    rstd = ss_pool.get()
                    k.rsq(rstd, ss, 1.0 / D, EPS)
                    k.stt(xt, xt, rstd, gfin, ALU.mult, ALU.mult)
                    k.dma(out_d.sl(n, np.s_[c0 - NTC:c0 - NTC + 128, :]), xt, q="pool")
                tl_ = list(range(2, NTILE))
                q_ = [finA(tl_[0]), finA(tl_[1])]
                for i_, n in enumerate(tl_):
                    if i_ + 2 < len(tl_):
                        q_.append(finA(tl_[i_ + 2]))
                    finB(n, q_.pop(0))
        k.barrier()
        print("ninst", k.ninst, k.cnt, flush=True)
    return nc


_NC_CACHE = {}


def kernel(**inputs):
    inp = {kk: np.asarray(v) for kk, v in inputs.items()}
    shared = _prep_shared(inp)
    if "nc" not in _NC_CACHE:
        _NC_CACHE["nc"] = build()
    nc = _NC_CACHE["nc"]
    in_maps = []
    for b in range(8):
        m = dict(shared)
        m.update(_prep_core(inp, b))
        in_maps.append(m)
    res = run_bass_kernel_spmd(nc, in_maps, core_ids=list(range(8)))
    out = np.stack([np.asarray(r["out"], dtype=np.float32) for r in res.results], 0)
    return out
```

```python
import numpy as np
from contextlib import ExitStack
import concourse.bass as bass
import concourse.mybir as mybir
from concourse.bass_utils import run_bass_kernel_spmd

F32 = mybir.dt.float32
BF16 = mybir.dt.bfloat16
I32 = mybir.dt.int32
U32 = mybir.dt.uint32
AF = mybir.ActivationFunctionType
ALU = mybir.AluOpType
AX = mybir.AxisListType


class T:
    __slots__ = ("ap", "key")

    def __init__(self, ap, key):
        self.ap = ap
        self.key = key

    def __getitem__(self, idx):
        return T(self.ap[idx], self.key)

    def sub(self, suffix):
        return T(self.ap, (self.key, suffix))

    def sl(self, suffix, idx):
        return T(self.ap[idx], (self.key, suffix))


def _key(x):
    return x.key if isinstance(x, T) else x


class KB:
    CE = ("pe", "act", "dve", "pool")

    def __init__(self, nc, es, nds=14):
        self.nc = nc
        self.es = es
        self.E = {"pe": nc.tensor, "act": nc.scalar, "dve": nc.vector,
                  "pool": nc.gpsimd, "sp": nc.sync}
        self.csem = {e: es.enter_context(nc.semaphore("c_" + e)) for e in self.CE}
        self.cnt = {e: 0 for e in self.CE}
        self.NDS = nds
        self.dsem = [es.enter_context(nc.semaphore("d%d" % i)) for i in range(nds)]
        self.dcnt = [0] * nds
        self.dnext = 0
        self.waited = {e: {} for e in self.E}
        self.lastw = {}
        self.readers = {}
        self.nalloc = 0
        self.ninst = 0

    def sb(self, shape, dtype, name=None, es=None):
        self.nalloc += 1
        name = name or ("t%d" % self.nalloc)
        t = (es or self.es).enter_context(self.nc.sbuf_tensor(name + "_%d" % self.nalloc, list(shape), dtype))
        return T(t[:], name + "_%d" % self.nalloc)

    def ps(self, shape, dtype, name=None, es=None):
        self.nalloc += 1
        name = name or ("p%d" % self.nalloc)
        t = (es or self.es).enter_context(self.nc.psum_tensor(name + "_%d" % self.nalloc, list(shape), dtype))
        return T(t[:], name + "_%d" % self.nalloc)

    def dram(self, name, shape, dtype, kind="Internal"):
        t = self.nc.dram_tensor(name, list(shape), dtype, kind=kind)
        return T(t.ap(), name)

    def _semobj(self, semkey):
        return self.csem[semkey[1]] if semkey[0] == "c" else self.dsem[semkey[1]]

    def _wait(self, e, semkey, val):
        if self.waited[e].get(semkey, 0) >= val:
            return
        self.waited[e][semkey] = val
        self.E[e].wait_ge(self._semobj(semkey), val)

    def _deps(self, e, reads, writes, is_dma):
        for r in reads:
            lw = self.lastw.get(_key(r))
            if lw is not None:
                self._wait(e, lw[0], lw[1])
        for w in writes:
            k = _key(w)
            lw = self.lastw.get(k)
            if lw is not None:
                if not (lw[0] == ("c", e) and e == "pe" and not is_dma):
                    self._wait(e, lw[0], lw[1])
            for sk, v in self.readers.get(k, {}).items():
                if sk == ("c", e) and e == "pe" and not is_dma:
                    continue
                self._wait(e, sk, v)

    def _record(self, tok, reads, writes):
        for r in reads:
            d = self.readers.setdefault(_key(r), {})
            if d.get(tok[0], 0) < tok[1]:
                d[tok[0]] = tok[1]
        for w in writes:
            k = _key(w)
            self.lastw[k] = tok
            self.readers[k] = {}

    def op(self, e, fn, reads=(), writes=()):
        self._deps(e, reads, writes, False)
        ins = fn(self.E[e])
        self.cnt[e] += 1
        ins.then_inc(self.csem[e], 1)
        self._record((("c", e), self.cnt[e]), reads, writes)
        self.ninst += 1
        return ins

    def dma(self, out, in_, q="sp", fn=None, reads=None, writes=None, **kw):
        reads = [in_] if reads is None else reads
        writes = [out] if writes is None else writes
        slot = self.dnext
        self.dnext = (slot + 1) % self.NDS
        if self.dcnt[slot] > 0:
            self._wait(q, ("d", slot), 16 * self.dcnt[slot])
        self._deps(q, reads, writes, True)
        if fn is None:
            ins = self.E[q].dma_start(out=out.ap, in_=in_.ap, **kw)
        else:
            ins = fn(self.E[q])
        self.dcnt[slot] += 1
        ins.then_inc(self.dsem[slot], 16)
        self._record((("d", slot), 16 * self.dcnt[slot]), reads, writes)
        self.ninst += 1
        return ins

    def barrier(self):
        for e in self.E:
            for e2 in self.CE:
                if e2 != e and self.cnt[e2] > 0:
                    self._wait(e, ("c", e2), self.cnt[e2])
            for s in range(self.NDS):
                if self.dcnt[s] > 0:
                    self._wait(e, ("d", s), 16 * self.dcnt[s])

    def mm(self, out, lhsT, rhs, start=True, stop=True, extra_reads=()):
        return self.op("pe", lambda e: e.matmul(out.ap, lhsT.ap, rhs.ap, start=start, stop=stop),
                       reads=[lhsT, rhs, *extra_reads], writes=[out])

    def tr(self, out, in_, ident):
        return self.op("pe", lambda e: e.transpose(out.ap, in_.ap, ident.ap),
                       reads=[in_, ident], writes=[out])

    def act(self, out, in_, func, bias=None, scale=None, accum=None, e="act"):
        kw = {}
        rd = [in_]
        wr = [out]
        if bias is not None:
            if isinstance(bias, T):
                kw["bias"] = bias.ap
                rd.append(bias)
            else:
                kw["bias"] = bias
        if scale is not None:
            if isinstance(scale, T):
                kw["scale"] = scale.ap
                rd.append(scale)
            else:
                kw["scale"] = scale
        if accum is not None:
            kw["accum_out"] = accum.ap
            wr.append(accum)
        return self.op(e, lambda en: en.activation(out.ap, in_.ap, func, **kw), reads=rd, writes=wr)

    def tt(self, out, a, b, op, e="dve"):
        return self.op(e, lambda en: en.tensor_tensor(out.ap, a.ap, b.ap, op), reads=[a, b], writes=[out])

    def ts(self, out, a, s1, op0, s2=None, op1=None, e="dve", accum=None):
        rd = [a]
        wr = [out]
        v1 = s1
        v2 = s2
        if isinstance(s1, T):
            rd.append(s1)
            v1 = s1.ap
        if isinstance(s2, T):
            rd.append(s2)
            v2 = s2.ap
        kw = {}
        if op1 is not None:
            kw["op1"] = op1
        if accum is not None:
            kw["accum_out"] = accum.ap
            wr.append(accum)
        return self.op(e, lambda en: en.tensor_scalar(out.ap, a.ap, v1, v2, op0, **kw), reads=rd, writes=wr)

    def stt(self, out, a, s, b, op0, op1, e="dve"):
        rd = [a, b]
        v = s
        if isinstance(s, T):
            rd.append(s)
            v = s.ap
        return self.op(e, lambda en: en.scalar_tensor_tensor(out.ap, a.ap, v, b.ap, op0, op1), reads=rd, writes=[out])

    def copy(self, out, in_, e="dve"):
        if e == "act":
            return self.op(e, lambda en: en.activation(out.ap, in_.ap, AF.Copy), reads=[in_], writes=[out])
        return self.op(e, lambda en: en.tensor_copy(out.ap, in_.ap), reads=[in_], writes=[out])

    def memset(self, out, v, e="dve"):
        return self.op(e, lambda en: en.memset(out.ap, v), reads=[], writes=[out])

    def rsq(self, dst, src, mul, add):
        self.ts(dst, src, mul, ALU.mult, add, ALU.add)
        self.act(dst, dst, AF.Sqrt)
        self.op("dve", lambda e: e.reciprocal(dst.ap, dst.ap), reads=[dst], writes=[dst])

L_ = 2
D = 1024
NTX = 4096
NTC = 256
NT = NTX + NTC
NTILE = NT // 128
NCOL = 2464
SCALE_MLA = float((64 + 32) ** -0.5)
LN2 = float(np.log(2.0))


def _rope_tables():
    t = np.arange(NTX)
    row = (t // 64).astype(np.float32)
    col = (t % 64).astype(np.float32)

    def tab(dh_half):
        dh = dh_half
        inv = 10000.0 ** (-np.arange(0, dh, 2, dtype=np.float32) / dh)
        return inv

    def build(dtot):
        half = dtot // 2
        inv = tab(half)
        nf = half // 2
        cos = np.zeros((dtot, NTX), np.float32)
        sins = np.zeros((dtot, NTX), np.float32)
        perm = np.zeros((dtot, dtot), np.float32)
        for part, pos in ((0, row), (1, col)):
            base = part * half
            ang = pos[None, :] * inv[:, None]
            c, s = np.cos(ang), np.sin(ang)
            for j in range(nf):
                cos[base + j] = c[j]
                cos[base + nf + j] = c[j]
                sins[base + j] = -s[j]
                sins[base + nf + j] = s[j]
                perm[base + nf + j, base + j] = 1.0
                perm[base + j, base + nf + j] = 1.0
        return cos, sins, perm

    c32, s32, p32 = build(32)
    c64, s64, p64 = build(64)
    c128 = np.concatenate([c64, c64], 0)
    s128 = np.concatenate([s64, s64], 0)
    p128 = np.zeros((128, 128), np.float32)
    p128[:64, :64] = p64
    p128[64:, 64:] = p64
    rope32 = np.stack([np.tile(c32, (4, 1)), np.tile(s32, (4, 1))]).astype(np.float32)
    rope128 = np.stack([c128, s128]).astype(np.float32)
    return rope32, rope128, p32, p128


_CST = {}


def _cst_layout():
    off = 0
    for name, w in (("ident", 128), ("A", 128), ("B", 128), ("C1", 128), ("C2", 128),
                    ("colK1", 1), ("colK2", 1), ("iota", 512), ("triU", 128),
                    ("mprev", 128), ("mnext", 128), ("p128", 128), ("p32", 32), ("pswap", 128), ("bmat", 128), ("p32x4", 128),
                    ("tokid", NTILE * 16 * 2)):
        _CST[name] = (off, w)
        off += w
    return off


NCST = _cst_layout()


def _const_table():
    rope32, rope128, p32, p128 = _rope_tables()
    cst = np.zeros((128, NCST), np.float32)
    p = np.arange(128, dtype=np.float32)[:, None]
    c = np.arange(128, dtype=np.float32)[None, :]

    def put(name, arr):
        o, w = _CST[name]
        cst[:arr.shape[0], o:o + w] = arr
    put("ident", np.eye(128, dtype=np.float32))
    put("A", np.maximum(c - p, 0.0))
    put("B", np.maximum(p - c, 0.0))
    put("C1", np.broadcast_to(c + 1.0, (128, 128)))
    put("C2", np.broadcast_to(128.0 - c, (128, 128)))
    put("colK1", 127.0 - p)
    put("colK2", p)
    put("iota", np.broadcast_to(np.arange(512, dtype=np.float32)[None, :], (128, 512)))
    put("triU", (p <= c).astype(np.float32))
    put("mprev", (p >= c).astype(np.float32))
    put("mnext", (p <= c).astype(np.float32))
    put("p128", p128)
    put("p32", p32)
    p32x4 = np.zeros((128, 128), np.float32)
    for i_ in range(4):
        p32x4[i_ * 32:(i_ + 1) * 32, i_ * 32:(i_ + 1) * 32] = p32
    put("p32x4", p32x4)
    put("pswap", (np.arange(128)[:, None] == (np.arange(128)[None, :] + 64) % 128).astype(np.float32))
    put("bmat", (np.arange(128)[:, None] // 8 == np.arange(128)[None, :] // 8).astype(np.float32))
    rows = (np.arange(NTILE)[None, :] * 128 + np.arange(128)[:, None])
    tok = np.stack([rows // 64, rows % 64], -1).astype(np.float32)
    tok = np.broadcast_to(tok[:, :, None, :], (128, NTILE, 16, 2)).reshape(128, -1)
    put("tokid", tok)
    return cst, rope32, rope128


def _prep_shared(inp):
    f = lambda a: np.ascontiguousarray(a, dtype=np.float32)
    sh = {}
    w_in = inp["w_in"]
    s = np.cumsum([0, 256, 256, 256, 256, 256, 128, 32, 256, 128, 128])
    rq, rk, rv, rg, cq, ckv, kr, wq, wk, wv = [w_in[:, :, s[i]:s[i + 1]] for i in range(10)]
    wk2 = np.concatenate([wk[:, :, 0:64], wk[:, :, 0:64], wk[:, :, 64:128], wk[:, :, 64:128]], -1)
    wv2 = np.concatenate([wv[:, :, 0:64], wv[:, :, 0:64], wv[:, :, 64:128], wv[:, :, 64:128]], -1)
    sh["w_in_r"] = f(np.concatenate([rq, rk, cq, ckv, wq, wk2, kr, rk, rv, rg, wv2], -1))
    assert sh["w_in_r"].shape[-1] == NCOL
    uq = inp["mla_w_uq"]
    sh["w_uq_n"] = f(uq[:, :, :, :64].reshape(L_, 256, 512))
    sh["w_uq_r"] = f(uq[:, :, :, 64:].reshape(L_, 256, 256))
    sh["w_uk"] = f(inp["mla_w_uk"].reshape(L_, 128, 512))
    sh["w_uv"] = f(inp["mla_w_uv"].reshape(L_, 128, 512))
    sh["w_out"] = f(inp["w_out"])
    sh["router_w"] = f(inp["router_w"])
    sh["ada_w"] = f(inp["ada_w"])
    sh["ada_b"] = f(inp["ada_b"].reshape(L_, 1, 6 * D))
    sh["exp_wg"] = f(inp["exp_w_gate"])
    sh["exp_wu"] = f(inp["exp_w_up"])
    sh["exp_wd"] = f(inp["exp_w_down"])
    gb = np.stack([inp["norm1_g"][0], inp["norm2_g"][0], inp["norm1_g"][1], inp["norm2_g"][1], inp["final_g"]])
    sh["g_bc"] = f(np.broadcast_to(gb[:, None, :], (5, 128, D)))
    cols = []
    for l in range(L_):
        qg = inp["mla_qnorm_g"][l].reshape(2, 128).T
        kg = inp["mla_kvnorm_g"][l].reshape(1, 128).T
        df, db, sk = inp["ret_decay_f"][l], inp["ret_decay_b"][l], inp["win_sink"][l]
        rep = np.broadcast_to(np.concatenate([df, db, sk])[None, :], (128, 12))
        hp = (np.arange(128) >= 64).astype(np.int64)
        pp = np.stack([df[0 + hp], df[2 + hp], db[0 + hp], db[2 + hp]], -1)
        cols += [qg, kg, rep, pp]
    sh["small"] = f(np.concatenate(cols, -1))
    cst, rope32, rope128 = _const_table()
    sh["cst"] = cst
    sh["rope32"] = rope32
    sh["rope128"] = rope128
    return sh


def _prep_core(inp, b):
    f = lambda a: np.ascontiguousarray(a, dtype=np.float32)
    d = {}
    d["x0"] = f(np.concatenate([inp["ctx"][b], inp["x"][b]], 0))
    cv = np.stack([inp["c_ctx"], inp["c"][b]])
    cr = cv.reshape(2, 8, 128).transpose(0, 2, 1)
    d["crep"] = f(np.broadcast_to(cr[:, :, :, None], (2, 128, 8, 128)))
    return d

class Rot:
    def __init__(self, tiles):
        self.t = tiles
        self.i = 0

    def get(self):
        t = self.t[self.i % len(self.t)]
        self.i += 1
        return t


def TD(t, pattern, **kw):
    return T(t.ap.rearrange(pattern, **kw), t.key)


def build(upto=None, dbg=False, nlayers=L_):
    nc = bass.Bass("TRN2", target_bir_lowering=False)
    es0 = ExitStack()
    with es0:
        k = KB(nc, es0)
        kind_dbg = "ExternalOutput" if dbg else "Internal"
        din = lambda n, s, dt=F32: k.dram(n, s, dt, kind="ExternalInput")
        x0 = din("x0", [NT, D])
        crep_d = din("crep", [2, 128, 8, 128])
        w_in_d = din("w_in_r", [L_, D, NCOL])
        w_uq_n_d = din("w_uq_n", [L_, 256, 512])
        w_uq_r_d = din("w_uq_r", [L_, 256, 256])
        w_uk_d = din("w_uk", [L_, 128, 512])
        w_uv_d = din("w_uv", [L_, 128, 512])
        w_out_d = din("w_out", [L_, D, D])
        router_d = din("router_w", [L_, D, 16])
        ada_w_d = din("ada_w", [L_, D, 6 * D])
        ada_b_d = din("ada_b", [L_, 1, 6 * D])
        wg_d = din("exp_wg", [L_, 16, D, 768])
        wu_d = din("exp_wu", [L_, 16, D, 768])
        wd_d = din("exp_wd", [L_, 16, 768, D])
        g_bc_d = din("g_bc", [5, 128, D])
        small_d = din("small", [128, L_ * 19])
        cst_d = din("cst", [128, NCST])
        rope32_d = din("rope32", [2, 128, NTX])
        rope128_d = din("rope128", [2, 128, NTX])
        out_d = k.dram("out", [NTX, D], F32, kind="ExternalOutput")
        X = k.dram("X", [NT, D], F32, kind=kind_dbg)
        MODBC = k.dram("MODBC", [2, 6, 128, D], F32, kind=kind_dbg)
        QT = k.dram("QT", [2, 128, NT], BF16, kind=kind_dbg)
        KT = k.dram("KT", [2, 128, NT], BF16, kind=kind_dbg)
        TMo = k.dram("TMo", [NT, 1024], BF16, kind=kind_dbg)
        QN = k.dram("QN", [4, 128, NT], BF16, kind=kind_dbg)
        KN = k.dram("KN", [4, 128, NT], BF16, kind=kind_dbg)
        QR = k.dram("QR", [8, 32, NT], BF16, kind=kind_dbg)
        KVT = k.dram("KVT", [128, NT], BF16, kind=kind_dbg)
        KRT = k.dram("KRT", [32, NT], BF16, kind=kind_dbg)
        VP = k.dram("VP", [NT, 512], BF16, kind=kind_dbg)
        WQT = k.dram("WQT", [2, 128, NT], BF16, kind=kind_dbg)
        WKT = k.dram("WKT", [2, 128, NT], BF16, kind=kind_dbg)
        YT = k.dram("YT", [8, 128, NT], BF16, kind=kind_dbg)
        H2 = k.dram("H2", [NT, D], BF16, kind=kind_dbg)
        AFFD = k.dram("AFFD", [16, NTX], F32)
        THRD = k.dram("THRD", [128, 1], F32)

        cst = k.sb([128, NCST], F32, "cst")
        k.dma(cst, cst_d)
        small = k.sb([128, L_ * 19], F32, "small")
        k.dma(small, small_d)

        def C(name, rows=128):
            o, w = _CST[name]
            return cst[0:rows, o:o + w]
        ident = C("ident")
        ones_f = k.sb([128, 128], F32, "ones_f")
        k.memset(ones_f, 1.0)
        ones_b = k.sb([128, 128], BF16, "ones_b")
        k.memset(ones_b, 1.0)
        ident_b = k.sb([128, 128], BF16, "ident_b")
        k.copy(ident_b, ident)
        triU_b = k.sb([128, 128], BF16, "triU_b")
        k.copy(triU_b, C("triU"))
        mprev_b = k.sb([128, 128], BF16, "mprev_b")
        k.copy(mprev_b, C("mprev"))
        mnext_b = k.sb([128, 128], BF16, "mnext_b")
        k.copy(mnext_b, C("mnext"))
        Rtab = k.sb([128, NTILE, 16, 4], BF16, "Rtab")
        o_tok, w_tok = _CST["tokid"]
        k.copy(Rtab[:, :, :, 0:2], T(cst.ap[:, o_tok:o_tok + w_tok].rearrange("p (n e c) -> p n e c", n=NTILE, e=16), cst.key))
        affT = k.sb([128, NTILE, 16], F32, "affT")
        posm = k.sb([128, NTILE, 16], F32, "posm")

        EPS = 1e-6
        blocks = [(0, 256, 0)] + [(256 + 512 * i, 512, 1) for i in range(8)]

        def rows_key(t, n):
            return t.sub(("r", n))

        def stop_here(name):
            return upto is not None and upto == name

        def rstd_from_ss(ss, n_feat, es, eps=EPS):
            r = k.sb([128, 1], F32, es=es)
            k.rsq(r, ss, 1.0 / n_feat, eps)
            return r

        for l in range(nlayers):
            need_ctx = l < L_ - 1
            sm0 = l * 19
            Xsrc = x0 if l == 0 else X
            with ExitStack() as s:
                crep = k.sb([128, 2, 8, 128], F32, es=s)
                for st in range(2):
                    k.dma(crep[:, st], crep_d[st])
                sil = k.sb([128, 2, 8, 128], F32, es=s)
                k.act(sil, crep, AF.Silu)
                wb = Rot([k.sb([128, 8, 512], F32, es=s) for _ in range(3)])
                br = Rot([k.sb([1, 512], F32, es=s) for _ in range(2)])
                pp = Rot([k.ps([128, 512], F32, es=s) for _ in range(4)])
                ob = Rot([k.sb([128, 512], F32, es=s) for _ in range(4)])
                aw = TD(ada_w_d[l], "(kc p) n -> p kc n", p=128)
                for nb in range(12):
                    w = wb.get()
                    for q4 in range(4):
                        k.dma(w.sl(q4, np.s_[:, q4 * 2:(q4 + 1) * 2, :]), aw[:, q4 * 2:(q4 + 1) * 2, nb * 512:(nb + 1) * 512])
                    b_ = br.get()
                    k.dma(b_, ada_b_d[l, :, nb * 512:(nb + 1) * 512])
                    for st in range(2):
                        ps = pp.get()
                        for kc in range(8):
                            k.mm(ps, sil[:, st, kc, :], w.sl(kc // 2, np.s_[:, kc, :]), start=(kc == 0), stop=False)
                        k.mm(ps, ones_f[0:1, :], b_, start=False, stop=True)
                        o = ob.get()
                        k.copy(o, ps, e=("act" if st == 0 else "dve"))
                        j, half = nb // 2, nb % 2
                        k.dma(MODBC.sl((st, j), np.s_[st, j, :, half * 512:(half + 1) * 512]), o)
            k.barrier()
            if stop_here("mod%d" % l):
                break

            def load_mod(st, j, es, eng_q="sp"):
                t = k.sb([128, D], F32, es=es)
                k.dma(t, MODBC.sl((st, j), np.s_[st, j]))
                return t

            def load_gs(st, j_scale, gidx, es):
                sc = load_mod(st, j_scale, es)
                gb = k.sb([128, D], F32, es=es)
                k.dma(gb, g_bc_d[gidx])
                k.stt(sc, sc, 1.0, gb, ALU.add, ALU.mult)
                return sc

            def load_cast(dst, src, stg, e="pool"):
                t = stg.get()
                sh = list(src.ap.shape)
                tv = t[0:sh[0], 0:sh[1]]
                k.dma(tv, src)
                k.copy(dst, tv, e=e)

            with ExitStack() as s:
                w_in = k.sb([128, 8, NCOL], BF16, es=s)
                stg = Rot([k.sb([128, NCOL], F32, es=s) for _ in range(2)])
                for kc in range(8):
                    load_cast(w_in[:, kc, :], w_in_d[l, kc * 128:(kc + 1) * 128, :], stg, e=("pool" if kc % 2 else "dve"))
                w_uq_n = k.sb([128, 2, 512], BF16, es=s)
                w_uq_r = k.sb([128, 2, 256], BF16, es=s)
                for kc in range(2):
                    load_cast(w_uq_n[:, kc, :], w_uq_n_d[l, kc * 128:(kc + 1) * 128, :], stg)
                    load_cast(w_uq_r[:, kc, :], w_uq_r_d[l, kc * 128:(kc + 1) * 128, :], stg)
                w_uk = k.sb([128, 512], BF16, es=s)
                load_cast(w_uk, w_uk_d[l], stg)
                w_uv = k.sb([128, 512], BF16, es=s)
                load_cast(w_uv, w_uv_d[l], stg)
                gs1 = [load_gs(st, 1, l * 2 + 0, s) for st in range(2)]
                sh1 = [load_mod(st, 0, s) for st in range(2)]
                qg = small[:, sm0 + 0:sm0 + 2]
                kg = small[:, sm0 + 2:sm0 + 3]
                xt_r = Rot([k.sb([128, D], F32, es=s) for _ in range(3)])
                xs_r = Rot([k.sb([128, D], F32, es=s) for _ in range(3)])
                junk = k.sb([128, D], F32, es=s)
                ss_pool = Rot([k.sb([128, 1], F32, es=s) for _ in range(8)])
                hT_r = Rot([k.sb([128, 8, 512], BF16, es=s) for _ in range(2)])
                ptr = Rot([k.ps([128, 8, 128], BF16, es=s) for _ in range(2)])
                xb_r = Rot([k.sb([128, D], BF16, es=s) for _ in range(5)])
                pfm = Rot([k.ps([128, 512], F32, es=s) for _ in range(3)])
                pex = Rot([k.ps([128, 512], F32, es=s) for _ in range(3)])
                ob16 = Rot([k.sb([128, 512], BF16, es=s) for _ in range(6)])
                f32t = Rot([k.sb([128, 512], F32, es=s) for _ in range(6)])
                rp32 = Rot([k.sb([128, 2, 512], F32, es=s) for _ in range(2)])
                rp128 = Rot([k.sb([128, 2, 512], F32, es=s) for _ in range(2)])
                cqg = k.sb([128, 2, 512], BF16, es=s)
                rstdq = k.sb([128, 512], F32, es=s)
                kvT_sb = k.sb([128, 512], BF16, es=s)
                tmo = Rot([k.sb([128, 1024], BF16, es=s) for _ in range(2)])

                def bc_rstd(dst, sq_list, nfeat, n):
                    ps = pex.get()
                    for i, sq in enumerate(sq_list):
                        k.mm(ps[:, :n], ones_f, sq, start=(i == 0), stop=(i == len(sq_list) - 1))
                    k.rsq(dst[:, :n], ps[:, :n], 1.0 / nfeat, EPS)

                def rope_apply(src_f32, M, n, tabs, perm, out_bf):
                    ps = pex.get()
                    k.mm(ps[0:M, :n], perm, src_f32[0:M, :n])
                    t1 = f32t.get()
                    k.tt(t1[0:M, :n], src_f32[0:M, :n], tabs[0:M, 0, :n], ALU.mult)
                    t2 = f32t.get()
                    k.tt(t2[0:M, :n], ps[0:M, :n], tabs[0:M, 1, :n], ALU.mult)
                    k.tt(out_bf[0:M, :n], t1[0:M, :n], t2[0:M, :n], ALU.add, e="pool")

                class NormState:
                    pass

                def norm_begin(blk):
                    (r0, n, st) = blk
                    ns = NormState()
                    ns.blk = blk
                    ns.hT = hT_r.get()
                    ns.r32 = ns.r128 = None
                    ns.xbs = []
                    if st == 1:
                        t0 = r0 - NTC
                        ns.r32 = rp32.get()
                        k.dma(ns.r32[:, :, :n], TD(rope32_d, "a p t -> p a t")[:, :, t0:t0 + n])
                        ns.r128 = rp128.get()
                        k.dma(ns.r128[:, :, :n], TD(rope128_d, "a p t -> p a t")[:, :, t0:t0 + n])
                    return ns

                def norm_ew(ns, ti):
                    (r0, n, st) = ns.blk
                    if ti >= n // 128:
                        return
                    rr = r0 + ti * 128
                    xt = xt_r.get()
                    k.dma(xt, rows_key(Xsrc, rr // 128)[rr:rr + 128, :])
                    ss = ss_pool.get()
                    k.act(junk, xt, AF.Square, accum=ss)
                    rstd = ss_pool.get()
                    k.rsq(rstd, ss, 1.0 / D, EPS)
                    xs = xs_r.get()
                    k.stt(xs, xt, rstd, gs1[st], ALU.mult, ALU.mult)
                    xb = xb_r.get()
                    k.tt(xb, xs, sh1[st], ALU.add)
                    ns.xbs.append(xb)

                def norm_tr(ns):
                    (r0, n, st) = ns.blk
                    for ti, xb in enumerate(ns.xbs):
                        pt = ptr.get()
                        for kc in range(8):
                            k.tr(pt[:, kc, :], xb[:, kc * 128:(kc + 1) * 128], ident_b)
                        k.copy(ns.hT[:, :, ti * 128:(ti + 1) * 128], pt, e="act")
                    return (ns.hT, ns.r32, ns.r128)

                def proj_phase(blk, ctx_, hook):
                    (r0, n, st) = blk
                    nt = n // 128
                    hT, r32, r128 = ctx_
                    def fm(c, M=128):
                        ps = pfm.get()
                        for kc in range(8):
                            k.mm(ps[0:M, :n], w_in[:, kc, c * 128:c * 128 + M], hT[:, kc, :n], start=(kc == 0), stop=(kc == 7))
                        return ps
                    for c in range(4):
                        ps = fm(c)
                        o = ob16.get()
                        k.copy(o[:, :n], ps[:, :n], e=("act" if c % 2 == 0 else "dve"))
                        dst = (QT if c < 2 else KT)
                        k.dma(dst.sl((c % 2, r0), np.s_[c % 2, :, r0:r0 + n]), o[:, :n])
                    hook(0)
                    sqs = []
                    for c2 in range(2):
                        ps = fm(4 + c2)
                        sq = f32t.get()
                        k.act(sq[:, :n], ps[:, :n], AF.Square)
                        sqs.append(sq[:, :n])
                        k.act(cqg[:, c2, :n], ps[:, :n], AF.Copy, scale=qg[:, c2:c2 + 1])
                    bc_rstd(rstdq, sqs, 256, n)
                    for pr in range(4):
                        ps = pex.get()
                        for c2 in range(2):
                            k.mm(ps[:, :n], w_uq_n[:, c2, pr * 128:(pr + 1) * 128], cqg[:, c2, :n], start=(c2 == 0), stop=(c2 == 1))
                        qn = ob16.get()
                        k.tt(qn[:, :n], ps[:, :n], rstdq[:, :n], ALU.mult)
                        k.dma(QN.sl((pr, r0), np.s_[pr, :, r0:r0 + n]), qn[:, :n])
                    for g4 in range(2):
                        ps = pex.get()
                        for c2 in range(2):
                            k.mm(ps[:, :n], w_uq_r[:, c2, g4 * 128:(g4 + 1) * 128], cqg[:, c2, :n], start=(c2 == 0), stop=(c2 == 1))
                        qr = f32t.get()
                        k.tt(qr[:, :n], ps[:, :n], rstdq[:, :n], ALU.mult)
                        o = ob16.get()
                        if st == 1:
                            rope_apply(qr, 128, n, r32, C("p32x4"), o)
                        else:
                            k.copy(o[:, :n], qr[:, :n], e="pool")
                        for hh in range(4):
                            h = g4 * 4 + hh
                            k.dma(QR.sl((h, r0), np.s_[h, :, r0:r0 + n]), o[hh * 32:(hh + 1) * 32, :n])
                    hook(1)
                    ps = fm(6)
                    sq = f32t.get()
                    k.act(sq[:, :n], ps[:, :n], AF.Square)
                    kvg = f32t.get()
                    k.act(kvg[:, :n], ps[:, :n], AF.Copy, scale=kg[:, 0:1])
                    rk_ = f32t.get()
                    bc_rstd(rk_, [sq[:, :n]], 128, n)
                    k.tt(kvT_sb[:, :n], kvg[:, :n], rk_[:, :n], ALU.mult)
                    k.dma(KVT.sl(r0, np.s_[:, r0:r0 + n]), kvT_sb[:, :n])
                    for pr in range(4):
                        ps = pex.get()
                        k.mm(ps[:, :n], w_uk[:, pr * 128:(pr + 1) * 128], kvT_sb[:, :n])
                        o = ob16.get()
                        k.copy(o[:, :n], ps[:, :n], e=("act" if pr % 2 == 0 else "dve"))
                        k.dma(KN.sl((pr, r0), np.s_[pr, :, r0:r0 + n]), o[:, :n])
                    for ti in range(nt):
                        ps = pex.get()
                        k.mm(ps, kvT_sb[:, ti * 128:(ti + 1) * 128], w_uv)
                        o = ob16.get()
                        k.copy(o, ps, e="act")
                        rr = r0 + ti * 128
                        k.dma(VP.sl(rr // 128, np.s_[rr:rr + 128, :]), o)
                    hook(2)
                    for c in range(4):
                        ps = fm(7 + c)
                        o = ob16.get()
                        if st == 1:
                            sf = f32t.get()
                            k.copy(sf[:, :n], ps[:, :n], e="act")
                            rope_apply(sf, 128, n, r128, C("p128"), o)
                        else:
                            k.copy(o[:, :n], ps[:, :n], e="act")
                        dst = (WQT if c < 2 else WKT)
                        k.dma(dst.sl((c % 2, r0), np.s_[c % 2, :, r0:r0 + n]), o[:, :n])
                    ps = fm(11, 32)
                    o = ob16.get()
                    if st == 1:
                        sf = f32t.get()
                        k.copy(sf[0:32, :n], ps[0:32, :n], e="act")
                        rope_apply(sf, 32, n, r32, C("p32", 32), o)
                    else:
                        k.copy(o[0:32, :n], ps[0:32, :n], e="act")
                    k.dma(KRT.sl(r0, np.s_[:, r0:r0 + n]), o[0:32, :n])
                    hook(3)
                    for ti in range(nt):
                        o = tmo.get()
                        for hf in range(2):
                            ps = pfm.get()
                            c0 = 1440 + hf * 512
                            for kc in range(8):
                                k.mm(ps, hT[:, kc, ti * 128:(ti + 1) * 128], w_in[:, kc, c0:c0 + 512], start=(kc == 0), stop=(kc == 7))
                            k.copy(o[:, hf * 512:(hf + 1) * 512], ps, e=("act" if hf == 0 else "dve"))
                        rr = r0 + ti * 128
                        k.dma(TMo.sl(rr // 128, np.s_[rr:rr + 128, :]), o)
                ns0 = norm_begin(blocks[0])
                for ti_ in range(4):
                    norm_ew(ns0, ti_)
                pctx = norm_tr(ns0)
                for bi, blk in enumerate(blocks):
                    nsn = norm_begin(blocks[bi + 1]) if bi + 1 < len(blocks) else None
                    proj_phase(blk, pctx, (lambda i_: norm_ew(nsn, i_)) if nsn is not None else (lambda i_: None))
                    pctx = norm_tr(nsn) if nsn is not None else None
            k.barrier()
            if stop_here("s1_%d" % l):
                break
            es_mla_pre = ExitStack()
            vp2 = k.sb([128, NTILE, 8, 128], BF16, es=es_mla_pre)
            VPv = TD(VP, "(t p) (h d) -> p t h d", p=128, d=64)
            for par in range(2):
                k.memset(vp2[:, :, par::2, (1 - par) * 64:(2 - par) * 64], 1.0, e="pool")
                for h in range(par, 8, 2):
                    k.dma(vp2[:, :, h, par * 64:(par + 1) * 64], VPv[:, :, h, :])
            with ExitStack() as s:
                lg_rep = k.sb([128, 8], F32, es=s)
                lg_pp = k.sb([128, 4], F32, es=s)
                for dst, src in ((lg_rep, small[:, sm0 + 3:sm0 + 11]), (lg_pp, small[:, sm0 + 15:sm0 + 19])):
                    k.act(dst, src, AF.Exp, scale=LN2)
                    k.ts(dst, dst, -1.0, ALU.mult, 1.0, ALU.add)
                    k.act(dst, dst, AF.Ln)
                LN8 = float(np.log(0.125))
                DT = k.sb([128, 4, 128], F32, es=s)
                tmpA = k.sb([128, 128], F32, es=s)
                for h in range(4):
                    k.ts(tmpA, C("A"), lg_rep[:, h:h + 1], ALU.mult)
                    k.stt(tmpA, C("B"), lg_rep[:, 4 + h:5 + h], tmpA, ALU.mult, ALU.add)
                    k.act(DT[:, h, :], tmpA, AF.Exp)
                    k.ts(DT[:, h, :], DT[:, h, :], 0.125, ALU.mult)
                QWF = k.sb([128, 2, 128], F32, es=s)
                QWB = k.sb([128, 2, 128], F32, es=s)
                gch = k.sb([128, 4], F32, es=s)
                for pr in range(2):
                    k.act(QWF[:, pr, :], C("C1"), AF.Exp, scale=lg_pp[:, pr:pr + 1])
                    k.act(QWB[:, pr, :], C("C2"), AF.Exp, scale=lg_pp[:, 2 + pr:3 + pr])
                k.act(gch, lg_pp, AF.Exp, scale=128.0)
                KWF = k.sb([128, 256], F32, es=s)
                KWB = k.sb([128, 256], F32, es=s)
                kcol = k.sb([128, 8], F32, es=s)
                for h in range(4):
                    k.act(kcol[:, h:h + 1], C("colK1"), AF.Exp, scale=lg_rep[:, h:h + 1])
                    k.act(kcol[:, 4 + h:5 + h], C("colK2"), AF.Exp, scale=lg_rep[:, 4 + h:5 + h])
                k.ts(kcol, kcol, 0.125, ALU.mult)
                for h in range(4):
                    k.copy(KWF[:, h * 64:(h + 1) * 64], T(kcol.ap[:, h:h + 1].to_broadcast([128, 64]), kcol.key))
                    k.copy(KWB[:, h * 64:(h + 1) * 64], T(kcol.ap[:, 4 + h:5 + h].to_broadcast([128, 64]), kcol.key))
                KVd = k.sb([128, NTILE, 4, 64], F32, es=s)
                Sfb = k.sb([128, NTILE, 2, 64], BF16, es=s)
                Sbb = k.sb([128, NTILE, 2, 64], BF16, es=s)
                kv_r = Rot([k.sb([128, 512], BF16, es=s) for _ in range(3)])
                kw_r = Rot([k.sb([128, 2, 256], BF16, es=s) for _ in range(2)])
                pkv = Rot([k.ps([128, 4, 128], F32, es=s) for _ in range(2)])
                fwd_order = list(range(NTILE))
                bwd_order = [1, 0] + list(range(NTILE - 1, 1, -1))
                r1_order = []
                for a_, b_ in zip(fwd_order, bwd_order):
                    for t_ in (a_, b_):
                        if t_ not in r1_order:
                            r1_order.append(t_)
                curs = []
                for d_ in range(2):
                    c_ = k.sb([128, 2, 64], F32, es=s)
                    k.memset(c_, 0.0)
                    curs.append(c_)
                ptrs = [0, 0]
                done = set()

                def scan_step(d_, n):
                    Sb_ = Sfb if d_ == 0 else Sbb
                    cur = curs[d_]
                    k.copy(Sb_.sl(n, np.s_[:, n, :, :]), cur)
                    for pr in range(2):
                        k.stt(cur[:, pr, :], cur[:, pr, :], gch[:, d_ * 2 + pr:d_ * 2 + pr + 1],
                              KVd.sl(n, np.s_[:, n, d_ * 2 + pr, :]), ALU.mult, ALU.add)
                for n in r1_order:
                    kvt = kv_r.get()
                    k.dma(kvt, TMo.sl(n, np.s_[n * 128:(n + 1) * 128, 0:512]))
                    kw = kw_r.get()
                    k.tt(kw[:, 0, :], kvt[:, 0:256], KWF, ALU.mult)
                    k.tt(kw[:, 1, :], kvt[:, 0:256], KWB, ALU.mult)
                    ps = pkv.get()
                    for d_ in range(2):
                        for pr in range(2):
                            k.mm(ps[:, d_ * 2 + pr, :], kw[:, d_, pr * 128:(pr + 1) * 128], kvt[:, 256 + pr * 128:256 + (pr + 1) * 128])
                    k.copy(KVd.sl(n, np.s_[0:64, n, :, :]), ps[0:64, :, 0:64], e="act")
                    k.copy(KVd.sl(n, np.s_[64:128, n, :, :]), ps[64:128, :, 64:128], e="act")
                    done.add(n)
                    for d_, order in ((0, fwd_order), (1, bwd_order)):
                        while ptrs[d_] < NTILE and order[ptrs[d_]] in done:
                            scan_step(d_, order[ptrs[d_]])
                            ptrs[d_] += 1
                qk_r = Rot([k.sb([128, 2, 2, 128], BF16, es=s) for _ in range(3)])
                vg_r = Rot([k.sb([128, 512], BF16, es=s) for _ in range(3)])
                qw_r = Rot([k.sb([128, 2, 2, 128], BF16, es=s) for _ in range(2)])
                sm_r = Rot([k.sb([128, 512], BF16, es=s) for _ in range(4)])
                pss = Rot([k.ps([128, 512], F32, es=s) for _ in range(2)] + [T(p_.ap.rearrange("p a b -> p (a b)"), p_.key) for p_ in pkv.t])
                psy = Rot([k.ps([128, 512], F32, es=s) for _ in range(2)])
                ysb_r = Rot([k.sb([128, 256], F32, es=s) for _ in range(3)])
                sq_r = Rot([k.sb([128, 256], F32, es=s) for _ in range(2)])
                st_r = Rot([k.sb([128, 16], F32, es=s) for _ in range(2)])
                yo_r = Rot([k.sb([128, 2, 128], BF16, es=s) for _ in range(2)])
                yb_r = Rot([k.sb([128, 256], BF16, es=s) for _ in range(2)])
                ptb2 = Rot([k.ps([128, 2, 128], BF16, es=s) for _ in range(2)])

                sgall = k.sb([128, NTILE, 256], BF16, es=s)
                for n in range(0 if need_ctx else 2, NTILE):
                    gt_ = vg_r.get()
                    k.dma(gt_[:, 0:256], TMo.sl(n, np.s_[n * 128:(n + 1) * 128, 512:768]))
                    k.act(sgall[:, n, :], gt_[:, 0:256], AF.Silu)

                def r2A(n):
                    c0 = n * 128
                    qk = qk_r.get()
                    k.dma(qk[:, 0], TD(QT, "c p t -> p c t")[:, :, c0:c0 + 128])
                    k.dma(qk[:, 1], TD(KT, "c p t -> p c t")[:, :, c0:c0 + 128])
                    vg = vg_r.get()
                    k.dma(vg, TMo.sl(n, np.s_[c0:c0 + 128, 256:768]))
                    qw = qw_r.get()
                    k.tt(qw[:, 0], qk[:, 0], QWF, ALU.mult)
                    k.tt(qw[:, 1], qk[:, 0], QWB, ALU.mult)
                    py = psy.get()
                    psA = pss.get()
                    psB = pss.get()
                    for h in range(4):
                        pr, off = h // 2, (h % 2) * 64
                        pb = psA if h % 2 == 0 else psB
                        k.mm(pb[:, pr * 128:(pr + 1) * 128], qk[off:off + 64, 1, pr, :], qk[off:off + 64, 0, pr, :])
                    sm = sm_r.get()
                    smv = T(sm.ap.rearrange("p (a c) -> p a c", a=4), sm.key)
                    k.tt(smv[:, 0::2, :], T(psA.ap[:, 0:256].rearrange("p (a c) -> p a c", a=2), psA.key), DT[:, 0::2, :], ALU.mult)
                    k.tt(smv[:, 1::2, :], T(psB.ap[:, 0:256].rearrange("p (a c) -> p a c", a=2), psB.key), DT[:, 1::2, :], ALU.mult)
                    for h in range(4):
                        pr, off = h // 2, (h % 2) * 64
                        yo_ = py[:, h * 64:(h + 1) * 64]
                        k.mm(yo_, sm[:, h * 128:(h + 1) * 128], vg[:, h * 64:(h + 1) * 64], start=True, stop=False)
                        k.mm(yo_, qw[off:off + 64, 0, pr, :], Sfb.sl(n, np.s_[off:off + 64, n, pr, :]), start=False, stop=False)
                        k.mm(yo_, qw[off:off + 64, 1, pr, :], Sbb.sl(n, np.s_[off:off + 64, n, pr, :]), start=False, stop=True)
                    ysb = ysb_r.get()
                    k.copy(ysb, py[:, 0:256], e="act")
                    return (ysb, sgall[:, n, :])

                def r2B(n, ctx_):
                    ysb, sg = ctx_
                    c0 = n * 128
                    stt_ = st_r.get()
                    k.op("dve", lambda e: e.reduce_sum(stt_.ap[:, 0:4], ysb.ap.rearrange("p (h d) -> p h d", h=4), AX.X), reads=[ysb], writes=[stt_])
                    sq = sq_r.get()
                    k.tt(sq, ysb, ysb, ALU.mult)
                    k.op("dve", lambda e: e.reduce_sum(stt_.ap[:, 4:8], sq.ap.rearrange("p (h d) -> p h d", h=4), AX.X), reads=[sq], writes=[stt_])
                    k.ts(stt_[:, 0:8], stt_[:, 0:8], 1.0 / 64, ALU.mult)
                    k.tt(stt_[:, 8:12], stt_[:, 0:4], stt_[:, 0:4], ALU.mult)
                    k.tt(stt_[:, 12:16], stt_[:, 4:8], stt_[:, 8:12], ALU.subtract)
                    k.rsq(stt_[:, 12:16], stt_[:, 12:16], 1.0, 1e-5)
                    for h in range(4):
                        k.ts(ysb[:, h * 64:(h + 1) * 64], ysb[:, h * 64:(h + 1) * 64], stt_[:, h:h + 1], ALU.subtract,
                             stt_[:, 12 + h:13 + h], ALU.mult)
                    yb = yb_r.get()
                    k.tt(yb, ysb, sg, ALU.mult)
                    pt = ptb2.get()
                    for c in range(2):
                        k.tr(pt[:, c, :], yb[:, c * 128:(c + 1) * 128], ident_b)
                    yo = yo_r.get()
                    k.copy(yo, pt, e="act")
                    k.dma(TD(YT, "c p t -> p c t").sl(("ret", n), np.s_[:, 0:2, c0:c0 + 128]), yo, q="pool")
                tiles_r = list(range(0 if need_ctx else 2, NTILE))
                pc_ = r2A(tiles_r[0])
                for ti_, n in enumerate(tiles_r):
                    nc_ = r2A(tiles_r[ti_ + 1]) if ti_ + 1 < len(tiles_r) else None
                    r2B(n, pc_)
                    pc_ = nc_
            k.barrier()
            if stop_here("ret%d" % l):
                es_mla_pre.close()
                break
            with ExitStack() as s:
                kh_l = [k.sb([128, NT], BF16, es=s) for _ in range(2)]
                for t_ in kh_l:
                    k.memset(t_, 0.0, e="pool")
                kh_r = Rot(kh_l)
                q_l = [k.sb([128, 512], BF16, es=s) for _ in range(3)]
                for t_ in q_l:
                    k.memset(t_, 0.0)
                q_r = Rot(q_l)
                rd_sets = []
                for par in range(2):
                    tl = [k.sb([128, 512], F32, es=s) for _ in range(2)]
                    for t_ in tl:
                        k.memset(t_, 0.0)
                    rd_sets.append(Rot(tl))
                pT_r = Rot([k.sb([128, 2, 512], BF16, es=s) for _ in range(4)])
                pss = Rot([k.ps([128, 2, 512], F32, es=s) for _ in range(2)])
                pacc = Rot([k.ps([128, 512], F32, es=s) for _ in range(4)])
                sw_r = Rot([k.sb([128, 512], F32, es=s) for _ in range(2)])
                yp_r = Rot([k.sb([128, 512], BF16, es=s) for _ in range(3)])
                qblocks = ([(0, 256, [0, 1])] if need_ctx else []) + [(256 + 512 * i, 512, list(range(NTILE))) for i in range(8)]
                LOOK = 1
                pend = []

                def emit_pv2(item):
                    h, r0, n, j, npair, kt2, pT, pv = item
                    for u_ in range(2):
                        first = (j == 0 and u_ == 0)
                        last = (j == npair - 1 and u_ == 1)
                        k.mm(pv[:, :n], vp2[:, kt2[u_], h, :], pT[:, u_, :n], start=first, stop=last)
                    if j == npair - 1:
                        off = (h % 2) * 64
                        dof = 64 - off
                        rd = rd_sets[h % 2].get()
                        k.op("dve", lambda e: e.reciprocal(rd.ap[dof:dof + 64, :n], pv.ap[dof:dof + 64, :n]), reads=[pv], writes=[rd])
                        sws = sw_r.get()
                        k.dma(sws[off:off + 64, :n], rd[dof:dof + 64, :n])
                        yp = yp_r.get()
                        k.tt(yp[off:off + 64, :n], pv[off:off + 64, :n], sws[off:off + 64, :n], ALU.mult)
                        k.dma(YT.sl(("mla", h, r0), np.s_[2 + h // 2, off:off + 64, r0:r0 + n]), yp[off:off + 64, :n], q="pool")
                for h in range(8):
                    off = (h % 2) * 64
                    kh = kh_r.get()
                    k.dma(kh[0:64, :], KN[h // 2, off:off + 64, :])
                    k.dma(kh[64:96, :], KRT)
                    for (r0, n, kts) in qblocks:
                        q = q_r.get()
                        k.dma(q[0:64, :n], QN[h // 2, off:off + 64, r0:r0 + n])
                        k.dma(q[64:96, :n], QR[h, :, r0:r0 + n])
                        pv = pacc.get()
                        npair = len(kts) // 2
                        for j in range(npair):
                            kt2 = (kts[2 * j], kts[2 * j + 1])
                            ps = pss.get()
                            for u_ in range(2):
                                k.mm(ps[:, u_, :n], kh[:, kt2[u_] * 128:(kt2[u_] + 1) * 128], q[:, :n])
                            pT = pT_r.get()
                            k.act(pT[:, :, :n], ps[:, :, :n], AF.Exp, scale=SCALE_MLA)
                            pend.append((h, r0, n, j, npair, kt2, pT, pv))
                            if len(pend) > LOOK:
                                emit_pv2(pend.pop(0))
                while pend:
                    emit_pv2(pend.pop(0))
            k.barrier()
            es_mla_pre.close()
            if stop_here("mla%d" % l):
                break
            with ExitStack() as s:
                wkT = k.sb([128, 2, 2, NT], BF16, es=s)
                k.memset(wkT, 0.0, e="pool")
                for hk_ in range(2):
                    for g_ in range(2):
                        k.dma(wkT[g_ * 64:(g_ + 1) * 64, hk_, g_, :], WKT[hk_, g_ * 64:(g_ + 1) * 64, :])
                wqT = k.sb([128, 2, NT], BF16, es=s)
                k.dma(wqT, TD(WQT, "c p t -> p c t"))
                wv3 = k.sb([128, NTILE, 4, 128], BF16, es=s)
                WVv = T(TMo.ap[:, 768:1024].rearrange("(t p) (q d) -> p t q d", p=128, d=64), TMo.key)
                for par in range(2):
                    k.memset(wv3[:, :, par::2, (1 - par) * 64:(2 - par) * 64], 1.0, e=("dve" if par == 0 else "pool"))
                    for q_ in range(par, 4, 2):
                        k.dma(wv3[:, :, q_, par * 64:(par + 1) * 64], WVv[:, :, q_, :])
                esink = k.sb([128, 4], F32, es=s)
                k.act(esink, small[:, sm0 + 11:sm0 + 15], AF.Exp)
                rd_sets = []
                for par in range(2):
                    tl = [k.sb([128, 128], F32, es=s) for _ in range(2)]
                    for t_ in tl:
                        k.memset(t_, 0.0)
                    rd_sets.append(Rot(tl))
                pT_r = Rot([k.sb([128, 5, 128], BF16, es=s) for _ in range(3)])
                pss = Rot([k.ps([128, 512], F32, es=s) for _ in range(4)])
                pacc = Rot([k.ps([128, 512], F32, es=s) for _ in range(4)])
                sw_r = Rot([k.sb([128, 128], F32, es=s) for _ in range(4)])
                yp_r = Rot([k.sb([128, 128], BF16, es=s) for _ in range(4)])
                pend = []

                def emit_pvw(item):
                    n, qh, keys, pT, pv, yp = item
                    hk, g = qh // 2, qh % 2
                    off = g * 64
                    dof = 64 - off
                    c0 = n * 128
                    nk = len(keys)
                    for i, (kt, msk) in enumerate(keys):
                        k.mm(pv[:, 0:128], wv3[:, kt, qh, :], pT[:, i, :], start=(i == 0), stop=(i == nk - 1))
                    rd = rd_sets[g].get()
                    k.ts(rd[dof:dof + 64, :], pv[dof:dof + 64, 0:128], esink[dof:dof + 64, qh:qh + 1], ALU.add)
                    k.op("dve", lambda e: e.reciprocal(rd.ap[dof:dof + 64, :], rd.ap[dof:dof + 64, :]), reads=[rd], writes=[rd])
                    sws = sw_r.get()
                    k.dma(sws[off:off + 64, :], rd[dof:dof + 64, :])
                    pendF.append((n, qh, pv, yp, sws))
                    if len(pendF) > 1:
                        emit_fin(pendF.pop(0))

                def emit_fin(item):
                    n, qh, pv, yp, sws = item
                    hk, g = qh // 2, qh % 2
                    off = g * 64
                    c0 = n * 128
                    k.tt(yp[off:off + 64, :], pv[off:off + 64, 0:128], sws[off:off + 64, :], ALU.mult)
                    if g == 1:
                        k.dma(YT.sl(("win", hk, n), np.s_[6 + hk, :, c0:c0 + 128]), yp, q="pool")
                pendF = []
                yp = None
                for n in range(0 if need_ctx else 2, NTILE):
                    c0 = n * 128
                    if n < 2:
                        keys = [(0, None), (1, None)]
                    else:
                        keys = []
                        if n - 1 >= 2:
                            keys.append((n - 1, mprev_b))
                        keys.append((n, None))
                        if n + 1 < NTILE:
                            keys.append((n + 1, mnext_b))
                        keys += [(0, None), (1, None)]
                    for qh in range(4):
                        hk, g = qh // 2, qh % 2
                        pv = pacc.get()
                        if g == 0:
                            yp = yp_r.get()
                        psA = pss.get()
                        psB = pss.get() if len(keys) > 4 else None
                        for i, (kt, msk) in enumerate(keys):
                            dst = psA[:, i * 128:(i + 1) * 128] if i < 4 else psB[:, 0:128]
                            k.mm(dst, wkT[:, hk, g, kt * 128:(kt + 1) * 128], wqT[:, hk, c0:c0 + 128])
                        pT = pT_r.get()
                        na = min(4, len(keys))
                        k.act(T(pT.ap[:, 0:na, :].rearrange("p a c -> p (a c)"), pT.key), psA[:, 0:na * 128], AF.Exp, scale=0.125)
                        if psB is not None:
                            k.act(pT[:, 4, :], psB[:, 0:128], AF.Exp, scale=0.125)
                        for i, (kt, msk) in enumerate(keys):
                            if msk is not None:
                                k.tt(pT[:, i, :], pT[:, i, :], msk, ALU.mult)
                        pend.append((n, qh, keys, pT, pv, yp))
                        if len(pend) > 1:
                            emit_pvw(pend.pop(0))
                while pend:
                    emit_pvw(pend.pop(0))
                while pendF:
                    emit_fin(pendF.pop(0))
            k.barrier()
            if stop_here("win%d" % l):
                break
            es_aff = ExitStack()
            aff_e = k.sb([16, NT], F32, "aff_e", es=es_aff)
            with ExitStack() as s:
                w_out = k.sb([128, 8, D], BF16, es=s)
                stg = Rot([k.sb([128, D], F32, es=s) for _ in range(2)])
                for kc in range(8):
                    load_cast(w_out[:, kc, :], w_out_d[l, kc * 128:(kc + 1) * 128, :], stg, e=("pool" if kc % 2 else "dve"))
                rw = k.sb([128, 8, 16], F32, es=s)
                k.dma(rw, TD(router_d[l], "(kc p) e -> p kc e", p=128))
                sts = [0, 1] if need_ctx else [1]
                mod2 = {st: load_mod(st, 2, s) for st in sts}
                gs2 = {st: load_gs(st, 4, l * 2 + 1, s) for st in sts}
                sh2 = {st: load_mod(st, 3, s) for st in sts}
                yT_r = Rot([k.sb([128, 8, 128], BF16, es=s) for _ in range(3)])
                xt_r = Rot([k.sb([128, D], F32, es=s) for _ in range(3)])
                xn_r = Rot([k.sb([128, D], F32, es=s) for _ in range(4)])
                xs_r = Rot([k.sb([128, D], F32, es=s) for _ in range(3)])
                h2b_r = Rot([k.sb([128, D], BF16, es=s) for _ in range(2)])
                h2T_r = Rot([k.sb([128, 8, 128], F32, es=s) for _ in range(3)])
                junk = k.sb([128, D], F32, es=s)
                ss_pool = Rot([k.sb([128, 1], F32, es=s) for _ in range(16)])
                lgall = k.sb([128, NTILE, 16], F32, es=s)
                pso = Rot([k.ps([128, 512], F32, es=s) for _ in range(3)])
                ptr = Rot([k.ps([128, 4, 128], F32, es=s) for _ in range(2)])
                psl = Rot([k.ps([128, 512], F32, es=s) for _ in range(2)])
                def oA(n):
                    st = 0 if n < 2 else 1
                    c0 = n * 128
                    yT = yT_r.get()
                    k.dma(yT, TD(YT, "c p t -> p c t")[:, :, c0:c0 + 128])
                    xt = xt_r.get()
                    k.dma(xt, rows_key(Xsrc, n)[c0:c0 + 128, :])
                    xn = xn_r.get()
                    for hf in range(2):
                        ps = pso.get()
                        for kc in range(8):
                            k.mm(ps, yT[:, kc, :], w_out[:, kc, hf * 512:(hf + 1) * 512], start=(kc == 0), stop=(kc == 7))
                        sl_ = np.s_[:, hf * 512:(hf + 1) * 512]
                        k.tt(xn[sl_], ps, mod2[st][sl_], ALU.mult)
                        k.tt(xn[sl_], xn[sl_], xt[sl_], ALU.add)
                    k.dma(rows_key(X, n)[c0:c0 + 128, :], xn, q="pool")
                    ss = ss_pool.get()
                    k.act(junk, xn, AF.Square, accum=ss)
                    return (xn, ss)

                def oB(n, c_):
                    xn, ss = c_
                    st = 0 if n < 2 else 1
                    c0 = n * 128
                    rstd = ss_pool.get()
                    k.rsq(rstd, ss, 1.0 / D, EPS)
                    xs = xs_r.get()
                    k.stt(xs, xn, rstd, gs2[st], ALU.mult, ALU.mult)
                    k.tt(xs, xs, sh2[st], ALU.add)
                    h2b = h2b_r.get()
                    k.copy(h2b, xs, e="act")
                    k.dma(H2.sl(n, np.s_[c0:c0 + 128, :]), h2b, q="pool")
                    h2T = h2T_r.get()
                    for hf in range(2):
                        pt = ptr.get()
                        for q4 in range(4):
                            kc = hf * 4 + q4
                            k.tr(pt[:, q4, :], xs[:, kc * 128:(kc + 1) * 128], ident)
                        k.copy(h2T[:, hf * 4:(hf + 1) * 4, :], pt, e="act")
                    return h2T

                def oC(n, h2T):
                    c0 = n * 128
                    pl = psl.get()
                    for kc in range(8):
                        k.mm(pl[:, 0:16], h2T[:, kc, :], rw[:, kc, :], start=(kc == 0), stop=(kc == 7))
                    k.copy(lgall.sl(n, np.s_[:, n, :]), pl[:, 0:16])
                tiles_o = list(range(0 if need_ctx else 2, NTILE))
                NO = len(tiles_o)
                ca = {}
                cb = {}
                for step in range(NO + 2):
                    if step < NO:
                        ca[step] = oA(tiles_o[step])
                    if 0 <= step - 1 < NO:
                        cb[step - 1] = oB(tiles_o[step - 1], ca.pop(step - 1))
                    if 0 <= step - 2 < NO:
                        oC(tiles_o[step - 2], cb.pop(step - 2))
                n0_ = tiles_o[0]
                lgv = T(lgall.ap[:, n0_:NTILE, :], lgall.key)
                exall = k.sb([128, NTILE, 16], F32, es=s)
                exv = T(exall.ap[:, n0_:NTILE, :], exall.key)
                k.act(exv, lgv, AF.Exp, reads_extra=[lgall.sub(n) for n in tiles_o]) if False else k.op(
                    "act", lambda e: e.activation(exv.ap, lgv.ap, AF.Exp), reads=[lgall.sub(n) for n in tiles_o], writes=[exall])
                small_ = k.sb([128, NTILE], F32, es=s)
                k.op("dve", lambda e: e.reduce_sum(small_.ap[:, n0_:NTILE], exv.ap, AX.X), reads=[exall], writes=[small_])
                k.op("dve", lambda e: e.reciprocal(small_.ap[:, n0_:NTILE], small_.ap[:, n0_:NTILE]), reads=[small_], writes=[small_])
                for n in tiles_o:
                    c0 = n * 128
                    k.ts(affT.sl(n, np.s_[:, n, :]), exall[:, n, :], small_[:, n:n + 1], ALU.mult)
                    pl2 = psl.get()
                    k.tr(pl2[0:16, 0:128], affT.sl(n, np.s_[:, n, :]), ident)
                    k.copy(aff_e.sl(n, np.s_[:, c0:c0 + 128]), pl2[0:16, 0:128], e="act")
            k.barrier()
            if stop_here("o%d" % l):
                es_aff.close()
                break
            streams = ([(0, 2, 32)] if need_ctx else []) + [(2, 32, 512)]
            with ExitStack() as s:
                work = k.sb([16, NTX], F32, es=s)
                m8 = k.sb([16, 8], F32, es=s)
                thr = k.sb([16, 1], F32, es=s)
                aff128 = k.sb([128, 512], F32, es=s)
                junk16 = k.sb([128, 512], BF16, es=s)
                lo = k.sb([128, 1], F32, es=s)
                hi = k.sb([128, 1], F32, es=s)
                mid = k.sb([128, 1], F32, es=s)
                cnt = k.sb([128, 1], F32, es=s)
                half = k.sb([128, 1], F32, es=s)
                k.memset(half, 0.5)
                mge = k.sb([128, 1], U32, es=s)
                mlt = k.sb([128, 1], U32, es=s)
                thr16 = k.sb([16, 8], F32, es=s)
                mask_e = k.sb([16, NTX], F32, es=s)
                maskTb = k.sb([128, 32, 16], BF16, es=s)
                carry = k.sb([128, 16], F32, es=s)
                pos_r = Rot([k.sb([128, 16], F32, es=s) for _ in range(2)])
                ptk = Rot([k.ps([128, 512], F32, es=s) for _ in range(4)])
                ahi = k.sb([128, NTILE, 16], BF16, es=s)
                alo = k.sb([128, NTILE, 16], F32, es=s)
                k.copy(ahi, affT)
                k.tt(alo, affT, ahi, ALU.subtract)
                k.copy(Rtab[:, :, :, 2], ahi, e="pool")
                k.copy(Rtab[:, :, :, 3], alo, e="pool")
                for (t0, ntl, cap) in streams:
                    ntok = ntl * 128
                    cs = np.s_[:, t0 * 128:t0 * 128 + ntok]
                    if cap <= 64:
                        k.copy(work[:, :ntok], aff_e[cs])
                        for r in range(cap // 8):
                            k.op("dve", lambda e: e.max(out=m8.ap, in_=work.ap[:, :ntok]), reads=[work], writes=[m8])
                            if r < cap // 8 - 1:
                                k.op("dve", lambda e: e.match_replace(out=work.ap[:, :ntok], in_to_replace=m8.ap,
                                                                      in_values=work.ap[:, :ntok], imm_value=-1.0),
                                     reads=[work, m8], writes=[work])
                        k.copy(thr, m8[:, 7:8])
                    else:
                        k.dma(AFFD, aff_e[cs])
                        k.dma(aff128, TD(AFFD, "e (s t) -> (e s) t", s=8))
                        k.memset(lo, 0.0)
                        k.memset(hi, 2.0)
                        for it in range(40):
                            k.stt(mid, lo, hi, half, ALU.add, ALU.mult)
                            k.ts(junk16, aff128, mid, ALU.is_ge, 0.0, ALU.add, accum=cnt)
                            pc = ptk.get()
                            k.mm(pc[:, 0:1], C("bmat"), cnt)
                            k.ts(mge, pc[:, 0:1], cap - 0.5, ALU.is_ge)
                            k.ts(mlt, pc[:, 0:1], cap - 0.5, ALU.is_lt)
                            k.op("dve", lambda e: e.copy_predicated(lo.ap, mge.ap, mid.ap), reads=[mge, mid], writes=[lo])
                            k.op("dve", lambda e: e.copy_predicated(hi.ap, mlt.ap, mid.ap), reads=[mlt, mid], writes=[hi])
                        k.dma(THRD, lo)
                        k.dma(thr16, TD(THRD, "(e s) o -> e (s o)", s=8))
                        k.copy(thr, thr16[:, 0:1])
                    k.ts(mask_e[:, :ntok], aff_e[cs], thr, ALU.is_ge)
                    k.memset(carry, 0.0)
                    for i in range(ntl):
                        n = t0 + i
                        pt = ptk.get()
                        k.tr(pt[:, 0:16], mask_e[:, i * 128:(i + 1) * 128], ident[0:16, 0:16])
                        k.copy(maskTb[:, i, :], pt[:, 0:16], e="act")
                        pc = ptk.get()
                        k.mm(pc[:, 0:16], triU_b, maskTb[:, i, :])
                        k.mm(pc[:, 16:32], ones_b, maskTb[:, i, :])
                        pos = pos_r.get()
                        k.tt(pos, pc[:, 0:16], carry, ALU.add)
                        k.tt(pos, pos, maskTb[:, i, :], ALU.mult)
                        k.ts(posm.sl(n, np.s_[:, n, :]), pos, -1.0, ALU.add)
                        k.tt(carry, carry, pc[:, 16:32], ALU.add)
            k.barrier()
            if stop_here("topk%d" % l):
                es_aff.close()
                break
            es_aff.close()
            with ExitStack() as s:
                mod5 = {st: load_mod(st, 5, s) for st in ([0, 1] if need_ctx else [1])}
                wsets = Rot([(k.sb([128, 8, 768], BF16, es=s), k.sb([128, 8, 768], BF16, es=s), k.sb([128, 6, D], BF16, es=s))
                             for _ in range(2)])
                stg = Rot([k.sb([128, 768], F32, es=s) for _ in range(4)])
                Sel = k.sb([128, 32, 512], BF16, es=s)
                iota16 = k.sb([128, 512], mybir.dt.int16, es=s)
                k.copy(iota16, C("iota"))
                r4T_r = Rot([k.sb([4, 512], F32, es=s) for _ in range(2)])
                r4_r = Rot([k.sb([128, 16], F32, es=s) for _ in range(3)])
                idx_r = Rot([k.sb([128, 1], I32, es=s) for _ in range(16)])
                idf_r = Rot([k.sb([128, 1], F32, es=s) for _ in range(4)])
                gt_r = Rot([k.sb([128, 1], F32, es=s) for _ in range(16)])
                xs_r = Rot([k.sb([128, D], BF16, es=s) for _ in range(10)])
                xsT_r = Rot([k.sb([128, 8, 512], BF16, es=s) for _ in range(2)])
                hid = k.sb([128, 6, 512], BF16, es=s)
                sg_r = Rot([k.sb([128, 512], F32, es=s) for _ in range(2)])
                ys_r = Rot([k.sb([128, D], F32, es=s) for _ in range(2)])
                p4 = Rot([k.ps([128, 512], F32, es=s) for _ in range(1)])
                ptb = Rot([k.ps([128, 8, 128], BF16, es=s) for _ in range(1)])
                pgu = Rot([k.ps([128, 512], F32, es=s) for _ in range(4)])
                pdn = Rot([k.ps([128, 512], F32, es=s) for _ in range(2)])
                Xall = [rows_key(X, n) for n in range(NTILE)] + [X.sub("all")]
                cast_eng = Rot(["act", "dve"])
                wcache = {}

                wgen = {"g": None}

                def weights_gen(e_, wg, wu, wd):
                    for kc in range(8):
                        load_cast(wg[:, kc, :], wg_d[l, e_, kc * 128:(kc + 1) * 128, :], stg, e=cast_eng.get())
                        yield
                        load_cast(wu[:, kc, :], wu_d[l, e_, kc * 128:(kc + 1) * 128, :], stg, e=cast_eng.get())
                        yield
                    for fc in range(6):
                        for hf_ in range(2):
                            load_cast(wd[:, fc, hf_ * 512:(hf_ + 1) * 512], wd_d[l, e_, fc * 128:(fc + 1) * 128, hf_ * 512:(hf_ + 1) * 512], stg, e=cast_eng.get())
                            yield

                def feed(n_):
                    for _ in range(n_):
                        if wgen["g"] is None:
                            return
                        try:
                            next(wgen["g"])
                        except StopIteration:
                            wgen["g"] = None

                def weights(e_, now=False):
                    feed(10000)
                    wg, wu, wd = wsets.get()
                    wcache[e_] = (wg, wu, wd)
                    wgen["g"] = weights_gen(e_, wg, wu, wd)
                    if now:
                        feed(10000)

                sgen = {"g": None}

                def sel_gen(u):
                    e_, (t0, ntl, cap) = u
                    for i in range(ntl):
                        k.ts(Sel[:, i, :cap], iota16[:, :cap], posm[:, t0 + i, e_:e_ + 1], ALU.is_equal)
                        yield

                def sfeed(n_):
                    for _ in range(n_):
                        if sgen["g"] is None:
                            return
                        try:
                            next(sgen["g"])
                        except StopIteration:
                            sgen["g"] = None

                def selbuild(u, now=True):
                    sfeed(10000)
                    sgen["g"] = sel_gen(u)
                    if now:
                        sfeed(10000)

                def idxpart(u):
                    e_, (t0, ntl, cap) = u
                    ps = p4.get()
                    for i in range(ntl):
                        k.mm(ps[0:4, :cap], Rtab[:, t0 + i, e_, :], Sel[:, i, :cap], start=(i == 0), stop=(i == ntl - 1))
                    r4T = r4T_r.get()
                    k.copy(r4T[:, :cap], ps[0:4, :cap])
                    stiles = [(s0, min(128, cap - s0)) for s0 in range(0, cap, 128)]
                    for si, (s0, nsl) in enumerate(stiles):
                        k.tr(ps[0:nsl, si * 4:(si + 1) * 4], r4T[0:4, s0:s0 + nsl], ident[0:4, 0:4])
                    nsl0 = stiles[0][1]
                    r4 = r4_r.get()
                    k.copy(r4[0:nsl0, 0:4 * len(stiles)], ps[0:nsl0, 0:4 * len(stiles)])
                    meta = []
                    xss = []
                    for si, (s0, nsl) in enumerate(stiles):
                        c4 = si * 4
                        idf = idf_r.get()
                        k.stt(idf[0:nsl, :], r4[0:nsl, c4:c4 + 1], 64.0, r4[0:nsl, c4 + 1:c4 + 2], ALU.mult, ALU.add)
                        idx = idx_r.get()
                        k.copy(idx[0:nsl, :], idf[0:nsl, :])
                        gt = gt_r.get()
                        k.tt(gt[0:nsl, :], r4[0:nsl, c4 + 2:c4 + 3], r4[0:nsl, c4 + 3:c4 + 4], ALU.add)
                        meta.append((idx, gt))
                        xs = xs_r.get()
                        k.dma(xs[0:nsl, :], H2, q="pool", reads=[idx],
                              fn=lambda en: en.indirect_dma_start(out=xs.ap[0:nsl, :], out_offset=None, in_=H2.ap,
                                                                  in_offset=bass.IndirectOffsetOnAxis(ap=idx.ap[0:nsl, :], axis=0)))
                        xss.append(xs)
                    return [u, stiles, meta, xss, None]

                def xtrans(item):
                    u, stiles, meta, xss, _ = item
                    xsT = xsT_r.get()
                    for si, (s0, nsl) in enumerate(stiles):
                        xs = xss[si]
                        pt = ptb.get()
                        for kc in range(8):
                            k.tr(pt[:, kc, 0:nsl], xs[0:nsl, kc * 128:(kc + 1) * 128], ident_b[0:nsl, 0:nsl])
                        k.copy(xsT[:, :, s0:s0 + nsl], pt[:, :, 0:nsl], e="act")
                    item[4] = xsT

                def ffn(item):
                    (e_, (t0, ntl, cap)), stiles, meta, xss, xsT = item
                    wg, wu, wd = wcache[e_]
                    for fc in range(6):
                        pg = pgu.get()
                        pu = pgu.get()
                        for kc in range(8):
                            k.mm(pg[:, :cap], wg[:, kc, fc * 128:(fc + 1) * 128], xsT[:, kc, :cap], start=(kc == 0), stop=(kc == 7))
                        for kc in range(8):
                            k.mm(pu[:, :cap], wu[:, kc, fc * 128:(fc + 1) * 128], xsT[:, kc, :cap], start=(kc == 0), stop=(kc == 7))
                        sg = sg_r.get()
                        k.act(sg[:, :cap], pg[:, :cap], AF.Silu)
                        k.tt(hid[:, fc, :cap], sg[:, :cap], pu[:, :cap], ALU.mult)
                        feed(2)
                        sfeed(6)

                def down(item):
                    (e_, (t0, ntl, cap)), stiles, meta, xss, xsT = item
                    st = 0 if t0 == 0 else 1
                    wg, wu, wd = wcache[e_]
                    for si, (s0, nsl) in enumerate(stiles):
                        idx, gt = meta[si]
                        ys = ys_r.get()
                        for hf in range(2):
                            pd = pdn.get()
                            for fc in range(6):
                                k.mm(pd[0:nsl, :], hid[:, fc, s0:s0 + nsl], wd[:, fc, hf * 512:(hf + 1) * 512], start=(fc == 0), stop=(fc == 5))
                            k.stt(ys[0:nsl, hf * 512:(hf + 1) * 512], pd[0:nsl, :], gt[0:nsl, 0:1],
                                  mod5[st][0:nsl, hf * 512:(hf + 1) * 512], ALU.mult, ALU.mult)
                            feed(2)
                        k.dma(X, ys[0:nsl, :], q="pool", reads=[ys, idx], writes=Xall,
                              fn=lambda en: en.indirect_dma_start(out=X.ap, out_offset=bass.IndirectOffsetOnAxis(ap=idx.ap[0:nsl, :], axis=0),
                                                                  in_=ys.ap[0:nsl, :], in_offset=None, compute_op=ALU.add))

                units = [(e_, stm) for e_ in range(16) for stm in reversed(streams)]
                NU = len(units)
                weights(0, now=True)
                weights(1, now=True)
                nxt_w = 2
                selbuild(units[0])
                items = {0: idxpart(units[0])}
                if NU > 1:
                    selbuild(units[1])
                    items[1] = idxpart(units[1])
                xtrans(items[0])
                if NU > 2:
                    selbuild(units[2])
                for ui in range(NU):
                    ffn(items[ui])
                    if ui + 1 < NU:
                        xtrans(items[ui + 1])
                    if ui + 2 < NU:
                        sfeed(10000)
                        items[ui + 2] = idxpart(units[ui + 2])
                    down(items[ui])
                    if ui + 3 < NU:
                        selbuild(units[ui + 3], now=False)
                    e_done = units[ui][0]
                    if (ui + 1 == NU or units[ui + 1][0] != e_done) and nxt_w < 16:
                        weights(nxt_w)
                        nxt_w += 1
                    del items[ui]
            k.barrier()
            if stop_here("exp%d" % l):
                break
        else:
            with ExitStack() as s:
                gfin = k.sb([128, D], F32, es=s)
                k.dma(gfin, g_bc_d[4])
                xt_r = Rot([k.sb([128, D], F32, es=s) for _ in range(5)])
                junk = k.sb([128, D], F32, es=s)
                ss_pool = Rot([k.sb([128, 1], F32, es=s) for _ in range(12)])
                def finA(n):
                    c0 = n * 128
                    xt = xt_r.get()
                    k.dma(xt, rows_key(X, n)[c0:c0 + 128, :])
                    ss = ss_pool.get()
                    k.act(junk, xt, AF.Square, accum=ss)
                    return (xt, ss)

                def finB(n, c_):
                    xt, ss = c_
                    c0 = n * 128
                    rstd = ss_pool.get()
                    k.rsq(rstd, ss, 1.0 / D, EPS)
                    k.stt(xt, xt, rstd, gfin, ALU.mult, ALU.mult)
                    k.dma(out_d.sl(n, np.s_[c0 - NTC:c0 - NTC + 128, :]), xt, q="pool")
                tl_ = list(range(2, NTILE))
                q_ = [finA(tl_[0]), finA(tl_[1])]
                for i_, n in enumerate(tl_):
                    if i_ + 2 < len(tl_):
                        q_.append(finA(tl_[i_ + 2]))
                    finB(n, q_.pop(0))
        k.barrier()
        print("ninst", k.ninst, k.cnt, flush=True)
    return nc


_NC_CACHE = {}


def kernel(**inputs):
    inp = {kk: np.asarray(v) for kk, v in inputs.items()}
    shared = _prep_shared(inp)
    if "nc" not in _NC_CACHE:
        _NC_CACHE["nc"] = build()
    nc = _NC_CACHE["nc"]
    in_maps = []
    for b in range(8):
        m = dict(shared)
        m.update(_prep_core(inp, b))
        in_maps.append(m)
    res = run_bass_kernel_spmd(nc, in_maps, core_ids=list(range(8)))
    out = np.stack([np.asarray(r["out"], dtype=np.float32) for r in res.results], 0)
    return out
```

```python
import numpy as np
from contextlib import ExitStack
import concourse.bass as bass
import concourse.mybir as mybir
from concourse.bass_utils import run_bass_kernel_spmd

F32 = mybir.dt.float32
BF16 = mybir.dt.bfloat16
I32 = mybir.dt.int32
U32 = mybir.dt.uint32
AF = mybir.ActivationFunctionType
ALU = mybir.AluOpType
AX = mybir.AxisListType


class T:
    __slots__ = ("ap", "key")

    def __init__(self, ap, key):
        self.ap = ap
        self.key = key

    def __getitem__(self, idx):
        return T(self.ap[idx], self.key)

    def sub(self, suffix):
        return T(self.ap, (self.key, suffix))

    def sl(self, suffix, idx):
        return T(self.ap[idx], (self.key, suffix))


def _key(x):
    return x.key if isinstance(x, T) else x


class KB:
    CE = ("pe", "act", "dve", "pool")

    def __init__(self, nc, es, nds=14):
        self.nc = nc
        self.es = es
        self.E = {"pe": nc.tensor, "act": nc.scalar, "dve": nc.vector,
                  "pool": nc.gpsimd, "sp": nc.sync}
        self.csem = {e: es.enter_context(nc.semaphore("c_" + e)) for e in self.CE}
        self.cnt = {e: 0 for e in self.CE}
        self.NDS = nds
        self.dsem = [es.enter_context(nc.semaphore("d%d" % i)) for i in range(nds)]
        self.dcnt = [0] * nds
        self.dnext = 0
        self.waited = {e: {} for e in self.E}
        self.lastw = {}
        self.readers = {}
        self.nalloc = 0
        self.ninst = 0

    def sb(self, shape, dtype, name=None, es=None):
        self.nalloc += 1
        name = name or ("t%d" % self.nalloc)
        t = (es or self.es).enter_context(self.nc.sbuf_tensor(name + "_%d" % self.nalloc, list(shape), dtype))
        return T(t[:], name + "_%d" % self.nalloc)

    def ps(self, shape, dtype, name=None, es=None):
        self.nalloc += 1
        name = name or ("p%d" % self.nalloc)
        t = (es or self.es).enter_context(self.nc.psum_tensor(name + "_%d" % self.nalloc, list(shape), dtype))
        return T(t[:], name + "_%d" % self.nalloc)

    def dram(self, name, shape, dtype, kind="Internal"):
        t = self.nc.dram_tensor(name, list(shape), dtype, kind=kind)
        return T(t.ap(), name)

    def _semobj(self, semkey):
        return self.csem[semkey[1]] if semkey[0] == "c" else self.dsem[semkey[1]]

    def _wait(self, e, semkey, val):
        if self.waited[e].get(semkey, 0) >= val:
            return
        self.waited[e][semkey] = val
        self.E[e].wait_ge(self._semobj(semkey), val)

    def _deps(self, e, reads, writes, is_dma):
        for r in reads:
            lw = self.lastw.get(_key(r))
            if lw is not None:
                self._wait(e, lw[0], lw[1])
        for w in writes:
            k = _key(w)
            lw = self.lastw.get(k)
            if lw is not None:
                if not (lw[0] == ("c", e) and e == "pe" and not is_dma):
                    self._wait(e, lw[0], lw[1])
            for sk, v in self.readers.get(k, {}).items():
                if sk == ("c", e) and e == "pe" and not is_dma:
                    continue
                self._wait(e, sk, v)

    def _record(self, tok, reads, writes):
        for r in reads:
            d = self.readers.setdefault(_key(r), {})
            if d.get(tok[0], 0) < tok[1]:
                d[tok[0]] = tok[1]
        for w in writes:
            k = _key(w)
            self.lastw[k] = tok
            self.readers[k] = {}

    def op(self, e, fn, reads=(), writes=()):
        self._deps(e, reads, writes, False)
        ins = fn(self.E[e])
        self.cnt[e] += 1
        ins.then_inc(self.csem[e], 1)
        self._record((("c", e), self.cnt[e]), reads, writes)
        self.ninst += 1
        return ins

    def dma(self, out, in_, q="sp", fn=None, reads=None, writes=None, **kw):
        reads = [in_] if reads is None else reads
        writes = [out] if writes is None else writes
        slot = self.dnext
        self.dnext = (slot + 1) % self.NDS
        if self.dcnt[slot] > 0:
            self._wait(q, ("d", slot), 16 * self.dcnt[slot])
        self._deps(q, reads, writes, True)
        if fn is None:
            ins = self.E[q].dma_start(out=out.ap, in_=in_.ap, **kw)
        else:
            ins = fn(self.E[q])
        self.dcnt[slot] += 1
        ins.then_inc(self.dsem[slot], 16)
        self._record((("d", slot), 16 * self.dcnt[slot]), reads, writes)
        self.ninst += 1
        return ins

    def barrier(self):
        for e in self.E:
            for e2 in self.CE:
                if e2 != e and self.cnt[e2] > 0:
                    self._wait(e, ("c", e2), self.cnt[e2])
            for s in range(self.NDS):
                if self.dcnt[s] > 0:
                    self._wait(e, ("d", s), 16 * self.dcnt[s])

    def mm(self, out, lhsT, rhs, start=True, stop=True, extra_reads=()):
        return self.op("pe", lambda e: e.matmul(out.ap, lhsT.ap, rhs.ap, start=start, stop=stop),
                       reads=[lhsT, rhs, *extra_reads], writes=[out])

    def tr(self, out, in_, ident):
        return self.op("pe", lambda e: e.transpose(out.ap, in_.ap, ident.ap),
                       reads=[in_, ident], writes=[out])

    def act(self, out, in_, func, bias=None, scale=None, accum=None, e="act"):
        kw = {}
        rd = [in_]
        wr = [out]
        if bias is not None:
            if isinstance(bias, T):
                kw["bias"] = bias.ap
                rd.append(bias)
            else:
                kw["bias"] = bias
        if scale is not None:
            if isinstance(scale, T):
                kw["scale"] = scale.ap
                rd.append(scale)
            else:
                kw["scale"] = scale
        if accum is not None:
            kw["accum_out"] = accum.ap
            wr.append(accum)
        return self.op(e, lambda en: en.activation(out.ap, in_.ap, func, **kw), reads=rd, writes=wr)

    def tt(self, out, a, b, op, e="dve"):
        return self.op(e, lambda en: en.tensor_tensor(out.ap, a.ap, b.ap, op), reads=[a, b], writes=[out])

    def ts(self, out, a, s1, op0, s2=None, op1=None, e="dve", accum=None):
        rd = [a]
        wr = [out]
        v1 = s1
        v2 = s2
        if isinstance(s1, T):
            rd.append(s1)
            v1 = s1.ap
        if isinstance(s2, T):
            rd.append(s2)
            v2 = s2.ap
        kw = {}
        if op1 is not None:
            kw["op1"] = op1
        if accum is not None:
            kw["accum_out"] = accum.ap
            wr.append(accum)
        return self.op(e, lambda en: en.tensor_scalar(out.ap, a.ap, v1, v2, op0, **kw), reads=rd, writes=wr)

    def stt(self, out, a, s, b, op0, op1, e="dve"):
        rd = [a, b]
        v = s
        if isinstance(s, T):
            rd.append(s)
            v = s.ap
        return self.op(e, lambda en: en.scalar_tensor_tensor(out.ap, a.ap, v, b.ap, op0, op1), reads=rd, writes=[out])

    def copy(self, out, in_, e="dve"):
        if e == "act":
            return self.op(e, lambda en: en.activation(out.ap, in_.ap, AF.Copy), reads=[in_], writes=[out])
        return self.op(e, lambda en: en.tensor_copy(out.ap, in_.ap), reads=[in_], writes=[out])

    def memset(self, out, v, e="dve"):
        return self.op(e, lambda en: en.memset(out.ap, v), reads=[], writes=[out])

    def rsq(self, dst, src, mul, add):
        self.ts(dst, src, mul, ALU.mult, add, ALU.add)
        self.act(dst, dst, AF.Sqrt)
        self.op("dve", lambda e: e.reciprocal(dst.ap, dst.ap), reads=[dst], writes=[dst])

L_ = 2
D = 1024
NTX = 4096
NTC = 256
NT = NTX + NTC
NTILE = NT // 128
NCOL = 2464
SCALE_MLA = float((64 + 32) ** -0.5)
LN2 = float(np.log(2.0))


def _rope_tables():
    t = np.arange(NTX)
    row = (t // 64).astype(np.float32)
    col = (t % 64).astype(np.float32)

    def tab(dh_half):
        dh = dh_half
        inv = 10000.0 ** (-np.arange(0, dh, 2, dtype=np.float32) / dh)
        return inv

    def build(dtot):
        half = dtot // 2
        inv = tab(half)
        nf = half // 2
        cos = np.zeros((dtot, NTX), np.float32)
        sins = np.zeros((dtot, NTX), np.float32)
        perm = np.zeros((dtot, dtot), np.float32)
        for part, pos in ((0, row), (1, col)):
            base = part * half
            ang = pos[None, :] * inv[:, None]
            c, s = np.cos(ang), np.sin(ang)
            for j in range(nf):
                cos[base + j] = c[j]
                cos[base + nf + j] = c[j]
                sins[base + j] = -s[j]
                sins[base + nf + j] = s[j]
                perm[base + nf + j, base + j] = 1.0
                perm[base + j, base + nf + j] = 1.0
        return cos, sins, perm

    c32, s32, p32 = build(32)
    c64, s64, p64 = build(64)
    c128 = np.concatenate([c64, c64], 0)
    s128 = np.concatenate([s64, s64], 0)
    p128 = np.zeros((128, 128), np.float32)
    p128[:64, :64] = p64
    p128[64:, 64:] = p64
    rope32 = np.stack([np.tile(c32, (4, 1)), np.tile(s32, (4, 1))]).astype(np.float32)
    rope128 = np.stack([c128, s128]).astype(np.float32)
    return rope32, rope128, p32, p128


_CST = {}


def _cst_layout():
    off = 0
    for name, w in (("ident", 128), ("A", 128), ("B", 128), ("C1", 128), ("C2", 128),
                    ("colK1", 1), ("colK2", 1), ("iota", 512), ("triU", 128),
                    ("mprev", 128), ("mnext", 128), ("p128", 128), ("p32", 32), ("pswap", 128), ("bmat", 128), ("p32x4", 128),
                    ("tokid", NTILE * 16 * 2)):
        _CST[name] = (off, w)
        off += w
    return off


NCST = _cst_layout()


def _const_table():
    rope32, rope128, p32, p128 = _rope_tables()
    cst = np.zeros((128, NCST), np.float32)
    p = np.arange(128, dtype=np.float32)[:, None]
    c = np.arange(128, dtype=np.float32)[None, :]

    def put(name, arr):
        o, w = _CST[name]
        cst[:arr.shape[0], o:o + w] = arr
    put("ident", np.eye(128, dtype=np.float32))
    put("A", np.maximum(c - p, 0.0))
    put("B", np.maximum(p - c, 0.0))
    put("C1", np.broadcast_to(c + 1.0, (128, 128)))
    put("C2", np.broadcast_to(128.0 - c, (128, 128)))
    put("colK1", 127.0 - p)
    put("colK2", p)
    put("iota", np.broadcast_to(np.arange(512, dtype=np.float32)[None, :], (128, 512)))
    put("triU", (p <= c).astype(np.float32))
    put("mprev", (p >= c).astype(np.float32))
    put("mnext", (p <= c).astype(np.float32))
    put("p128", p128)
    put("p32", p32)
    p32x4 = np.zeros((128, 128), np.float32)
    for i_ in range(4):
        p32x4[i_ * 32:(i_ + 1) * 32, i_ * 32:(i_ + 1) * 32] = p32
    put("p32x4", p32x4)
    put("pswap", (np.arange(128)[:, None] == (np.arange(128)[None, :] + 64) % 128).astype(np.float32))
    put("bmat", (np.arange(128)[:, None] // 8 == np.arange(128)[None, :] // 8).astype(np.float32))
    rows = (np.arange(NTILE)[None, :] * 128 + np.arange(128)[:, None])
    tok = np.stack([rows // 64, rows % 64], -1).astype(np.float32)
    tok = np.broadcast_to(tok[:, :, None, :], (128, NTILE, 16, 2)).reshape(128, -1)
    put("tokid", tok)
    return cst, rope32, rope128


def _prep_shared(inp):
    f = lambda a: np.ascontiguousarray(a, dtype=np.float32)
    sh = {}
    w_in = inp["w_in"]
    s = np.cumsum([0, 256, 256, 256, 256, 256, 128, 32, 256, 128, 128])
    rq, rk, rv, rg, cq, ckv, kr, wq, wk, wv = [w_in[:, :, s[i]:s[i + 1]] for i in range(10)]
    wk2 = np.concatenate([wk[:, :, 0:64], wk[:, :, 0:64], wk[:, :, 64:128], wk[:, :, 64:128]], -1)
    wv2 = np.concatenate([wv[:, :, 0:64], wv[:, :, 0:64], wv[:, :, 64:128], wv[:, :, 64:128]], -1)
    sh["w_in_r"] = f(np.concatenate([rq, rk, cq, ckv, wq, wk2, kr, rk, rv, rg, wv2], -1))
    assert sh["w_in_r"].shape[-1] == NCOL
    uq = inp["mla_w_uq"]
    sh["w_uq_n"] = f(uq[:, :, :, :64].reshape(L_, 256, 512))
    sh["w_uq_r"] = f(uq[:, :, :, 64:].reshape(L_, 256, 256))
    sh["w_uk"] = f(inp["mla_w_uk"].reshape(L_, 128, 512))
    sh["w_uv"] = f(inp["mla_w_uv"].reshape(L_, 128, 512))
    sh["w_out"] = f(inp["w_out"])
    sh["router_w"] = f(inp["router_w"])
    sh["ada_w"] = f(inp["ada_w"])
    sh["ada_b"] = f(inp["ada_b"].reshape(L_, 1, 6 * D))
    sh["exp_wg"] = f(inp["exp_w_gate"])
    sh["exp_wu"] = f(inp["exp_w_up"])
    sh["exp_wd"] = f(inp["exp_w_down"])
    gb = np.stack([inp["norm1_g"][0], inp["norm2_g"][0], inp["norm1_g"][1], inp["norm2_g"][1], inp["final_g"]])
    sh["g_bc"] = f(np.broadcast_to(gb[:, None, :], (5, 128, D)))
    cols = []
    for l in range(L_):
        qg = inp["mla_qnorm_g"][l].reshape(2, 128).T
        kg = inp["mla_kvnorm_g"][l].reshape(1, 128).T
        df, db, sk = inp["ret_decay_f"][l], inp["ret_decay_b"][l], inp["win_sink"][l]
        rep = np.broadcast_to(np.concatenate([df, db, sk])[None, :], (128, 12))
        hp = (np.arange(128) >= 64).astype(np.int64)
        pp = np.stack([df[0 + hp], df[2 + hp], db[0 + hp], db[2 + hp]], -1)
        cols += [qg, kg, rep, pp]
    sh["small"] = f(np.concatenate(cols, -1))
    cst, rope32, rope128 = _const_table()
    sh["cst"] = cst
    sh["rope32"] = rope32
    sh["rope128"] = rope128
    return sh


def _prep_core(inp, b):
    f = lambda a: np.ascontiguousarray(a, dtype=np.float32)
    d = {}
    d["x0"] = f(np.concatenate([inp["ctx"][b], inp["x"][b]], 0))
    cv = np.stack([inp["c_ctx"], inp["c"][b]])
    cr = cv.reshape(2, 8, 128).transpose(0, 2, 1)
    d["crep"] = f(np.broadcast_to(cr[:, :, :, None], (2, 128, 8, 128)))
    return d

class Rot:
    def __init__(self, tiles):
        self.t = tiles
        self.i = 0

    def get(self):
        t = self.t[self.i % len(self.t)]
        self.i += 1
        return t


def TD(t, pattern, **kw):
    return T(t.ap.rearrange(pattern, **kw), t.key)


def build(upto=None, dbg=False, nlayers=L_):
    nc = bass.Bass("TRN2", target_bir_lowering=False)
    es0 = ExitStack()
    with es0:
        k = KB(nc, es0)
        kind_dbg = "ExternalOutput" if dbg else "Internal"
        din = lambda n, s, dt=F32: k.dram(n, s, dt, kind="ExternalInput")
        x0 = din("x0", [NT, D])
        crep_d = din("crep", [2, 128, 8, 128])
        w_in_d = din("w_in_r", [L_, D, NCOL])
        w_uq_n_d = din("w_uq_n", [L_, 256, 512])
        w_uq_r_d = din("w_uq_r", [L_, 256, 256])
        w_uk_d = din("w_uk", [L_, 128, 512])
        w_uv_d = din("w_uv", [L_, 128, 512])
        w_out_d = din("w_out", [L_, D, D])
        router_d = din("router_w", [L_, D, 16])
        ada_w_d = din("ada_w", [L_, D, 6 * D])
        ada_b_d = din("ada_b", [L_, 1, 6 * D])
        wg_d = din("exp_wg", [L_, 16, D, 768])
        wu_d = din("exp_wu", [L_, 16, D, 768])
        wd_d = din("exp_wd", [L_, 16, 768, D])
        g_bc_d = din("g_bc", [5, 128, D])
        small_d = din("small", [128, L_ * 19])
        cst_d = din("cst", [128, NCST])
        rope32_d = din("rope32", [2, 128, NTX])
        rope128_d = din("rope128", [2, 128, NTX])
        out_d = k.dram("out", [NTX, D], F32, kind="ExternalOutput")
        X = k.dram("X", [NT, D], F32, kind=kind_dbg)
        MODBC = k.dram("MODBC", [2, 6, 128, D], F32, kind=kind_dbg)
        QT = k.dram("QT", [2, 128, NT], BF16, kind=kind_dbg)
        KT = k.dram("KT", [2, 128, NT], BF16, kind=kind_dbg)
        TMo = k.dram("TMo", [NT, 1024], BF16, kind=kind_dbg)
        QN = k.dram("QN", [4, 128, NT], BF16, kind=kind_dbg)
        KN = k.dram("KN", [4, 128, NT], BF16, kind=kind_dbg)
        QR = k.dram("QR", [8, 32, NT], BF16, kind=kind_dbg)
        KVT = k.dram("KVT", [128, NT], BF16, kind=kind_dbg)
        KRT = k.dram("KRT", [32, NT], BF16, kind=kind_dbg)
        VP = k.dram("VP", [NT, 512], BF16, kind=kind_dbg)
        WQT = k.dram("WQT", [2, 128, NT], BF16, kind=kind_dbg)
        WKT = k.dram("WKT", [2, 128, NT], BF16, kind=kind_dbg)
        YT = k.dram("YT", [8, 128, NT], BF16, kind=kind_dbg)
        H2 = k.dram("H2", [NT, D], BF16, kind=kind_dbg)
        AFFD = k.dram("AFFD", [16, NTX], F32)
        THRD = k.dram("THRD", [128, 1], F32)

        cst = k.sb([128, NCST], F32, "cst")
        k.dma(cst, cst_d)
        small = k.sb([128, L_ * 19], F32, "small")
        k.dma(small, small_d)

        def C(name, rows=128):
            o, w = _CST[name]
            return cst[0:rows, o:o + w]
        ident = C("ident")
        ones_f = k.sb([128, 128], F32, "ones_f")
        k.memset(ones_f, 1.0)
        ones_b = k.sb([128, 128], BF16, "ones_b")
        k.memset(ones_b, 1.0)
        ident_b = k.sb([128, 128], BF16, "ident_b")
        k.copy(ident_b, ident)
        triU_b = k.sb([128, 128], BF16, "triU_b")
        k.copy(triU_b, C("triU"))
        mprev_b = k.sb([128, 128], BF16, "mprev_b")
        k.copy(mprev_b, C("mprev"))
        mnext_b = k.sb([128, 128], BF16, "mnext_b")
        k.copy(mnext_b, C("mnext"))
        Rtab = k.sb([128, NTILE, 16, 4], BF16, "Rtab")
        o_tok, w_tok = _CST["tokid"]
        k.copy(Rtab[:, :, :, 0:2], T(cst.ap[:, o_tok:o_tok + w_tok].rearrange("p (n e c) -> p n e c", n=NTILE, e=16), cst.key))
        affT = k.sb([128, NTILE, 16], F32, "affT")
        posm = k.sb([128, NTILE, 16], F32, "posm")

        EPS = 1e-6
        blocks = [(0, 256, 0)] + [(256 + 512 * i, 512, 1) for i in range(8)]

        def rows_key(t, n):
            return t.sub(("r", n))

        def stop_here(name):
            return upto is not None and upto == name

        def rstd_from_ss(ss, n_feat, es, eps=EPS):
            r = k.sb([128, 1], F32, es=es)
            k.rsq(r, ss, 1.0 / n_feat, eps)
            return r

        for l in range(nlayers):
            need_ctx = l < L_ - 1
            sm0 = l * 19
            Xsrc = x0 if l == 0 else X
            with ExitStack() as s:
                crep = k.sb([128, 2, 8, 128], F32, es=s)
                for st in range(2):
                    k.dma(crep[:, st], crep_d[st])
                sil = k.sb([128, 2, 8, 128], F32, es=s)
                k.act(sil, crep, AF.Silu)
                wb = Rot([k.sb([128, 8, 512], F32, es=s) for _ in range(3)])
                br = Rot([k.sb([1, 512], F32, es=s) for _ in range(2)])
                pp = Rot([k.ps([128, 512], F32, es=s) for _ in range(4)])
                ob = Rot([k.sb([128, 512], F32, es=s) for _ in range(4)])
                aw = TD(ada_w_d[l], "(kc p) n -> p kc n", p=128)
                for nb in range(12):
                    w = wb.get()
                    for q4 in range(4):
                        k.dma(w.sl(q4, np.s_[:, q4 * 2:(q4 + 1) * 2, :]), aw[:, q4 * 2:(q4 + 1) * 2, nb * 512:(nb + 1) * 512])
                    b_ = br.get()
                    k.dma(b_, ada_b_d[l, :, nb * 512:(nb + 1) * 512])
                    for st in range(2):
                        ps = pp.get()
                        for kc in range(8):
                            k.mm(ps, sil[:, st, kc, :], w.sl(kc // 2, np.s_[:, kc, :]), start=(kc == 0), stop=False)
                        k.mm(ps, ones_f[0:1, :], b_, start=False, stop=True)
                        o = ob.get()
                        k.copy(o, ps, e=("act" if st == 0 else "dve"))
                        j, half = nb // 2, nb % 2
                        k.dma(MODBC.sl((st, j), np.s_[st, j, :, half * 512:(half + 1) * 512]), o)
            k.barrier()
            if stop_here("mod%d" % l):
                break

            def load_mod(st, j, es, eng_q="sp"):
                t = k.sb([128, D], F32, es=es)
                k.dma(t, MODBC.sl((st, j), np.s_[st, j]))
                return t

            def load_gs(st, j_scale, gidx, es):
                sc = load_mod(st, j_scale, es)
                gb = k.sb([128, D], F32, es=es)
                k.dma(gb, g_bc_d[gidx])
                k.stt(sc, sc, 1.0, gb, ALU.add, ALU.mult)
                return sc

            def load_cast(dst, src, stg, e="pool"):
                t = stg.get()
                sh = list(src.ap.shape)
                tv = t[0:sh[0], 0:sh[1]]
                k.dma(tv, src)
                k.copy(dst, tv, e=e)

            with ExitStack() as s:
                w_in = k.sb([128, 8, NCOL], BF16, es=s)
                stg = Rot([k.sb([128, NCOL], F32, es=s) for _ in range(2)])
                for kc in range(8):
                    load_cast(w_in[:, kc, :], w_in_d[l, kc * 128:(kc + 1) * 128, :], stg, e=("pool" if kc % 2 else "dve"))
                w_uq_n = k.sb([128, 2, 512], BF16, es=s)
                w_uq_r = k.sb([128, 2, 256], BF16, es=s)
                for kc in range(2):
                    load_cast(w_uq_n[:, kc, :], w_uq_n_d[l, kc * 128:(kc + 1) * 128, :], stg)
                    load_cast(w_uq_r[:, kc, :], w_uq_r_d[l, kc * 128:(kc + 1) * 128, :], stg)
                w_uk = k.sb([128, 512], BF16, es=s)
                load_cast(w_uk, w_uk_d[l], stg)
                w_uv = k.sb([128, 512], BF16, es=s)
                load_cast(w_uv, w_uv_d[l], stg)
                gs1 = [load_gs(st, 1, l * 2 + 0, s) for st in range(2)]
                sh1 = [load_mod(st, 0, s) for st in range(2)]
                qg = small[:, sm0 + 0:sm0 + 2]
                kg = small[:, sm0 + 2:sm0 + 3]
                xt_r = Rot([k.sb([128, D], F32, es=s) for _ in range(3)])
                xs_r = Rot([k.sb([128, D], F32, es=s) for _ in range(3)])
                junk = k.sb([128, D], F32, es=s)
                ss_pool = Rot([k.sb([128, 1], F32, es=s) for _ in range(8)])
                hT_r = Rot([k.sb([128, 8, 512], BF16, es=s) for _ in range(2)])
                ptr = Rot([k.ps([128, 8, 128], BF16, es=s) for _ in range(2)])
                xb_r = Rot([k.sb([128, D], BF16, es=s) for _ in range(5)])
                pfm = Rot([k.ps([128, 512], F32, es=s) for _ in range(3)])
                pex = Rot([k.ps([128, 512], F32, es=s) for _ in range(3)])
                ob16 = Rot([k.sb([128, 512], BF16, es=s) for _ in range(6)])
                f32t = Rot([k.sb([128, 512], F32, es=s) for _ in range(6)])
                rp32 = Rot([k.sb([128, 2, 512], F32, es=s) for _ in range(2)])
                rp128 = Rot([k.sb([128, 2, 512], F32, es=s) for _ in range(2)])
                cqg = k.sb([128, 2, 512], BF16, es=s)
                rstdq = k.sb([128, 512], F32, es=s)
                kvT_sb = k.sb([128, 512], BF16, es=s)
                tmo = Rot([k.sb([128, 1024], BF16, es=s) for _ in range(2)])

                def bc_rstd(dst, sq_list, nfeat, n):
                    ps = pex.get()
                    for i, sq in enumerate(sq_list):
                        k.mm(ps[:, :n], ones_f, sq, start=(i == 0), stop=(i == len(sq_list) - 1))
                    k.rsq(dst[:, :n], ps[:, :n], 1.0 / nfeat, EPS)

                def rope_apply(src_f32, M, n, tabs, perm, out_bf):
                    ps = pex.get()
                    k.mm(ps[0:M, :n], perm, src_f32[0:M, :n])
                    t1 = f32t.get()
                    k.tt(t1[0:M, :n], src_f32[0:M, :n], tabs[0:M, 0, :n], ALU.mult)
                    t2 = f32t.get()
                    k.tt(t2[0:M, :n], ps[0:M, :n], tabs[0:M, 1, :n], ALU.mult)
                    k.tt(out_bf[0:M, :n], t1[0:M, :n], t2[0:M, :n], ALU.add, e="pool")

                class NormState:
                    pass

                def norm_begin(blk):
                    (r0, n, st) = blk
                    ns = NormState()
                    ns.blk = blk
                    ns.hT = hT_r.get()
                    ns.r32 = ns.r128 = None
                    ns.xbs = []
                    if st == 1:
                        t0 = r0 - NTC
                        ns.r32 = rp32.get()
                        k.dma(ns.r32[:, :, :n], TD(rope32_d, "a p t -> p a t")[:, :, t0:t0 + n])
                        ns.r128 = rp128.get()
                        k.dma(ns.r128[:, :, :n], TD(rope128_d, "a p t -> p a t")[:, :, t0:t0 + n])
                    return ns

                def norm_ew(ns, ti):
                    (r0, n, st) = ns.blk
                    if ti >= n // 128:
                        return
                    rr = r0 + ti * 128
                    xt = xt_r.get()
                    k.dma(xt, rows_key(Xsrc, rr // 128)[rr:rr + 128, :])
                    ss = ss_pool.get()
                    k.act(junk, xt, AF.Square, accum=ss)
                    rstd = ss_pool.get()
                    k.rsq(rstd, ss, 1.0 / D, EPS)
                    xs = xs_r.get()
                    k.stt(xs, xt, rstd, gs1[st], ALU.mult, ALU.mult)
                    xb = xb_r.get()
                    k.tt(xb, xs, sh1[st], ALU.add)
                    ns.xbs.append(xb)

                def norm_tr(ns):
                    (r0, n, st) = ns.blk
                    for ti, xb in enumerate(ns.xbs):
                        pt = ptr.get()
                        for kc in range(8):
                            k.tr(pt[:, kc, :], xb[:, kc * 128:(kc + 1) * 128], ident_b)
                        k.copy(ns.hT[:, :, ti * 128:(ti + 1) * 128], pt, e="act")
                    return (ns.hT, ns.r32, ns.r128)

                def proj_phase(blk, ctx_, hook):
                    (r0, n, st) = blk
                    nt = n // 128
                    hT, r32, r128 = ctx_
                    def fm(c, M=128):
                        ps = pfm.get()
                        for kc in range(8):
                            k.mm(ps[0:M, :n], w_in[:, kc, c * 128:c * 128 + M], hT[:, kc, :n], start=(kc == 0), stop=(kc == 7))
                        return ps
                    for c in range(4):
                        ps = fm(c)
                        o = ob16.get()
                        k.copy(o[:, :n], ps[:, :n], e=("act" if c % 2 == 0 else "dve"))
                        dst = (QT if c < 2 else KT)
                        k.dma(dst.sl((c % 2, r0), np.s_[c % 2, :, r0:r0 + n]), o[:, :n])
                    hook(0)
                    sqs = []
                    for c2 in range(2):
                        ps = fm(4 + c2)
                        sq = f32t.get()
                        k.act(sq[:, :n], ps[:, :n], AF.Square)
                        sqs.append(sq[:, :n])
                        k.act(cqg[:, c2, :n], ps[:, :n], AF.Copy, scale=qg[:, c2:c2 + 1])
                    bc_rstd(rstdq, sqs, 256, n)
                    for pr in range(4):
                        ps = pex.get()
                        for c2 in range(2):
                            k.mm(ps[:, :n], w_uq_n[:, c2, pr * 128:(pr + 1) * 128], cqg[:, c2, :n], start=(c2 == 0), stop=(c2 == 1))
                        qn = ob16.get()
                        k.tt(qn[:, :n], ps[:, :n], rstdq[:, :n], ALU.mult)
                        k.dma(QN.sl((pr, r0), np.s_[pr, :, r0:r0 + n]), qn[:, :n])
                    for g4 in range(2):
                        ps = pex.get()
                        for c2 in range(2):
                            k.mm(ps[:, :n], w_uq_r[:, c2, g4 * 128:(g4 + 1) * 128], cqg[:, c2, :n], start=(c2 == 0), stop=(c2 == 1))
                        qr = f32t.get()
                        k.tt(qr[:, :n], ps[:, :n], rstdq[:, :n], ALU.mult)
                        o = ob16.get()
                        if st == 1:
                            rope_apply(qr, 128, n, r32, C("p32x4"), o)
                        else:
                            k.copy(o[:, :n], qr[:, :n], e="pool")
                        for hh in range(4):
                            h = g4 * 4 + hh
                            k.dma(QR.sl((h, r0), np.s_[h, :, r0:r0 + n]), o[hh * 32:(hh + 1) * 32, :n])
                    hook(1)
                    ps = fm(6)
                    sq = f32t.get()
                    k.act(sq[:, :n], ps[:, :n], AF.Square)
                    kvg = f32t.get()
                    k.act(kvg[:, :n], ps[:, :n], AF.Copy, scale=kg[:, 0:1])
                    rk_ = f32t.get()
                    bc_rstd(rk_, [sq[:, :n]], 128, n)
                    k.tt(kvT_sb[:, :n], kvg[:, :n], rk_[:, :n], ALU.mult)
                    k.dma(KVT.sl(r0, np.s_[:, r0:r0 + n]), kvT_sb[:, :n])
                    for pr in range(4):
                        ps = pex.get()
                        k.mm(ps[:, :n], w_uk[:, pr * 128:(pr + 1) * 128], kvT_sb[:, :n])
                        o = ob16.get()
                        k.copy(o[:, :n], ps[:, :n], e=("act" if pr % 2 == 0 else "dve"))
                        k.dma(KN.sl((pr, r0), np.s_[pr, :, r0:r0 + n]), o[:, :n])
                    for ti in range(nt):
                        ps = pex.get()
                        k.mm(ps, kvT_sb[:, ti * 128:(ti + 1) * 128], w_uv)
                        o = ob16.get()
                        k.copy(o, ps, e="act")
                        rr = r0 + ti * 128
                        k.dma(VP.sl(rr // 128, np.s_[rr:rr + 128, :]), o)
                    hook(2)
                    for c in range(4):
                        ps = fm(7 + c)
                        o = ob16.get()
                        if st == 1:
                            sf = f32t.get()
                            k.copy(sf[:, :n], ps[:, :n], e="act")
                            rope_apply(sf, 128, n, r128, C("p128"), o)
                        else:
                            k.copy(o[:, :n], ps[:, :n], e="act")
                        dst = (WQT if c < 2 else WKT)
                        k.dma(dst.sl((c % 2, r0), np.s_[c % 2, :, r0:r0 + n]), o[:, :n])
                    ps = fm(11, 32)
                    o = ob16.get()
                    if st == 1:
                        sf = f32t.get()
                        k.copy(sf[0:32, :n], ps[0:32, :n], e="act")
                        rope_apply(sf, 32, n, r32, C("p32", 32), o)
                    else:
                        k.copy(o[0:32, :n], ps[0:32, :n], e="act")
                    k.dma(KRT.sl(r0, np.s_[:, r0:r0 + n]), o[0:32, :n])
                    hook(3)
                    for ti in range(nt):
                        o = tmo.get()
                        for hf in range(2):
                            ps = pfm.get()
                            c0 = 1440 + hf * 512
                            for kc in range(8):
                                k.mm(ps, hT[:, kc, ti * 128:(ti + 1) * 128], w_in[:, kc, c0:c0 + 512], start=(kc == 0), stop=(kc == 7))
                            k.copy(o[:, hf * 512:(hf + 1) * 512], ps, e=("act" if hf == 0 else "dve"))
                        rr = r0 + ti * 128
                        k.dma(TMo.sl(rr // 128, np.s_[rr:rr + 128, :]), o)
                ns0 = norm_begin(blocks[0])
                for ti_ in range(4):
                    norm_ew(ns0, ti_)
                pctx = norm_tr(ns0)
                for bi, blk in enumerate(blocks):
                    nsn = norm_begin(blocks[bi + 1]) if bi + 1 < len(blocks) else None
                    proj_phase(blk, pctx, (lambda i_: norm_ew(nsn, i_)) if nsn is not None else (lambda i_: None))
                    pctx = norm_tr(nsn) if nsn is not None else None
            k.barrier()
            if stop_here("s1_%d" % l):
                break
            es_mla_pre = ExitStack()
            vp2 = k.sb([128, NTILE, 8, 128], BF16, es=es_mla_pre)
            VPv = TD(VP, "(t p) (h d) -> p t h d", p=128, d=64)
            for par in range(2):
                k.memset(vp2[:, :, par::2, (1 - par) * 64:(2 - par) * 64], 1.0, e="pool")
                for h in range(par, 8, 2):
                    k.dma(vp2[:, :, h, par * 64:(par + 1) * 64], VPv[:, :, h, :])
            with ExitStack() as s:
                lg_rep = k.sb([128, 8], F32, es=s)
                lg_pp = k.sb([128, 4], F32, es=s)
                for dst, src in ((lg_rep, small[:, sm0 + 3:sm0 + 11]), (lg_pp, small[:, sm0 + 15:sm0 + 19])):
                    k.act(dst, src, AF.Exp, scale=LN2)
                    k.ts(dst, dst, -1.0, ALU.mult, 1.0, ALU.add)
                    k.act(dst, dst, AF.Ln)
                LN8 = float(np.log(0.125))
                DT = k.sb([128, 4, 128], F32, es=s)
                tmpA = k.sb([128, 128], F32, es=s)
                for h in range(4):
                    k.ts(tmpA, C("A"), lg_rep[:, h:h + 1], ALU.mult)
                    k.stt(tmpA, C("B"), lg_rep[:, 4 + h:5 + h], tmpA, ALU.mult, ALU.add)
                    k.act(DT[:, h, :], tmpA, AF.Exp)
                    k.ts(DT[:, h, :], DT[:, h, :], 0.125, ALU.mult)
                QWF = k.sb([128, 2, 128], F32, es=s)
                QWB = k.sb([128, 2, 128], F32, es=s)
                gch = k.sb([128, 4], F32, es=s)
                for pr in range(2):
                    k.act(QWF[:, pr, :], C("C1"), AF.Exp, scale=lg_pp[:, pr:pr + 1])
                    k.act(QWB[:, pr, :], C("C2"), AF.Exp, scale=lg_pp[:, 2 + pr:3 + pr])
                k.act(gch, lg_pp, AF.Exp, scale=128.0)
                KWF = k.sb([128, 256], F32, es=s)
                KWB = k.sb([128, 256], F32, es=s)
                kcol = k.sb([128, 8], F32, es=s)
                for h in range(4):
                    k.act(kcol[:, h:h + 1], C("colK1"), AF.Exp, scale=lg_rep[:, h:h + 1])
                    k.act(kcol[:, 4 + h:5 + h], C("colK2"), AF.Exp, scale=lg_rep[:, 4 + h:5 + h])
                k.ts(kcol, kcol, 0.125, ALU.mult)
                for h in range(4):
                    k.copy(KWF[:, h * 64:(h + 1) * 64], T(kcol.ap[:, h:h + 1].to_broadcast([128, 64]), kcol.key))
                    k.copy(KWB[:, h * 64:(h + 1) * 64], T(kcol.ap[:, 4 + h:5 + h].to_broadcast([128, 64]), kcol.key))
                KVd = k.sb([128, NTILE, 4, 64], F32, es=s)
                Sfb = k.sb([128, NTILE, 2, 64], BF16, es=s)
                Sbb = k.sb([128, NTILE, 2, 64], BF16, es=s)
                kv_r = Rot([k.sb([128, 512], BF16, es=s) for _ in range(3)])
                kw_r = Rot([k.sb([128, 2, 256], BF16, es=s) for _ in range(2)])
                pkv = Rot([k.ps([128, 4, 128], F32, es=s) for _ in range(2)])
                fwd_order = list(range(NTILE))
                bwd_order = [1, 0] + list(range(NTILE - 1, 1, -1))
                r1_order = []
                for a_, b_ in zip(fwd_order, bwd_order):
                    for t_ in (a_, b_):
                        if t_ not in r1_order:
                            r1_order.append(t_)
                curs = []
                for d_ in range(2):
                    c_ = k.sb([128, 2, 64], F32, es=s)
                    k.memset(c_, 0.0)
                    curs.append(c_)
                ptrs = [0, 0]
                done = set()

                def scan_step(d_, n):
                    Sb_ = Sfb if d_ == 0 else Sbb
                    cur = curs[d_]
                    k.copy(Sb_.sl(n, np.s_[:, n, :, :]), cur)
                    for pr in range(2):
                        k.stt(cur[:, pr, :], cur[:, pr, :], gch[:, d_ * 2 + pr:d_ * 2 + pr + 1],
                              KVd.sl(n, np.s_[:, n, d_ * 2 + pr, :]), ALU.mult, ALU.add)
                for n in r1_order:
                    kvt = kv_r.get()
                    k.dma(kvt, TMo.sl(n, np.s_[n * 128:(n + 1) * 128, 0:512]))
                    kw = kw_r.get()
                    k.tt(kw[:, 0, :], kvt[:, 0:256], KWF, ALU.mult)
                    k.tt(kw[:, 1, :], kvt[:, 0:256], KWB, ALU.mult)
                    ps = pkv.get()
                    for d_ in range(2):
                        for pr in range(2):
                            k.mm(ps[:, d_ * 2 + pr, :], kw[:, d_, pr * 128:(pr + 1) * 128], kvt[:, 256 + pr * 128:256 + (pr + 1) * 128])
                    k.copy(KVd.sl(n, np.s_[0:64, n, :, :]), ps[0:64, :, 0:64], e="act")
                    k.copy(KVd.sl(n, np.s_[64:128, n, :, :]), ps[64:128, :, 64:128], e="act")
                    done.add(n)
                    for d_, order in ((0, fwd_order), (1, bwd_order)):
                        while ptrs[d_] < NTILE and order[ptrs[d_]] in done:
                            scan_step(d_, order[ptrs[d_]])
                            ptrs[d_] += 1
                qk_r = Rot([k.sb([128, 2, 2, 128], BF16, es=s) for _ in range(3)])
                vg_r = Rot([k.sb([128, 512], BF16, es=s) for _ in range(3)])
                qw_r = Rot([k.sb([128, 2, 2, 128], BF16, es=s) for _ in range(2)])
                sm_r = Rot([k.sb([128, 512], BF16, es=s) for _ in range(4)])
                pss = Rot([k.ps([128, 512], F32, es=s) for _ in range(2)] + [T(p_.ap.rearrange("p a b -> p (a b)"), p_.key) for p_ in pkv.t])
                psy = Rot([k.ps([128, 512], F32, es=s) for _ in range(2)])
                ysb_r = Rot([k.sb([128, 256], F32, es=s) for _ in range(3)])
                sq_r = Rot([k.sb([128, 256], F32, es=s) for _ in range(2)])
                st_r = Rot([k.sb([128, 16], F32, es=s) for _ in range(2)])
                yo_r = Rot([k.sb([128, 2, 128], BF16, es=s) for _ in range(2)])
                yb_r = Rot([k.sb([128, 256], BF16, es=s) for _ in range(2)])
                ptb2 = Rot([k.ps([128, 2, 128], BF16, es=s) for _ in range(2)])

                sgall = k.sb([128, NTILE, 256], BF16, es=s)
                for n in range(0 if need_ctx else 2, NTILE):
                    gt_ = vg_r.get()
                    k.dma(gt_[:, 0:256], TMo.sl(n, np.s_[n * 128:(n + 1) * 128, 512:768]))
                    k.act(sgall[:, n, :], gt_[:, 0:256], AF.Silu)

                def r2A(n):
                    c0 = n * 128
                    qk = qk_r.get()
                    k.dma(qk[:, 0], TD(QT, "c p t -> p c t")[:, :, c0:c0 + 128])
                    k.dma(qk[:, 1], TD(KT, "c p t -> p c t")[:, :, c0:c0 + 128])
                    vg = vg_r.get()
                    k.dma(vg, TMo.sl(n, np.s_[c0:c0 + 128, 256:768]))
                    qw = qw_r.get()
                    k.tt(qw[:, 0], qk[:, 0], QWF, ALU.mult)
                    k.tt(qw[:, 1], qk[:, 0], QWB, ALU.mult)
                    py = psy.get()
                    psA = pss.get()
                    psB = pss.get()
                    for h in range(4):
                        pr, off = h // 2, (h % 2) * 64
                        pb = psA if h % 2 == 0 else psB
                        k.mm(pb[:, pr * 128:(pr + 1) * 128], qk[off:off + 64, 1, pr, :], qk[off:off + 64, 0, pr, :])
                    sm = sm_r.get()
                    smv = T(sm.ap.rearrange("p (a c) -> p a c", a=4), sm.key)
                    k.tt(smv[:, 0::2, :], T(psA.ap[:, 0:256].rearrange("p (a c) -> p a c", a=2), psA.key), DT[:, 0::2, :], ALU.mult)
                    k.tt(smv[:, 1::2, :], T(psB.ap[:, 0:256].rearrange("p (a c) -> p a c", a=2), psB.key), DT[:, 1::2, :], ALU.mult)
                    for h in range(4):
                        pr, off = h // 2, (h % 2) * 64
                        yo_ = py[:, h * 64:(h + 1) * 64]
                        k.mm(yo_, sm[:, h * 128:(h + 1) * 128], vg[:, h * 64:(h + 1) * 64], start=True, stop=False)
                        k.mm(yo_, qw[off:off + 64, 0, pr, :], Sfb.sl(n, np.s_[off:off + 64, n, pr, :]), start=False, stop=False)
                        k.mm(yo_, qw[off:off + 64, 1, pr, :], Sbb.sl(n, np.s_[off:off + 64, n, pr, :]), start=False, stop=True)
                    ysb = ysb_r.get()
                    k.copy(ysb, py[:, 0:256], e="act")
                    return (ysb, sgall[:, n, :])

                def r2B(n, ctx_):
                    ysb, sg = ctx_
                    c0 = n * 128
                    stt_ = st_r.get()
                    k.op("dve", lambda e: e.reduce_sum(stt_.ap[:, 0:4], ysb.ap.rearrange("p (h d) -> p h d", h=4), AX.X), reads=[ysb], writes=[stt_])
                    sq = sq_r.get()
                    k.tt(sq, ysb, ysb, ALU.mult)
                    k.op("dve", lambda e: e.reduce_sum(stt_.ap[:, 4:8], sq.ap.rearrange("p (h d) -> p h d", h=4), AX.X), reads=[sq], writes=[stt_])
                    k.ts(stt_[:, 0:8], stt_[:, 0:8], 1.0 / 64, ALU.mult)
                    k.tt(stt_[:, 8:12], stt_[:, 0:4], stt_[:, 0:4], ALU.mult)
                    k.tt(stt_[:, 12:16], stt_[:, 4:8], stt_[:, 8:12], ALU.subtract)
                    k.rsq(stt_[:, 12:16], stt_[:, 12:16], 1.0, 1e-5)
                    for h in range(4):
                        k.ts(ysb[:, h * 64:(h + 1) * 64], ysb[:, h * 64:(h + 1) * 64], stt_[:, h:h + 1], ALU.subtract,
                             stt_[:, 12 + h:13 + h], ALU.mult)
                    yb = yb_r.get()
                    k.tt(yb, ysb, sg, ALU.mult)
                    pt = ptb2.get()
                    for c in range(2):
                        k.tr(pt[:, c, :], yb[:, c * 128:(c + 1) * 128], ident_b)
                    yo = yo_r.get()
                    k.copy(yo, pt, e="act")
                    k.dma(TD(YT, "c p t -> p c t").sl(("ret", n), np.s_[:, 0:2, c0:c0 + 128]), yo, q="pool")
                tiles_r = list(range(0 if need_ctx else 2, NTILE))
                pc_ = r2A(tiles_r[0])
                for ti_, n in enumerate(tiles_r):
                    nc_ = r2A(tiles_r[ti_ + 1]) if ti_ + 1 < len(tiles_r) else None
                    r2B(n, pc_)
                    pc_ = nc_
            k.barrier()
            if stop_here("ret%d" % l):
                es_mla_pre.close()
                break
            with ExitStack() as s:
                kh_l = [k.sb([128, NT], BF16, es=s) for _ in range(2)]
                for t_ in kh_l:
                    k.memset(t_, 0.0, e="dve")
                kh_r = Rot(kh_l)
                q_l = [k.sb([128, 512], BF16, es=s) for _ in range(3)]
                for t_ in q_l:
                    k.memset(t_, 0.0)
                q_r = Rot(q_l)
                rd_sets = []
                for par in range(2):
                    tl = [k.sb([128, 512], F32, es=s) for _ in range(2)]
                    for t_ in tl:
                        k.memset(t_, 0.0)
                    rd_sets.append(Rot(tl))
                pT_r = Rot([k.sb([128, 2, 512], BF16, es=s) for _ in range(5)])
                pss = Rot([k.ps([128, 2, 512], F32, es=s) for _ in range(3)])
                pacc = Rot([k.ps([128, 512], F32, es=s) for _ in range(2)])
                sw_r = Rot([k.sb([128, 512], F32, es=s) for _ in range(2)])
                yp_r = Rot([k.sb([128, 512], BF16, es=s) for _ in range(3)])
                qblocks = ([(0, 256, [0, 1])] if need_ctx else []) + [(256 + 512 * i, 512, list(range(NTILE))) for i in range(8)]
                LOOK = 2
                pend = []

                def emit_pv2(item):
                    h, r0, n, j, npair, kt2, pT, pv = item
                    for u_ in range(2):
                        first = (j == 0 and u_ == 0)
                        last = (j == npair - 1 and u_ == 1)
                        k.mm(pv[:, :n], vp2[:, kt2[u_], h, :], pT[:, u_, :n], start=first, stop=last)
                    if j == npair - 1:
                        off = (h % 2) * 64
                        dof = 64 - off
                        rd = rd_sets[h % 2].get()
                        k.op("dve", lambda e: e.reciprocal(rd.ap[dof:dof + 64, :n], pv.ap[dof:dof + 64, :n]), reads=[pv], writes=[rd])
                        sws = sw_r.get()
                        k.dma(sws[off:off + 64, :n], rd[dof:dof + 64, :n])
                        yp = yp_r.get()
                        k.tt(yp[off:off + 64, :n], pv[off:off + 64, :n], sws[off:off + 64, :n], ALU.mult)
                        k.dma(YT.sl(("mla", h, r0), np.s_[2 + h // 2, off:off + 64, r0:r0 + n]), yp[off:off + 64, :n], q="pool")
                for h in range(8):
                    off = (h % 2) * 64
                    kh = kh_r.get()
                    k.dma(kh[0:64, :], KN[h // 2, off:off + 64, :])
                    k.dma(kh[64:96, :], KRT)
                    for (r0, n, kts) in qblocks:
                        q = q_r.get()
                        k.dma(q[0:64, :n], QN[h // 2, off:off + 64, r0:r0 + n])
                        k.dma(q[64:96, :n], QR[h, :, r0:r0 + n])
                        pv = pacc.get()
                        npair = len(kts) // 2
                        for j in range(npair):
                            kt2 = (kts[2 * j], kts[2 * j + 1])
                            ps = pss.get()
                            for u_ in range(2):
                                k.mm(ps[:, u_, :n], kh[:, kt2[u_] * 128:(kt2[u_] + 1) * 128], q[:, :n])
                            pT = pT_r.get()
                            k.act(pT[:, :, :n], ps[:, :, :n], AF.Exp, scale=SCALE_MLA)
                            pend.append((h, r0, n, j, npair, kt2, pT, pv))
                            if len(pend) > LOOK:
                                emit_pv2(pend.pop(0))
                while pend:
                    emit_pv2(pend.pop(0))
            k.barrier()
            es_mla_pre.close()
            if stop_here("mla%d" % l):
                break
            with ExitStack() as s:
                wkT = k.sb([128, 2, 2, NT], BF16, es=s)
                k.memset(wkT, 0.0, e="dve")
                for hk_ in range(2):
                    for g_ in range(2):
                        k.dma(wkT[g_ * 64:(g_ + 1) * 64, hk_, g_, :], WKT[hk_, g_ * 64:(g_ + 1) * 64, :])
                wqT = k.sb([128, 2, NT], BF16, es=s)
                k.dma(wqT, TD(WQT, "c p t -> p c t"))
                wv3 = k.sb([128, NTILE, 4, 128], BF16, es=s)
                WVv = T(TMo.ap[:, 768:1024].rearrange("(t p) (q d) -> p t q d", p=128, d=64), TMo.key)
                for par in range(2):
                    k.memset(wv3[:, :, par::2, (1 - par) * 64:(2 - par) * 64], 1.0, e="dve")
                    for q_ in range(par, 4, 2):
                        k.dma(wv3[:, :, q_, par * 64:(par + 1) * 64], WVv[:, :, q_, :])
                esink = k.sb([128, 4], F32, es=s)
                k.act(esink, small[:, sm0 + 11:sm0 + 15], AF.Exp)
                rd_sets = []
                for par in range(2):
                    tl = [k.sb([128, 128], F32, es=s) for _ in range(2)]
                    for t_ in tl:
                        k.memset(t_, 0.0)
                    rd_sets.append(Rot(tl))
                pT_r = Rot([k.sb([128, 5, 128], BF16, es=s) for _ in range(3)])
                pss = Rot([k.ps([128, 512], F32, es=s) for _ in range(4)])
                pacc = Rot([k.ps([128, 512], F32, es=s) for _ in range(4)])
                sw_r = Rot([k.sb([128, 128], F32, es=s) for _ in range(4)])
                yp_r = Rot([k.sb([128, 128], BF16, es=s) for _ in range(4)])
                pend = []

                def emit_pvw(item):
                    n, qh, keys, pT, pv, yp = item
                    hk, g = qh // 2, qh % 2
                    off = g * 64
                    dof = 64 - off
                    c0 = n * 128
                    nk = len(keys)
                    for i, (kt, msk) in enumerate(keys):
                        k.mm(pv[:, 0:128], wv3[:, kt, qh, :], pT[:, i, :], start=(i == 0), stop=(i == nk - 1))
                    rd = rd_sets[g].get()
                    k.ts(rd[dof:dof + 64, :], pv[dof:dof + 64, 0:128], esink[dof:dof + 64, qh:qh + 1], ALU.add)
                    k.op("dve", lambda e: e.reciprocal(rd.ap[dof:dof + 64, :], rd.ap[dof:dof + 64, :]), reads=[rd], writes=[rd])
                    sws = sw_r.get()
                    k.dma(sws[off:off + 64, :], rd[dof:dof + 64, :])
                    pendF.append((n, qh, pv, yp, sws))
                    if len(pendF) > 1:
                        emit_fin(pendF.pop(0))

                def emit_fin(item):
                    n, qh, pv, yp, sws = item
                    hk, g = qh // 2, qh % 2
                    off = g * 64
                    c0 = n * 128
                    k.tt(yp[off:off + 64, :], pv[off:off + 64, 0:128], sws[off:off + 64, :], ALU.mult)
                    if g == 1:
                        k.dma(YT.sl(("win", hk, n), np.s_[6 + hk, :, c0:c0 + 128]), yp, q="pool")
                pendF = []
                yp = None
                for n in range(0 if need_ctx else 2, NTILE):
                    c0 = n * 128
                    if n < 2:
                        keys = [(0, None), (1, None)]
                    else:
                        keys = []
                        if n - 1 >= 2:
                            keys.append((n - 1, mprev_b))
                        keys.append((n, None))
                        if n + 1 < NTILE:
                            keys.append((n + 1, mnext_b))
                        keys += [(0, None), (1, None)]
                    for qh in range(4):
                        hk, g = qh // 2, qh % 2
                        pv = pacc.get()
                        if g == 0:
                            yp = yp_r.get()
                        psA = pss.get()
                        psB = pss.get() if len(keys) > 4 else None
                        for i, (kt, msk) in enumerate(keys):
                            dst = psA[:, i * 128:(i + 1) * 128] if i < 4 else psB[:, 0:128]
                            k.mm(dst, wkT[:, hk, g, kt * 128:(kt + 1) * 128], wqT[:, hk, c0:c0 + 128])
                        pT = pT_r.get()
                        na = min(4, len(keys))
                        k.act(T(pT.ap[:, 0:na, :].rearrange("p a c -> p (a c)"), pT.key), psA[:, 0:na * 128], AF.Exp, scale=0.125)
                        if psB is not None:
                            k.act(pT[:, 4, :], psB[:, 0:128], AF.Exp, scale=0.125)
                        for i, (kt, msk) in enumerate(keys):
                            if msk is not None:
                                k.tt(pT[:, i, :], pT[:, i, :], msk, ALU.mult)
                        pend.append((n, qh, keys, pT, pv, yp))
                        if len(pend) > 1:
                            emit_pvw(pend.pop(0))
                while pend:
                    emit_pvw(pend.pop(0))
                while pendF:
                    emit_fin(pendF.pop(0))
            k.barrier()
            if stop_here("win%d" % l):
                break
            es_aff = ExitStack()
            aff_e = k.sb([16, NT], F32, "aff_e", es=es_aff)
            with ExitStack() as s:
                w_out = k.sb([128, 8, D], BF16, es=s)
                stg = Rot([k.sb([128, D], F32, es=s) for _ in range(2)])
                for kc in range(8):
                    load_cast(w_out[:, kc, :], w_out_d[l, kc * 128:(kc + 1) * 128, :], stg, e=("pool" if kc % 2 else "dve"))
                rw = k.sb([128, 8, 16], F32, es=s)
                k.dma(rw, TD(router_d[l], "(kc p) e -> p kc e", p=128))
                sts = [0, 1] if need_ctx else [1]
                mod2 = {st: load_mod(st, 2, s) for st in sts}
                gs2 = {st: load_gs(st, 4, l * 2 + 1, s) for st in sts}
                sh2 = {st: load_mod(st, 3, s) for st in sts}
                yT_r = Rot([k.sb([128, 8, 128], BF16, es=s) for _ in range(3)])
                xt_r = Rot([k.sb([128, D], F32, es=s) for _ in range(3)])
                xn_r = Rot([k.sb([128, D], F32, es=s) for _ in range(4)])
                xs_r = Rot([k.sb([128, D], F32, es=s) for _ in range(3)])
                h2b_r = Rot([k.sb([128, D], BF16, es=s) for _ in range(2)])
                h2T_r = Rot([k.sb([128, 8, 128], F32, es=s) for _ in range(3)])
                junk = k.sb([128, D], F32, es=s)
                ss_pool = Rot([k.sb([128, 1], F32, es=s) for _ in range(16)])
                lgall = k.sb([128, NTILE, 16], F32, es=s)
                pso = Rot([k.ps([128, 512], F32, es=s) for _ in range(3)])
                ptr = Rot([k.ps([128, 4, 128], F32, es=s) for _ in range(2)])
                psl = Rot([k.ps([128, 512], F32, es=s) for _ in range(2)])
                def oA(n):
                    st = 0 if n < 2 else 1
                    c0 = n * 128
                    yT = yT_r.get()
                    k.dma(yT, TD(YT, "c p t -> p c t")[:, :, c0:c0 + 128])
                    xt = xt_r.get()
                    k.dma(xt, rows_key(Xsrc, n)[c0:c0 + 128, :])
                    xn = xn_r.get()
                    for hf in range(2):
                        ps = pso.get()
                        for kc in range(8):
                            k.mm(ps, yT[:, kc, :], w_out[:, kc, hf * 512:(hf + 1) * 512], start=(kc == 0), stop=(kc == 7))
                        sl_ = np.s_[:, hf * 512:(hf + 1) * 512]
                        k.tt(xn[sl_], ps, mod2[st][sl_], ALU.mult)
                        k.tt(xn[sl_], xn[sl_], xt[sl_], ALU.add)
                    k.dma(rows_key(X, n)[c0:c0 + 128, :], xn, q="pool")
                    ss = ss_pool.get()
                    k.act(junk, xn, AF.Square, accum=ss)
                    return (xn, ss)

                def oB(n, c_):
                    xn, ss = c_
                    st = 0 if n < 2 else 1
                    c0 = n * 128
                    rstd = ss_pool.get()
                    k.rsq(rstd, ss, 1.0 / D, EPS)
                    xs = xs_r.get()
                    k.stt(xs, xn, rstd, gs2[st], ALU.mult, ALU.mult)
                    k.tt(xs, xs, sh2[st], ALU.add)
                    h2b = h2b_r.get()
                    k.copy(h2b, xs, e="act")
                    k.dma(H2.sl(n, np.s_[c0:c0 + 128, :]), h2b, q="pool")
                    h2T = h2T_r.get()
                    for hf in range(2):
                        pt = ptr.get()
                        for q4 in range(4):
                            kc = hf * 4 + q4
                            k.tr(pt[:, q4, :], xs[:, kc * 128:(kc + 1) * 128], ident)
                        k.copy(h2T[:, hf * 4:(hf + 1) * 4, :], pt, e="act")
                    return h2T

                def oC(n, h2T):
                    c0 = n * 128
                    pl = psl.get()
                    for kc in range(8):
                        k.mm(pl[:, 0:16], h2T[:, kc, :], rw[:, kc, :], start=(kc == 0), stop=(kc == 7))
                    k.copy(lgall.sl(n, np.s_[:, n, :]), pl[:, 0:16])
                tiles_o = list(range(0 if need_ctx else 2, NTILE))
                NO = len(tiles_o)
                ca = {}
                cb = {}
                for step in range(NO + 2):
                    if step < NO:
                        ca[step] = oA(tiles_o[step])
                    if 0 <= step - 1 < NO:
                        cb[step - 1] = oB(tiles_o[step - 1], ca.pop(step - 1))
                    if 0 <= step - 2 < NO:
                        oC(tiles_o[step - 2], cb.pop(step - 2))
                n0_ = tiles_o[0]
                lgv = T(lgall.ap[:, n0_:NTILE, :], lgall.key)
                exall = k.sb([128, NTILE, 16], F32, es=s)
                exv = T(exall.ap[:, n0_:NTILE, :], exall.key)
                k.act(exv, lgv, AF.Exp, reads_extra=[lgall.sub(n) for n in tiles_o]) if False else k.op(
                    "act", lambda e: e.activation(exv.ap, lgv.ap, AF.Exp), reads=[lgall.sub(n) for n in tiles_o], writes=[exall])
                small_ = k.sb([128, NTILE], F32, es=s)
                k.op("dve", lambda e: e.reduce_sum(small_.ap[:, n0_:NTILE], exv.ap, AX.X), reads=[exall], writes=[small_])
                k.op("dve", lambda e: e.reciprocal(small_.ap[:, n0_:NTILE], small_.ap[:, n0_:NTILE]), reads=[small_], writes=[small_])
                for n in tiles_o:
                    c0 = n * 128
                    k.ts(affT.sl(n, np.s_[:, n, :]), exall[:, n, :], small_[:, n:n + 1], ALU.mult)
                    pl2 = psl.get()
                    k.tr(pl2[0:16, 0:128], affT.sl(n, np.s_[:, n, :]), ident)
                    k.copy(aff_e.sl(n, np.s_[:, c0:c0 + 128]), pl2[0:16, 0:128], e="act")
            k.barrier()
            if stop_here("o%d" % l):
                es_aff.close()
                break
            streams = ([(0, 2, 32)] if need_ctx else []) + [(2, 32, 512)]
            with ExitStack() as s:
                work = k.sb([16, NTX], F32, es=s)
                m8 = k.sb([16, 8], F32, es=s)
                thr = k.sb([16, 1], F32, es=s)
                aff128 = k.sb([128, 512], F32, es=s)
                junk16 = k.sb([128, 512], BF16, es=s)
                lo = k.sb([128, 1], F32, es=s)
                hi = k.sb([128, 1], F32, es=s)
                mid = k.sb([128, 1], F32, es=s)
                cnt = k.sb([128, 1], F32, es=s)
                half = k.sb([128, 1], F32, es=s)
                k.memset(half, 0.5)
                mge = k.sb([128, 1], U32, es=s)
                mlt = k.sb([128, 1], U32, es=s)
                thr16 = k.sb([16, 8], F32, es=s)
                mask_e = k.sb([16, NTX], F32, es=s)
                maskTb = k.sb([128, 32, 16], BF16, es=s)
                carry = k.sb([128, 16], F32, es=s)
                pos_r = Rot([k.sb([128, 16], F32, es=s) for _ in range(2)])
                ptk = Rot([k.ps([128, 512], F32, es=s) for _ in range(4)])
                ahi = k.sb([128, NTILE, 16], BF16, es=s)
                alo = k.sb([128, NTILE, 16], F32, es=s)
                k.copy(ahi, affT)
                k.tt(alo, affT, ahi, ALU.subtract)
                k.copy(Rtab[:, :, :, 2], ahi, e="pool")
                k.copy(Rtab[:, :, :, 3], alo, e="pool")
                for (t0, ntl, cap) in streams:
                    ntok = ntl * 128
                    cs = np.s_[:, t0 * 128:t0 * 128 + ntok]
                    if cap <= 64:
                        k.copy(work[:, :ntok], aff_e[cs])
                        for r in range(cap // 8):
                            k.op("dve", lambda e: e.max(out=m8.ap, in_=work.ap[:, :ntok]), reads=[work], writes=[m8])
                            if r < cap // 8 - 1:
                                k.op("dve", lambda e: e.match_replace(out=work.ap[:, :ntok], in_to_replace=m8.ap,
                                                                      in_values=work.ap[:, :ntok], imm_value=-1.0),
                                     reads=[work, m8], writes=[work])
                        k.copy(thr, m8[:, 7:8])
                    else:
                        k.dma(AFFD, aff_e[cs])
                        k.dma(aff128, TD(AFFD, "e (s t) -> (e s) t", s=8))
                        k.memset(lo, 0.0)
                        k.memset(hi, 2.0)
                        for it in range(40):
                            k.stt(mid, lo, hi, half, ALU.add, ALU.mult)
                            k.ts(junk16, aff128, mid, ALU.is_ge, 0.0, ALU.add, accum=cnt)
                            pc = ptk.get()
                            k.mm(pc[:, 0:1], C("bmat"), cnt)
                            k.ts(mge, pc[:, 0:1], cap - 0.5, ALU.is_ge)
                            k.ts(mlt, pc[:, 0:1], cap - 0.5, ALU.is_lt)
                            k.op("dve", lambda e: e.copy_predicated(lo.ap, mge.ap, mid.ap), reads=[mge, mid], writes=[lo])
                            k.op("dve", lambda e: e.copy_predicated(hi.ap, mlt.ap, mid.ap), reads=[mlt, mid], writes=[hi])
                        k.dma(THRD, lo)
                        k.dma(thr16, TD(THRD, "(e s) o -> e (s o)", s=8))
                        k.copy(thr, thr16[:, 0:1])
                    k.ts(mask_e[:, :ntok], aff_e[cs], thr, ALU.is_ge)
                    k.memset(carry, 0.0)
                    for i in range(ntl):
                        n = t0 + i
                        pt = ptk.get()
                        k.tr(pt[:, 0:16], mask_e[:, i * 128:(i + 1) * 128], ident[0:16, 0:16])
                        k.copy(maskTb[:, i, :], pt[:, 0:16], e="act")
                        pc = ptk.get()
                        k.mm(pc[:, 0:16], triU_b, maskTb[:, i, :])
                        k.mm(pc[:, 16:32], ones_b, maskTb[:, i, :])
                        pos = pos_r.get()
                        k.tt(pos, pc[:, 0:16], carry, ALU.add)
                        k.tt(pos, pos, maskTb[:, i, :], ALU.mult)
                        k.ts(posm.sl(n, np.s_[:, n, :]), pos, -1.0, ALU.add)
                        k.tt(carry, carry, pc[:, 16:32], ALU.add)
            k.barrier()
            if stop_here("topk%d" % l):
                es_aff.close()
                break
            es_aff.close()
            with ExitStack() as s:
                mod5 = {st: load_mod(st, 5, s) for st in ([0, 1] if need_ctx else [1])}
                wsets = Rot([(k.sb([128, 8, 768], BF16, es=s), k.sb([128, 8, 768], BF16, es=s), k.sb([128, 6, D], BF16, es=s))
                             for _ in range(2)])
                stg = Rot([k.sb([128, 768], F32, es=s) for _ in range(4)])
                Sel = k.sb([128, 32, 512], BF16, es=s)
                iota16 = k.sb([128, 512], mybir.dt.int16, es=s)
                k.copy(iota16, C("iota"))
                r4T_r = Rot([k.sb([4, 512], F32, es=s) for _ in range(2)])
                r4_r = Rot([k.sb([128, 16], F32, es=s) for _ in range(3)])
                idx_r = Rot([k.sb([128, 1], I32, es=s) for _ in range(16)])
                idf_r = Rot([k.sb([128, 1], F32, es=s) for _ in range(4)])
                gt_r = Rot([k.sb([128, 1], F32, es=s) for _ in range(16)])
                xs_r = Rot([k.sb([128, D], BF16, es=s) for _ in range(10)])
                xsT_r = Rot([k.sb([128, 8, 512], BF16, es=s) for _ in range(2)])
                hid = k.sb([128, 6, 512], BF16, es=s)
                sg_r = Rot([k.sb([128, 512], F32, es=s) for _ in range(2)])
                ys_r = Rot([k.sb([128, D], F32, es=s) for _ in range(2)])
                p4 = Rot([k.ps([128, 512], F32, es=s) for _ in range(1)])
                ptb = Rot([k.ps([128, 8, 128], BF16, es=s) for _ in range(1)])
                pgu = Rot([k.ps([128, 512], F32, es=s) for _ in range(4)])
                pdn = Rot([k.ps([128, 512], F32, es=s) for _ in range(2)])
                Xall = [rows_key(X, n) for n in range(NTILE)] + [X.sub("all")]
                cast_eng = Rot(["act", "dve"])
                wcache = {}

                wgen = {"g": None}

                def weights_gen(e_, wg, wu, wd):
                    for kc in range(8):
                        load_cast(wg[:, kc, :], wg_d[l, e_, kc * 128:(kc + 1) * 128, :], stg, e=cast_eng.get())
                        yield
                        load_cast(wu[:, kc, :], wu_d[l, e_, kc * 128:(kc + 1) * 128, :], stg, e=cast_eng.get())
                        yield
                    for fc in range(6):
                        for hf_ in range(2):
                            load_cast(wd[:, fc, hf_ * 512:(hf_ + 1) * 512], wd_d[l, e_, fc * 128:(fc + 1) * 128, hf_ * 512:(hf_ + 1) * 512], stg, e=cast_eng.get())
                            yield

                def feed(n_):
                    for _ in range(n_):
                        if wgen["g"] is None:
                            return
                        try:
                            next(wgen["g"])
                        except StopIteration:
                            wgen["g"] = None

                def weights(e_, now=False):
                    feed(10000)
                    wg, wu, wd = wsets.get()
                    wcache[e_] = (wg, wu, wd)
                    wgen["g"] = weights_gen(e_, wg, wu, wd)
                    if now:
                        feed(10000)

                sgen = {"g": None}

                def sel_gen(u):
                    e_, (t0, ntl, cap) = u
                    for i in range(ntl):
                        k.ts(Sel[:, i, :cap], iota16[:, :cap], posm[:, t0 + i, e_:e_ + 1], ALU.is_equal)
                        yield

                def sfeed(n_):
                    for _ in range(n_):
                        if sgen["g"] is None:
                            return
                        try:
                            next(sgen["g"])
                        except StopIteration:
                            sgen["g"] = None

                def selbuild(u, now=True):
                    sfeed(10000)
                    sgen["g"] = sel_gen(u)
                    if now:
                        sfeed(10000)

                def idxpart(u):
                    e_, (t0, ntl, cap) = u
                    ps = p4.get()
                    for i in range(ntl):
                        k.mm(ps[0:4, :cap], Rtab[:, t0 + i, e_, :], Sel[:, i, :cap], start=(i == 0), stop=(i == ntl - 1))
                    r4T = r4T_r.get()
                    k.copy(r4T[:, :cap], ps[0:4, :cap])
                    stiles = [(s0, min(128, cap - s0)) for s0 in range(0, cap, 128)]
                    for si, (s0, nsl) in enumerate(stiles):
                        k.tr(ps[0:nsl, si * 4:(si + 1) * 4], r4T[0:4, s0:s0 + nsl], ident[0:4, 0:4])
                    nsl0 = stiles[0][1]
                    r4 = r4_r.get()
                    k.copy(r4[0:nsl0, 0:4 * len(stiles)], ps[0:nsl0, 0:4 * len(stiles)])
                    meta = []
                    xss = []
                    for si, (s0, nsl) in enumerate(stiles):
                        c4 = si * 4
                        idf = idf_r.get()
                        k.stt(idf[0:nsl, :], r4[0:nsl, c4:c4 + 1], 64.0, r4[0:nsl, c4 + 1:c4 + 2], ALU.mult, ALU.add)
                        idx = idx_r.get()
                        k.copy(idx[0:nsl, :], idf[0:nsl, :])
                        gt = gt_r.get()
                        k.tt(gt[0:nsl, :], r4[0:nsl, c4 + 2:c4 + 3], r4[0:nsl, c4 + 3:c4 + 4], ALU.add)
                        meta.append((idx, gt))
                        xs = xs_r.get()
                        k.dma(xs[0:nsl, :], H2, q="pool", reads=[idx],
                              fn=lambda en: en.indirect_dma_start(out=xs.ap[0:nsl, :], out_offset=None, in_=H2.ap,
                                                                  in_offset=bass.IndirectOffsetOnAxis(ap=idx.ap[0:nsl, :], axis=0)))
                        xss.append(xs)
                    return [u, stiles, meta, xss, None]

                def xtrans(item):
                    u, stiles, meta, xss, _ = item
                    xsT = xsT_r.get()
                    for si, (s0, nsl) in enumerate(stiles):
                        xs = xss[si]
                        pt = ptb.get()
                        for kc in range(8):
                            k.tr(pt[:, kc, 0:nsl], xs[0:nsl, kc * 128:(kc + 1) * 128], ident_b[0:nsl, 0:nsl])
                        k.copy(xsT[:, :, s0:s0 + nsl], pt[:, :, 0:nsl], e="act")
                    item[4] = xsT

                def ffn(item):
                    (e_, (t0, ntl, cap)), stiles, meta, xss, xsT = item
                    wg, wu, wd = wcache[e_]
                    for fc in range(6):
                        pg = pgu.get()
                        pu = pgu.get()
                        for kc in range(8):
                            k.mm(pg[:, :cap], wg[:, kc, fc * 128:(fc + 1) * 128], xsT[:, kc, :cap], start=(kc == 0), stop=(kc == 7))
                        for kc in range(8):
                            k.mm(pu[:, :cap], wu[:, kc, fc * 128:(fc + 1) * 128], xsT[:, kc, :cap], start=(kc == 0), stop=(kc == 7))
                        sg = sg_r.get()
                        k.act(sg[:, :cap], pg[:, :cap], AF.Silu)
                        k.tt(hid[:, fc, :cap], sg[:, :cap], pu[:, :cap], ALU.mult)
                        feed(2)
                        sfeed(6)

                def down(item):
                    (e_, (t0, ntl, cap)), stiles, meta, xss, xsT = item
                    st = 0 if t0 == 0 else 1
                    wg, wu, wd = wcache[e_]
                    for si, (s0, nsl) in enumerate(stiles):
                        idx, gt = meta[si]
                        ys = ys_r.get()
                        for hf in range(2):
                            pd = pdn.get()
                            for fc in range(6):
                                k.mm(pd[0:nsl, :], hid[:, fc, s0:s0 + nsl], wd[:, fc, hf * 512:(hf + 1) * 512], start=(fc == 0), stop=(fc == 5))
                            k.stt(ys[0:nsl, hf * 512:(hf + 1) * 512], pd[0:nsl, :], gt[0:nsl, 0:1],
                                  mod5[st][0:nsl, hf * 512:(hf + 1) * 512], ALU.mult, ALU.mult)
                            feed(2)
                        k.dma(X, ys[0:nsl, :], q="pool", reads=[ys, idx], writes=Xall,
                              fn=lambda en: en.indirect_dma_start(out=X.ap, out_offset=bass.IndirectOffsetOnAxis(ap=idx.ap[0:nsl, :], axis=0),
                                                                  in_=ys.ap[0:nsl, :], in_offset=None, compute_op=ALU.add))

                units = [(e_, stm) for e_ in range(16) for stm in reversed(streams)]
                NU = len(units)
                weights(0, now=True)
                weights(1, now=True)
                nxt_w = 2
                selbuild(units[0])
                items = {0: idxpart(units[0])}
                if NU > 1:
                    selbuild(units[1])
                    items[1] = idxpart(units[1])
                xtrans(items[0])
                if NU > 2:
                    selbuild(units[2])
                for ui in range(NU):
                    ffn(items[ui])
                    if ui + 1 < NU:
                        xtrans(items[ui + 1])
                    if ui + 2 < NU:
                        sfeed(10000)
                        items[ui + 2] = idxpart(units[ui + 2])
                    down(items[ui])
                    if ui + 3 < NU:
                        selbuild(units[ui + 3], now=False)
                    e_done = units[ui][0]
                    if (ui + 1 == NU or units[ui + 1][0] != e_done) and nxt_w < 16:
                        weights(nxt_w)
                        nxt_w += 1
                    del items[ui]
            k.barrier()
            if stop_here("exp%d" % l):
                break
        else:
            with ExitStack() as s:
                gfin = k.sb([128, D], F32, es=s)
                k.dma(gfin, g_bc_d[4])
                xt_r = Rot([k.sb([128, D], F32, es=s) for _ in range(5)])
                junk = k.sb([128, D], F32, es=s)
                ss_pool = Rot([k.sb([128, 1], F32, es=s) for _ in range(12)])
                def finA(n):
                    c0 = n * 128
                    xt = xt_r.get()
                    k.dma(xt, rows_key(X, n)[c0:c0 + 128, :])
                    ss = ss_pool.get()
                    k.act(junk, xt, AF.Square, accum=ss)
                    return (xt, ss)

                def finB(n, c_):
                    xt, ss = c_
                    c0 = n * 128
                    rstd = ss_pool.get()
                    k.rsq(rstd, ss, 1.0 / D, EPS)
                    k.stt(xt, xt, rstd, gfin, ALU.mult, ALU.mult)
                    k.dma(out_d.sl(n, np.s_[c0 - NTC:c0 - NTC + 128, :]), xt, q="pool")
                tl_ = list(range(2, NTILE))
                q_ = [finA(tl_[0]), finA(tl_[1])]
                for i_, n in enumerate(tl_):
                    if i_ + 2 < len(tl_):
                        q_.append(finA(tl_[i_ + 2]))
                    finB(n, q_.pop(0))
        k.barrier()
        print("ninst", k.ninst, k.cnt, flush=True)
    return nc


_NC_CACHE = {}


def kernel(**inputs):
    inp = {kk: np.asarray(v) for kk, v in inputs.items()}
    shared = _prep_shared(inp)
    if "nc" not in _NC_CACHE:
        _NC_CACHE["nc"] = build()
    nc = _NC_CACHE["nc"]
    in_maps = []
    for b in range(8):
        m = dict(shared)
        m.update(_prep_core(inp, b))
        in_maps.append(m)
    res = run_bass_kernel_spmd(nc, in_maps, core_ids=list(range(8)))
    out = np.stack([np.asarray(r["out"], dtype=np.float32) for r in res.results], 0)
    return out
```
